# Optimizing a Trainium2 kernel written in Bass

```python
import math
import jax, jax.numpy as jnp
from jax import lax
import numpy as np

D_MODEL = 1024
BATCH = 2
SEQ = 8192
DEPTH = 4

CTX_LEN = 256
GRID_W = 64
EPS = 1e-6
NEG_INF = -1e30

NA_HEADS = 4
NA_HEAD_DIM = D_MODEL // 16
NA_WIDTH = NA_HEADS * NA_HEAD_DIM
NA_WIN_ROWS = 8
NA_WIN_COLS = 16

RG_WIDTH = D_MODEL // 2
RG_BLOCKS = 8
RG_BLOCK_W = RG_WIDTH // RG_BLOCKS
RG_CONV_W = 4
RG_CONV_LEFT = 2
RG_C = 8.0

DIFF_HEADS = 4
DIFF_HEAD_DIM = D_MODEL // 32
DIFF_V_DIM = 2 * DIFF_HEAD_DIM
DIFF_WIDTH = DIFF_HEADS * DIFF_V_DIM
Q_BLOCK = 128
ROPE_BASE = 10000.0

MIX_WIDTH = NA_WIDTH + RG_WIDTH + DIFF_WIDTH
IN_SPLIT_SIZES = (NA_WIDTH, NA_WIDTH, NA_WIDTH, RG_WIDTH, RG_WIDTH, DIFF_WIDTH, DIFF_WIDTH, DIFF_WIDTH)
IN_COLS = sum(IN_SPLIT_SIZES)
IN_SPLIT_POINTS = tuple(sum(IN_SPLIT_SIZES[:i + 1]) for i in range(len(IN_SPLIT_SIZES) - 1))

N_GROUPS = 4
EXP_PER_GROUP = 8
N_EXPERTS = N_GROUPS * EXP_PER_GROUP
MOE_TOP_K = 2
D_EXPERT = D_MODEL // 2
MOE_BLOCK = 128

kernel_name = 'hybrid_flow_natten_rglru_diffattn_hmoe'


def rmsnorm(x, g):
    xf = x.astype(jnp.float32)
    y = xf * lax.rsqrt(jnp.mean(xf * xf, axis=-1, keepdims=True) + EPS)
    return (y * g.astype(jnp.float32)).astype(x.dtype)


def axial_rope_tables(n):
    t = jnp.arange(n)
    row = (t // GRID_W).astype(jnp.float32)
    col = (t % GRID_W).astype(jnp.float32)
    half = DIFF_HEAD_DIM // 2
    inv_freq = ROPE_BASE ** (-jnp.arange(0, half, 2, dtype=jnp.float32) / half)
    ang_r = row[:, None] * inv_freq
    ang_c = col[:, None] * inv_freq
    return jnp.cos(ang_r), jnp.sin(ang_r), jnp.cos(ang_c), jnp.sin(ang_c)


def _rotate(x, cos, sin):
    cos = cos[:, None, None, :].astype(x.dtype)
    sin = sin[:, None, None, :].astype(x.dtype)
    x1, x2 = jnp.split(x, 2, axis=-1)
    return jnp.concatenate([x1 * cos - x2 * sin, x1 * sin + x2 * cos], axis=-1)


def apply_axial_rope(x, tables):
    cos_r, sin_r, cos_c, sin_c = tables
    half = x.shape[-1] // 2
    return jnp.concatenate([_rotate(x[..., :half], cos_r, sin_r),
                            _rotate(x[..., half:], cos_c, sin_c)], axis=-1)


def neighbourhood_attention(q, k, v, k_ctx, v_ctx, rpb):
    B, S, _ = q.shape
    rows = S // GRID_W
    wr = min(NA_WIN_ROWS, rows)
    n_loc = wr * GRID_W
    grid = lambda t: t.reshape(B, rows, GRID_W, NA_HEADS, NA_HEAD_DIM)
    qg, kg, vg = grid(q), grid(k), grid(v)
    r = jnp.arange(rows)
    row_idx = jnp.clip(r - wr // 2, 0, rows - wr)[:, None] + jnp.arange(wr)
    kb = kg[:, row_idx].reshape(B, rows, n_loc, NA_HEADS, NA_HEAD_DIM)
    vb = vg[:, row_idx].reshape(B, rows, n_loc, NA_HEADS, NA_HEAD_DIM)
    col = jnp.arange(GRID_W)
    col_start = jnp.clip(col - NA_WIN_COLS // 2, 0, GRID_W - NA_WIN_COLS)
    kcol = col[None, :]
    in_win = (kcol >= col_start[:, None]) & (kcol < col_start[:, None] + NA_WIN_COLS)
    dc = jnp.clip(kcol - col[:, None], 1 - NA_WIN_COLS, NA_WIN_COLS - 1)
    dr = row_idx - r[:, None]
    bias = rpb[:, dr[:, None, :, None] + (NA_WIN_ROWS - 1), dc[None, :, None, :] + (NA_WIN_COLS - 1)]
    bias = bias.reshape(NA_HEADS, rows, GRID_W, n_loc).astype(jnp.float32)
    mask = jnp.broadcast_to(in_win[:, None, :], (GRID_W, wr, GRID_W)).reshape(GRID_W, n_loc)
    scale = NA_HEAD_DIM ** -0.5
    s_loc = jnp.einsum('brqhd,brkhd->bhrqk', qg, kb).astype(jnp.float32) * scale
    s_loc = jnp.where(mask, s_loc + bias, NEG_INF)
    s_ctx = jnp.einsum('brqhd,bkhd->bhrqk', qg, k_ctx).astype(jnp.float32) * scale
    p = jax.nn.softmax(jnp.concatenate([s_loc, s_ctx], axis=-1), axis=-1).astype(v.dtype)
    o = (jnp.einsum('bhrqk,brkhd->brqhd', p[..., :n_loc], vb)
         + jnp.einsum('bhrqk,bkhd->brqhd', p[..., n_loc:], v_ctx))
    return o.reshape(B, S, NA_WIDTH)


def ctx_attention(q, k, v):
    s = jnp.einsum('bqhd,bkhd->bhqk', q, k).astype(jnp.float32) * (q.shape[-1] ** -0.5)
    p = jax.nn.softmax(s, axis=-1).astype(v.dtype)
    return jnp.einsum('bhqk,bkhd->bqhd', p, v)


def short_conv(x, w, b):
    T = x.shape[1]
    xp = jnp.pad(x, ((0, 0), (RG_CONV_LEFT, RG_CONV_W - 1 - RG_CONV_LEFT), (0, 0)))
    return sum(xp[:, j:j + T] * w[j] for j in range(RG_CONV_W)) + b


def rglru_coeffs(xcv, w_r, b_r, w_i, b_i, lam):
    B, T, C = xcv.shape
    xh = xcv.reshape(B, T, RG_BLOCKS, RG_BLOCK_W)
    r = jax.nn.sigmoid((jnp.einsum('btnc,ncd->btnd', xh, w_r).reshape(B, T, C) + b_r).astype(jnp.float32))
    i = jax.nn.sigmoid((jnp.einsum('btnc,ncd->btnd', xh, w_i).reshape(B, T, C) + b_i).astype(jnp.float32))
    log_a = -RG_C * r * jax.nn.softplus(-lam.astype(jnp.float32))
    a = jnp.exp(log_a)
    bx = jnp.sqrt(-jnp.expm1(2.0 * log_a)) * (i * xcv.astype(jnp.float32))
    return a, bx


def _scan_combine(left, right):
    a_l, b_l = left
    a_r, b_r = right
    return a_l * a_r, a_r * b_l + b_r


def linear_scan(a, bx, h0):
    a_cum, h = lax.associative_scan(_scan_combine, (a, bx), axis=1)
    return h + a_cum * h0[:, None, :]


def rglru_bidir(xr, conv_w, conv_b, w_r, b_r, w_i, b_i, lam, h0):
    xcv = short_conv(xr, conv_w, conv_b)
    a_f, bx_f = rglru_coeffs(xcv, w_r[0], b_r[0], w_i[0], b_i[0], lam[0])
    h_f = linear_scan(a_f, bx_f, h0[0])
    a_b, bx_b = rglru_coeffs(xcv, w_r[1], b_r[1], w_i[1], b_i[1], lam[1])
    h_b = linear_scan(a_b[:, ::-1], bx_b[:, ::-1], h0[1])[:, ::-1]
    return h_f, h_b


def diff_attend(q, k, v, lam):
    s = jnp.einsum('bqhcd,bkhcd->bhcqk', q, k).astype(jnp.float32) * (DIFF_HEAD_DIM ** -0.5)
    p = jax.nn.softmax(s, axis=-1)
    p_diff = (p[:, :, 0] - lam * p[:, :, 1]).astype(v.dtype)
    return jnp.einsum('bhqk,bkhe->bqhe', p_diff, v)


def diff_attention_latent(q, k, v, k_ctx, v_ctx, lam):
    B, S = q.shape[0], q.shape[1]
    k_all = jnp.concatenate([k, k_ctx], axis=1)
    v_all = jnp.concatenate([v, v_ctx], axis=1)
    n_blk = S // Q_BLOCK
    qb = jnp.moveaxis(q.reshape(B, n_blk, Q_BLOCK, DIFF_HEADS, 2, DIFF_HEAD_DIM), 1, 0)
    o = lax.map(lambda qi: diff_attend(qi, k_all, v_all, lam), qb)
    return jnp.moveaxis(o, 0, 1).reshape(B, S, DIFF_HEADS, DIFF_V_DIM)


def grouped_expert_ffn(h, expert_id, weight, w1, w3, w2):
    N, D = h.shape
    A = N * MOE_TOP_K
    e_flat = expert_id.reshape(A)
    tok_flat = jnp.repeat(jnp.arange(N, dtype=jnp.int32), MOE_TOP_K)
    order = jnp.argsort(e_flat)
    e_sorted = e_flat[order]
    tok_sorted = tok_flat[order]
    w_sorted = weight.reshape(A)[order]
    counts = jnp.bincount(e_flat, length=N_EXPERTS)
    padded = (counts + MOE_BLOCK - 1) // MOE_BLOCK * MOE_BLOCK
    pad_end = jnp.cumsum(padded)
    pad_start = pad_end - padded
    start = jnp.cumsum(counts) - counts
    dest = pad_start[e_sorted] + jnp.arange(A) - start[e_sorted]
    n_blocks = -(-A // MOE_BLOCK) + N_EXPERTS
    n_rows = n_blocks * MOE_BLOCK
    row_tok = jnp.full((n_rows,), N, dtype=jnp.int32).at[dest].set(tok_sorted)
    blk_expert = jnp.minimum(jnp.searchsorted(pad_end, jnp.arange(n_blocks) * MOE_BLOCK, side='right'),
                             N_EXPERTS - 1)
    h_pad = jnp.concatenate([h, jnp.zeros((1, D), h.dtype)], axis=0)
    xb = h_pad[row_tok].reshape(n_blocks, MOE_BLOCK, D)

    def expert_block(args):
        xi, e = args
        return (jax.nn.silu(xi @ w1[e]) * (xi @ w3[e])) @ w2[e]

    yb = lax.map(expert_block, (xb, blk_expert)).reshape(n_rows, D)
    contrib = yb[dest] * w_sorted[:, None].astype(yb.dtype)
    return jnp.zeros((N, D), h.dtype).at[tok_sorted].add(contrib)


def hierarchical_moe(h, w_g, b_g, w_e, b_e, w1, w3, w2):
    N = h.shape[0]
    g_prob = jax.nn.softmax((h @ w_g + b_g).astype(jnp.float32), axis=-1)
    g_top, g_idx = lax.top_k(g_prob, 1)
    e_logits = (h @ w_e + b_e).astype(jnp.float32).reshape(N, N_GROUPS, EXP_PER_GROUP)
    e_logits = jnp.take_along_axis(e_logits, g_idx[:, :, None], axis=1)[:, 0]
    e_top, e_idx = lax.top_k(jax.nn.softmax(e_logits, axis=-1), MOE_TOP_K)
    weight = g_top * e_top / jnp.sum(e_top, axis=-1, keepdims=True)
    expert_id = g_idx * EXP_PER_GROUP + e_idx
    return grouped_expert_ffn(h, expert_id, weight, w1, w3, w2)


def setup_inputs(seed: int = 0) -> dict:
    key = jax.random.key(seed)
    ks = jax.random.split(key, 28)
    f32 = jnp.float32
    D = D_MODEL
    nrm = lambda k, shape, s: jax.random.normal(k, shape, f32) * s
    u = jax.random.uniform(ks[17], (DEPTH, 2, RG_WIDTH), f32, 0.9, 0.999)
    a = u ** (1.0 / RG_C)
    return {
        'x': nrm(ks[0], (BATCH, SEQ, D), 1.0),
        'c': nrm(ks[1], (BATCH, D), 1.0),
        'ctx': nrm(ks[2], (BATCH, CTX_LEN, D), 1.0),
        'c_ctx': nrm(ks[3], (D,), 1.0),
        'w_ada': nrm(ks[4], (DEPTH, D, 6 * D), 0.5 * D ** -0.5),
        'b_ada': nrm(ks[5], (DEPTH, 6 * D), 0.02),
        'g_mix': 1.0 + nrm(ks[6], (DEPTH, D), 0.02),
        'g_ffn': 1.0 + nrm(ks[7], (DEPTH, D), 0.02),
        'w_in': nrm(ks[8], (DEPTH, D, IN_COLS), D ** -0.5),
        'w_out': nrm(ks[9], (DEPTH, MIX_WIDTH, D), MIX_WIDTH ** -0.5),
        'na_rpb': nrm(ks[10], (DEPTH, NA_HEADS, 2 * NA_WIN_ROWS - 1, 2 * NA_WIN_COLS - 1), 0.1),
        'rg_conv_w': nrm(ks[11], (DEPTH, RG_CONV_W, RG_WIDTH), RG_CONV_W ** -0.5),
        'rg_conv_b': nrm(ks[12], (DEPTH, RG_WIDTH), 0.02),
        'rg_w_r': nrm(ks[13], (DEPTH, 2, RG_BLOCKS, RG_BLOCK_W, RG_BLOCK_W), RG_BLOCK_W ** -0.5),
        'rg_b_r': nrm(ks[14], (DEPTH, 2, RG_WIDTH), 0.02),
        'rg_w_i': nrm(ks[15], (DEPTH, 2, RG_BLOCKS, RG_BLOCK_W, RG_BLOCK_W), RG_BLOCK_W ** -0.5),
        'rg_b_i': nrm(ks[16], (DEPTH, 2, RG_WIDTH), 0.02),
        'rg_lambda': jnp.log(a) - jnp.log1p(-a),
        'diff_lambda': nrm(ks[18], (DEPTH, 4, DIFF_HEAD_DIM), 0.1),
        'diff_subln_g': 1.0 + nrm(ks[19], (DEPTH, DIFF_V_DIM), 0.02),
        'router_w_group': nrm(ks[20], (DEPTH, D, N_GROUPS), D ** -0.5),
        'router_b_group': nrm(ks[21], (DEPTH, N_GROUPS), 0.01),
        'router_w_expert': nrm(ks[22], (DEPTH, D, N_EXPERTS), D ** -0.5),
        'router_b_expert': nrm(ks[23], (DEPTH, N_EXPERTS), 0.01),
        'moe_w1': nrm(ks[24], (DEPTH, N_EXPERTS, D, D_EXPERT), D ** -0.5),
        'moe_w3': nrm(ks[25], (DEPTH, N_EXPERTS, D, D_EXPERT), D ** -0.5),
        'moe_w2': nrm(ks[26], (DEPTH, N_EXPERTS, D_EXPERT, D), D_EXPERT ** -0.5),
        'g_final': 1.0 + nrm(ks[27], (D,), 0.02),
    }


def reference(x, c, ctx, c_ctx, w_ada, b_ada, g_mix, g_ffn, w_in, w_out, na_rpb,
              rg_conv_w, rg_conv_b, rg_w_r, rg_b_r, rg_w_i, rg_b_i, rg_lambda,
              diff_lambda, diff_subln_g, router_w_group, router_b_group,
              router_w_expert, router_b_expert, moe_w1, moe_w3, moe_w2, g_final):
    B, S, D = x.shape
    L = ctx.shape[1]
    rope = axial_rope_tables(S)
    s_c = jax.nn.silu(c)
    s_cc = jax.nn.silu(c_ctx)
    xl, xc = x, ctx
    for l in range(DEPTH):
        last = l == DEPTH - 1
        sh1, sc1, ga1, sh2, sc2, ga2 = [m[:, None, :] for m in jnp.split(s_c @ w_ada[l] + b_ada[l], 6, axis=-1)]
        csh1, csc1, cga1, csh2, csc2, cga2 = jnp.split(s_cc @ w_ada[l] + b_ada[l], 6)
        hl = rmsnorm(xl, g_mix[l]) * (1 + sc1) + sh1
        hc = rmsnorm(xc, g_mix[l]) * (1 + csc1) + csh1
        qa, ka, va, xr, gr, qd, kd, vd = jnp.split(hl @ w_in[l], IN_SPLIT_POINTS, axis=-1)
        qa_c, ka_c, va_c, xr_c, gr_c, qd_c, kd_c, vd_c = jnp.split(hc @ w_in[l], IN_SPLIT_POINTS, axis=-1)

        ka_c = ka_c.reshape(B, L, NA_HEADS, NA_HEAD_DIM)
        va_c = va_c.reshape(B, L, NA_HEADS, NA_HEAD_DIM)
        y_na = neighbourhood_attention(qa, ka, va, ka_c, va_c, na_rpb[l])

        rg_args = (rg_conv_w[l], rg_conv_b[l], rg_w_r[l], rg_b_r[l], rg_w_i[l], rg_b_i[l], rg_lambda[l])
        hf_c, hb_c = rglru_bidir(xr_c, *rg_args, jnp.zeros((2, B, RG_WIDTH), jnp.float32))
        hf, hb = rglru_bidir(xr, *rg_args, jnp.stack([hf_c[:, -1], hb_c[:, 0]]))
        y_rg = (hf + hb).astype(xr.dtype) * jax.nn.gelu(gr)

        lam_init = 0.8 - 0.6 * math.exp(-0.3 * l)
        lvec = diff_lambda[l].astype(jnp.float32)
        lam = jnp.exp(jnp.sum(lvec[0] * lvec[1])) - jnp.exp(jnp.sum(lvec[2] * lvec[3])) + lam_init
        qd_h = apply_axial_rope(qd.reshape(B, S, DIFF_HEADS, 2, DIFF_HEAD_DIM), rope)
        kd_h = apply_axial_rope(kd.reshape(B, S, DIFF_HEADS, 2, DIFF_HEAD_DIM), rope)
        vd_h = vd.reshape(B, S, DIFF_HEADS, DIFF_V_DIM)
        kd_c = kd_c.reshape(B, L, DIFF_HEADS, 2, DIFF_HEAD_DIM)
        vd_c = vd_c.reshape(B, L, DIFF_HEADS, DIFF_V_DIM)
        y_diff = diff_attention_latent(qd_h, kd_h, vd_h, kd_c, vd_c, lam)
        y_diff = (rmsnorm(y_diff, diff_subln_g[l]) * (1 - lam_init)).reshape(B, S, DIFF_WIDTH)

        xl = xl + ga1 * (jnp.concatenate([y_na, y_rg, y_diff], axis=-1) @ w_out[l])
        hl2 = rmsnorm(xl, g_ffn[l]) * (1 + sc2) + sh2
        moe_args = (router_w_group[l], router_b_group[l], router_w_expert[l], router_b_expert[l],
                    moe_w1[l], moe_w3[l], moe_w2[l])
        if last:
            xl = xl + ga2 * hierarchical_moe(hl2.reshape(B * S, D), *moe_args).reshape(B, S, D)
        else:
            y_na_c = ctx_attention(qa_c.reshape(B, L, NA_HEADS, NA_HEAD_DIM), ka_c, va_c).reshape(B, L, NA_WIDTH)
            y_rg_c = (hf_c + hb_c).astype(xr_c.dtype) * jax.nn.gelu(gr_c)
            y_diff_c = diff_attend(qd_c.reshape(B, L, DIFF_HEADS, 2, DIFF_HEAD_DIM), kd_c, vd_c, lam)
            y_diff_c = (rmsnorm(y_diff_c, diff_subln_g[l]) * (1 - lam_init)).reshape(B, L, DIFF_WIDTH)
            xc = xc + cga1 * (jnp.concatenate([y_na_c, y_rg_c, y_diff_c], axis=-1) @ w_out[l])
            hc2 = rmsnorm(xc, g_ffn[l]) * (1 + csc2) + csh2
            y = hierarchical_moe(jnp.concatenate([hl2.reshape(B * S, D), hc2.reshape(B * L, D)], axis=0), *moe_args)
            xl = xl + ga2 * y[:B * S].reshape(B, S, D)
            xc = xc + cga2 * y[B * S:].reshape(B, L, D)
    return rmsnorm(xl, g_final)
```

```python
import math
import numpy as np
import ml_dtypes
import concourse.bass as bass
import concourse.mybir as mybir
from concourse.bass_utils import run_bass_kernel_spmd

F32 = mybir.dt.float32
BF16 = mybir.dt.bfloat16
I32 = mybir.dt.int32
U32 = mybir.dt.uint32
AF = mybir.ActivationFunctionType
ALU = mybir.AluOpType
AX = mybir.AxisListType

NCORES = 8
D = 1024
B = 2
S = 8192
L = 256
DEPTH = 4
GRID_W = 64
EPS = 1e-6


class Buf:
    __slots__ = ("name", "w", "r", "dsem")

    def __init__(self, name, dsem=None):
        self.name = name
        self.w = None
        self.r = []
        self.dsem = dsem


class DmaSem:
    def __init__(self, sched, name):
        self.sem = sched.nc.alloc_semaphore(name)
        self.key = ("dma", name)
        self.total = 0
        sched.sems[self.key] = self


class Sched:
    def __init__(self, nc):
        self.nc = nc
        self.eng = {"pe": nc.tensor, "dve": nc.vector, "act": nc.scalar,
                    "pool": nc.gpsimd, "sp": nc.sync}
        self.sems = {}
        self.esem = {}
        self.cnt = {}
        for k in self.eng:
            self.esem[k] = nc.alloc_semaphore("e_" + k)
            self.cnt[k] = 0
        self.seen = {}
        self.nbuf = 0
        self.out_tokens = []

    def buf(self, name=None, dsem=None):
        self.nbuf += 1
        return Buf(name or f"b{self.nbuf}", dsem)

    def dsem(self, name):
        return DmaSem(self, name)

    def _semof(self, key):
        if key[0] == "dma":
            return self.sems[key].sem
        return self.esem[key[0]]

    def _wait(self, engname, deps):
        e = self.eng[engname]
        for key, val in deps.items():
            if key[0] == "dma":
                val = max(val, 0)
            if self.seen.get((engname, key), 0) >= val:
                continue
            self.seen[(engname, key)] = val
            e.wait_ge(self._semof(key), val)

    def _deps(self, R, W):
        deps = {}

        def add(tok):
            if tok is None:
                return
            key, val = tok
            if key[0] == "dma":
                val = self.sems[key].total
            if deps.get(key, 0) < val:
                deps[key] = val
        for b in R:
            add(b.w)
        for b in W:
            add(b.w)
            for t in b.r:
                add(t)
        return deps

    def _commit(self, tok, R, W):
        for b in R:
            b.r.append(tok)
        for b in W:
            b.w = tok
            b.r = []

    def op(self, engname, fn, R=(), W=()):
        deps = self._deps(R, W)
        if engname == "pe":
            deps.pop(("pe",), None)
        self._wait(engname, deps)
        ins = fn(self.eng[engname])
        self.cnt[engname] += 1
        ins.then_inc(self.esem[engname], 1)
        tok = ((engname,), self.cnt[engname])
        self._commit(tok, R, W)
        return tok

    def dma(self, q, out, in_, R=(), W=(), sem=None, **kw):
        deps = self._deps(R, W)
        self._wait(q, deps)
        ds = sem
        if ds is None:
            for b in list(W) + list(R):
                if b.dsem is not None:
                    ds = b.dsem
                    break
        assert ds is not None, "dma needs a DmaSem"
        ins = self.eng[q].dma_start(out=out, in_=in_, **kw)
        ds.total += 16
        ins.then_inc(ds.sem, 16)
        tok = (ds.key, ds.total)
        self._commit(tok, R, W)
        return tok

    def barrier(self, bufs=()):
        deps = {}
        for k in self.eng:
            if self.cnt[k]:
                deps[(k,)] = self.cnt[k]
        for key, ds in self.sems.items():
            if ds.total:
                deps[key] = ds.total
        for k in self.eng:
            d = {kk: v for kk, v in deps.items() if kk != (k,)}
            self._wait(k, d)

    def coll(self, kind, ins, outs, R=(), W=(), groups=None):
        deps = self._deps(R, W)
        self._wait("pool", deps)
        ds = None
        for b in list(W) + list(R):
            if b.dsem is not None:
                ds = b.dsem
                break
        g = groups or [[0, 1, 2, 3], [4, 5, 6, 7]]
        ins_ = self.nc.gpsimd.collective_compute(kind, ALU.bypass, replica_groups=g, ins=ins, outs=outs)
        ds.total += 16
        ins_.then_inc(ds.sem, 16)
        tok = (ds.key, ds.total)
        self._commit(tok, R, W)
        return tok

    def finish(self, bufs, engname="sp"):
        deps = {}
        for b in bufs:
            for tok in ([b.w] if b.w else []) + b.r:
                key, val = tok
                if key[0] == "dma":
                    val = self.sems[key].total
                deps[key] = max(deps.get(key, 0), val)
        self._wait(engname, deps)


def mm(s, out, lhsT, rhs, start, stop, R, W):
    return s.op("pe", lambda e: e.matmul(out, lhsT, rhs, start=start, stop=stop), R=R, W=W)


def build_p0():
    nc = bass.Bass("TRN2", target_bir_lowering=False)
    NCOL = 3072
    NJ = NCOL // 128
    cT = nc.dram_tensor("cT", [128, 8, 4], F32, kind="ExternalInput").ap()
    w = nc.dram_tensor("w", [D, NCOL], F32, kind="ExternalInput").ap()
    bvec = nc.dram_tensor("bvec", [128, NJ], F32, kind="ExternalInput").ap()
    out = nc.dram_tensor("out", [128, NJ, 4], F32, kind="ExternalOutput").ap()
    s = Sched(nc)
    wv = w.rearrange("(kc p) n -> p kc n", p=128)
    with (nc.sbuf_tensor("w_sb", [128, 8, NCOL], F32) as w_sb,
          nc.sbuf_tensor("c_sb", [128, 8, 4], F32) as c_sb,
          nc.sbuf_tensor("s_sb", [128, 8, 4], F32) as s_sb,
          nc.sbuf_tensor("b_sb", [128, NJ], F32) as b_sb,
          nc.sbuf_tensor("r_sb", [128, NJ, 4], F32) as r_sb,
          nc.psum_tensor("ps", [128, 8, 512], F32) as ps):
        bw = [s.buf(f"w{k}", s.dsem(f"w{k}")) for k in range(8)]
        bc = s.buf("c", s.dsem("c"))
        bb = s.buf("b", bc.dsem)
        bs = s.buf("s")
        br = s.buf("r", s.dsem("r"))
        bps = [s.buf(f"ps{i}") for i in range(8)]
        s.dma("sp", c_sb[:], cT, W=[bc])
        s.dma("sp", b_sb[:], bvec, W=[bb])
        for k in range(8):
            s.dma("sp" if k % 2 == 0 else "act", w_sb[:, k, :], wv[:, k, :], W=[bw[k]])
        s.op("act", lambda e: e.activation(out=s_sb[:], in_=c_sb[:], func=AF.Silu), R=[bc], W=[bs])
        for j in range(NJ):
            pb = bps[j % 8]
            for k in range(8):
                mm(s, ps[:, j % 8, 0:4], w_sb[:, k, j * 128:(j + 1) * 128], s_sb[:, k, :],
                   k == 0, k == 7, R=[bw[k], bs], W=[pb])
            s.op("dve", lambda e: e.tensor_scalar(out=r_sb[:, j, :], in0=ps[:, j % 8, 0:4],
                                                  scalar1=b_sb[:, j:j + 1], scalar2=None, op0=ALU.add),
                 R=[pb, bb], W=[br])
        s.dma("sp", out, r_sb[:], R=[br])
        s.finish([br])
    return nc


def silu_np_layout_c(c, c_ctx):
    cc = np.stack([c[0], c[1], c_ctx, c_ctx], axis=1)
    return np.ascontiguousarray(cc.reshape(8, 128, 4).transpose(1, 0, 2))


def run_p0(c, c_ctx, w_ada, b_ada):
    nc = build_p0()
    cT = silu_np_layout_c(c, c_ctx)
    in_maps = []
    for i in range(NCORES):
        l, h = i // 2, i % 2
        in_maps.append({
            "cT": cT,
            "w": np.ascontiguousarray(w_ada[l][:, h * 3072:(h + 1) * 3072]),
            "bvec": np.ascontiguousarray(b_ada[l][h * 3072:(h + 1) * 3072].reshape(24, 128).T),
        })
    res = run_bass_kernel_spmd(nc, in_maps, core_ids=list(range(NCORES)))
    mods = np.zeros((DEPTH, 3, 6 * D), np.float32)
    for i in range(NCORES):
        l, h = i // 2, i % 2
        o = res.results[i]["out"]
        m = o.transpose(1, 0, 2).reshape(3072, 4)
        mods[l, :, h * 3072:(h + 1) * 3072] = m[:, :3].T
    return mods


NT1 = 2112
NW1 = 3072
BLKS1 = [(0, 512), (512, 512), (1024, 512), (1536, 512), (2048, 64)]


def build_p1():
    nc = bass.Bass("TRN2", target_bir_lowering=False)
    xT = nc.dram_tensor("xT", [128, 8, NT1], F32, kind="ExternalInput").ap()
    w = nc.dram_tensor("w", [D, NW1], F32, kind="ExternalInput").ap()
    gsc = nc.dram_tensor("gsc", [128, 5, 8], F32, kind="ExternalInput").ap()
    cosT = nc.dram_tensor("cosT", [128, 2048], F32, kind="ExternalInput").ap()
    sinT = nc.dram_tensor("sinT", [128, 2048], F32, kind="ExternalInput").ap()
    fmb = nc.dram_tensor("fmb", [128, 8, NT1], BF16, kind="ExternalOutput").ap()
    fmf = nc.dram_tensor("fmf", [128, 8, NT1], F32, kind="ExternalOutput").ap()
    tm = nc.dram_tensor("tm", [NT1, 512], BF16, kind="ExternalOutput").ap()
    s = Sched(nc)
    wv = w.rearrange("(kc p) n -> p kc n", p=128)
    with (nc.sbuf_tensor("x_sb", [128, 2, 8, 512], F32) as x_sb,
          nc.sbuf_tensor("h_sb", [128, 8, NT1], BF16) as h_sb,
          nc.sbuf_tensor("w_bf", [128, 8, NW1], BF16) as w_bf,
          nc.sbuf_tensor("w_st", [128, 2, NW1], F32) as w_st,
          nc.sbuf_tensor("o_sb", [128, 4, 512], F32) as o_sb,
          nc.sbuf_tensor("ob_sb", [128, 4, 512], BF16) as ob_sb,
          nc.sbuf_tensor("cos_sb", [128, 2048], F32) as cos_sb,
          nc.sbuf_tensor("sin_sb", [128, 2048], F32) as sin_sb,
          nc.sbuf_tensor("gsc_sb", [128, 5, 8], F32) as gsc_sb,
          nc.sbuf_tensor("ab_sb", [128, 2, 8], F32) as ab_sb,
          nc.sbuf_tensor("sq_sb", [128, 8, 512], BF16) as sq_sb,
          nc.sbuf_tensor("ones_sb", [128, 128], BF16) as ones_sb,
          nc.sbuf_tensor("rstd_sb", [128, 2, 512], F32) as rstd_sb,
          nc.sbuf_tensor("tmp_sb", [128, 4, 512], F32) as tmp_sb,
          nc.psum_tensor("ps", [128, 8, 512], F32) as ps):
        bx = [s.buf(f"x{i}", s.dsem(f"x{i}")) for i in range(2)]
        bh = [s.buf(f"h{i}") for i in range(len(BLKS1))]
        bwst = [s.buf(f"wst{i}", s.dsem(f"wst{i}")) for i in range(2)]
        bwbf = [s.buf(f"wbf{k}") for k in range(8)]
        bo = [s.buf(f"o{i}", s.dsem(f"o{i}")) for i in range(4)]
        bob = [s.buf(f"ob{i}", s.dsem(f"ob{i}")) for i in range(4)]
        cs = s.dsem("const")
        bcos = s.buf("cos", cs); bsin = s.buf("sin", cs); bgsc = s.buf("gsc", cs)
        bab = s.buf("ab"); bsq = s.buf("sq"); bones = s.buf("ones")
        brs = [s.buf(f"rs{i}") for i in range(2)]
        btmp = [s.buf(f"tmp{i}") for i in range(4)]
        bps = [s.buf(f"ps{i}") for i in range(8)]

        s.dma("sp", gsc_sb[:], gsc, W=[bgsc])
        s.dma("sp", cos_sb[:], cosT, W=[bcos])
        s.dma("sp", sin_sb[:], sinT, W=[bsin])
        s.op("pool", lambda e: e.memset(ones_sb[:], 1.0), W=[bones])
        for t in range(2):
            s.op("dve", lambda e: e.scalar_tensor_tensor(
                out=ab_sb[:, t, :], in0=gsc_sb[:, 1 + 2 * t, :], scalar=1.0, in1=gsc_sb[:, 0, :],
                op0=ALU.add, op1=ALU.mult), R=[bgsc], W=[bab])
        for k in range(8):
            sl = k % 2
            s.dma("act", w_st[:, sl, :], wv[:, k, :], W=[bwst[sl]])
            s.op("pool", lambda e: e.tensor_copy(out=w_bf[:, k, :], in_=w_st[:, sl, :]),
                 R=[bwst[sl]], W=[bwbf[k]])
        for bi, (t0, n) in enumerate(BLKS1):
            sl = bi % 2
            isctx = bi == 4
            s.dma("sp", x_sb[:, sl, :, 0:n], xT[:, :, t0:t0 + n], W=[bx[sl]])
            s.op("act", lambda e: e.activation(out=sq_sb[:, :, 0:n], in_=x_sb[:, sl, :, 0:n], func=AF.Square),
                 R=[bx[sl]], W=[bsq])
            pst = bps[sl]
            for k in range(8):
                mm(s, ps[:, sl, 0:n], ones_sb[:], sq_sb[:, k, 0:n], k == 0, k == 7, R=[bones, bsq], W=[pst])
            s.op("act", lambda e: e.activation(out=rstd_sb[:, sl, 0:n], in_=ps[:, sl, 0:n], func=AF.Sqrt,
                                               scale=1.0 / D, bias=EPS), R=[pst], W=[brs[sl]])
            s.op("dve", lambda e: e.reciprocal(out=rstd_sb[:, sl, 0:n], in_=rstd_sb[:, sl, 0:n]),
                 R=[brs[sl]], W=[brs[sl]])
            ai = 1 if isctx else 0
            shrow = 4 if isctx else 2
            for k in range(8):
                tb = k % 4
                s.op("dve", lambda e: e.scalar_tensor_tensor(
                    out=tmp_sb[:, tb, 0:n], in0=x_sb[:, sl, k, 0:n], scalar=ab_sb[:, ai, k:k + 1],
                    in1=rstd_sb[:, sl, 0:n], op0=ALU.mult, op1=ALU.mult),
                    R=[bx[sl], bab, brs[sl]], W=[btmp[tb]])
                s.op("act", lambda e: e.activation(out=h_sb[:, k, t0:t0 + n], in_=tmp_sb[:, tb, 0:n],
                                                   func=AF.Identity, bias=gsc_sb[:, shrow, k:k + 1], scale=1.0),
                     R=[btmp[tb], bgsc], W=[bh[bi]])
        oi = 0
        obi = 0
        pi = 0
        for bi, (t0, n) in enumerate(BLKS1):
            isctx = bi == 4
            for c in range(16):
                rope = (c >= 12) and not isctx
                isb = c < 4 or c >= 12
                dst = (fmb[:, c if c < 4 else c - 8, t0:t0 + n]) if isb else fmf[:, c - 4, t0:t0 + n]
                pA = 2 + (pi % 6); pi += 1
                for k in range(8):
                    mm(s, ps[:, pA, 0:n], w_bf[:, k, c * 128:(c + 1) * 128], h_sb[:, k, t0:t0 + n],
                       k == 0, k == 7, R=[bwbf[k], bh[bi]], W=[bps[pA]])
                if isb:
                    ob = obi % 4; obi += 1
                    osl = ob_sb[:, ob, 0:n]; obuf = bob[ob]
                else:
                    ob = oi % 4; oi += 1
                    osl = o_sb[:, ob, 0:n]; obuf = bo[ob]
                if not rope:
                    if c % 2 == 0:
                        s.op("act", lambda e: e.copy(out=osl, in_=ps[:, pA, 0:n]), R=[bps[pA]], W=[obuf])
                    else:
                        s.op("dve", lambda e: e.tensor_copy(out=osl, in_=ps[:, pA, 0:n]), R=[bps[pA]], W=[obuf])
                else:
                    pB = 2 + (pi % 6); pi += 1
                    c2 = c + 4
                    for k in range(8):
                        mm(s, ps[:, pB, 0:n], w_bf[:, k, c2 * 128:(c2 + 1) * 128], h_sb[:, k, t0:t0 + n],
                           k == 0, k == 7, R=[bwbf[k], bh[bi]], W=[bps[pB]])
                    s.op("dve", lambda e: e.tensor_tensor(out=tmp_sb[:, 0, 0:n], in0=ps[:, pA, 0:n],
                                                          in1=cos_sb[:, t0:t0 + n], op=ALU.mult),
                         R=[bps[pA], bcos], W=[btmp[0]])
                    s.op("dve", lambda e: e.tensor_tensor(out=tmp_sb[:, 1, 0:n], in0=ps[:, pB, 0:n],
                                                          in1=sin_sb[:, t0:t0 + n], op=ALU.mult),
                         R=[bps[pB], bsin], W=[btmp[1]])
                    s.op("pool", lambda e: e.tensor_tensor(out=osl, in0=tmp_sb[:, 0, 0:n],
                                                           in1=tmp_sb[:, 1, 0:n], op=ALU.add),
                         R=[btmp[0], btmp[1]], W=[obuf])
                s.dma("sp", dst, osl, R=[obuf])
        ntile = NT1 // 128 + 1
        for ti in range(ntile):
            t0 = ti * 128
            n = min(128, NT1 - t0)
            bi = min(t0 // 512, 4)
            pA = 2 + (pi % 6); pi += 1
            for k in range(8):
                mm(s, ps[0:n, pA, :], h_sb[:, k, t0:t0 + n], w_bf[:, k, 2560:3072],
                   k == 0, k == 7, R=[bwbf[k], bh[bi]], W=[bps[pA]])
            ob = obi % 4; obi += 1
            s.op("act", lambda e: e.copy(out=ob_sb[0:n, ob, :], in_=ps[0:n, pA, :]), R=[bps[pA]], W=[bob[ob]])
            s.dma("sp", tm[t0:t0 + n, :], ob_sb[0:n, ob, :], R=[bob[ob]])
        s.finish(bo + bob)
    return nc


def rope_tables():
    t = np.arange(S)
    row = (t // GRID_W).astype(np.float32)
    col = (t % GRID_W).astype(np.float32)
    inv = (10000.0 ** (-np.arange(0, 16, 2, dtype=np.float32) / 16.0)).astype(np.float32)
    ang_r = row[:, None] * inv
    ang_c = col[:, None] * inv
    cosT = np.zeros((32, S), np.float32)
    sinT = np.zeros((32, S), np.float32)
    for d in range(32):
        ang = ang_r if d < 16 else ang_c
        i = d % 8
        cosT[d] = np.cos(ang[:, i])
        sgn = -1.0 if (d % 16) < 8 else 1.0
        sinT[d] = sgn * np.sin(ang[:, i])
    return np.tile(cosT, (4, 1)), np.tile(sinT, (4, 1))


def p1_wcols():
    sw = np.array([(d + 8) if (d % 16) < 8 else (d - 8) for d in range(32)])
    f = np.arange(256)
    swf = (f // 32) * 32 + sw[f % 32]
    cols = np.concatenate([np.arange(0, 256), np.arange(256, 512), np.arange(768, 1280), np.arange(1280, 1792),
                           np.arange(1792, 2048), np.arange(2048, 2304), 1792 + swf, 2048 + swf,
                           np.arange(512, 768), np.arange(2304, 2560)])
    return cols


def chunkT(a):
    T = a.shape[0]
    return np.ascontiguousarray(a.T.reshape(8, 128, T).transpose(1, 0, 2))


def vec_pk(v):
    return np.ascontiguousarray(v.reshape(8, 128).T)


def run_p1(nc1, xl, xc, mods_l, g_mix_l, w_in_l, cosT, sinT):
    wl = np.ascontiguousarray(w_in_l[:, p1_wcols()])
    in_maps = []
    for i in range(NCORES):
        b, j = i // 4, i % 4
        xx = np.concatenate([xl[b, 2048 * j:2048 * (j + 1)], xc[b, 64 * j:64 * (j + 1)]], axis=0)
        gsc = np.stack([vec_pk(g_mix_l), vec_pk(mods_l[b, 1024:2048]), vec_pk(mods_l[b, 0:1024]),
                        vec_pk(mods_l[2, 1024:2048]), vec_pk(mods_l[2, 0:1024])], axis=1)
        in_maps.append({"xT": chunkT(xx), "w": wl, "gsc": np.ascontiguousarray(gsc),
                        "cosT": np.ascontiguousarray(cosT[:, 2048 * j:2048 * (j + 1)]),
                        "sinT": np.ascontiguousarray(sinT[:, 2048 * j:2048 * (j + 1)])})
    res = run_bass_kernel_spmd(nc1, in_maps, core_ids=list(range(NCORES)))
    fmb = np.zeros((B, 1024, S + L), ml_dtypes.bfloat16)
    fmf = np.zeros((B, 1024, S + L), np.float32)
    tm = np.zeros((B, S + L, 512), ml_dtypes.bfloat16)
    for i in range(NCORES):
        b, j = i // 4, i % 4
        r = res.results[i]
        for dst, key in ((fmb, "fmb"), (fmf, "fmf")):
            f = r[key].transpose(1, 0, 2).reshape(1024, NT1)
            dst[b, :, 2048 * j:2048 * (j + 1)] = f[:, :2048]
            dst[b, :, S + 64 * j:S + 64 * (j + 1)] = f[:, 2048:]
        t = r["tm"]
        tm[b, 2048 * j:2048 * (j + 1)] = t[:2048]
        tm[b, S + 64 * j:S + 64 * (j + 1)] = t[2048:]
    return fmb, fmf, tm


NTOK = S + L
NKT = NTOK // 128
NEB = 21


def na_tile_lists():
    out = []
    for m in range(64):
        if 2 <= m <= 61:
            out.append(([m - 2, m - 1, m, m + 1, m + 2], 0))
        elif m < 2:
            out.append(([0, 1, 2, 3], 5 + 4 * m))
        else:
            out.append(([60, 61, 62, 63], 5 + 4 * (m - 60)))
    return out


def na_bias_index():
    MASKED = 15 * 31
    idx = np.full((NEB, 128, 128), MASKED, np.int64)
    lists = na_tile_lists()
    reps = {0: 10}
    qq = np.arange(128); kk = np.arange(128)

    def fill(e0, m, kts):
        for ii, n in enumerate(kts):
            qr = 2 * m + qq // 64; qc = qq % 64
            kr = 2 * n + kk // 64; kc = kk % 64
            r0 = np.clip(qr - 4, 0, 120)
            cs = np.clip(qc - 8, 0, 48)
            valid = ((kr[:, None] >= r0[None, :]) & (kr[:, None] < r0[None, :] + 8) &
                     (kc[:, None] >= cs[None, :]) & (kc[:, None] < cs[None, :] + 16))
            dr = kr[:, None] - qr[None, :]
            dc = np.clip(kc[:, None] - qc[None, :], -15, 15)
            v = (np.clip(dr, -7, 7) + 7) * 31 + dc + 15
            idx[e0 + ii] = np.where(valid, v, MASKED)
    fill(0, 10, lists[10][0])
    for m in (0, 1, 62, 63):
        fill(lists[m][1], m, lists[m][0])
    return idx


def build_p2(lam_init):
    nc = bass.Bass("TRN2", target_bir_lowering=False)
    din = lambda n, shp, dt: nc.dram_tensor(n, shp, dt, kind="ExternalInput").ap()
    dout = lambda n, shp, dt: nc.dram_tensor(n, shp, dt, kind="ExternalOutput").ap()
    xrg = din("xrg", [2, 128, NTOK], F32)
    wbd = din("wbd", [4, 128, 128], F32)
    rgv = din("rgv", [128, 2, 8], F32)
    hout = dout("hout", [2, 128, NTOK], F32)
    qaT = din("qaT", [64, NTOK], BF16)
    kaT = din("kaT", [64, NTOK], BF16)
    vaP = din("vaP", [128, NKT, 64], BF16)
    btT = din("btT", [128, NEB, 128], F32)
    ynaP = dout("ynaP", [128, NKT, 64], BF16)
    qdT = din("qdT", [2, 32, NTOK], BF16)
    kdT = din("kdT", [2, 32, NTOK], BF16)
    vdP = din("vdP", [128, NKT, 64], BF16)
    dlam = din("dlam", [128, 128], F32)
    dg = din("dg", [128, 64], F32)
    ydfP = dout("ydfP", [128, NKT, 64], BF16)
    s = Sched(nc)
    with nc.psum_tensor("ps", [128, 8, 512], F32) as ps:
        bps = [s.buf(f"ps{i}") for i in range(8)]

        CH = 2048
        with (nc.sbuf_tensor("x_sb", [128, NTOK], F32) as x_sb,
              nc.sbuf_tensor("wst_sb", [128, 4, 128], F32) as wst_sb,
              nc.sbuf_tensor("wbf_sb", [128, 4, 128], BF16) as wbf_sb,
              nc.sbuf_tensor("rgv_sb", [128, 2, 8], F32) as rgv_sb,
              nc.sbuf_tensor("cneg_sb", [128, 2], F32) as cneg_sb,
              nc.sbuf_tensor("xcv_sb", [128, CH], F32) as xcv_sb,
              nc.sbuf_tensor("xcb_sb", [128, CH], BF16) as xcb_sb,
              nc.sbuf_tensor("r_sb", [128, CH], F32) as r_sb,
              nc.sbuf_tensor("i_sb", [128, CH], F32) as i_sb,
              nc.sbuf_tensor("a_sb", [128, CH], F32) as a_sb,
              nc.sbuf_tensor("q_sb", [128, CH], F32) as q_sb,
              nc.sbuf_tensor("h_sb", [128, 2, CH], F32) as h_sb,
              nc.sbuf_tensor("carry_sb", [128, 1], F32) as carry_sb):
            bx = s.buf("x", s.dsem("rgx"))
            bw = s.buf("w", s.dsem("rgw"))
            bwb = s.buf("wb")
            bv = s.buf("v", bw.dsem)
            bcn = s.buf("cneg")
            bxcv = s.buf("xcv"); bxcb = s.buf("xcb"); br = s.buf("r"); bi_ = s.buf("i")
            ba = s.buf("a"); bq = s.buf("q"); bcar = s.buf("carry")
            bh = [s.buf(f"h{i}", s.dsem(f"rgh{i}")) for i in range(2)]
            s.dma("act", wst_sb[:], wbd.rearrange("f c d -> c f d"), W=[bw])
            s.dma("act", rgv_sb[:], rgv, W=[bv])
            s.op("pool", lambda e: e.tensor_copy(out=wbf_sb[:], in_=wst_sb[:]), R=[bw], W=[bwb])
            s.op("act", lambda e: e.activation(out=cneg_sb[:], in_=rgv_sb[:, :, 7], func=AF.Exp, scale=-1.0),
                 R=[bv], W=[bcn])
            s.op("act", lambda e: e.activation(out=cneg_sb[:], in_=cneg_sb[:], func=AF.Ln, bias=1.0, scale=1.0),
                 R=[bcn], W=[bcn])
            s.op("dve", lambda e: e.tensor_scalar(out=cneg_sb[:], in0=cneg_sb[:], scalar1=-8.0, scalar2=None,
                                                  op0=ALU.mult), R=[bcn], W=[bcn])
            hi = 0
            for dr in range(2):
                offs = [-2, -1, 0, 1] if dr == 0 else [2, 1, 0, -1]
                s.dma("sp", x_sb[:, 0:4224], xrg[dr, :, 0:4224], W=[bx])
                s.dma("sp", x_sb[:, 4224:NTOK], xrg[dr, :, 4224:NTOK], W=[bx])
                chunks = [(0, 256, 0, 256)] + [(256 + CH * i, CH, 256, NTOK) for i in range(4)]
                for ci, (c0, n, s0, s1) in enumerate(chunks):
                    s.op("dve", lambda e: e.tensor_scalar(
                        out=xcv_sb[:, 0:n], in0=x_sb[:, c0:c0 + n], scalar1=rgv_sb[:, dr, 2:3],
                        scalar2=rgv_sb[:, dr, 4:5], op0=ALU.mult, op1=ALU.add), R=[bx, bv], W=[bxcv])
                    for jt in (0, 1, 3):
                        o = offs[jt]
                        lo = max(c0, s0 - o); hi_ = min(c0 + n, s1 - o)
                        s.op("dve", lambda e: e.scalar_tensor_tensor(
                            out=xcv_sb[:, lo - c0:hi_ - c0], in0=x_sb[:, lo + o:hi_ + o],
                            scalar=rgv_sb[:, dr, jt:jt + 1], in1=xcv_sb[:, lo - c0:hi_ - c0],
                            op0=ALU.mult, op1=ALU.add), R=[bx, bv, bxcv], W=[bxcv])
                    s.op("pool", lambda e: e.tensor_copy(out=xcb_sb[:, 0:n], in_=xcv_sb[:, 0:n]), R=[bxcv], W=[bxcb])
                    nsb = (n + 511) // 512
                    for sb in range(nsb):
                        w_ = min(512, n - sb * 512)
                        mm(s, ps[:, sb, 0:w_], wbf_sb[:, 2 * dr, :], xcb_sb[:, sb * 512:sb * 512 + w_], True, True,
                           R=[bwb, bxcb], W=[bps[sb]])
                        mm(s, ps[:, 4 + sb, 0:w_], wbf_sb[:, 2 * dr + 1, :], xcb_sb[:, sb * 512:sb * 512 + w_], True, True,
                           R=[bwb, bxcb], W=[bps[4 + sb]])
                    if n == CH:
                        rin = ps[:, 0:4, :]; iin = ps[:, 4:8, :]
                        rout = r_sb[:, 0:n].rearrange("p (a b) -> p a b", b=512)
                        iout = i_sb[:, 0:n].rearrange("p (a b) -> p a b", b=512)
                    else:
                        rin = ps[:, 0, 0:n]; iin = ps[:, 4, 0:n]
                        rout = r_sb[:, 0:n]; iout = i_sb[:, 0:n]
                    s.op("act", lambda e: e.activation(out=rout, in_=rin, func=AF.Sigmoid,
                                                       bias=rgv_sb[:, dr, 5:6], scale=1.0),
                         R=bps[0:4] + [bv], W=[br])
                    s.op("act", lambda e: e.activation(out=iout, in_=iin, func=AF.Sigmoid,
                                                       bias=rgv_sb[:, dr, 6:7], scale=1.0),
                         R=bps[4:8] + [bv], W=[bi_])
                    s.op("act", lambda e: e.activation(out=a_sb[:, 0:n], in_=r_sb[:, 0:n], func=AF.Exp,
                                                       scale=cneg_sb[:, dr:dr + 1]), R=[br, bcn], W=[ba])
                    s.op("pool", lambda e: e.tensor_tensor(out=q_sb[:, 0:n], in0=a_sb[:, 0:n], in1=a_sb[:, 0:n],
                                                           op=ALU.mult), R=[ba], W=[bq])
                    s.op("act", lambda e: e.activation(out=q_sb[:, 0:n], in_=q_sb[:, 0:n], func=AF.Sqrt,
                                                       scale=-1.0, bias=1.0), R=[bq], W=[bq])
                    s.op("pool", lambda e: e.tensor_tensor(out=i_sb[:, 0:n], in0=i_sb[:, 0:n], in1=xcv_sb[:, 0:n],
                                                           op=ALU.mult), R=[bi_, bxcv], W=[bi_])
                    s.op("pool", lambda e: e.tensor_tensor(out=q_sb[:, 0:n], in0=q_sb[:, 0:n], in1=i_sb[:, 0:n],
                                                           op=ALU.mult), R=[bq, bi_], W=[bq])
                    hs = hi % 2; hi += 1
                    init = 0.0 if ci == 0 else carry_sb[:, 0:1]
                    s.op("dve", lambda e: e.tensor_tensor_scan(out=h_sb[:, hs, 0:n], data0=a_sb[:, 0:n],
                                                               data1=q_sb[:, 0:n], initial=init,
                                                               op0=ALU.mult, op1=ALU.add),
                         R=[ba, bq] + ([bcar] if ci else []), W=[bh[hs]])
                    s.op("dve", lambda e: e.tensor_copy(out=carry_sb[:, 0:1], in_=h_sb[:, hs, n - 1:n]),
                         R=[bh[hs]], W=[bcar])
                    s.dma("sp", hout[dr, :, c0:c0 + n], h_sb[:, hs, 0:n], R=[bh[hs]])
            s.barrier(bh)

        with (nc.sbuf_tensor("na_q_sb", [64, NTOK], BF16) as q_sb,
              nc.sbuf_tensor("na_k_sb", [64, NTOK], BF16) as k_sb,
              nc.sbuf_tensor("na_v_sb", [128, NKT, 65], BF16) as v_sb,
              nc.sbuf_tensor("na_bt_sb", [128, NEB * 128], F32) as bt_sb,
              nc.sbuf_tensor("na_eb_sb", [128, NEB * 128], BF16) as eb_sb,
              nc.sbuf_tensor("na_e_sb", [128, 2, 640], F32) as e_sb,
              nc.sbuf_tensor("na_p_sb", [128, 2, 896], BF16) as p_sb,
              nc.sbuf_tensor("na_y_sb", [128, NKT, 64], BF16) as y_sb,
              nc.sbuf_tensor("na_rec_sb", [128, 2], F32) as rec_sb):
            ld = s.dsem("nald")
            bq = s.buf("q", ld); bk = s.buf("k", ld); bv = s.buf("v", ld); bbt = s.buf("bt", ld)
            beb = s.buf("eb")
            be = [s.buf(f"e{i}") for i in range(2)]
            bp = [s.buf(f"p{i}") for i in range(2)]
            brec = [s.buf(f"rec{i}") for i in range(2)]
            by = s.buf("y", s.dsem("nay"))
            s.dma("sp", q_sb[:], qaT, W=[bq])
            s.dma("act", k_sb[:], kaT, W=[bk])
            s.dma("sp", v_sb[:, :, 0:64], vaP, W=[bv])
            s.dma("act", bt_sb[:], btT.rearrange("p e q -> p (e q)"), W=[bbt])
            s.op("pool", lambda e: e.memset(v_sb[:, :, 64:65], 1.0), W=[bv])
            s.op("act", lambda e: e.activation(out=eb_sb[:], in_=bt_sb[:], func=AF.Exp), R=[bbt], W=[beb])
            lists = na_tile_lists()
            for m in range(NKT):
                sl = m % 2
                if m < 64:
                    kts, eb0 = lists[m]
                else:
                    kts, eb0 = [], 0
                nl = len(kts)
                bA = bps[2 * sl]; bB = bps[2 * sl + 1]; bAcc = bps[4 + sl]
                qs = q_sb[:, m * 128:(m + 1) * 128]
                for ii, n in enumerate(kts):
                    bank = 2 * sl + (0 if ii < 4 else 1)
                    col = (ii % 4) * 128
                    mm(s, ps[:, bank, col:col + 128], k_sb[:, n * 128:(n + 1) * 128], qs, True, True,
                       R=[bk, bq], W=[bps[bank]])
                for ci in range(2):
                    n = 64 + ci
                    mm(s, ps[:, 2 * sl + 1, 128 + ci * 128:256 + ci * 128], k_sb[:, n * 128:(n + 1) * 128], qs,
                       True, True, R=[bk, bq], W=[bB])
                if nl:
                    na_ = min(nl, 4) * 128
                    s.op("act", lambda e: e.activation(out=e_sb[:, sl, 0:na_], in_=ps[:, 2 * sl, 0:na_], func=AF.Exp,
                                                       scale=0.125), R=[bA], W=[be[sl]])
                    if nl == 5:
                        s.op("act", lambda e: e.activation(out=e_sb[:, sl, 512:640], in_=ps[:, 2 * sl + 1, 0:128],
                                                           func=AF.Exp, scale=0.125), R=[bB], W=[be[sl]])
                s.op("act", lambda e: e.activation(out=p_sb[:, sl, 640:896], in_=ps[:, 2 * sl + 1, 128:384],
                                                   func=AF.Exp, scale=0.125), R=[bB], W=[bp[sl]])
                if nl:
                    s.op("dve", lambda e: e.tensor_tensor(out=p_sb[:, sl, 0:nl * 128], in0=e_sb[:, sl, 0:nl * 128],
                                                          in1=eb_sb[:, eb0 * 128:(eb0 + nl) * 128], op=ALU.mult),
                         R=[be[sl], beb], W=[bp[sl]])
                tiles = [(ii * 128, n) for ii, n in enumerate(kts)] + [(640, 64), (768, 65)]
                for ti, (pc, n) in enumerate(tiles):
                    mm(s, ps[:, 4 + sl, 0:65], p_sb[:, sl, pc:pc + 128], v_sb[:, n, :], ti == 0, ti == len(tiles) - 1,
                       R=[bp[sl], bv], W=[bAcc])
                s.op("dve", lambda e: e.reciprocal(out=rec_sb[:, sl:sl + 1], in_=ps[:, 4 + sl, 64:65]),
                     R=[bAcc], W=[brec[sl]])
                s.op("dve", lambda e: e.tensor_scalar(out=y_sb[:, m, :], in0=ps[:, 4 + sl, 0:64],
                                                      scalar1=rec_sb[:, sl:sl + 1], scalar2=None, op0=ALU.mult),
                     R=[bAcc, brec[sl]], W=[by])
            s.dma("sp", ynaP, y_sb[:], R=[by])
            s.barrier([by])

        with (nc.sbuf_tensor("df_q0_sb", [32, NTOK], BF16) as q0_sb,
              nc.sbuf_tensor("df_q1_sb", [32, NTOK], BF16) as q1_sb,
              nc.sbuf_tensor("df_k0_sb", [32, NTOK], BF16) as k0_sb,
              nc.sbuf_tensor("df_k1_sb", [32, NTOK], BF16) as k1_sb,
              nc.sbuf_tensor("df_v_sb", [128, NKT, 65], BF16) as v_sb,
              nc.sbuf_tensor("df_p_sb", [128, 4, 512], BF16) as p_sb,
              nc.sbuf_tensor("df_y_sb", [128, NKT, 64], BF16) as y_sb,
              nc.sbuf_tensor("df_dl_sb", [128, 128], F32) as dl_sb,
              nc.sbuf_tensor("df_g_sb", [128, 64], F32) as g_sb,
              nc.sbuf_tensor("df_lam_sb", [128, 4], F32) as lam_sb,
              nc.sbuf_tensor("df_r_sb", [128, 2, 2, 4], F32) as r_sb,
              nc.sbuf_tensor("df_ss_sb", [128, 2, 4], F32) as ss_sb,
              nc.sbuf_tensor("df_o_sb", [128, 2, 4, 64], F32) as o_sb,
              nc.sbuf_tensor("df_t_sb", [128, 2, 4, 64], F32) as t_sb,
              nc.sbuf_tensor("df_junk_sb", [128, 64], F32) as junk_sb):
            ld = s.dsem("dfld")
            qs_ = [q0_sb, q1_sb]; ks_ = [k0_sb, k1_sb]
            bq = s.buf("q", ld); bk = s.buf("k", ld); bv = s.buf("v", ld); bdl = s.buf("dl", ld); bg = s.buf("g", ld)
            blam = s.buf("lam")
            bp = [s.buf(f"p{i}") for i in range(4)]
            by = s.buf("y", s.dsem("dfy"))
            br = [s.buf(f"r{i}") for i in range(2)]
            bss = [s.buf(f"ss{i}") for i in range(2)]
            bo = [s.buf(f"o{i}") for i in range(2)]
            bt = [s.buf(f"t{i}") for i in range(2)]
            bj = s.buf("junk")
            for c in range(2):
                s.dma("sp", qs_[c][:], qdT[c], W=[bq])
                s.dma("act", ks_[c][:], kdT[c], W=[bk])
            s.dma("sp", v_sb[:, :, 0:64], vdP, W=[bv])
            s.dma("act", dl_sb[:], dlam, W=[bdl])
            s.dma("act", g_sb[:], dg, W=[bg])
            s.op("pool", lambda e: e.memset(v_sb[:, :, 64:65], 1.0), W=[bv])
            s.op("dve", lambda e: e.scalar_tensor_tensor(out=junk_sb[:, 0:32], in0=dl_sb[:, 0:32], scalar=1.0,
                                                         in1=dl_sb[:, 32:64], op0=ALU.mult, op1=ALU.mult,
                                                         accum_out=lam_sb[:, 0:1]), R=[bdl], W=[bj, blam])
            s.op("dve", lambda e: e.scalar_tensor_tensor(out=junk_sb[:, 0:32], in0=dl_sb[:, 64:96], scalar=1.0,
                                                         in1=dl_sb[:, 96:128], op0=ALU.mult, op1=ALU.mult,
                                                         accum_out=lam_sb[:, 1:2]), R=[bdl, bj], W=[bj, blam])
            s.op("act", lambda e: e.activation(out=lam_sb[:, 0:2], in_=lam_sb[:, 0:2], func=AF.Exp), R=[blam], W=[blam])
            s.op("dve", lambda e: e.tensor_tensor(out=lam_sb[:, 2:3], in0=lam_sb[:, 1:2], in1=lam_sb[:, 0:1],
                                                  op=ALU.subtract), R=[blam], W=[blam])
            s.op("dve", lambda e: e.tensor_scalar(out=lam_sb[:, 2:3], in0=lam_sb[:, 2:3], scalar1=-lam_init,
                                                  scalar2=None, op0=ALU.add), R=[blam], W=[blam])
            s.op("dve", lambda e: e.tensor_scalar(out=g_sb[:], in0=g_sb[:], scalar1=1.0 - lam_init, scalar2=None,
                                                  op0=ALU.mult), R=[bg], W=[bg])
            qblocks = [(512 * i, 512, list(range(NKT))) for i in range(16)] + [(S, 256, [64, 65])]
            LA = 2
            sc = 32 ** -0.5
            gstep = 0
            for qi, (q0, nq, kts) in enumerate(qblocks):
                par = qi % 2
                nsub = nq // 128
                steps = [(kt, c) for kt in kts for c in range(2)]
                ns = len(steps)
                started = [False, False]
                for i in range(ns + LA):
                    if i < ns:
                        kt, c = steps[i]
                        g = gstep + i
                        mm(s, ps[:, g % 4, 0:nq], ks_[c][:, kt * 128:(kt + 1) * 128], qs_[c][:, q0:q0 + nq], True, True,
                           R=[bk, bq], W=[bps[g % 4]])
                        s.op("act", lambda e: e.activation(out=p_sb[:, g % 4, 0:nq], in_=ps[:, g % 4, 0:nq],
                                                           func=AF.Exp, scale=sc), R=[bps[g % 4]], W=[bp[g % 4]])
                    if i >= LA:
                        kt, c = steps[i - LA]
                        g = gstep + i - LA
                        bank = 4 + 2 * par + c
                        for sub in range(nsub):
                            st = not started[c]
                            started[c] = True
                            s.op("pe", lambda e: e.matmul(ps[:, bank, sub * 65:(sub + 1) * 65],
                                                          p_sb[:, g % 4, sub * 128:(sub + 1) * 128], v_sb[:, kt, :],
                                                          start=st, stop=(kt == kts[-1]), skip_group_check=True),
                                 R=[bp[g % 4], bv], W=[bps[bank]])
                gstep += ns
                a0 = ps[:, 4 + 2 * par, 0:260].rearrange("p (s e) -> p s e", e=65)
                a1 = ps[:, 5 + 2 * par, 0:260].rearrange("p (s e) -> p s e", e=65)
                b0 = bps[4 + 2 * par]; b1 = bps[5 + 2 * par]
                s.op("dve", lambda e: e.reciprocal(out=r_sb[:, par, 0, 0:nsub], in_=a0[:, 0:nsub, 64]), R=[b0], W=[br[par]])
                s.op("dve", lambda e: e.reciprocal(out=r_sb[:, par, 1, 0:nsub], in_=a1[:, 0:nsub, 64]), R=[b1], W=[br[par]])
                s.op("dve", lambda e: e.tensor_scalar(out=r_sb[:, par, 1, 0:nsub], in0=r_sb[:, par, 1, 0:nsub],
                                                      scalar1=lam_sb[:, 2:3], scalar2=None, op0=ALU.mult),
                     R=[br[par], blam], W=[br[par]])
                for sub in range(nsub):
                    s.op("dve", lambda e: e.tensor_scalar(out=t_sb[:, par, sub, :], in0=a1[:, sub, 0:64],
                                                          scalar1=r_sb[:, par, 1, sub:sub + 1], scalar2=None,
                                                          op0=ALU.mult), R=[b1, br[par]], W=[bt[par]])
                    s.op("dve", lambda e: e.scalar_tensor_tensor(out=o_sb[:, par, sub, :], in0=a0[:, sub, 0:64],
                                                                 scalar=r_sb[:, par, 0, sub:sub + 1],
                                                                 in1=t_sb[:, par, sub, :], op0=ALU.mult, op1=ALU.add),
                         R=[b0, br[par], bt[par]], W=[bo[par]])
                    s.op("dve", lambda e: e.scalar_tensor_tensor(out=junk_sb[:], in0=o_sb[:, par, sub, :], scalar=1.0,
                                                                 in1=o_sb[:, par, sub, :], op0=ALU.mult, op1=ALU.mult,
                                                                 accum_out=ss_sb[:, par, sub:sub + 1]),
                         R=[bo[par], bj], W=[bj, bss[par]])
                s.op("act", lambda e: e.activation(out=ss_sb[:, par, 0:nsub], in_=ss_sb[:, par, 0:nsub], func=AF.Ln,
                                                   scale=1.0 / 64, bias=EPS), R=[bss[par]], W=[bss[par]])
                s.op("act", lambda e: e.activation(out=ss_sb[:, par, 0:nsub], in_=ss_sb[:, par, 0:nsub], func=AF.Exp,
                                                   scale=-0.5), R=[bss[par]], W=[bss[par]])
                for sub in range(nsub):
                    s.op("dve", lambda e: e.scalar_tensor_tensor(out=y_sb[:, q0 // 128 + sub, :], in0=o_sb[:, par, sub, :],
                                                                 scalar=ss_sb[:, par, sub:sub + 1], in1=g_sb[:],
                                                                 op0=ALU.mult, op1=ALU.mult),
                         R=[bo[par], bss[par], bg], W=[by])
            s.dma("sp", ydfP, y_sb[:], R=[by])
            s.finish([by])
    return nc


def tileP(a):
    return np.ascontiguousarray(a.reshape(NKT, 128, a.shape[1]).transpose(1, 0, 2))


def untileP(a):
    return a.transpose(1, 0, 2).reshape(NTOK, a.shape[2])


_NA_IDX = None


def run_p2(nc2, l, fmb, fmf, tm, inp):
    global _NA_IDX
    if _NA_IDX is None:
        _NA_IDX = na_bias_index()
    in_maps = []
    for i in range(NCORES):
        b, j = i // 4, i % 4
        xr = fmf[b, 128 * j:128 * (j + 1)]
        xf = np.concatenate([xr[:, S:], xr[:, :S]], axis=1)
        xb = np.concatenate([xr[:, S:][:, ::-1], xr[:, :S][:, ::-1]], axis=1)
        wbd = np.zeros((4, 128, 128), np.float32)
        rgv = np.zeros((128, 2, 8), np.float32)
        ch = slice(128 * j, 128 * (j + 1))
        for dr in range(2):
            for gi, wk in enumerate(("rg_w_r", "rg_w_i")):
                for bb in range(2):
                    wbd[dr * 2 + gi, 64 * bb:64 * (bb + 1), 64 * bb:64 * (bb + 1)] = inp[wk][l, dr, 2 * j + bb]
            rgv[:, dr, 0:4] = inp["rg_conv_w"][l][:, ch].T
            rgv[:, dr, 4] = inp["rg_conv_b"][l][ch]
            rgv[:, dr, 5] = inp["rg_b_r"][l, dr, ch]
            rgv[:, dr, 6] = inp["rg_b_i"][l, dr, ch]
            rgv[:, dr, 7] = inp["rg_lambda"][l, dr, ch]
        rext = np.concatenate([inp["na_rpb"][l, j].ravel(), np.array([-30000.0], np.float32)])
        bt = rext[_NA_IDX]
        in_maps.append({
            "xrg": np.ascontiguousarray(np.stack([xf, xb])), "wbd": wbd, "rgv": rgv,
            "qaT": np.ascontiguousarray(fmb[b, 64 * j:64 * (j + 1)]),
            "kaT": np.ascontiguousarray(fmb[b, 256 + 64 * j:256 + 64 * (j + 1)]),
            "vaP": tileP(tm[b][:, 64 * j:64 * (j + 1)]),
            "btT": np.ascontiguousarray(bt.transpose(1, 0, 2)),
            "qdT": np.ascontiguousarray(fmb[b, 512 + 64 * j:512 + 64 * (j + 1)].reshape(2, 32, NTOK)),
            "kdT": np.ascontiguousarray(fmb[b, 768 + 64 * j:768 + 64 * (j + 1)].reshape(2, 32, NTOK)),
            "vdP": tileP(tm[b][:, 256 + 64 * j:256 + 64 * (j + 1)]),
            "dlam": np.ascontiguousarray(np.tile(inp["diff_lambda"][l].reshape(1, 128), (128, 1))),
            "dg": np.ascontiguousarray(np.tile(inp["diff_subln_g"][l].reshape(1, 64), (128, 1))),
        })
    res = run_bass_kernel_spmd(nc2, in_maps, core_ids=list(range(NCORES)))
    yna = np.zeros((B, NTOK, 256), ml_dtypes.bfloat16)
    ydf = np.zeros((B, NTOK, 256), ml_dtypes.bfloat16)
    hf = np.zeros((B, 512, NTOK), np.float32)
    hb = np.zeros((B, 512, NTOK), np.float32)
    for i in range(NCORES):
        b, j = i // 4, i % 4
        r = res.results[i]
        yna[b, :, 64 * j:64 * (j + 1)] = untileP(r["ynaP"])
        ydf[b, :, 64 * j:64 * (j + 1)] = untileP(r["ydfP"])
        h = r["hout"]
        hf[b, 128 * j:128 * (j + 1), S:] = h[0][:, :L]
        hf[b, 128 * j:128 * (j + 1), :S] = h[0][:, L:]
        hb[b, 128 * j:128 * (j + 1), S:] = h[1][:, :L][:, ::-1]
        hb[b, 128 * j:128 * (j + 1), :S] = h[1][:, L:][:, ::-1]
    return yna, ydf, hf, hb


NEXP = 32
GELU_C = 1.5957691216057308


def build_p3(final):
    nc = bass.Bass("TRN2", target_bir_lowering=False)
    din = lambda n, shp, dt: nc.dram_tensor(n, shp, dt, kind="ExternalInput").ap()
    xT = din("xT", [128, 8, NT1], F32)
    nadf = din("nadf", [128, 4, NT1], BF16)
    hg = din("hg", [128, 12, NT1], F32)
    wout = din("wout", [D, D], F32)
    mod = din("mod", [128, 10, 8], F32)
    wge = din("wge", [128, 8, 36], F32)
    bge = din("bge", [128, 36], F32)
    selc = din("selc", [32, NEXP * 128], BF16)
    ident = din("ident", [128, 128], F32)
    w1 = din("w1", [NEXP, D, 512], F32)
    w3 = din("w3", [NEXP, D, 512], F32)
    w2 = din("w2", [NEXP, 512, D], F32)
    xo = nc.dram_tensor("xo", [128, 8, NT1], F32, kind="ExternalOutput").ap()
    s = Sched(nc)
    wov = wout.rearrange("(kc p) n -> p kc n", p=128)
    with (nc.psum_tensor("ps", [128, 8, 512], F32) as ps,
          nc.sbuf_tensor("x_sb", [128, 8, NT1], F32) as x_sb,
          nc.sbuf_tensor("mod_sb", [128, 10, 8], F32) as mod_sb,
          nc.sbuf_tensor("hl2_sb", [128, 8, NT1], BF16) as hl2_sb,
          nc.sbuf_tensor("wdt_sb", [32, 2, NT1], BF16) as wdt_sb,
          nc.sbuf_tensor("ones_sb", [128, 128], BF16) as ones_sb):
        bps = [s.buf(f"ps{i}") for i in range(8)]
        cs = s.dsem("const")
        bxb = [s.buf(f"x{i}", s.dsem(f"x{i}")) for i in range(len(BLKS1))]
        bmod = s.buf("mod", cs)
        bhl2 = [s.buf(f"hl2_{i}") for i in range(len(BLKS1))]
        bwdt = [s.buf(f"wdt{i}") for i in range(len(BLKS1))]
        bones = s.buf("ones")
        s.dma("sp", mod_sb[:], mod, W=[bmod])
        for bi, (t0, n) in enumerate(BLKS1):
            s.dma("sp", x_sb[:, :, t0:t0 + n], xT[:, :, t0:t0 + n], W=[bxb[bi]])
        s.op("pool", lambda e: e.memset(ones_sb[:], 1.0), W=[bones])

        with (nc.sbuf_tensor("s1_mix", [128, 8, NT1], BF16) as mix_sb,
              nc.sbuf_tensor("s1_wobf", [128, 8, D], BF16) as wo_bf,
              nc.sbuf_tensor("s1_wost", [128, 2, D], F32) as wo_st,
              nc.sbuf_tensor("s1_hg", [128, 1, 12, 512], F32) as hg_sb,
              nc.sbuf_tensor("s1_t", [128, 4, 512], F32) as t_sb):
            bmixl = s.buf("mixl", s.dsem("mixl"))
            bmix = [s.buf(f"mix{i}") for i in range(len(BLKS1))]
            bwost = [s.buf(f"wost{i}", s.dsem(f"wost{i}")) for i in range(2)]
            bwobf = [s.buf(f"wobf{k}") for k in range(8)]
            bhg = [s.buf(f"hg{i}", s.dsem(f"hg{i}")) for i in range(2)]
            bt = [s.buf(f"t{i}") for i in range(4)]
            s.dma("act", mix_sb[:, 0:2, :], nadf[:, 0:2, :], W=[bmixl])
            s.dma("act", mix_sb[:, 6:8, :], nadf[:, 2:4, :], W=[bmixl])
            for k in range(8):
                sl = k % 2
                s.dma("act", wo_st[:, sl, :], wov[:, k, :], W=[bwost[sl]])
                s.op("pool", lambda e: e.tensor_copy(out=wo_bf[:, k, :], in_=wo_st[:, sl, :]), R=[bwost[sl]], W=[bwobf[k]])
            for bi, (t0, n) in enumerate(BLKS1):
                sl = 0
                s.dma("sp", hg_sb[:, sl, :, 0:n], hg[:, :, t0:t0 + n], W=[bhg[sl]])
                for ch in range(4):
                    hf_ = hg_sb[:, sl, ch, 0:n]; hb_ = hg_sb[:, sl, 4 + ch, 0:n]; gr_ = hg_sb[:, sl, 8 + ch, 0:n]
                    s.op("dve", lambda e: e.tensor_tensor(out=t_sb[:, 0, 0:n], in0=hf_, in1=hb_, op=ALU.add),
                         R=[bhg[sl]], W=[bt[0]])
                    s.op("dve", lambda e: e.tensor_tensor(out=t_sb[:, 1, 0:n], in0=gr_, in1=gr_, op=ALU.mult),
                         R=[bhg[sl]], W=[bt[1]])
                    s.op("dve", lambda e: e.tensor_scalar(out=t_sb[:, 1, 0:n], in0=t_sb[:, 1, 0:n], scalar1=0.044715,
                                                          scalar2=1.0, op0=ALU.mult, op1=ALU.add), R=[bt[1]], W=[bt[1]])
                    s.op("pool", lambda e: e.tensor_tensor(out=t_sb[:, 2, 0:n], in0=t_sb[:, 1, 0:n], in1=gr_, op=ALU.mult),
                         R=[bt[1], bhg[sl]], W=[bt[2]])
                    s.op("act", lambda e: e.activation(out=t_sb[:, 2, 0:n], in_=t_sb[:, 2, 0:n], func=AF.Sigmoid,
                                                       scale=GELU_C), R=[bt[2]], W=[bt[2]])
                    s.op("pool", lambda e: e.tensor_tensor(out=t_sb[:, 3, 0:n], in0=t_sb[:, 2, 0:n], in1=gr_, op=ALU.mult),
                         R=[bt[2], bhg[sl]], W=[bt[3]])
                    s.op("pool", lambda e: e.tensor_tensor(out=mix_sb[:, 2 + ch, t0:t0 + n], in0=t_sb[:, 3, 0:n],
                                                           in1=t_sb[:, 0, 0:n], op=ALU.mult),
                         R=[bt[3], bt[0]], W=[bmix[bi]])
            pi = 0
            for bi, (t0, n) in enumerate(BLKS1):
                garow = 5 if bi == 4 else 1
                for dc in range(8):
                    pb = pi % 8; pi += 1
                    for k in range(8):
                        mm(s, ps[:, pb, 0:n], wo_bf[:, k, dc * 128:(dc + 1) * 128], mix_sb[:, k, t0:t0 + n],
                           k == 0, k == 7, R=[bwobf[k], bmix[bi], bmixl], W=[bps[pb]])
                    s.op("dve", lambda e: e.scalar_tensor_tensor(out=x_sb[:, dc, t0:t0 + n], in0=ps[:, pb, 0:n],
                                                                 scalar=mod_sb[:, garow, dc:dc + 1],
                                                                 in1=x_sb[:, dc, t0:t0 + n], op0=ALU.mult, op1=ALU.add),
                         R=[bps[pb], bmod, bxb[bi]], W=[bxb[bi]])
            s.barrier()

        with (nc.sbuf_tensor("s2_sq", [128, 8, 512], BF16) as sq_sb,
              nc.sbuf_tensor("s2_rstd", [128, 2, 512], F32) as rstd_sb,
              nc.sbuf_tensor("s2_tmp", [128, 4, 512], F32) as tmp_sb,
              nc.sbuf_tensor("s2_hf", [128, 2, 8, 512], F32) as hf_sb,
              nc.sbuf_tensor("s2_ab", [128, 2, 8], F32) as ab_sb,
              nc.sbuf_tensor("s2_wge", [128, 8, 36], F32) as wge_sb,
              nc.sbuf_tensor("s2_bge", [128, 36], F32) as bge_sb,
              nc.sbuf_tensor("s2_id", [128, 128], F32) as id_sb,
              nc.sbuf_tensor("s2_rt", [128, 2, 128], F32) as rt_sb):
            bsq = s.buf("sq"); brs = [s.buf(f"rs{i}") for i in range(2)]
            btmp = [s.buf(f"tmp{i}") for i in range(4)]
            bhf = [s.buf(f"hf{i}") for i in range(2)]
            bab = s.buf("ab")
            bwge = s.buf("wge", cs); bbge = s.buf("bge", cs); bid = s.buf("id", cs)
            brt = [s.buf(f"rt{i}") for i in range(2)]
            s.dma("act", wge_sb[:], wge, W=[bwge])
            s.dma("act", bge_sb[:], bge, W=[bbge])
            s.dma("act", id_sb[:], ident, W=[bid])
            for t in range(2):
                s.op("dve", lambda e: e.scalar_tensor_tensor(
                    out=ab_sb[:, t, :], in0=mod_sb[:, 2 + 4 * t, :], scalar=1.0, in1=mod_sb[:, 0, :],
                    op0=ALU.add, op1=ALU.mult), R=[bmod], W=[bab])
            ti_g = 0
            for bi, (t0, n) in enumerate(BLKS1):
                sl = bi % 2
                isctx = bi == 4
                s.op("act", lambda e: e.activation(out=sq_sb[:, :, 0:n], in_=x_sb[:, :, t0:t0 + n], func=AF.Square),
                     R=[bxb[bi]], W=[bsq])
                for k in range(8):
                    mm(s, ps[:, sl, 0:n], ones_sb[:], sq_sb[:, k, 0:n], k == 0, k == 7, R=[bones, bsq], W=[bps[sl]])
                s.op("act", lambda e: e.activation(out=rstd_sb[:, sl, 0:n], in_=ps[:, sl, 0:n], func=AF.Sqrt,
                                                   scale=1.0 / D, bias=EPS), R=[bps[sl]], W=[brs[sl]])
                s.op("dve", lambda e: e.reciprocal(out=rstd_sb[:, sl, 0:n], in_=rstd_sb[:, sl, 0:n]),
                     R=[brs[sl]], W=[brs[sl]])
                ai = 1 if isctx else 0
                shrow = 7 if isctx else 3
                for k in range(8):
                    tb = k % 4
                    s.op("dve", lambda e: e.scalar_tensor_tensor(
                        out=tmp_sb[:, tb, 0:n], in0=x_sb[:, k, t0:t0 + n], scalar=ab_sb[:, ai, k:k + 1],
                        in1=rstd_sb[:, sl, 0:n], op0=ALU.mult, op1=ALU.mult),
                        R=[bxb[bi], bab, brs[sl]], W=[btmp[tb]])
                    s.op("act", lambda e: e.activation(out=hf_sb[:, sl, k, 0:n], in_=tmp_sb[:, tb, 0:n],
                                                       func=AF.Identity, bias=mod_sb[:, shrow, k:k + 1], scale=1.0),
                         R=[btmp[tb], bmod], W=[bhf[sl]])
                s.op("pool", lambda e: e.tensor_copy(out=hl2_sb[:, :, t0:t0 + n], in_=hf_sb[:, sl, :, 0:n]),
                     R=[bhf[sl]], W=[bhl2[bi]])
                for tt in range((n + 127) // 128):
                    c0 = tt * 128
                    m = min(128, n - c0)
                    rs_ = ti_g % 2; ti_g += 1
                    pb = 2 + rs_
                    rt = rt_sb[0:m, rs_, :]
                    brr = brt[rs_]
                    for k in range(8):
                        mm(s, ps[0:m, pb, 0:36], hf_sb[:, sl, k, c0:c0 + m], wge_sb[:, k, :], k == 0, k == 7,
                           R=[bhf[sl], bwge], W=[bps[pb]])
                    V = lambda eng, fn, R_=(), W_=(): s.op(eng, fn, R=[brr] + list(R_), W=[brr] + list(W_))
                    lg = rt[:, 0:36]
                    s.op("dve", lambda e: e.tensor_tensor(out=lg, in0=ps[0:m, pb, 0:36], in1=bge_sb[0:m, :], op=ALU.add),
                         R=[bps[pb], bbge], W=[brr])
                    gmax = rt[:, 36:37]; ngmax = rt[:, 37:38]; sume = rt[:, 38:39]; gtop = rt[:, 39:40]
                    eg = rt[:, 40:44]; ohg = rt[:, 44:48]; sel = rt[:, 48:56]; top8 = rt[:, 56:64]
                    dd = rt[:, 64:65]; ed = rt[:, 65:66]; w1_ = rt[:, 66:67]; wt1 = rt[:, 67:68]; wt2 = rt[:, 68:69]
                    ea = rt[:, 72:80]; eb_ = rt[:, 80:88]; wd = rt[:, 96:128]
                    V("dve", lambda e: e.reduce_max(out=gmax, in_=lg[:, 0:4], axis=AX.X))
                    V("dve", lambda e: e.tensor_scalar(out=ngmax, in0=gmax, scalar1=-1.0, scalar2=None, op0=ALU.mult))
                    V("act", lambda e: e.activation(out=eg, in_=lg[:, 0:4], func=AF.Exp, bias=ngmax, scale=1.0,
                                                    accum_out=sume))
                    V("dve", lambda e: e.reciprocal(out=gtop, in_=sume))
                    V("dve", lambda e: e.tensor_scalar(out=ohg, in0=lg[:, 0:4], scalar1=gmax, scalar2=None,
                                                       op0=ALU.is_equal))
                    V("dve", lambda e: e.tensor_scalar(out=sel, in0=lg[:, 4:12], scalar1=ohg[:, 0:1], scalar2=None,
                                                       op0=ALU.mult))
                    for g in range(1, 4):
                        V("dve", lambda e: e.scalar_tensor_tensor(out=sel, in0=lg[:, 4 + 8 * g:12 + 8 * g],
                                                                  scalar=ohg[:, g:g + 1], in1=sel,
                                                                  op0=ALU.mult, op1=ALU.add))
                    V("dve", lambda e: e.max(out=top8, in_=sel))
                    V("dve", lambda e: e.tensor_tensor(out=dd, in0=top8[:, 1:2], in1=top8[:, 0:1], op=ALU.subtract))
                    V("act", lambda e: e.activation(out=ed, in_=dd, func=AF.Exp))
                    V("dve", lambda e: e.tensor_scalar(out=w1_, in0=ed, scalar1=1.0, scalar2=None, op0=ALU.add))
                    V("dve", lambda e: e.reciprocal(out=w1_, in_=w1_))
                    V("dve", lambda e: e.tensor_tensor(out=wt1, in0=w1_, in1=gtop, op=ALU.mult))
                    V("dve", lambda e: e.tensor_tensor(out=wt2, in0=wt1, in1=ed, op=ALU.mult))
                    V("dve", lambda e: e.tensor_scalar(out=ea, in0=sel, scalar1=top8[:, 0:1], scalar2=wt1,
                                                       op0=ALU.is_equal, op1=ALU.mult))
                    V("dve", lambda e: e.tensor_scalar(out=eb_, in0=sel, scalar1=top8[:, 1:2], scalar2=wt2,
                                                       op0=ALU.is_equal, op1=ALU.mult))
                    V("dve", lambda e: e.tensor_tensor(out=ea, in0=ea, in1=eb_, op=ALU.add))
                    for g in range(4):
                        V("dve", lambda e: e.tensor_scalar(out=wd[:, 8 * g:8 * g + 8], in0=ea, scalar1=ohg[:, g:g + 1],
                                                           scalar2=None, op0=ALU.mult))
                    pt = 4 + rs_
                    s.op("pe", lambda e: e.transpose(ps[0:32, pt, 0:m], wd, id_sb[0:m, 0:m]), R=[brr, bid], W=[bps[pt]])
                    s.op("act", lambda e: e.copy(out=wdt_sb[:, 0, t0 + c0:t0 + c0 + m], in_=ps[0:32, pt, 0:m]),
                         R=[bps[pt]], W=[bwdt[bi]])
                    s.op("dve", lambda e: e.tensor_tensor(out=wdt_sb[:, 1, t0 + c0:t0 + c0 + m], in0=ps[0:32, pt, 0:m],
                                                          in1=wdt_sb[:, 0, t0 + c0:t0 + c0 + m], op=ALU.subtract),
                         R=[bps[pt], bwdt[bi]], W=[bwdt[bi]])
            s.barrier()

        with (nc.sbuf_tensor("s3_st", [128, 3, 2048], F32) as st_sb,
              nc.sbuf_tensor("s3_wb", [128, 2, 6, 2048], BF16) as wb_sb,
              nc.sbuf_tensor("s3_sel", [32, NEXP * 128], BF16) as sel_sb,
              nc.sbuf_tensor("s3_wbc", [128, 2, 512], F32) as wbc_sb,
              nc.sbuf_tensor("s3_sg", [128, 2, 512], F32) as sg_sb,
              nc.sbuf_tensor("s3_t", [128, 2, 512], F32) as t3_sb,
              nc.sbuf_tensor("s3_g", [128, 2, 4, 512], BF16) as g_sb):
            bst = [s.buf(f"st{i}", s.dsem(f"st{i}")) for i in range(3)]
            bwb = [[s.buf(f"wb{a}_{p}") for p in range(6)] for a in range(2)]
            bsel = s.buf("sel", cs)
            bwbc = [s.buf(f"wbc{i}") for i in range(2)]
            bsg = [s.buf(f"sg{i}") for i in range(2)]
            bt3 = [s.buf(f"t3{i}") for i in range(2)]
            bg = [s.buf(f"g{i}") for i in range(2)]
            s.dma("act", sel_sb[:], selc, W=[bsel])
            w1v = w1.rearrange("e (kc p) f -> e p kc f", p=128)
            w3v = w3.rearrange("e (kc p) f -> e p kc f", p=128)
            w2v = w2.rearrange("e (fc p) d -> e p fc d", p=128)

            def piece_src(e, p):
                if p < 2:
                    return w1v[e, :, 4 * p:4 * p + 4, :]
                if p < 4:
                    return w3v[e, :, 4 * (p - 2):4 * (p - 2) + 4, :]
                return w2v[e, :, 2 * (p - 4):2 * (p - 4) + 2, :]

            def piece_dma(P):
                e, p = divmod(P, 6)
                if e >= NEXP:
                    return
                sl = P % 3
                dst = st_sb[:, sl, :]
                dst = dst.rearrange("q (a b) -> q a b", a=4) if p < 4 else dst.rearrange("q (a b) -> q a b", a=2)
                s.dma("sp", dst, piece_src(e, p), W=[bst[sl]])

            def piece_cast(P):
                e, p = divmod(P, 6)
                if e >= NEXP:
                    return
                sl = P % 3
                s.op("pool", lambda en: en.tensor_copy(out=wb_sb[:, e % 2, p, :], in_=st_sb[:, sl, :]),
                     R=[bst[sl]], W=[bwb[e % 2][p]])

            for P in range(3):
                piece_dma(P)
            for P in range(6):
                piece_cast(P)
                piece_dma(P + 3)
            gi = 0
            for ex in range(NEXP):
                a = ex % 2
                for bi, (t0, n) in enumerate(BLKS1):
                    if bi < 3:
                        for P in (6 * (ex + 1) + 2 * bi, 6 * (ex + 1) + 2 * bi + 1):
                            piece_cast(P)
                            piece_dma(P + 3)
                    garow = 8 if bi == 4 else 4
                    wr = gi % 2
                    gs = gi % 2
                    gi += 1
                    mm(s, ps[:, 6, 0:n], sel_sb[:, ex * 128:(ex + 1) * 128], wdt_sb[:, 0, t0:t0 + n], True, False,
                       R=[bsel, bwdt[bi]], W=[bps[6]])
                    mm(s, ps[:, 6, 0:n], sel_sb[:, ex * 128:(ex + 1) * 128], wdt_sb[:, 1, t0:t0 + n], False, True,
                       R=[bsel, bwdt[bi]], W=[bps[6]])
                    s.op("act", lambda e: e.copy(out=wbc_sb[:, wr, 0:n], in_=ps[:, 6, 0:n]), R=[bps[6]], W=[bwbc[wr]])
                    for fc in range(4):
                        pr = fc % 2
                        for which in range(2):
                            bank = 2 * pr + which
                            for k in range(8):
                                wv = wb_sb[:, a, 2 * which + k // 4, :].rearrange("q (a b) -> q a b", a=4)
                                mm(s, ps[:, bank, 0:n], wv[:, k % 4, fc * 128:(fc + 1) * 128], hl2_sb[:, k, t0:t0 + n],
                                   k == 0, k == 7, R=[bwb[a][2 * which + k // 4], bhl2[bi]], W=[bps[bank]])
                        s.op("act", lambda e: e.activation(out=sg_sb[:, pr, 0:n], in_=ps[:, 2 * pr, 0:n], func=AF.Silu),
                             R=[bps[2 * pr]], W=[bsg[pr]])
                        s.op("dve", lambda e: e.tensor_tensor(out=t3_sb[:, pr, 0:n], in0=ps[:, 2 * pr + 1, 0:n],
                                                              in1=sg_sb[:, pr, 0:n], op=ALU.mult),
                             R=[bps[2 * pr + 1], bsg[pr]], W=[bt3[pr]])
                        s.op("pool", lambda e: e.tensor_tensor(out=g_sb[:, gs, fc, 0:n], in0=t3_sb[:, pr, 0:n],
                                                               in1=wbc_sb[:, wr, 0:n], op=ALU.mult),
                             R=[bt3[pr], bwbc[wr]], W=[bg[gs]])
                    for dc in range(8):
                        bank = 4 + dc % 2
                        for fc in range(4):
                            wv = wb_sb[:, a, 4 + fc // 2, :].rearrange("q (a b) -> q a b", a=2)
                            mm(s, ps[:, bank, 0:n], wv[:, fc % 2, dc * 128:(dc + 1) * 128], g_sb[:, gs, fc, 0:n],
                               fc == 0, fc == 3, R=[bwb[a][4 + fc // 2], bg[gs]], W=[bps[bank]])
                        s.op("dve", lambda e: e.scalar_tensor_tensor(out=x_sb[:, dc, t0:t0 + n], in0=ps[:, bank, 0:n],
                                                                     scalar=mod_sb[:, garow, dc:dc + 1],
                                                                     in1=x_sb[:, dc, t0:t0 + n], op0=ALU.mult, op1=ALU.add),
                             R=[bps[bank], bmod, bxb[bi]], W=[bxb[bi]])
            s.barrier()

        with (nc.sbuf_tensor("s4_sq", [128, 8, 512], BF16) as sq_sb,
              nc.sbuf_tensor("s4_rstd", [128, 2, 512], F32) as rstd_sb):
            bsq = s.buf("sq4"); brs = [s.buf(f"rs4{i}") for i in range(2)]
            for bi, (t0, n) in enumerate(BLKS1):
                if final:
                    sl = bi % 2
                    s.op("act", lambda e: e.activation(out=sq_sb[:, :, 0:n], in_=x_sb[:, :, t0:t0 + n], func=AF.Square),
                         R=[bxb[bi]], W=[bsq])
                    for k in range(8):
                        mm(s, ps[:, sl, 0:n], ones_sb[:], sq_sb[:, k, 0:n], k == 0, k == 7, R=[bones, bsq], W=[bps[sl]])
                    s.op("act", lambda e: e.activation(out=rstd_sb[:, sl, 0:n], in_=ps[:, sl, 0:n], func=AF.Sqrt,
                                                       scale=1.0 / D, bias=EPS), R=[bps[sl]], W=[brs[sl]])
                    s.op("dve", lambda e: e.reciprocal(out=rstd_sb[:, sl, 0:n], in_=rstd_sb[:, sl, 0:n]),
                         R=[brs[sl]], W=[brs[sl]])
                    for k in range(8):
                        s.op("dve", lambda e: e.scalar_tensor_tensor(
                            out=x_sb[:, k, t0:t0 + n], in0=x_sb[:, k, t0:t0 + n], scalar=mod_sb[:, 9, k:k + 1],
                            in1=rstd_sb[:, sl, 0:n], op0=ALU.mult, op1=ALU.mult),
                            R=[bxb[bi], bmod, brs[sl]], W=[bxb[bi]])
                s.dma("sp", xo[:, :, t0:t0 + n], x_sb[:, :, t0:t0 + n], R=[bxb[bi]])
            s.finish(bxb)
    return nc


def run_p3(nc3, l, xl, xc, yna, ydf, hf, hb, fmf, mods_l, inp, g_final):
    wge = np.concatenate([inp["router_w_group"][l], inp["router_w_expert"][l]], axis=1)
    wge = np.ascontiguousarray(wge.reshape(8, 128, 36).transpose(1, 0, 2))
    bge = np.concatenate([inp["router_b_group"][l], inp["router_b_expert"][l]])
    bge = np.ascontiguousarray(np.tile(bge[None, :], (128, 1))).astype(np.float32)
    selc = np.zeros((32, NEXP, 128), np.float32)
    for e in range(NEXP):
        selc[e, e, :] = 1.0
    selc = selc.reshape(32, NEXP * 128).astype(ml_dtypes.bfloat16)
    ident = np.eye(128, dtype=np.float32)
    in_maps = []
    for i in range(NCORES):
        b, j = i // 4, i % 4
        lat = slice(2048 * j, 2048 * (j + 1))
        ctxs = slice(S + 64 * j, S + 64 * (j + 1))
        xx = np.concatenate([xl[b, lat], xc[b, 64 * j:64 * (j + 1)]], axis=0)
        na = np.concatenate([yna[b, lat], yna[b, ctxs]], axis=0)
        df = np.concatenate([ydf[b, lat], ydf[b, ctxs]], axis=0)
        nadf = np.concatenate([na, df], axis=1)
        nadf = np.ascontiguousarray(nadf.T.reshape(4, 128, NT1).transpose(1, 0, 2))
        def tk(a):
            aa = np.concatenate([a[:, lat], a[:, ctxs]], axis=1)
            return aa.reshape(4, 128, NT1).transpose(1, 0, 2)
        hgt = np.ascontiguousarray(np.concatenate([tk(hf[b]), tk(hb[b]), tk(fmf[b, 512:1024])], axis=1))
        m = mods_l
        rows = [inp["g_ffn"][l], m[b, 2048:3072], m[b, 4096:5120], m[b, 3072:4096], m[b, 5120:6144],
                m[2, 2048:3072], m[2, 4096:5120], m[2, 3072:4096], m[2, 5120:6144], g_final]
        mod = np.ascontiguousarray(np.stack([vec_pk(r) for r in rows], axis=1)).astype(np.float32)
        in_maps.append({"xT": chunkT(xx), "nadf": nadf, "hg": hgt, "wout": inp["w_out"][l], "mod": mod,
                        "wge": wge, "bge": bge, "selc": selc, "ident": ident,
                        "w1": inp["moe_w1"][l], "w3": inp["moe_w3"][l], "w2": inp["moe_w2"][l]})
    res = run_bass_kernel_spmd(nc3, in_maps, core_ids=list(range(NCORES)))
    xl2 = np.zeros_like(xl); xc2 = np.zeros_like(xc)
    for i in range(NCORES):
        b, j = i // 4, i % 4
        o = res.results[i]["xo"].transpose(1, 0, 2).reshape(D, NT1).T
        xl2[b, 2048 * j:2048 * (j + 1)] = o[:2048]
        xc2[b, 64 * j:64 * (j + 1)] = o[2048:]
    return xl2, xc2


def kernel(**inputs):
    inp = {k: np.asarray(v) for k, v in inputs.items()}
    x = np.ascontiguousarray(inp["x"], dtype=np.float32)
    ctx = np.ascontiguousarray(inp["ctx"], dtype=np.float32)
    mods = run_p0(inp["c"], inp["c_ctx"], inp["w_ada"], inp["b_ada"])
    cosT, sinT = rope_tables()
    xl, xc = x, ctx
    for l in range(DEPTH):
        fmb, fmf, tm = run_p1(build_p1(), xl, xc, mods[l], inp["g_mix"][l], inp["w_in"][l], cosT, sinT)
        lam_init = 0.8 - 0.6 * math.exp(-0.3 * l)
        yna, ydf, hf, hb = run_p2(build_p2(lam_init), l, fmb, fmf, tm, inp)
        xl, xc = run_p3(build_p3(l == DEPTH - 1), l, xl, xc, yna, ydf, hf, hb, fmf, mods[l], inp, inp["g_final"])
    return np.ascontiguousarray(xl, dtype=np.float32)
```

```python
import math
import numpy as np
import ml_dtypes
import concourse.bass as bass
import concourse.mybir as mybir
from concourse.bass_utils import run_bass_kernel_spmd

F32 = mybir.dt.float32
BF16 = mybir.dt.bfloat16
I32 = mybir.dt.int32
U32 = mybir.dt.uint32
AF = mybir.ActivationFunctionType
ALU = mybir.AluOpType
AX = mybir.AxisListType

NCORES = 8
D = 1024
B = 2
S = 8192
L = 256
DEPTH = 4
GRID_W = 64
EPS = 1e-6


class Buf:
    __slots__ = ("name", "w", "r", "dsem")

    def __init__(self, name, dsem=None):
        self.name = name
        self.w = None
        self.r = []
        self.dsem = dsem


class DmaSem:
    def __init__(self, sched, name):
        self.sem = sched.nc.alloc_semaphore(name)
        self.key = ("dma", name)
        self.total = 0
        sched.sems[self.key] = self


class Sched:
    def __init__(self, nc):
        self.nc = nc
        self.eng = {"pe": nc.tensor, "dve": nc.vector, "act": nc.scalar,
                    "pool": nc.gpsimd, "sp": nc.sync}
        self.sems = {}
        self.esem = {}
        self.cnt = {}
        for k in self.eng:
            self.esem[k] = nc.alloc_semaphore("e_" + k)
            self.cnt[k] = 0
        self.seen = {}
        self.nbuf = 0
        self.out_tokens = []

    def buf(self, name=None, dsem=None):
        self.nbuf += 1
        return Buf(name or f"b{self.nbuf}", dsem)

    def dsem(self, name):
        return DmaSem(self, name)

    def _semof(self, key):
        if key[0] == "dma":
            return self.sems[key].sem
        return self.esem[key[0]]

    def _wait(self, engname, deps):
        e = self.eng[engname]
        for key, val in deps.items():
            if key[0] == "dma":
                val = max(val, 0)
            if self.seen.get((engname, key), 0) >= val:
                continue
            self.seen[(engname, key)] = val
            e.wait_ge(self._semof(key), val)

    def _deps(self, R, W):
        deps = {}

        def add(tok):
            if tok is None:
                return
            key, val = tok
            if key[0] == "dma":
                val = self.sems[key].total
            if deps.get(key, 0) < val:
                deps[key] = val
        for b in R:
            add(b.w)
        for b in W:
            add(b.w)
            for t in b.r:
                add(t)
        return deps

    def _commit(self, tok, R, W):
        for b in R:
            b.r.append(tok)
        for b in W:
            b.w = tok
            b.r = []

    def op(self, engname, fn, R=(), W=()):
        deps = self._deps(R, W)
        if engname == "pe":
            deps.pop(("pe",), None)
        self._wait(engname, deps)
        ins = fn(self.eng[engname])
        self.cnt[engname] += 1
        ins.then_inc(self.esem[engname], 1)
        tok = ((engname,), self.cnt[engname])
        self._commit(tok, R, W)
        return tok

    def dma(self, q, out, in_, R=(), W=(), sem=None, **kw):
        deps = self._deps(R, W)
        self._wait(q, deps)
        ds = sem
        if ds is None:
            for b in list(W) + list(R):
                if b.dsem is not None:
                    ds = b.dsem
                    break
        assert ds is not None, "dma needs a DmaSem"
        ins = self.eng[q].dma_start(out=out, in_=in_, **kw)
        ds.total += 16
        ins.then_inc(ds.sem, 16)
        tok = (ds.key, ds.total)
        self._commit(tok, R, W)
        return tok

    def barrier(self, bufs=()):
        deps = {}
        for k in self.eng:
            if self.cnt[k]:
                deps[(k,)] = self.cnt[k]
        for key, ds in self.sems.items():
            if ds.total:
                deps[key] = ds.total
        for k in self.eng:
            d = {kk: v for kk, v in deps.items() if kk != (k,)}
            self._wait(k, d)

    def coll(self, kind, ins, outs, R=(), W=(), groups=None):
        deps = self._deps(R, W)
        self._wait("pool", deps)
        ds = None
        for b in list(W) + list(R):
            if b.dsem is not None:
                ds = b.dsem
                break
        g = groups or [[0, 1, 2, 3], [4, 5, 6, 7]]
        ins_ = self.nc.gpsimd.collective_compute(kind, ALU.bypass, replica_groups=g, ins=ins, outs=outs)
        ds.total += 16
        ins_.then_inc(ds.sem, 16)
        tok = (ds.key, ds.total)
        self._commit(tok, R, W)
        return tok

    def finish(self, bufs, engname="sp"):
        deps = {}
        for b in bufs:
            for tok in ([b.w] if b.w else []) + b.r:
                key, val = tok
                if key[0] == "dma":
                    val = self.sems[key].total
                deps[key] = max(deps.get(key, 0), val)
        self._wait(engname, deps)


def mm(s, out, lhsT, rhs, start, stop, R, W):
    return s.op("pe", lambda e: e.matmul(out, lhsT, rhs, start=start, stop=stop), R=R, W=W)


def build_p0():
    nc = bass.Bass("TRN2", target_bir_lowering=False)
    NCOL = 3072
    NJ = NCOL // 128
    cT = nc.dram_tensor("cT", [128, 8, 4], F32, kind="ExternalInput").ap()
    w = nc.dram_tensor("w", [D, NCOL], F32, kind="ExternalInput").ap()
    bvec = nc.dram_tensor("bvec", [128, NJ], F32, kind="ExternalInput").ap()
    out = nc.dram_tensor("out", [128, NJ, 4], F32, kind="ExternalOutput").ap()
    s = Sched(nc)
    wv = w.rearrange("(kc p) n -> p kc n", p=128)
    with (nc.sbuf_tensor("w_sb", [128, 8, NCOL], F32) as w_sb,
          nc.sbuf_tensor("c_sb", [128, 8, 4], F32) as c_sb,
          nc.sbuf_tensor("s_sb", [128, 8, 4], F32) as s_sb,
          nc.sbuf_tensor("b_sb", [128, NJ], F32) as b_sb,
          nc.sbuf_tensor("r_sb", [128, NJ, 4], F32) as r_sb,
          nc.psum_tensor("ps", [128, 8, 512], F32) as ps):
        bw = [s.buf(f"w{k}", s.dsem(f"w{k}")) for k in range(8)]
        bc = s.buf("c", s.dsem("c"))
        bb = s.buf("b", bc.dsem)
        bs = s.buf("s")
        br = s.buf("r", s.dsem("r"))
        bps = [s.buf(f"ps{i}") for i in range(8)]
        s.dma("sp", c_sb[:], cT, W=[bc])
        s.dma("sp", b_sb[:], bvec, W=[bb])
        for k in range(8):
            s.dma("sp" if k % 2 == 0 else "act", w_sb[:, k, :], wv[:, k, :], W=[bw[k]])
        s.op("act", lambda e: e.activation(out=s_sb[:], in_=c_sb[:], func=AF.Silu), R=[bc], W=[bs])
        for j in range(NJ):
            pb = bps[j % 8]
            for k in range(8):
                mm(s, ps[:, j % 8, 0:4], w_sb[:, k, j * 128:(j + 1) * 128], s_sb[:, k, :],
                   k == 0, k == 7, R=[bw[k], bs], W=[pb])
            s.op("dve", lambda e: e.tensor_scalar(out=r_sb[:, j, :], in0=ps[:, j % 8, 0:4],
                                                  scalar1=b_sb[:, j:j + 1], scalar2=None, op0=ALU.add),
                 R=[pb, bb], W=[br])
        s.dma("sp", out, r_sb[:], R=[br])
        s.finish([br])
    return nc


def silu_np_layout_c(c, c_ctx):
    cc = np.stack([c[0], c[1], c_ctx, c_ctx], axis=1)
    return np.ascontiguousarray(cc.reshape(8, 128, 4).transpose(1, 0, 2))


def run_p0(c, c_ctx, w_ada, b_ada):
    nc = build_p0()
    cT = silu_np_layout_c(c, c_ctx)
    in_maps = []
    for i in range(NCORES):
        l, h = i // 2, i % 2
        in_maps.append({
            "cT": cT,
            "w": np.ascontiguousarray(w_ada[l][:, h * 3072:(h + 1) * 3072]),
            "bvec": np.ascontiguousarray(b_ada[l][h * 3072:(h + 1) * 3072].reshape(24, 128).T),
        })
    res = run_bass_kernel_spmd(nc, in_maps, core_ids=list(range(NCORES)))
    mods = np.zeros((DEPTH, 3, 6 * D), np.float32)
    for i in range(NCORES):
        l, h = i // 2, i % 2
        o = res.results[i]["out"]
        m = o.transpose(1, 0, 2).reshape(3072, 4)
        mods[l, :, h * 3072:(h + 1) * 3072] = m[:, :3].T
    return mods


NT1 = 2112
NW1 = 3072
BLKS1 = [(0, 512), (512, 512), (1024, 512), (1536, 512), (2048, 64)]


def build_p1():
    nc = bass.Bass("TRN2", target_bir_lowering=False)
    xT = nc.dram_tensor("xT", [128, 8, NT1], F32, kind="ExternalInput").ap()
    w = nc.dram_tensor("w", [D, NW1], F32, kind="ExternalInput").ap()
    gsc = nc.dram_tensor("gsc", [128, 5, 8], F32, kind="ExternalInput").ap()
    cosT = nc.dram_tensor("cosT", [128, 2048], F32, kind="ExternalInput").ap()
    sinT = nc.dram_tensor("sinT", [128, 2048], F32, kind="ExternalInput").ap()
    fmb = nc.dram_tensor("fmb", [128, 8, NT1], BF16, kind="ExternalOutput").ap()
    fmf = nc.dram_tensor("fmf", [128, 8, NT1], F32, kind="ExternalOutput").ap()
    tm = nc.dram_tensor("tm", [NT1, 512], BF16, kind="ExternalOutput").ap()
    s = Sched(nc)
    wv = w.rearrange("(kc p) n -> p kc n", p=128)
    with (nc.sbuf_tensor("x_sb", [128, 2, 8, 512], F32) as x_sb,
          nc.sbuf_tensor("h_sb", [128, 8, NT1], BF16) as h_sb,
          nc.sbuf_tensor("w_bf", [128, 8, NW1], BF16) as w_bf,
          nc.sbuf_tensor("w_st", [128, 2, NW1], F32) as w_st,
          nc.sbuf_tensor("o_sb", [128, 4, 512], F32) as o_sb,
          nc.sbuf_tensor("ob_sb", [128, 4, 512], BF16) as ob_sb,
          nc.sbuf_tensor("cos_sb", [128, 2048], F32) as cos_sb,
          nc.sbuf_tensor("sin_sb", [128, 2048], F32) as sin_sb,
          nc.sbuf_tensor("gsc_sb", [128, 5, 8], F32) as gsc_sb,
          nc.sbuf_tensor("ab_sb", [128, 2, 8], F32) as ab_sb,
          nc.sbuf_tensor("sq_sb", [128, 8, 512], BF16) as sq_sb,
          nc.sbuf_tensor("ones_sb", [128, 128], BF16) as ones_sb,
          nc.sbuf_tensor("rstd_sb", [128, 2, 512], F32) as rstd_sb,
          nc.sbuf_tensor("tmp_sb", [128, 4, 512], F32) as tmp_sb,
          nc.psum_tensor("ps", [128, 8, 512], F32) as ps):
        bx = [s.buf(f"x{i}", s.dsem(f"x{i}")) for i in range(2)]
        bh = [s.buf(f"h{i}") for i in range(len(BLKS1))]
        bwst = [s.buf(f"wst{i}", s.dsem(f"wst{i}")) for i in range(2)]
        bwbf = [s.buf(f"wbf{k}") for k in range(8)]
        bo = [s.buf(f"o{i}", s.dsem(f"o{i}")) for i in range(4)]
        bob = [s.buf(f"ob{i}", s.dsem(f"ob{i}")) for i in range(4)]
        cs = s.dsem("const")
        bcos = s.buf("cos", cs); bsin = s.buf("sin", cs); bgsc = s.buf("gsc", cs)
        bab = s.buf("ab"); bsq = s.buf("sq"); bones = s.buf("ones")
        brs = [s.buf(f"rs{i}") for i in range(2)]
        btmp = [s.buf(f"tmp{i}") for i in range(4)]
        bps = [s.buf(f"ps{i}") for i in range(8)]

        s.dma("sp", gsc_sb[:], gsc, W=[bgsc])
        s.dma("sp", cos_sb[:], cosT, W=[bcos])
        s.dma("sp", sin_sb[:], sinT, W=[bsin])
        s.op("pool", lambda e: e.memset(ones_sb[:], 1.0), W=[bones])
        for t in range(2):
            s.op("dve", lambda e: e.scalar_tensor_tensor(
                out=ab_sb[:, t, :], in0=gsc_sb[:, 1 + 2 * t, :], scalar=1.0, in1=gsc_sb[:, 0, :],
                op0=ALU.add, op1=ALU.mult), R=[bgsc], W=[bab])
        for k in range(8):
            sl = k % 2
            s.dma("act", w_st[:, sl, :], wv[:, k, :], W=[bwst[sl]])
            s.op("pool", lambda e: e.tensor_copy(out=w_bf[:, k, :], in_=w_st[:, sl, :]),
                 R=[bwst[sl]], W=[bwbf[k]])
        for bi, (t0, n) in enumerate(BLKS1):
            sl = bi % 2
            isctx = bi == 4
            s.dma("sp", x_sb[:, sl, :, 0:n], xT[:, :, t0:t0 + n], W=[bx[sl]])
            s.op("act", lambda e: e.activation(out=sq_sb[:, :, 0:n], in_=x_sb[:, sl, :, 0:n], func=AF.Square),
                 R=[bx[sl]], W=[bsq])
            pst = bps[sl]
            for k in range(8):
                mm(s, ps[:, sl, 0:n], ones_sb[:], sq_sb[:, k, 0:n], k == 0, k == 7, R=[bones, bsq], W=[pst])
            s.op("act", lambda e: e.activation(out=rstd_sb[:, sl, 0:n], in_=ps[:, sl, 0:n], func=AF.Sqrt,
                                               scale=1.0 / D, bias=EPS), R=[pst], W=[brs[sl]])
            s.op("dve", lambda e: e.reciprocal(out=rstd_sb[:, sl, 0:n], in_=rstd_sb[:, sl, 0:n]),
                 R=[brs[sl]], W=[brs[sl]])
            ai = 1 if isctx else 0
            shrow = 4 if isctx else 2
            for k in range(8):
                tb = k % 4
                s.op("dve", lambda e: e.scalar_tensor_tensor(
                    out=tmp_sb[:, tb, 0:n], in0=x_sb[:, sl, k, 0:n], scalar=ab_sb[:, ai, k:k + 1],
                    in1=rstd_sb[:, sl, 0:n], op0=ALU.mult, op1=ALU.mult),
                    R=[bx[sl], bab, brs[sl]], W=[btmp[tb]])
                s.op("act", lambda e: e.activation(out=h_sb[:, k, t0:t0 + n], in_=tmp_sb[:, tb, 0:n],
                                                   func=AF.Identity, bias=gsc_sb[:, shrow, k:k + 1], scale=1.0),
                     R=[btmp[tb], bgsc], W=[bh[bi]])
        oi = 0
        obi = 0
        pi = 0
        for bi, (t0, n) in enumerate(BLKS1):
            isctx = bi == 4
            for c in range(16):
                rope = (c >= 12) and not isctx
                isb = c < 4 or c >= 12
                dst = (fmb[:, c if c < 4 else c - 8, t0:t0 + n]) if isb else fmf[:, c - 4, t0:t0 + n]
                pA = 2 + (pi % 6); pi += 1
                for k in range(8):
                    mm(s, ps[:, pA, 0:n], w_bf[:, k, c * 128:(c + 1) * 128], h_sb[:, k, t0:t0 + n],
                       k == 0, k == 7, R=[bwbf[k], bh[bi]], W=[bps[pA]])
                if isb:
                    ob = obi % 4; obi += 1
                    osl = ob_sb[:, ob, 0:n]; obuf = bob[ob]
                else:
                    ob = oi % 4; oi += 1
                    osl = o_sb[:, ob, 0:n]; obuf = bo[ob]
                if not rope:
                    if c % 2 == 0:
                        s.op("act", lambda e: e.copy(out=osl, in_=ps[:, pA, 0:n]), R=[bps[pA]], W=[obuf])
                    else:
                        s.op("dve", lambda e: e.tensor_copy(out=osl, in_=ps[:, pA, 0:n]), R=[bps[pA]], W=[obuf])
                else:
                    pB = 2 + (pi % 6); pi += 1
                    c2 = c + 4
                    for k in range(8):
                        mm(s, ps[:, pB, 0:n], w_bf[:, k, c2 * 128:(c2 + 1) * 128], h_sb[:, k, t0:t0 + n],
                           k == 0, k == 7, R=[bwbf[k], bh[bi]], W=[bps[pB]])
                    s.op("dve", lambda e: e.tensor_tensor(out=tmp_sb[:, 0, 0:n], in0=ps[:, pA, 0:n],
                                                          in1=cos_sb[:, t0:t0 + n], op=ALU.mult),
                         R=[bps[pA], bcos], W=[btmp[0]])
                    s.op("dve", lambda e: e.tensor_tensor(out=tmp_sb[:, 1, 0:n], in0=ps[:, pB, 0:n],
                                                          in1=sin_sb[:, t0:t0 + n], op=ALU.mult),
                         R=[bps[pB], bsin], W=[btmp[1]])
                    s.op("pool", lambda e: e.tensor_tensor(out=osl, in0=tmp_sb[:, 0, 0:n],
                                                           in1=tmp_sb[:, 1, 0:n], op=ALU.add),
                         R=[btmp[0], btmp[1]], W=[obuf])
                s.dma("sp", dst, osl, R=[obuf])
        ntile = NT1 // 128 + 1
        for ti in range(ntile):
            t0 = ti * 128
            n = min(128, NT1 - t0)
            bi = min(t0 // 512, 4)
            pA = 2 + (pi % 6); pi += 1
            for k in range(8):
                mm(s, ps[0:n, pA, :], h_sb[:, k, t0:t0 + n], w_bf[:, k, 2560:3072],
                   k == 0, k == 7, R=[bwbf[k], bh[bi]], W=[bps[pA]])
            ob = obi % 4; obi += 1
            s.op("act", lambda e: e.copy(out=ob_sb[0:n, ob, :], in_=ps[0:n, pA, :]), R=[bps[pA]], W=[bob[ob]])
            s.dma("sp", tm[t0:t0 + n, :], ob_sb[0:n, ob, :], R=[bob[ob]])
        s.finish(bo + bob)
    return nc


def rope_tables():
    t = np.arange(S)
    row = (t // GRID_W).astype(np.float32)
    col = (t % GRID_W).astype(np.float32)
    inv = (10000.0 ** (-np.arange(0, 16, 2, dtype=np.float32) / 16.0)).astype(np.float32)
    ang_r = row[:, None] * inv
    ang_c = col[:, None] * inv
    cosT = np.zeros((32, S), np.float32)
    sinT = np.zeros((32, S), np.float32)
    for d in range(32):
        ang = ang_r if d < 16 else ang_c
        i = d % 8
        cosT[d] = np.cos(ang[:, i])
        sgn = -1.0 if (d % 16) < 8 else 1.0
        sinT[d] = sgn * np.sin(ang[:, i])
    return np.tile(cosT, (4, 1)), np.tile(sinT, (4, 1))


def p1_wcols():
    sw = np.array([(d + 8) if (d % 16) < 8 else (d - 8) for d in range(32)])
    f = np.arange(256)
    swf = (f // 32) * 32 + sw[f % 32]
    cols = np.concatenate([np.arange(0, 256), np.arange(256, 512), np.arange(768, 1280), np.arange(1280, 1792),
                           np.arange(1792, 2048), np.arange(2048, 2304), 1792 + swf, 2048 + swf,
                           np.arange(512, 768), np.arange(2304, 2560)])
    return cols


def chunkT(a):
    T = a.shape[0]
    return np.ascontiguousarray(a.T.reshape(8, 128, T).transpose(1, 0, 2))


def vec_pk(v):
    return np.ascontiguousarray(v.reshape(8, 128).T)


def run_p1(nc1, xl, xc, mods_l, g_mix_l, w_in_l, cosT, sinT):
    wl = np.ascontiguousarray(w_in_l[:, p1_wcols()])
    in_maps = []
    for i in range(NCORES):
        b, j = i // 4, i % 4
        xx = np.concatenate([xl[b, 2048 * j:2048 * (j + 1)], xc[b, 64 * j:64 * (j + 1)]], axis=0)
        gsc = np.stack([vec_pk(g_mix_l), vec_pk(mods_l[b, 1024:2048]), vec_pk(mods_l[b, 0:1024]),
                        vec_pk(mods_l[2, 1024:2048]), vec_pk(mods_l[2, 0:1024])], axis=1)
        in_maps.append({"xT": chunkT(xx), "w": wl, "gsc": np.ascontiguousarray(gsc),
                        "cosT": np.ascontiguousarray(cosT[:, 2048 * j:2048 * (j + 1)]),
                        "sinT": np.ascontiguousarray(sinT[:, 2048 * j:2048 * (j + 1)])})
    res = run_bass_kernel_spmd(nc1, in_maps, core_ids=list(range(NCORES)))
    fmb = np.zeros((B, 1024, S + L), ml_dtypes.bfloat16)
    fmf = np.zeros((B, 1024, S + L), np.float32)
    tm = np.zeros((B, S + L, 512), ml_dtypes.bfloat16)
    for i in range(NCORES):
        b, j = i // 4, i % 4
        r = res.results[i]
        for dst, key in ((fmb, "fmb"), (fmf, "fmf")):
            f = r[key].transpose(1, 0, 2).reshape(1024, NT1)
            dst[b, :, 2048 * j:2048 * (j + 1)] = f[:, :2048]
            dst[b, :, S + 64 * j:S + 64 * (j + 1)] = f[:, 2048:]
        t = r["tm"]
        tm[b, 2048 * j:2048 * (j + 1)] = t[:2048]
        tm[b, S + 64 * j:S + 64 * (j + 1)] = t[2048:]
    return fmb, fmf, tm


NTOK = S + L
NKT = NTOK // 128
NEB = 21


def na_tile_lists():
    out = []
    for m in range(64):
        if 2 <= m <= 61:
            out.append(([m - 2, m - 1, m, m + 1, m + 2], 0))
        elif m < 2:
            out.append(([0, 1, 2, 3], 5 + 4 * m))
        else:
            out.append(([60, 61, 62, 63], 5 + 4 * (m - 60)))
    return out


def na_bias_index():
    MASKED = 15 * 31
    idx = np.full((NEB, 128, 128), MASKED, np.int64)
    lists = na_tile_lists()
    reps = {0: 10}
    qq = np.arange(128); kk = np.arange(128)

    def fill(e0, m, kts):
        for ii, n in enumerate(kts):
            qr = 2 * m + qq // 64; qc = qq % 64
            kr = 2 * n + kk // 64; kc = kk % 64
            r0 = np.clip(qr - 4, 0, 120)
            cs = np.clip(qc - 8, 0, 48)
            valid = ((kr[:, None] >= r0[None, :]) & (kr[:, None] < r0[None, :] + 8) &
                     (kc[:, None] >= cs[None, :]) & (kc[:, None] < cs[None, :] + 16))
            dr = kr[:, None] - qr[None, :]
            dc = np.clip(kc[:, None] - qc[None, :], -15, 15)
            v = (np.clip(dr, -7, 7) + 7) * 31 + dc + 15
            idx[e0 + ii] = np.where(valid, v, MASKED)
    fill(0, 10, lists[10][0])
    for m in (0, 1, 62, 63):
        fill(lists[m][1], m, lists[m][0])
    return idx


def build_p2(lam_init):
    nc = bass.Bass("TRN2", target_bir_lowering=False)
    din = lambda n, shp, dt: nc.dram_tensor(n, shp, dt, kind="ExternalInput").ap()
    dout = lambda n, shp, dt: nc.dram_tensor(n, shp, dt, kind="ExternalOutput").ap()
    xrg = din("xrg", [2, 128, NTOK], F32)
    wbd = din("wbd", [4, 128, 128], F32)
    rgv = din("rgv", [128, 2, 8], F32)
    hout = dout("hout", [2, 128, NTOK], F32)
    qaT = din("qaT", [64, NTOK], BF16)
    kaT = din("kaT", [64, NTOK], BF16)
    vaP = din("vaP", [128, NKT, 64], BF16)
    btT = din("btT", [128, NEB, 128], F32)
    ynaP = dout("ynaP", [128, NKT, 64], BF16)
    qdT = din("qdT", [2, 32, NTOK], BF16)
    kdT = din("kdT", [2, 32, NTOK], BF16)
    vdP = din("vdP", [128, NKT, 64], BF16)
    dlam = din("dlam", [128, 128], F32)
    dg = din("dg", [128, 64], F32)
    ydfP = dout("ydfP", [128, NKT, 64], BF16)
    s = Sched(nc)
    with nc.psum_tensor("ps", [128, 8, 512], F32) as ps:
        bps = [s.buf(f"ps{i}") for i in range(8)]

        CH = 2048
        with (nc.sbuf_tensor("x_sb", [128, NTOK], F32) as x_sb,
              nc.sbuf_tensor("wst_sb", [128, 4, 128], F32) as wst_sb,
              nc.sbuf_tensor("wbf_sb", [128, 4, 128], BF16) as wbf_sb,
              nc.sbuf_tensor("rgv_sb", [128, 2, 8], F32) as rgv_sb,
              nc.sbuf_tensor("cneg_sb", [128, 2], F32) as cneg_sb,
              nc.sbuf_tensor("xcv_sb", [128, CH], F32) as xcv_sb,
              nc.sbuf_tensor("xcb_sb", [128, CH], BF16) as xcb_sb,
              nc.sbuf_tensor("r_sb", [128, CH], F32) as r_sb,
              nc.sbuf_tensor("i_sb", [128, CH], F32) as i_sb,
              nc.sbuf_tensor("a_sb", [128, CH], F32) as a_sb,
              nc.sbuf_tensor("q_sb", [128, CH], F32) as q_sb,
              nc.sbuf_tensor("h_sb", [128, 2, CH], F32) as h_sb,
              nc.sbuf_tensor("carry_sb", [128, 1], F32) as carry_sb):
            bx = s.buf("x", s.dsem("rgx"))
            bw = s.buf("w", s.dsem("rgw"))
            bwb = s.buf("wb")
            bv = s.buf("v", bw.dsem)
            bcn = s.buf("cneg")
            bxcv = s.buf("xcv"); bxcb = s.buf("xcb"); br = s.buf("r"); bi_ = s.buf("i")
            ba = s.buf("a"); bq = s.buf("q"); bcar = s.buf("carry")
            bh = [s.buf(f"h{i}", s.dsem(f"rgh{i}")) for i in range(2)]
            s.dma("act", wst_sb[:], wbd.rearrange("f c d -> c f d"), W=[bw])
            s.dma("act", rgv_sb[:], rgv, W=[bv])
            s.op("pool", lambda e: e.tensor_copy(out=wbf_sb[:], in_=wst_sb[:]), R=[bw], W=[bwb])
            s.op("act", lambda e: e.activation(out=cneg_sb[:], in_=rgv_sb[:, :, 7], func=AF.Exp, scale=-1.0),
                 R=[bv], W=[bcn])
            s.op("act", lambda e: e.activation(out=cneg_sb[:], in_=cneg_sb[:], func=AF.Ln, bias=1.0, scale=1.0),
                 R=[bcn], W=[bcn])
            s.op("dve", lambda e: e.tensor_scalar(out=cneg_sb[:], in0=cneg_sb[:], scalar1=-8.0, scalar2=None,
                                                  op0=ALU.mult), R=[bcn], W=[bcn])
            hi = 0
            for dr in range(2):
                offs = [-2, -1, 0, 1] if dr == 0 else [2, 1, 0, -1]
                s.dma("sp", x_sb[:, 0:4224], xrg[dr, :, 0:4224], W=[bx])
                s.dma("sp", x_sb[:, 4224:NTOK], xrg[dr, :, 4224:NTOK], W=[bx])
                chunks = [(0, 256, 0, 256)] + [(256 + CH * i, CH, 256, NTOK) for i in range(4)]
                for ci, (c0, n, s0, s1) in enumerate(chunks):
                    s.op("dve", lambda e: e.tensor_scalar(
                        out=xcv_sb[:, 0:n], in0=x_sb[:, c0:c0 + n], scalar1=rgv_sb[:, dr, 2:3],
                        scalar2=rgv_sb[:, dr, 4:5], op0=ALU.mult, op1=ALU.add), R=[bx, bv], W=[bxcv])
                    for jt in (0, 1, 3):
                        o = offs[jt]
                        lo = max(c0, s0 - o); hi_ = min(c0 + n, s1 - o)
                        s.op("dve", lambda e: e.scalar_tensor_tensor(
                            out=xcv_sb[:, lo - c0:hi_ - c0], in0=x_sb[:, lo + o:hi_ + o],
                            scalar=rgv_sb[:, dr, jt:jt + 1], in1=xcv_sb[:, lo - c0:hi_ - c0],
                            op0=ALU.mult, op1=ALU.add), R=[bx, bv, bxcv], W=[bxcv])
                    s.op("pool", lambda e: e.tensor_copy(out=xcb_sb[:, 0:n], in_=xcv_sb[:, 0:n]), R=[bxcv], W=[bxcb])
                    nsb = (n + 511) // 512
                    for sb in range(nsb):
                        w_ = min(512, n - sb * 512)
                        mm(s, ps[:, sb, 0:w_], wbf_sb[:, 2 * dr, :], xcb_sb[:, sb * 512:sb * 512 + w_], True, True,
                           R=[bwb, bxcb], W=[bps[sb]])
                        mm(s, ps[:, 4 + sb, 0:w_], wbf_sb[:, 2 * dr + 1, :], xcb_sb[:, sb * 512:sb * 512 + w_], True, True,
                           R=[bwb, bxcb], W=[bps[4 + sb]])
                    if n == CH:
                        rin = ps[:, 0:4, :]; iin = ps[:, 4:8, :]
                        rout = r_sb[:, 0:n].rearrange("p (a b) -> p a b", b=512)
                        iout = i_sb[:, 0:n].rearrange("p (a b) -> p a b", b=512)
                    else:
                        rin = ps[:, 0, 0:n]; iin = ps[:, 4, 0:n]
                        rout = r_sb[:, 0:n]; iout = i_sb[:, 0:n]
                    s.op("act", lambda e: e.activation(out=rout, in_=rin, func=AF.Sigmoid,
                                                       bias=rgv_sb[:, dr, 5:6], scale=1.0),
                         R=bps[0:4] + [bv], W=[br])
                    s.op("act", lambda e: e.activation(out=iout, in_=iin, func=AF.Sigmoid,
                                                       bias=rgv_sb[:, dr, 6:7], scale=1.0),
                         R=bps[4:8] + [bv], W=[bi_])
                    s.op("act", lambda e: e.activation(out=a_sb[:, 0:n], in_=r_sb[:, 0:n], func=AF.Exp,
                                                       scale=cneg_sb[:, dr:dr + 1]), R=[br, bcn], W=[ba])
                    s.op("pool", lambda e: e.tensor_tensor(out=q_sb[:, 0:n], in0=a_sb[:, 0:n], in1=a_sb[:, 0:n],
                                                           op=ALU.mult), R=[ba], W=[bq])
                    s.op("act", lambda e: e.activation(out=q_sb[:, 0:n], in_=q_sb[:, 0:n], func=AF.Sqrt,
                                                       scale=-1.0, bias=1.0), R=[bq], W=[bq])
                    s.op("pool", lambda e: e.tensor_tensor(out=i_sb[:, 0:n], in0=i_sb[:, 0:n], in1=xcv_sb[:, 0:n],
                                                           op=ALU.mult), R=[bi_, bxcv], W=[bi_])
                    s.op("pool", lambda e: e.tensor_tensor(out=q_sb[:, 0:n], in0=q_sb[:, 0:n], in1=i_sb[:, 0:n],
                                                           op=ALU.mult), R=[bq, bi_], W=[bq])
                    hs = hi % 2; hi += 1
                    init = 0.0 if ci == 0 else carry_sb[:, 0:1]
                    s.op("dve", lambda e: e.tensor_tensor_scan(out=h_sb[:, hs, 0:n], data0=a_sb[:, 0:n],
                                                               data1=q_sb[:, 0:n], initial=init,
                                                               op0=ALU.mult, op1=ALU.add),
                         R=[ba, bq] + ([bcar] if ci else []), W=[bh[hs]])
                    s.op("dve", lambda e: e.tensor_copy(out=carry_sb[:, 0:1], in_=h_sb[:, hs, n - 1:n]),
                         R=[bh[hs]], W=[bcar])
                    s.dma("sp", hout[dr, :, c0:c0 + n], h_sb[:, hs, 0:n], R=[bh[hs]])
            s.barrier(bh)

        with (nc.sbuf_tensor("na_q_sb", [64, NTOK], BF16) as q_sb,
              nc.sbuf_tensor("na_k_sb", [64, NTOK], BF16) as k_sb,
              nc.sbuf_tensor("na_v_sb", [128, NKT, 65], BF16) as v_sb,
              nc.sbuf_tensor("na_bt_sb", [128, NEB * 128], F32) as bt_sb,
              nc.sbuf_tensor("na_eb_sb", [128, NEB * 128], BF16) as eb_sb,
              nc.sbuf_tensor("na_e_sb", [128, 2, 640], F32) as e_sb,
              nc.sbuf_tensor("na_p_sb", [128, 2, 896], BF16) as p_sb,
              nc.sbuf_tensor("na_y_sb", [128, NKT, 64], BF16) as y_sb,
              nc.sbuf_tensor("na_rec_sb", [128, 2], F32) as rec_sb):
            ld = s.dsem("nald")
            bq = s.buf("q", ld); bk = s.buf("k", ld); bv = s.buf("v", ld); bbt = s.buf("bt", ld)
            beb = s.buf("eb")
            be = [s.buf(f"e{i}") for i in range(2)]
            bp = [s.buf(f"p{i}") for i in range(2)]
            brec = [s.buf(f"rec{i}") for i in range(2)]
            by = s.buf("y", s.dsem("nay"))
            s.dma("sp", q_sb[:], qaT, W=[bq])
            s.dma("act", k_sb[:], kaT, W=[bk])
            s.dma("sp", v_sb[:, :, 0:64], vaP, W=[bv])
            s.dma("act", bt_sb[:], btT.rearrange("p e q -> p (e q)"), W=[bbt])
            s.op("pool", lambda e: e.memset(v_sb[:, :, 64:65], 1.0), W=[bv])
            s.op("act", lambda e: e.activation(out=eb_sb[:], in_=bt_sb[:], func=AF.Exp), R=[bbt], W=[beb])
            lists = na_tile_lists()
            for m in range(NKT):
                sl = m % 2
                if m < 64:
                    kts, eb0 = lists[m]
                else:
                    kts, eb0 = [], 0
                nl = len(kts)
                bA = bps[2 * sl]; bB = bps[2 * sl + 1]; bAcc = bps[4 + sl]
                qs = q_sb[:, m * 128:(m + 1) * 128]
                for ii, n in enumerate(kts):
                    bank = 2 * sl + (0 if ii < 4 else 1)
                    col = (ii % 4) * 128
                    mm(s, ps[:, bank, col:col + 128], k_sb[:, n * 128:(n + 1) * 128], qs, True, True,
                       R=[bk, bq], W=[bps[bank]])
                for ci in range(2):
                    n = 64 + ci
                    mm(s, ps[:, 2 * sl + 1, 128 + ci * 128:256 + ci * 128], k_sb[:, n * 128:(n + 1) * 128], qs,
                       True, True, R=[bk, bq], W=[bB])
                if nl:
                    na_ = min(nl, 4) * 128
                    s.op("act", lambda e: e.activation(out=e_sb[:, sl, 0:na_], in_=ps[:, 2 * sl, 0:na_], func=AF.Exp,
                                                       scale=0.125), R=[bA], W=[be[sl]])
                    if nl == 5:
                        s.op("act", lambda e: e.activation(out=e_sb[:, sl, 512:640], in_=ps[:, 2 * sl + 1, 0:128],
                                                           func=AF.Exp, scale=0.125), R=[bB], W=[be[sl]])
                s.op("act", lambda e: e.activation(out=p_sb[:, sl, 640:896], in_=ps[:, 2 * sl + 1, 128:384],
                                                   func=AF.Exp, scale=0.125), R=[bB], W=[bp[sl]])
                if nl:
                    s.op("dve", lambda e: e.tensor_tensor(out=p_sb[:, sl, 0:nl * 128], in0=e_sb[:, sl, 0:nl * 128],
                                                          in1=eb_sb[:, eb0 * 128:(eb0 + nl) * 128], op=ALU.mult),
                         R=[be[sl], beb], W=[bp[sl]])
                tiles = [(ii * 128, n) for ii, n in enumerate(kts)] + [(640, 64), (768, 65)]
                for ti, (pc, n) in enumerate(tiles):
                    mm(s, ps[:, 4 + sl, 0:65], p_sb[:, sl, pc:pc + 128], v_sb[:, n, :], ti == 0, ti == len(tiles) - 1,
                       R=[bp[sl], bv], W=[bAcc])
                s.op("dve", lambda e: e.reciprocal(out=rec_sb[:, sl:sl + 1], in_=ps[:, 4 + sl, 64:65]),
                     R=[bAcc], W=[brec[sl]])
                s.op("dve", lambda e: e.tensor_scalar(out=y_sb[:, m, :], in0=ps[:, 4 + sl, 0:64],
                                                      scalar1=rec_sb[:, sl:sl + 1], scalar2=None, op0=ALU.mult),
                     R=[bAcc, brec[sl]], W=[by])
            s.dma("sp", ynaP, y_sb[:], R=[by])
            s.barrier([by])

        with (nc.sbuf_tensor("df_q0_sb", [32, NTOK], BF16) as q0_sb,
              nc.sbuf_tensor("df_q1_sb", [32, NTOK], BF16) as q1_sb,
              nc.sbuf_tensor("df_k0_sb", [32, NTOK], BF16) as k0_sb,
              nc.sbuf_tensor("df_k1_sb", [32, NTOK], BF16) as k1_sb,
              nc.sbuf_tensor("df_v_sb", [128, NKT, 65], BF16) as v_sb,
              nc.sbuf_tensor("df_p_sb", [128, 4, 512], BF16) as p_sb,
              nc.sbuf_tensor("df_y_sb", [128, NKT, 64], BF16) as y_sb,
              nc.sbuf_tensor("df_dl_sb", [128, 128], F32) as dl_sb,
              nc.sbuf_tensor("df_g_sb", [128, 64], F32) as g_sb,
              nc.sbuf_tensor("df_lam_sb", [128, 4], F32) as lam_sb,
              nc.sbuf_tensor("df_r_sb", [128, 2, 2, 4], F32) as r_sb,
              nc.sbuf_tensor("df_ss_sb", [128, 2, 4], F32) as ss_sb,
              nc.sbuf_tensor("df_o_sb", [128, 2, 4, 64], F32) as o_sb,
              nc.sbuf_tensor("df_t_sb", [128, 2, 4, 64], F32) as t_sb,
              nc.sbuf_tensor("df_junk_sb", [128, 64], F32) as junk_sb):
            ld = s.dsem("dfld")
            qs_ = [q0_sb, q1_sb]; ks_ = [k0_sb, k1_sb]
            bq = s.buf("q", ld); bk = s.buf("k", ld); bv = s.buf("v", ld); bdl = s.buf("dl", ld); bg = s.buf("g", ld)
            blam = s.buf("lam")
            bp = [s.buf(f"p{i}") for i in range(4)]
            by = s.buf("y", s.dsem("dfy"))
            br = [s.buf(f"r{i}") for i in range(2)]
            bss = [s.buf(f"ss{i}") for i in range(2)]
            bo = [s.buf(f"o{i}") for i in range(2)]
            bt = [s.buf(f"t{i}") for i in range(2)]
            bj = s.buf("junk")
            for c in range(2):
                s.dma("sp", qs_[c][:], qdT[c], W=[bq])
                s.dma("act", ks_[c][:], kdT[c], W=[bk])
            s.dma("sp", v_sb[:, :, 0:64], vdP, W=[bv])
            s.dma("act", dl_sb[:], dlam, W=[bdl])
            s.dma("act", g_sb[:], dg, W=[bg])
            s.op("pool", lambda e: e.memset(v_sb[:, :, 64:65], 1.0), W=[bv])
            s.op("dve", lambda e: e.scalar_tensor_tensor(out=junk_sb[:, 0:32], in0=dl_sb[:, 0:32], scalar=1.0,
                                                         in1=dl_sb[:, 32:64], op0=ALU.mult, op1=ALU.mult,
                                                         accum_out=lam_sb[:, 0:1]), R=[bdl], W=[bj, blam])
            s.op("dve", lambda e: e.scalar_tensor_tensor(out=junk_sb[:, 0:32], in0=dl_sb[:, 64:96], scalar=1.0,
                                                         in1=dl_sb[:, 96:128], op0=ALU.mult, op1=ALU.mult,
                                                         accum_out=lam_sb[:, 1:2]), R=[bdl, bj], W=[bj, blam])
            s.op("act", lambda e: e.activation(out=lam_sb[:, 0:2], in_=lam_sb[:, 0:2], func=AF.Exp), R=[blam], W=[blam])
            s.op("dve", lambda e: e.tensor_tensor(out=lam_sb[:, 2:3], in0=lam_sb[:, 1:2], in1=lam_sb[:, 0:1],
                                                  op=ALU.subtract), R=[blam], W=[blam])
            s.op("dve", lambda e: e.tensor_scalar(out=lam_sb[:, 2:3], in0=lam_sb[:, 2:3], scalar1=-lam_init,
                                                  scalar2=None, op0=ALU.add), R=[blam], W=[blam])
            s.op("dve", lambda e: e.tensor_scalar(out=g_sb[:], in0=g_sb[:], scalar1=1.0 - lam_init, scalar2=None,
                                                  op0=ALU.mult), R=[bg], W=[bg])
            qblocks = [(512 * i, 512, list(range(NKT))) for i in range(16)] + [(S, 256, [64, 65])]
            LA = 2
            sc = 32 ** -0.5
            gstep = 0
            for qi, (q0, nq, kts) in enumerate(qblocks):
                par = qi % 2
                nsub = nq // 128
                steps = [(kt, c) for kt in kts for c in range(2)]
                ns = len(steps)
                started = [False, False]
                for i in range(ns + LA):
                    if i < ns:
                        kt, c = steps[i]
                        g = gstep + i
                        mm(s, ps[:, g % 4, 0:nq], ks_[c][:, kt * 128:(kt + 1) * 128], qs_[c][:, q0:q0 + nq], True, True,
                           R=[bk, bq], W=[bps[g % 4]])
                        s.op("act", lambda e: e.activation(out=p_sb[:, g % 4, 0:nq], in_=ps[:, g % 4, 0:nq],
                                                           func=AF.Exp, scale=sc), R=[bps[g % 4]], W=[bp[g % 4]])
                    if i >= LA:
                        kt, c = steps[i - LA]
                        g = gstep + i - LA
                        bank = 4 + 2 * par + c
                        for sub in range(nsub):
                            st = not started[c]
                            started[c] = True
                            s.op("pe", lambda e: e.matmul(ps[:, bank, sub * 65:(sub + 1) * 65],
                                                          p_sb[:, g % 4, sub * 128:(sub + 1) * 128], v_sb[:, kt, :],
                                                          start=st, stop=(kt == kts[-1]), skip_group_check=True),
                                 R=[bp[g % 4], bv], W=[bps[bank]])
                gstep += ns
                a0 = ps[:, 4 + 2 * par, 0:260].rearrange("p (s e) -> p s e", e=65)
                a1 = ps[:, 5 + 2 * par, 0:260].rearrange("p (s e) -> p s e", e=65)
                b0 = bps[4 + 2 * par]; b1 = bps[5 + 2 * par]
                s.op("dve", lambda e: e.reciprocal(out=r_sb[:, par, 0, 0:nsub], in_=a0[:, 0:nsub, 64]), R=[b0], W=[br[par]])
                s.op("dve", lambda e: e.reciprocal(out=r_sb[:, par, 1, 0:nsub], in_=a1[:, 0:nsub, 64]), R=[b1], W=[br[par]])
                s.op("dve", lambda e: e.tensor_scalar(out=r_sb[:, par, 1, 0:nsub], in0=r_sb[:, par, 1, 0:nsub],
                                                      scalar1=lam_sb[:, 2:3], scalar2=None, op0=ALU.mult),
                     R=[br[par], blam], W=[br[par]])
                for sub in range(nsub):
                    s.op("dve", lambda e: e.tensor_scalar(out=t_sb[:, par, sub, :], in0=a1[:, sub, 0:64],
                                                          scalar1=r_sb[:, par, 1, sub:sub + 1], scalar2=None,
                                                          op0=ALU.mult), R=[b1, br[par]], W=[bt[par]])
                    s.op("dve", lambda e: e.scalar_tensor_tensor(out=o_sb[:, par, sub, :], in0=a0[:, sub, 0:64],
                                                                 scalar=r_sb[:, par, 0, sub:sub + 1],
                                                                 in1=t_sb[:, par, sub, :], op0=ALU.mult, op1=ALU.add),
                         R=[b0, br[par], bt[par]], W=[bo[par]])
                    s.op("dve", lambda e: e.scalar_tensor_tensor(out=junk_sb[:], in0=o_sb[:, par, sub, :], scalar=1.0,
                                                                 in1=o_sb[:, par, sub, :], op0=ALU.mult, op1=ALU.mult,
                                                                 accum_out=ss_sb[:, par, sub:sub + 1]),
                         R=[bo[par], bj], W=[bj, bss[par]])
                s.op("act", lambda e: e.activation(out=ss_sb[:, par, 0:nsub], in_=ss_sb[:, par, 0:nsub], func=AF.Ln,
                                                   scale=1.0 / 64, bias=EPS), R=[bss[par]], W=[bss[par]])
                s.op("act", lambda e: e.activation(out=ss_sb[:, par, 0:nsub], in_=ss_sb[:, par, 0:nsub], func=AF.Exp,
                                                   scale=-0.5), R=[bss[par]], W=[bss[par]])
                for sub in range(nsub):
                    s.op("dve", lambda e: e.scalar_tensor_tensor(out=y_sb[:, q0 // 128 + sub, :], in0=o_sb[:, par, sub, :],
                                                                 scalar=ss_sb[:, par, sub:sub + 1], in1=g_sb[:],
                                                                 op0=ALU.mult, op1=ALU.mult),
                         R=[bo[par], bss[par], bg], W=[by])
            s.dma("sp", ydfP, y_sb[:], R=[by])
            s.finish([by])
    return nc


def tileP(a):
    return np.ascontiguousarray(a.reshape(NKT, 128, a.shape[1]).transpose(1, 0, 2))


def untileP(a):
    return a.transpose(1, 0, 2).reshape(NTOK, a.shape[2])


_NA_IDX = None


def run_p2(nc2, l, fmb, fmf, tm, inp):
    global _NA_IDX
    if _NA_IDX is None:
        _NA_IDX = na_bias_index()
    in_maps = []
    for i in range(NCORES):
        b, j = i // 4, i % 4
        xr = fmf[b, 128 * j:128 * (j + 1)]
        xf = np.concatenate([xr[:, S:], xr[:, :S]], axis=1)
        xb = np.concatenate([xr[:, S:][:, ::-1], xr[:, :S][:, ::-1]], axis=1)
        wbd = np.zeros((4, 128, 128), np.float32)
        rgv = np.zeros((128, 2, 8), np.float32)
        ch = slice(128 * j, 128 * (j + 1))
        for dr in range(2):
            for gi, wk in enumerate(("rg_w_r", "rg_w_i")):
                for bb in range(2):
                    wbd[dr * 2 + gi, 64 * bb:64 * (bb + 1), 64 * bb:64 * (bb + 1)] = inp[wk][l, dr, 2 * j + bb]
            rgv[:, dr, 0:4] = inp["rg_conv_w"][l][:, ch].T
            rgv[:, dr, 4] = inp["rg_conv_b"][l][ch]
            rgv[:, dr, 5] = inp["rg_b_r"][l, dr, ch]
            rgv[:, dr, 6] = inp["rg_b_i"][l, dr, ch]
            rgv[:, dr, 7] = inp["rg_lambda"][l, dr, ch]
        rext = np.concatenate([inp["na_rpb"][l, j].ravel(), np.array([-30000.0], np.float32)])
        bt = rext[_NA_IDX]
        in_maps.append({
            "xrg": np.ascontiguousarray(np.stack([xf, xb])), "wbd": wbd, "rgv": rgv,
            "qaT": np.ascontiguousarray(fmb[b, 64 * j:64 * (j + 1)]),
            "kaT": np.ascontiguousarray(fmb[b, 256 + 64 * j:256 + 64 * (j + 1)]),
            "vaP": tileP(tm[b][:, 64 * j:64 * (j + 1)]),
            "btT": np.ascontiguousarray(bt.transpose(1, 0, 2)),
            "qdT": np.ascontiguousarray(fmb[b, 512 + 64 * j:512 + 64 * (j + 1)].reshape(2, 32, NTOK)),
            "kdT": np.ascontiguousarray(fmb[b, 768 + 64 * j:768 + 64 * (j + 1)].reshape(2, 32, NTOK)),
            "vdP": tileP(tm[b][:, 256 + 64 * j:256 + 64 * (j + 1)]),
            "dlam": np.ascontiguousarray(np.tile(inp["diff_lambda"][l].reshape(1, 128), (128, 1))),
            "dg": np.ascontiguousarray(np.tile(inp["diff_subln_g"][l].reshape(1, 64), (128, 1))),
        })
    res = run_bass_kernel_spmd(nc2, in_maps, core_ids=list(range(NCORES)))
    yna = np.zeros((B, NTOK, 256), ml_dtypes.bfloat16)
    ydf = np.zeros((B, NTOK, 256), ml_dtypes.bfloat16)
    hf = np.zeros((B, 512, NTOK), np.float32)
    hb = np.zeros((B, 512, NTOK), np.float32)
    for i in range(NCORES):
        b, j = i // 4, i % 4
        r = res.results[i]
        yna[b, :, 64 * j:64 * (j + 1)] = untileP(r["ynaP"])
        ydf[b, :, 64 * j:64 * (j + 1)] = untileP(r["ydfP"])
        h = r["hout"]
        hf[b, 128 * j:128 * (j + 1), S:] = h[0][:, :L]
        hf[b, 128 * j:128 * (j + 1), :S] = h[0][:, L:]
        hb[b, 128 * j:128 * (j + 1), S:] = h[1][:, :L][:, ::-1]
        hb[b, 128 * j:128 * (j + 1), :S] = h[1][:, L:][:, ::-1]
    return yna, ydf, hf, hb


NEXP = 32
GELU_C = 1.5957691216057308


def build_p3(final):
    nc = bass.Bass("TRN2", target_bir_lowering=False)
    din = lambda n, shp, dt: nc.dram_tensor(n, shp, dt, kind="ExternalInput").ap()
    xT = din("xT", [128, 8, NT1], F32)
    nadf = din("nadf", [128, 4, NT1], BF16)
    hg = din("hg", [128, 12, NT1], F32)
    wout = din("wout", [D, D], F32)
    mod = din("mod", [128, 10, 8], F32)
    wge = din("wge", [128, 8, 36], F32)
    bge = din("bge", [128, 36], F32)
    selc = din("selc", [32, NEXP * 128], BF16)
    ident = din("ident", [128, 128], F32)
    w1 = din("w1", [NEXP, D, 512], F32)
    w3 = din("w3", [NEXP, D, 512], F32)
    w2 = din("w2", [NEXP, 512, D], F32)
    xo = nc.dram_tensor("xo", [128, 8, NT1], F32, kind="ExternalOutput").ap()
    s = Sched(nc)
    wov = wout.rearrange("(kc p) n -> p kc n", p=128)
    with (nc.psum_tensor("ps", [128, 8, 512], F32) as ps,
          nc.sbuf_tensor("x_sb", [128, 8, NT1], F32) as x_sb,
          nc.sbuf_tensor("mod_sb", [128, 10, 8], F32) as mod_sb,
          nc.sbuf_tensor("hl2_sb", [128, 8, NT1], BF16) as hl2_sb,
          nc.sbuf_tensor("wdt_sb", [32, 2, NT1], BF16) as wdt_sb,
          nc.sbuf_tensor("ones_sb", [128, 128], BF16) as ones_sb):
        bps = [s.buf(f"ps{i}") for i in range(8)]
        cs = s.dsem("const")
        bxb = [s.buf(f"x{i}", s.dsem(f"x{i}")) for i in range(len(BLKS1))]
        bmod = s.buf("mod", cs)
        bhl2 = [s.buf(f"hl2_{i}") for i in range(len(BLKS1))]
        bwdt = [s.buf(f"wdt{i}") for i in range(len(BLKS1))]
        bones = s.buf("ones")
        s.dma("sp", mod_sb[:], mod, W=[bmod])
        for bi, (t0, n) in enumerate(BLKS1):
            s.dma("sp", x_sb[:, :, t0:t0 + n], xT[:, :, t0:t0 + n], W=[bxb[bi]])
        s.op("pool", lambda e: e.memset(ones_sb[:], 1.0), W=[bones])

        with (nc.sbuf_tensor("s1_mix", [128, 8, NT1], BF16) as mix_sb,
              nc.sbuf_tensor("s1_wobf", [128, 8, D], BF16) as wo_bf,
              nc.sbuf_tensor("s1_wost", [128, 2, D], F32) as wo_st,
              nc.sbuf_tensor("s1_hg", [128, 1, 12, 512], F32) as hg_sb,
              nc.sbuf_tensor("s1_t", [128, 4, 512], F32) as t_sb):
            bmixl = s.buf("mixl", s.dsem("mixl"))
            bmix = [s.buf(f"mix{i}") for i in range(len(BLKS1))]
            bwost = [s.buf(f"wost{i}", s.dsem(f"wost{i}")) for i in range(2)]
            bwobf = [s.buf(f"wobf{k}") for k in range(8)]
            bhg = [s.buf(f"hg{i}", s.dsem(f"hg{i}")) for i in range(2)]
            bt = [s.buf(f"t{i}") for i in range(4)]
            s.dma("act", mix_sb[:, 0:2, :], nadf[:, 0:2, :], W=[bmixl])
            s.dma("act", mix_sb[:, 6:8, :], nadf[:, 2:4, :], W=[bmixl])
            for k in range(8):
                sl = k % 2
                s.dma("act", wo_st[:, sl, :], wov[:, k, :], W=[bwost[sl]])
                s.op("pool", lambda e: e.tensor_copy(out=wo_bf[:, k, :], in_=wo_st[:, sl, :]), R=[bwost[sl]], W=[bwobf[k]])
            for bi, (t0, n) in enumerate(BLKS1):
                sl = 0
                s.dma("sp", hg_sb[:, sl, :, 0:n], hg[:, :, t0:t0 + n], W=[bhg[sl]])
                for ch in range(4):
                    hf_ = hg_sb[:, sl, ch, 0:n]; hb_ = hg_sb[:, sl, 4 + ch, 0:n]; gr_ = hg_sb[:, sl, 8 + ch, 0:n]
                    s.op("dve", lambda e: e.tensor_tensor(out=t_sb[:, 0, 0:n], in0=hf_, in1=hb_, op=ALU.add),
                         R=[bhg[sl]], W=[bt[0]])
                    s.op("dve", lambda e: e.tensor_tensor(out=t_sb[:, 1, 0:n], in0=gr_, in1=gr_, op=ALU.mult),
                         R=[bhg[sl]], W=[bt[1]])
                    s.op("dve", lambda e: e.tensor_scalar(out=t_sb[:, 1, 0:n], in0=t_sb[:, 1, 0:n], scalar1=0.044715,
                                                          scalar2=1.0, op0=ALU.mult, op1=ALU.add), R=[bt[1]], W=[bt[1]])
                    s.op("pool", lambda e: e.tensor_tensor(out=t_sb[:, 2, 0:n], in0=t_sb[:, 1, 0:n], in1=gr_, op=ALU.mult),
                         R=[bt[1], bhg[sl]], W=[bt[2]])
                    s.op("act", lambda e: e.activation(out=t_sb[:, 2, 0:n], in_=t_sb[:, 2, 0:n], func=AF.Sigmoid,
                                                       scale=GELU_C), R=[bt[2]], W=[bt[2]])
                    s.op("pool", lambda e: e.tensor_tensor(out=t_sb[:, 3, 0:n], in0=t_sb[:, 2, 0:n], in1=gr_, op=ALU.mult),
                         R=[bt[2], bhg[sl]], W=[bt[3]])
                    s.op("pool", lambda e: e.tensor_tensor(out=mix_sb[:, 2 + ch, t0:t0 + n], in0=t_sb[:, 3, 0:n],
                                                           in1=t_sb[:, 0, 0:n], op=ALU.mult),
                         R=[bt[3], bt[0]], W=[bmix[bi]])
            pi = 0
            for bi, (t0, n) in enumerate(BLKS1):
                garow = 5 if bi == 4 else 1
                for dc in range(8):
                    pb = pi % 8; pi += 1
                    for k in range(8):
                        mm(s, ps[:, pb, 0:n], wo_bf[:, k, dc * 128:(dc + 1) * 128], mix_sb[:, k, t0:t0 + n],
                           k == 0, k == 7, R=[bwobf[k], bmix[bi], bmixl], W=[bps[pb]])
                    s.op("dve", lambda e: e.scalar_tensor_tensor(out=x_sb[:, dc, t0:t0 + n], in0=ps[:, pb, 0:n],
                                                                 scalar=mod_sb[:, garow, dc:dc + 1],
                                                                 in1=x_sb[:, dc, t0:t0 + n], op0=ALU.mult, op1=ALU.add),
                         R=[bps[pb], bmod, bxb[bi]], W=[bxb[bi]])
            s.barrier()

        with (nc.sbuf_tensor("s2_sq", [128, 8, 512], BF16) as sq_sb,
              nc.sbuf_tensor("s2_rstd", [128, 2, 512], F32) as rstd_sb,
              nc.sbuf_tensor("s2_tmp", [128, 4, 512], F32) as tmp_sb,
              nc.sbuf_tensor("s2_hf", [128, 2, 8, 512], F32) as hf_sb,
              nc.sbuf_tensor("s2_ab", [128, 2, 8], F32) as ab_sb,
              nc.sbuf_tensor("s2_wge", [128, 8, 36], F32) as wge_sb,
              nc.sbuf_tensor("s2_bge", [128, 36], F32) as bge_sb,
              nc.sbuf_tensor("s2_id", [128, 128], F32) as id_sb,
              nc.sbuf_tensor("s2_rt", [128, 2, 128], F32) as rt_sb):
            bsq = s.buf("sq"); brs = [s.buf(f"rs{i}") for i in range(2)]
            btmp = [s.buf(f"tmp{i}") for i in range(4)]
            bhf = [s.buf(f"hf{i}") for i in range(2)]
            bab = s.buf("ab")
            bwge = s.buf("wge", cs); bbge = s.buf("bge", cs); bid = s.buf("id", cs)
            brt = [s.buf(f"rt{i}") for i in range(2)]
            s.dma("act", wge_sb[:], wge, W=[bwge])
            s.dma("act", bge_sb[:], bge, W=[bbge])
            s.dma("act", id_sb[:], ident, W=[bid])
            for t in range(2):
                s.op("dve", lambda e: e.scalar_tensor_tensor(
                    out=ab_sb[:, t, :], in0=mod_sb[:, 2 + 4 * t, :], scalar=1.0, in1=mod_sb[:, 0, :],
                    op0=ALU.add, op1=ALU.mult), R=[bmod], W=[bab])
            ti_g = 0
            for bi, (t0, n) in enumerate(BLKS1):
                sl = bi % 2
                isctx = bi == 4
                s.op("act", lambda e: e.activation(out=sq_sb[:, :, 0:n], in_=x_sb[:, :, t0:t0 + n], func=AF.Square),
                     R=[bxb[bi]], W=[bsq])
                for k in range(8):
                    mm(s, ps[:, sl, 0:n], ones_sb[:], sq_sb[:, k, 0:n], k == 0, k == 7, R=[bones, bsq], W=[bps[sl]])
                s.op("act", lambda e: e.activation(out=rstd_sb[:, sl, 0:n], in_=ps[:, sl, 0:n], func=AF.Sqrt,
                                                   scale=1.0 / D, bias=EPS), R=[bps[sl]], W=[brs[sl]])
                s.op("dve", lambda e: e.reciprocal(out=rstd_sb[:, sl, 0:n], in_=rstd_sb[:, sl, 0:n]),
                     R=[brs[sl]], W=[brs[sl]])
                ai = 1 if isctx else 0
                shrow = 7 if isctx else 3
                for k in range(8):
                    tb = k % 4
                    s.op("dve", lambda e: e.scalar_tensor_tensor(
                        out=tmp_sb[:, tb, 0:n], in0=x_sb[:, k, t0:t0 + n], scalar=ab_sb[:, ai, k:k + 1],
                        in1=rstd_sb[:, sl, 0:n], op0=ALU.mult, op1=ALU.mult),
                        R=[bxb[bi], bab, brs[sl]], W=[btmp[tb]])
                    s.op("act", lambda e: e.activation(out=hf_sb[:, sl, k, 0:n], in_=tmp_sb[:, tb, 0:n],
                                                       func=AF.Identity, bias=mod_sb[:, shrow, k:k + 1], scale=1.0),
                         R=[btmp[tb], bmod], W=[bhf[sl]])
                s.op("pool", lambda e: e.tensor_copy(out=hl2_sb[:, :, t0:t0 + n], in_=hf_sb[:, sl, :, 0:n]),
                     R=[bhf[sl]], W=[bhl2[bi]])
                for tt in range((n + 127) // 128):
                    c0 = tt * 128
                    m = min(128, n - c0)
                    rs_ = ti_g % 2; ti_g += 1
                    pb = 2 + rs_
                    rt = rt_sb[0:m, rs_, :]
                    brr = brt[rs_]
                    for k in range(8):
                        mm(s, ps[0:m, pb, 0:36], hf_sb[:, sl, k, c0:c0 + m], wge_sb[:, k, :], k == 0, k == 7,
                           R=[bhf[sl], bwge], W=[bps[pb]])
                    V = lambda eng, fn, R_=(), W_=(): s.op(eng, fn, R=[brr] + list(R_), W=[brr] + list(W_))
                    lg = rt[:, 0:36]
                    s.op("dve", lambda e: e.tensor_tensor(out=lg, in0=ps[0:m, pb, 0:36], in1=bge_sb[0:m, :], op=ALU.add),
                         R=[bps[pb], bbge], W=[brr])
                    gmax = rt[:, 36:37]; ngmax = rt[:, 37:38]; sume = rt[:, 38:39]; gtop = rt[:, 39:40]
                    eg = rt[:, 40:44]; ohg = rt[:, 44:48]; sel = rt[:, 48:56]; top8 = rt[:, 56:64]
                    dd = rt[:, 64:65]; ed = rt[:, 65:66]; w1_ = rt[:, 66:67]; wt1 = rt[:, 67:68]; wt2 = rt[:, 68:69]
                    ea = rt[:, 72:80]; eb_ = rt[:, 80:88]; wd = rt[:, 96:128]
                    V("dve", lambda e: e.reduce_max(out=gmax, in_=lg[:, 0:4], axis=AX.X))
                    V("dve", lambda e: e.tensor_scalar(out=ngmax, in0=gmax, scalar1=-1.0, scalar2=None, op0=ALU.mult))
                    V("act", lambda e: e.activation(out=eg, in_=lg[:, 0:4], func=AF.Exp, bias=ngmax, scale=1.0,
                                                    accum_out=sume))
                    V("dve", lambda e: e.reciprocal(out=gtop, in_=sume))
                    V("dve", lambda e: e.tensor_scalar(out=ohg, in0=lg[:, 0:4], scalar1=gmax, scalar2=None,
                                                       op0=ALU.is_equal))
                    V("dve", lambda e: e.tensor_scalar(out=sel, in0=lg[:, 4:12], scalar1=ohg[:, 0:1], scalar2=None,
                                                       op0=ALU.mult))
                    for g in range(1, 4):
                        V("dve", lambda e: e.scalar_tensor_tensor(out=sel, in0=lg[:, 4 + 8 * g:12 + 8 * g],
                                                                  scalar=ohg[:, g:g + 1], in1=sel,
                                                                  op0=ALU.mult, op1=ALU.add))
                    V("dve", lambda e: e.max(out=top8, in_=sel))
                    V("dve", lambda e: e.tensor_tensor(out=dd, in0=top8[:, 1:2], in1=top8[:, 0:1], op=ALU.subtract))
                    V("act", lambda e: e.activation(out=ed, in_=dd, func=AF.Exp))
                    V("dve", lambda e: e.tensor_scalar(out=w1_, in0=ed, scalar1=1.0, scalar2=None, op0=ALU.add))
                    V("dve", lambda e: e.reciprocal(out=w1_, in_=w1_))
                    V("dve", lambda e: e.tensor_tensor(out=wt1, in0=w1_, in1=gtop, op=ALU.mult))
                    V("dve", lambda e: e.tensor_tensor(out=wt2, in0=wt1, in1=ed, op=ALU.mult))
                    V("dve", lambda e: e.tensor_scalar(out=ea, in0=sel, scalar1=top8[:, 0:1], scalar2=wt1,
                                                       op0=ALU.is_equal, op1=ALU.mult))
                    V("dve", lambda e: e.tensor_scalar(out=eb_, in0=sel, scalar1=top8[:, 1:2], scalar2=wt2,
                                                       op0=ALU.is_equal, op1=ALU.mult))
                    V("dve", lambda e: e.tensor_tensor(out=ea, in0=ea, in1=eb_, op=ALU.add))
                    for g in range(4):
                        V("dve", lambda e: e.tensor_scalar(out=wd[:, 8 * g:8 * g + 8], in0=ea, scalar1=ohg[:, g:g + 1],
                                                           scalar2=None, op0=ALU.mult))
                    pt = 4 + rs_
                    s.op("pe", lambda e: e.transpose(ps[0:32, pt, 0:m], wd, id_sb[0:m, 0:m]), R=[brr, bid], W=[bps[pt]])
                    s.op("act", lambda e: e.copy(out=wdt_sb[:, 0, t0 + c0:t0 + c0 + m], in_=ps[0:32, pt, 0:m]),
                         R=[bps[pt]], W=[bwdt[bi]])
                    s.op("dve", lambda e: e.tensor_tensor(out=wdt_sb[:, 1, t0 + c0:t0 + c0 + m], in0=ps[0:32, pt, 0:m],
                                                          in1=wdt_sb[:, 0, t0 + c0:t0 + c0 + m], op=ALU.subtract),
                         R=[bps[pt], bwdt[bi]], W=[bwdt[bi]])
            s.barrier()

        with (nc.sbuf_tensor("s3_st", [128, 3, 2048], F32) as st_sb,
              nc.sbuf_tensor("s3_wb", [128, 2, 6, 2048], BF16) as wb_sb,
              nc.sbuf_tensor("s3_sel", [32, NEXP * 128], BF16) as sel_sb,
              nc.sbuf_tensor("s3_wbc", [128, 2, 512], F32) as wbc_sb,
              nc.sbuf_tensor("s3_sg", [128, 2, 512], F32) as sg_sb,
              nc.sbuf_tensor("s3_t", [128, 2, 512], F32) as t3_sb,
              nc.sbuf_tensor("s3_g", [128, 2, 4, 512], BF16) as g_sb):
            bst = [s.buf(f"st{i}", s.dsem(f"st{i}")) for i in range(3)]
            bwb = [[s.buf(f"wb{a}_{p}") for p in range(6)] for a in range(2)]
            bsel = s.buf("sel", cs)
            bwbc = [s.buf(f"wbc{i}") for i in range(2)]
            bsg = [s.buf(f"sg{i}") for i in range(2)]
            bt3 = [s.buf(f"t3{i}") for i in range(2)]
            bg = [s.buf(f"g{i}") for i in range(2)]
            s.dma("act", sel_sb[:], selc, W=[bsel])
            w1v = w1.rearrange("e (kc p) f -> e p kc f", p=128)
            w3v = w3.rearrange("e (kc p) f -> e p kc f", p=128)
            w2v = w2.rearrange("e (fc p) d -> e p fc d", p=128)

            def piece_src(e, p):
                if p < 2:
                    return w1v[e, :, 4 * p:4 * p + 4, :]
                if p < 4:
                    return w3v[e, :, 4 * (p - 2):4 * (p - 2) + 4, :]
                return w2v[e, :, 2 * (p - 4):2 * (p - 4) + 2, :]

            def piece_dma(P):
                e, p = divmod(P, 6)
                if e >= NEXP:
                    return
                sl = P % 3
                dst = st_sb[:, sl, :]
                dst = dst.rearrange("q (a b) -> q a b", a=4) if p < 4 else dst.rearrange("q (a b) -> q a b", a=2)
                s.dma("sp", dst, piece_src(e, p), W=[bst[sl]])

            def piece_cast(P):
                e, p = divmod(P, 6)
                if e >= NEXP:
                    return
                sl = P % 3
                s.op("pool", lambda en: en.tensor_copy(out=wb_sb[:, e % 2, p, :], in_=st_sb[:, sl, :]),
                     R=[bst[sl]], W=[bwb[e % 2][p]])

            for P in range(3):
                piece_dma(P)
            for P in range(6):
                piece_cast(P)
                piece_dma(P + 3)
            gi = 0
            for ex in range(NEXP):
                a = ex % 2
                for bi, (t0, n) in enumerate(BLKS1):
                    if bi < 3:
                        for P in (6 * (ex + 1) + 2 * bi, 6 * (ex + 1) + 2 * bi + 1):
                            piece_cast(P)
                            piece_dma(P + 3)
                    garow = 8 if bi == 4 else 4
                    wr = gi % 2
                    gs = gi % 2
                    gi += 1
                    mm(s, ps[:, 6, 0:n], sel_sb[:, ex * 128:(ex + 1) * 128], wdt_sb[:, 0, t0:t0 + n], True, False,
                       R=[bsel, bwdt[bi]], W=[bps[6]])
                    mm(s, ps[:, 6, 0:n], sel_sb[:, ex * 128:(ex + 1) * 128], wdt_sb[:, 1, t0:t0 + n], False, True,
                       R=[bsel, bwdt[bi]], W=[bps[6]])
                    s.op("act", lambda e: e.copy(out=wbc_sb[:, wr, 0:n], in_=ps[:, 6, 0:n]), R=[bps[6]], W=[bwbc[wr]])
                    for fc in range(4):
                        pr = fc % 2
                        for which in range(2):
                            bank = 2 * pr + which
                            for k in range(8):
                                wv = wb_sb[:, a, 2 * which + k // 4, :].rearrange("q (a b) -> q a b", a=4)
                                mm(s, ps[:, bank, 0:n], wv[:, k % 4, fc * 128:(fc + 1) * 128], hl2_sb[:, k, t0:t0 + n],
                                   k == 0, k == 7, R=[bwb[a][2 * which + k // 4], bhl2[bi]], W=[bps[bank]])
                        s.op("act", lambda e: e.activation(out=sg_sb[:, pr, 0:n], in_=ps[:, 2 * pr, 0:n], func=AF.Silu),
                             R=[bps[2 * pr]], W=[bsg[pr]])
                        s.op("dve", lambda e: e.tensor_tensor(out=t3_sb[:, pr, 0:n], in0=ps[:, 2 * pr + 1, 0:n],
                                                              in1=sg_sb[:, pr, 0:n], op=ALU.mult),
                             R=[bps[2 * pr + 1], bsg[pr]], W=[bt3[pr]])
                        s.op("pool", lambda e: e.tensor_tensor(out=g_sb[:, gs, fc, 0:n], in0=t3_sb[:, pr, 0:n],
                                                               in1=wbc_sb[:, wr, 0:n], op=ALU.mult),
                             R=[bt3[pr], bwbc[wr]], W=[bg[gs]])
                    for dc in range(8):
                        bank = 4 + dc % 2
                        for fc in range(4):
                            wv = wb_sb[:, a, 4 + fc // 2, :].rearrange("q (a b) -> q a b", a=2)
                            mm(s, ps[:, bank, 0:n], wv[:, fc % 2, dc * 128:(dc + 1) * 128], g_sb[:, gs, fc, 0:n],
                               fc == 0, fc == 3, R=[bwb[a][4 + fc // 2], bg[gs]], W=[bps[bank]])
                        s.op("dve", lambda e: e.scalar_tensor_tensor(out=x_sb[:, dc, t0:t0 + n], in0=ps[:, bank, 0:n],
                                                                     scalar=mod_sb[:, garow, dc:dc + 1],
                                                                     in1=x_sb[:, dc, t0:t0 + n], op0=ALU.mult, op1=ALU.add),
                             R=[bps[bank], bmod, bxb[bi]], W=[bxb[bi]])
            s.barrier()

        with (nc.sbuf_tensor("s4_sq", [128, 8, 512], BF16) as sq_sb,
              nc.sbuf_tensor("s4_rstd", [128, 2, 512], F32) as rstd_sb):
            bsq = s.buf("sq4"); brs = [s.buf(f"rs4{i}") for i in range(2)]
            for bi, (t0, n) in enumerate(BLKS1):
                if final:
                    sl = bi % 2
                    s.op("act", lambda e: e.activation(out=sq_sb[:, :, 0:n], in_=x_sb[:, :, t0:t0 + n], func=AF.Square),
                         R=[bxb[bi]], W=[bsq])
                    for k in range(8):
                        mm(s, ps[:, sl, 0:n], ones_sb[:], sq_sb[:, k, 0:n], k == 0, k == 7, R=[bones, bsq], W=[bps[sl]])
                    s.op("act", lambda e: e.activation(out=rstd_sb[:, sl, 0:n], in_=ps[:, sl, 0:n], func=AF.Sqrt,
                                                       scale=1.0 / D, bias=EPS), R=[bps[sl]], W=[brs[sl]])
                    s.op("dve", lambda e: e.reciprocal(out=rstd_sb[:, sl, 0:n], in_=rstd_sb[:, sl, 0:n]),
                         R=[brs[sl]], W=[brs[sl]])
                    for k in range(8):
                        s.op("dve", lambda e: e.scalar_tensor_tensor(
                            out=x_sb[:, k, t0:t0 + n], in0=x_sb[:, k, t0:t0 + n], scalar=mod_sb[:, 9, k:k + 1],
                            in1=rstd_sb[:, sl, 0:n], op0=ALU.mult, op1=ALU.mult),
                            R=[bxb[bi], bmod, brs[sl]], W=[bxb[bi]])
                s.dma("sp", xo[:, :, t0:t0 + n], x_sb[:, :, t0:t0 + n], R=[bxb[bi]])
            s.finish(bxb)
    return nc


def build_p3a():
    nc = bass.Bass("TRN2", target_bir_lowering=False)
    din = lambda n, shp, dt: nc.dram_tensor(n, shp, dt, kind="ExternalInput").ap()
    xT = din("xT", [128, 8, NT1], F32)
    nadf = din("nadf", [128, 4, NT1], BF16)
    hg = din("hg", [128, 12, NT1], F32)
    wout = din("wout", [D, D], F32)
    mod = din("mod", [128, 10, 8], F32)
    wge = din("wge", [128, 8, 36], F32)
    bge = din("bge", [128, 36], F32)
    iota4 = din("iota4", [128, 4], F32)
    ident = din("ident", [128, 128], F32)
    xo = nc.dram_tensor("xo", [128, 8, NT1], F32, kind="ExternalOutput").ap()
    hl2o = nc.dram_tensor("hl2o", [128, 8, NT1], BF16, kind="ExternalOutput").ap()
    wdto = nc.dram_tensor("wdto", [32, 2, NT1], BF16, kind="ExternalOutput").ap()
    gido = nc.dram_tensor("gido", [128, 17], F32, kind="ExternalOutput").ap()
    s = Sched(nc)
    wov = wout.rearrange("(kc p) n -> p kc n", p=128)
    with (nc.psum_tensor("ps", [128, 8, 512], F32) as ps,
          nc.sbuf_tensor("x_sb", [128, 8, NT1], F32) as x_sb,
          nc.sbuf_tensor("mod_sb", [128, 10, 8], F32) as mod_sb,
          nc.sbuf_tensor("hl2_sb", [128, 8, NT1], BF16) as hl2_sb,
          nc.sbuf_tensor("wdt_sb", [32, 2, NT1], BF16) as wdt_sb,
          nc.sbuf_tensor("ones_sb", [128, 128], BF16) as ones_sb):
        bps = [s.buf(f"ps{i}") for i in range(8)]
        cs = s.dsem("const")
        bxb = [s.buf(f"x{i}", s.dsem(f"x{i}")) for i in range(len(BLKS1))]
        bmod = s.buf("mod", cs)
        bhl2 = [s.buf(f"hl2_{i}") for i in range(len(BLKS1))]
        bwdt = [s.buf(f"wdt{i}") for i in range(len(BLKS1))]
        bones = s.buf("ones")
        s.dma("sp", mod_sb[:], mod, W=[bmod])
        for bi, (t0, n) in enumerate(BLKS1):
            s.dma("sp", x_sb[:, :, t0:t0 + n], xT[:, :, t0:t0 + n], W=[bxb[bi]])
        s.op("pool", lambda e: e.memset(ones_sb[:], 1.0), W=[bones])

        with (nc.sbuf_tensor("s1_mix", [128, 8, NT1], BF16) as mix_sb,
              nc.sbuf_tensor("s1_wobf", [128, 8, D], BF16) as wo_bf,
              nc.sbuf_tensor("s1_wost", [128, 2, D], F32) as wo_st,
              nc.sbuf_tensor("s1_hg", [128, 1, 12, 512], F32) as hg_sb,
              nc.sbuf_tensor("s1_t", [128, 4, 512], F32) as t_sb):
            bmixl = s.buf("mixl", s.dsem("mixl"))
            bmix = [s.buf(f"mix{i}") for i in range(len(BLKS1))]
            bwost = [s.buf(f"wost{i}", s.dsem(f"wost{i}")) for i in range(2)]
            bwobf = [s.buf(f"wobf{k}") for k in range(8)]
            bhg = [s.buf(f"hg{i}", s.dsem(f"hg{i}")) for i in range(2)]
            bt = [s.buf(f"t{i}") for i in range(4)]
            s.dma("act", mix_sb[:, 0:2, :], nadf[:, 0:2, :], W=[bmixl])
            s.dma("act", mix_sb[:, 6:8, :], nadf[:, 2:4, :], W=[bmixl])
            for k in range(8):
                sl = k % 2
                s.dma("act", wo_st[:, sl, :], wov[:, k, :], W=[bwost[sl]])
                s.op("pool", lambda e: e.tensor_copy(out=wo_bf[:, k, :], in_=wo_st[:, sl, :]), R=[bwost[sl]], W=[bwobf[k]])
            for bi, (t0, n) in enumerate(BLKS1):
                sl = 0
                s.dma("sp", hg_sb[:, sl, :, 0:n], hg[:, :, t0:t0 + n], W=[bhg[sl]])
                for ch in range(4):
                    hf_ = hg_sb[:, sl, ch, 0:n]; hb_ = hg_sb[:, sl, 4 + ch, 0:n]; gr_ = hg_sb[:, sl, 8 + ch, 0:n]
                    s.op("dve", lambda e: e.tensor_tensor(out=t_sb[:, 0, 0:n], in0=hf_, in1=hb_, op=ALU.add),
                         R=[bhg[sl]], W=[bt[0]])
                    s.op("dve", lambda e: e.tensor_tensor(out=t_sb[:, 1, 0:n], in0=gr_, in1=gr_, op=ALU.mult),
                         R=[bhg[sl]], W=[bt[1]])
                    s.op("dve", lambda e: e.tensor_scalar(out=t_sb[:, 1, 0:n], in0=t_sb[:, 1, 0:n], scalar1=0.044715,
                                                          scalar2=1.0, op0=ALU.mult, op1=ALU.add), R=[bt[1]], W=[bt[1]])
                    s.op("pool", lambda e: e.tensor_tensor(out=t_sb[:, 2, 0:n], in0=t_sb[:, 1, 0:n], in1=gr_, op=ALU.mult),
                         R=[bt[1], bhg[sl]], W=[bt[2]])
                    s.op("act", lambda e: e.activation(out=t_sb[:, 2, 0:n], in_=t_sb[:, 2, 0:n], func=AF.Sigmoid,
                                                       scale=GELU_C), R=[bt[2]], W=[bt[2]])
                    s.op("pool", lambda e: e.tensor_tensor(out=t_sb[:, 3, 0:n], in0=t_sb[:, 2, 0:n], in1=gr_, op=ALU.mult),
                         R=[bt[2], bhg[sl]], W=[bt[3]])
                    s.op("pool", lambda e: e.tensor_tensor(out=mix_sb[:, 2 + ch, t0:t0 + n], in0=t_sb[:, 3, 0:n],
                                                           in1=t_sb[:, 0, 0:n], op=ALU.mult),
                         R=[bt[3], bt[0]], W=[bmix[bi]])
            pi = 0
            for bi, (t0, n) in enumerate(BLKS1):
                garow = 5 if bi == 4 else 1
                for dc in range(8):
                    pb = pi % 8; pi += 1
                    for k in range(8):
                        mm(s, ps[:, pb, 0:n], wo_bf[:, k, dc * 128:(dc + 1) * 128], mix_sb[:, k, t0:t0 + n],
                           k == 0, k == 7, R=[bwobf[k], bmix[bi], bmixl], W=[bps[pb]])
                    s.op("dve", lambda e: e.scalar_tensor_tensor(out=x_sb[:, dc, t0:t0 + n], in0=ps[:, pb, 0:n],
                                                                 scalar=mod_sb[:, garow, dc:dc + 1],
                                                                 in1=x_sb[:, dc, t0:t0 + n], op0=ALU.mult, op1=ALU.add),
                         R=[bps[pb], bmod, bxb[bi]], W=[bxb[bi]])
            s.barrier()

        with (nc.sbuf_tensor("s2_sq", [128, 8, 512], BF16) as sq_sb,
              nc.sbuf_tensor("s2_rstd", [128, 2, 512], F32) as rstd_sb,
              nc.sbuf_tensor("s2_tmp", [128, 4, 512], F32) as tmp_sb,
              nc.sbuf_tensor("s2_hf", [128, 2, 8, 512], F32) as hf_sb,
              nc.sbuf_tensor("s2_ab", [128, 2, 8], F32) as ab_sb,
              nc.sbuf_tensor("s2_wge", [128, 8, 36], F32) as wge_sb,
              nc.sbuf_tensor("s2_bge", [128, 36], F32) as bge_sb,
              nc.sbuf_tensor("s2_id", [128, 128], F32) as id_sb,
              nc.sbuf_tensor("s2_rt", [128, 2, 128], F32) as rt_sb,
              nc.sbuf_tensor("s2_io4", [128, 4], F32) as io4_sb,
              nc.sbuf_tensor("s2_gid", [128, 17], F32) as gid_sb,
              nc.sbuf_tensor("s2_junk", [128, 4], F32) as junk4_sb):
            bsq = s.buf("sq"); brs = [s.buf(f"rs{i}") for i in range(2)]
            btmp = [s.buf(f"tmp{i}") for i in range(4)]
            bhf = [s.buf(f"hf{i}") for i in range(2)]
            bab = s.buf("ab")
            bwge = s.buf("wge", cs); bbge = s.buf("bge", cs); bid = s.buf("id", cs)
            brt = [s.buf(f"rt{i}") for i in range(2)]
            s.dma("act", wge_sb[:], wge, W=[bwge])
            s.dma("act", bge_sb[:], bge, W=[bbge])
            s.dma("act", id_sb[:], ident, W=[bid])
            bio4 = s.buf("io4", cs); bgid = s.buf("gid", s.dsem("gid")); bjk = s.buf("junk4")
            s.dma("act", io4_sb[:], iota4, W=[bio4])
            s.op("pool", lambda e: e.memset(gid_sb[:], 0.0), W=[bgid])
            for t in range(2):
                s.op("dve", lambda e: e.scalar_tensor_tensor(
                    out=ab_sb[:, t, :], in0=mod_sb[:, 2 + 4 * t, :], scalar=1.0, in1=mod_sb[:, 0, :],
                    op0=ALU.add, op1=ALU.mult), R=[bmod], W=[bab])
            ti_g = 0
            for bi, (t0, n) in enumerate(BLKS1):
                sl = bi % 2
                isctx = bi == 4
                s.op("act", lambda e: e.activation(out=sq_sb[:, :, 0:n], in_=x_sb[:, :, t0:t0 + n], func=AF.Square),
                     R=[bxb[bi]], W=[bsq])
                for k in range(8):
                    mm(s, ps[:, sl, 0:n], ones_sb[:], sq_sb[:, k, 0:n], k == 0, k == 7, R=[bones, bsq], W=[bps[sl]])
                s.op("act", lambda e: e.activation(out=rstd_sb[:, sl, 0:n], in_=ps[:, sl, 0:n], func=AF.Sqrt,
                                                   scale=1.0 / D, bias=EPS), R=[bps[sl]], W=[brs[sl]])
                s.op("dve", lambda e: e.reciprocal(out=rstd_sb[:, sl, 0:n], in_=rstd_sb[:, sl, 0:n]),
                     R=[brs[sl]], W=[brs[sl]])
                ai = 1 if isctx else 0
                shrow = 7 if isctx else 3
                for k in range(8):
                    tb = k % 4
                    s.op("dve", lambda e: e.scalar_tensor_tensor(
                        out=tmp_sb[:, tb, 0:n], in0=x_sb[:, k, t0:t0 + n], scalar=ab_sb[:, ai, k:k + 1],
                        in1=rstd_sb[:, sl, 0:n], op0=ALU.mult, op1=ALU.mult),
                        R=[bxb[bi], bab, brs[sl]], W=[btmp[tb]])
                    s.op("act", lambda e: e.activation(out=hf_sb[:, sl, k, 0:n], in_=tmp_sb[:, tb, 0:n],
                                                       func=AF.Identity, bias=mod_sb[:, shrow, k:k + 1], scale=1.0),
                         R=[btmp[tb], bmod], W=[bhf[sl]])
                s.op("pool", lambda e: e.tensor_copy(out=hl2_sb[:, :, t0:t0 + n], in_=hf_sb[:, sl, :, 0:n]),
                     R=[bhf[sl]], W=[bhl2[bi]])
                for tt in range((n + 127) // 128):
                    c0 = tt * 128
                    m = min(128, n - c0)
                    rs_ = ti_g % 2; ti_g += 1
                    pb = 2 + rs_
                    rt = rt_sb[0:m, rs_, :]
                    brr = brt[rs_]
                    for k in range(8):
                        mm(s, ps[0:m, pb, 0:36], hf_sb[:, sl, k, c0:c0 + m], wge_sb[:, k, :], k == 0, k == 7,
                           R=[bhf[sl], bwge], W=[bps[pb]])
                    V = lambda eng, fn, R_=(), W_=(): s.op(eng, fn, R=[brr] + list(R_), W=[brr] + list(W_))
                    lg = rt[:, 0:36]
                    s.op("dve", lambda e: e.tensor_tensor(out=lg, in0=ps[0:m, pb, 0:36], in1=bge_sb[0:m, :], op=ALU.add),
                         R=[bps[pb], bbge], W=[brr])
                    gmax = rt[:, 36:37]; ngmax = rt[:, 37:38]; sume = rt[:, 38:39]; gtop = rt[:, 39:40]
                    eg = rt[:, 40:44]; ohg = rt[:, 44:48]; sel = rt[:, 48:56]; top8 = rt[:, 56:64]
                    dd = rt[:, 64:65]; ed = rt[:, 65:66]; w1_ = rt[:, 66:67]; wt1 = rt[:, 67:68]; wt2 = rt[:, 68:69]
                    ea = rt[:, 72:80]; eb_ = rt[:, 80:88]; wd = rt[:, 96:128]
                    V("dve", lambda e: e.reduce_max(out=gmax, in_=lg[:, 0:4], axis=AX.X))
                    V("dve", lambda e: e.tensor_scalar(out=ngmax, in0=gmax, scalar1=-1.0, scalar2=None, op0=ALU.mult))
                    V("act", lambda e: e.activation(out=eg, in_=lg[:, 0:4], func=AF.Exp, bias=ngmax, scale=1.0,
                                                    accum_out=sume))
                    V("dve", lambda e: e.reciprocal(out=gtop, in_=sume))
                    V("dve", lambda e: e.tensor_scalar(out=ohg, in0=lg[:, 0:4], scalar1=gmax, scalar2=None,
                                                       op0=ALU.is_equal))
                    tgl = (t0 + c0) // 128
                    s.op("dve", lambda e: e.scalar_tensor_tensor(out=junk4_sb[0:m, :], in0=ohg, scalar=1.0,
                                                                 in1=io4_sb[0:m, :], op0=ALU.mult, op1=ALU.mult,
                                                                 accum_out=gid_sb[0:m, tgl:tgl + 1]),
                         R=[brr, bio4, bjk], W=[bjk, bgid])
                    V("dve", lambda e: e.tensor_scalar(out=sel, in0=lg[:, 4:12], scalar1=ohg[:, 0:1], scalar2=None,
                                                       op0=ALU.mult))
                    for g in range(1, 4):
                        V("dve", lambda e: e.scalar_tensor_tensor(out=sel, in0=lg[:, 4 + 8 * g:12 + 8 * g],
                                                                  scalar=ohg[:, g:g + 1], in1=sel,
                                                                  op0=ALU.mult, op1=ALU.add))
                    V("dve", lambda e: e.max(out=top8, in_=sel))
                    V("dve", lambda e: e.tensor_tensor(out=dd, in0=top8[:, 1:2], in1=top8[:, 0:1], op=ALU.subtract))
                    V("act", lambda e: e.activation(out=ed, in_=dd, func=AF.Exp))
                    V("dve", lambda e: e.tensor_scalar(out=w1_, in0=ed, scalar1=1.0, scalar2=None, op0=ALU.add))
                    V("dve", lambda e: e.reciprocal(out=w1_, in_=w1_))
                    V("dve", lambda e: e.tensor_tensor(out=wt1, in0=w1_, in1=gtop, op=ALU.mult))
                    V("dve", lambda e: e.tensor_tensor(out=wt2, in0=wt1, in1=ed, op=ALU.mult))
                    V("dve", lambda e: e.tensor_scalar(out=ea, in0=sel, scalar1=top8[:, 0:1], scalar2=wt1,
                                                       op0=ALU.is_equal, op1=ALU.mult))
                    V("dve", lambda e: e.tensor_scalar(out=eb_, in0=sel, scalar1=top8[:, 1:2], scalar2=wt2,
                                                       op0=ALU.is_equal, op1=ALU.mult))
                    V("dve", lambda e: e.tensor_tensor(out=ea, in0=ea, in1=eb_, op=ALU.add))
                    for g in range(4):
                        V("dve", lambda e: e.tensor_scalar(out=wd[:, 8 * g:8 * g + 8], in0=ea, scalar1=ohg[:, g:g + 1],
                                                           scalar2=None, op0=ALU.mult))
                    pt = 4 + rs_
                    s.op("pe", lambda e: e.transpose(ps[0:32, pt, 0:m], wd, id_sb[0:m, 0:m]), R=[brr, bid], W=[bps[pt]])
                    s.op("act", lambda e: e.copy(out=wdt_sb[:, 0, t0 + c0:t0 + c0 + m], in_=ps[0:32, pt, 0:m]),
                         R=[bps[pt]], W=[bwdt[bi]])
                    s.op("dve", lambda e: e.tensor_tensor(out=wdt_sb[:, 1, t0 + c0:t0 + c0 + m], in0=ps[0:32, pt, 0:m],
                                                          in1=wdt_sb[:, 0, t0 + c0:t0 + c0 + m], op=ALU.subtract),
                         R=[bps[pt], bwdt[bi]], W=[bwdt[bi]])
            bxo = s.buf("xout", s.dsem("xout"))
            s.dma("sp", xo, x_sb[:], R=bxb, W=[bxo])
            s.dma("sp", hl2o, hl2_sb[:], R=bhl2, W=[bxo])
            s.dma("sp", wdto, wdt_sb[:], R=bwdt, W=[bxo])
            s.dma("sp", gido, gid_sb[:], R=[bgid], W=[bxo])
            s.finish([bxo])
    return nc


def build_p3b(ntb):
    NE = 8
    nblk_all = ntb // 512
    nh = 2 if ntb > 2048 else 1
    nblk = -(-nblk_all // nh)
    nth = nblk * 512
    nc = bass.Bass("TRN2", target_bir_lowering=False)
    din = lambda n, shp, dt: nc.dram_tensor(n, shp, dt, kind="ExternalInput").ap()
    hl2 = din("hl2", [128, 8, ntb], BF16)
    wdt = din("wdt", [8, 2, ntb], BF16)
    selc = din("selc", [8, NE * 128], BF16)
    w1 = din("w1", [NE, D, 512], F32)
    w3 = din("w3", [NE, D, 512], F32)
    w2 = din("w2", [NE, 512, D], F32)
    yo = nc.dram_tensor("yo", [128, 8, ntb], F32, kind="ExternalOutput").ap()
    s = Sched(nc)
    with (nc.psum_tensor("ps", [128, 8, 512], F32) as ps,
          nc.sbuf_tensor("y_sb", [128, 8, nth], F32) as y_sb,
          nc.sbuf_tensor("hl2_sb", [128, 8, nth], BF16) as hl2_sb,
          nc.sbuf_tensor("wdt_sb", [8, 2, ntb], BF16) as wdt_sb,
          nc.sbuf_tensor("s3_st", [128, 3, 2048], F32) as st_sb,
          nc.sbuf_tensor("s3_wb", [128, 2, 6, 2048], BF16) as wb_sb,
          nc.sbuf_tensor("s3_sel", [8, NE * 128], BF16) as sel_sb,
          nc.sbuf_tensor("s3_wbc", [128, 2, 512], F32) as wbc_sb,
          nc.sbuf_tensor("s3_sg", [128, 2, 512], F32) as sg_sb,
          nc.sbuf_tensor("s3_t", [128, 2, 512], F32) as t3_sb,
          nc.sbuf_tensor("s3_g", [128, 2, 4, 512], BF16) as g_sb):
        bps = [s.buf(f"ps{i}") for i in range(8)]
        cs = s.dsem("const")
        by = [s.buf(f"y{i}", s.dsem(f"y{i}")) for i in range(nblk)]
        bhl2 = [s.buf(f"hl2_{i}", s.dsem(f"hl{i}")) for i in range(nblk)]
        bwdt = s.buf("wdt", cs)
        bst = [s.buf(f"st{i}", s.dsem(f"st{i}")) for i in range(3)]
        bwb = [[s.buf(f"wb{a}_{p}") for p in range(6)] for a in range(2)]
        bsel = s.buf("sel", cs)
        bwbc = [s.buf(f"wbc{i}") for i in range(2)]
        bsg = [s.buf(f"sg{i}") for i in range(2)]
        bt3 = [s.buf(f"t3{i}") for i in range(2)]
        bg = [s.buf(f"g{i}") for i in range(2)]
        s.dma("act", sel_sb[:], selc, W=[bsel])
        s.dma("act", wdt_sb[:], wdt, W=[bwdt])
        w1v = w1.rearrange("e (kc p) f -> e p kc f", p=128)
        w3v = w3.rearrange("e (kc p) f -> e p kc f", p=128)
        w2v = w2.rearrange("e (fc p) d -> e p fc d", p=128)

        def piece_src(e, p):
            if p < 2:
                return w1v[e, :, 4 * p:4 * p + 4, :]
            if p < 4:
                return w3v[e, :, 4 * (p - 2):4 * (p - 2) + 4, :]
            return w2v[e, :, 2 * (p - 4):2 * (p - 4) + 2, :]

        def piece_dma(P):
            e, p = divmod(P, 6)
            if e >= NE * nh:
                return
            e = e % NE
            sl = P % 3
            dst = st_sb[:, sl, :]
            dst = dst.rearrange("q (a b) -> q a b", a=4) if p < 4 else dst.rearrange("q (a b) -> q a b", a=2)
            s.dma("sp", dst, piece_src(e, p), W=[bst[sl]])

        def piece_cast(P):
            e, p = divmod(P, 6)
            if e >= NE * nh:
                return
            sl = P % 3
            s.op("pool", lambda en: en.tensor_copy(out=wb_sb[:, e % 2, p, :], in_=st_sb[:, sl, :]),
                 R=[bst[sl]], W=[bwb[e % 2][p]])

        for P in range(3):
            piece_dma(P)
        for P in range(6):
            piece_cast(P)
            piece_dma(P + 3)
        gi = 0
        n = 512
        pend = [6 * 1 + i for i in range(6)]
        for vx in range(NE * nh):
            half, ex = divmod(vx, NE)
            a = vx % 2
            pend = [6 * (vx + 1) + i for i in range(6)]
            hb0 = half * nblk
            nb_h = min(nblk, nblk_all - hb0)
            if ex == 0:
                for bi in range(nb_h):
                    s.dma("act", hl2_sb[:, :, bi * 512:(bi + 1) * 512], hl2[:, :, (hb0 + bi) * 512:(hb0 + bi + 1) * 512],
                          W=[bhl2[bi]])
            for bi in range(nb_h):
                t0 = bi * 512
                tg = (hb0 + bi) * 512
                npc = (6 + nb_h - 1) // nb_h
                for P in pend[bi * npc:(bi + 1) * npc]:
                    piece_cast(P)
                    piece_dma(P + 3)
                wr = gi % 2
                gs = gi % 2
                gi += 1
                mm(s, ps[:, 6, 0:n], sel_sb[:, ex * 128:(ex + 1) * 128], wdt_sb[:, 0, tg:tg + n], True, False,
                   R=[bsel, bwdt], W=[bps[6]])
                mm(s, ps[:, 6, 0:n], sel_sb[:, ex * 128:(ex + 1) * 128], wdt_sb[:, 1, tg:tg + n], False, True,
                   R=[bsel, bwdt], W=[bps[6]])
                s.op("act", lambda e: e.copy(out=wbc_sb[:, wr, 0:n], in_=ps[:, 6, 0:n]), R=[bps[6]], W=[bwbc[wr]])
                for fc in range(4):
                    pr = fc % 2
                    for which in range(2):
                        bank = 2 * pr + which
                        for k in range(8):
                            wv = wb_sb[:, a, 2 * which + k // 4, :].rearrange("q (a b) -> q a b", a=4)
                            mm(s, ps[:, bank, 0:n], wv[:, k % 4, fc * 128:(fc + 1) * 128], hl2_sb[:, k, t0:t0 + n],
                               k == 0, k == 7, R=[bwb[a][2 * which + k // 4], bhl2[bi]], W=[bps[bank]])
                    s.op("act", lambda e: e.activation(out=sg_sb[:, pr, 0:n], in_=ps[:, 2 * pr, 0:n], func=AF.Silu),
                         R=[bps[2 * pr]], W=[bsg[pr]])
                    s.op("dve", lambda e: e.tensor_tensor(out=t3_sb[:, pr, 0:n], in0=ps[:, 2 * pr + 1, 0:n],
                                                          in1=sg_sb[:, pr, 0:n], op=ALU.mult),
                         R=[bps[2 * pr + 1], bsg[pr]], W=[bt3[pr]])
                    s.op("pool", lambda e: e.tensor_tensor(out=g_sb[:, gs, fc, 0:n], in0=t3_sb[:, pr, 0:n],
                                                           in1=wbc_sb[:, wr, 0:n], op=ALU.mult),
                         R=[bt3[pr], bwbc[wr]], W=[bg[gs]])
                for dc in range(8):
                    bank = 4 + dc % 2
                    for fc in range(4):
                        wv = wb_sb[:, a, 4 + fc // 2, :].rearrange("q (a b) -> q a b", a=2)
                        mm(s, ps[:, bank, 0:n], wv[:, fc % 2, dc * 128:(dc + 1) * 128], g_sb[:, gs, fc, 0:n],
                           fc == 0, fc == 3, R=[bwb[a][4 + fc // 2], bg[gs]], W=[bps[bank]])
                    if ex == 0:
                        s.op("dve", lambda e: e.tensor_copy(out=y_sb[:, dc, t0:t0 + n], in_=ps[:, bank, 0:n]),
                             R=[bps[bank]], W=[by[bi]])
                    else:
                        s.op("dve", lambda e: e.tensor_tensor(out=y_sb[:, dc, t0:t0 + n], in0=ps[:, bank, 0:n],
                                                              in1=y_sb[:, dc, t0:t0 + n], op=ALU.add),
                             R=[bps[bank], by[bi]], W=[by[bi]])
            if ex == NE - 1:
                for bi in range(nb_h):
                    s.dma("sp", yo[:, :, (hb0 + bi) * 512:(hb0 + bi + 1) * 512], y_sb[:, :, bi * 512:(bi + 1) * 512],
                          R=[by[bi]])
        s.finish(by)
    return nc


def build_pc(final):
    nc = bass.Bass("TRN2", target_bir_lowering=False)
    din = lambda n, shp, dt: nc.dram_tensor(n, shp, dt, kind="ExternalInput").ap()
    xT = din("xT", [128, 8, NT1], F32)
    yT = din("yT", [128, 8, NT1], F32)
    mod = din("mod", [128, 3, 8], F32)
    xo = nc.dram_tensor("xo", [128, 8, NT1], F32, kind="ExternalOutput").ap()
    s = Sched(nc)
    with (nc.psum_tensor("ps", [128, 2, 512], F32) as ps,
          nc.sbuf_tensor("x_sb", [128, 8, NT1], F32) as x_sb,
          nc.sbuf_tensor("y_sb", [128, 8, NT1], F32) as y_sb,
          nc.sbuf_tensor("mod_sb", [128, 3, 8], F32) as mod_sb,
          nc.sbuf_tensor("ones_sb", [128, 128], BF16) as ones_sb,
          nc.sbuf_tensor("sq_sb", [128, 8, 512], BF16) as sq_sb,
          nc.sbuf_tensor("rstd_sb", [128, 2, 512], F32) as rstd_sb):
        bxb = [s.buf(f"x{i}", s.dsem(f"x{i}")) for i in range(len(BLKS1))]
        byb = [s.buf(f"y{i}", s.dsem(f"yy{i}")) for i in range(len(BLKS1))]
        bmod = s.buf("mod", s.dsem("mod"))
        bones = s.buf("ones"); bsq = s.buf("sq"); brs = [s.buf(f"rs{i}") for i in range(2)]
        bps = [s.buf(f"ps{i}") for i in range(2)]
        s.dma("sp", mod_sb[:], mod, W=[bmod])
        s.op("pool", lambda e: e.memset(ones_sb[:], 1.0), W=[bones])
        for bi, (t0, n) in enumerate(BLKS1):
            s.dma("sp", x_sb[:, :, t0:t0 + n], xT[:, :, t0:t0 + n], W=[bxb[bi]])
            s.dma("act", y_sb[:, :, t0:t0 + n], yT[:, :, t0:t0 + n], W=[byb[bi]])
        for bi, (t0, n) in enumerate(BLKS1):
            garow = 1 if bi == 4 else 0
            sl = bi % 2
            for k in range(8):
                s.op("dve", lambda e: e.scalar_tensor_tensor(
                    out=x_sb[:, k, t0:t0 + n], in0=y_sb[:, k, t0:t0 + n], scalar=mod_sb[:, garow, k:k + 1],
                    in1=x_sb[:, k, t0:t0 + n], op0=ALU.mult, op1=ALU.add),
                    R=[byb[bi], bmod, bxb[bi]], W=[bxb[bi]])
            if final:
                s.op("act", lambda e: e.activation(out=sq_sb[:, :, 0:n], in_=x_sb[:, :, t0:t0 + n], func=AF.Square),
                     R=[bxb[bi]], W=[bsq])
                for k in range(8):
                    mm(s, ps[:, sl, 0:n], ones_sb[:], sq_sb[:, k, 0:n], k == 0, k == 7, R=[bones, bsq], W=[bps[sl]])
                s.op("act", lambda e: e.activation(out=rstd_sb[:, sl, 0:n], in_=ps[:, sl, 0:n], func=AF.Sqrt,
                                                   scale=1.0 / D, bias=EPS), R=[bps[sl]], W=[brs[sl]])
                s.op("dve", lambda e: e.reciprocal(out=rstd_sb[:, sl, 0:n], in_=rstd_sb[:, sl, 0:n]),
                     R=[brs[sl]], W=[brs[sl]])
                for k in range(8):
                    s.op("dve", lambda e: e.scalar_tensor_tensor(
                        out=x_sb[:, k, t0:t0 + n], in0=x_sb[:, k, t0:t0 + n], scalar=mod_sb[:, 2, k:k + 1],
                        in1=rstd_sb[:, sl, 0:n], op0=ALU.mult, op1=ALU.mult),
                        R=[bxb[bi], bmod, brs[sl]], W=[bxb[bi]])
            s.dma("sp", xo[:, :, t0:t0 + n], x_sb[:, :, t0:t0 + n], R=[bxb[bi]])
        s.finish(bxb)
    return nc


def run_p3(nc3, l, xl, xc, yna, ydf, hf, hb, fmf, mods_l, inp, g_final):
    wge = np.concatenate([inp["router_w_group"][l], inp["router_w_expert"][l]], axis=1)
    wge = np.ascontiguousarray(wge.reshape(8, 128, 36).transpose(1, 0, 2))
    bge = np.concatenate([inp["router_b_group"][l], inp["router_b_expert"][l]])
    bge = np.ascontiguousarray(np.tile(bge[None, :], (128, 1))).astype(np.float32)
    selc = np.zeros((32, NEXP, 128), np.float32)
    for e in range(NEXP):
        selc[e, e, :] = 1.0
    selc = selc.reshape(32, NEXP * 128).astype(ml_dtypes.bfloat16)
    ident = np.eye(128, dtype=np.float32)
    in_maps = []
    for i in range(NCORES):
        b, j = i // 4, i % 4
        lat = slice(2048 * j, 2048 * (j + 1))
        ctxs = slice(S + 64 * j, S + 64 * (j + 1))
        xx = np.concatenate([xl[b, lat], xc[b, 64 * j:64 * (j + 1)]], axis=0)
        na = np.concatenate([yna[b, lat], yna[b, ctxs]], axis=0)
        df = np.concatenate([ydf[b, lat], ydf[b, ctxs]], axis=0)
        nadf = np.concatenate([na, df], axis=1)
        nadf = np.ascontiguousarray(nadf.T.reshape(4, 128, NT1).transpose(1, 0, 2))
        def tk(a):
            aa = np.concatenate([a[:, lat], a[:, ctxs]], axis=1)
            return aa.reshape(4, 128, NT1).transpose(1, 0, 2)
        hgt = np.ascontiguousarray(np.concatenate([tk(hf[b]), tk(hb[b]), tk(fmf[b, 512:1024])], axis=1))
        m = mods_l
        rows = [inp["g_ffn"][l], m[b, 2048:3072], m[b, 4096:5120], m[b, 3072:4096], m[b, 5120:6144],
                m[2, 2048:3072], m[2, 4096:5120], m[2, 3072:4096], m[2, 5120:6144], g_final]
        mod = np.ascontiguousarray(np.stack([vec_pk(r) for r in rows], axis=1)).astype(np.float32)
        in_maps.append({"xT": chunkT(xx), "nadf": nadf, "hg": hgt, "wout": inp["w_out"][l], "mod": mod,
                        "wge": wge, "bge": bge, "selc": selc, "ident": ident,
                        "w1": inp["moe_w1"][l], "w3": inp["moe_w3"][l], "w2": inp["moe_w2"][l]})
    res = run_bass_kernel_spmd(nc3, in_maps, core_ids=list(range(NCORES)))
    xl2 = np.zeros_like(xl); xc2 = np.zeros_like(xc)
    for i in range(NCORES):
        b, j = i // 4, i % 4
        o = res.results[i]["xo"].transpose(1, 0, 2).reshape(D, NT1).T
        xl2[b, 2048 * j:2048 * (j + 1)] = o[:2048]
        xc2[b, 64 * j:64 * (j + 1)] = o[2048:]
    return xl2, xc2


def p3_inmaps_common(l, xl, xc, yna, ydf, hf, hb, fmf, mods_l, inp, g_final):
    wge = np.concatenate([inp["router_w_group"][l], inp["router_w_expert"][l]], axis=1)
    wge = np.ascontiguousarray(wge.reshape(8, 128, 36).transpose(1, 0, 2))
    bge = np.concatenate([inp["router_b_group"][l], inp["router_b_expert"][l]])
    bge = np.ascontiguousarray(np.tile(bge[None, :], (128, 1))).astype(np.float32)
    ident = np.eye(128, dtype=np.float32)
    iota4 = np.ascontiguousarray(np.tile(np.arange(4, dtype=np.float32)[None, :], (128, 1)))
    in_maps = []
    for i in range(NCORES):
        b, j = i // 4, i % 4
        lat = slice(2048 * j, 2048 * (j + 1))
        ctxs = slice(S + 64 * j, S + 64 * (j + 1))
        xx = np.concatenate([xl[b, lat], xc[b, 64 * j:64 * (j + 1)]], axis=0)
        na = np.concatenate([yna[b, lat], yna[b, ctxs]], axis=0)
        df = np.concatenate([ydf[b, lat], ydf[b, ctxs]], axis=0)
        nadf = np.concatenate([na, df], axis=1)
        nadf = np.ascontiguousarray(nadf.T.reshape(4, 128, NT1).transpose(1, 0, 2))

        def tk(a):
            aa = np.concatenate([a[:, lat], a[:, ctxs]], axis=1)
            return aa.reshape(4, 128, NT1).transpose(1, 0, 2)
        hgt = np.ascontiguousarray(np.concatenate([tk(hf[b]), tk(hb[b]), tk(fmf[b, 512:1024])], axis=1))
        m = mods_l
        rows = [inp["g_ffn"][l], m[b, 2048:3072], m[b, 4096:5120], m[b, 3072:4096], m[b, 5120:6144],
                m[2, 2048:3072], m[2, 4096:5120], m[2, 3072:4096], m[2, 5120:6144], g_final]
        mod = np.ascontiguousarray(np.stack([vec_pk(r) for r in rows], axis=1)).astype(np.float32)
        in_maps.append({"xT": chunkT(xx), "nadf": nadf, "hg": hgt, "wout": inp["w_out"][l], "mod": mod,
                        "wge": wge, "bge": bge, "ident": ident, "iota4": iota4})
    return in_maps


def run_p3_sparse(l, xl, xc, yna, ydf, hf, hb, fmf, mods_l, inp, g_final, final):
    in_maps = p3_inmaps_common(l, xl, xc, yna, ydf, hf, hb, fmf, mods_l, inp, g_final)
    resa = run_bass_kernel_spmd(build_p3a(), in_maps, core_ids=list(range(NCORES))).results
    NTT = NCORES * NT1
    HL2 = np.zeros((D, NTT), ml_dtypes.bfloat16)
    WDT = np.zeros((32, 2, NTT), ml_dtypes.bfloat16)
    gid = np.zeros(NTT, np.int64)
    for i in range(NCORES):
        HL2[:, i * NT1:(i + 1) * NT1] = resa[i]["hl2o"].transpose(1, 0, 2).reshape(D, NT1)
        WDT[:, :, i * NT1:(i + 1) * NT1] = resa[i]["wdto"]
        g = resa[i]["gido"]
        gid[i * NT1:(i + 1) * NT1] = np.rint(g.T.reshape(-1)[:NT1]).astype(np.int64)
    toks = [np.nonzero(gid == g)[0] for g in range(4)]
    ncg = [1, 1, 1, 1]
    for _ in range(NCORES - 4):
        gbig = max(range(4), key=lambda g: len(toks[g]) / ncg[g])
        ncg[gbig] += 1
    idxs = []
    cgroup = []
    for g in range(4):
        parts = np.array_split(toks[g], ncg[g])
        for pp in parts:
            idxs.append(pp); cgroup.append(g)
    ntb = max(512, int(-(-max(len(ix) for ix in idxs) // 512) * 512))
    sel8 = np.zeros((8, 8, 128), np.float32)
    for e in range(8):
        sel8[e, e, :] = 1.0
    sel8 = sel8.reshape(8, 1024).astype(ml_dtypes.bfloat16)
    mapsb = []
    for c in range(NCORES):
        g = cgroup[c]
        ix = idxs[c]
        h2 = np.zeros((D, ntb), ml_dtypes.bfloat16)
        h2[:, :len(ix)] = HL2[:, ix]
        wd = np.zeros((8, 2, ntb), ml_dtypes.bfloat16)
        wd[:, :, :len(ix)] = WDT[8 * g:8 * g + 8][:, :, ix]
        mapsb.append({"hl2": np.ascontiguousarray(h2.reshape(8, 128, ntb).transpose(1, 0, 2)), "wdt": wd, "selc": sel8,
                      "w1": np.ascontiguousarray(inp["moe_w1"][l][8 * g:8 * g + 8]),
                      "w3": np.ascontiguousarray(inp["moe_w3"][l][8 * g:8 * g + 8]),
                      "w2": np.ascontiguousarray(inp["moe_w2"][l][8 * g:8 * g + 8])})
    resb = run_bass_kernel_spmd(build_p3b(ntb), mapsb, core_ids=list(range(NCORES))).results
    Y = np.zeros((D, NTT), np.float32)
    for c in range(NCORES):
        ix = idxs[c]
        Y[:, ix] = resb[c]["yo"].transpose(1, 0, 2).reshape(D, ntb)[:, :len(ix)]
    mapsc = []
    for i in range(NCORES):
        b = i // 4
        modc = np.stack([vec_pk(mods_l[b, 5120:6144]), vec_pk(mods_l[2, 5120:6144]), vec_pk(g_final)], axis=1)
        mapsc.append({"xT": resa[i]["xo"], "mod": np.ascontiguousarray(modc).astype(np.float32),
                      "yT": np.ascontiguousarray(Y[:, i * NT1:(i + 1) * NT1].reshape(8, 128, NT1).transpose(1, 0, 2))})
    resc = run_bass_kernel_spmd(build_pc(final), mapsc, core_ids=list(range(NCORES))).results
    xl2 = np.zeros_like(xl); xc2 = np.zeros_like(xc)
    for i in range(NCORES):
        b, j = i // 4, i % 4
        o = resc[i]["xo"].transpose(1, 0, 2).reshape(D, NT1).T
        xl2[b, 2048 * j:2048 * (j + 1)] = o[:2048]
        xc2[b, 64 * j:64 * (j + 1)] = o[2048:]
    return xl2, xc2


def kernel(**inputs):
    inp = {k: np.asarray(v) for k, v in inputs.items()}
    x = np.ascontiguousarray(inp["x"], dtype=np.float32)
    ctx = np.ascontiguousarray(inp["ctx"], dtype=np.float32)
    mods = run_p0(inp["c"], inp["c_ctx"], inp["w_ada"], inp["b_ada"])
    cosT, sinT = rope_tables()
    xl, xc = x, ctx
    for l in range(DEPTH):
        fmb, fmf, tm = run_p1(build_p1(), xl, xc, mods[l], inp["g_mix"][l], inp["w_in"][l], cosT, sinT)
        lam_init = 0.8 - 0.6 * math.exp(-0.3 * l)
        yna, ydf, hf, hb = run_p2(build_p2(lam_init), l, fmb, fmf, tm, inp)
        xl, xc = run_p3_sparse(l, xl, xc, yna, ydf, hf, hb, fmf, mods[l], inp, inp["g_final"], l == DEPTH - 1)
    return np.ascontiguousarray(xl, dtype=np.float32)
```

```python
import math
import numpy as np
import ml_dtypes
import concourse.bass as bass
import concourse.mybir as mybir
from concourse.bass_utils import run_bass_kernel_spmd

F32 = mybir.dt.float32
BF16 = mybir.dt.bfloat16
I32 = mybir.dt.int32
U32 = mybir.dt.uint32
AF = mybir.ActivationFunctionType
ALU = mybir.AluOpType
AX = mybir.AxisListType

NCORES = 8
D = 1024
B = 2
S = 8192
L = 256
DEPTH = 4
GRID_W = 64
EPS = 1e-6


class Buf:
    __slots__ = ("name", "w", "r", "dsem")

    def __init__(self, name, dsem=None):
        self.name = name
        self.w = None
        self.r = []
        self.dsem = dsem


class DmaSem:
    def __init__(self, sched, name):
        self.sem = sched.nc.alloc_semaphore(name)
        self.key = ("dma", name)
        self.total = 0
        sched.sems[self.key] = self


class Sched:
    def __init__(self, nc):
        self.nc = nc
        self.eng = {"pe": nc.tensor, "dve": nc.vector, "act": nc.scalar,
                    "pool": nc.gpsimd, "sp": nc.sync}
        self.sems = {}
        self.esem = {}
        self.cnt = {}
        for k in self.eng:
            self.esem[k] = nc.alloc_semaphore("e_" + k)
            self.cnt[k] = 0
        self.seen = {}
        self.nbuf = 0
        self.out_tokens = []

    def buf(self, name=None, dsem=None):
        self.nbuf += 1
        return Buf(name or f"b{self.nbuf}", dsem)

    def dsem(self, name):
        return DmaSem(self, name)

    def _semof(self, key):
        if key[0] == "dma":
            return self.sems[key].sem
        return self.esem[key[0]]

    def _wait(self, engname, deps):
        e = self.eng[engname]
        for key, val in deps.items():
            if key[0] == "dma":
                val = max(val, 0)
            if self.seen.get((engname, key), 0) >= val:
                continue
            self.seen[(engname, key)] = val
            e.wait_ge(self._semof(key), val)

    def _deps(self, R, W):
        deps = {}

        def add(tok):
            if tok is None:
                return
            key, val = tok
            if key[0] == "dma":
                val = self.sems[key].total
            if deps.get(key, 0) < val:
                deps[key] = val
        for b in R:
            add(b.w)
        for b in W:
            add(b.w)
            for t in b.r:
                add(t)
        return deps

    def _commit(self, tok, R, W):
        for b in R:
            b.r.append(tok)
        for b in W:
            b.w = tok
            b.r = []

    def op(self, engname, fn, R=(), W=()):
        deps = self._deps(R, W)
        if engname == "pe":
            deps.pop(("pe",), None)
        self._wait(engname, deps)
        ins = fn(self.eng[engname])
        self.cnt[engname] += 1
        ins.then_inc(self.esem[engname], 1)
        tok = ((engname,), self.cnt[engname])
        self._commit(tok, R, W)
        return tok

    def dma(self, q, out, in_, R=(), W=(), sem=None, **kw):
        deps = self._deps(R, W)
        self._wait(q, deps)
        ds = sem
        if ds is None:
            for b in list(W) + list(R):
                if b.dsem is not None:
                    ds = b.dsem
                    break
        assert ds is not None, "dma needs a DmaSem"
        ins = self.eng[q].dma_start(out=out, in_=in_, **kw)
        ds.total += 16
        ins.then_inc(ds.sem, 16)
        tok = (ds.key, ds.total)
        self._commit(tok, R, W)
        return tok

    def barrier(self, bufs=()):
        deps = {}
        for k in self.eng:
            if self.cnt[k]:
                deps[(k,)] = self.cnt[k]
        for key, ds in self.sems.items():
            if ds.total:
                deps[key] = ds.total
        for k in self.eng:
            d = {kk: v for kk, v in deps.items() if kk != (k,)}
            self._wait(k, d)

    def coll(self, kind, ins, outs, R=(), W=(), groups=None):
        deps = self._deps(R, W)
        self._wait("pool", deps)
        ds = None
        for b in list(W) + list(R):
            if b.dsem is not None:
                ds = b.dsem
                break
        g = groups or [[0, 1, 2, 3], [4, 5, 6, 7]]
        ins_ = self.nc.gpsimd.collective_compute(kind, ALU.bypass, replica_groups=g, ins=ins, outs=outs)
        ds.total += 16
        ins_.then_inc(ds.sem, 16)
        tok = (ds.key, ds.total)
        self._commit(tok, R, W)
        return tok

    def finish(self, bufs, engname="sp"):
        deps = {}
        for b in bufs:
            for tok in ([b.w] if b.w else []) + b.r:
                key, val = tok
                if key[0] == "dma":
                    val = self.sems[key].total
                deps[key] = max(deps.get(key, 0), val)
        self._wait(engname, deps)


def mm(s, out, lhsT, rhs, start, stop, R, W):
    return s.op("pe", lambda e: e.matmul(out, lhsT, rhs, start=start, stop=stop), R=R, W=W)


def build_p0():
    nc = bass.Bass("TRN2", target_bir_lowering=False)
    NCOL = 3072
    NJ = NCOL // 128
    cT = nc.dram_tensor("cT", [128, 8, 4], F32, kind="ExternalInput").ap()
    w = nc.dram_tensor("w", [D, NCOL], F32, kind="ExternalInput").ap()
    bvec = nc.dram_tensor("bvec", [128, NJ], F32, kind="ExternalInput").ap()
    out = nc.dram_tensor("out", [128, NJ, 4], F32, kind="ExternalOutput").ap()
    s = Sched(nc)
    wv = w.rearrange("(kc p) n -> p kc n", p=128)
    with (nc.sbuf_tensor("w_sb", [128, 8, NCOL], F32) as w_sb,
          nc.sbuf_tensor("c_sb", [128, 8, 4], F32) as c_sb,
          nc.sbuf_tensor("s_sb", [128, 8, 4], F32) as s_sb,
          nc.sbuf_tensor("b_sb", [128, NJ], F32) as b_sb,
          nc.sbuf_tensor("r_sb", [128, NJ, 4], F32) as r_sb,
          nc.psum_tensor("ps", [128, 8, 512], F32) as ps):
        bw = [s.buf(f"w{k}", s.dsem(f"w{k}")) for k in range(8)]
        bc = s.buf("c", s.dsem("c"))
        bb = s.buf("b", bc.dsem)
        bs = s.buf("s")
        br = s.buf("r", s.dsem("r"))
        bps = [s.buf(f"ps{i}") for i in range(8)]
        s.dma("sp", c_sb[:], cT, W=[bc])
        s.dma("sp", b_sb[:], bvec, W=[bb])
        for k in range(8):
            s.dma("sp" if k % 2 == 0 else "act", w_sb[:, k, :], wv[:, k, :], W=[bw[k]])
        s.op("act", lambda e: e.activation(out=s_sb[:], in_=c_sb[:], func=AF.Silu), R=[bc], W=[bs])
        for j in range(NJ):
            pb = bps[j % 8]
            for k in range(8):
                mm(s, ps[:, j % 8, 0:4], w_sb[:, k, j * 128:(j + 1) * 128], s_sb[:, k, :],
                   k == 0, k == 7, R=[bw[k], bs], W=[pb])
            s.op("dve", lambda e: e.tensor_scalar(out=r_sb[:, j, :], in0=ps[:, j % 8, 0:4],
                                                  scalar1=b_sb[:, j:j + 1], scalar2=None, op0=ALU.add),
                 R=[pb, bb], W=[br])
        s.dma("sp", out, r_sb[:], R=[br])
        s.finish([br])
    return nc


def silu_np_layout_c(c, c_ctx):
    cc = np.stack([c[0], c[1], c_ctx, c_ctx], axis=1)
    return np.ascontiguousarray(cc.reshape(8, 128, 4).transpose(1, 0, 2))


def run_p0(c, c_ctx, w_ada, b_ada):
    nc = build_p0()
    cT = silu_np_layout_c(c, c_ctx)
    in_maps = []
    for i in range(NCORES):
        l, h = i // 2, i % 2
        in_maps.append({
            "cT": cT,
            "w": np.ascontiguousarray(w_ada[l][:, h * 3072:(h + 1) * 3072]),
            "bvec": np.ascontiguousarray(b_ada[l][h * 3072:(h + 1) * 3072].reshape(24, 128).T),
        })
    res = run_bass_kernel_spmd(nc, in_maps, core_ids=list(range(NCORES)))
    mods = np.zeros((DEPTH, 3, 6 * D), np.float32)
    for i in range(NCORES):
        l, h = i // 2, i % 2
        o = res.results[i]["out"]
        m = o.transpose(1, 0, 2).reshape(3072, 4)
        mods[l, :, h * 3072:(h + 1) * 3072] = m[:, :3].T
    return mods


NT1 = 2112
NW1 = 3072
BLKS1 = [(0, 512), (512, 512), (1024, 512), (1536, 512), (2048, 64)]


def build_p1():
    nc = bass.Bass("TRN2", target_bir_lowering=False)
    xT = nc.dram_tensor("xT", [128, 8, NT1], F32, kind="ExternalInput").ap()
    w = nc.dram_tensor("w", [D, NW1], F32, kind="ExternalInput").ap()
    gsc = nc.dram_tensor("gsc", [128, 5, 8], F32, kind="ExternalInput").ap()
    cosT = nc.dram_tensor("cosT", [128, 2048], F32, kind="ExternalInput").ap()
    sinT = nc.dram_tensor("sinT", [128, 2048], F32, kind="ExternalInput").ap()
    fmb = nc.dram_tensor("fmb", [128, 8, NT1], BF16, kind="ExternalOutput").ap()
    fmf = nc.dram_tensor("fmf", [128, 8, NT1], F32, kind="ExternalOutput").ap()
    tm = nc.dram_tensor("tm", [NT1, 512], BF16, kind="ExternalOutput").ap()
    s = Sched(nc)
    wv = w.rearrange("(kc p) n -> p kc n", p=128)
    with (nc.sbuf_tensor("x_sb", [128, 2, 8, 512], F32) as x_sb,
          nc.sbuf_tensor("h_sb", [128, 8, NT1], BF16) as h_sb,
          nc.sbuf_tensor("w_bf", [128, 8, NW1], BF16) as w_bf,
          nc.sbuf_tensor("w_st", [128, 2, NW1], F32) as w_st,
          nc.sbuf_tensor("o_sb", [128, 4, 512], F32) as o_sb,
          nc.sbuf_tensor("ob_sb", [128, 4, 512], BF16) as ob_sb,
          nc.sbuf_tensor("cos_sb", [128, 2048], F32) as cos_sb,
          nc.sbuf_tensor("sin_sb", [128, 2048], F32) as sin_sb,
          nc.sbuf_tensor("gsc_sb", [128, 5, 8], F32) as gsc_sb,
          nc.sbuf_tensor("ab_sb", [128, 2, 8], F32) as ab_sb,
          nc.sbuf_tensor("sq_sb", [128, 8, 512], BF16) as sq_sb,
          nc.sbuf_tensor("ones_sb", [128, 128], BF16) as ones_sb,
          nc.sbuf_tensor("rstd_sb", [128, 2, 512], F32) as rstd_sb,
          nc.sbuf_tensor("tmp_sb", [128, 4, 512], F32) as tmp_sb,
          nc.psum_tensor("ps", [128, 8, 512], F32) as ps):
        bx = [s.buf(f"x{i}", s.dsem(f"x{i}")) for i in range(2)]
        bh = [s.buf(f"h{i}") for i in range(len(BLKS1))]
        bwst = [s.buf(f"wst{i}", s.dsem(f"wst{i}")) for i in range(2)]
        bwbf = [s.buf(f"wbf{k}") for k in range(8)]
        bo = [s.buf(f"o{i}", s.dsem(f"o{i}")) for i in range(4)]
        bob = [s.buf(f"ob{i}", s.dsem(f"ob{i}")) for i in range(4)]
        cs = s.dsem("const")
        bcos = s.buf("cos", cs); bsin = s.buf("sin", cs); bgsc = s.buf("gsc", cs)
        bab = s.buf("ab"); bsq = s.buf("sq"); bones = s.buf("ones")
        brs = [s.buf(f"rs{i}") for i in range(2)]
        btmp = [s.buf(f"tmp{i}") for i in range(4)]
        bps = [s.buf(f"ps{i}") for i in range(8)]

        s.dma("sp", gsc_sb[:], gsc, W=[bgsc])
        s.dma("sp", cos_sb[:], cosT, W=[bcos])
        s.dma("sp", sin_sb[:], sinT, W=[bsin])
        s.op("pool", lambda e: e.memset(ones_sb[:], 1.0), W=[bones])
        for t in range(2):
            s.op("dve", lambda e: e.scalar_tensor_tensor(
                out=ab_sb[:, t, :], in0=gsc_sb[:, 1 + 2 * t, :], scalar=1.0, in1=gsc_sb[:, 0, :],
                op0=ALU.add, op1=ALU.mult), R=[bgsc], W=[bab])
        for k in range(8):
            sl = k % 2
            s.dma("act", w_st[:, sl, :], wv[:, k, :], W=[bwst[sl]])
            s.op("pool", lambda e: e.tensor_copy(out=w_bf[:, k, :], in_=w_st[:, sl, :]),
                 R=[bwst[sl]], W=[bwbf[k]])
        for bi, (t0, n) in enumerate(BLKS1):
            sl = bi % 2
            isctx = bi == 4
            s.dma("sp", x_sb[:, sl, :, 0:n], xT[:, :, t0:t0 + n], W=[bx[sl]])
            s.op("act", lambda e: e.activation(out=sq_sb[:, :, 0:n], in_=x_sb[:, sl, :, 0:n], func=AF.Square),
                 R=[bx[sl]], W=[bsq])
            pst = bps[sl]
            for k in range(8):
                mm(s, ps[:, sl, 0:n], ones_sb[:], sq_sb[:, k, 0:n], k == 0, k == 7, R=[bones, bsq], W=[pst])
            s.op("act", lambda e: e.activation(out=rstd_sb[:, sl, 0:n], in_=ps[:, sl, 0:n], func=AF.Sqrt,
                                               scale=1.0 / D, bias=EPS), R=[pst], W=[brs[sl]])
            s.op("dve", lambda e: e.reciprocal(out=rstd_sb[:, sl, 0:n], in_=rstd_sb[:, sl, 0:n]),
                 R=[brs[sl]], W=[brs[sl]])
            ai = 1 if isctx else 0
            shrow = 4 if isctx else 2
            for k in range(8):
                tb = k % 4
                s.op("dve", lambda e: e.scalar_tensor_tensor(
                    out=tmp_sb[:, tb, 0:n], in0=x_sb[:, sl, k, 0:n], scalar=ab_sb[:, ai, k:k + 1],
                    in1=rstd_sb[:, sl, 0:n], op0=ALU.mult, op1=ALU.mult),
                    R=[bx[sl], bab, brs[sl]], W=[btmp[tb]])
                s.op("act", lambda e: e.activation(out=h_sb[:, k, t0:t0 + n], in_=tmp_sb[:, tb, 0:n],
                                                   func=AF.Identity, bias=gsc_sb[:, shrow, k:k + 1], scale=1.0),
                     R=[btmp[tb], bgsc], W=[bh[bi]])
        oi = 0
        obi = 0
        pi = 0
        for bi, (t0, n) in enumerate(BLKS1):
            isctx = bi == 4
            for c in range(16):
                rope = (c >= 12) and not isctx
                isb = c < 4 or c >= 12
                dst = (fmb[:, c if c < 4 else c - 8, t0:t0 + n]) if isb else fmf[:, c - 4, t0:t0 + n]
                pA = 2 + (pi % 6); pi += 1
                for k in range(8):
                    mm(s, ps[:, pA, 0:n], w_bf[:, k, c * 128:(c + 1) * 128], h_sb[:, k, t0:t0 + n],
                       k == 0, k == 7, R=[bwbf[k], bh[bi]], W=[bps[pA]])
                if isb:
                    ob = obi % 4; obi += 1
                    osl = ob_sb[:, ob, 0:n]; obuf = bob[ob]
                else:
                    ob = oi % 4; oi += 1
                    osl = o_sb[:, ob, 0:n]; obuf = bo[ob]
                if not rope:
                    if c % 2 == 0:
                        s.op("act", lambda e: e.copy(out=osl, in_=ps[:, pA, 0:n]), R=[bps[pA]], W=[obuf])
                    else:
                        s.op("dve", lambda e: e.tensor_copy(out=osl, in_=ps[:, pA, 0:n]), R=[bps[pA]], W=[obuf])
                else:
                    pB = 2 + (pi % 6); pi += 1
                    c2 = c + 4
                    for k in range(8):
                        mm(s, ps[:, pB, 0:n], w_bf[:, k, c2 * 128:(c2 + 1) * 128], h_sb[:, k, t0:t0 + n],
                           k == 0, k == 7, R=[bwbf[k], bh[bi]], W=[bps[pB]])
                    s.op("dve", lambda e: e.tensor_tensor(out=tmp_sb[:, 0, 0:n], in0=ps[:, pA, 0:n],
                                                          in1=cos_sb[:, t0:t0 + n], op=ALU.mult),
                         R=[bps[pA], bcos], W=[btmp[0]])
                    s.op("dve", lambda e: e.tensor_tensor(out=tmp_sb[:, 1, 0:n], in0=ps[:, pB, 0:n],
                                                          in1=sin_sb[:, t0:t0 + n], op=ALU.mult),
                         R=[bps[pB], bsin], W=[btmp[1]])
                    s.op("pool", lambda e: e.tensor_tensor(out=osl, in0=tmp_sb[:, 0, 0:n],
                                                           in1=tmp_sb[:, 1, 0:n], op=ALU.add),
                         R=[btmp[0], btmp[1]], W=[obuf])
                s.dma("sp", dst, osl, R=[obuf])
        ntile = NT1 // 128 + 1
        for ti in range(ntile):
            t0 = ti * 128
            n = min(128, NT1 - t0)
            bi = min(t0 // 512, 4)
            pA = 2 + (pi % 6); pi += 1
            for k in range(8):
                mm(s, ps[0:n, pA, :], h_sb[:, k, t0:t0 + n], w_bf[:, k, 2560:3072],
                   k == 0, k == 7, R=[bwbf[k], bh[bi]], W=[bps[pA]])
            ob = obi % 4; obi += 1
            s.op("act", lambda e: e.copy(out=ob_sb[0:n, ob, :], in_=ps[0:n, pA, :]), R=[bps[pA]], W=[bob[ob]])
            s.dma("sp", tm[t0:t0 + n, :], ob_sb[0:n, ob, :], R=[bob[ob]])
        s.finish(bo + bob)
    return nc


def rope_tables():
    t = np.arange(S)
    row = (t // GRID_W).astype(np.float32)
    col = (t % GRID_W).astype(np.float32)
    inv = (10000.0 ** (-np.arange(0, 16, 2, dtype=np.float32) / 16.0)).astype(np.float32)
    ang_r = row[:, None] * inv
    ang_c = col[:, None] * inv
    cosT = np.zeros((32, S), np.float32)
    sinT = np.zeros((32, S), np.float32)
    for d in range(32):
        ang = ang_r if d < 16 else ang_c
        i = d % 8
        cosT[d] = np.cos(ang[:, i])
        sgn = -1.0 if (d % 16) < 8 else 1.0
        sinT[d] = sgn * np.sin(ang[:, i])
    return np.tile(cosT, (4, 1)), np.tile(sinT, (4, 1))


def p1_wcols():
    sw = np.array([(d + 8) if (d % 16) < 8 else (d - 8) for d in range(32)])
    f = np.arange(256)
    swf = (f // 32) * 32 + sw[f % 32]
    cols = np.concatenate([np.arange(0, 256), np.arange(256, 512), np.arange(768, 1280), np.arange(1280, 1792),
                           np.arange(1792, 2048), np.arange(2048, 2304), 1792 + swf, 2048 + swf,
                           np.arange(512, 768), np.arange(2304, 2560)])
    return cols


def chunkT(a):
    T = a.shape[0]
    return np.ascontiguousarray(a.T.reshape(8, 128, T).transpose(1, 0, 2))


def vec_pk(v):
    return np.ascontiguousarray(v.reshape(8, 128).T)


def run_p1(nc1, xl, xc, mods_l, g_mix_l, w_in_l, cosT, sinT):
    wl = np.ascontiguousarray(w_in_l[:, p1_wcols()])
    in_maps = []
    for i in range(NCORES):
        b, j = i // 4, i % 4
        xx = np.concatenate([xl[b, 2048 * j:2048 * (j + 1)], xc[b, 64 * j:64 * (j + 1)]], axis=0)
        gsc = np.stack([vec_pk(g_mix_l), vec_pk(mods_l[b, 1024:2048]), vec_pk(mods_l[b, 0:1024]),
                        vec_pk(mods_l[2, 1024:2048]), vec_pk(mods_l[2, 0:1024])], axis=1)
        in_maps.append({"xT": chunkT(xx), "w": wl, "gsc": np.ascontiguousarray(gsc),
                        "cosT": np.ascontiguousarray(cosT[:, 2048 * j:2048 * (j + 1)]),
                        "sinT": np.ascontiguousarray(sinT[:, 2048 * j:2048 * (j + 1)])})
    res = run_bass_kernel_spmd(nc1, in_maps, core_ids=list(range(NCORES)))
    fmb = np.zeros((B, 1024, S + L), ml_dtypes.bfloat16)
    fmf = np.zeros((B, 1024, S + L), np.float32)
    tm = np.zeros((B, S + L, 512), ml_dtypes.bfloat16)
    for i in range(NCORES):
        b, j = i // 4, i % 4
        r = res.results[i]
        for dst, key in ((fmb, "fmb"), (fmf, "fmf")):
            f = r[key].transpose(1, 0, 2).reshape(1024, NT1)
            dst[b, :, 2048 * j:2048 * (j + 1)] = f[:, :2048]
            dst[b, :, S + 64 * j:S + 64 * (j + 1)] = f[:, 2048:]
        t = r["tm"]
        tm[b, 2048 * j:2048 * (j + 1)] = t[:2048]
        tm[b, S + 64 * j:S + 64 * (j + 1)] = t[2048:]
    return fmb, fmf, tm


NTOK = S + L
NKT = NTOK // 128
NEB = 21


def na_tile_lists():
    out = []
    for m in range(64):
        if 2 <= m <= 61:
            out.append(([m - 2, m - 1, m, m + 1, m + 2], 0))
        elif m < 2:
            out.append(([0, 1, 2, 3], 5 + 4 * m))
        else:
            out.append(([60, 61, 62, 63], 5 + 4 * (m - 60)))
    return out


def na_bias_index():
    MASKED = 15 * 31
    idx = np.full((NEB, 128, 128), MASKED, np.int64)
    lists = na_tile_lists()
    reps = {0: 10}
    qq = np.arange(128); kk = np.arange(128)

    def fill(e0, m, kts):
        for ii, n in enumerate(kts):
            qr = 2 * m + qq // 64; qc = qq % 64
            kr = 2 * n + kk // 64; kc = kk % 64
            r0 = np.clip(qr - 4, 0, 120)
            cs = np.clip(qc - 8, 0, 48)
            valid = ((kr[:, None] >= r0[None, :]) & (kr[:, None] < r0[None, :] + 8) &
                     (kc[:, None] >= cs[None, :]) & (kc[:, None] < cs[None, :] + 16))
            dr = kr[:, None] - qr[None, :]
            dc = np.clip(kc[:, None] - qc[None, :], -15, 15)
            v = (np.clip(dr, -7, 7) + 7) * 31 + dc + 15
            idx[e0 + ii] = np.where(valid, v, MASKED)
    fill(0, 10, lists[10][0])
    for m in (0, 1, 62, 63):
        fill(lists[m][1], m, lists[m][0])
    return idx


def build_p2(lam_init):
    nc = bass.Bass("TRN2", target_bir_lowering=False)
    din = lambda n, shp, dt: nc.dram_tensor(n, shp, dt, kind="ExternalInput").ap()
    dout = lambda n, shp, dt: nc.dram_tensor(n, shp, dt, kind="ExternalOutput").ap()
    xrg = din("xrg", [2, 128, NTOK], F32)
    wbd = din("wbd", [4, 128, 128], F32)
    rgv = din("rgv", [128, 2, 8], F32)
    hout = dout("hout", [2, 128, NTOK], F32)
    qaT = din("qaT", [64, NTOK], BF16)
    kaT = din("kaT", [64, NTOK], BF16)
    vaP = din("vaP", [128, NKT, 64], BF16)
    btT = din("btT", [128, NEB, 128], F32)
    ynaP = dout("ynaP", [128, NKT, 64], BF16)
    qdT = din("qdT", [2, 32, NTOK], BF16)
    kdT = din("kdT", [2, 32, NTOK], BF16)
    vdP = din("vdP", [128, NKT, 64], BF16)
    dlam = din("dlam", [128, 128], F32)
    dg = din("dg", [128, 64], F32)
    ydfP = dout("ydfP", [128, NKT, 64], BF16)
    s = Sched(nc)
    with nc.psum_tensor("ps", [128, 8, 512], F32) as ps:
        bps = [s.buf(f"ps{i}") for i in range(8)]

        CH = 2048
        with (nc.sbuf_tensor("x_sb", [128, NTOK], F32) as x_sb,
              nc.sbuf_tensor("wst_sb", [128, 4, 128], F32) as wst_sb,
              nc.sbuf_tensor("wbf_sb", [128, 4, 128], BF16) as wbf_sb,
              nc.sbuf_tensor("rgv_sb", [128, 2, 8], F32) as rgv_sb,
              nc.sbuf_tensor("cneg_sb", [128, 2], F32) as cneg_sb,
              nc.sbuf_tensor("xcv_sb", [128, CH], F32) as xcv_sb,
              nc.sbuf_tensor("xcb_sb", [128, CH], BF16) as xcb_sb,
              nc.sbuf_tensor("r_sb", [128, CH], F32) as r_sb,
              nc.sbuf_tensor("i_sb", [128, CH], F32) as i_sb,
              nc.sbuf_tensor("a_sb", [128, CH], F32) as a_sb,
              nc.sbuf_tensor("q_sb", [128, CH], F32) as q_sb,
              nc.sbuf_tensor("h_sb", [128, 2, CH], F32) as h_sb,
              nc.sbuf_tensor("carry_sb", [128, 1], F32) as carry_sb):
            bx = s.buf("x", s.dsem("rgx"))
            bw = s.buf("w", s.dsem("rgw"))
            bwb = s.buf("wb")
            bv = s.buf("v", bw.dsem)
            bcn = s.buf("cneg")
            bxcv = s.buf("xcv"); bxcb = s.buf("xcb"); br = s.buf("r"); bi_ = s.buf("i")
            ba = s.buf("a"); bq = s.buf("q"); bcar = s.buf("carry")
            bh = [s.buf(f"h{i}", s.dsem(f"rgh{i}")) for i in range(2)]
            s.dma("act", wst_sb[:], wbd.rearrange("f c d -> c f d"), W=[bw])
            s.dma("act", rgv_sb[:], rgv, W=[bv])
            s.op("pool", lambda e: e.tensor_copy(out=wbf_sb[:], in_=wst_sb[:]), R=[bw], W=[bwb])
            s.op("act", lambda e: e.activation(out=cneg_sb[:], in_=rgv_sb[:, :, 7], func=AF.Exp, scale=-1.0),
                 R=[bv], W=[bcn])
            s.op("act", lambda e: e.activation(out=cneg_sb[:], in_=cneg_sb[:], func=AF.Ln, bias=1.0, scale=1.0),
                 R=[bcn], W=[bcn])
            s.op("dve", lambda e: e.tensor_scalar(out=cneg_sb[:], in0=cneg_sb[:], scalar1=-8.0, scalar2=None,
                                                  op0=ALU.mult), R=[bcn], W=[bcn])
            hi = 0
            for dr in range(2):
                offs = [-2, -1, 0, 1] if dr == 0 else [2, 1, 0, -1]
                s.dma("sp", x_sb[:, 0:4224], xrg[dr, :, 0:4224], W=[bx])
                s.dma("sp", x_sb[:, 4224:NTOK], xrg[dr, :, 4224:NTOK], W=[bx])
                chunks = [(0, 256, 0, 256)] + [(256 + CH * i, CH, 256, NTOK) for i in range(4)]
                for ci, (c0, n, s0, s1) in enumerate(chunks):
                    s.op("dve", lambda e: e.tensor_scalar(
                        out=xcv_sb[:, 0:n], in0=x_sb[:, c0:c0 + n], scalar1=rgv_sb[:, dr, 2:3],
                        scalar2=rgv_sb[:, dr, 4:5], op0=ALU.mult, op1=ALU.add), R=[bx, bv], W=[bxcv])
                    for jt in (0, 1, 3):
                        o = offs[jt]
                        lo = max(c0, s0 - o); hi_ = min(c0 + n, s1 - o)
                        s.op("dve", lambda e: e.scalar_tensor_tensor(
                            out=xcv_sb[:, lo - c0:hi_ - c0], in0=x_sb[:, lo + o:hi_ + o],
                            scalar=rgv_sb[:, dr, jt:jt + 1], in1=xcv_sb[:, lo - c0:hi_ - c0],
                            op0=ALU.mult, op1=ALU.add), R=[bx, bv, bxcv], W=[bxcv])
                    s.op("pool", lambda e: e.tensor_copy(out=xcb_sb[:, 0:n], in_=xcv_sb[:, 0:n]), R=[bxcv], W=[bxcb])
                    nsb = (n + 511) // 512
                    for sb in range(nsb):
                        w_ = min(512, n - sb * 512)
                        mm(s, ps[:, sb, 0:w_], wbf_sb[:, 2 * dr, :], xcb_sb[:, sb * 512:sb * 512 + w_], True, True,
                           R=[bwb, bxcb], W=[bps[sb]])
                        mm(s, ps[:, 4 + sb, 0:w_], wbf_sb[:, 2 * dr + 1, :], xcb_sb[:, sb * 512:sb * 512 + w_], True, True,
                           R=[bwb, bxcb], W=[bps[4 + sb]])
                    if n == CH:
                        rin = ps[:, 0:4, :]; iin = ps[:, 4:8, :]
                        rout = r_sb[:, 0:n].rearrange("p (a b) -> p a b", b=512)
                        iout = i_sb[:, 0:n].rearrange("p (a b) -> p a b", b=512)
                    else:
                        rin = ps[:, 0, 0:n]; iin = ps[:, 4, 0:n]
                        rout = r_sb[:, 0:n]; iout = i_sb[:, 0:n]
                    s.op("act", lambda e: e.activation(out=rout, in_=rin, func=AF.Sigmoid,
                                                       bias=rgv_sb[:, dr, 5:6], scale=1.0),
                         R=bps[0:4] + [bv], W=[br])
                    s.op("act", lambda e: e.activation(out=iout, in_=iin, func=AF.Sigmoid,
                                                       bias=rgv_sb[:, dr, 6:7], scale=1.0),
                         R=bps[4:8] + [bv], W=[bi_])
                    s.op("act", lambda e: e.activation(out=a_sb[:, 0:n], in_=r_sb[:, 0:n], func=AF.Exp,
                                                       scale=cneg_sb[:, dr:dr + 1]), R=[br, bcn], W=[ba])
                    s.op("pool", lambda e: e.tensor_tensor(out=q_sb[:, 0:n], in0=a_sb[:, 0:n], in1=a_sb[:, 0:n],
                                                           op=ALU.mult), R=[ba], W=[bq])
                    s.op("act", lambda e: e.activation(out=q_sb[:, 0:n], in_=q_sb[:, 0:n], func=AF.Sqrt,
                                                       scale=-1.0, bias=1.0), R=[bq], W=[bq])
                    s.op("pool", lambda e: e.tensor_tensor(out=i_sb[:, 0:n], in0=i_sb[:, 0:n], in1=xcv_sb[:, 0:n],
                                                           op=ALU.mult), R=[bi_, bxcv], W=[bi_])
                    s.op("pool", lambda e: e.tensor_tensor(out=q_sb[:, 0:n], in0=q_sb[:, 0:n], in1=i_sb[:, 0:n],
                                                           op=ALU.mult), R=[bq, bi_], W=[bq])
                    hs = hi % 2; hi += 1
                    init = 0.0 if ci == 0 else carry_sb[:, 0:1]
                    s.op("dve", lambda e: e.tensor_tensor_scan(out=h_sb[:, hs, 0:n], data0=a_sb[:, 0:n],
                                                               data1=q_sb[:, 0:n], initial=init,
                                                               op0=ALU.mult, op1=ALU.add),
                         R=[ba, bq] + ([bcar] if ci else []), W=[bh[hs]])
                    s.op("dve", lambda e: e.tensor_copy(out=carry_sb[:, 0:1], in_=h_sb[:, hs, n - 1:n]),
                         R=[bh[hs]], W=[bcar])
                    s.dma("sp", hout[dr, :, c0:c0 + n], h_sb[:, hs, 0:n], R=[bh[hs]])
            s.barrier(bh)

        with (nc.sbuf_tensor("na_q_sb", [64, NTOK], BF16) as q_sb,
              nc.sbuf_tensor("na_k_sb", [64, NTOK], BF16) as k_sb,
              nc.sbuf_tensor("na_v_sb", [128, NKT, 65], BF16) as v_sb,
              nc.sbuf_tensor("na_bt_sb", [128, NEB * 128], F32) as bt_sb,
              nc.sbuf_tensor("na_eb_sb", [128, NEB * 128], BF16) as eb_sb,
              nc.sbuf_tensor("na_e_sb", [128, 2, 640], F32) as e_sb,
              nc.sbuf_tensor("na_p_sb", [128, 2, 896], BF16) as p_sb,
              nc.sbuf_tensor("na_y_sb", [128, NKT, 64], BF16) as y_sb,
              nc.sbuf_tensor("na_rec_sb", [128, 2], F32) as rec_sb):
            ld = s.dsem("nald")
            bq = s.buf("q", ld); bk = s.buf("k", ld); bv = s.buf("v", ld); bbt = s.buf("bt", ld)
            beb = s.buf("eb")
            be = [s.buf(f"e{i}") for i in range(2)]
            bp = [s.buf(f"p{i}") for i in range(2)]
            brec = [s.buf(f"rec{i}") for i in range(2)]
            by = s.buf("y", s.dsem("nay"))
            s.dma("sp", q_sb[:], qaT, W=[bq])
            s.dma("act", k_sb[:], kaT, W=[bk])
            s.dma("sp", v_sb[:, :, 0:64], vaP, W=[bv])
            s.dma("act", bt_sb[:], btT.rearrange("p e q -> p (e q)"), W=[bbt])
            s.op("pool", lambda e: e.memset(v_sb[:, :, 64:65], 1.0), W=[bv])
            s.op("act", lambda e: e.activation(out=eb_sb[:], in_=bt_sb[:], func=AF.Exp), R=[bbt], W=[beb])
            lists = na_tile_lists()
            for m in range(NKT):
                sl = m % 2
                if m < 64:
                    kts, eb0 = lists[m]
                else:
                    kts, eb0 = [], 0
                nl = len(kts)
                bA = bps[2 * sl]; bB = bps[2 * sl + 1]; bAcc = bps[4 + sl]
                qs = q_sb[:, m * 128:(m + 1) * 128]
                for ii, n in enumerate(kts):
                    bank = 2 * sl + (0 if ii < 4 else 1)
                    col = (ii % 4) * 128
                    mm(s, ps[:, bank, col:col + 128], k_sb[:, n * 128:(n + 1) * 128], qs, True, True,
                       R=[bk, bq], W=[bps[bank]])
                for ci in range(2):
                    n = 64 + ci
                    mm(s, ps[:, 2 * sl + 1, 128 + ci * 128:256 + ci * 128], k_sb[:, n * 128:(n + 1) * 128], qs,
                       True, True, R=[bk, bq], W=[bB])
                if nl:
                    na_ = min(nl, 4) * 128
                    s.op("act", lambda e: e.activation(out=e_sb[:, sl, 0:na_], in_=ps[:, 2 * sl, 0:na_], func=AF.Exp,
                                                       scale=0.125), R=[bA], W=[be[sl]])
                    if nl == 5:
                        s.op("act", lambda e: e.activation(out=e_sb[:, sl, 512:640], in_=ps[:, 2 * sl + 1, 0:128],
                                                           func=AF.Exp, scale=0.125), R=[bB], W=[be[sl]])
                s.op("act", lambda e: e.activation(out=p_sb[:, sl, 640:896], in_=ps[:, 2 * sl + 1, 128:384],
                                                   func=AF.Exp, scale=0.125), R=[bB], W=[bp[sl]])
                if nl:
                    s.op("dve", lambda e: e.tensor_tensor(out=p_sb[:, sl, 0:nl * 128], in0=e_sb[:, sl, 0:nl * 128],
                                                          in1=eb_sb[:, eb0 * 128:(eb0 + nl) * 128], op=ALU.mult),
                         R=[be[sl], beb], W=[bp[sl]])
                tiles = [(ii * 128, n) for ii, n in enumerate(kts)] + [(640, 64), (768, 65)]
                for ti, (pc, n) in enumerate(tiles):
                    mm(s, ps[:, 4 + sl, 0:65], p_sb[:, sl, pc:pc + 128], v_sb[:, n, :], ti == 0, ti == len(tiles) - 1,
                       R=[bp[sl], bv], W=[bAcc])
                s.op("dve", lambda e: e.reciprocal(out=rec_sb[:, sl:sl + 1], in_=ps[:, 4 + sl, 64:65]),
                     R=[bAcc], W=[brec[sl]])
                s.op("dve", lambda e: e.tensor_scalar(out=y_sb[:, m, :], in0=ps[:, 4 + sl, 0:64],
                                                      scalar1=rec_sb[:, sl:sl + 1], scalar2=None, op0=ALU.mult),
                     R=[bAcc, brec[sl]], W=[by])
            s.dma("sp", ynaP, y_sb[:], R=[by])
            s.barrier([by])

        with (nc.sbuf_tensor("df_q4_sb", [96, NTOK], BF16) as q4_sb,
              nc.sbuf_tensor("df_k4_sb", [96, NTOK], BF16) as k4_sb,
              nc.sbuf_tensor("df_q4b_sb", [96, NTOK], BF16) as q4b_sb,
              nc.sbuf_tensor("df_k4b_sb", [96, NTOK], BF16) as k4b_sb,
              nc.sbuf_tensor("df_v_sb", [128, NKT, 65], BF16) as v_sb,
              nc.sbuf_tensor("df_p_sb", [128, 8, 512], BF16) as p_sb,
              nc.sbuf_tensor("df_y_sb", [128, NKT, 64], BF16) as y_sb,
              nc.sbuf_tensor("df_dl_sb", [128, 128], F32) as dl_sb,
              nc.sbuf_tensor("df_g_sb", [128, 64], F32) as g_sb,
              nc.sbuf_tensor("df_lam_sb", [128, 4], F32) as lam_sb,
              nc.sbuf_tensor("df_r_sb", [128, 2, 2, 4], F32) as r_sb,
              nc.sbuf_tensor("df_ss_sb", [128, 2, 4], F32) as ss_sb,
              nc.sbuf_tensor("df_o_sb", [128, 2, 4, 64], F32) as o_sb,
              nc.sbuf_tensor("df_t_sb", [128, 2, 4, 64], F32) as t_sb,
              nc.sbuf_tensor("df_junk_sb", [128, 64], F32) as junk_sb):
            ld = s.dsem("dfld")
            bq = s.buf("q", ld); bk = s.buf("k", ld); bv = s.buf("v", ld); bdl = s.buf("dl", ld); bg = s.buf("g", ld)
            blam = s.buf("lam")
            bp = [s.buf(f"p{i}") for i in range(8)]
            by = s.buf("y", s.dsem("dfy"))
            br = [s.buf(f"r{i}") for i in range(2)]
            bss = [s.buf(f"ss{i}") for i in range(2)]
            bo = [s.buf(f"o{i}") for i in range(2)]
            bt = [s.buf(f"t{i}") for i in range(2)]
            bj = s.buf("junk")
            for (qt, kt_, order) in ((q4_sb, k4_sb, (0, 1, 0)), (q4b_sb, k4b_sb, (1, 0, 1))):
                for ri, cc in enumerate(order):
                    s.dma("sp", qt[32 * ri:32 * ri + 32, :], qdT[cc], W=[bq])
                    s.dma("act", kt_[32 * ri:32 * ri + 32, :], kdT[cc], W=[bk])
            s.dma("sp", v_sb[:, :, 0:64], vdP, W=[bv])
            s.dma("act", dl_sb[:], dlam, W=[bdl])
            s.dma("act", g_sb[:], dg, W=[bg])
            s.op("pool", lambda e: e.memset(v_sb[:, :, 64:65], 1.0), W=[bv])
            s.op("dve", lambda e: e.scalar_tensor_tensor(out=junk_sb[:, 0:32], in0=dl_sb[:, 0:32], scalar=1.0,
                                                         in1=dl_sb[:, 32:64], op0=ALU.mult, op1=ALU.mult,
                                                         accum_out=lam_sb[:, 0:1]), R=[bdl], W=[bj, blam])
            s.op("dve", lambda e: e.scalar_tensor_tensor(out=junk_sb[:, 0:32], in0=dl_sb[:, 64:96], scalar=1.0,
                                                         in1=dl_sb[:, 96:128], op0=ALU.mult, op1=ALU.mult,
                                                         accum_out=lam_sb[:, 1:2]), R=[bdl, bj], W=[bj, blam])
            s.op("act", lambda e: e.activation(out=lam_sb[:, 0:2], in_=lam_sb[:, 0:2], func=AF.Exp), R=[blam], W=[blam])
            s.op("dve", lambda e: e.tensor_tensor(out=lam_sb[:, 2:3], in0=lam_sb[:, 1:2], in1=lam_sb[:, 0:1],
                                                  op=ALU.subtract), R=[blam], W=[blam])
            s.op("dve", lambda e: e.tensor_scalar(out=lam_sb[:, 2:3], in0=lam_sb[:, 2:3], scalar1=-lam_init,
                                                  scalar2=None, op0=ALU.add), R=[blam], W=[blam])
            s.op("dve", lambda e: e.tensor_scalar(out=g_sb[:], in0=g_sb[:], scalar1=1.0 - lam_init, scalar2=None,
                                                  op0=ALU.mult), R=[bg], W=[bg])
            qblocks = [(512 * i, 512, list(range(NKT))) for i in range(16)] + [(S, 256, [64, 65])]
            sc = 32 ** -0.5
            gs_ = 0
            for qi, (q0, nq, kts) in enumerate(qblocks):
                par = qi % 2
                nsub = nq // 128
                steps = [(kt, c) for kt in kts for c in range(2)]
                ns = len(steps)
                groups = [list(range(i, min(i + 3, ns))) for i in range(0, ns, 3)]
                started = [False, False]
                for gi_ in range(len(groups) + 1):
                    if gi_ < len(groups):
                        grp = groups[gi_]
                        layB = steps[grp[0]][1] == 1
                        qt = q4b_sb if layB else q4_sb
                        kt_t = k4b_sb if layB else k4_sb
                        for ri, si in enumerate(grp):
                            kt, c = steps[si]
                            g = gs_ + si
                            mm(s, ps[:, g % 4, 0:nq], kt_t[32 * ri:32 * ri + 32, kt * 128:(kt + 1) * 128],
                               qt[32 * ri:32 * ri + 32, q0:q0 + nq], True, True, R=[bk, bq], W=[bps[g % 4]])
                        for ri, si in enumerate(grp):
                            g = gs_ + si
                            s.op("act", lambda e: e.activation(out=p_sb[:, g % 8, 0:nq], in_=ps[:, g % 4, 0:nq],
                                                               func=AF.Exp, scale=sc), R=[bps[g % 4]], W=[bp[g % 8]])
                    if gi_ >= 1:
                        for si in groups[gi_ - 1]:
                            kt, c = steps[si]
                            g = gs_ + si
                            bank = 4 + 2 * par + c
                            for sub in range(nsub):
                                st = not started[c]
                                started[c] = True
                                s.op("pe", lambda e: e.matmul(ps[:, bank, sub * 65:(sub + 1) * 65],
                                                              p_sb[:, g % 8, sub * 128:(sub + 1) * 128], v_sb[:, kt, :],
                                                              start=st, stop=(kt == kts[-1]), skip_group_check=True),
                                     R=[bp[g % 8], bv], W=[bps[bank]])
                gs_ += ns
                a0 = ps[:, 4 + 2 * par, 0:260].rearrange("p (s e) -> p s e", e=65)
                a1 = ps[:, 5 + 2 * par, 0:260].rearrange("p (s e) -> p s e", e=65)
                b0 = bps[4 + 2 * par]; b1 = bps[5 + 2 * par]
                s.op("dve", lambda e: e.reciprocal(out=r_sb[:, par, 0, 0:nsub], in_=a0[:, 0:nsub, 64]), R=[b0], W=[br[par]])
                s.op("dve", lambda e: e.reciprocal(out=r_sb[:, par, 1, 0:nsub], in_=a1[:, 0:nsub, 64]), R=[b1], W=[br[par]])
                s.op("dve", lambda e: e.tensor_scalar(out=r_sb[:, par, 1, 0:nsub], in0=r_sb[:, par, 1, 0:nsub],
                                                      scalar1=lam_sb[:, 2:3], scalar2=None, op0=ALU.mult),
                     R=[br[par], blam], W=[br[par]])
                for sub in range(nsub):
                    s.op("dve", lambda e: e.tensor_scalar(out=t_sb[:, par, sub, :], in0=a1[:, sub, 0:64],
                                                          scalar1=r_sb[:, par, 1, sub:sub + 1], scalar2=None,
                                                          op0=ALU.mult), R=[b1, br[par]], W=[bt[par]])
                    s.op("dve", lambda e: e.scalar_tensor_tensor(out=o_sb[:, par, sub, :], in0=a0[:, sub, 0:64],
                                                                 scalar=r_sb[:, par, 0, sub:sub + 1],
                                                                 in1=t_sb[:, par, sub, :], op0=ALU.mult, op1=ALU.add),
                         R=[b0, br[par], bt[par]], W=[bo[par]])
                    s.op("dve", lambda e: e.scalar_tensor_tensor(out=junk_sb[:], in0=o_sb[:, par, sub, :], scalar=1.0,
                                                                 in1=o_sb[:, par, sub, :], op0=ALU.mult, op1=ALU.mult,
                                                                 accum_out=ss_sb[:, par, sub:sub + 1]),
                         R=[bo[par], bj], W=[bj, bss[par]])
                s.op("act", lambda e: e.activation(out=ss_sb[:, par, 0:nsub], in_=ss_sb[:, par, 0:nsub], func=AF.Ln,
                                                   scale=1.0 / 64, bias=EPS), R=[bss[par]], W=[bss[par]])
                s.op("act", lambda e: e.activation(out=ss_sb[:, par, 0:nsub], in_=ss_sb[:, par, 0:nsub], func=AF.Exp,
                                                   scale=-0.5), R=[bss[par]], W=[bss[par]])
                for sub in range(nsub):
                    s.op("dve", lambda e: e.scalar_tensor_tensor(out=y_sb[:, q0 // 128 + sub, :], in0=o_sb[:, par, sub, :],
                                                                 scalar=ss_sb[:, par, sub:sub + 1], in1=g_sb[:],
                                                                 op0=ALU.mult, op1=ALU.mult),
                         R=[bo[par], bss[par], bg], W=[by])
            s.dma("sp", ydfP, y_sb[:], R=[by])
            s.finish([by])
    return nc


def tileP(a):
    return np.ascontiguousarray(a.reshape(NKT, 128, a.shape[1]).transpose(1, 0, 2))


def untileP(a):
    return a.transpose(1, 0, 2).reshape(NTOK, a.shape[2])


_NA_IDX = None


def run_p2(nc2, l, fmb, fmf, tm, inp):
    global _NA_IDX
    if _NA_IDX is None:
        _NA_IDX = na_bias_index()
    in_maps = []
    for i in range(NCORES):
        b, j = i // 4, i % 4
        xr = fmf[b, 128 * j:128 * (j + 1)]
        xf = np.concatenate([xr[:, S:], xr[:, :S]], axis=1)
        xb = np.concatenate([xr[:, S:][:, ::-1], xr[:, :S][:, ::-1]], axis=1)
        wbd = np.zeros((4, 128, 128), np.float32)
        rgv = np.zeros((128, 2, 8), np.float32)
        ch = slice(128 * j, 128 * (j + 1))
        for dr in range(2):
            for gi, wk in enumerate(("rg_w_r", "rg_w_i")):
                for bb in range(2):
                    wbd[dr * 2 + gi, 64 * bb:64 * (bb + 1), 64 * bb:64 * (bb + 1)] = inp[wk][l, dr, 2 * j + bb]
            rgv[:, dr, 0:4] = inp["rg_conv_w"][l][:, ch].T
            rgv[:, dr, 4] = inp["rg_conv_b"][l][ch]
            rgv[:, dr, 5] = inp["rg_b_r"][l, dr, ch]
            rgv[:, dr, 6] = inp["rg_b_i"][l, dr, ch]
            rgv[:, dr, 7] = inp["rg_lambda"][l, dr, ch]
        rext = np.concatenate([inp["na_rpb"][l, j].ravel(), np.array([-30000.0], np.float32)])
        bt = rext[_NA_IDX]
        in_maps.append({
            "xrg": np.ascontiguousarray(np.stack([xf, xb])), "wbd": wbd, "rgv": rgv,
            "qaT": np.ascontiguousarray(fmb[b, 64 * j:64 * (j + 1)]),
            "kaT": np.ascontiguousarray(fmb[b, 256 + 64 * j:256 + 64 * (j + 1)]),
            "vaP": tileP(tm[b][:, 64 * j:64 * (j + 1)]),
            "btT": np.ascontiguousarray(bt.transpose(1, 0, 2)),
            "qdT": np.ascontiguousarray(fmb[b, 512 + 64 * j:512 + 64 * (j + 1)].reshape(2, 32, NTOK)),
            "kdT": np.ascontiguousarray(fmb[b, 768 + 64 * j:768 + 64 * (j + 1)].reshape(2, 32, NTOK)),
            "vdP": tileP(tm[b][:, 256 + 64 * j:256 + 64 * (j + 1)]),
            "dlam": np.ascontiguousarray(np.tile(inp["diff_lambda"][l].reshape(1, 128), (128, 1))),
            "dg": np.ascontiguousarray(np.tile(inp["diff_subln_g"][l].reshape(1, 64), (128, 1))),
        })
    res = run_bass_kernel_spmd(nc2, in_maps, core_ids=list(range(NCORES)))
    yna = np.zeros((B, NTOK, 256), ml_dtypes.bfloat16)
    ydf = np.zeros((B, NTOK, 256), ml_dtypes.bfloat16)
    hf = np.zeros((B, 512, NTOK), np.float32)
    hb = np.zeros((B, 512, NTOK), np.float32)
    for i in range(NCORES):
        b, j = i // 4, i % 4
        r = res.results[i]
        yna[b, :, 64 * j:64 * (j + 1)] = untileP(r["ynaP"])
        ydf[b, :, 64 * j:64 * (j + 1)] = untileP(r["ydfP"])
        h = r["hout"]
        hf[b, 128 * j:128 * (j + 1), S:] = h[0][:, :L]
        hf[b, 128 * j:128 * (j + 1), :S] = h[0][:, L:]
        hb[b, 128 * j:128 * (j + 1), S:] = h[1][:, :L][:, ::-1]
        hb[b, 128 * j:128 * (j + 1), :S] = h[1][:, L:][:, ::-1]
    return yna, ydf, hf, hb


NEXP = 32
GELU_C = 1.5957691216057308


def build_p3(final):
    nc = bass.Bass("TRN2", target_bir_lowering=False)
    din = lambda n, shp, dt: nc.dram_tensor(n, shp, dt, kind="ExternalInput").ap()
    xT = din("xT", [128, 8, NT1], F32)
    nadf = din("nadf", [128, 4, NT1], BF16)
    hg = din("hg", [128, 12, NT1], F32)
    wout = din("wout", [D, D], F32)
    mod = din("mod", [128, 10, 8], F32)
    wge = din("wge", [128, 8, 36], F32)
    bge = din("bge", [128, 36], F32)
    selc = din("selc", [32, NEXP * 128], BF16)
    ident = din("ident", [128, 128], F32)
    w1 = din("w1", [NEXP, D, 512], F32)
    w3 = din("w3", [NEXP, D, 512], F32)
    w2 = din("w2", [NEXP, 512, D], F32)
    xo = nc.dram_tensor("xo", [128, 8, NT1], F32, kind="ExternalOutput").ap()
    s = Sched(nc)
    wov = wout.rearrange("(kc p) n -> p kc n", p=128)
    with (nc.psum_tensor("ps", [128, 8, 512], F32) as ps,
          nc.sbuf_tensor("x_sb", [128, 8, NT1], F32) as x_sb,
          nc.sbuf_tensor("mod_sb", [128, 10, 8], F32) as mod_sb,
          nc.sbuf_tensor("hl2_sb", [128, 8, NT1], BF16) as hl2_sb,
          nc.sbuf_tensor("wdt_sb", [32, 2, NT1], BF16) as wdt_sb,
          nc.sbuf_tensor("ones_sb", [128, 128], BF16) as ones_sb):
        bps = [s.buf(f"ps{i}") for i in range(8)]
        cs = s.dsem("const")
        bxb = [s.buf(f"x{i}", s.dsem(f"x{i}")) for i in range(len(BLKS1))]
        bmod = s.buf("mod", cs)
        bhl2 = [s.buf(f"hl2_{i}") for i in range(len(BLKS1))]
        bwdt = [s.buf(f"wdt{i}") for i in range(len(BLKS1))]
        bones = s.buf("ones")
        s.dma("sp", mod_sb[:], mod, W=[bmod])
        for bi, (t0, n) in enumerate(BLKS1):
            s.dma("sp", x_sb[:, :, t0:t0 + n], xT[:, :, t0:t0 + n], W=[bxb[bi]])
        s.op("pool", lambda e: e.memset(ones_sb[:], 1.0), W=[bones])

        with (nc.sbuf_tensor("s1_mix", [128, 8, NT1], BF16) as mix_sb,
              nc.sbuf_tensor("s1_wobf", [128, 8, D], BF16) as wo_bf,
              nc.sbuf_tensor("s1_wost", [128, 2, D], F32) as wo_st,
              nc.sbuf_tensor("s1_hg", [128, 1, 12, 512], F32) as hg_sb,
              nc.sbuf_tensor("s1_t", [128, 4, 512], F32) as t_sb):
            bmixl = s.buf("mixl", s.dsem("mixl"))
            bmix = [s.buf(f"mix{i}") for i in range(len(BLKS1))]
            bwost = [s.buf(f"wost{i}", s.dsem(f"wost{i}")) for i in range(2)]
            bwobf = [s.buf(f"wobf{k}") for k in range(8)]
            bhg = [s.buf(f"hg{i}", s.dsem(f"hg{i}")) for i in range(2)]
            bt = [s.buf(f"t{i}") for i in range(4)]
            s.dma("act", mix_sb[:, 0:2, :], nadf[:, 0:2, :], W=[bmixl])
            s.dma("act", mix_sb[:, 6:8, :], nadf[:, 2:4, :], W=[bmixl])
            for k in range(8):
                sl = k % 2
                s.dma("act", wo_st[:, sl, :], wov[:, k, :], W=[bwost[sl]])
                s.op("pool", lambda e: e.tensor_copy(out=wo_bf[:, k, :], in_=wo_st[:, sl, :]), R=[bwost[sl]], W=[bwobf[k]])
            for bi, (t0, n) in enumerate(BLKS1):
                sl = 0
                s.dma("sp", hg_sb[:, sl, :, 0:n], hg[:, :, t0:t0 + n], W=[bhg[sl]])
                for ch in range(4):
                    hf_ = hg_sb[:, sl, ch, 0:n]; hb_ = hg_sb[:, sl, 4 + ch, 0:n]; gr_ = hg_sb[:, sl, 8 + ch, 0:n]
                    s.op("dve", lambda e: e.tensor_tensor(out=t_sb[:, 0, 0:n], in0=hf_, in1=hb_, op=ALU.add),
                         R=[bhg[sl]], W=[bt[0]])
                    s.op("dve", lambda e: e.tensor_tensor(out=t_sb[:, 1, 0:n], in0=gr_, in1=gr_, op=ALU.mult),
                         R=[bhg[sl]], W=[bt[1]])
                    s.op("dve", lambda e: e.tensor_scalar(out=t_sb[:, 1, 0:n], in0=t_sb[:, 1, 0:n], scalar1=0.044715,
                                                          scalar2=1.0, op0=ALU.mult, op1=ALU.add), R=[bt[1]], W=[bt[1]])
                    s.op("pool", lambda e: e.tensor_tensor(out=t_sb[:, 2, 0:n], in0=t_sb[:, 1, 0:n], in1=gr_, op=ALU.mult),
                         R=[bt[1], bhg[sl]], W=[bt[2]])
                    s.op("act", lambda e: e.activation(out=t_sb[:, 2, 0:n], in_=t_sb[:, 2, 0:n], func=AF.Sigmoid,
                                                       scale=GELU_C), R=[bt[2]], W=[bt[2]])
                    s.op("pool", lambda e: e.tensor_tensor(out=t_sb[:, 3, 0:n], in0=t_sb[:, 2, 0:n], in1=gr_, op=ALU.mult),
                         R=[bt[2], bhg[sl]], W=[bt[3]])
                    s.op("pool", lambda e: e.tensor_tensor(out=mix_sb[:, 2 + ch, t0:t0 + n], in0=t_sb[:, 3, 0:n],
                                                           in1=t_sb[:, 0, 0:n], op=ALU.mult),
                         R=[bt[3], bt[0]], W=[bmix[bi]])
            pi = 0
            for bi, (t0, n) in enumerate(BLKS1):
                garow = 5 if bi == 4 else 1
                for dc in range(8):
                    pb = pi % 8; pi += 1
                    for k in range(8):
                        mm(s, ps[:, pb, 0:n], wo_bf[:, k, dc * 128:(dc + 1) * 128], mix_sb[:, k, t0:t0 + n],
                           k == 0, k == 7, R=[bwobf[k], bmix[bi], bmixl], W=[bps[pb]])
                    s.op("dve", lambda e: e.scalar_tensor_tensor(out=x_sb[:, dc, t0:t0 + n], in0=ps[:, pb, 0:n],
                                                                 scalar=mod_sb[:, garow, dc:dc + 1],
                                                                 in1=x_sb[:, dc, t0:t0 + n], op0=ALU.mult, op1=ALU.add),
                         R=[bps[pb], bmod, bxb[bi]], W=[bxb[bi]])
            s.barrier()

        with (nc.sbuf_tensor("s2_sq", [128, 8, 512], BF16) as sq_sb,
              nc.sbuf_tensor("s2_rstd", [128, 2, 512], F32) as rstd_sb,
              nc.sbuf_tensor("s2_tmp", [128, 4, 512], F32) as tmp_sb,
              nc.sbuf_tensor("s2_hf", [128, 2, 8, 512], F32) as hf_sb,
              nc.sbuf_tensor("s2_ab", [128, 2, 8], F32) as ab_sb,
              nc.sbuf_tensor("s2_wge", [128, 8, 36], F32) as wge_sb,
              nc.sbuf_tensor("s2_bge", [128, 36], F32) as bge_sb,
              nc.sbuf_tensor("s2_id", [128, 128], F32) as id_sb,
              nc.sbuf_tensor("s2_rt", [128, 2, 128], F32) as rt_sb):
            bsq = s.buf("sq"); brs = [s.buf(f"rs{i}") for i in range(2)]
            btmp = [s.buf(f"tmp{i}") for i in range(4)]
            bhf = [s.buf(f"hf{i}") for i in range(2)]
            bab = s.buf("ab")
            bwge = s.buf("wge", cs); bbge = s.buf("bge", cs); bid = s.buf("id", cs)
            brt = [s.buf(f"rt{i}") for i in range(2)]
            s.dma("act", wge_sb[:], wge, W=[bwge])
            s.dma("act", bge_sb[:], bge, W=[bbge])
            s.dma("act", id_sb[:], ident, W=[bid])
            for t in range(2):
                s.op("dve", lambda e: e.scalar_tensor_tensor(
                    out=ab_sb[:, t, :], in0=mod_sb[:, 2 + 4 * t, :], scalar=1.0, in1=mod_sb[:, 0, :],
                    op0=ALU.add, op1=ALU.mult), R=[bmod], W=[bab])
            ti_g = 0
            for bi, (t0, n) in enumerate(BLKS1):
                sl = bi % 2
                isctx = bi == 4
                s.op("act", lambda e: e.activation(out=sq_sb[:, :, 0:n], in_=x_sb[:, :, t0:t0 + n], func=AF.Square),
                     R=[bxb[bi]], W=[bsq])
                for k in range(8):
                    mm(s, ps[:, sl, 0:n], ones_sb[:], sq_sb[:, k, 0:n], k == 0, k == 7, R=[bones, bsq], W=[bps[sl]])
                s.op("act", lambda e: e.activation(out=rstd_sb[:, sl, 0:n], in_=ps[:, sl, 0:n], func=AF.Sqrt,
                                                   scale=1.0 / D, bias=EPS), R=[bps[sl]], W=[brs[sl]])
                s.op("dve", lambda e: e.reciprocal(out=rstd_sb[:, sl, 0:n], in_=rstd_sb[:, sl, 0:n]),
                     R=[brs[sl]], W=[brs[sl]])
                ai = 1 if isctx else 0
                shrow = 7 if isctx else 3
                for k in range(8):
                    tb = k % 4
                    s.op("dve", lambda e: e.scalar_tensor_tensor(
                        out=tmp_sb[:, tb, 0:n], in0=x_sb[:, k, t0:t0 + n], scalar=ab_sb[:, ai, k:k + 1],
                        in1=rstd_sb[:, sl, 0:n], op0=ALU.mult, op1=ALU.mult),
                        R=[bxb[bi], bab, brs[sl]], W=[btmp[tb]])
                    s.op("act", lambda e: e.activation(out=hf_sb[:, sl, k, 0:n], in_=tmp_sb[:, tb, 0:n],
                                                       func=AF.Identity, bias=mod_sb[:, shrow, k:k + 1], scale=1.0),
                         R=[btmp[tb], bmod], W=[bhf[sl]])
                s.op("pool", lambda e: e.tensor_copy(out=hl2_sb[:, :, t0:t0 + n], in_=hf_sb[:, sl, :, 0:n]),
                     R=[bhf[sl]], W=[bhl2[bi]])
                for tt in range((n + 127) // 128):
                    c0 = tt * 128
                    m = min(128, n - c0)
                    rs_ = ti_g % 2; ti_g += 1
                    pb = 2 + rs_
                    rt = rt_sb[0:m, rs_, :]
                    brr = brt[rs_]
                    for k in range(8):
                        mm(s, ps[0:m, pb, 0:36], hf_sb[:, sl, k, c0:c0 + m], wge_sb[:, k, :], k == 0, k == 7,
                           R=[bhf[sl], bwge], W=[bps[pb]])
                    V = lambda eng, fn, R_=(), W_=(): s.op(eng, fn, R=[brr] + list(R_), W=[brr] + list(W_))
                    lg = rt[:, 0:36]
                    s.op("dve", lambda e: e.tensor_tensor(out=lg, in0=ps[0:m, pb, 0:36], in1=bge_sb[0:m, :], op=ALU.add),
                         R=[bps[pb], bbge], W=[brr])
                    gmax = rt[:, 36:37]; ngmax = rt[:, 37:38]; sume = rt[:, 38:39]; gtop = rt[:, 39:40]
                    eg = rt[:, 40:44]; ohg = rt[:, 44:48]; sel = rt[:, 48:56]; top8 = rt[:, 56:64]
                    dd = rt[:, 64:65]; ed = rt[:, 65:66]; w1_ = rt[:, 66:67]; wt1 = rt[:, 67:68]; wt2 = rt[:, 68:69]
                    ea = rt[:, 72:80]; eb_ = rt[:, 80:88]; wd = rt[:, 96:128]
                    V("dve", lambda e: e.reduce_max(out=gmax, in_=lg[:, 0:4], axis=AX.X))
                    V("dve", lambda e: e.tensor_scalar(out=ngmax, in0=gmax, scalar1=-1.0, scalar2=None, op0=ALU.mult))
                    V("act", lambda e: e.activation(out=eg, in_=lg[:, 0:4], func=AF.Exp, bias=ngmax, scale=1.0,
                                                    accum_out=sume))
                    V("dve", lambda e: e.reciprocal(out=gtop, in_=sume))
                    V("dve", lambda e: e.tensor_scalar(out=ohg, in0=lg[:, 0:4], scalar1=gmax, scalar2=None,
                                                       op0=ALU.is_equal))
                    V("dve", lambda e: e.tensor_scalar(out=sel, in0=lg[:, 4:12], scalar1=ohg[:, 0:1], scalar2=None,
                                                       op0=ALU.mult))
                    for g in range(1, 4):
                        V("dve", lambda e: e.scalar_tensor_tensor(out=sel, in0=lg[:, 4 + 8 * g:12 + 8 * g],
                                                                  scalar=ohg[:, g:g + 1], in1=sel,
                                                                  op0=ALU.mult, op1=ALU.add))
                    V("dve", lambda e: e.max(out=top8, in_=sel))
                    V("dve", lambda e: e.tensor_tensor(out=dd, in0=top8[:, 1:2], in1=top8[:, 0:1], op=ALU.subtract))
                    V("act", lambda e: e.activation(out=ed, in_=dd, func=AF.Exp))
                    V("dve", lambda e: e.tensor_scalar(out=w1_, in0=ed, scalar1=1.0, scalar2=None, op0=ALU.add))
                    V("dve", lambda e: e.reciprocal(out=w1_, in_=w1_))
                    V("dve", lambda e: e.tensor_tensor(out=wt1, in0=w1_, in1=gtop, op=ALU.mult))
                    V("dve", lambda e: e.tensor_tensor(out=wt2, in0=wt1, in1=ed, op=ALU.mult))
                    V("dve", lambda e: e.tensor_scalar(out=ea, in0=sel, scalar1=top8[:, 0:1], scalar2=wt1,
                                                       op0=ALU.is_equal, op1=ALU.mult))
                    V("dve", lambda e: e.tensor_scalar(out=eb_, in0=sel, scalar1=top8[:, 1:2], scalar2=wt2,
                                                       op0=ALU.is_equal, op1=ALU.mult))
                    V("dve", lambda e: e.tensor_tensor(out=ea, in0=ea, in1=eb_, op=ALU.add))
                    for g in range(4):
                        V("dve", lambda e: e.tensor_scalar(out=wd[:, 8 * g:8 * g + 8], in0=ea, scalar1=ohg[:, g:g + 1],
                                                           scalar2=None, op0=ALU.mult))
                    pt = 4 + rs_
                    s.op("pe", lambda e: e.transpose(ps[0:32, pt, 0:m], wd, id_sb[0:m, 0:m]), R=[brr, bid], W=[bps[pt]])
                    s.op("act", lambda e: e.copy(out=wdt_sb[:, 0, t0 + c0:t0 + c0 + m], in_=ps[0:32, pt, 0:m]),
                         R=[bps[pt]], W=[bwdt[bi]])
                    s.op("dve", lambda e: e.tensor_tensor(out=wdt_sb[:, 1, t0 + c0:t0 + c0 + m], in0=ps[0:32, pt, 0:m],
                                                          in1=wdt_sb[:, 0, t0 + c0:t0 + c0 + m], op=ALU.subtract),
                         R=[bps[pt], bwdt[bi]], W=[bwdt[bi]])
            s.barrier()

        with (nc.sbuf_tensor("s3_st", [128, 3, 2048], F32) as st_sb,
              nc.sbuf_tensor("s3_wb", [128, 2, 6, 2048], BF16) as wb_sb,
              nc.sbuf_tensor("s3_sel", [32, NEXP * 128], BF16) as sel_sb,
              nc.sbuf_tensor("s3_wbc", [128, 2, 512], F32) as wbc_sb,
              nc.sbuf_tensor("s3_sg", [128, 2, 512], F32) as sg_sb,
              nc.sbuf_tensor("s3_t", [128, 2, 512], F32) as t3_sb,
              nc.sbuf_tensor("s3_g", [128, 2, 4, 512], BF16) as g_sb):
            bst = [s.buf(f"st{i}", s.dsem(f"st{i}")) for i in range(3)]
            bwb = [[s.buf(f"wb{a}_{p}") for p in range(6)] for a in range(2)]
            bsel = s.buf("sel", cs)
            bwbc = [s.buf(f"wbc{i}") for i in range(2)]
            bsg = [s.buf(f"sg{i}") for i in range(2)]
            bt3 = [s.buf(f"t3{i}") for i in range(2)]
            bg = [s.buf(f"g{i}") for i in range(2)]
            s.dma("act", sel_sb[:], selc, W=[bsel])
            w1v = w1.rearrange("e (kc p) f -> e p kc f", p=128)
            w3v = w3.rearrange("e (kc p) f -> e p kc f", p=128)
            w2v = w2.rearrange("e (fc p) d -> e p fc d", p=128)

            def piece_src(e, p):
                if p < 2:
                    return w1v[e, :, 4 * p:4 * p + 4, :]
                if p < 4:
                    return w3v[e, :, 4 * (p - 2):4 * (p - 2) + 4, :]
                return w2v[e, :, 2 * (p - 4):2 * (p - 4) + 2, :]

            def piece_dma(P):
                e, p = divmod(P, 6)
                if e >= NEXP:
                    return
                sl = P % 3
                dst = st_sb[:, sl, :]
                dst = dst.rearrange("q (a b) -> q a b", a=4) if p < 4 else dst.rearrange("q (a b) -> q a b", a=2)
                s.dma("sp", dst, piece_src(e, p), W=[bst[sl]])

            def piece_cast(P):
                e, p = divmod(P, 6)
                if e >= NEXP:
                    return
                sl = P % 3
                s.op("pool", lambda en: en.tensor_copy(out=wb_sb[:, e % 2, p, :], in_=st_sb[:, sl, :]),
                     R=[bst[sl]], W=[bwb[e % 2][p]])

            for P in range(3):
                piece_dma(P)
            for P in range(6):
                piece_cast(P)
                piece_dma(P + 3)
            gi = 0
            for ex in range(NEXP):
                a = ex % 2
                for bi, (t0, n) in enumerate(BLKS1):
                    if bi < 3:
                        for P in (6 * (ex + 1) + 2 * bi, 6 * (ex + 1) + 2 * bi + 1):
                            piece_cast(P)
                            piece_dma(P + 3)
                    garow = 8 if bi == 4 else 4
                    wr = gi % 2
                    gs = gi % 2
                    gi += 1
                    mm(s, ps[:, 6, 0:n], sel_sb[:, ex * 128:(ex + 1) * 128], wdt_sb[:, 0, t0:t0 + n], True, False,
                       R=[bsel, bwdt[bi]], W=[bps[6]])
                    mm(s, ps[:, 6, 0:n], sel_sb[:, ex * 128:(ex + 1) * 128], wdt_sb[:, 1, t0:t0 + n], False, True,
                       R=[bsel, bwdt[bi]], W=[bps[6]])
                    s.op("act", lambda e: e.copy(out=wbc_sb[:, wr, 0:n], in_=ps[:, 6, 0:n]), R=[bps[6]], W=[bwbc[wr]])
                    for fc in range(4):
                        pr = fc % 2
                        for which in range(2):
                            bank = 2 * pr + which
                            for k in range(8):
                                wv = wb_sb[:, a, 2 * which + k // 4, :].rearrange("q (a b) -> q a b", a=4)
                                mm(s, ps[:, bank, 0:n], wv[:, k % 4, fc * 128:(fc + 1) * 128], hl2_sb[:, k, t0:t0 + n],
                                   k == 0, k == 7, R=[bwb[a][2 * which + k // 4], bhl2[bi]], W=[bps[bank]])
                        s.op("act", lambda e: e.activation(out=sg_sb[:, pr, 0:n], in_=ps[:, 2 * pr, 0:n], func=AF.Silu),
                             R=[bps[2 * pr]], W=[bsg[pr]])
                        s.op("dve", lambda e: e.tensor_tensor(out=t3_sb[:, pr, 0:n], in0=ps[:, 2 * pr + 1, 0:n],
                                                              in1=sg_sb[:, pr, 0:n], op=ALU.mult),
                             R=[bps[2 * pr + 1], bsg[pr]], W=[bt3[pr]])
                        s.op("pool", lambda e: e.tensor_tensor(out=g_sb[:, gs, fc, 0:n], in0=t3_sb[:, pr, 0:n],
                                                               in1=wbc_sb[:, wr, 0:n], op=ALU.mult),
                             R=[bt3[pr], bwbc[wr]], W=[bg[gs]])
                    for dc in range(8):
                        bank = 4 + dc % 2
                        for fc in range(4):
                            wv = wb_sb[:, a, 4 + fc // 2, :].rearrange("q (a b) -> q a b", a=2)
                            mm(s, ps[:, bank, 0:n], wv[:, fc % 2, dc * 128:(dc + 1) * 128], g_sb[:, gs, fc, 0:n],
                               fc == 0, fc == 3, R=[bwb[a][4 + fc // 2], bg[gs]], W=[bps[bank]])
                        s.op("dve", lambda e: e.scalar_tensor_tensor(out=x_sb[:, dc, t0:t0 + n], in0=ps[:, bank, 0:n],
                                                                     scalar=mod_sb[:, garow, dc:dc + 1],
                                                                     in1=x_sb[:, dc, t0:t0 + n], op0=ALU.mult, op1=ALU.add),
                             R=[bps[bank], bmod, bxb[bi]], W=[bxb[bi]])
            s.barrier()

        with (nc.sbuf_tensor("s4_sq", [128, 8, 512], BF16) as sq_sb,
              nc.sbuf_tensor("s4_rstd", [128, 2, 512], F32) as rstd_sb):
            bsq = s.buf("sq4"); brs = [s.buf(f"rs4{i}") for i in range(2)]
            for bi, (t0, n) in enumerate(BLKS1):
                if final:
                    sl = bi % 2
                    s.op("act", lambda e: e.activation(out=sq_sb[:, :, 0:n], in_=x_sb[:, :, t0:t0 + n], func=AF.Square),
                         R=[bxb[bi]], W=[bsq])
                    for k in range(8):
                        mm(s, ps[:, sl, 0:n], ones_sb[:], sq_sb[:, k, 0:n], k == 0, k == 7, R=[bones, bsq], W=[bps[sl]])
                    s.op("act", lambda e: e.activation(out=rstd_sb[:, sl, 0:n], in_=ps[:, sl, 0:n], func=AF.Sqrt,
                                                       scale=1.0 / D, bias=EPS), R=[bps[sl]], W=[brs[sl]])
                    s.op("dve", lambda e: e.reciprocal(out=rstd_sb[:, sl, 0:n], in_=rstd_sb[:, sl, 0:n]),
                         R=[brs[sl]], W=[brs[sl]])
                    for k in range(8):
                        s.op("dve", lambda e: e.scalar_tensor_tensor(
                            out=x_sb[:, k, t0:t0 + n], in0=x_sb[:, k, t0:t0 + n], scalar=mod_sb[:, 9, k:k + 1],
                            in1=rstd_sb[:, sl, 0:n], op0=ALU.mult, op1=ALU.mult),
                            R=[bxb[bi], bmod, brs[sl]], W=[bxb[bi]])
                s.dma("sp", xo[:, :, t0:t0 + n], x_sb[:, :, t0:t0 + n], R=[bxb[bi]])
            s.finish(bxb)
    return nc


def build_p3a():
    nc = bass.Bass("TRN2", target_bir_lowering=False)
    din = lambda n, shp, dt: nc.dram_tensor(n, shp, dt, kind="ExternalInput").ap()
    xT = din("xT", [128, 8, NT1], F32)
    nadf = din("nadf", [128, 4, NT1], BF16)
    hg = din("hg", [128, 12, NT1], F32)
    wout = din("wout", [D, D], F32)
    mod = din("mod", [128, 10, 8], F32)
    wge = din("wge", [128, 8, 36], F32)
    bge = din("bge", [128, 36], F32)
    iota4 = din("iota4", [128, 4], F32)
    ident = din("ident", [128, 128], F32)
    xo = nc.dram_tensor("xo", [128, 8, NT1], F32, kind="ExternalOutput").ap()
    hl2o = nc.dram_tensor("hl2o", [128, 8, NT1], BF16, kind="ExternalOutput").ap()
    wdto = nc.dram_tensor("wdto", [32, 2, NT1], BF16, kind="ExternalOutput").ap()
    gido = nc.dram_tensor("gido", [128, 17], F32, kind="ExternalOutput").ap()
    s = Sched(nc)
    wov = wout.rearrange("(kc p) n -> p kc n", p=128)
    with (nc.psum_tensor("ps", [128, 8, 512], F32) as ps,
          nc.sbuf_tensor("x_sb", [128, 8, NT1], F32) as x_sb,
          nc.sbuf_tensor("mod_sb", [128, 10, 8], F32) as mod_sb,
          nc.sbuf_tensor("hl2_sb", [128, 8, NT1], BF16) as hl2_sb,
          nc.sbuf_tensor("wdt_sb", [32, 2, NT1], BF16) as wdt_sb,
          nc.sbuf_tensor("ones_sb", [128, 128], BF16) as ones_sb):
        bps = [s.buf(f"ps{i}") for i in range(8)]
        cs = s.dsem("const")
        bxb = [s.buf(f"x{i}", s.dsem(f"x{i}")) for i in range(len(BLKS1))]
        bmod = s.buf("mod", cs)
        bhl2 = [s.buf(f"hl2_{i}") for i in range(len(BLKS1))]
        bwdt = [s.buf(f"wdt{i}") for i in range(len(BLKS1))]
        bones = s.buf("ones")
        s.dma("sp", mod_sb[:], mod, W=[bmod])
        for bi, (t0, n) in enumerate(BLKS1):
            s.dma("sp", x_sb[:, :, t0:t0 + n], xT[:, :, t0:t0 + n], W=[bxb[bi]])
        s.op("pool", lambda e: e.memset(ones_sb[:], 1.0), W=[bones])

        with (nc.sbuf_tensor("s1_mix", [128, 8, NT1], BF16) as mix_sb,
              nc.sbuf_tensor("s1_wobf", [128, 8, D], BF16) as wo_bf,
              nc.sbuf_tensor("s1_wost", [128, 2, D], F32) as wo_st,
              nc.sbuf_tensor("s1_hg", [128, 1, 12, 512], F32) as hg_sb,
              nc.sbuf_tensor("s1_t", [128, 4, 512], F32) as t_sb):
            bmixl = s.buf("mixl", s.dsem("mixl"))
            bmix = [s.buf(f"mix{i}") for i in range(len(BLKS1))]
            bwost = [s.buf(f"wost{i}", s.dsem(f"wost{i}")) for i in range(2)]
            bwobf = [s.buf(f"wobf{k}") for k in range(8)]
            bhg = [s.buf(f"hg{i}", s.dsem(f"hg{i}")) for i in range(2)]
            bt = [s.buf(f"t{i}") for i in range(4)]
            s.dma("act", mix_sb[:, 0:2, :], nadf[:, 0:2, :], W=[bmixl])
            s.dma("act", mix_sb[:, 6:8, :], nadf[:, 2:4, :], W=[bmixl])
            for k in range(8):
                sl = k % 2
                s.dma("act", wo_st[:, sl, :], wov[:, k, :], W=[bwost[sl]])
                s.op("pool", lambda e: e.tensor_copy(out=wo_bf[:, k, :], in_=wo_st[:, sl, :]), R=[bwost[sl]], W=[bwobf[k]])
            for bi, (t0, n) in enumerate(BLKS1):
                sl = 0
                s.dma("sp", hg_sb[:, sl, :, 0:n], hg[:, :, t0:t0 + n], W=[bhg[sl]])
                for ch in range(4):
                    hf_ = hg_sb[:, sl, ch, 0:n]; hb_ = hg_sb[:, sl, 4 + ch, 0:n]; gr_ = hg_sb[:, sl, 8 + ch, 0:n]
                    s.op("dve", lambda e: e.tensor_tensor(out=t_sb[:, 0, 0:n], in0=hf_, in1=hb_, op=ALU.add),
                         R=[bhg[sl]], W=[bt[0]])
                    s.op("dve", lambda e: e.tensor_tensor(out=t_sb[:, 1, 0:n], in0=gr_, in1=gr_, op=ALU.mult),
                         R=[bhg[sl]], W=[bt[1]])
                    s.op("dve", lambda e: e.tensor_scalar(out=t_sb[:, 1, 0:n], in0=t_sb[:, 1, 0:n], scalar1=0.044715,
                                                          scalar2=1.0, op0=ALU.mult, op1=ALU.add), R=[bt[1]], W=[bt[1]])
                    s.op("pool", lambda e: e.tensor_tensor(out=t_sb[:, 2, 0:n], in0=t_sb[:, 1, 0:n], in1=gr_, op=ALU.mult),
                         R=[bt[1], bhg[sl]], W=[bt[2]])
                    s.op("act", lambda e: e.activation(out=t_sb[:, 2, 0:n], in_=t_sb[:, 2, 0:n], func=AF.Sigmoid,
                                                       scale=GELU_C), R=[bt[2]], W=[bt[2]])
                    s.op("pool", lambda e: e.tensor_tensor(out=t_sb[:, 3, 0:n], in0=t_sb[:, 2, 0:n], in1=gr_, op=ALU.mult),
                         R=[bt[2], bhg[sl]], W=[bt[3]])
                    s.op("pool", lambda e: e.tensor_tensor(out=mix_sb[:, 2 + ch, t0:t0 + n], in0=t_sb[:, 3, 0:n],
                                                           in1=t_sb[:, 0, 0:n], op=ALU.mult),
                         R=[bt[3], bt[0]], W=[bmix[bi]])
            pi = 0
            for bi, (t0, n) in enumerate(BLKS1):
                garow = 5 if bi == 4 else 1
                for dc in range(8):
                    pb = pi % 8; pi += 1
                    for k in range(8):
                        mm(s, ps[:, pb, 0:n], wo_bf[:, k, dc * 128:(dc + 1) * 128], mix_sb[:, k, t0:t0 + n],
                           k == 0, k == 7, R=[bwobf[k], bmix[bi], bmixl], W=[bps[pb]])
                    s.op("dve", lambda e: e.scalar_tensor_tensor(out=x_sb[:, dc, t0:t0 + n], in0=ps[:, pb, 0:n],
                                                                 scalar=mod_sb[:, garow, dc:dc + 1],
                                                                 in1=x_sb[:, dc, t0:t0 + n], op0=ALU.mult, op1=ALU.add),
                         R=[bps[pb], bmod, bxb[bi]], W=[bxb[bi]])
            s.barrier()

        with (nc.sbuf_tensor("s2_sq", [128, 8, 512], BF16) as sq_sb,
              nc.sbuf_tensor("s2_rstd", [128, 2, 512], F32) as rstd_sb,
              nc.sbuf_tensor("s2_tmp", [128, 4, 512], F32) as tmp_sb,
              nc.sbuf_tensor("s2_hf", [128, 2, 8, 512], F32) as hf_sb,
              nc.sbuf_tensor("s2_ab", [128, 2, 8], F32) as ab_sb,
              nc.sbuf_tensor("s2_wge", [128, 8, 36], F32) as wge_sb,
              nc.sbuf_tensor("s2_bge", [128, 36], F32) as bge_sb,
              nc.sbuf_tensor("s2_id", [128, 128], F32) as id_sb,
              nc.sbuf_tensor("s2_rt", [128, 2, 128], F32) as rt_sb,
              nc.sbuf_tensor("s2_io4", [128, 4], F32) as io4_sb,
              nc.sbuf_tensor("s2_gid", [128, 17], F32) as gid_sb,
              nc.sbuf_tensor("s2_junk", [128, 4], F32) as junk4_sb):
            bsq = s.buf("sq"); brs = [s.buf(f"rs{i}") for i in range(2)]
            btmp = [s.buf(f"tmp{i}") for i in range(4)]
            bhf = [s.buf(f"hf{i}") for i in range(2)]
            bab = s.buf("ab")
            bwge = s.buf("wge", cs); bbge = s.buf("bge", cs); bid = s.buf("id", cs)
            brt = [s.buf(f"rt{i}") for i in range(2)]
            s.dma("act", wge_sb[:], wge, W=[bwge])
            s.dma("act", bge_sb[:], bge, W=[bbge])
            s.dma("act", id_sb[:], ident, W=[bid])
            bio4 = s.buf("io4", cs); bgid = s.buf("gid", s.dsem("gid")); bjk = s.buf("junk4")
            s.dma("act", io4_sb[:], iota4, W=[bio4])
            s.op("pool", lambda e: e.memset(gid_sb[:], 0.0), W=[bgid])
            for t in range(2):
                s.op("dve", lambda e: e.scalar_tensor_tensor(
                    out=ab_sb[:, t, :], in0=mod_sb[:, 2 + 4 * t, :], scalar=1.0, in1=mod_sb[:, 0, :],
                    op0=ALU.add, op1=ALU.mult), R=[bmod], W=[bab])
            ti_g = 0
            for bi, (t0, n) in enumerate(BLKS1):
                sl = bi % 2
                isctx = bi == 4
                s.op("act", lambda e: e.activation(out=sq_sb[:, :, 0:n], in_=x_sb[:, :, t0:t0 + n], func=AF.Square),
                     R=[bxb[bi]], W=[bsq])
                for k in range(8):
                    mm(s, ps[:, sl, 0:n], ones_sb[:], sq_sb[:, k, 0:n], k == 0, k == 7, R=[bones, bsq], W=[bps[sl]])
                s.op("act", lambda e: e.activation(out=rstd_sb[:, sl, 0:n], in_=ps[:, sl, 0:n], func=AF.Sqrt,
                                                   scale=1.0 / D, bias=EPS), R=[bps[sl]], W=[brs[sl]])
                s.op("dve", lambda e: e.reciprocal(out=rstd_sb[:, sl, 0:n], in_=rstd_sb[:, sl, 0:n]),
                     R=[brs[sl]], W=[brs[sl]])
                ai = 1 if isctx else 0
                shrow = 7 if isctx else 3
                for k in range(8):
                    tb = k % 4
                    s.op("dve", lambda e: e.scalar_tensor_tensor(
                        out=tmp_sb[:, tb, 0:n], in0=x_sb[:, k, t0:t0 + n], scalar=ab_sb[:, ai, k:k + 1],
                        in1=rstd_sb[:, sl, 0:n], op0=ALU.mult, op1=ALU.mult),
                        R=[bxb[bi], bab, brs[sl]], W=[btmp[tb]])
                    s.op("act", lambda e: e.activation(out=hf_sb[:, sl, k, 0:n], in_=tmp_sb[:, tb, 0:n],
                                                       func=AF.Identity, bias=mod_sb[:, shrow, k:k + 1], scale=1.0),
                         R=[btmp[tb], bmod], W=[bhf[sl]])
                s.op("pool", lambda e: e.tensor_copy(out=hl2_sb[:, :, t0:t0 + n], in_=hf_sb[:, sl, :, 0:n]),
                     R=[bhf[sl]], W=[bhl2[bi]])
                for tt in range((n + 127) // 128):
                    c0 = tt * 128
                    m = min(128, n - c0)
                    rs_ = ti_g % 2; ti_g += 1
                    pb = 2 + rs_
                    rt = rt_sb[0:m, rs_, :]
                    brr = brt[rs_]
                    for k in range(8):
                        mm(s, ps[0:m, pb, 0:36], hf_sb[:, sl, k, c0:c0 + m], wge_sb[:, k, :], k == 0, k == 7,
                           R=[bhf[sl], bwge], W=[bps[pb]])
                    V = lambda eng, fn, R_=(), W_=(): s.op(eng, fn, R=[brr] + list(R_), W=[brr] + list(W_))
                    lg = rt[:, 0:36]
                    s.op("dve", lambda e: e.tensor_tensor(out=lg, in0=ps[0:m, pb, 0:36], in1=bge_sb[0:m, :], op=ALU.add),
                         R=[bps[pb], bbge], W=[brr])
                    gmax = rt[:, 36:37]; ngmax = rt[:, 37:38]; sume = rt[:, 38:39]; gtop = rt[:, 39:40]
                    eg = rt[:, 40:44]; ohg = rt[:, 44:48]; sel = rt[:, 48:56]; top8 = rt[:, 56:64]
                    dd = rt[:, 64:65]; ed = rt[:, 65:66]; w1_ = rt[:, 66:67]; wt1 = rt[:, 67:68]; wt2 = rt[:, 68:69]
                    ea = rt[:, 72:80]; eb_ = rt[:, 80:88]; wd = rt[:, 96:128]
                    V("dve", lambda e: e.reduce_max(out=gmax, in_=lg[:, 0:4], axis=AX.X))
                    V("dve", lambda e: e.tensor_scalar(out=ngmax, in0=gmax, scalar1=-1.0, scalar2=None, op0=ALU.mult))
                    V("act", lambda e: e.activation(out=eg, in_=lg[:, 0:4], func=AF.Exp, bias=ngmax, scale=1.0,
                                                    accum_out=sume))
                    V("dve", lambda e: e.reciprocal(out=gtop, in_=sume))
                    V("dve", lambda e: e.tensor_scalar(out=ohg, in0=lg[:, 0:4], scalar1=gmax, scalar2=None,
                                                       op0=ALU.is_equal))
                    tgl = (t0 + c0) // 128
                    s.op("dve", lambda e: e.scalar_tensor_tensor(out=junk4_sb[0:m, :], in0=ohg, scalar=1.0,
                                                                 in1=io4_sb[0:m, :], op0=ALU.mult, op1=ALU.mult,
                                                                 accum_out=gid_sb[0:m, tgl:tgl + 1]),
                         R=[brr, bio4, bjk], W=[bjk, bgid])
                    V("dve", lambda e: e.tensor_scalar(out=sel, in0=lg[:, 4:12], scalar1=ohg[:, 0:1], scalar2=None,
                                                       op0=ALU.mult))
                    for g in range(1, 4):
                        V("dve", lambda e: e.scalar_tensor_tensor(out=sel, in0=lg[:, 4 + 8 * g:12 + 8 * g],
                                                                  scalar=ohg[:, g:g + 1], in1=sel,
                                                                  op0=ALU.mult, op1=ALU.add))
                    V("dve", lambda e: e.max(out=top8, in_=sel))
                    V("dve", lambda e: e.tensor_tensor(out=dd, in0=top8[:, 1:2], in1=top8[:, 0:1], op=ALU.subtract))
                    V("act", lambda e: e.activation(out=ed, in_=dd, func=AF.Exp))
                    V("dve", lambda e: e.tensor_scalar(out=w1_, in0=ed, scalar1=1.0, scalar2=None, op0=ALU.add))
                    V("dve", lambda e: e.reciprocal(out=w1_, in_=w1_))
                    V("dve", lambda e: e.tensor_tensor(out=wt1, in0=w1_, in1=gtop, op=ALU.mult))
                    V("dve", lambda e: e.tensor_tensor(out=wt2, in0=wt1, in1=ed, op=ALU.mult))
                    V("dve", lambda e: e.tensor_scalar(out=ea, in0=sel, scalar1=top8[:, 0:1], scalar2=wt1,
                                                       op0=ALU.is_equal, op1=ALU.mult))
                    V("dve", lambda e: e.tensor_scalar(out=eb_, in0=sel, scalar1=top8[:, 1:2], scalar2=wt2,
                                                       op0=ALU.is_equal, op1=ALU.mult))
                    V("dve", lambda e: e.tensor_tensor(out=ea, in0=ea, in1=eb_, op=ALU.add))
                    for g in range(4):
                        V("dve", lambda e: e.tensor_scalar(out=wd[:, 8 * g:8 * g + 8], in0=ea, scalar1=ohg[:, g:g + 1],
                                                           scalar2=None, op0=ALU.mult))
                    pt = 4 + rs_
                    s.op("pe", lambda e: e.transpose(ps[0:32, pt, 0:m], wd, id_sb[0:m, 0:m]), R=[brr, bid], W=[bps[pt]])
                    s.op("act", lambda e: e.copy(out=wdt_sb[:, 0, t0 + c0:t0 + c0 + m], in_=ps[0:32, pt, 0:m]),
                         R=[bps[pt]], W=[bwdt[bi]])
                    s.op("dve", lambda e: e.tensor_tensor(out=wdt_sb[:, 1, t0 + c0:t0 + c0 + m], in0=ps[0:32, pt, 0:m],
                                                          in1=wdt_sb[:, 0, t0 + c0:t0 + c0 + m], op=ALU.subtract),
                         R=[bps[pt], bwdt[bi]], W=[bwdt[bi]])
            bxo = s.buf("xout", s.dsem("xout"))
            s.dma("sp", xo, x_sb[:], R=bxb, W=[bxo])
            s.dma("sp", hl2o, hl2_sb[:], R=bhl2, W=[bxo])
            s.dma("sp", wdto, wdt_sb[:], R=bwdt, W=[bxo])
            s.dma("sp", gido, gid_sb[:], R=[bgid], W=[bxo])
            s.finish([bxo])
    return nc


def build_p3b(ntb):
    NE = 8
    nblk_all = ntb // 512
    nh = 2 if ntb > 2048 else 1
    nblk = -(-nblk_all // nh)
    nth = nblk * 512
    nc = bass.Bass("TRN2", target_bir_lowering=False)
    din = lambda n, shp, dt: nc.dram_tensor(n, shp, dt, kind="ExternalInput").ap()
    hl2 = din("hl2", [128, 8, ntb], BF16)
    wdt = din("wdt", [8, 2, ntb], BF16)
    selc = din("selc", [8, NE * 128], BF16)
    w1 = din("w1", [NE, D, 512], F32)
    w3 = din("w3", [NE, D, 512], F32)
    w2 = din("w2", [NE, 512, D], F32)
    yo = nc.dram_tensor("yo", [128, 8, ntb], F32, kind="ExternalOutput").ap()
    s = Sched(nc)
    with (nc.psum_tensor("ps", [128, 8, 512], F32) as ps,
          nc.sbuf_tensor("y_sb", [128, 8, nth], F32) as y_sb,
          nc.sbuf_tensor("hl2_sb", [128, 8, nth], BF16) as hl2_sb,
          nc.sbuf_tensor("wdt_sb", [8, 2, ntb], BF16) as wdt_sb,
          nc.sbuf_tensor("s3_st", [128, 3, 2048], F32) as st_sb,
          nc.sbuf_tensor("s3_wb", [128, 2, 6, 2048], BF16) as wb_sb,
          nc.sbuf_tensor("s3_sel", [8, NE * 128], BF16) as sel_sb,
          nc.sbuf_tensor("s3_wbc", [128, 2, 512], F32) as wbc_sb,
          nc.sbuf_tensor("s3_sg", [128, 2, 512], F32) as sg_sb,
          nc.sbuf_tensor("s3_t", [128, 2, 512], F32) as t3_sb,
          nc.sbuf_tensor("s3_g", [128, 2, 4, 512], BF16) as g_sb):
        bps = [s.buf(f"ps{i}") for i in range(8)]
        cs = s.dsem("const")
        by = [s.buf(f"y{i}", s.dsem(f"y{i}")) for i in range(nblk)]
        bhl2 = [s.buf(f"hl2_{i}", s.dsem(f"hl{i}")) for i in range(nblk)]
        bwdt = s.buf("wdt", cs)
        bst = [s.buf(f"st{i}", s.dsem(f"st{i}")) for i in range(3)]
        bwb = [[s.buf(f"wb{a}_{p}") for p in range(6)] for a in range(2)]
        bsel = s.buf("sel", cs)
        bwbc = [s.buf(f"wbc{i}") for i in range(2)]
        bsg = [s.buf(f"sg{i}") for i in range(2)]
        bt3 = [s.buf(f"t3{i}") for i in range(2)]
        bg = [s.buf(f"g{i}") for i in range(2)]
        s.dma("act", sel_sb[:], selc, W=[bsel])
        s.dma("act", wdt_sb[:], wdt, W=[bwdt])
        w1v = w1.rearrange("e (kc p) f -> e p kc f", p=128)
        w3v = w3.rearrange("e (kc p) f -> e p kc f", p=128)
        w2v = w2.rearrange("e (fc p) d -> e p fc d", p=128)

        def piece_src(e, p):
            if p < 2:
                return w1v[e, :, 4 * p:4 * p + 4, :]
            if p < 4:
                return w3v[e, :, 4 * (p - 2):4 * (p - 2) + 4, :]
            return w2v[e, :, 2 * (p - 4):2 * (p - 4) + 2, :]

        def piece_dma(P):
            e, p = divmod(P, 6)
            if e >= NE * nh:
                return
            e = e % NE
            sl = P % 3
            dst = st_sb[:, sl, :]
            dst = dst.rearrange("q (a b) -> q a b", a=4) if p < 4 else dst.rearrange("q (a b) -> q a b", a=2)
            s.dma("sp", dst, piece_src(e, p), W=[bst[sl]])

        def piece_cast(P):
            e, p = divmod(P, 6)
            if e >= NE * nh:
                return
            sl = P % 3
            s.op("pool", lambda en: en.tensor_copy(out=wb_sb[:, e % 2, p, :], in_=st_sb[:, sl, :]),
                 R=[bst[sl]], W=[bwb[e % 2][p]])

        for P in range(3):
            piece_dma(P)
        for P in range(6):
            piece_cast(P)
            piece_dma(P + 3)
        gi = 0
        n = 512
        pend = [6 * 1 + i for i in range(6)]
        for vx in range(NE * nh):
            half, ex = divmod(vx, NE)
            a = vx % 2
            pend = [6 * (vx + 1) + i for i in range(6)]
            hb0 = half * nblk
            nb_h = min(nblk, nblk_all - hb0)
            if ex == 0:
                for bi in range(nb_h):
                    s.dma("act", hl2_sb[:, :, bi * 512:(bi + 1) * 512], hl2[:, :, (hb0 + bi) * 512:(hb0 + bi + 1) * 512],
                          W=[bhl2[bi]])
            for bi in range(nb_h):
                t0 = bi * 512
                tg = (hb0 + bi) * 512
                npc = (6 + nb_h - 1) // nb_h
                for P in pend[bi * npc:(bi + 1) * npc]:
                    piece_cast(P)
                    piece_dma(P + 3)
                wr = gi % 2
                gs = gi % 2
                gi += 1
                mm(s, ps[:, 6, 0:n], sel_sb[:, ex * 128:(ex + 1) * 128], wdt_sb[:, 0, tg:tg + n], True, False,
                   R=[bsel, bwdt], W=[bps[6]])
                mm(s, ps[:, 6, 0:n], sel_sb[:, ex * 128:(ex + 1) * 128], wdt_sb[:, 1, tg:tg + n], False, True,
                   R=[bsel, bwdt], W=[bps[6]])
                s.op("act", lambda e: e.copy(out=wbc_sb[:, wr, 0:n], in_=ps[:, 6, 0:n]), R=[bps[6]], W=[bwbc[wr]])
                for fc in range(4):
                    pr = fc % 2
                    for which in range(2):
                        bank = 2 * pr + which
                        for k in range(8):
                            wv = wb_sb[:, a, 2 * which + k // 4, :].rearrange("q (a b) -> q a b", a=4)
                            mm(s, ps[:, bank, 0:n], wv[:, k % 4, fc * 128:(fc + 1) * 128], hl2_sb[:, k, t0:t0 + n],
                               k == 0, k == 7, R=[bwb[a][2 * which + k // 4], bhl2[bi]], W=[bps[bank]])
                    s.op("act", lambda e: e.activation(out=sg_sb[:, pr, 0:n], in_=ps[:, 2 * pr, 0:n], func=AF.Silu),
                         R=[bps[2 * pr]], W=[bsg[pr]])
                    s.op("dve", lambda e: e.tensor_tensor(out=t3_sb[:, pr, 0:n], in0=ps[:, 2 * pr + 1, 0:n],
                                                          in1=sg_sb[:, pr, 0:n], op=ALU.mult),
                         R=[bps[2 * pr + 1], bsg[pr]], W=[bt3[pr]])
                    s.op("pool", lambda e: e.tensor_tensor(out=g_sb[:, gs, fc, 0:n], in0=t3_sb[:, pr, 0:n],
                                                           in1=wbc_sb[:, wr, 0:n], op=ALU.mult),
                         R=[bt3[pr], bwbc[wr]], W=[bg[gs]])
                for dc in range(8):
                    bank = 4 + dc % 2
                    for fc in range(4):
                        wv = wb_sb[:, a, 4 + fc // 2, :].rearrange("q (a b) -> q a b", a=2)
                        mm(s, ps[:, bank, 0:n], wv[:, fc % 2, dc * 128:(dc + 1) * 128], g_sb[:, gs, fc, 0:n],
                           fc == 0, fc == 3, R=[bwb[a][4 + fc // 2], bg[gs]], W=[bps[bank]])
                    if ex == 0:
                        s.op("dve", lambda e: e.tensor_copy(out=y_sb[:, dc, t0:t0 + n], in_=ps[:, bank, 0:n]),
                             R=[bps[bank]], W=[by[bi]])
                    else:
                        s.op("dve", lambda e: e.tensor_tensor(out=y_sb[:, dc, t0:t0 + n], in0=ps[:, bank, 0:n],
                                                              in1=y_sb[:, dc, t0:t0 + n], op=ALU.add),
                             R=[bps[bank], by[bi]], W=[by[bi]])
            if ex == NE - 1:
                for bi in range(nb_h):
                    s.dma("sp", yo[:, :, (hb0 + bi) * 512:(hb0 + bi + 1) * 512], y_sb[:, :, bi * 512:(bi + 1) * 512],
                          R=[by[bi]])
        s.finish(by)
    return nc


def build_pc(final):
    nc = bass.Bass("TRN2", target_bir_lowering=False)
    din = lambda n, shp, dt: nc.dram_tensor(n, shp, dt, kind="ExternalInput").ap()
    xT = din("xT", [128, 8, NT1], F32)
    yT = din("yT", [128, 8, NT1], F32)
    mod = din("mod", [128, 3, 8], F32)
    xo = nc.dram_tensor("xo", [128, 8, NT1], F32, kind="ExternalOutput").ap()
    s = Sched(nc)
    with (nc.psum_tensor("ps", [128, 2, 512], F32) as ps,
          nc.sbuf_tensor("x_sb", [128, 8, NT1], F32) as x_sb,
          nc.sbuf_tensor("y_sb", [128, 8, NT1], F32) as y_sb,
          nc.sbuf_tensor("mod_sb", [128, 3, 8], F32) as mod_sb,
          nc.sbuf_tensor("ones_sb", [128, 128], BF16) as ones_sb,
          nc.sbuf_tensor("sq_sb", [128, 8, 512], BF16) as sq_sb,
          nc.sbuf_tensor("rstd_sb", [128, 2, 512], F32) as rstd_sb):
        bxb = [s.buf(f"x{i}", s.dsem(f"x{i}")) for i in range(len(BLKS1))]
        byb = [s.buf(f"y{i}", s.dsem(f"yy{i}")) for i in range(len(BLKS1))]
        bmod = s.buf("mod", s.dsem("mod"))
        bones = s.buf("ones"); bsq = s.buf("sq"); brs = [s.buf(f"rs{i}") for i in range(2)]
        bps = [s.buf(f"ps{i}") for i in range(2)]
        s.dma("sp", mod_sb[:], mod, W=[bmod])
        s.op("pool", lambda e: e.memset(ones_sb[:], 1.0), W=[bones])
        for bi, (t0, n) in enumerate(BLKS1):
            s.dma("sp", x_sb[:, :, t0:t0 + n], xT[:, :, t0:t0 + n], W=[bxb[bi]])
            s.dma("act", y_sb[:, :, t0:t0 + n], yT[:, :, t0:t0 + n], W=[byb[bi]])
        for bi, (t0, n) in enumerate(BLKS1):
            garow = 1 if bi == 4 else 0
            sl = bi % 2
            for k in range(8):
                s.op("dve", lambda e: e.scalar_tensor_tensor(
                    out=x_sb[:, k, t0:t0 + n], in0=y_sb[:, k, t0:t0 + n], scalar=mod_sb[:, garow, k:k + 1],
                    in1=x_sb[:, k, t0:t0 + n], op0=ALU.mult, op1=ALU.add),
                    R=[byb[bi], bmod, bxb[bi]], W=[bxb[bi]])
            if final:
                s.op("act", lambda e: e.activation(out=sq_sb[:, :, 0:n], in_=x_sb[:, :, t0:t0 + n], func=AF.Square),
                     R=[bxb[bi]], W=[bsq])
                for k in range(8):
                    mm(s, ps[:, sl, 0:n], ones_sb[:], sq_sb[:, k, 0:n], k == 0, k == 7, R=[bones, bsq], W=[bps[sl]])
                s.op("act", lambda e: e.activation(out=rstd_sb[:, sl, 0:n], in_=ps[:, sl, 0:n], func=AF.Sqrt,
                                                   scale=1.0 / D, bias=EPS), R=[bps[sl]], W=[brs[sl]])
                s.op("dve", lambda e: e.reciprocal(out=rstd_sb[:, sl, 0:n], in_=rstd_sb[:, sl, 0:n]),
                     R=[brs[sl]], W=[brs[sl]])
                for k in range(8):
                    s.op("dve", lambda e: e.scalar_tensor_tensor(
                        out=x_sb[:, k, t0:t0 + n], in0=x_sb[:, k, t0:t0 + n], scalar=mod_sb[:, 2, k:k + 1],
                        in1=rstd_sb[:, sl, 0:n], op0=ALU.mult, op1=ALU.mult),
                        R=[bxb[bi], bmod, brs[sl]], W=[bxb[bi]])
            s.dma("sp", xo[:, :, t0:t0 + n], x_sb[:, :, t0:t0 + n], R=[bxb[bi]])
        s.finish(bxb)
    return nc


def run_p3(nc3, l, xl, xc, yna, ydf, hf, hb, fmf, mods_l, inp, g_final):
    wge = np.concatenate([inp["router_w_group"][l], inp["router_w_expert"][l]], axis=1)
    wge = np.ascontiguousarray(wge.reshape(8, 128, 36).transpose(1, 0, 2))
    bge = np.concatenate([inp["router_b_group"][l], inp["router_b_expert"][l]])
    bge = np.ascontiguousarray(np.tile(bge[None, :], (128, 1))).astype(np.float32)
    selc = np.zeros((32, NEXP, 128), np.float32)
    for e in range(NEXP):
        selc[e, e, :] = 1.0
    selc = selc.reshape(32, NEXP * 128).astype(ml_dtypes.bfloat16)
    ident = np.eye(128, dtype=np.float32)
    in_maps = []
    for i in range(NCORES):
        b, j = i // 4, i % 4
        lat = slice(2048 * j, 2048 * (j + 1))
        ctxs = slice(S + 64 * j, S + 64 * (j + 1))
        xx = np.concatenate([xl[b, lat], xc[b, 64 * j:64 * (j + 1)]], axis=0)
        na = np.concatenate([yna[b, lat], yna[b, ctxs]], axis=0)
        df = np.concatenate([ydf[b, lat], ydf[b, ctxs]], axis=0)
        nadf = np.concatenate([na, df], axis=1)
        nadf = np.ascontiguousarray(nadf.T.reshape(4, 128, NT1).transpose(1, 0, 2))
        def tk(a):
            aa = np.concatenate([a[:, lat], a[:, ctxs]], axis=1)
            return aa.reshape(4, 128, NT1).transpose(1, 0, 2)
        hgt = np.ascontiguousarray(np.concatenate([tk(hf[b]), tk(hb[b]), tk(fmf[b, 512:1024])], axis=1))
        m = mods_l
        rows = [inp["g_ffn"][l], m[b, 2048:3072], m[b, 4096:5120], m[b, 3072:4096], m[b, 5120:6144],
                m[2, 2048:3072], m[2, 4096:5120], m[2, 3072:4096], m[2, 5120:6144], g_final]
        mod = np.ascontiguousarray(np.stack([vec_pk(r) for r in rows], axis=1)).astype(np.float32)
        in_maps.append({"xT": chunkT(xx), "nadf": nadf, "hg": hgt, "wout": inp["w_out"][l], "mod": mod,
                        "wge": wge, "bge": bge, "selc": selc, "ident": ident,
                        "w1": inp["moe_w1"][l], "w3": inp["moe_w3"][l], "w2": inp["moe_w2"][l]})
    res = run_bass_kernel_spmd(nc3, in_maps, core_ids=list(range(NCORES)))
    xl2 = np.zeros_like(xl); xc2 = np.zeros_like(xc)
    for i in range(NCORES):
        b, j = i // 4, i % 4
        o = res.results[i]["xo"].transpose(1, 0, 2).reshape(D, NT1).T
        xl2[b, 2048 * j:2048 * (j + 1)] = o[:2048]
        xc2[b, 64 * j:64 * (j + 1)] = o[2048:]
    return xl2, xc2


def p3_inmaps_common(l, xl, xc, yna, ydf, hf, hb, fmf, mods_l, inp, g_final):
    wge = np.concatenate([inp["router_w_group"][l], inp["router_w_expert"][l]], axis=1)
    wge = np.ascontiguousarray(wge.reshape(8, 128, 36).transpose(1, 0, 2))
    bge = np.concatenate([inp["router_b_group"][l], inp["router_b_expert"][l]])
    bge = np.ascontiguousarray(np.tile(bge[None, :], (128, 1))).astype(np.float32)
    ident = np.eye(128, dtype=np.float32)
    iota4 = np.ascontiguousarray(np.tile(np.arange(4, dtype=np.float32)[None, :], (128, 1)))
    in_maps = []
    for i in range(NCORES):
        b, j = i // 4, i % 4
        lat = slice(2048 * j, 2048 * (j + 1))
        ctxs = slice(S + 64 * j, S + 64 * (j + 1))
        xx = np.concatenate([xl[b, lat], xc[b, 64 * j:64 * (j + 1)]], axis=0)
        na = np.concatenate([yna[b, lat], yna[b, ctxs]], axis=0)
        df = np.concatenate([ydf[b, lat], ydf[b, ctxs]], axis=0)
        nadf = np.concatenate([na, df], axis=1)
        nadf = np.ascontiguousarray(nadf.T.reshape(4, 128, NT1).transpose(1, 0, 2))

        def tk(a):
            aa = np.concatenate([a[:, lat], a[:, ctxs]], axis=1)
            return aa.reshape(4, 128, NT1).transpose(1, 0, 2)
        hgt = np.ascontiguousarray(np.concatenate([tk(hf[b]), tk(hb[b]), tk(fmf[b, 512:1024])], axis=1))
        m = mods_l
        rows = [inp["g_ffn"][l], m[b, 2048:3072], m[b, 4096:5120], m[b, 3072:4096], m[b, 5120:6144],
                m[2, 2048:3072], m[2, 4096:5120], m[2, 3072:4096], m[2, 5120:6144], g_final]
        mod = np.ascontiguousarray(np.stack([vec_pk(r) for r in rows], axis=1)).astype(np.float32)
        in_maps.append({"xT": chunkT(xx), "nadf": nadf, "hg": hgt, "wout": inp["w_out"][l], "mod": mod,
                        "wge": wge, "bge": bge, "ident": ident, "iota4": iota4})
    return in_maps


def run_p3_sparse(l, xl, xc, yna, ydf, hf, hb, fmf, mods_l, inp, g_final, final):
    in_maps = p3_inmaps_common(l, xl, xc, yna, ydf, hf, hb, fmf, mods_l, inp, g_final)
    resa = run_bass_kernel_spmd(build_p3a(), in_maps, core_ids=list(range(NCORES))).results
    NTT = NCORES * NT1
    HL2 = np.zeros((D, NTT), ml_dtypes.bfloat16)
    WDT = np.zeros((32, 2, NTT), ml_dtypes.bfloat16)
    gid = np.zeros(NTT, np.int64)
    for i in range(NCORES):
        HL2[:, i * NT1:(i + 1) * NT1] = resa[i]["hl2o"].transpose(1, 0, 2).reshape(D, NT1)
        WDT[:, :, i * NT1:(i + 1) * NT1] = resa[i]["wdto"]
        g = resa[i]["gido"]
        gid[i * NT1:(i + 1) * NT1] = np.rint(g.T.reshape(-1)[:NT1]).astype(np.int64)
    toks = [np.nonzero(gid == g)[0] for g in range(4)]
    ncg = [1, 1, 1, 1]
    for _ in range(NCORES - 4):
        gbig = max(range(4), key=lambda g: len(toks[g]) / ncg[g])
        ncg[gbig] += 1
    idxs = []
    cgroup = []
    for g in range(4):
        parts = np.array_split(toks[g], ncg[g])
        for pp in parts:
            idxs.append(pp); cgroup.append(g)
    ntb = max(512, int(-(-max(len(ix) for ix in idxs) // 512) * 512))
    sel8 = np.zeros((8, 8, 128), np.float32)
    for e in range(8):
        sel8[e, e, :] = 1.0
    sel8 = sel8.reshape(8, 1024).astype(ml_dtypes.bfloat16)
    mapsb = []
    for c in range(NCORES):
        g = cgroup[c]
        ix = idxs[c]
        h2 = np.zeros((D, ntb), ml_dtypes.bfloat16)
        h2[:, :len(ix)] = HL2[:, ix]
        wd = np.zeros((8, 2, ntb), ml_dtypes.bfloat16)
        wd[:, :, :len(ix)] = WDT[8 * g:8 * g + 8][:, :, ix]
        mapsb.append({"hl2": np.ascontiguousarray(h2.reshape(8, 128, ntb).transpose(1, 0, 2)), "wdt": wd, "selc": sel8,
                      "w1": np.ascontiguousarray(inp["moe_w1"][l][8 * g:8 * g + 8]),
                      "w3": np.ascontiguousarray(inp["moe_w3"][l][8 * g:8 * g + 8]),
                      "w2": np.ascontiguousarray(inp["moe_w2"][l][8 * g:8 * g + 8])})
    resb = run_bass_kernel_spmd(build_p3b(ntb), mapsb, core_ids=list(range(NCORES))).results
    Y = np.zeros((D, NTT), np.float32)
    for c in range(NCORES):
        ix = idxs[c]
        Y[:, ix] = resb[c]["yo"].transpose(1, 0, 2).reshape(D, ntb)[:, :len(ix)]
    mapsc = []
    for i in range(NCORES):
        b = i // 4
        modc = np.stack([vec_pk(mods_l[b, 5120:6144]), vec_pk(mods_l[2, 5120:6144]), vec_pk(g_final)], axis=1)
        mapsc.append({"xT": resa[i]["xo"], "mod": np.ascontiguousarray(modc).astype(np.float32),
                      "yT": np.ascontiguousarray(Y[:, i * NT1:(i + 1) * NT1].reshape(8, 128, NT1).transpose(1, 0, 2))})
    resc = run_bass_kernel_spmd(build_pc(final), mapsc, core_ids=list(range(NCORES))).results
    xl2 = np.zeros_like(xl); xc2 = np.zeros_like(xc)
    for i in range(NCORES):
        b, j = i // 4, i % 4
        o = resc[i]["xo"].transpose(1, 0, 2).reshape(D, NT1).T
        xl2[b, 2048 * j:2048 * (j + 1)] = o[:2048]
        xc2[b, 64 * j:64 * (j + 1)] = o[2048:]
    return xl2, xc2


def kernel(**inputs):
    inp = {k: np.asarray(v) for k, v in inputs.items()}
    x = np.ascontiguousarray(inp["x"], dtype=np.float32)
    ctx = np.ascontiguousarray(inp["ctx"], dtype=np.float32)
    mods = run_p0(inp["c"], inp["c_ctx"], inp["w_ada"], inp["b_ada"])
    cosT, sinT = rope_tables()
    xl, xc = x, ctx
    for l in range(DEPTH):
        fmb, fmf, tm = run_p1(build_p1(), xl, xc, mods[l], inp["g_mix"][l], inp["w_in"][l], cosT, sinT)
        lam_init = 0.8 - 0.6 * math.exp(-0.3 * l)
        yna, ydf, hf, hb = run_p2(build_p2(lam_init), l, fmb, fmf, tm, inp)
        xl, xc = run_p3_sparse(l, xl, xc, yna, ydf, hf, hb, fmf, mods[l], inp, inp["g_final"], l == DEPTH - 1)
    return np.ascontiguousarray(xl, dtype=np.float32)
```

```python
import math
from contextlib import ExitStack
import numpy as np
import ml_dtypes
import concourse.bass as bass
import concourse.mybir as mybir
from concourse.bass_utils import run_bass_kernel_spmd

F32 = mybir.dt.float32
BF16 = mybir.dt.bfloat16
I32 = mybir.dt.int32
U32 = mybir.dt.uint32
AF = mybir.ActivationFunctionType
ALU = mybir.AluOpType
AX = mybir.AxisListType

NCORES = 8
D = 1024
B = 2
S = 8192
L = 256
DEPTH = 4
GRID_W = 64
EPS = 1e-6


class Buf:
    __slots__ = ("name", "w", "r", "dsem")

    def __init__(self, name, dsem=None):
        self.name = name
        self.w = None
        self.r = []
        self.dsem = dsem


class DmaSem:
    def __init__(self, sched, name):
        self.sem = sched.nc.alloc_semaphore(name)
        self.key = ("dma", name)
        self.total = 0
        sched.sems[self.key] = self


class Sched:
    def __init__(self, nc):
        self.nc = nc
        self.eng = {"pe": nc.tensor, "dve": nc.vector, "act": nc.scalar,
                    "pool": nc.gpsimd, "sp": nc.sync}
        self.sems = {}
        self.esem = {}
        self.cnt = {}
        for k in self.eng:
            self.esem[k] = nc.alloc_semaphore("e_" + k)
            self.cnt[k] = 0
        self.seen = {}
        self.nbuf = 0
        self.out_tokens = []

    def buf(self, name=None, dsem=None):
        self.nbuf += 1
        return Buf(name or f"b{self.nbuf}", dsem)

    def dsem(self, name):
        return DmaSem(self, name)

    def _semof(self, key):
        if key[0] == "dma":
            return self.sems[key].sem
        return self.esem[key[0]]

    def _wait(self, engname, deps):
        e = self.eng[engname]
        for key, val in deps.items():
            if key[0] == "dma":
                val = max(val, 0)
            if self.seen.get((engname, key), 0) >= val:
                continue
            self.seen[(engname, key)] = val
            e.wait_ge(self._semof(key), val)

    def _deps(self, R, W):
        deps = {}

        def add(tok):
            if tok is None:
                return
            key, val = tok
            if key[0] == "dma":
                val = self.sems[key].total
            if deps.get(key, 0) < val:
                deps[key] = val
        for b in R:
            add(b.w)
        for b in W:
            add(b.w)
            for t in b.r:
                add(t)
        return deps

    def _commit(self, tok, R, W):
        for b in R:
            b.r.append(tok)
        for b in W:
            b.w = tok
            b.r = []

    def op(self, engname, fn, R=(), W=()):
        deps = self._deps(R, W)
        if engname == "pe":
            deps.pop(("pe",), None)
        self._wait(engname, deps)
        ins = fn(self.eng[engname])
        self.cnt[engname] += 1
        ins.then_inc(self.esem[engname], 1)
        tok = ((engname,), self.cnt[engname])
        self._commit(tok, R, W)
        return tok

    def dma(self, q, out, in_, R=(), W=(), sem=None, **kw):
        deps = self._deps(R, W)
        self._wait(q, deps)
        ds = sem
        if ds is None:
            for b in list(W) + list(R):
                if b.dsem is not None:
                    ds = b.dsem
                    break
        assert ds is not None, "dma needs a DmaSem"
        ins = self.eng[q].dma_start(out=out, in_=in_, **kw)
        ds.total += 16
        ins.then_inc(ds.sem, 16)
        tok = (ds.key, ds.total)
        self._commit(tok, R, W)
        return tok

    def barrier(self, bufs=()):
        deps = {}
        for k in self.eng:
            if self.cnt[k]:
                deps[(k,)] = self.cnt[k]
        for key, ds in self.sems.items():
            if ds.total:
                deps[key] = ds.total
        for k in self.eng:
            d = {kk: v for kk, v in deps.items() if kk != (k,)}
            self._wait(k, d)

    def coll(self, kind, ins, outs, R=(), W=(), groups=None):
        deps = self._deps(R, W)
        self._wait("pool", deps)
        ds = None
        for b in list(W) + list(R):
            if b.dsem is not None:
                ds = b.dsem
                break
        g = groups or [[0, 1, 2, 3], [4, 5, 6, 7]]
        ins_ = self.nc.gpsimd.collective_compute(kind, ALU.bypass, replica_groups=g, ins=ins, outs=outs)
        ds.total += 16
        ins_.then_inc(ds.sem, 16)
        tok = (ds.key, ds.total)
        self._commit(tok, R, W)
        return tok

    def finish(self, bufs, engname="sp"):
        deps = {}
        for b in bufs:
            for tok in ([b.w] if b.w else []) + b.r:
                key, val = tok
                if key[0] == "dma":
                    val = self.sems[key].total
                deps[key] = max(deps.get(key, 0), val)
        self._wait(engname, deps)


def mm(s, out, lhsT, rhs, start, stop, R, W):
    return s.op("pe", lambda e: e.matmul(out, lhsT, rhs, start=start, stop=stop), R=R, W=W)


def build_p0():
    nc = bass.Bass("TRN2", target_bir_lowering=False)
    NCOL = 3072
    NJ = NCOL // 128
    cT = nc.dram_tensor("cT", [128, 8, 4], F32, kind="ExternalInput").ap()
    w = nc.dram_tensor("w", [D, NCOL], F32, kind="ExternalInput").ap()
    bvec = nc.dram_tensor("bvec", [128, NJ], F32, kind="ExternalInput").ap()
    out = nc.dram_tensor("out", [128, NJ, 4], F32, kind="ExternalOutput").ap()
    s = Sched(nc)
    wv = w.rearrange("(kc p) n -> p kc n", p=128)
    with (nc.sbuf_tensor("w_sb", [128, 8, NCOL], F32) as w_sb,
          nc.sbuf_tensor("c_sb", [128, 8, 4], F32) as c_sb,
          nc.sbuf_tensor("s_sb", [128, 8, 4], F32) as s_sb,
          nc.sbuf_tensor("b_sb", [128, NJ], F32) as b_sb,
          nc.sbuf_tensor("r_sb", [128, NJ, 4], F32) as r_sb,
          nc.psum_tensor("ps", [128, 8, 512], F32) as ps):
        bw = [s.buf(f"w{k}", s.dsem(f"w{k}")) for k in range(8)]
        bc = s.buf("c", s.dsem("c"))
        bb = s.buf("b", bc.dsem)
        bs = s.buf("s")
        br = s.buf("r", s.dsem("r"))
        bps = [s.buf(f"ps{i}") for i in range(8)]
        s.dma("sp", c_sb[:], cT, W=[bc])
        s.dma("sp", b_sb[:], bvec, W=[bb])
        for k in range(8):
            s.dma("sp" if k % 2 == 0 else "act", w_sb[:, k, :], wv[:, k, :], W=[bw[k]])
        s.op("act", lambda e: e.activation(out=s_sb[:], in_=c_sb[:], func=AF.Silu), R=[bc], W=[bs])
        for j in range(NJ):
            pb = bps[j % 8]
            for k in range(8):
                mm(s, ps[:, j % 8, 0:4], w_sb[:, k, j * 128:(j + 1) * 128], s_sb[:, k, :],
                   k == 0, k == 7, R=[bw[k], bs], W=[pb])
            s.op("dve", lambda e: e.tensor_scalar(out=r_sb[:, j, :], in0=ps[:, j % 8, 0:4],
                                                  scalar1=b_sb[:, j:j + 1], scalar2=None, op0=ALU.add),
                 R=[pb, bb], W=[br])
        s.dma("sp", out, r_sb[:], R=[br])
        s.finish([br])
    return nc


def silu_np_layout_c(c, c_ctx):
    cc = np.stack([c[0], c[1], c_ctx, c_ctx], axis=1)
    return np.ascontiguousarray(cc.reshape(8, 128, 4).transpose(1, 0, 2))


def run_p0(c, c_ctx, w_ada, b_ada):
    nc = build_p0()
    cT = silu_np_layout_c(c, c_ctx)
    in_maps = []
    for i in range(NCORES):
        l, h = i // 2, i % 2
        in_maps.append({
            "cT": cT,
            "w": np.ascontiguousarray(w_ada[l][:, h * 3072:(h + 1) * 3072]),
            "bvec": np.ascontiguousarray(b_ada[l][h * 3072:(h + 1) * 3072].reshape(24, 128).T),
        })
    res = run_bass_kernel_spmd(nc, in_maps, core_ids=list(range(NCORES)))
    mods = np.zeros((DEPTH, 3, 6 * D), np.float32)
    for i in range(NCORES):
        l, h = i // 2, i % 2
        o = res.results[i]["out"]
        m = o.transpose(1, 0, 2).reshape(3072, 4)
        mods[l, :, h * 3072:(h + 1) * 3072] = m[:, :3].T
    return mods


NT1 = 2112
NW1 = 3072
BLKS1 = [(0, 512), (512, 512), (1024, 512), (1536, 512), (2048, 64)]


def build_p1():
    nc = bass.Bass("TRN2", target_bir_lowering=False)
    xT = nc.dram_tensor("xT", [128, 8, NT1], F32, kind="ExternalInput").ap()
    w = nc.dram_tensor("w", [D, NW1], F32, kind="ExternalInput").ap()
    gsc = nc.dram_tensor("gsc", [128, 5, 8], F32, kind="ExternalInput").ap()
    cosT = nc.dram_tensor("cosT", [128, 2048], F32, kind="ExternalInput").ap()
    sinT = nc.dram_tensor("sinT", [128, 2048], F32, kind="ExternalInput").ap()
    fmb = nc.dram_tensor("fmb", [128, 8, NT1], BF16, kind="ExternalOutput").ap()
    fmf = nc.dram_tensor("fmf", [128, 8, NT1], F32, kind="ExternalOutput").ap()
    tm = nc.dram_tensor("tm", [NT1, 512], BF16, kind="ExternalOutput").ap()
    s = Sched(nc)
    wv = w.rearrange("(kc p) n -> p kc n", p=128)
    with (nc.sbuf_tensor("x_sb", [128, 2, 8, 512], F32) as x_sb,
          nc.sbuf_tensor("h_sb", [128, 8, NT1], BF16) as h_sb,
          nc.sbuf_tensor("w_bf", [128, 8, NW1], BF16) as w_bf,
          nc.sbuf_tensor("w_st", [128, 2, NW1], F32) as w_st,
          nc.sbuf_tensor("o_sb", [128, 4, 512], F32) as o_sb,
          nc.sbuf_tensor("ob_sb", [128, 4, 512], BF16) as ob_sb,
          nc.sbuf_tensor("cos_sb", [128, 2048], F32) as cos_sb,
          nc.sbuf_tensor("sin_sb", [128, 2048], F32) as sin_sb,
          nc.sbuf_tensor("gsc_sb", [128, 5, 8], F32) as gsc_sb,
          nc.sbuf_tensor("ab_sb", [128, 2, 8], F32) as ab_sb,
          nc.sbuf_tensor("sq_sb", [128, 8, 512], BF16) as sq_sb,
          nc.sbuf_tensor("ones_sb", [128, 128], BF16) as ones_sb,
          nc.sbuf_tensor("rstd_sb", [128, 2, 512], F32) as rstd_sb,
          nc.sbuf_tensor("tmp_sb", [128, 4, 512], F32) as tmp_sb,
          nc.psum_tensor("ps", [128, 8, 512], F32) as ps):
        bx = [s.buf(f"x{i}", s.dsem(f"x{i}")) for i in range(2)]
        bh = [s.buf(f"h{i}") for i in range(len(BLKS1))]
        bwst = [s.buf(f"wst{i}", s.dsem(f"wst{i}")) for i in range(2)]
        bwbf = [s.buf(f"wbf{k}") for k in range(8)]
        bo = [s.buf(f"o{i}", s.dsem(f"o{i}")) for i in range(4)]
        bob = [s.buf(f"ob{i}", s.dsem(f"ob{i}")) for i in range(4)]
        cs = s.dsem("const")
        bcos = s.buf("cos", cs); bsin = s.buf("sin", cs); bgsc = s.buf("gsc", cs)
        bab = s.buf("ab"); bsq = s.buf("sq"); bones = s.buf("ones")
        brs = [s.buf(f"rs{i}") for i in range(2)]
        btmp = [s.buf(f"tmp{i}") for i in range(4)]
        bps = [s.buf(f"ps{i}") for i in range(8)]

        s.dma("sp", gsc_sb[:], gsc, W=[bgsc])
        s.dma("sp", cos_sb[:], cosT, W=[bcos])
        s.dma("sp", sin_sb[:], sinT, W=[bsin])
        s.op("pool", lambda e: e.memset(ones_sb[:], 1.0), W=[bones])
        for t in range(2):
            s.op("dve", lambda e: e.scalar_tensor_tensor(
                out=ab_sb[:, t, :], in0=gsc_sb[:, 1 + 2 * t, :], scalar=1.0, in1=gsc_sb[:, 0, :],
                op0=ALU.add, op1=ALU.mult), R=[bgsc], W=[bab])
        for k in range(8):
            sl = k % 2
            s.dma("act", w_st[:, sl, :], wv[:, k, :], W=[bwst[sl]])
            s.op("pool", lambda e: e.tensor_copy(out=w_bf[:, k, :], in_=w_st[:, sl, :]),
                 R=[bwst[sl]], W=[bwbf[k]])
        for bi, (t0, n) in enumerate(BLKS1):
            sl = bi % 2
            isctx = bi == 4
            s.dma("sp", x_sb[:, sl, :, 0:n], xT[:, :, t0:t0 + n], W=[bx[sl]])
            s.op("act", lambda e: e.activation(out=sq_sb[:, :, 0:n], in_=x_sb[:, sl, :, 0:n], func=AF.Square),
                 R=[bx[sl]], W=[bsq])
            pst = bps[sl]
            for k in range(8):
                mm(s, ps[:, sl, 0:n], ones_sb[:], sq_sb[:, k, 0:n], k == 0, k == 7, R=[bones, bsq], W=[pst])
            s.op("act", lambda e: e.activation(out=rstd_sb[:, sl, 0:n], in_=ps[:, sl, 0:n], func=AF.Sqrt,
                                               scale=1.0 / D, bias=EPS), R=[pst], W=[brs[sl]])
            s.op("dve", lambda e: e.reciprocal(out=rstd_sb[:, sl, 0:n], in_=rstd_sb[:, sl, 0:n]),
                 R=[brs[sl]], W=[brs[sl]])
            ai = 1 if isctx else 0
            shrow = 4 if isctx else 2
            for k in range(8):
                tb = k % 4
                s.op("dve", lambda e: e.scalar_tensor_tensor(
                    out=tmp_sb[:, tb, 0:n], in0=x_sb[:, sl, k, 0:n], scalar=ab_sb[:, ai, k:k + 1],
                    in1=rstd_sb[:, sl, 0:n], op0=ALU.mult, op1=ALU.mult),
                    R=[bx[sl], bab, brs[sl]], W=[btmp[tb]])
                s.op("act", lambda e: e.activation(out=h_sb[:, k, t0:t0 + n], in_=tmp_sb[:, tb, 0:n],
                                                   func=AF.Identity, bias=gsc_sb[:, shrow, k:k + 1], scale=1.0),
                     R=[btmp[tb], bgsc], W=[bh[bi]])
        oi = 0
        obi = 0
        pi = 0
        for bi, (t0, n) in enumerate(BLKS1):
            isctx = bi == 4
            for c in range(16):
                rope = (c >= 12) and not isctx
                isb = c < 4 or c >= 12
                dst = (fmb[:, c if c < 4 else c - 8, t0:t0 + n]) if isb else fmf[:, c - 4, t0:t0 + n]
                pA = 2 + (pi % 6); pi += 1
                for k in range(8):
                    mm(s, ps[:, pA, 0:n], w_bf[:, k, c * 128:(c + 1) * 128], h_sb[:, k, t0:t0 + n],
                       k == 0, k == 7, R=[bwbf[k], bh[bi]], W=[bps[pA]])
                if isb:
                    ob = obi % 4; obi += 1
                    osl = ob_sb[:, ob, 0:n]; obuf = bob[ob]
                else:
                    ob = oi % 4; oi += 1
                    osl = o_sb[:, ob, 0:n]; obuf = bo[ob]
                if not rope:
                    if c % 2 == 0:
                        s.op("act", lambda e: e.copy(out=osl, in_=ps[:, pA, 0:n]), R=[bps[pA]], W=[obuf])
                    else:
                        s.op("dve", lambda e: e.tensor_copy(out=osl, in_=ps[:, pA, 0:n]), R=[bps[pA]], W=[obuf])
                else:
                    pB = 2 + (pi % 6); pi += 1
                    c2 = c + 4
                    for k in range(8):
                        mm(s, ps[:, pB, 0:n], w_bf[:, k, c2 * 128:(c2 + 1) * 128], h_sb[:, k, t0:t0 + n],
                           k == 0, k == 7, R=[bwbf[k], bh[bi]], W=[bps[pB]])
                    s.op("dve", lambda e: e.tensor_tensor(out=tmp_sb[:, 0, 0:n], in0=ps[:, pA, 0:n],
                                                          in1=cos_sb[:, t0:t0 + n], op=ALU.mult),
                         R=[bps[pA], bcos], W=[btmp[0]])
                    s.op("dve", lambda e: e.tensor_tensor(out=tmp_sb[:, 1, 0:n], in0=ps[:, pB, 0:n],
                                                          in1=sin_sb[:, t0:t0 + n], op=ALU.mult),
                         R=[bps[pB], bsin], W=[btmp[1]])
                    s.op("pool", lambda e: e.tensor_tensor(out=osl, in0=tmp_sb[:, 0, 0:n],
                                                           in1=tmp_sb[:, 1, 0:n], op=ALU.add),
                         R=[btmp[0], btmp[1]], W=[obuf])
                s.dma("sp", dst, osl, R=[obuf])
        ntile = NT1 // 128 + 1
        for ti in range(ntile):
            t0 = ti * 128
            n = min(128, NT1 - t0)
            bi = min(t0 // 512, 4)
            pA = 2 + (pi % 6); pi += 1
            for k in range(8):
                mm(s, ps[0:n, pA, :], h_sb[:, k, t0:t0 + n], w_bf[:, k, 2560:3072],
                   k == 0, k == 7, R=[bwbf[k], bh[bi]], W=[bps[pA]])
            ob = obi % 4; obi += 1
            s.op("act", lambda e: e.copy(out=ob_sb[0:n, ob, :], in_=ps[0:n, pA, :]), R=[bps[pA]], W=[bob[ob]])
            s.dma("sp", tm[t0:t0 + n, :], ob_sb[0:n, ob, :], R=[bob[ob]])
        s.finish(bo + bob)
    return nc


def rope_tables():
    t = np.arange(S)
    row = (t // GRID_W).astype(np.float32)
    col = (t % GRID_W).astype(np.float32)
    inv = (10000.0 ** (-np.arange(0, 16, 2, dtype=np.float32) / 16.0)).astype(np.float32)
    ang_r = row[:, None] * inv
    ang_c = col[:, None] * inv
    cosT = np.zeros((32, S), np.float32)
    sinT = np.zeros((32, S), np.float32)
    for d in range(32):
        ang = ang_r if d < 16 else ang_c
        i = d % 8
        cosT[d] = np.cos(ang[:, i])
        sgn = -1.0 if (d % 16) < 8 else 1.0
        sinT[d] = sgn * np.sin(ang[:, i])
    return np.tile(cosT, (4, 1)), np.tile(sinT, (4, 1))


def p1_wcols():
    sw = np.array([(d + 8) if (d % 16) < 8 else (d - 8) for d in range(32)])
    f = np.arange(256)
    swf = (f // 32) * 32 + sw[f % 32]
    cols = np.concatenate([np.arange(0, 256), np.arange(256, 512), np.arange(768, 1280), np.arange(1280, 1792),
                           np.arange(1792, 2048), np.arange(2048, 2304), 1792 + swf, 2048 + swf,
                           np.arange(512, 768), np.arange(2304, 2560)])
    return cols


def chunkT(a):
    T = a.shape[0]
    return np.ascontiguousarray(a.T.reshape(8, 128, T).transpose(1, 0, 2))


def vec_pk(v):
    return np.ascontiguousarray(v.reshape(8, 128).T)


def run_p1(nc1, xl, xc, mods_l, g_mix_l, w_in_l, cosT, sinT):
    wl = np.ascontiguousarray(w_in_l[:, p1_wcols()])
    in_maps = []
    for i in range(NCORES):
        b, j = i // 4, i % 4
        xx = np.concatenate([xl[b, 2048 * j:2048 * (j + 1)], xc[b, 64 * j:64 * (j + 1)]], axis=0)
        gsc = np.stack([vec_pk(g_mix_l), vec_pk(mods_l[b, 1024:2048]), vec_pk(mods_l[b, 0:1024]),
                        vec_pk(mods_l[2, 1024:2048]), vec_pk(mods_l[2, 0:1024])], axis=1)
        in_maps.append({"xT": chunkT(xx), "w": wl, "gsc": np.ascontiguousarray(gsc),
                        "cosT": np.ascontiguousarray(cosT[:, 2048 * j:2048 * (j + 1)]),
                        "sinT": np.ascontiguousarray(sinT[:, 2048 * j:2048 * (j + 1)])})
    res = run_bass_kernel_spmd(nc1, in_maps, core_ids=list(range(NCORES)))
    fmb = np.zeros((B, 1024, S + L), ml_dtypes.bfloat16)
    fmf = np.zeros((B, 1024, S + L), np.float32)
    tm = np.zeros((B, S + L, 512), ml_dtypes.bfloat16)
    for i in range(NCORES):
        b, j = i // 4, i % 4
        r = res.results[i]
        for dst, key in ((fmb, "fmb"), (fmf, "fmf")):
            f = r[key].transpose(1, 0, 2).reshape(1024, NT1)
            dst[b, :, 2048 * j:2048 * (j + 1)] = f[:, :2048]
            dst[b, :, S + 64 * j:S + 64 * (j + 1)] = f[:, 2048:]
        t = r["tm"]
        tm[b, 2048 * j:2048 * (j + 1)] = t[:2048]
        tm[b, S + 64 * j:S + 64 * (j + 1)] = t[2048:]
    return fmb, fmf, tm


NTOK = S + L
NKT = NTOK // 128
NEB = 21


def na_tile_lists():
    out = []
    for m in range(64):
        if 2 <= m <= 61:
            out.append(([m - 2, m - 1, m, m + 1, m + 2], 0))
        elif m < 2:
            out.append(([0, 1, 2, 3], 5 + 4 * m))
        else:
            out.append(([60, 61, 62, 63], 5 + 4 * (m - 60)))
    return out


def na_bias_index():
    MASKED = 15 * 31
    idx = np.full((NEB, 128, 128), MASKED, np.int64)
    lists = na_tile_lists()
    reps = {0: 10}
    qq = np.arange(128); kk = np.arange(128)

    def fill(e0, m, kts):
        for ii, n in enumerate(kts):
            qr = 2 * m + qq // 64; qc = qq % 64
            kr = 2 * n + kk // 64; kc = kk % 64
            r0 = np.clip(qr - 4, 0, 120)
            cs = np.clip(qc - 8, 0, 48)
            valid = ((kr[:, None] >= r0[None, :]) & (kr[:, None] < r0[None, :] + 8) &
                     (kc[:, None] >= cs[None, :]) & (kc[:, None] < cs[None, :] + 16))
            dr = kr[:, None] - qr[None, :]
            dc = np.clip(kc[:, None] - qc[None, :], -15, 15)
            v = (np.clip(dr, -7, 7) + 7) * 31 + dc + 15
            idx[e0 + ii] = np.where(valid, v, MASKED)
    fill(0, 10, lists[10][0])
    for m in (0, 1, 62, 63):
        fill(lists[m][1], m, lists[m][0])
    return idx


def build_p2(lam_init):
    nc = bass.Bass("TRN2", target_bir_lowering=False)
    din = lambda n, shp, dt: nc.dram_tensor(n, shp, dt, kind="ExternalInput").ap()
    dout = lambda n, shp, dt: nc.dram_tensor(n, shp, dt, kind="ExternalOutput").ap()
    xrg = din("xrg", [2, 128, NTOK], F32)
    wbd = din("wbd", [4, 128, 128], F32)
    rgv = din("rgv", [128, 2, 8], F32)
    hout = dout("hout", [2, 128, NTOK], F32)
    qaT = din("qaT", [64, NTOK], BF16)
    kaT = din("kaT", [64, NTOK], BF16)
    vaP = din("vaP", [128, NKT, 64], BF16)
    btT = din("btT", [128, NEB, 128], F32)
    ynaP = dout("ynaP", [128, NKT, 64], BF16)
    qdT = din("qdT", [2, 32, NTOK], BF16)
    kdT = din("kdT", [2, 32, NTOK], BF16)
    vdP = din("vdP", [128, NKT, 64], BF16)
    dlam = din("dlam", [128, 128], F32)
    dg = din("dg", [128, 64], F32)
    ydfP = dout("ydfP", [128, NKT, 64], BF16)
    s = Sched(nc)
    with nc.psum_tensor("ps", [128, 8, 512], F32) as ps:
        bps = [s.buf(f"ps{i}") for i in range(8)]

        CH = 2048
        with (nc.sbuf_tensor("x_sb", [128, NTOK], F32) as x_sb,
              nc.sbuf_tensor("wst_sb", [128, 4, 128], F32) as wst_sb,
              nc.sbuf_tensor("wbf_sb", [128, 4, 128], BF16) as wbf_sb,
              nc.sbuf_tensor("rgv_sb", [128, 2, 8], F32) as rgv_sb,
              nc.sbuf_tensor("cneg_sb", [128, 2], F32) as cneg_sb,
              nc.sbuf_tensor("xcv_sb", [128, CH], F32) as xcv_sb,
              nc.sbuf_tensor("xcb_sb", [128, CH], BF16) as xcb_sb,
              nc.sbuf_tensor("r_sb", [128, CH], F32) as r_sb,
              nc.sbuf_tensor("i_sb", [128, CH], F32) as i_sb,
              nc.sbuf_tensor("a_sb", [128, CH], F32) as a_sb,
              nc.sbuf_tensor("q_sb", [128, CH], F32) as q_sb,
              nc.sbuf_tensor("h_sb", [128, 2, CH], F32) as h_sb,
              nc.sbuf_tensor("carry_sb", [128, 1], F32) as carry_sb):
            bx = s.buf("x", s.dsem("rgx"))
            bw = s.buf("w", s.dsem("rgw"))
            bwb = s.buf("wb")
            bv = s.buf("v", bw.dsem)
            bcn = s.buf("cneg")
            bxcv = s.buf("xcv"); bxcb = s.buf("xcb"); br = s.buf("r"); bi_ = s.buf("i")
            ba = s.buf("a"); bq = s.buf("q"); bcar = s.buf("carry")
            bh = [s.buf(f"h{i}", s.dsem(f"rgh{i}")) for i in range(2)]
            s.dma("act", wst_sb[:], wbd.rearrange("f c d -> c f d"), W=[bw])
            s.dma("act", rgv_sb[:], rgv, W=[bv])
            s.op("pool", lambda e: e.tensor_copy(out=wbf_sb[:], in_=wst_sb[:]), R=[bw], W=[bwb])
            s.op("act", lambda e: e.activation(out=cneg_sb[:], in_=rgv_sb[:, :, 7], func=AF.Exp, scale=-1.0),
                 R=[bv], W=[bcn])
            s.op("act", lambda e: e.activation(out=cneg_sb[:], in_=cneg_sb[:], func=AF.Ln, bias=1.0, scale=1.0),
                 R=[bcn], W=[bcn])
            s.op("dve", lambda e: e.tensor_scalar(out=cneg_sb[:], in0=cneg_sb[:], scalar1=-8.0, scalar2=None,
                                                  op0=ALU.mult), R=[bcn], W=[bcn])
            hi = 0
            for dr in range(2):
                offs = [-2, -1, 0, 1] if dr == 0 else [2, 1, 0, -1]
                s.dma("sp", x_sb[:, 0:4224], xrg[dr, :, 0:4224], W=[bx])
                s.dma("sp", x_sb[:, 4224:NTOK], xrg[dr, :, 4224:NTOK], W=[bx])
                chunks = [(0, 256, 0, 256)] + [(256 + CH * i, CH, 256, NTOK) for i in range(4)]
                for ci, (c0, n, s0, s1) in enumerate(chunks):
                    s.op("dve", lambda e: e.tensor_scalar(
                        out=xcv_sb[:, 0:n], in0=x_sb[:, c0:c0 + n], scalar1=rgv_sb[:, dr, 2:3],
                        scalar2=rgv_sb[:, dr, 4:5], op0=ALU.mult, op1=ALU.add), R=[bx, bv], W=[bxcv])
                    for jt in (0, 1, 3):
                        o = offs[jt]
                        lo = max(c0, s0 - o); hi_ = min(c0 + n, s1 - o)
                        s.op("dve", lambda e: e.scalar_tensor_tensor(
                            out=xcv_sb[:, lo - c0:hi_ - c0], in0=x_sb[:, lo + o:hi_ + o],
                            scalar=rgv_sb[:, dr, jt:jt + 1], in1=xcv_sb[:, lo - c0:hi_ - c0],
                            op0=ALU.mult, op1=ALU.add), R=[bx, bv, bxcv], W=[bxcv])
                    s.op("pool", lambda e: e.tensor_copy(out=xcb_sb[:, 0:n], in_=xcv_sb[:, 0:n]), R=[bxcv], W=[bxcb])
                    nsb = (n + 511) // 512
                    for sb in range(nsb):
                        w_ = min(512, n - sb * 512)
                        mm(s, ps[:, sb, 0:w_], wbf_sb[:, 2 * dr, :], xcb_sb[:, sb * 512:sb * 512 + w_], True, True,
                           R=[bwb, bxcb], W=[bps[sb]])
                        mm(s, ps[:, 4 + sb, 0:w_], wbf_sb[:, 2 * dr + 1, :], xcb_sb[:, sb * 512:sb * 512 + w_], True, True,
                           R=[bwb, bxcb], W=[bps[4 + sb]])
                    if n == CH:
                        rin = ps[:, 0:4, :]; iin = ps[:, 4:8, :]
                        rout = r_sb[:, 0:n].rearrange("p (a b) -> p a b", b=512)
                        iout = i_sb[:, 0:n].rearrange("p (a b) -> p a b", b=512)
                    else:
                        rin = ps[:, 0, 0:n]; iin = ps[:, 4, 0:n]
                        rout = r_sb[:, 0:n]; iout = i_sb[:, 0:n]
                    s.op("act", lambda e: e.activation(out=rout, in_=rin, func=AF.Sigmoid,
                                                       bias=rgv_sb[:, dr, 5:6], scale=1.0),
                         R=bps[0:4] + [bv], W=[br])
                    s.op("act", lambda e: e.activation(out=iout, in_=iin, func=AF.Sigmoid,
                                                       bias=rgv_sb[:, dr, 6:7], scale=1.0),
                         R=bps[4:8] + [bv], W=[bi_])
                    s.op("act", lambda e: e.activation(out=a_sb[:, 0:n], in_=r_sb[:, 0:n], func=AF.Exp,
                                                       scale=cneg_sb[:, dr:dr + 1]), R=[br, bcn], W=[ba])
                    s.op("pool", lambda e: e.tensor_tensor(out=q_sb[:, 0:n], in0=a_sb[:, 0:n], in1=a_sb[:, 0:n],
                                                           op=ALU.mult), R=[ba], W=[bq])
                    s.op("act", lambda e: e.activation(out=q_sb[:, 0:n], in_=q_sb[:, 0:n], func=AF.Sqrt,
                                                       scale=-1.0, bias=1.0), R=[bq], W=[bq])
                    s.op("pool", lambda e: e.tensor_tensor(out=i_sb[:, 0:n], in0=i_sb[:, 0:n], in1=xcv_sb[:, 0:n],
                                                           op=ALU.mult), R=[bi_, bxcv], W=[bi_])
                    s.op("pool", lambda e: e.tensor_tensor(out=q_sb[:, 0:n], in0=q_sb[:, 0:n], in1=i_sb[:, 0:n],
                                                           op=ALU.mult), R=[bq, bi_], W=[bq])
                    hs = hi % 2; hi += 1
                    init = 0.0 if ci == 0 else carry_sb[:, 0:1]
                    s.op("dve", lambda e: e.tensor_tensor_scan(out=h_sb[:, hs, 0:n], data0=a_sb[:, 0:n],
                                                               data1=q_sb[:, 0:n], initial=init,
                                                               op0=ALU.mult, op1=ALU.add),
                         R=[ba, bq] + ([bcar] if ci else []), W=[bh[hs]])
                    s.op("dve", lambda e: e.tensor_copy(out=carry_sb[:, 0:1], in_=h_sb[:, hs, n - 1:n]),
                         R=[bh[hs]], W=[bcar])
                    s.dma("sp", hout[dr, :, c0:c0 + n], h_sb[:, hs, 0:n], R=[bh[hs]])
            s.barrier(bh)

        with (nc.sbuf_tensor("na_q_sb", [64, NTOK], BF16) as q_sb,
              nc.sbuf_tensor("na_k_sb", [64, NTOK], BF16) as k_sb,
              nc.sbuf_tensor("na_v_sb", [128, NKT, 65], BF16) as v_sb,
              nc.sbuf_tensor("na_bt_sb", [128, NEB * 128], F32) as bt_sb,
              nc.sbuf_tensor("na_eb_sb", [128, NEB * 128], BF16) as eb_sb,
              nc.sbuf_tensor("na_e_sb", [128, 2, 640], F32) as e_sb,
              nc.sbuf_tensor("na_p_sb", [128, 2, 896], BF16) as p_sb,
              nc.sbuf_tensor("na_y_sb", [128, NKT, 64], BF16) as y_sb,
              nc.sbuf_tensor("na_rec_sb", [128, 2], F32) as rec_sb):
            ld = s.dsem("nald")
            bq = s.buf("q", ld); bk = s.buf("k", ld); bv = s.buf("v", ld); bbt = s.buf("bt", ld)
            beb = s.buf("eb")
            be = [s.buf(f"e{i}") for i in range(2)]
            bp = [s.buf(f"p{i}") for i in range(2)]
            brec = [s.buf(f"rec{i}") for i in range(2)]
            by = s.buf("y", s.dsem("nay"))
            s.dma("sp", q_sb[:], qaT, W=[bq])
            s.dma("act", k_sb[:], kaT, W=[bk])
            s.dma("sp", v_sb[:, :, 0:64], vaP, W=[bv])
            s.dma("act", bt_sb[:], btT.rearrange("p e q -> p (e q)"), W=[bbt])
            s.op("pool", lambda e: e.memset(v_sb[:, :, 64:65], 1.0), W=[bv])
            s.op("act", lambda e: e.activation(out=eb_sb[:], in_=bt_sb[:], func=AF.Exp), R=[bbt], W=[beb])
            lists = na_tile_lists()
            for m in range(NKT):
                sl = m % 2
                if m < 64:
                    kts, eb0 = lists[m]
                else:
                    kts, eb0 = [], 0
                nl = len(kts)
                bA = bps[2 * sl]; bB = bps[2 * sl + 1]; bAcc = bps[4 + sl]
                qs = q_sb[:, m * 128:(m + 1) * 128]
                for ii, n in enumerate(kts):
                    bank = 2 * sl + (0 if ii < 4 else 1)
                    col = (ii % 4) * 128
                    mm(s, ps[:, bank, col:col + 128], k_sb[:, n * 128:(n + 1) * 128], qs, True, True,
                       R=[bk, bq], W=[bps[bank]])
                for ci in range(2):
                    n = 64 + ci
                    mm(s, ps[:, 2 * sl + 1, 128 + ci * 128:256 + ci * 128], k_sb[:, n * 128:(n + 1) * 128], qs,
                       True, True, R=[bk, bq], W=[bB])
                if nl:
                    na_ = min(nl, 4) * 128
                    s.op("act", lambda e: e.activation(out=e_sb[:, sl, 0:na_], in_=ps[:, 2 * sl, 0:na_], func=AF.Exp,
                                                       scale=0.125), R=[bA], W=[be[sl]])
                    if nl == 5:
                        s.op("act", lambda e: e.activation(out=e_sb[:, sl, 512:640], in_=ps[:, 2 * sl + 1, 0:128],
                                                           func=AF.Exp, scale=0.125), R=[bB], W=[be[sl]])
                s.op("act", lambda e: e.activation(out=p_sb[:, sl, 640:896], in_=ps[:, 2 * sl + 1, 128:384],
                                                   func=AF.Exp, scale=0.125), R=[bB], W=[bp[sl]])
                if nl:
                    s.op("dve", lambda e: e.tensor_tensor(out=p_sb[:, sl, 0:nl * 128], in0=e_sb[:, sl, 0:nl * 128],
                                                          in1=eb_sb[:, eb0 * 128:(eb0 + nl) * 128], op=ALU.mult),
                         R=[be[sl], beb], W=[bp[sl]])
                tiles = [(ii * 128, n) for ii, n in enumerate(kts)] + [(640, 64), (768, 65)]
                for ti, (pc, n) in enumerate(tiles):
                    mm(s, ps[:, 4 + sl, 0:65], p_sb[:, sl, pc:pc + 128], v_sb[:, n, :], ti == 0, ti == len(tiles) - 1,
                       R=[bp[sl], bv], W=[bAcc])
                s.op("dve", lambda e: e.reciprocal(out=rec_sb[:, sl:sl + 1], in_=ps[:, 4 + sl, 64:65]),
                     R=[bAcc], W=[brec[sl]])
                s.op("dve", lambda e: e.tensor_scalar(out=y_sb[:, m, :], in0=ps[:, 4 + sl, 0:64],
                                                      scalar1=rec_sb[:, sl:sl + 1], scalar2=None, op0=ALU.mult),
                     R=[bAcc, brec[sl]], W=[by])
            s.dma("sp", ynaP, y_sb[:], R=[by])
            s.barrier([by])

        with (nc.sbuf_tensor("df_q4_sb", [96, NTOK], BF16) as q4_sb,
              nc.sbuf_tensor("df_k4_sb", [96, NTOK], BF16) as k4_sb,
              nc.sbuf_tensor("df_q4b_sb", [96, NTOK], BF16) as q4b_sb,
              nc.sbuf_tensor("df_k4b_sb", [96, NTOK], BF16) as k4b_sb,
              nc.sbuf_tensor("df_v_sb", [128, NKT, 65], BF16) as v_sb,
              nc.sbuf_tensor("df_p_sb", [128, 8, 512], BF16) as p_sb,
              nc.sbuf_tensor("df_y_sb", [128, NKT, 64], BF16) as y_sb,
              nc.sbuf_tensor("df_dl_sb", [128, 128], F32) as dl_sb,
              nc.sbuf_tensor("df_g_sb", [128, 64], F32) as g_sb,
              nc.sbuf_tensor("df_lam_sb", [128, 4], F32) as lam_sb,
              nc.sbuf_tensor("df_r_sb", [128, 2, 2, 4], F32) as r_sb,
              nc.sbuf_tensor("df_ss_sb", [128, 2, 4], F32) as ss_sb,
              nc.sbuf_tensor("df_o_sb", [128, 2, 4, 64], F32) as o_sb,
              nc.sbuf_tensor("df_t_sb", [128, 2, 4, 64], F32) as t_sb,
              nc.sbuf_tensor("df_junk_sb", [128, 64], F32) as junk_sb):
            ld = s.dsem("dfld")
            bq = s.buf("q", ld); bk = s.buf("k", ld); bv = s.buf("v", ld); bdl = s.buf("dl", ld); bg = s.buf("g", ld)
            blam = s.buf("lam")
            bp = [s.buf(f"p{i}") for i in range(8)]
            by = s.buf("y", s.dsem("dfy"))
            br = [s.buf(f"r{i}") for i in range(2)]
            bss = [s.buf(f"ss{i}") for i in range(2)]
            bo = [s.buf(f"o{i}") for i in range(2)]
            bt = [s.buf(f"t{i}") for i in range(2)]
            bj = s.buf("junk")
            for (qt, kt_, order) in ((q4_sb, k4_sb, (0, 1, 0)), (q4b_sb, k4b_sb, (1, 0, 1))):
                for ri, cc in enumerate(order):
                    s.dma("sp", qt[32 * ri:32 * ri + 32, :], qdT[cc], W=[bq])
                    s.dma("act", kt_[32 * ri:32 * ri + 32, :], kdT[cc], W=[bk])
            s.dma("sp", v_sb[:, :, 0:64], vdP, W=[bv])
            s.dma("act", dl_sb[:], dlam, W=[bdl])
            s.dma("act", g_sb[:], dg, W=[bg])
            s.op("pool", lambda e: e.memset(v_sb[:, :, 64:65], 1.0), W=[bv])
            s.op("dve", lambda e: e.scalar_tensor_tensor(out=junk_sb[:, 0:32], in0=dl_sb[:, 0:32], scalar=1.0,
                                                         in1=dl_sb[:, 32:64], op0=ALU.mult, op1=ALU.mult,
                                                         accum_out=lam_sb[:, 0:1]), R=[bdl], W=[bj, blam])
            s.op("dve", lambda e: e.scalar_tensor_tensor(out=junk_sb[:, 0:32], in0=dl_sb[:, 64:96], scalar=1.0,
                                                         in1=dl_sb[:, 96:128], op0=ALU.mult, op1=ALU.mult,
                                                         accum_out=lam_sb[:, 1:2]), R=[bdl, bj], W=[bj, blam])
            s.op("act", lambda e: e.activation(out=lam_sb[:, 0:2], in_=lam_sb[:, 0:2], func=AF.Exp), R=[blam], W=[blam])
            s.op("dve", lambda e: e.tensor_tensor(out=lam_sb[:, 2:3], in0=lam_sb[:, 1:2], in1=lam_sb[:, 0:1],
                                                  op=ALU.subtract), R=[blam], W=[blam])
            s.op("dve", lambda e: e.tensor_scalar(out=lam_sb[:, 2:3], in0=lam_sb[:, 2:3], scalar1=-lam_init,
                                                  scalar2=None, op0=ALU.add), R=[blam], W=[blam])
            s.op("dve", lambda e: e.tensor_scalar(out=g_sb[:], in0=g_sb[:], scalar1=1.0 - lam_init, scalar2=None,
                                                  op0=ALU.mult), R=[bg], W=[bg])
            qblocks = [(512 * i, 512, list(range(NKT))) for i in range(16)] + [(S, 256, [64, 65])]
            sc = 32 ** -0.5
            gs_ = 0
            for qi, (q0, nq, kts) in enumerate(qblocks):
                par = qi % 2
                nsub = nq // 128
                steps = [(kt, c) for kt in kts for c in range(2)]
                ns = len(steps)
                groups = [list(range(i, min(i + 3, ns))) for i in range(0, ns, 3)]
                started = [False, False]
                for gi_ in range(len(groups) + 1):
                    if gi_ < len(groups):
                        grp = groups[gi_]
                        layB = steps[grp[0]][1] == 1
                        qt = q4b_sb if layB else q4_sb
                        kt_t = k4b_sb if layB else k4_sb
                        for ri, si in enumerate(grp):
                            kt, c = steps[si]
                            g = gs_ + si
                            mm(s, ps[:, g % 4, 0:nq], kt_t[32 * ri:32 * ri + 32, kt * 128:(kt + 1) * 128],
                               qt[32 * ri:32 * ri + 32, q0:q0 + nq], True, True, R=[bk, bq], W=[bps[g % 4]])
                        for ri, si in enumerate(grp):
                            g = gs_ + si
                            s.op("act", lambda e: e.activation(out=p_sb[:, g % 8, 0:nq], in_=ps[:, g % 4, 0:nq],
                                                               func=AF.Exp, scale=sc), R=[bps[g % 4]], W=[bp[g % 8]])
                    if gi_ >= 1:
                        for si in groups[gi_ - 1]:
                            kt, c = steps[si]
                            g = gs_ + si
                            bank = 4 + 2 * par + c
                            for sub in range(nsub):
                                st = not started[c]
                                started[c] = True
                                s.op("pe", lambda e: e.matmul(ps[:, bank, sub * 65:(sub + 1) * 65],
                                                              p_sb[:, g % 8, sub * 128:(sub + 1) * 128], v_sb[:, kt, :],
                                                              start=st, stop=(kt == kts[-1]), skip_group_check=True),
                                     R=[bp[g % 8], bv], W=[bps[bank]])
                gs_ += ns
                a0 = ps[:, 4 + 2 * par, 0:260].rearrange("p (s e) -> p s e", e=65)
                a1 = ps[:, 5 + 2 * par, 0:260].rearrange("p (s e) -> p s e", e=65)
                b0 = bps[4 + 2 * par]; b1 = bps[5 + 2 * par]
                s.op("dve", lambda e: e.reciprocal(out=r_sb[:, par, 0, 0:nsub], in_=a0[:, 0:nsub, 64]), R=[b0], W=[br[par]])
                s.op("dve", lambda e: e.reciprocal(out=r_sb[:, par, 1, 0:nsub], in_=a1[:, 0:nsub, 64]), R=[b1], W=[br[par]])
                s.op("dve", lambda e: e.tensor_scalar(out=r_sb[:, par, 1, 0:nsub], in0=r_sb[:, par, 1, 0:nsub],
                                                      scalar1=lam_sb[:, 2:3], scalar2=None, op0=ALU.mult),
                     R=[br[par], blam], W=[br[par]])
                for sub in range(nsub):
                    s.op("dve", lambda e: e.tensor_scalar(out=t_sb[:, par, sub, :], in0=a1[:, sub, 0:64],
                                                          scalar1=r_sb[:, par, 1, sub:sub + 1], scalar2=None,
                                                          op0=ALU.mult), R=[b1, br[par]], W=[bt[par]])
                    s.op("dve", lambda e: e.scalar_tensor_tensor(out=o_sb[:, par, sub, :], in0=a0[:, sub, 0:64],
                                                                 scalar=r_sb[:, par, 0, sub:sub + 1],
                                                                 in1=t_sb[:, par, sub, :], op0=ALU.mult, op1=ALU.add),
                         R=[b0, br[par], bt[par]], W=[bo[par]])
                    s.op("dve", lambda e: e.scalar_tensor_tensor(out=junk_sb[:], in0=o_sb[:, par, sub, :], scalar=1.0,
                                                                 in1=o_sb[:, par, sub, :], op0=ALU.mult, op1=ALU.mult,
                                                                 accum_out=ss_sb[:, par, sub:sub + 1]),
                         R=[bo[par], bj], W=[bj, bss[par]])
                s.op("act", lambda e: e.activation(out=ss_sb[:, par, 0:nsub], in_=ss_sb[:, par, 0:nsub], func=AF.Ln,
                                                   scale=1.0 / 64, bias=EPS), R=[bss[par]], W=[bss[par]])
                s.op("act", lambda e: e.activation(out=ss_sb[:, par, 0:nsub], in_=ss_sb[:, par, 0:nsub], func=AF.Exp,
                                                   scale=-0.5), R=[bss[par]], W=[bss[par]])
                for sub in range(nsub):
                    s.op("dve", lambda e: e.scalar_tensor_tensor(out=y_sb[:, q0 // 128 + sub, :], in0=o_sb[:, par, sub, :],
                                                                 scalar=ss_sb[:, par, sub:sub + 1], in1=g_sb[:],
                                                                 op0=ALU.mult, op1=ALU.mult),
                         R=[bo[par], bss[par], bg], W=[by])
            s.dma("sp", ydfP, y_sb[:], R=[by])
            s.finish([by])
    return nc


def tileP(a):
    return np.ascontiguousarray(a.reshape(NKT, 128, a.shape[1]).transpose(1, 0, 2))


def untileP(a):
    return a.transpose(1, 0, 2).reshape(NTOK, a.shape[2])


_NA_IDX = None


def run_p2(nc2, l, fmb, fmf, tm, inp):
    global _NA_IDX
    if _NA_IDX is None:
        _NA_IDX = na_bias_index()
    in_maps = []
    for i in range(NCORES):
        b, j = i // 4, i % 4
        xr = fmf[b, 128 * j:128 * (j + 1)]
        xf = np.concatenate([xr[:, S:], xr[:, :S]], axis=1)
        xb = np.concatenate([xr[:, S:][:, ::-1], xr[:, :S][:, ::-1]], axis=1)
        wbd = np.zeros((4, 128, 128), np.float32)
        rgv = np.zeros((128, 2, 8), np.float32)
        ch = slice(128 * j, 128 * (j + 1))
        for dr in range(2):
            for gi, wk in enumerate(("rg_w_r", "rg_w_i")):
                for bb in range(2):
                    wbd[dr * 2 + gi, 64 * bb:64 * (bb + 1), 64 * bb:64 * (bb + 1)] = inp[wk][l, dr, 2 * j + bb]
            rgv[:, dr, 0:4] = inp["rg_conv_w"][l][:, ch].T
            rgv[:, dr, 4] = inp["rg_conv_b"][l][ch]
            rgv[:, dr, 5] = inp["rg_b_r"][l, dr, ch]
            rgv[:, dr, 6] = inp["rg_b_i"][l, dr, ch]
            rgv[:, dr, 7] = inp["rg_lambda"][l, dr, ch]
        rext = np.concatenate([inp["na_rpb"][l, j].ravel(), np.array([-30000.0], np.float32)])
        bt = rext[_NA_IDX]
        in_maps.append({
            "xrg": np.ascontiguousarray(np.stack([xf, xb])), "wbd": wbd, "rgv": rgv,
            "qaT": np.ascontiguousarray(fmb[b, 64 * j:64 * (j + 1)]),
            "kaT": np.ascontiguousarray(fmb[b, 256 + 64 * j:256 + 64 * (j + 1)]),
            "vaP": tileP(tm[b][:, 64 * j:64 * (j + 1)]),
            "btT": np.ascontiguousarray(bt.transpose(1, 0, 2)),
            "qdT": np.ascontiguousarray(fmb[b, 512 + 64 * j:512 + 64 * (j + 1)].reshape(2, 32, NTOK)),
            "kdT": np.ascontiguousarray(fmb[b, 768 + 64 * j:768 + 64 * (j + 1)].reshape(2, 32, NTOK)),
            "vdP": tileP(tm[b][:, 256 + 64 * j:256 + 64 * (j + 1)]),
            "dlam": np.ascontiguousarray(np.tile(inp["diff_lambda"][l].reshape(1, 128), (128, 1))),
            "dg": np.ascontiguousarray(np.tile(inp["diff_subln_g"][l].reshape(1, 64), (128, 1))),
        })
    res = run_bass_kernel_spmd(nc2, in_maps, core_ids=list(range(NCORES)))
    yna = np.zeros((B, NTOK, 256), ml_dtypes.bfloat16)
    ydf = np.zeros((B, NTOK, 256), ml_dtypes.bfloat16)
    hf = np.zeros((B, 512, NTOK), np.float32)
    hb = np.zeros((B, 512, NTOK), np.float32)
    for i in range(NCORES):
        b, j = i // 4, i % 4
        r = res.results[i]
        yna[b, :, 64 * j:64 * (j + 1)] = untileP(r["ynaP"])
        ydf[b, :, 64 * j:64 * (j + 1)] = untileP(r["ydfP"])
        h = r["hout"]
        hf[b, 128 * j:128 * (j + 1), S:] = h[0][:, :L]
        hf[b, 128 * j:128 * (j + 1), :S] = h[0][:, L:]
        hb[b, 128 * j:128 * (j + 1), S:] = h[1][:, :L][:, ::-1]
        hb[b, 128 * j:128 * (j + 1), :S] = h[1][:, L:][:, ::-1]
    return yna, ydf, hf, hb


NEXP = 32
GELU_C = 1.5957691216057308


def build_p3(final):
    nc = bass.Bass("TRN2", target_bir_lowering=False)
    din = lambda n, shp, dt: nc.dram_tensor(n, shp, dt, kind="ExternalInput").ap()
    xT = din("xT", [128, 8, NT1], F32)
    nadf = din("nadf", [128, 4, NT1], BF16)
    hg = din("hg", [128, 12, NT1], F32)
    wout = din("wout", [D, D], F32)
    mod = din("mod", [128, 10, 8], F32)
    wge = din("wge", [128, 8, 36], F32)
    bge = din("bge", [128, 36], F32)
    selc = din("selc", [32, NEXP * 128], BF16)
    ident = din("ident", [128, 128], F32)
    w1 = din("w1", [NEXP, D, 512], F32)
    w3 = din("w3", [NEXP, D, 512], F32)
    w2 = din("w2", [NEXP, 512, D], F32)
    xo = nc.dram_tensor("xo", [128, 8, NT1], F32, kind="ExternalOutput").ap()
    s = Sched(nc)
    wov = wout.rearrange("(kc p) n -> p kc n", p=128)
    with (nc.psum_tensor("ps", [128, 8, 512], F32) as ps,
          nc.sbuf_tensor("x_sb", [128, 8, NT1], F32) as x_sb,
          nc.sbuf_tensor("mod_sb", [128, 10, 8], F32) as mod_sb,
          nc.sbuf_tensor("hl2_sb", [128, 8, NT1], BF16) as hl2_sb,
          nc.sbuf_tensor("wdt_sb", [32, 2, NT1], BF16) as wdt_sb,
          nc.sbuf_tensor("ones_sb", [128, 128], BF16) as ones_sb):
        bps = [s.buf(f"ps{i}") for i in range(8)]
        cs = s.dsem("const")
        bxb = [s.buf(f"x{i}", s.dsem(f"x{i}")) for i in range(len(BLKS1))]
        bmod = s.buf("mod", cs)
        bhl2 = [s.buf(f"hl2_{i}") for i in range(len(BLKS1))]
        bwdt = [s.buf(f"wdt{i}") for i in range(len(BLKS1))]
        bones = s.buf("ones")
        s.dma("sp", mod_sb[:], mod, W=[bmod])
        for bi, (t0, n) in enumerate(BLKS1):
            s.dma("sp", x_sb[:, :, t0:t0 + n], xT[:, :, t0:t0 + n], W=[bxb[bi]])
        s.op("pool", lambda e: e.memset(ones_sb[:], 1.0), W=[bones])

        with (nc.sbuf_tensor("s1_mix", [128, 8, NT1], BF16) as mix_sb,
              nc.sbuf_tensor("s1_wobf", [128, 8, D], BF16) as wo_bf,
              nc.sbuf_tensor("s1_wost", [128, 2, D], F32) as wo_st,
              nc.sbuf_tensor("s1_hg", [128, 1, 12, 512], F32) as hg_sb,
              nc.sbuf_tensor("s1_t", [128, 4, 512], F32) as t_sb):
            bmixl = s.buf("mixl", s.dsem("mixl"))
            bmix = [s.buf(f"mix{i}") for i in range(len(BLKS1))]
            bwost = [s.buf(f"wost{i}", s.dsem(f"wost{i}")) for i in range(2)]
            bwobf = [s.buf(f"wobf{k}") for k in range(8)]
            bhg = [s.buf(f"hg{i}", s.dsem(f"hg{i}")) for i in range(2)]
            bt = [s.buf(f"t{i}") for i in range(4)]
            s.dma("act", mix_sb[:, 0:2, :], nadf[:, 0:2, :], W=[bmixl])
            s.dma("act", mix_sb[:, 6:8, :], nadf[:, 2:4, :], W=[bmixl])
            for k in range(8):
                sl = k % 2
                s.dma("act", wo_st[:, sl, :], wov[:, k, :], W=[bwost[sl]])
                s.op("pool", lambda e: e.tensor_copy(out=wo_bf[:, k, :], in_=wo_st[:, sl, :]), R=[bwost[sl]], W=[bwobf[k]])
            for bi, (t0, n) in enumerate(BLKS1):
                sl = 0
                s.dma("sp", hg_sb[:, sl, :, 0:n], hg[:, :, t0:t0 + n], W=[bhg[sl]])
                for ch in range(4):
                    hf_ = hg_sb[:, sl, ch, 0:n]; hb_ = hg_sb[:, sl, 4 + ch, 0:n]; gr_ = hg_sb[:, sl, 8 + ch, 0:n]
                    s.op("dve", lambda e: e.tensor_tensor(out=t_sb[:, 0, 0:n], in0=hf_, in1=hb_, op=ALU.add),
                         R=[bhg[sl]], W=[bt[0]])
                    s.op("dve", lambda e: e.tensor_tensor(out=t_sb[:, 1, 0:n], in0=gr_, in1=gr_, op=ALU.mult),
                         R=[bhg[sl]], W=[bt[1]])
                    s.op("dve", lambda e: e.tensor_scalar(out=t_sb[:, 1, 0:n], in0=t_sb[:, 1, 0:n], scalar1=0.044715,
                                                          scalar2=1.0, op0=ALU.mult, op1=ALU.add), R=[bt[1]], W=[bt[1]])
                    s.op("pool", lambda e: e.tensor_tensor(out=t_sb[:, 2, 0:n], in0=t_sb[:, 1, 0:n], in1=gr_, op=ALU.mult),
                         R=[bt[1], bhg[sl]], W=[bt[2]])
                    s.op("act", lambda e: e.activation(out=t_sb[:, 2, 0:n], in_=t_sb[:, 2, 0:n], func=AF.Sigmoid,
                                                       scale=GELU_C), R=[bt[2]], W=[bt[2]])
                    s.op("pool", lambda e: e.tensor_tensor(out=t_sb[:, 3, 0:n], in0=t_sb[:, 2, 0:n], in1=gr_, op=ALU.mult),
                         R=[bt[2], bhg[sl]], W=[bt[3]])
                    s.op("pool", lambda e: e.tensor_tensor(out=mix_sb[:, 2 + ch, t0:t0 + n], in0=t_sb[:, 3, 0:n],
                                                           in1=t_sb[:, 0, 0:n], op=ALU.mult),
                         R=[bt[3], bt[0]], W=[bmix[bi]])
            pi = 0
            for bi, (t0, n) in enumerate(BLKS1):
                garow = 5 if bi == 4 else 1
                for dc in range(8):
                    pb = pi % 8; pi += 1
                    for k in range(8):
                        mm(s, ps[:, pb, 0:n], wo_bf[:, k, dc * 128:(dc + 1) * 128], mix_sb[:, k, t0:t0 + n],
                           k == 0, k == 7, R=[bwobf[k], bmix[bi], bmixl], W=[bps[pb]])
                    s.op("dve", lambda e: e.scalar_tensor_tensor(out=x_sb[:, dc, t0:t0 + n], in0=ps[:, pb, 0:n],
                                                                 scalar=mod_sb[:, garow, dc:dc + 1],
                                                                 in1=x_sb[:, dc, t0:t0 + n], op0=ALU.mult, op1=ALU.add),
                         R=[bps[pb], bmod, bxb[bi]], W=[bxb[bi]])
            s.barrier()

        with (nc.sbuf_tensor("s2_sq", [128, 8, 512], BF16) as sq_sb,
              nc.sbuf_tensor("s2_rstd", [128, 2, 512], F32) as rstd_sb,
              nc.sbuf_tensor("s2_tmp", [128, 4, 512], F32) as tmp_sb,
              nc.sbuf_tensor("s2_hf", [128, 2, 8, 512], F32) as hf_sb,
              nc.sbuf_tensor("s2_ab", [128, 2, 8], F32) as ab_sb,
              nc.sbuf_tensor("s2_wge", [128, 8, 36], F32) as wge_sb,
              nc.sbuf_tensor("s2_bge", [128, 36], F32) as bge_sb,
              nc.sbuf_tensor("s2_id", [128, 128], F32) as id_sb,
              nc.sbuf_tensor("s2_rt", [128, 2, 128], F32) as rt_sb):
            bsq = s.buf("sq"); brs = [s.buf(f"rs{i}") for i in range(2)]
            btmp = [s.buf(f"tmp{i}") for i in range(4)]
            bhf = [s.buf(f"hf{i}") for i in range(2)]
            bab = s.buf("ab")
            bwge = s.buf("wge", cs); bbge = s.buf("bge", cs); bid = s.buf("id", cs)
            brt = [s.buf(f"rt{i}") for i in range(2)]
            s.dma("act", wge_sb[:], wge, W=[bwge])
            s.dma("act", bge_sb[:], bge, W=[bbge])
            s.dma("act", id_sb[:], ident, W=[bid])
            for t in range(2):
                s.op("dve", lambda e: e.scalar_tensor_tensor(
                    out=ab_sb[:, t, :], in0=mod_sb[:, 2 + 4 * t, :], scalar=1.0, in1=mod_sb[:, 0, :],
                    op0=ALU.add, op1=ALU.mult), R=[bmod], W=[bab])
            ti_g = 0
            for bi, (t0, n) in enumerate(BLKS1):
                sl = bi % 2
                isctx = bi == 4
                s.op("act", lambda e: e.activation(out=sq_sb[:, :, 0:n], in_=x_sb[:, :, t0:t0 + n], func=AF.Square),
                     R=[bxb[bi]], W=[bsq])
                for k in range(8):
                    mm(s, ps[:, sl, 0:n], ones_sb[:], sq_sb[:, k, 0:n], k == 0, k == 7, R=[bones, bsq], W=[bps[sl]])
                s.op("act", lambda e: e.activation(out=rstd_sb[:, sl, 0:n], in_=ps[:, sl, 0:n], func=AF.Sqrt,
                                                   scale=1.0 / D, bias=EPS), R=[bps[sl]], W=[brs[sl]])
                s.op("dve", lambda e: e.reciprocal(out=rstd_sb[:, sl, 0:n], in_=rstd_sb[:, sl, 0:n]),
                     R=[brs[sl]], W=[brs[sl]])
                ai = 1 if isctx else 0
                shrow = 7 if isctx else 3
                for k in range(8):
                    tb = k % 4
                    s.op("dve", lambda e: e.scalar_tensor_tensor(
                        out=tmp_sb[:, tb, 0:n], in0=x_sb[:, k, t0:t0 + n], scalar=ab_sb[:, ai, k:k + 1],
                        in1=rstd_sb[:, sl, 0:n], op0=ALU.mult, op1=ALU.mult),
                        R=[bxb[bi], bab, brs[sl]], W=[btmp[tb]])
                    s.op("act", lambda e: e.activation(out=hf_sb[:, sl, k, 0:n], in_=tmp_sb[:, tb, 0:n],
                                                       func=AF.Identity, bias=mod_sb[:, shrow, k:k + 1], scale=1.0),
                         R=[btmp[tb], bmod], W=[bhf[sl]])
                s.op("pool", lambda e: e.tensor_copy(out=hl2_sb[:, :, t0:t0 + n], in_=hf_sb[:, sl, :, 0:n]),
                     R=[bhf[sl]], W=[bhl2[bi]])
                for tt in range((n + 127) // 128):
                    c0 = tt * 128
                    m = min(128, n - c0)
                    rs_ = ti_g % 2; ti_g += 1
                    pb = 2 + rs_
                    rt = rt_sb[0:m, rs_, :]
                    brr = brt[rs_]
                    for k in range(8):
                        mm(s, ps[0:m, pb, 0:36], hf_sb[:, sl, k, c0:c0 + m], wge_sb[:, k, :], k == 0, k == 7,
                           R=[bhf[sl], bwge], W=[bps[pb]])
                    V = lambda eng, fn, R_=(), W_=(): s.op(eng, fn, R=[brr] + list(R_), W=[brr] + list(W_))
                    lg = rt[:, 0:36]
                    s.op("dve", lambda e: e.tensor_tensor(out=lg, in0=ps[0:m, pb, 0:36], in1=bge_sb[0:m, :], op=ALU.add),
                         R=[bps[pb], bbge], W=[brr])
                    gmax = rt[:, 36:37]; ngmax = rt[:, 37:38]; sume = rt[:, 38:39]; gtop = rt[:, 39:40]
                    eg = rt[:, 40:44]; ohg = rt[:, 44:48]; sel = rt[:, 48:56]; top8 = rt[:, 56:64]
                    dd = rt[:, 64:65]; ed = rt[:, 65:66]; w1_ = rt[:, 66:67]; wt1 = rt[:, 67:68]; wt2 = rt[:, 68:69]
                    ea = rt[:, 72:80]; eb_ = rt[:, 80:88]; wd = rt[:, 96:128]
                    V("dve", lambda e: e.reduce_max(out=gmax, in_=lg[:, 0:4], axis=AX.X))
                    V("dve", lambda e: e.tensor_scalar(out=ngmax, in0=gmax, scalar1=-1.0, scalar2=None, op0=ALU.mult))
                    V("act", lambda e: e.activation(out=eg, in_=lg[:, 0:4], func=AF.Exp, bias=ngmax, scale=1.0,
                                                    accum_out=sume))
                    V("dve", lambda e: e.reciprocal(out=gtop, in_=sume))
                    V("dve", lambda e: e.tensor_scalar(out=ohg, in0=lg[:, 0:4], scalar1=gmax, scalar2=None,
                                                       op0=ALU.is_equal))
                    V("dve", lambda e: e.tensor_scalar(out=sel, in0=lg[:, 4:12], scalar1=ohg[:, 0:1], scalar2=None,
                                                       op0=ALU.mult))
                    for g in range(1, 4):
                        V("dve", lambda e: e.scalar_tensor_tensor(out=sel, in0=lg[:, 4 + 8 * g:12 + 8 * g],
                                                                  scalar=ohg[:, g:g + 1], in1=sel,
                                                                  op0=ALU.mult, op1=ALU.add))
                    V("dve", lambda e: e.max(out=top8, in_=sel))
                    V("dve", lambda e: e.tensor_tensor(out=dd, in0=top8[:, 1:2], in1=top8[:, 0:1], op=ALU.subtract))
                    V("act", lambda e: e.activation(out=ed, in_=dd, func=AF.Exp))
                    V("dve", lambda e: e.tensor_scalar(out=w1_, in0=ed, scalar1=1.0, scalar2=None, op0=ALU.add))
                    V("dve", lambda e: e.reciprocal(out=w1_, in_=w1_))
                    V("dve", lambda e: e.tensor_tensor(out=wt1, in0=w1_, in1=gtop, op=ALU.mult))
                    V("dve", lambda e: e.tensor_tensor(out=wt2, in0=wt1, in1=ed, op=ALU.mult))
                    V("dve", lambda e: e.tensor_scalar(out=ea, in0=sel, scalar1=top8[:, 0:1], scalar2=wt1,
                                                       op0=ALU.is_equal, op1=ALU.mult))
                    V("dve", lambda e: e.tensor_scalar(out=eb_, in0=sel, scalar1=top8[:, 1:2], scalar2=wt2,
                                                       op0=ALU.is_equal, op1=ALU.mult))
                    V("dve", lambda e: e.tensor_tensor(out=ea, in0=ea, in1=eb_, op=ALU.add))
                    for g in range(4):
                        V("dve", lambda e: e.tensor_scalar(out=wd[:, 8 * g:8 * g + 8], in0=ea, scalar1=ohg[:, g:g + 1],
                                                           scalar2=None, op0=ALU.mult))
                    pt = 4 + rs_
                    s.op("pe", lambda e: e.transpose(ps[0:32, pt, 0:m], wd, id_sb[0:m, 0:m]), R=[brr, bid], W=[bps[pt]])
                    s.op("act", lambda e: e.copy(out=wdt_sb[:, 0, t0 + c0:t0 + c0 + m], in_=ps[0:32, pt, 0:m]),
                         R=[bps[pt]], W=[bwdt[bi]])
                    s.op("dve", lambda e: e.tensor_tensor(out=wdt_sb[:, 1, t0 + c0:t0 + c0 + m], in0=ps[0:32, pt, 0:m],
                                                          in1=wdt_sb[:, 0, t0 + c0:t0 + c0 + m], op=ALU.subtract),
                         R=[bps[pt], bwdt[bi]], W=[bwdt[bi]])
            s.barrier()

        with (nc.sbuf_tensor("s3_st", [128, 3, 2048], F32) as st_sb,
              nc.sbuf_tensor("s3_wb", [128, 2, 6, 2048], BF16) as wb_sb,
              nc.sbuf_tensor("s3_sel", [32, NEXP * 128], BF16) as sel_sb,
              nc.sbuf_tensor("s3_wbc", [128, 2, 512], F32) as wbc_sb,
              nc.sbuf_tensor("s3_sg", [128, 2, 512], F32) as sg_sb,
              nc.sbuf_tensor("s3_t", [128, 2, 512], F32) as t3_sb,
              nc.sbuf_tensor("s3_g", [128, 2, 4, 512], BF16) as g_sb):
            bst = [s.buf(f"st{i}", s.dsem(f"st{i}")) for i in range(3)]
            bwb = [[s.buf(f"wb{a}_{p}") for p in range(6)] for a in range(2)]
            bsel = s.buf("sel", cs)
            bwbc = [s.buf(f"wbc{i}") for i in range(2)]
            bsg = [s.buf(f"sg{i}") for i in range(2)]
            bt3 = [s.buf(f"t3{i}") for i in range(2)]
            bg = [s.buf(f"g{i}") for i in range(2)]
            s.dma("act", sel_sb[:], selc, W=[bsel])
            w1v = w1.rearrange("e (kc p) f -> e p kc f", p=128)
            w3v = w3.rearrange("e (kc p) f -> e p kc f", p=128)
            w2v = w2.rearrange("e (fc p) d -> e p fc d", p=128)

            def piece_src(e, p):
                if p < 2:
                    return w1v[e, :, 4 * p:4 * p + 4, :]
                if p < 4:
                    return w3v[e, :, 4 * (p - 2):4 * (p - 2) + 4, :]
                return w2v[e, :, 2 * (p - 4):2 * (p - 4) + 2, :]

            def piece_dma(P):
                e, p = divmod(P, 6)
                if e >= NEXP:
                    return
                sl = P % 3
                dst = st_sb[:, sl, :]
                dst = dst.rearrange("q (a b) -> q a b", a=4) if p < 4 else dst.rearrange("q (a b) -> q a b", a=2)
                s.dma("sp", dst, piece_src(e, p), W=[bst[sl]])

            def piece_cast(P):
                e, p = divmod(P, 6)
                if e >= NEXP:
                    return
                sl = P % 3
                s.op("pool", lambda en: en.tensor_copy(out=wb_sb[:, e % 2, p, :], in_=st_sb[:, sl, :]),
                     R=[bst[sl]], W=[bwb[e % 2][p]])

            for P in range(3):
                piece_dma(P)
            for P in range(6):
                piece_cast(P)
                piece_dma(P + 3)
            gi = 0
            for ex in range(NEXP):
                a = ex % 2
                for bi, (t0, n) in enumerate(BLKS1):
                    if bi < 3:
                        for P in (6 * (ex + 1) + 2 * bi, 6 * (ex + 1) + 2 * bi + 1):
                            piece_cast(P)
                            piece_dma(P + 3)
                    garow = 8 if bi == 4 else 4
                    wr = gi % 2
                    gs = gi % 2
                    gi += 1
                    mm(s, ps[:, 6, 0:n], sel_sb[:, ex * 128:(ex + 1) * 128], wdt_sb[:, 0, t0:t0 + n], True, False,
                       R=[bsel, bwdt[bi]], W=[bps[6]])
                    mm(s, ps[:, 6, 0:n], sel_sb[:, ex * 128:(ex + 1) * 128], wdt_sb[:, 1, t0:t0 + n], False, True,
                       R=[bsel, bwdt[bi]], W=[bps[6]])
                    s.op("act", lambda e: e.copy(out=wbc_sb[:, wr, 0:n], in_=ps[:, 6, 0:n]), R=[bps[6]], W=[bwbc[wr]])
                    for fc in range(4):
                        pr = fc % 2
                        for which in range(2):
                            bank = 2 * pr + which
                            for k in range(8):
                                wv = wb_sb[:, a, 2 * which + k // 4, :].rearrange("q (a b) -> q a b", a=4)
                                mm(s, ps[:, bank, 0:n], wv[:, k % 4, fc * 128:(fc + 1) * 128], hl2_sb[:, k, t0:t0 + n],
                                   k == 0, k == 7, R=[bwb[a][2 * which + k // 4], bhl2[bi]], W=[bps[bank]])
                        s.op("act", lambda e: e.activation(out=sg_sb[:, pr, 0:n], in_=ps[:, 2 * pr, 0:n], func=AF.Silu),
                             R=[bps[2 * pr]], W=[bsg[pr]])
                        s.op("dve", lambda e: e.tensor_tensor(out=t3_sb[:, pr, 0:n], in0=ps[:, 2 * pr + 1, 0:n],
                                                              in1=sg_sb[:, pr, 0:n], op=ALU.mult),
                             R=[bps[2 * pr + 1], bsg[pr]], W=[bt3[pr]])
                        s.op("pool", lambda e: e.tensor_tensor(out=g_sb[:, gs, fc, 0:n], in0=t3_sb[:, pr, 0:n],
                                                               in1=wbc_sb[:, wr, 0:n], op=ALU.mult),
                             R=[bt3[pr], bwbc[wr]], W=[bg[gs]])
                    for dc in range(8):
                        bank = 4 + dc % 2
                        for fc in range(4):
                            wv = wb_sb[:, a, 4 + fc // 2, :].rearrange("q (a b) -> q a b", a=2)
                            mm(s, ps[:, bank, 0:n], wv[:, fc % 2, dc * 128:(dc + 1) * 128], g_sb[:, gs, fc, 0:n],
                               fc == 0, fc == 3, R=[bwb[a][4 + fc // 2], bg[gs]], W=[bps[bank]])
                        s.op("dve", lambda e: e.scalar_tensor_tensor(out=x_sb[:, dc, t0:t0 + n], in0=ps[:, bank, 0:n],
                                                                     scalar=mod_sb[:, garow, dc:dc + 1],
                                                                     in1=x_sb[:, dc, t0:t0 + n], op0=ALU.mult, op1=ALU.add),
                             R=[bps[bank], bmod, bxb[bi]], W=[bxb[bi]])
            s.barrier()

        with (nc.sbuf_tensor("s4_sq", [128, 8, 512], BF16) as sq_sb,
              nc.sbuf_tensor("s4_rstd", [128, 2, 512], F32) as rstd_sb):
            bsq = s.buf("sq4"); brs = [s.buf(f"rs4{i}") for i in range(2)]
            for bi, (t0, n) in enumerate(BLKS1):
                if final:
                    sl = bi % 2
                    s.op("act", lambda e: e.activation(out=sq_sb[:, :, 0:n], in_=x_sb[:, :, t0:t0 + n], func=AF.Square),
                         R=[bxb[bi]], W=[bsq])
                    for k in range(8):
                        mm(s, ps[:, sl, 0:n], ones_sb[:], sq_sb[:, k, 0:n], k == 0, k == 7, R=[bones, bsq], W=[bps[sl]])
                    s.op("act", lambda e: e.activation(out=rstd_sb[:, sl, 0:n], in_=ps[:, sl, 0:n], func=AF.Sqrt,
                                                       scale=1.0 / D, bias=EPS), R=[bps[sl]], W=[brs[sl]])
                    s.op("dve", lambda e: e.reciprocal(out=rstd_sb[:, sl, 0:n], in_=rstd_sb[:, sl, 0:n]),
                         R=[brs[sl]], W=[brs[sl]])
                    for k in range(8):
                        s.op("dve", lambda e: e.scalar_tensor_tensor(
                            out=x_sb[:, k, t0:t0 + n], in0=x_sb[:, k, t0:t0 + n], scalar=mod_sb[:, 9, k:k + 1],
                            in1=rstd_sb[:, sl, 0:n], op0=ALU.mult, op1=ALU.mult),
                            R=[bxb[bi], bmod, brs[sl]], W=[bxb[bi]])
                s.dma("sp", xo[:, :, t0:t0 + n], x_sb[:, :, t0:t0 + n], R=[bxb[bi]])
            s.finish(bxb)
    return nc


def build_p3a():
    nc = bass.Bass("TRN2", target_bir_lowering=False)
    din = lambda n, shp, dt: nc.dram_tensor(n, shp, dt, kind="ExternalInput").ap()
    xT = din("xT", [128, 8, NT1], F32)
    nadf = din("nadf", [128, 4, NT1], BF16)
    hg = din("hg", [128, 12, NT1], F32)
    wout = din("wout", [D, D], F32)
    mod = din("mod", [128, 10, 8], F32)
    wge = din("wge", [128, 8, 36], F32)
    bge = din("bge", [128, 36], F32)
    iota4 = din("iota4", [128, 4], F32)
    ident = din("ident", [128, 128], F32)
    xo = nc.dram_tensor("xo", [128, 8, NT1], F32, kind="ExternalOutput").ap()
    hl2o = nc.dram_tensor("hl2o", [128, 8, NT1], BF16, kind="ExternalOutput").ap()
    wdto = nc.dram_tensor("wdto", [32, 2, NT1], BF16, kind="ExternalOutput").ap()
    gido = nc.dram_tensor("gido", [128, 17], F32, kind="ExternalOutput").ap()
    wd32o = nc.dram_tensor("wd32o", [128, 17, 32], F32, kind="ExternalOutput").ap()
    s = Sched(nc)
    wov = wout.rearrange("(kc p) n -> p kc n", p=128)
    with (nc.psum_tensor("ps", [128, 8, 512], F32) as ps,
          nc.sbuf_tensor("x_sb", [128, 8, NT1], F32) as x_sb,
          nc.sbuf_tensor("mod_sb", [128, 10, 8], F32) as mod_sb,
          nc.sbuf_tensor("hl2_sb", [128, 8, NT1], BF16) as hl2_sb,
          nc.sbuf_tensor("wdt_sb", [32, 2, NT1], BF16) as wdt_sb,
          nc.sbuf_tensor("ones_sb", [128, 128], BF16) as ones_sb):
        bps = [s.buf(f"ps{i}") for i in range(8)]
        cs = s.dsem("const")
        bxb = [s.buf(f"x{i}", s.dsem(f"x{i}")) for i in range(len(BLKS1))]
        bmod = s.buf("mod", cs)
        bhl2 = [s.buf(f"hl2_{i}") for i in range(len(BLKS1))]
        bwdt = [s.buf(f"wdt{i}") for i in range(len(BLKS1))]
        bones = s.buf("ones")
        s.dma("sp", mod_sb[:], mod, W=[bmod])
        for bi, (t0, n) in enumerate(BLKS1):
            s.dma("sp", x_sb[:, :, t0:t0 + n], xT[:, :, t0:t0 + n], W=[bxb[bi]])
        s.op("pool", lambda e: e.memset(ones_sb[:], 1.0), W=[bones])

        with (nc.sbuf_tensor("s1_mix", [128, 8, NT1], BF16) as mix_sb,
              nc.sbuf_tensor("s1_wobf", [128, 8, D], BF16) as wo_bf,
              nc.sbuf_tensor("s1_wost", [128, 2, D], F32) as wo_st,
              nc.sbuf_tensor("s1_hg", [128, 1, 12, 512], F32) as hg_sb,
              nc.sbuf_tensor("s1_t", [128, 4, 512], F32) as t_sb):
            bmixl = s.buf("mixl", s.dsem("mixl"))
            bmix = [s.buf(f"mix{i}") for i in range(len(BLKS1))]
            bwost = [s.buf(f"wost{i}", s.dsem(f"wost{i}")) for i in range(2)]
            bwobf = [s.buf(f"wobf{k}") for k in range(8)]
            bhg = [s.buf(f"hg{i}", s.dsem(f"hg{i}")) for i in range(2)]
            bt = [s.buf(f"t{i}") for i in range(4)]
            s.dma("act", mix_sb[:, 0:2, :], nadf[:, 0:2, :], W=[bmixl])
            s.dma("act", mix_sb[:, 6:8, :], nadf[:, 2:4, :], W=[bmixl])
            for k in range(8):
                sl = k % 2
                s.dma("act", wo_st[:, sl, :], wov[:, k, :], W=[bwost[sl]])
                s.op("pool", lambda e: e.tensor_copy(out=wo_bf[:, k, :], in_=wo_st[:, sl, :]), R=[bwost[sl]], W=[bwobf[k]])
            for bi, (t0, n) in enumerate(BLKS1):
                sl = 0
                s.dma("sp", hg_sb[:, sl, :, 0:n], hg[:, :, t0:t0 + n], W=[bhg[sl]])
                for ch in range(4):
                    hf_ = hg_sb[:, sl, ch, 0:n]; hb_ = hg_sb[:, sl, 4 + ch, 0:n]; gr_ = hg_sb[:, sl, 8 + ch, 0:n]
                    s.op("dve", lambda e: e.tensor_tensor(out=t_sb[:, 0, 0:n], in0=hf_, in1=hb_, op=ALU.add),
                         R=[bhg[sl]], W=[bt[0]])
                    s.op("dve", lambda e: e.tensor_tensor(out=t_sb[:, 1, 0:n], in0=gr_, in1=gr_, op=ALU.mult),
                         R=[bhg[sl]], W=[bt[1]])
                    s.op("dve", lambda e: e.tensor_scalar(out=t_sb[:, 1, 0:n], in0=t_sb[:, 1, 0:n], scalar1=0.044715,
                                                          scalar2=1.0, op0=ALU.mult, op1=ALU.add), R=[bt[1]], W=[bt[1]])
                    s.op("pool", lambda e: e.tensor_tensor(out=t_sb[:, 2, 0:n], in0=t_sb[:, 1, 0:n], in1=gr_, op=ALU.mult),
                         R=[bt[1], bhg[sl]], W=[bt[2]])
                    s.op("act", lambda e: e.activation(out=t_sb[:, 2, 0:n], in_=t_sb[:, 2, 0:n], func=AF.Sigmoid,
                                                       scale=GELU_C), R=[bt[2]], W=[bt[2]])
                    s.op("pool", lambda e: e.tensor_tensor(out=t_sb[:, 3, 0:n], in0=t_sb[:, 2, 0:n], in1=gr_, op=ALU.mult),
                         R=[bt[2], bhg[sl]], W=[bt[3]])
                    s.op("pool", lambda e: e.tensor_tensor(out=mix_sb[:, 2 + ch, t0:t0 + n], in0=t_sb[:, 3, 0:n],
                                                           in1=t_sb[:, 0, 0:n], op=ALU.mult),
                         R=[bt[3], bt[0]], W=[bmix[bi]])
            pi = 0
            for bi, (t0, n) in enumerate(BLKS1):
                garow = 5 if bi == 4 else 1
                for dc in range(8):
                    pb = pi % 8; pi += 1
                    for k in range(8):
                        mm(s, ps[:, pb, 0:n], wo_bf[:, k, dc * 128:(dc + 1) * 128], mix_sb[:, k, t0:t0 + n],
                           k == 0, k == 7, R=[bwobf[k], bmix[bi], bmixl], W=[bps[pb]])
                    s.op("dve", lambda e: e.scalar_tensor_tensor(out=x_sb[:, dc, t0:t0 + n], in0=ps[:, pb, 0:n],
                                                                 scalar=mod_sb[:, garow, dc:dc + 1],
                                                                 in1=x_sb[:, dc, t0:t0 + n], op0=ALU.mult, op1=ALU.add),
                         R=[bps[pb], bmod, bxb[bi]], W=[bxb[bi]])
            s.barrier()

        es_ = ExitStack()
        io4_sb = es_.enter_context(nc.sbuf_tensor("s2_io4", [128, 4], F32))
        gid_sb = es_.enter_context(nc.sbuf_tensor("s2_gid", [128, 17], F32))
        junk4_sb = es_.enter_context(nc.sbuf_tensor("s2_junk", [128, 4], F32))
        wd32_sb = es_.enter_context(nc.sbuf_tensor("s2_wd32", [128, 17, 32], F32))
        with (nc.sbuf_tensor("s2_sq", [128, 8, 512], BF16) as sq_sb,
              nc.sbuf_tensor("s2_rstd", [128, 2, 512], F32) as rstd_sb,
              nc.sbuf_tensor("s2_tmp", [128, 4, 512], F32) as tmp_sb,
              nc.sbuf_tensor("s2_hf", [128, 2, 8, 512], F32) as hf_sb,
              nc.sbuf_tensor("s2_ab", [128, 2, 8], F32) as ab_sb,
              nc.sbuf_tensor("s2_wge", [128, 8, 36], F32) as wge_sb,
              nc.sbuf_tensor("s2_bge", [128, 36], F32) as bge_sb,
              nc.sbuf_tensor("s2_id", [128, 128], F32) as id_sb,
              nc.sbuf_tensor("s2_rt", [128, 2, 128], F32) as rt_sb):
            bsq = s.buf("sq"); brs = [s.buf(f"rs{i}") for i in range(2)]
            btmp = [s.buf(f"tmp{i}") for i in range(4)]
            bhf = [s.buf(f"hf{i}") for i in range(2)]
            bab = s.buf("ab")
            bwge = s.buf("wge", cs); bbge = s.buf("bge", cs); bid = s.buf("id", cs)
            brt = [s.buf(f"rt{i}") for i in range(2)]
            s.dma("act", wge_sb[:], wge, W=[bwge])
            s.dma("act", bge_sb[:], bge, W=[bbge])
            s.dma("act", id_sb[:], ident, W=[bid])
            bio4 = s.buf("io4", cs); bgid = s.buf("gid", s.dsem("gid")); bjk = s.buf("junk4")
            s.dma("act", io4_sb[:], iota4, W=[bio4])
            s.op("pool", lambda e: e.memset(gid_sb[:], 0.0), W=[bgid])
            bwd32 = s.buf("wd32", s.dsem("wd32"))
            s.op("pool", lambda e: e.memset(wd32_sb[:], 0.0), W=[bwd32])
            for t in range(2):
                s.op("dve", lambda e: e.scalar_tensor_tensor(
                    out=ab_sb[:, t, :], in0=mod_sb[:, 2 + 4 * t, :], scalar=1.0, in1=mod_sb[:, 0, :],
                    op0=ALU.add, op1=ALU.mult), R=[bmod], W=[bab])
            ti_g = 0
            for bi, (t0, n) in enumerate(BLKS1):
                sl = bi % 2
                isctx = bi == 4
                s.op("act", lambda e: e.activation(out=sq_sb[:, :, 0:n], in_=x_sb[:, :, t0:t0 + n], func=AF.Square),
                     R=[bxb[bi]], W=[bsq])
                for k in range(8):
                    mm(s, ps[:, sl, 0:n], ones_sb[:], sq_sb[:, k, 0:n], k == 0, k == 7, R=[bones, bsq], W=[bps[sl]])
                s.op("act", lambda e: e.activation(out=rstd_sb[:, sl, 0:n], in_=ps[:, sl, 0:n], func=AF.Sqrt,
                                                   scale=1.0 / D, bias=EPS), R=[bps[sl]], W=[brs[sl]])
                s.op("dve", lambda e: e.reciprocal(out=rstd_sb[:, sl, 0:n], in_=rstd_sb[:, sl, 0:n]),
                     R=[brs[sl]], W=[brs[sl]])
                ai = 1 if isctx else 0
                shrow = 7 if isctx else 3
                for k in range(8):
                    tb = k % 4
                    s.op("dve", lambda e: e.scalar_tensor_tensor(
                        out=tmp_sb[:, tb, 0:n], in0=x_sb[:, k, t0:t0 + n], scalar=ab_sb[:, ai, k:k + 1],
                        in1=rstd_sb[:, sl, 0:n], op0=ALU.mult, op1=ALU.mult),
                        R=[bxb[bi], bab, brs[sl]], W=[btmp[tb]])
                    s.op("act", lambda e: e.activation(out=hf_sb[:, sl, k, 0:n], in_=tmp_sb[:, tb, 0:n],
                                                       func=AF.Identity, bias=mod_sb[:, shrow, k:k + 1], scale=1.0),
                         R=[btmp[tb], bmod], W=[bhf[sl]])
                s.op("pool", lambda e: e.tensor_copy(out=hl2_sb[:, :, t0:t0 + n], in_=hf_sb[:, sl, :, 0:n]),
                     R=[bhf[sl]], W=[bhl2[bi]])
                for tt in range((n + 127) // 128):
                    c0 = tt * 128
                    m = min(128, n - c0)
                    rs_ = ti_g % 2; ti_g += 1
                    pb = 2 + rs_
                    rt = rt_sb[0:m, rs_, :]
                    brr = brt[rs_]
                    for k in range(8):
                        mm(s, ps[0:m, pb, 0:36], hf_sb[:, sl, k, c0:c0 + m], wge_sb[:, k, :], k == 0, k == 7,
                           R=[bhf[sl], bwge], W=[bps[pb]])
                    V = lambda eng, fn, R_=(), W_=(): s.op(eng, fn, R=[brr] + list(R_), W=[brr] + list(W_))
                    lg = rt[:, 0:36]
                    s.op("dve", lambda e: e.tensor_tensor(out=lg, in0=ps[0:m, pb, 0:36], in1=bge_sb[0:m, :], op=ALU.add),
                         R=[bps[pb], bbge], W=[brr])
                    gmax = rt[:, 36:37]; ngmax = rt[:, 37:38]; sume = rt[:, 38:39]; gtop = rt[:, 39:40]
                    eg = rt[:, 40:44]; ohg = rt[:, 44:48]; sel = rt[:, 48:56]; top8 = rt[:, 56:64]
                    dd = rt[:, 64:65]; ed = rt[:, 65:66]; w1_ = rt[:, 66:67]; wt1 = rt[:, 67:68]; wt2 = rt[:, 68:69]
                    ea = rt[:, 72:80]; eb_ = rt[:, 80:88]; wd = rt[:, 96:128]
                    V("dve", lambda e: e.reduce_max(out=gmax, in_=lg[:, 0:4], axis=AX.X))
                    V("dve", lambda e: e.tensor_scalar(out=ngmax, in0=gmax, scalar1=-1.0, scalar2=None, op0=ALU.mult))
                    V("act", lambda e: e.activation(out=eg, in_=lg[:, 0:4], func=AF.Exp, bias=ngmax, scale=1.0,
                                                    accum_out=sume))
                    V("dve", lambda e: e.reciprocal(out=gtop, in_=sume))
                    V("dve", lambda e: e.tensor_scalar(out=ohg, in0=lg[:, 0:4], scalar1=gmax, scalar2=None,
                                                       op0=ALU.is_equal))
                    tgl = (t0 + c0) // 128
                    s.op("dve", lambda e: e.scalar_tensor_tensor(out=junk4_sb[0:m, :], in0=ohg, scalar=1.0,
                                                                 in1=io4_sb[0:m, :], op0=ALU.mult, op1=ALU.mult,
                                                                 accum_out=gid_sb[0:m, tgl:tgl + 1]),
                         R=[brr, bio4, bjk], W=[bjk, bgid])
                    V("dve", lambda e: e.tensor_scalar(out=sel, in0=lg[:, 4:12], scalar1=ohg[:, 0:1], scalar2=None,
                                                       op0=ALU.mult))
                    for g in range(1, 4):
                        V("dve", lambda e: e.scalar_tensor_tensor(out=sel, in0=lg[:, 4 + 8 * g:12 + 8 * g],
                                                                  scalar=ohg[:, g:g + 1], in1=sel,
                                                                  op0=ALU.mult, op1=ALU.add))
                    V("dve", lambda e: e.max(out=top8, in_=sel))
                    V("dve", lambda e: e.tensor_tensor(out=dd, in0=top8[:, 1:2], in1=top8[:, 0:1], op=ALU.subtract))
                    V("act", lambda e: e.activation(out=ed, in_=dd, func=AF.Exp))
                    V("dve", lambda e: e.tensor_scalar(out=w1_, in0=ed, scalar1=1.0, scalar2=None, op0=ALU.add))
                    V("dve", lambda e: e.reciprocal(out=w1_, in_=w1_))
                    V("dve", lambda e: e.tensor_tensor(out=wt1, in0=w1_, in1=gtop, op=ALU.mult))
                    V("dve", lambda e: e.tensor_tensor(out=wt2, in0=wt1, in1=ed, op=ALU.mult))
                    V("dve", lambda e: e.tensor_scalar(out=ea, in0=sel, scalar1=top8[:, 0:1], scalar2=wt1,
                                                       op0=ALU.is_equal, op1=ALU.mult))
                    V("dve", lambda e: e.tensor_scalar(out=eb_, in0=sel, scalar1=top8[:, 1:2], scalar2=wt2,
                                                       op0=ALU.is_equal, op1=ALU.mult))
                    V("dve", lambda e: e.tensor_tensor(out=ea, in0=ea, in1=eb_, op=ALU.add))
                    for g in range(4):
                        V("dve", lambda e: e.tensor_scalar(out=wd[:, 8 * g:8 * g + 8], in0=ea, scalar1=ohg[:, g:g + 1],
                                                           scalar2=None, op0=ALU.mult))
                    s.op("dve", lambda e: e.tensor_copy(out=wd32_sb[0:m, tgl, :], in_=wd), R=[brr], W=[bwd32])
                    pt = 4 + rs_
                    s.op("pe", lambda e: e.transpose(ps[0:32, pt, 0:m], wd, id_sb[0:m, 0:m]), R=[brr, bid], W=[bps[pt]])
                    s.op("act", lambda e: e.copy(out=wdt_sb[:, 0, t0 + c0:t0 + c0 + m], in_=ps[0:32, pt, 0:m]),
                         R=[bps[pt]], W=[bwdt[bi]])
                    s.op("dve", lambda e: e.tensor_tensor(out=wdt_sb[:, 1, t0 + c0:t0 + c0 + m], in0=ps[0:32, pt, 0:m],
                                                          in1=wdt_sb[:, 0, t0 + c0:t0 + c0 + m], op=ALU.subtract),
                         R=[bps[pt], bwdt[bi]], W=[bwdt[bi]])
            bxo = s.buf("xout", s.dsem("xout"))
            s.dma("sp", xo, x_sb[:], R=bxb, W=[bxo])
            s.dma("sp", hl2o, hl2_sb[:], R=bhl2, W=[bxo])
            s.dma("sp", wdto, wdt_sb[:], R=bwdt, W=[bxo])
            s.dma("sp", gido, gid_sb[:], R=[bgid], W=[bxo])
            s.dma("sp", wd32o, wd32_sb[:], R=[bwd32], W=[bxo])
            s.finish([bxo])
        es_.close()
    return nc


def build_p3b(ntb):
    NE = 8
    nblk_all = ntb // 512
    nh = 2 if ntb > 2048 else 1
    nblk = -(-nblk_all // nh)
    nth = nblk * 512
    nc = bass.Bass("TRN2", target_bir_lowering=False)
    din = lambda n, shp, dt: nc.dram_tensor(n, shp, dt, kind="ExternalInput").ap()
    hl2 = din("hl2", [128, 8, ntb], BF16)
    wdt = din("wdt", [8, 2, ntb], BF16)
    selc = din("selc", [8, NE * 128], BF16)
    w1 = din("w1", [NE, D, 512], F32)
    w3 = din("w3", [NE, D, 512], F32)
    w2 = din("w2", [NE, 512, D], F32)
    yo = nc.dram_tensor("yo", [128, 8, ntb], F32, kind="ExternalOutput").ap()
    s = Sched(nc)
    with (nc.psum_tensor("ps", [128, 8, 512], F32) as ps,
          nc.sbuf_tensor("y_sb", [128, 8, nth], F32) as y_sb,
          nc.sbuf_tensor("hl2_sb", [128, 8, nth], BF16) as hl2_sb,
          nc.sbuf_tensor("wdt_sb", [8, 2, ntb], BF16) as wdt_sb,
          nc.sbuf_tensor("s3_st", [128, 3, 2048], F32) as st_sb,
          nc.sbuf_tensor("s3_wb", [128, 2, 6, 2048], BF16) as wb_sb,
          nc.sbuf_tensor("s3_sel", [8, NE * 128], BF16) as sel_sb,
          nc.sbuf_tensor("s3_wbc", [128, 2, 512], F32) as wbc_sb,
          nc.sbuf_tensor("s3_sg", [128, 2, 512], F32) as sg_sb,
          nc.sbuf_tensor("s3_t", [128, 2, 512], F32) as t3_sb,
          nc.sbuf_tensor("s3_g", [128, 2, 4, 512], BF16) as g_sb):
        bps = [s.buf(f"ps{i}") for i in range(8)]
        cs = s.dsem("const")
        by = [s.buf(f"y{i}", s.dsem(f"y{i}")) for i in range(nblk)]
        bhl2 = [s.buf(f"hl2_{i}", s.dsem(f"hl{i}")) for i in range(nblk)]
        bwdt = s.buf("wdt", cs)
        bst = [s.buf(f"st{i}", s.dsem(f"st{i}")) for i in range(3)]
        bwb = [[s.buf(f"wb{a}_{p}") for p in range(6)] for a in range(2)]
        bsel = s.buf("sel", cs)
        bwbc = [s.buf(f"wbc{i}") for i in range(2)]
        bsg = [s.buf(f"sg{i}") for i in range(2)]
        bt3 = [s.buf(f"t3{i}") for i in range(2)]
        bg = [s.buf(f"g{i}") for i in range(2)]
        s.dma("act", sel_sb[:], selc, W=[bsel])
        s.dma("act", wdt_sb[:], wdt, W=[bwdt])
        w1v = w1.rearrange("e (kc p) f -> e p kc f", p=128)
        w3v = w3.rearrange("e (kc p) f -> e p kc f", p=128)
        w2v = w2.rearrange("e (fc p) d -> e p fc d", p=128)

        def piece_src(e, p):
            if p < 2:
                return w1v[e, :, 4 * p:4 * p + 4, :]
            if p < 4:
                return w3v[e, :, 4 * (p - 2):4 * (p - 2) + 4, :]
            return w2v[e, :, 2 * (p - 4):2 * (p - 4) + 2, :]

        def piece_dma(P):
            e, p = divmod(P, 6)
            if e >= NE * nh:
                return
            e = e % NE
            sl = P % 3
            dst = st_sb[:, sl, :]
            dst = dst.rearrange("q (a b) -> q a b", a=4) if p < 4 else dst.rearrange("q (a b) -> q a b", a=2)
            s.dma("sp", dst, piece_src(e, p), W=[bst[sl]])

        def piece_cast(P):
            e, p = divmod(P, 6)
            if e >= NE * nh:
                return
            sl = P % 3
            s.op("pool", lambda en: en.tensor_copy(out=wb_sb[:, e % 2, p, :], in_=st_sb[:, sl, :]),
                 R=[bst[sl]], W=[bwb[e % 2][p]])

        for P in range(3):
            piece_dma(P)
        for P in range(6):
            piece_cast(P)
            piece_dma(P + 3)
        gi = 0
        n = 512
        pend = [6 * 1 + i for i in range(6)]
        for vx in range(NE * nh):
            half, ex = divmod(vx, NE)
            a = vx % 2
            pend = [6 * (vx + 1) + i for i in range(6)]
            hb0 = half * nblk
            nb_h = min(nblk, nblk_all - hb0)
            if ex == 0:
                for bi in range(nb_h):
                    s.dma("act", hl2_sb[:, :, bi * 512:(bi + 1) * 512], hl2[:, :, (hb0 + bi) * 512:(hb0 + bi + 1) * 512],
                          W=[bhl2[bi]])
            for bi in range(nb_h):
                t0 = bi * 512
                tg = (hb0 + bi) * 512
                npc = (6 + nb_h - 1) // nb_h
                for P in pend[bi * npc:(bi + 1) * npc]:
                    piece_cast(P)
                    piece_dma(P + 3)
                wr = gi % 2
                gs = gi % 2
                gi += 1
                mm(s, ps[:, 6, 0:n], sel_sb[:, ex * 128:(ex + 1) * 128], wdt_sb[:, 0, tg:tg + n], True, False,
                   R=[bsel, bwdt], W=[bps[6]])
                mm(s, ps[:, 6, 0:n], sel_sb[:, ex * 128:(ex + 1) * 128], wdt_sb[:, 1, tg:tg + n], False, True,
                   R=[bsel, bwdt], W=[bps[6]])
                s.op("act", lambda e: e.copy(out=wbc_sb[:, wr, 0:n], in_=ps[:, 6, 0:n]), R=[bps[6]], W=[bwbc[wr]])
                for fc in range(4):
                    pr = fc % 2
                    for which in range(2):
                        bank = 2 * pr + which
                        for k in range(8):
                            wv = wb_sb[:, a, 2 * which + k // 4, :].rearrange("q (a b) -> q a b", a=4)
                            mm(s, ps[:, bank, 0:n], wv[:, k % 4, fc * 128:(fc + 1) * 128], hl2_sb[:, k, t0:t0 + n],
                               k == 0, k == 7, R=[bwb[a][2 * which + k // 4], bhl2[bi]], W=[bps[bank]])
                    s.op("act", lambda e: e.activation(out=sg_sb[:, pr, 0:n], in_=ps[:, 2 * pr, 0:n], func=AF.Silu),
                         R=[bps[2 * pr]], W=[bsg[pr]])
                    s.op("dve", lambda e: e.tensor_tensor(out=t3_sb[:, pr, 0:n], in0=ps[:, 2 * pr + 1, 0:n],
                                                          in1=sg_sb[:, pr, 0:n], op=ALU.mult),
                         R=[bps[2 * pr + 1], bsg[pr]], W=[bt3[pr]])
                    s.op("pool", lambda e: e.tensor_tensor(out=g_sb[:, gs, fc, 0:n], in0=t3_sb[:, pr, 0:n],
                                                           in1=wbc_sb[:, wr, 0:n], op=ALU.mult),
                         R=[bt3[pr], bwbc[wr]], W=[bg[gs]])
                for dc in range(8):
                    bank = 4 + dc % 2
                    for fc in range(4):
                        wv = wb_sb[:, a, 4 + fc // 2, :].rearrange("q (a b) -> q a b", a=2)
                        mm(s, ps[:, bank, 0:n], wv[:, fc % 2, dc * 128:(dc + 1) * 128], g_sb[:, gs, fc, 0:n],
                           fc == 0, fc == 3, R=[bwb[a][4 + fc // 2], bg[gs]], W=[bps[bank]])
                    if ex == 0:
                        s.op("dve", lambda e: e.tensor_copy(out=y_sb[:, dc, t0:t0 + n], in_=ps[:, bank, 0:n]),
                             R=[bps[bank]], W=[by[bi]])
                    else:
                        s.op("dve", lambda e: e.tensor_tensor(out=y_sb[:, dc, t0:t0 + n], in0=ps[:, bank, 0:n],
                                                              in1=y_sb[:, dc, t0:t0 + n], op=ALU.add),
                             R=[bps[bank], by[bi]], W=[by[bi]])
            if ex == NE - 1:
                for bi in range(nb_h):
                    s.dma("sp", yo[:, :, (hb0 + bi) * 512:(hb0 + bi + 1) * 512], y_sb[:, :, bi * 512:(bi + 1) * 512],
                          R=[by[bi]])
        s.finish(by)
    return nc


def build_pc(final):
    nc = bass.Bass("TRN2", target_bir_lowering=False)
    din = lambda n, shp, dt: nc.dram_tensor(n, shp, dt, kind="ExternalInput").ap()
    xT = din("xT", [128, 8, NT1], F32)
    yT = din("yT", [128, 8, NT1], F32)
    mod = din("mod", [128, 3, 8], F32)
    xo = nc.dram_tensor("xo", [128, 8, NT1], F32, kind="ExternalOutput").ap()
    s = Sched(nc)
    with (nc.psum_tensor("ps", [128, 2, 512], F32) as ps,
          nc.sbuf_tensor("x_sb", [128, 8, NT1], F32) as x_sb,
          nc.sbuf_tensor("y_sb", [128, 8, NT1], F32) as y_sb,
          nc.sbuf_tensor("mod_sb", [128, 3, 8], F32) as mod_sb,
          nc.sbuf_tensor("ones_sb", [128, 128], BF16) as ones_sb,
          nc.sbuf_tensor("sq_sb", [128, 8, 512], BF16) as sq_sb,
          nc.sbuf_tensor("rstd_sb", [128, 2, 512], F32) as rstd_sb):
        bxb = [s.buf(f"x{i}", s.dsem(f"x{i}")) for i in range(len(BLKS1))]
        byb = [s.buf(f"y{i}", s.dsem(f"yy{i}")) for i in range(len(BLKS1))]
        bmod = s.buf("mod", s.dsem("mod"))
        bones = s.buf("ones"); bsq = s.buf("sq"); brs = [s.buf(f"rs{i}") for i in range(2)]
        bps = [s.buf(f"ps{i}") for i in range(2)]
        s.dma("sp", mod_sb[:], mod, W=[bmod])
        s.op("pool", lambda e: e.memset(ones_sb[:], 1.0), W=[bones])
        for bi, (t0, n) in enumerate(BLKS1):
            s.dma("sp", x_sb[:, :, t0:t0 + n], xT[:, :, t0:t0 + n], W=[bxb[bi]])
            s.dma("act", y_sb[:, :, t0:t0 + n], yT[:, :, t0:t0 + n], W=[byb[bi]])
        for bi, (t0, n) in enumerate(BLKS1):
            garow = 1 if bi == 4 else 0
            sl = bi % 2
            for k in range(8):
                s.op("dve", lambda e: e.scalar_tensor_tensor(
                    out=x_sb[:, k, t0:t0 + n], in0=y_sb[:, k, t0:t0 + n], scalar=mod_sb[:, garow, k:k + 1],
                    in1=x_sb[:, k, t0:t0 + n], op0=ALU.mult, op1=ALU.add),
                    R=[byb[bi], bmod, bxb[bi]], W=[bxb[bi]])
            if final:
                s.op("act", lambda e: e.activation(out=sq_sb[:, :, 0:n], in_=x_sb[:, :, t0:t0 + n], func=AF.Square),
                     R=[bxb[bi]], W=[bsq])
                for k in range(8):
                    mm(s, ps[:, sl, 0:n], ones_sb[:], sq_sb[:, k, 0:n], k == 0, k == 7, R=[bones, bsq], W=[bps[sl]])
                s.op("act", lambda e: e.activation(out=rstd_sb[:, sl, 0:n], in_=ps[:, sl, 0:n], func=AF.Sqrt,
                                                   scale=1.0 / D, bias=EPS), R=[bps[sl]], W=[brs[sl]])
                s.op("dve", lambda e: e.reciprocal(out=rstd_sb[:, sl, 0:n], in_=rstd_sb[:, sl, 0:n]),
                     R=[brs[sl]], W=[brs[sl]])
                for k in range(8):
                    s.op("dve", lambda e: e.scalar_tensor_tensor(
                        out=x_sb[:, k, t0:t0 + n], in0=x_sb[:, k, t0:t0 + n], scalar=mod_sb[:, 2, k:k + 1],
                        in1=rstd_sb[:, sl, 0:n], op0=ALU.mult, op1=ALU.mult),
                        R=[bxb[bi], bmod, brs[sl]], W=[bxb[bi]])
            s.dma("sp", xo[:, :, t0:t0 + n], x_sb[:, :, t0:t0 + n], R=[bxb[bi]])
        s.finish(bxb)
    return nc


def run_p3(nc3, l, xl, xc, yna, ydf, hf, hb, fmf, mods_l, inp, g_final):
    wge = np.concatenate([inp["router_w_group"][l], inp["router_w_expert"][l]], axis=1)
    wge = np.ascontiguousarray(wge.reshape(8, 128, 36).transpose(1, 0, 2))
    bge = np.concatenate([inp["router_b_group"][l], inp["router_b_expert"][l]])
    bge = np.ascontiguousarray(np.tile(bge[None, :], (128, 1))).astype(np.float32)
    selc = np.zeros((32, NEXP, 128), np.float32)
    for e in range(NEXP):
        selc[e, e, :] = 1.0
    selc = selc.reshape(32, NEXP * 128).astype(ml_dtypes.bfloat16)
    ident = np.eye(128, dtype=np.float32)
    in_maps = []
    for i in range(NCORES):
        b, j = i // 4, i % 4
        lat = slice(2048 * j, 2048 * (j + 1))
        ctxs = slice(S + 64 * j, S + 64 * (j + 1))
        xx = np.concatenate([xl[b, lat], xc[b, 64 * j:64 * (j + 1)]], axis=0)
        na = np.concatenate([yna[b, lat], yna[b, ctxs]], axis=0)
        df = np.concatenate([ydf[b, lat], ydf[b, ctxs]], axis=0)
        nadf = np.concatenate([na, df], axis=1)
        nadf = np.ascontiguousarray(nadf.T.reshape(4, 128, NT1).transpose(1, 0, 2))
        def tk(a):
            aa = np.concatenate([a[:, lat], a[:, ctxs]], axis=1)
            return aa.reshape(4, 128, NT1).transpose(1, 0, 2)
        hgt = np.ascontiguousarray(np.concatenate([tk(hf[b]), tk(hb[b]), tk(fmf[b, 512:1024])], axis=1))
        m = mods_l
        rows = [inp["g_ffn"][l], m[b, 2048:3072], m[b, 4096:5120], m[b, 3072:4096], m[b, 5120:6144],
                m[2, 2048:3072], m[2, 4096:5120], m[2, 3072:4096], m[2, 5120:6144], g_final]
        mod = np.ascontiguousarray(np.stack([vec_pk(r) for r in rows], axis=1)).astype(np.float32)
        in_maps.append({"xT": chunkT(xx), "nadf": nadf, "hg": hgt, "wout": inp["w_out"][l], "mod": mod,
                        "wge": wge, "bge": bge, "selc": selc, "ident": ident,
                        "w1": inp["moe_w1"][l], "w3": inp["moe_w3"][l], "w2": inp["moe_w2"][l]})
    res = run_bass_kernel_spmd(nc3, in_maps, core_ids=list(range(NCORES)))
    xl2 = np.zeros_like(xl); xc2 = np.zeros_like(xc)
    for i in range(NCORES):
        b, j = i // 4, i % 4
        o = res.results[i]["xo"].transpose(1, 0, 2).reshape(D, NT1).T
        xl2[b, 2048 * j:2048 * (j + 1)] = o[:2048]
        xc2[b, 64 * j:64 * (j + 1)] = o[2048:]
    return xl2, xc2


def build_p3e(caps):
    NE = 4
    captot = sum(caps)
    nc = bass.Bass("TRN2", target_bir_lowering=False)
    din = lambda n, shp, dt: nc.dram_tensor(n, shp, dt, kind="ExternalInput").ap()
    xs = din("xs", [128, 8, captot], BF16)
    w1 = din("w1", [NE, D, 512], F32)
    w3 = din("w3", [NE, D, 512], F32)
    w2 = din("w2", [NE, 512, D], F32)
    yo = nc.dram_tensor("yo", [128, 8, captot], F32, kind="ExternalOutput").ap()
    s = Sched(nc)
    with (nc.psum_tensor("ps", [128, 8, 512], F32) as ps,
          nc.sbuf_tensor("x_sb", [128, 2, 8, 512], BF16) as x_sb,
          nc.sbuf_tensor("y_sb", [128, 2, 8, 512], F32) as y_sb,
          nc.sbuf_tensor("s3_st", [128, 3, 2048], F32) as st_sb,
          nc.sbuf_tensor("s3_wb", [128, 2, 6, 2048], BF16) as wb_sb,
          nc.sbuf_tensor("s3_sg", [128, 2, 512], F32) as sg_sb,
          nc.sbuf_tensor("s3_g", [128, 2, 4, 512], BF16) as g_sb):
        bps = [s.buf(f"ps{i}") for i in range(8)]
        bx = [s.buf(f"x{i}", s.dsem(f"x{i}")) for i in range(2)]
        by = [s.buf(f"y{i}", s.dsem(f"y{i}")) for i in range(2)]
        bst = [s.buf(f"st{i}", s.dsem(f"st{i}")) for i in range(3)]
        bwb = [[s.buf(f"wb{a}_{p}") for p in range(6)] for a in range(2)]
        bsg = [s.buf(f"sg{i}") for i in range(2)]
        bg = [s.buf(f"g{i}") for i in range(2)]
        w1v = w1.rearrange("e (kc p) f -> e p kc f", p=128)
        w3v = w3.rearrange("e (kc p) f -> e p kc f", p=128)
        w2v = w2.rearrange("e (fc p) d -> e p fc d", p=128)

        def piece_src(e, p):
            if p < 2:
                return w1v[e, :, 4 * p:4 * p + 4, :]
            if p < 4:
                return w3v[e, :, 4 * (p - 2):4 * (p - 2) + 4, :]
            return w2v[e, :, 2 * (p - 4):2 * (p - 4) + 2, :]

        def piece_dma(P):
            e, p = divmod(P, 6)
            if e >= NE:
                return
            sl = P % 3
            dst = st_sb[:, sl, :]
            dst = dst.rearrange("q (a b) -> q a b", a=4) if p < 4 else dst.rearrange("q (a b) -> q a b", a=2)
            s.dma("sp", dst, piece_src(e, p), W=[bst[sl]])

        def piece_cast(P):
            e, p = divmod(P, 6)
            if e >= NE:
                return
            sl = P % 3
            s.op("pool", lambda en: en.tensor_copy(out=wb_sb[:, e % 2, p, :], in_=st_sb[:, sl, :]),
                 R=[bst[sl]], W=[bwb[e % 2][p]])

        for P in range(3):
            piece_dma(P)
        for P in range(6):
            piece_cast(P)
            piece_dma(P + 3)
        gi = 0
        seg0 = 0
        for ex in range(NE):
            a = ex % 2
            pend = [6 * (ex + 1) + i for i in range(6)]
            blocks = [(seg0 + t, min(512, caps[ex] - t)) for t in range(0, caps[ex], 512)]
            seg0 += caps[ex]
            nb = len(blocks)
            npc = (6 + nb - 1) // nb
            for bi, (t0, n) in enumerate(blocks):
                for P in pend[bi * npc:(bi + 1) * npc]:
                    piece_cast(P)
                    piece_dma(P + 3)
                xs_ = gi % 2
                gs = gi % 2
                gi += 1
                s.dma("act", x_sb[:, xs_, :, 0:n], xs[:, :, t0:t0 + n], W=[bx[xs_]])
                for fc in range(4):
                    pr = fc % 2
                    for which in range(2):
                        bank = 2 * pr + which
                        for k in range(8):
                            wv = wb_sb[:, a, 2 * which + k // 4, :].rearrange("q (a b) -> q a b", a=4)
                            mm(s, ps[:, bank, 0:n], wv[:, k % 4, fc * 128:(fc + 1) * 128], x_sb[:, xs_, k, 0:n],
                               k == 0, k == 7, R=[bwb[a][2 * which + k // 4], bx[xs_]], W=[bps[bank]])
                    s.op("act", lambda e: e.activation(out=sg_sb[:, pr, 0:n], in_=ps[:, 2 * pr, 0:n], func=AF.Silu),
                         R=[bps[2 * pr]], W=[bsg[pr]])
                    s.op("dve", lambda e: e.tensor_tensor(out=g_sb[:, gs, fc, 0:n], in0=ps[:, 2 * pr + 1, 0:n],
                                                          in1=sg_sb[:, pr, 0:n], op=ALU.mult),
                         R=[bps[2 * pr + 1], bsg[pr]], W=[bg[gs]])
                for dc in range(8):
                    bank = 4 + dc % 4
                    for fc in range(4):
                        wv = wb_sb[:, a, 4 + fc // 2, :].rearrange("q (a b) -> q a b", a=2)
                        mm(s, ps[:, bank, 0:n], wv[:, fc % 2, dc * 128:(dc + 1) * 128], g_sb[:, gs, fc, 0:n],
                           fc == 0, fc == 3, R=[bwb[a][4 + fc // 2], bg[gs]], W=[bps[bank]])
                    if dc % 2 == 0:
                        s.op("dve", lambda e: e.tensor_copy(out=y_sb[:, xs_, dc, 0:n], in_=ps[:, bank, 0:n]),
                             R=[bps[bank]], W=[by[xs_]])
                    else:
                        s.op("act", lambda e: e.copy(out=y_sb[:, xs_, dc, 0:n], in_=ps[:, bank, 0:n]),
                             R=[bps[bank]], W=[by[xs_]])
                s.dma("sp", yo[:, :, t0:t0 + n], y_sb[:, xs_, :, 0:n], R=[by[xs_]])
        s.finish(by)
    return nc


def build_pc2(final):
    nc = bass.Bass("TRN2", target_bir_lowering=False)
    din = lambda n, shp, dt: nc.dram_tensor(n, shp, dt, kind="ExternalInput").ap()
    xT = din("xT", [128, 8, NT1], F32)
    yA = din("yA", [128, 8, NT1], F32)
    yB = din("yB", [128, 8, NT1], F32)
    wab = din("wab", [128, 2, NT1], F32)
    mod = din("mod", [128, 3, 8], F32)
    xo = nc.dram_tensor("xo", [128, 8, NT1], F32, kind="ExternalOutput").ap()
    s = Sched(nc)
    with (nc.psum_tensor("ps", [128, 2, 512], F32) as ps,
          nc.sbuf_tensor("x_sb", [128, 8, NT1], F32) as x_sb,
          nc.sbuf_tensor("ya_sb", [128, 2, 8, 512], F32) as ya_sb,
          nc.sbuf_tensor("yb_sb", [128, 2, 8, 512], F32) as yb_sb,
          nc.sbuf_tensor("wab_sb", [128, 2, NT1], F32) as wab_sb,
          nc.sbuf_tensor("mod_sb", [128, 3, 8], F32) as mod_sb,
          nc.sbuf_tensor("ones_sb", [128, 128], BF16) as ones_sb,
          nc.sbuf_tensor("sq_sb", [128, 8, 512], BF16) as sq_sb,
          nc.sbuf_tensor("rstd_sb", [128, 2, 512], F32) as rstd_sb):
        bxb = [s.buf(f"x{i}", s.dsem(f"x{i}")) for i in range(len(BLKS1))]
        bya = [s.buf(f"ya{i}", s.dsem(f"ya{i}")) for i in range(2)]
        byb = [s.buf(f"yb{i}", s.dsem(f"yb{i}")) for i in range(2)]
        bmod = s.buf("mod", s.dsem("mod"))
        bwab = s.buf("wab", bmod.dsem)
        bones = s.buf("ones"); bsq = s.buf("sq"); brs = [s.buf(f"rs{i}") for i in range(2)]
        bps = [s.buf(f"ps{i}") for i in range(2)]
        s.dma("sp", mod_sb[:], mod, W=[bmod])
        s.dma("sp", wab_sb[:], wab, W=[bwab])
        s.op("pool", lambda e: e.memset(ones_sb[:], 1.0), W=[bones])
        for bi, (t0, n) in enumerate(BLKS1):
            s.dma("sp", x_sb[:, :, t0:t0 + n], xT[:, :, t0:t0 + n], W=[bxb[bi]])
        for bi, (t0, n) in enumerate(BLKS1):
            garow = 1 if bi == 4 else 0
            sl = bi % 2
            s.dma("act", ya_sb[:, sl, :, 0:n], yA[:, :, t0:t0 + n], W=[bya[sl]])
            s.dma("act", yb_sb[:, sl, :, 0:n], yB[:, :, t0:t0 + n], W=[byb[sl]])
            for k in range(8):
                s.op("pool", lambda e: e.tensor_tensor(out=ya_sb[:, sl, k, 0:n], in0=ya_sb[:, sl, k, 0:n],
                                                       in1=wab_sb[:, 0, t0:t0 + n], op=ALU.mult),
                     R=[bya[sl], bwab], W=[bya[sl]])
                s.op("dve", lambda e: e.tensor_tensor(out=yb_sb[:, sl, k, 0:n], in0=yb_sb[:, sl, k, 0:n],
                                                      in1=wab_sb[:, 1, t0:t0 + n], op=ALU.mult),
                     R=[byb[sl], bwab], W=[byb[sl]])
                s.op("dve", lambda e: e.tensor_tensor(out=ya_sb[:, sl, k, 0:n], in0=ya_sb[:, sl, k, 0:n],
                                                      in1=yb_sb[:, sl, k, 0:n], op=ALU.add),
                     R=[bya[sl], byb[sl]], W=[bya[sl]])
                s.op("dve", lambda e: e.scalar_tensor_tensor(
                    out=x_sb[:, k, t0:t0 + n], in0=ya_sb[:, sl, k, 0:n], scalar=mod_sb[:, garow, k:k + 1],
                    in1=x_sb[:, k, t0:t0 + n], op0=ALU.mult, op1=ALU.add),
                    R=[bya[sl], bmod, bxb[bi]], W=[bxb[bi]])
            if final:
                s.op("act", lambda e: e.activation(out=sq_sb[:, :, 0:n], in_=x_sb[:, :, t0:t0 + n], func=AF.Square),
                     R=[bxb[bi]], W=[bsq])
                for k in range(8):
                    mm(s, ps[:, sl, 0:n], ones_sb[:], sq_sb[:, k, 0:n], k == 0, k == 7, R=[bones, bsq], W=[bps[sl]])
                s.op("act", lambda e: e.activation(out=rstd_sb[:, sl, 0:n], in_=ps[:, sl, 0:n], func=AF.Sqrt,
                                                   scale=1.0 / D, bias=EPS), R=[bps[sl]], W=[brs[sl]])
                s.op("dve", lambda e: e.reciprocal(out=rstd_sb[:, sl, 0:n], in_=rstd_sb[:, sl, 0:n]),
                     R=[brs[sl]], W=[brs[sl]])
                for k in range(8):
                    s.op("dve", lambda e: e.scalar_tensor_tensor(
                        out=x_sb[:, k, t0:t0 + n], in0=x_sb[:, k, t0:t0 + n], scalar=mod_sb[:, 2, k:k + 1],
                        in1=rstd_sb[:, sl, 0:n], op0=ALU.mult, op1=ALU.mult),
                        R=[bxb[bi], bmod, brs[sl]], W=[bxb[bi]])
            s.dma("sp", xo[:, :, t0:t0 + n], x_sb[:, :, t0:t0 + n], R=[bxb[bi]])
        s.finish(bxb)
    return nc


def run_p3_expert(l, xl, xc, yna, ydf, hf, hb, fmf, mods_l, inp, g_final, final):
    in_maps = p3_inmaps_common(l, xl, xc, yna, ydf, hf, hb, fmf, mods_l, inp, g_final)
    resa = run_bass_kernel_spmd(build_p3a(), in_maps, core_ids=list(range(NCORES))).results
    NTT = NCORES * NT1
    HL2 = np.zeros((D, NTT), ml_dtypes.bfloat16)
    WD = np.zeros((NTT, 32), np.float32)
    for i in range(NCORES):
        HL2[:, i * NT1:(i + 1) * NT1] = resa[i]["hl2o"].transpose(1, 0, 2).reshape(D, NT1)
        WD[i * NT1:(i + 1) * NT1] = resa[i]["wd32o"].transpose(1, 0, 2).reshape(17 * 128, 32)[:NT1]
    top2 = np.sort(np.argpartition(-WD, 1, axis=1)[:, :2], axis=1)
    eA, eB = top2[:, 0], top2[:, 1]
    ar = np.arange(NTT)
    wA = WD[ar, eA]; wB = WD[ar, eB]
    tokE = []
    for e in range(32):
        tokE.append(np.nonzero((eA == e) | (eB == e))[0])
    caps = []
    for sl in range(4):
        mx = max(len(tokE[4 * c + sl]) for c in range(NCORES))
        caps.append(max(128, int(-(-mx // 128) * 128)))
    captot = sum(caps)
    mapse = []
    for c in range(NCORES):
        xsa = np.zeros((D, captot), ml_dtypes.bfloat16)
        o = 0
        for sl in range(4):
            tk_ = tokE[4 * c + sl]
            xsa[:, o:o + len(tk_)] = HL2[:, tk_]
            o += caps[sl]
        mapse.append({"xs": np.ascontiguousarray(xsa.reshape(8, 128, captot).transpose(1, 0, 2)),
                      "w1": np.ascontiguousarray(inp["moe_w1"][l][4 * c:4 * c + 4]),
                      "w3": np.ascontiguousarray(inp["moe_w3"][l][4 * c:4 * c + 4]),
                      "w2": np.ascontiguousarray(inp["moe_w2"][l][4 * c:4 * c + 4])})
    rese = run_bass_kernel_spmd(build_p3e(caps), mapse, core_ids=list(range(NCORES))).results
    YA = np.zeros((D, NTT), np.float32)
    YB = np.zeros((D, NTT), np.float32)
    for c in range(NCORES):
        yy = rese[c]["yo"].transpose(1, 0, 2).reshape(D, captot)
        o = 0
        for sl in range(4):
            e = 4 * c + sl
            tk_ = tokE[e]
            cols = yy[:, o:o + len(tk_)]
            isA = eA[tk_] == e
            YA[:, tk_[isA]] = cols[:, isA]
            YB[:, tk_[~isA]] = cols[:, ~isA]
            o += caps[sl]
    mapsc = []
    for i in range(NCORES):
        b = i // 4
        sl_ = slice(i * NT1, (i + 1) * NT1)
        modc = np.stack([vec_pk(mods_l[b, 5120:6144]), vec_pk(mods_l[2, 5120:6144]), vec_pk(g_final)], axis=1)
        wab = np.stack([np.tile(wA[sl_][None, :], (128, 1)), np.tile(wB[sl_][None, :], (128, 1))], axis=1)
        mapsc.append({"xT": resa[i]["xo"], "mod": np.ascontiguousarray(modc).astype(np.float32),
                      "wab": np.ascontiguousarray(wab).astype(np.float32),
                      "yA": np.ascontiguousarray(YA[:, sl_].reshape(8, 128, NT1).transpose(1, 0, 2)),
                      "yB": np.ascontiguousarray(YB[:, sl_].reshape(8, 128, NT1).transpose(1, 0, 2))})
    resc = run_bass_kernel_spmd(build_pc2(final), mapsc, core_ids=list(range(NCORES))).results
    xl2 = np.zeros_like(xl); xc2 = np.zeros_like(xc)
    for i in range(NCORES):
        b, j = i // 4, i % 4
        o = resc[i]["xo"].transpose(1, 0, 2).reshape(D, NT1).T
        xl2[b, 2048 * j:2048 * (j + 1)] = o[:2048]
        xc2[b, 64 * j:64 * (j + 1)] = o[2048:]
    return xl2, xc2


def p3_inmaps_common(l, xl, xc, yna, ydf, hf, hb, fmf, mods_l, inp, g_final):
    wge = np.concatenate([inp["router_w_group"][l], inp["router_w_expert"][l]], axis=1)
    wge = np.ascontiguousarray(wge.reshape(8, 128, 36).transpose(1, 0, 2))
    bge = np.concatenate([inp["router_b_group"][l], inp["router_b_expert"][l]])
    bge = np.ascontiguousarray(np.tile(bge[None, :], (128, 1))).astype(np.float32)
    ident = np.eye(128, dtype=np.float32)
    iota4 = np.ascontiguousarray(np.tile(np.arange(4, dtype=np.float32)[None, :], (128, 1)))
    in_maps = []
    for i in range(NCORES):
        b, j = i // 4, i % 4
        lat = slice(2048 * j, 2048 * (j + 1))
        ctxs = slice(S + 64 * j, S + 64 * (j + 1))
        xx = np.concatenate([xl[b, lat], xc[b, 64 * j:64 * (j + 1)]], axis=0)
        na = np.concatenate([yna[b, lat], yna[b, ctxs]], axis=0)
        df = np.concatenate([ydf[b, lat], ydf[b, ctxs]], axis=0)
        nadf = np.concatenate([na, df], axis=1)
        nadf = np.ascontiguousarray(nadf.T.reshape(4, 128, NT1).transpose(1, 0, 2))

        def tk(a):
            aa = np.concatenate([a[:, lat], a[:, ctxs]], axis=1)
            return aa.reshape(4, 128, NT1).transpose(1, 0, 2)
        hgt = np.ascontiguousarray(np.concatenate([tk(hf[b]), tk(hb[b]), tk(fmf[b, 512:1024])], axis=1))
        m = mods_l
        rows = [inp["g_ffn"][l], m[b, 2048:3072], m[b, 4096:5120], m[b, 3072:4096], m[b, 5120:6144],
                m[2, 2048:3072], m[2, 4096:5120], m[2, 3072:4096], m[2, 5120:6144], g_final]
        mod = np.ascontiguousarray(np.stack([vec_pk(r) for r in rows], axis=1)).astype(np.float32)
        in_maps.append({"xT": chunkT(xx), "nadf": nadf, "hg": hgt, "wout": inp["w_out"][l], "mod": mod,
                        "wge": wge, "bge": bge, "ident": ident, "iota4": iota4})
    return in_maps


def run_p3_sparse(l, xl, xc, yna, ydf, hf, hb, fmf, mods_l, inp, g_final, final):
    in_maps = p3_inmaps_common(l, xl, xc, yna, ydf, hf, hb, fmf, mods_l, inp, g_final)
    resa = run_bass_kernel_spmd(build_p3a(), in_maps, core_ids=list(range(NCORES))).results
    NTT = NCORES * NT1
    HL2 = np.zeros((D, NTT), ml_dtypes.bfloat16)
    WDT = np.zeros((32, 2, NTT), ml_dtypes.bfloat16)
    gid = np.zeros(NTT, np.int64)
    for i in range(NCORES):
        HL2[:, i * NT1:(i + 1) * NT1] = resa[i]["hl2o"].transpose(1, 0, 2).reshape(D, NT1)
        WDT[:, :, i * NT1:(i + 1) * NT1] = resa[i]["wdto"]
        g = resa[i]["gido"]
        gid[i * NT1:(i + 1) * NT1] = np.rint(g.T.reshape(-1)[:NT1]).astype(np.int64)
    toks = [np.nonzero(gid == g)[0] for g in range(4)]
    ncg = [1, 1, 1, 1]
    for _ in range(NCORES - 4):
        gbig = max(range(4), key=lambda g: len(toks[g]) / ncg[g])
        ncg[gbig] += 1
    idxs = []
    cgroup = []
    for g in range(4):
        parts = np.array_split(toks[g], ncg[g])
        for pp in parts:
            idxs.append(pp); cgroup.append(g)
    ntb = max(512, int(-(-max(len(ix) for ix in idxs) // 512) * 512))
    sel8 = np.zeros((8, 8, 128), np.float32)
    for e in range(8):
        sel8[e, e, :] = 1.0
    sel8 = sel8.reshape(8, 1024).astype(ml_dtypes.bfloat16)
    mapsb = []
    for c in range(NCORES):
        g = cgroup[c]
        ix = idxs[c]
        h2 = np.zeros((D, ntb), ml_dtypes.bfloat16)
        h2[:, :len(ix)] = HL2[:, ix]
        wd = np.zeros((8, 2, ntb), ml_dtypes.bfloat16)
        wd[:, :, :len(ix)] = WDT[8 * g:8 * g + 8][:, :, ix]
        mapsb.append({"hl2": np.ascontiguousarray(h2.reshape(8, 128, ntb).transpose(1, 0, 2)), "wdt": wd, "selc": sel8,
                      "w1": np.ascontiguousarray(inp["moe_w1"][l][8 * g:8 * g + 8]),
                      "w3": np.ascontiguousarray(inp["moe_w3"][l][8 * g:8 * g + 8]),
                      "w2": np.ascontiguousarray(inp["moe_w2"][l][8 * g:8 * g + 8])})
    resb = run_bass_kernel_spmd(build_p3b(ntb), mapsb, core_ids=list(range(NCORES))).results
    Y = np.zeros((D, NTT), np.float32)
    for c in range(NCORES):
        ix = idxs[c]
        Y[:, ix] = resb[c]["yo"].transpose(1, 0, 2).reshape(D, ntb)[:, :len(ix)]
    mapsc = []
    for i in range(NCORES):
        b = i // 4
        modc = np.stack([vec_pk(mods_l[b, 5120:6144]), vec_pk(mods_l[2, 5120:6144]), vec_pk(g_final)], axis=1)
        mapsc.append({"xT": resa[i]["xo"], "mod": np.ascontiguousarray(modc).astype(np.float32),
                      "yT": np.ascontiguousarray(Y[:, i * NT1:(i + 1) * NT1].reshape(8, 128, NT1).transpose(1, 0, 2))})
    resc = run_bass_kernel_spmd(build_pc(final), mapsc, core_ids=list(range(NCORES))).results
    xl2 = np.zeros_like(xl); xc2 = np.zeros_like(xc)
    for i in range(NCORES):
        b, j = i // 4, i % 4
        o = resc[i]["xo"].transpose(1, 0, 2).reshape(D, NT1).T
        xl2[b, 2048 * j:2048 * (j + 1)] = o[:2048]
        xc2[b, 64 * j:64 * (j + 1)] = o[2048:]
    return xl2, xc2


def kernel(**inputs):
    inp = {k: np.asarray(v) for k, v in inputs.items()}
    x = np.ascontiguousarray(inp["x"], dtype=np.float32)
    ctx = np.ascontiguousarray(inp["ctx"], dtype=np.float32)
    mods = run_p0(inp["c"], inp["c_ctx"], inp["w_ada"], inp["b_ada"])
    cosT, sinT = rope_tables()
    xl, xc = x, ctx
    for l in range(DEPTH):
        fmb, fmf, tm = run_p1(build_p1(), xl, xc, mods[l], inp["g_mix"][l], inp["w_in"][l], cosT, sinT)
        lam_init = 0.8 - 0.6 * math.exp(-0.3 * l)
        yna, ydf, hf, hb = run_p2(build_p2(lam_init), l, fmb, fmf, tm, inp)
        xl, xc = run_p3_expert(l, xl, xc, yna, ydf, hf, hb, fmf, mods[l], inp, inp["g_final"], l == DEPTH - 1)
    return np.ascontiguousarray(xl, dtype=np.float32)
```

```python
import math
from contextlib import ExitStack
import numpy as np
import ml_dtypes
import concourse.bass as bass
import concourse.mybir as mybir
from concourse.bass_utils import run_bass_kernel_spmd

F32 = mybir.dt.float32
BF16 = mybir.dt.bfloat16
I32 = mybir.dt.int32
U32 = mybir.dt.uint32
AF = mybir.ActivationFunctionType
ALU = mybir.AluOpType
AX = mybir.AxisListType

NCORES = 8
D = 1024
B = 2
S = 8192
L = 256
DEPTH = 4
GRID_W = 64
EPS = 1e-6


class Buf:
    __slots__ = ("name", "w", "r", "dsem")

    def __init__(self, name, dsem=None):
        self.name = name
        self.w = None
        self.r = []
        self.dsem = dsem


class DmaSem:
    def __init__(self, sched, name):
        self.sem = sched.nc.alloc_semaphore(name)
        self.key = ("dma", name)
        self.total = 0
        sched.sems[self.key] = self


class Sched:
    def __init__(self, nc):
        self.nc = nc
        self.eng = {"pe": nc.tensor, "dve": nc.vector, "act": nc.scalar,
                    "pool": nc.gpsimd, "sp": nc.sync}
        self.sems = {}
        self.esem = {}
        self.cnt = {}
        for k in self.eng:
            self.esem[k] = nc.alloc_semaphore("e_" + k)
            self.cnt[k] = 0
        self.seen = {}
        self.nbuf = 0
        self.out_tokens = []

    def buf(self, name=None, dsem=None):
        self.nbuf += 1
        return Buf(name or f"b{self.nbuf}", dsem)

    def dsem(self, name):
        return DmaSem(self, name)

    def _semof(self, key):
        if key[0] == "dma":
            return self.sems[key].sem
        return self.esem[key[0]]

    def _wait(self, engname, deps):
        e = self.eng[engname]
        for key, val in deps.items():
            if key[0] == "dma":
                val = max(val, 0)
            if self.seen.get((engname, key), 0) >= val:
                continue
            self.seen[(engname, key)] = val
            e.wait_ge(self._semof(key), val)

    def _deps(self, R, W):
        deps = {}

        def add(tok):
            if tok is None:
                return
            key, val = tok
            if key[0] == "dma":
                val = self.sems[key].total
            if deps.get(key, 0) < val:
                deps[key] = val
        for b in R:
            add(b.w)
        for b in W:
            add(b.w)
            for t in b.r:
                add(t)
        return deps

    def _commit(self, tok, R, W):
        for b in R:
            b.r.append(tok)
        for b in W:
            b.w = tok
            b.r = []

    def op(self, engname, fn, R=(), W=()):
        deps = self._deps(R, W)
        if engname == "pe":
            deps.pop(("pe",), None)
        self._wait(engname, deps)
        ins = fn(self.eng[engname])
        self.cnt[engname] += 1
        ins.then_inc(self.esem[engname], 1)
        tok = ((engname,), self.cnt[engname])
        self._commit(tok, R, W)
        return tok

    def dma(self, q, out, in_, R=(), W=(), sem=None, **kw):
        deps = self._deps(R, W)
        self._wait(q, deps)
        ds = sem
        if ds is None:
            for b in list(W) + list(R):
                if b.dsem is not None:
                    ds = b.dsem
                    break
        assert ds is not None, "dma needs a DmaSem"
        ins = self.eng[q].dma_start(out=out, in_=in_, **kw)
        ds.total += 16
        ins.then_inc(ds.sem, 16)
        tok = (ds.key, ds.total)
        self._commit(tok, R, W)
        return tok

    def barrier(self, bufs=()):
        deps = {}
        for k in self.eng:
            if self.cnt[k]:
                deps[(k,)] = self.cnt[k]
        for key, ds in self.sems.items():
            if ds.total:
                deps[key] = ds.total
        for k in self.eng:
            d = {kk: v for kk, v in deps.items() if kk != (k,)}
            self._wait(k, d)

    def coll(self, kind, ins, outs, R=(), W=(), groups=None):
        deps = self._deps(R, W)
        self._wait("pool", deps)
        ds = None
        for b in list(W) + list(R):
            if b.dsem is not None:
                ds = b.dsem
                break
        g = groups or [[0, 1, 2, 3], [4, 5, 6, 7]]
        ins_ = self.nc.gpsimd.collective_compute(kind, ALU.bypass, replica_groups=g, ins=ins, outs=outs)
        ds.total += 16
        ins_.then_inc(ds.sem, 16)
        tok = (ds.key, ds.total)
        self._commit(tok, R, W)
        return tok

    def finish(self, bufs, engname="sp"):
        deps = {}
        for b in bufs:
            for tok in ([b.w] if b.w else []) + b.r:
                key, val = tok
                if key[0] == "dma":
                    val = self.sems[key].total
                deps[key] = max(deps.get(key, 0), val)
        self._wait(engname, deps)


def mm(s, out, lhsT, rhs, start, stop, R, W):
    return s.op("pe", lambda e: e.matmul(out, lhsT, rhs, start=start, stop=stop), R=R, W=W)


def build_p0():
    nc = bass.Bass("TRN2", target_bir_lowering=False)
    NCOL = 3072
    NJ = NCOL // 128
    cT = nc.dram_tensor("cT", [128, 8, 4], F32, kind="ExternalInput").ap()
    w = nc.dram_tensor("w", [D, NCOL], F32, kind="ExternalInput").ap()
    bvec = nc.dram_tensor("bvec", [128, NJ], F32, kind="ExternalInput").ap()
    out = nc.dram_tensor("out", [128, NJ, 4], F32, kind="ExternalOutput").ap()
    s = Sched(nc)
    wv = w.rearrange("(kc p) n -> p kc n", p=128)
    with (nc.sbuf_tensor("w_sb", [128, 8, NCOL], F32) as w_sb,
          nc.sbuf_tensor("c_sb", [128, 8, 4], F32) as c_sb,
          nc.sbuf_tensor("s_sb", [128, 8, 4], F32) as s_sb,
          nc.sbuf_tensor("b_sb", [128, NJ], F32) as b_sb,
          nc.sbuf_tensor("r_sb", [128, NJ, 4], F32) as r_sb,
          nc.psum_tensor("ps", [128, 8, 512], F32) as ps):
        bw = [s.buf(f"w{k}", s.dsem(f"w{k}")) for k in range(8)]
        bc = s.buf("c", s.dsem("c"))
        bb = s.buf("b", bc.dsem)
        bs = s.buf("s")
        br = s.buf("r", s.dsem("r"))
        bps = [s.buf(f"ps{i}") for i in range(8)]
        s.dma("sp", c_sb[:], cT, W=[bc])
        s.dma("sp", b_sb[:], bvec, W=[bb])
        for k in range(8):
            s.dma("sp" if k % 2 == 0 else "act", w_sb[:, k, :], wv[:, k, :], W=[bw[k]])
        s.op("act", lambda e: e.activation(out=s_sb[:], in_=c_sb[:], func=AF.Silu), R=[bc], W=[bs])
        for j in range(NJ):
            pb = bps[j % 8]
            for k in range(8):
                mm(s, ps[:, j % 8, 0:4], w_sb[:, k, j * 128:(j + 1) * 128], s_sb[:, k, :],
                   k == 0, k == 7, R=[bw[k], bs], W=[pb])
            s.op("dve", lambda e: e.tensor_scalar(out=r_sb[:, j, :], in0=ps[:, j % 8, 0:4],
                                                  scalar1=b_sb[:, j:j + 1], scalar2=None, op0=ALU.add),
                 R=[pb, bb], W=[br])
        s.dma("sp", out, r_sb[:], R=[br])
        s.finish([br])
    return nc


def silu_np_layout_c(c, c_ctx):
    cc = np.stack([c[0], c[1], c_ctx, c_ctx], axis=1)
    return np.ascontiguousarray(cc.reshape(8, 128, 4).transpose(1, 0, 2))


def run_p0(c, c_ctx, w_ada, b_ada):
    nc = build_p0()
    cT = silu_np_layout_c(c, c_ctx)
    in_maps = []
    for i in range(NCORES):
        l, h = i // 2, i % 2
        in_maps.append({
            "cT": cT,
            "w": np.ascontiguousarray(w_ada[l][:, h * 3072:(h + 1) * 3072]),
            "bvec": np.ascontiguousarray(b_ada[l][h * 3072:(h + 1) * 3072].reshape(24, 128).T),
        })
    res = run_bass_kernel_spmd(nc, in_maps, core_ids=list(range(NCORES)))
    mods = np.zeros((DEPTH, 3, 6 * D), np.float32)
    for i in range(NCORES):
        l, h = i // 2, i % 2
        o = res.results[i]["out"]
        m = o.transpose(1, 0, 2).reshape(3072, 4)
        mods[l, :, h * 3072:(h + 1) * 3072] = m[:, :3].T
    return mods


NT1 = 2112
NW1 = 3072
BLKS1 = [(0, 512), (512, 512), (1024, 512), (1536, 512), (2048, 64)]


def build_p1():
    nc = bass.Bass("TRN2", target_bir_lowering=False)
    xT = nc.dram_tensor("xT", [128, 8, NT1], F32, kind="ExternalInput").ap()
    w = nc.dram_tensor("w", [D, NW1], F32, kind="ExternalInput").ap()
    gsc = nc.dram_tensor("gsc", [128, 5, 8], F32, kind="ExternalInput").ap()
    cosT = nc.dram_tensor("cosT", [128, 2048], F32, kind="ExternalInput").ap()
    sinT = nc.dram_tensor("sinT", [128, 2048], F32, kind="ExternalInput").ap()
    fmb = nc.dram_tensor("fmb", [128, 8, NT1], BF16, kind="ExternalOutput").ap()
    fmf = nc.dram_tensor("fmf", [128, 8, NT1], F32, kind="ExternalOutput").ap()
    tm = nc.dram_tensor("tm", [NT1, 512], BF16, kind="ExternalOutput").ap()
    s = Sched(nc)
    wv = w.rearrange("(kc p) n -> p kc n", p=128)
    with (nc.sbuf_tensor("x_sb", [128, 2, 8, 512], F32) as x_sb,
          nc.sbuf_tensor("h_sb", [128, 8, NT1], BF16) as h_sb,
          nc.sbuf_tensor("w_bf", [128, 8, NW1], BF16) as w_bf,
          nc.sbuf_tensor("w_st", [128, 2, NW1], F32) as w_st,
          nc.sbuf_tensor("o_sb", [128, 4, 512], F32) as o_sb,
          nc.sbuf_tensor("ob_sb", [128, 4, 512], BF16) as ob_sb,
          nc.sbuf_tensor("cos_sb", [128, 2048], F32) as cos_sb,
          nc.sbuf_tensor("sin_sb", [128, 2048], F32) as sin_sb,
          nc.sbuf_tensor("gsc_sb", [128, 5, 8], F32) as gsc_sb,
          nc.sbuf_tensor("ab_sb", [128, 2, 8], F32) as ab_sb,
          nc.sbuf_tensor("sq_sb", [128, 8, 512], BF16) as sq_sb,
          nc.sbuf_tensor("ones_sb", [128, 128], BF16) as ones_sb,
          nc.sbuf_tensor("rstd_sb", [128, 2, 512], F32) as rstd_sb,
          nc.sbuf_tensor("tmp_sb", [128, 4, 512], F32) as tmp_sb,
          nc.psum_tensor("ps", [128, 8, 512], F32) as ps):
        bx = [s.buf(f"x{i}", s.dsem(f"x{i}")) for i in range(2)]
        bh = [s.buf(f"h{i}") for i in range(len(BLKS1))]
        bwst = [s.buf(f"wst{i}", s.dsem(f"wst{i}")) for i in range(2)]
        bwbf = [s.buf(f"wbf{k}") for k in range(8)]
        bo = [s.buf(f"o{i}", s.dsem(f"o{i}")) for i in range(4)]
        bob = [s.buf(f"ob{i}", s.dsem(f"ob{i}")) for i in range(4)]
        cs = s.dsem("const")
        bcos = s.buf("cos", cs); bsin = s.buf("sin", cs); bgsc = s.buf("gsc", cs)
        bab = s.buf("ab"); bsq = s.buf("sq"); bones = s.buf("ones")
        brs = [s.buf(f"rs{i}") for i in range(2)]
        btmp = [s.buf(f"tmp{i}") for i in range(4)]
        bps = [s.buf(f"ps{i}") for i in range(8)]

        s.dma("sp", gsc_sb[:], gsc, W=[bgsc])
        s.dma("sp", cos_sb[:], cosT, W=[bcos])
        s.dma("sp", sin_sb[:], sinT, W=[bsin])
        s.op("pool", lambda e: e.memset(ones_sb[:], 1.0), W=[bones])
        for t in range(2):
            s.op("dve", lambda e: e.scalar_tensor_tensor(
                out=ab_sb[:, t, :], in0=gsc_sb[:, 1 + 2 * t, :], scalar=1.0, in1=gsc_sb[:, 0, :],
                op0=ALU.add, op1=ALU.mult), R=[bgsc], W=[bab])
        for k in range(8):
            sl = k % 2
            s.dma("act", w_st[:, sl, :], wv[:, k, :], W=[bwst[sl]])
            s.op("pool", lambda e: e.tensor_copy(out=w_bf[:, k, :], in_=w_st[:, sl, :]),
                 R=[bwst[sl]], W=[bwbf[k]])
        for bi, (t0, n) in enumerate(BLKS1):
            sl = bi % 2
            isctx = bi == 4
            s.dma("sp", x_sb[:, sl, :, 0:n], xT[:, :, t0:t0 + n], W=[bx[sl]])
            s.op("act", lambda e: e.activation(out=sq_sb[:, :, 0:n], in_=x_sb[:, sl, :, 0:n], func=AF.Square),
                 R=[bx[sl]], W=[bsq])
            pst = bps[sl]
            for k in range(8):
                mm(s, ps[:, sl, 0:n], ones_sb[:], sq_sb[:, k, 0:n], k == 0, k == 7, R=[bones, bsq], W=[pst])
            s.op("act", lambda e: e.activation(out=rstd_sb[:, sl, 0:n], in_=ps[:, sl, 0:n], func=AF.Sqrt,
                                               scale=1.0 / D, bias=EPS), R=[pst], W=[brs[sl]])
            s.op("dve", lambda e: e.reciprocal(out=rstd_sb[:, sl, 0:n], in_=rstd_sb[:, sl, 0:n]),
                 R=[brs[sl]], W=[brs[sl]])
            ai = 1 if isctx else 0
            shrow = 4 if isctx else 2
            for k in range(8):
                tb = k % 4
                s.op("dve", lambda e: e.scalar_tensor_tensor(
                    out=tmp_sb[:, tb, 0:n], in0=x_sb[:, sl, k, 0:n], scalar=ab_sb[:, ai, k:k + 1],
                    in1=rstd_sb[:, sl, 0:n], op0=ALU.mult, op1=ALU.mult),
                    R=[bx[sl], bab, brs[sl]], W=[btmp[tb]])
                s.op("act", lambda e: e.activation(out=h_sb[:, k, t0:t0 + n], in_=tmp_sb[:, tb, 0:n],
                                                   func=AF.Identity, bias=gsc_sb[:, shrow, k:k + 1], scale=1.0),
                     R=[btmp[tb], bgsc], W=[bh[bi]])
        oi = 0
        obi = 0
        pi = 0
        for bi, (t0, n) in enumerate(BLKS1):
            isctx = bi == 4
            for c in range(16):
                rope = (c >= 12) and not isctx
                isb = c < 4 or c >= 12
                dst = (fmb[:, c if c < 4 else c - 8, t0:t0 + n]) if isb else fmf[:, c - 4, t0:t0 + n]
                pA = 2 + (pi % 6); pi += 1
                for k in range(8):
                    mm(s, ps[:, pA, 0:n], w_bf[:, k, c * 128:(c + 1) * 128], h_sb[:, k, t0:t0 + n],
                       k == 0, k == 7, R=[bwbf[k], bh[bi]], W=[bps[pA]])
                if isb:
                    ob = obi % 4; obi += 1
                    osl = ob_sb[:, ob, 0:n]; obuf = bob[ob]
                else:
                    ob = oi % 4; oi += 1
                    osl = o_sb[:, ob, 0:n]; obuf = bo[ob]
                if not rope:
                    if c % 2 == 0:
                        s.op("act", lambda e: e.copy(out=osl, in_=ps[:, pA, 0:n]), R=[bps[pA]], W=[obuf])
                    else:
                        s.op("dve", lambda e: e.tensor_copy(out=osl, in_=ps[:, pA, 0:n]), R=[bps[pA]], W=[obuf])
                else:
                    pB = 2 + (pi % 6); pi += 1
                    c2 = c + 4
                    for k in range(8):
                        mm(s, ps[:, pB, 0:n], w_bf[:, k, c2 * 128:(c2 + 1) * 128], h_sb[:, k, t0:t0 + n],
                           k == 0, k == 7, R=[bwbf[k], bh[bi]], W=[bps[pB]])
                    s.op("dve", lambda e: e.tensor_tensor(out=tmp_sb[:, 0, 0:n], in0=ps[:, pA, 0:n],
                                                          in1=cos_sb[:, t0:t0 + n], op=ALU.mult),
                         R=[bps[pA], bcos], W=[btmp[0]])
                    s.op("dve", lambda e: e.tensor_tensor(out=tmp_sb[:, 1, 0:n], in0=ps[:, pB, 0:n],
                                                          in1=sin_sb[:, t0:t0 + n], op=ALU.mult),
                         R=[bps[pB], bsin], W=[btmp[1]])
                    s.op("pool", lambda e: e.tensor_tensor(out=osl, in0=tmp_sb[:, 0, 0:n],
                                                           in1=tmp_sb[:, 1, 0:n], op=ALU.add),
                         R=[btmp[0], btmp[1]], W=[obuf])
                s.dma("sp", dst, osl, R=[obuf])
        ntile = NT1 // 128 + 1
        for ti in range(ntile):
            t0 = ti * 128
            n = min(128, NT1 - t0)
            bi = min(t0 // 512, 4)
            pA = 2 + (pi % 6); pi += 1
            for k in range(8):
                mm(s, ps[0:n, pA, :], h_sb[:, k, t0:t0 + n], w_bf[:, k, 2560:3072],
                   k == 0, k == 7, R=[bwbf[k], bh[bi]], W=[bps[pA]])
            ob = obi % 4; obi += 1
            s.op("act", lambda e: e.copy(out=ob_sb[0:n, ob, :], in_=ps[0:n, pA, :]), R=[bps[pA]], W=[bob[ob]])
            s.dma("sp", tm[t0:t0 + n, :], ob_sb[0:n, ob, :], R=[bob[ob]])
        s.finish(bo + bob)
    return nc


def rope_tables():
    t = np.arange(S)
    row = (t // GRID_W).astype(np.float32)
    col = (t % GRID_W).astype(np.float32)
    inv = (10000.0 ** (-np.arange(0, 16, 2, dtype=np.float32) / 16.0)).astype(np.float32)
    ang_r = row[:, None] * inv
    ang_c = col[:, None] * inv
    cosT = np.zeros((32, S), np.float32)
    sinT = np.zeros((32, S), np.float32)
    for d in range(32):
        ang = ang_r if d < 16 else ang_c
        i = d % 8
        cosT[d] = np.cos(ang[:, i])
        sgn = -1.0 if (d % 16) < 8 else 1.0
        sinT[d] = sgn * np.sin(ang[:, i])
    return np.tile(cosT, (4, 1)), np.tile(sinT, (4, 1))


def p1_wcols():
    sw = np.array([(d + 8) if (d % 16) < 8 else (d - 8) for d in range(32)])
    f = np.arange(256)
    swf = (f // 32) * 32 + sw[f % 32]
    cols = np.concatenate([np.arange(0, 256), np.arange(256, 512), np.arange(768, 1280), np.arange(1280, 1792),
                           np.arange(1792, 2048), np.arange(2048, 2304), 1792 + swf, 2048 + swf,
                           np.arange(512, 768), np.arange(2304, 2560)])
    return cols


def chunkT(a):
    T = a.shape[0]
    return np.ascontiguousarray(a.T.reshape(8, 128, T).transpose(1, 0, 2))


def vec_pk(v):
    return np.ascontiguousarray(v.reshape(8, 128).T)


def run_p1(nc1, xl, xc, mods_l, g_mix_l, w_in_l, cosT, sinT):
    wl = np.ascontiguousarray(w_in_l[:, p1_wcols()])
    in_maps = []
    for i in range(NCORES):
        b, j = i // 4, i % 4
        xx = np.concatenate([xl[b, 2048 * j:2048 * (j + 1)], xc[b, 64 * j:64 * (j + 1)]], axis=0)
        gsc = np.stack([vec_pk(g_mix_l), vec_pk(mods_l[b, 1024:2048]), vec_pk(mods_l[b, 0:1024]),
                        vec_pk(mods_l[2, 1024:2048]), vec_pk(mods_l[2, 0:1024])], axis=1)
        in_maps.append({"xT": chunkT(xx), "w": wl, "gsc": np.ascontiguousarray(gsc),
                        "cosT": np.ascontiguousarray(cosT[:, 2048 * j:2048 * (j + 1)]),
                        "sinT": np.ascontiguousarray(sinT[:, 2048 * j:2048 * (j + 1)])})
    res = run_bass_kernel_spmd(nc1, in_maps, core_ids=list(range(NCORES)))
    fmb = np.zeros((B, 1024, S + L), ml_dtypes.bfloat16)
    fmf = np.zeros((B, 1024, S + L), np.float32)
    tm = np.zeros((B, S + L, 512), ml_dtypes.bfloat16)
    for i in range(NCORES):
        b, j = i // 4, i % 4
        r = res.results[i]
        for dst, key in ((fmb, "fmb"), (fmf, "fmf")):
            f = r[key].transpose(1, 0, 2).reshape(1024, NT1)
            dst[b, :, 2048 * j:2048 * (j + 1)] = f[:, :2048]
            dst[b, :, S + 64 * j:S + 64 * (j + 1)] = f[:, 2048:]
        t = r["tm"]
        tm[b, 2048 * j:2048 * (j + 1)] = t[:2048]
        tm[b, S + 64 * j:S + 64 * (j + 1)] = t[2048:]
    return fmb, fmf, tm


NTOK = S + L
NKT = NTOK // 128
NEB = 21


def na_tile_lists():
    out = []
    for m in range(64):
        if 2 <= m <= 61:
            out.append(([m - 2, m - 1, m, m + 1, m + 2], 0))
        elif m < 2:
            out.append(([0, 1, 2, 3], 5 + 4 * m))
        else:
            out.append(([60, 61, 62, 63], 5 + 4 * (m - 60)))
    return out


def na_bias_index():
    MASKED = 15 * 31
    idx = np.full((NEB, 128, 128), MASKED, np.int64)
    lists = na_tile_lists()
    reps = {0: 10}
    qq = np.arange(128); kk = np.arange(128)

    def fill(e0, m, kts):
        for ii, n in enumerate(kts):
            qr = 2 * m + qq // 64; qc = qq % 64
            kr = 2 * n + kk // 64; kc = kk % 64
            r0 = np.clip(qr - 4, 0, 120)
            cs = np.clip(qc - 8, 0, 48)
            valid = ((kr[:, None] >= r0[None, :]) & (kr[:, None] < r0[None, :] + 8) &
                     (kc[:, None] >= cs[None, :]) & (kc[:, None] < cs[None, :] + 16))
            dr = kr[:, None] - qr[None, :]
            dc = np.clip(kc[:, None] - qc[None, :], -15, 15)
            v = (np.clip(dr, -7, 7) + 7) * 31 + dc + 15
            idx[e0 + ii] = np.where(valid, v, MASKED)
    fill(0, 10, lists[10][0])
    for m in (0, 1, 62, 63):
        fill(lists[m][1], m, lists[m][0])
    return idx


def build_p2(lam_init):
    nc = bass.Bass("TRN2", target_bir_lowering=False)
    din = lambda n, shp, dt: nc.dram_tensor(n, shp, dt, kind="ExternalInput").ap()
    dout = lambda n, shp, dt: nc.dram_tensor(n, shp, dt, kind="ExternalOutput").ap()
    xrg = din("xrg", [2, 128, NTOK], F32)
    wbd = din("wbd", [4, 128, 128], F32)
    rgv = din("rgv", [128, 2, 8], F32)
    hout = dout("hout", [2, 128, NTOK], F32)
    qaT = din("qaT", [64, NTOK], BF16)
    kaT = din("kaT", [64, NTOK], BF16)
    vaP = din("vaP", [128, NKT, 64], BF16)
    btT = din("btT", [128, NEB, 128], F32)
    ynaP = dout("ynaP", [128, NKT, 64], BF16)
    qdT = din("qdT", [2, 32, NTOK], BF16)
    kdT = din("kdT", [2, 32, NTOK], BF16)
    vdP = din("vdP", [128, NKT, 64], BF16)
    dlam = din("dlam", [128, 128], F32)
    dg = din("dg", [128, 64], F32)
    ydfP = dout("ydfP", [128, NKT, 64], BF16)
    s = Sched(nc)
    with nc.psum_tensor("ps", [128, 8, 512], F32) as ps:
        bps = [s.buf(f"ps{i}") for i in range(8)]

        CH = 2048
        with (nc.sbuf_tensor("x_sb", [128, NTOK], F32) as x_sb,
              nc.sbuf_tensor("wst_sb", [128, 4, 128], F32) as wst_sb,
              nc.sbuf_tensor("wbf_sb", [128, 4, 128], BF16) as wbf_sb,
              nc.sbuf_tensor("rgv_sb", [128, 2, 8], F32) as rgv_sb,
              nc.sbuf_tensor("cneg_sb", [128, 2], F32) as cneg_sb,
              nc.sbuf_tensor("xcv_sb", [128, CH], F32) as xcv_sb,
              nc.sbuf_tensor("xcb_sb", [128, CH], BF16) as xcb_sb,
              nc.sbuf_tensor("r_sb", [128, CH], F32) as r_sb,
              nc.sbuf_tensor("i_sb", [128, CH], F32) as i_sb,
              nc.sbuf_tensor("a_sb", [128, CH], F32) as a_sb,
              nc.sbuf_tensor("q_sb", [128, CH], F32) as q_sb,
              nc.sbuf_tensor("h_sb", [128, 2, CH], F32) as h_sb,
              nc.sbuf_tensor("carry_sb", [128, 1], F32) as carry_sb):
            bx = s.buf("x", s.dsem("rgx"))
            bw = s.buf("w", s.dsem("rgw"))
            bwb = s.buf("wb")
            bv = s.buf("v", bw.dsem)
            bcn = s.buf("cneg")
            bxcv = s.buf("xcv"); bxcb = s.buf("xcb"); br = s.buf("r"); bi_ = s.buf("i")
            ba = s.buf("a"); bq = s.buf("q"); bcar = s.buf("carry")
            bh = [s.buf(f"h{i}", s.dsem(f"rgh{i}")) for i in range(2)]
            s.dma("act", wst_sb[:], wbd.rearrange("f c d -> c f d"), W=[bw])
            s.dma("act", rgv_sb[:], rgv, W=[bv])
            s.op("pool", lambda e: e.tensor_copy(out=wbf_sb[:], in_=wst_sb[:]), R=[bw], W=[bwb])
            s.op("act", lambda e: e.activation(out=cneg_sb[:], in_=rgv_sb[:, :, 7], func=AF.Exp, scale=-1.0),
                 R=[bv], W=[bcn])
            s.op("act", lambda e: e.activation(out=cneg_sb[:], in_=cneg_sb[:], func=AF.Ln, bias=1.0, scale=1.0),
                 R=[bcn], W=[bcn])
            s.op("dve", lambda e: e.tensor_scalar(out=cneg_sb[:], in0=cneg_sb[:], scalar1=-8.0, scalar2=None,
                                                  op0=ALU.mult), R=[bcn], W=[bcn])
            hi = 0
            for dr in range(2):
                offs = [-2, -1, 0, 1] if dr == 0 else [2, 1, 0, -1]
                s.dma("sp", x_sb[:, 0:4224], xrg[dr, :, 0:4224], W=[bx])
                s.dma("sp", x_sb[:, 4224:NTOK], xrg[dr, :, 4224:NTOK], W=[bx])
                chunks = [(0, 256, 0, 256)] + [(256 + CH * i, CH, 256, NTOK) for i in range(4)]
                for ci, (c0, n, s0, s1) in enumerate(chunks):
                    s.op("dve", lambda e: e.tensor_scalar(
                        out=xcv_sb[:, 0:n], in0=x_sb[:, c0:c0 + n], scalar1=rgv_sb[:, dr, 2:3],
                        scalar2=rgv_sb[:, dr, 4:5], op0=ALU.mult, op1=ALU.add), R=[bx, bv], W=[bxcv])
                    for jt in (0, 1, 3):
                        o = offs[jt]
                        lo = max(c0, s0 - o); hi_ = min(c0 + n, s1 - o)
                        s.op("dve", lambda e: e.scalar_tensor_tensor(
                            out=xcv_sb[:, lo - c0:hi_ - c0], in0=x_sb[:, lo + o:hi_ + o],
                            scalar=rgv_sb[:, dr, jt:jt + 1], in1=xcv_sb[:, lo - c0:hi_ - c0],
                            op0=ALU.mult, op1=ALU.add), R=[bx, bv, bxcv], W=[bxcv])
                    s.op("pool", lambda e: e.tensor_copy(out=xcb_sb[:, 0:n], in_=xcv_sb[:, 0:n]), R=[bxcv], W=[bxcb])
                    nsb = (n + 511) // 512
                    for sb in range(nsb):
                        w_ = min(512, n - sb * 512)
                        mm(s, ps[:, sb, 0:w_], wbf_sb[:, 2 * dr, :], xcb_sb[:, sb * 512:sb * 512 + w_], True, True,
                           R=[bwb, bxcb], W=[bps[sb]])
                        mm(s, ps[:, 4 + sb, 0:w_], wbf_sb[:, 2 * dr + 1, :], xcb_sb[:, sb * 512:sb * 512 + w_], True, True,
                           R=[bwb, bxcb], W=[bps[4 + sb]])
                    if n == CH:
                        rin = ps[:, 0:4, :]; iin = ps[:, 4:8, :]
                        rout = r_sb[:, 0:n].rearrange("p (a b) -> p a b", b=512)
                        iout = i_sb[:, 0:n].rearrange("p (a b) -> p a b", b=512)
                    else:
                        rin = ps[:, 0, 0:n]; iin = ps[:, 4, 0:n]
                        rout = r_sb[:, 0:n]; iout = i_sb[:, 0:n]
                    s.op("act", lambda e: e.activation(out=rout, in_=rin, func=AF.Sigmoid,
                                                       bias=rgv_sb[:, dr, 5:6], scale=1.0),
                         R=bps[0:4] + [bv], W=[br])
                    s.op("act", lambda e: e.activation(out=iout, in_=iin, func=AF.Sigmoid,
                                                       bias=rgv_sb[:, dr, 6:7], scale=1.0),
                         R=bps[4:8] + [bv], W=[bi_])
                    s.op("act", lambda e: e.activation(out=a_sb[:, 0:n], in_=r_sb[:, 0:n], func=AF.Exp,
                                                       scale=cneg_sb[:, dr:dr + 1]), R=[br, bcn], W=[ba])
                    s.op("pool", lambda e: e.tensor_tensor(out=q_sb[:, 0:n], in0=a_sb[:, 0:n], in1=a_sb[:, 0:n],
                                                           op=ALU.mult), R=[ba], W=[bq])
                    s.op("act", lambda e: e.activation(out=q_sb[:, 0:n], in_=q_sb[:, 0:n], func=AF.Sqrt,
                                                       scale=-1.0, bias=1.0), R=[bq], W=[bq])
                    s.op("pool", lambda e: e.tensor_tensor(out=i_sb[:, 0:n], in0=i_sb[:, 0:n], in1=xcv_sb[:, 0:n],
                                                           op=ALU.mult), R=[bi_, bxcv], W=[bi_])
                    s.op("pool", lambda e: e.tensor_tensor(out=q_sb[:, 0:n], in0=q_sb[:, 0:n], in1=i_sb[:, 0:n],
                                                           op=ALU.mult), R=[bq, bi_], W=[bq])
                    hs = hi % 2; hi += 1
                    init = 0.0 if ci == 0 else carry_sb[:, 0:1]
                    s.op("dve", lambda e: e.tensor_tensor_scan(out=h_sb[:, hs, 0:n], data0=a_sb[:, 0:n],
                                                               data1=q_sb[:, 0:n], initial=init,
                                                               op0=ALU.mult, op1=ALU.add),
                         R=[ba, bq] + ([bcar] if ci else []), W=[bh[hs]])
                    s.op("dve", lambda e: e.tensor_copy(out=carry_sb[:, 0:1], in_=h_sb[:, hs, n - 1:n]),
                         R=[bh[hs]], W=[bcar])
                    s.dma("sp", hout[dr, :, c0:c0 + n], h_sb[:, hs, 0:n], R=[bh[hs]])
            s.barrier(bh)

        with (nc.sbuf_tensor("na_q_sb", [64, NTOK], BF16) as q_sb,
              nc.sbuf_tensor("na_k_sb", [64, NTOK], BF16) as k_sb,
              nc.sbuf_tensor("na_v_sb", [128, NKT, 65], BF16) as v_sb,
              nc.sbuf_tensor("na_bt_sb", [128, NEB * 128], F32) as bt_sb,
              nc.sbuf_tensor("na_eb_sb", [128, NEB * 128], BF16) as eb_sb,
              nc.sbuf_tensor("na_e_sb", [128, 2, 640], F32) as e_sb,
              nc.sbuf_tensor("na_p_sb", [128, 2, 896], BF16) as p_sb,
              nc.sbuf_tensor("na_y_sb", [128, NKT, 64], BF16) as y_sb,
              nc.sbuf_tensor("na_rec_sb", [128, 2], F32) as rec_sb):
            ld = s.dsem("nald")
            bq = s.buf("q", ld); bk = s.buf("k", ld); bv = s.buf("v", ld); bbt = s.buf("bt", ld)
            beb = s.buf("eb")
            be = [s.buf(f"e{i}") for i in range(2)]
            bp = [s.buf(f"p{i}") for i in range(2)]
            brec = [s.buf(f"rec{i}") for i in range(2)]
            by = s.buf("y", s.dsem("nay"))
            s.dma("sp", q_sb[:], qaT, W=[bq])
            s.dma("act", k_sb[:], kaT, W=[bk])
            s.dma("sp", v_sb[:, :, 0:64], vaP, W=[bv])
            s.dma("act", bt_sb[:], btT.rearrange("p e q -> p (e q)"), W=[bbt])
            s.op("pool", lambda e: e.memset(v_sb[:, :, 64:65], 1.0), W=[bv])
            s.op("act", lambda e: e.activation(out=eb_sb[:], in_=bt_sb[:], func=AF.Exp), R=[bbt], W=[beb])
            lists = na_tile_lists()
            for m in range(NKT):
                sl = m % 2
                if m < 64:
                    kts, eb0 = lists[m]
                else:
                    kts, eb0 = [], 0
                nl = len(kts)
                bA = bps[2 * sl]; bB = bps[2 * sl + 1]; bAcc = bps[4 + sl]
                qs = q_sb[:, m * 128:(m + 1) * 128]
                for ii, n in enumerate(kts):
                    bank = 2 * sl + (0 if ii < 4 else 1)
                    col = (ii % 4) * 128
                    mm(s, ps[:, bank, col:col + 128], k_sb[:, n * 128:(n + 1) * 128], qs, True, True,
                       R=[bk, bq], W=[bps[bank]])
                for ci in range(2):
                    n = 64 + ci
                    mm(s, ps[:, 2 * sl + 1, 128 + ci * 128:256 + ci * 128], k_sb[:, n * 128:(n + 1) * 128], qs,
                       True, True, R=[bk, bq], W=[bB])
                if nl:
                    na_ = min(nl, 4) * 128
                    s.op("act", lambda e: e.activation(out=e_sb[:, sl, 0:na_], in_=ps[:, 2 * sl, 0:na_], func=AF.Exp,
                                                       scale=0.125), R=[bA], W=[be[sl]])
                    if nl == 5:
                        s.op("act", lambda e: e.activation(out=e_sb[:, sl, 512:640], in_=ps[:, 2 * sl + 1, 0:128],
                                                           func=AF.Exp, scale=0.125), R=[bB], W=[be[sl]])
                s.op("act", lambda e: e.activation(out=p_sb[:, sl, 640:896], in_=ps[:, 2 * sl + 1, 128:384],
                                                   func=AF.Exp, scale=0.125), R=[bB], W=[bp[sl]])
                if nl:
                    s.op("dve", lambda e: e.tensor_tensor(out=p_sb[:, sl, 0:nl * 128], in0=e_sb[:, sl, 0:nl * 128],
                                                          in1=eb_sb[:, eb0 * 128:(eb0 + nl) * 128], op=ALU.mult),
                         R=[be[sl], beb], W=[bp[sl]])
                tiles = [(ii * 128, n) for ii, n in enumerate(kts)] + [(640, 64), (768, 65)]
                for ti, (pc, n) in enumerate(tiles):
                    mm(s, ps[:, 4 + sl, 0:65], p_sb[:, sl, pc:pc + 128], v_sb[:, n, :], ti == 0, ti == len(tiles) - 1,
                       R=[bp[sl], bv], W=[bAcc])
                s.op("dve", lambda e: e.reciprocal(out=rec_sb[:, sl:sl + 1], in_=ps[:, 4 + sl, 64:65]),
                     R=[bAcc], W=[brec[sl]])
                s.op("dve", lambda e: e.tensor_scalar(out=y_sb[:, m, :], in0=ps[:, 4 + sl, 0:64],
                                                      scalar1=rec_sb[:, sl:sl + 1], scalar2=None, op0=ALU.mult),
                     R=[bAcc, brec[sl]], W=[by])
            s.dma("sp", ynaP, y_sb[:], R=[by])
            s.barrier([by])

        with (nc.sbuf_tensor("df_q4_sb", [96, NTOK], BF16) as q4_sb,
              nc.sbuf_tensor("df_k4_sb", [96, NTOK], BF16) as k4_sb,
              nc.sbuf_tensor("df_q4b_sb", [96, NTOK], BF16) as q4b_sb,
              nc.sbuf_tensor("df_k4b_sb", [96, NTOK], BF16) as k4b_sb,
              nc.sbuf_tensor("df_v_sb", [128, NKT, 65], BF16) as v_sb,
              nc.sbuf_tensor("df_p_sb", [128, 8, 512], BF16) as p_sb,
              nc.sbuf_tensor("df_y_sb", [128, NKT, 64], BF16) as y_sb,
              nc.sbuf_tensor("df_dl_sb", [128, 128], F32) as dl_sb,
              nc.sbuf_tensor("df_g_sb", [128, 64], F32) as g_sb,
              nc.sbuf_tensor("df_lam_sb", [128, 4], F32) as lam_sb,
              nc.sbuf_tensor("df_r_sb", [128, 2, 2, 4], F32) as r_sb,
              nc.sbuf_tensor("df_ss_sb", [128, 2, 4], F32) as ss_sb,
              nc.sbuf_tensor("df_o_sb", [128, 2, 4, 64], F32) as o_sb,
              nc.sbuf_tensor("df_t_sb", [128, 2, 4, 64], F32) as t_sb,
              nc.sbuf_tensor("df_junk_sb", [128, 64], F32) as junk_sb):
            ld = s.dsem("dfld")
            bq = s.buf("q", ld); bk = s.buf("k", ld); bv = s.buf("v", ld); bdl = s.buf("dl", ld); bg = s.buf("g", ld)
            blam = s.buf("lam")
            bp = [s.buf(f"p{i}") for i in range(8)]
            by = s.buf("y", s.dsem("dfy"))
            br = [s.buf(f"r{i}") for i in range(2)]
            bss = [s.buf(f"ss{i}") for i in range(2)]
            bo = [s.buf(f"o{i}") for i in range(2)]
            bt = [s.buf(f"t{i}") for i in range(2)]
            bj = s.buf("junk")
            for (qt, kt_, order) in ((q4_sb, k4_sb, (0, 1, 0)), (q4b_sb, k4b_sb, (1, 0, 1))):
                for ri, cc in enumerate(order):
                    s.dma("sp", qt[32 * ri:32 * ri + 32, :], qdT[cc], W=[bq])
                    s.dma("act", kt_[32 * ri:32 * ri + 32, :], kdT[cc], W=[bk])
            s.dma("sp", v_sb[:, :, 0:64], vdP, W=[bv])
            s.dma("act", dl_sb[:], dlam, W=[bdl])
            s.dma("act", g_sb[:], dg, W=[bg])
            s.op("pool", lambda e: e.memset(v_sb[:, :, 64:65], 1.0), W=[bv])
            s.op("dve", lambda e: e.scalar_tensor_tensor(out=junk_sb[:, 0:32], in0=dl_sb[:, 0:32], scalar=1.0,
                                                         in1=dl_sb[:, 32:64], op0=ALU.mult, op1=ALU.mult,
                                                         accum_out=lam_sb[:, 0:1]), R=[bdl], W=[bj, blam])
            s.op("dve", lambda e: e.scalar_tensor_tensor(out=junk_sb[:, 0:32], in0=dl_sb[:, 64:96], scalar=1.0,
                                                         in1=dl_sb[:, 96:128], op0=ALU.mult, op1=ALU.mult,
                                                         accum_out=lam_sb[:, 1:2]), R=[bdl, bj], W=[bj, blam])
            s.op("act", lambda e: e.activation(out=lam_sb[:, 0:2], in_=lam_sb[:, 0:2], func=AF.Exp), R=[blam], W=[blam])
            s.op("dve", lambda e: e.tensor_tensor(out=lam_sb[:, 2:3], in0=lam_sb[:, 1:2], in1=lam_sb[:, 0:1],
                                                  op=ALU.subtract), R=[blam], W=[blam])
            s.op("dve", lambda e: e.tensor_scalar(out=lam_sb[:, 2:3], in0=lam_sb[:, 2:3], scalar1=-lam_init,
                                                  scalar2=None, op0=ALU.add), R=[blam], W=[blam])
            s.op("dve", lambda e: e.tensor_scalar(out=g_sb[:], in0=g_sb[:], scalar1=1.0 - lam_init, scalar2=None,
                                                  op0=ALU.mult), R=[bg], W=[bg])
            qblocks = [(512 * i, 512, list(range(NKT))) for i in range(16)] + [(S, 256, [64, 65])]
            sc = 32 ** -0.5
            gs_ = 0
            for qi, (q0, nq, kts) in enumerate(qblocks):
                par = qi % 2
                nsub = nq // 128
                steps = [(kt, c) for kt in kts for c in range(2)]
                ns = len(steps)
                groups = [list(range(i, min(i + 3, ns))) for i in range(0, ns, 3)]
                started = [False, False]
                for gi_ in range(len(groups) + 1):
                    if gi_ < len(groups):
                        grp = groups[gi_]
                        layB = steps[grp[0]][1] == 1
                        qt = q4b_sb if layB else q4_sb
                        kt_t = k4b_sb if layB else k4_sb
                        for ri, si in enumerate(grp):
                            kt, c = steps[si]
                            g = gs_ + si
                            mm(s, ps[:, g % 4, 0:nq], kt_t[32 * ri:32 * ri + 32, kt * 128:(kt + 1) * 128],
                               qt[32 * ri:32 * ri + 32, q0:q0 + nq], True, True, R=[bk, bq], W=[bps[g % 4]])
                        for ri, si in enumerate(grp):
                            g = gs_ + si
                            s.op("act", lambda e: e.activation(out=p_sb[:, g % 8, 0:nq], in_=ps[:, g % 4, 0:nq],
                                                               func=AF.Exp, scale=sc), R=[bps[g % 4]], W=[bp[g % 8]])
                    if gi_ >= 1:
                        for si in groups[gi_ - 1]:
                            kt, c = steps[si]
                            g = gs_ + si
                            bank = 4 + 2 * par + c
                            for sub in range(nsub):
                                st = not started[c]
                                started[c] = True
                                s.op("pe", lambda e: e.matmul(ps[:, bank, sub * 65:(sub + 1) * 65],
                                                              p_sb[:, g % 8, sub * 128:(sub + 1) * 128], v_sb[:, kt, :],
                                                              start=st, stop=(kt == kts[-1]), skip_group_check=True),
                                     R=[bp[g % 8], bv], W=[bps[bank]])
                gs_ += ns
                a0 = ps[:, 4 + 2 * par, 0:260].rearrange("p (s e) -> p s e", e=65)
                a1 = ps[:, 5 + 2 * par, 0:260].rearrange("p (s e) -> p s e", e=65)
                b0 = bps[4 + 2 * par]; b1 = bps[5 + 2 * par]
                s.op("dve", lambda e: e.reciprocal(out=r_sb[:, par, 0, 0:nsub], in_=a0[:, 0:nsub, 64]), R=[b0], W=[br[par]])
                s.op("dve", lambda e: e.reciprocal(out=r_sb[:, par, 1, 0:nsub], in_=a1[:, 0:nsub, 64]), R=[b1], W=[br[par]])
                s.op("dve", lambda e: e.tensor_scalar(out=r_sb[:, par, 1, 0:nsub], in0=r_sb[:, par, 1, 0:nsub],
                                                      scalar1=lam_sb[:, 2:3], scalar2=None, op0=ALU.mult),
                     R=[br[par], blam], W=[br[par]])
                for sub in range(nsub):
                    s.op("dve", lambda e: e.tensor_scalar(out=t_sb[:, par, sub, :], in0=a1[:, sub, 0:64],
                                                          scalar1=r_sb[:, par, 1, sub:sub + 1], scalar2=None,
                                                          op0=ALU.mult), R=[b1, br[par]], W=[bt[par]])
                    s.op("dve", lambda e: e.scalar_tensor_tensor(out=o_sb[:, par, sub, :], in0=a0[:, sub, 0:64],
                                                                 scalar=r_sb[:, par, 0, sub:sub + 1],
                                                                 in1=t_sb[:, par, sub, :], op0=ALU.mult, op1=ALU.add),
                         R=[b0, br[par], bt[par]], W=[bo[par]])
                    s.op("dve", lambda e: e.scalar_tensor_tensor(out=junk_sb[:], in0=o_sb[:, par, sub, :], scalar=1.0,
                                                                 in1=o_sb[:, par, sub, :], op0=ALU.mult, op1=ALU.mult,
                                                                 accum_out=ss_sb[:, par, sub:sub + 1]),
                         R=[bo[par], bj], W=[bj, bss[par]])
                s.op("act", lambda e: e.activation(out=ss_sb[:, par, 0:nsub], in_=ss_sb[:, par, 0:nsub], func=AF.Ln,
                                                   scale=1.0 / 64, bias=EPS), R=[bss[par]], W=[bss[par]])
                s.op("act", lambda e: e.activation(out=ss_sb[:, par, 0:nsub], in_=ss_sb[:, par, 0:nsub], func=AF.Exp,
                                                   scale=-0.5), R=[bss[par]], W=[bss[par]])
                for sub in range(nsub):
                    s.op("dve", lambda e: e.scalar_tensor_tensor(out=y_sb[:, q0 // 128 + sub, :], in0=o_sb[:, par, sub, :],
                                                                 scalar=ss_sb[:, par, sub:sub + 1], in1=g_sb[:],
                                                                 op0=ALU.mult, op1=ALU.mult),
                         R=[bo[par], bss[par], bg], W=[by])
            s.dma("sp", ydfP, y_sb[:], R=[by])
            s.finish([by])
    return nc


def tileP(a):
    return np.ascontiguousarray(a.reshape(NKT, 128, a.shape[1]).transpose(1, 0, 2))


def untileP(a):
    return a.transpose(1, 0, 2).reshape(NTOK, a.shape[2])


_NA_IDX = None


def run_p2(nc2, l, fmb, fmf, tm, inp):
    global _NA_IDX
    if _NA_IDX is None:
        _NA_IDX = na_bias_index()
    in_maps = []
    for i in range(NCORES):
        b, j = i // 4, i % 4
        xr = fmf[b, 128 * j:128 * (j + 1)]
        xf = np.concatenate([xr[:, S:], xr[:, :S]], axis=1)
        xb = np.concatenate([xr[:, S:][:, ::-1], xr[:, :S][:, ::-1]], axis=1)
        wbd = np.zeros((4, 128, 128), np.float32)
        rgv = np.zeros((128, 2, 8), np.float32)
        ch = slice(128 * j, 128 * (j + 1))
        for dr in range(2):
            for gi, wk in enumerate(("rg_w_r", "rg_w_i")):
                for bb in range(2):
                    wbd[dr * 2 + gi, 64 * bb:64 * (bb + 1), 64 * bb:64 * (bb + 1)] = inp[wk][l, dr, 2 * j + bb]
            rgv[:, dr, 0:4] = inp["rg_conv_w"][l][:, ch].T
            rgv[:, dr, 4] = inp["rg_conv_b"][l][ch]
            rgv[:, dr, 5] = inp["rg_b_r"][l, dr, ch]
            rgv[:, dr, 6] = inp["rg_b_i"][l, dr, ch]
            rgv[:, dr, 7] = inp["rg_lambda"][l, dr, ch]
        rext = np.concatenate([inp["na_rpb"][l, j].ravel(), np.array([-30000.0], np.float32)])
        bt = rext[_NA_IDX]
        in_maps.append({
            "xrg": np.ascontiguousarray(np.stack([xf, xb])), "wbd": wbd, "rgv": rgv,
            "qaT": np.ascontiguousarray(fmb[b, 64 * j:64 * (j + 1)]),
            "kaT": np.ascontiguousarray(fmb[b, 256 + 64 * j:256 + 64 * (j + 1)]),
            "vaP": tileP(tm[b][:, 64 * j:64 * (j + 1)]),
            "btT": np.ascontiguousarray(bt.transpose(1, 0, 2)),
            "qdT": np.ascontiguousarray(fmb[b, 512 + 64 * j:512 + 64 * (j + 1)].reshape(2, 32, NTOK)),
            "kdT": np.ascontiguousarray(fmb[b, 768 + 64 * j:768 + 64 * (j + 1)].reshape(2, 32, NTOK)),
            "vdP": tileP(tm[b][:, 256 + 64 * j:256 + 64 * (j + 1)]),
            "dlam": np.ascontiguousarray(np.tile(inp["diff_lambda"][l].reshape(1, 128), (128, 1))),
            "dg": np.ascontiguousarray(np.tile(inp["diff_subln_g"][l].reshape(1, 64), (128, 1))),
        })
    res = run_bass_kernel_spmd(nc2, in_maps, core_ids=list(range(NCORES)))
    yna = np.zeros((B, NTOK, 256), ml_dtypes.bfloat16)
    ydf = np.zeros((B, NTOK, 256), ml_dtypes.bfloat16)
    hf = np.zeros((B, 512, NTOK), np.float32)
    hb = np.zeros((B, 512, NTOK), np.float32)
    for i in range(NCORES):
        b, j = i // 4, i % 4
        r = res.results[i]
        yna[b, :, 64 * j:64 * (j + 1)] = untileP(r["ynaP"])
        ydf[b, :, 64 * j:64 * (j + 1)] = untileP(r["ydfP"])
        h = r["hout"]
        hf[b, 128 * j:128 * (j + 1), S:] = h[0][:, :L]
        hf[b, 128 * j:128 * (j + 1), :S] = h[0][:, L:]
        hb[b, 128 * j:128 * (j + 1), S:] = h[1][:, :L][:, ::-1]
        hb[b, 128 * j:128 * (j + 1), :S] = h[1][:, L:][:, ::-1]
    return yna, ydf, hf, hb


NEXP = 32
GELU_C = 1.5957691216057308


def build_p3(final):
    nc = bass.Bass("TRN2", target_bir_lowering=False)
    din = lambda n, shp, dt: nc.dram_tensor(n, shp, dt, kind="ExternalInput").ap()
    xT = din("xT", [128, 8, NT1], F32)
    nadf = din("nadf", [128, 4, NT1], BF16)
    hg = din("hg", [128, 12, NT1], F32)
    wout = din("wout", [D, D], F32)
    mod = din("mod", [128, 10, 8], F32)
    wge = din("wge", [128, 8, 36], F32)
    bge = din("bge", [128, 36], F32)
    selc = din("selc", [32, NEXP * 128], BF16)
    ident = din("ident", [128, 128], F32)
    w1 = din("w1", [NEXP, D, 512], F32)
    w3 = din("w3", [NEXP, D, 512], F32)
    w2 = din("w2", [NEXP, 512, D], F32)
    xo = nc.dram_tensor("xo", [128, 8, NT1], F32, kind="ExternalOutput").ap()
    s = Sched(nc)
    wov = wout.rearrange("(kc p) n -> p kc n", p=128)
    with (nc.psum_tensor("ps", [128, 8, 512], F32) as ps,
          nc.sbuf_tensor("x_sb", [128, 8, NT1], F32) as x_sb,
          nc.sbuf_tensor("mod_sb", [128, 10, 8], F32) as mod_sb,
          nc.sbuf_tensor("hl2_sb", [128, 8, NT1], BF16) as hl2_sb,
          nc.sbuf_tensor("wdt_sb", [32, 2, NT1], BF16) as wdt_sb,
          nc.sbuf_tensor("ones_sb", [128, 128], BF16) as ones_sb):
        bps = [s.buf(f"ps{i}") for i in range(8)]
        cs = s.dsem("const")
        bxb = [s.buf(f"x{i}", s.dsem(f"x{i}")) for i in range(len(BLKS1))]
        bmod = s.buf("mod", cs)
        bhl2 = [s.buf(f"hl2_{i}") for i in range(len(BLKS1))]
        bwdt = [s.buf(f"wdt{i}") for i in range(len(BLKS1))]
        bones = s.buf("ones")
        s.dma("sp", mod_sb[:], mod, W=[bmod])
        for bi, (t0, n) in enumerate(BLKS1):
            s.dma("sp", x_sb[:, :, t0:t0 + n], xT[:, :, t0:t0 + n], W=[bxb[bi]])
        s.op("pool", lambda e: e.memset(ones_sb[:], 1.0), W=[bones])

        with (nc.sbuf_tensor("s1_mix", [128, 8, NT1], BF16) as mix_sb,
              nc.sbuf_tensor("s1_wobf", [128, 8, D], BF16) as wo_bf,
              nc.sbuf_tensor("s1_wost", [128, 2, D], F32) as wo_st,
              nc.sbuf_tensor("s1_hg", [128, 1, 12, 512], F32) as hg_sb,
              nc.sbuf_tensor("s1_t", [128, 4, 512], F32) as t_sb):
            bmixl = s.buf("mixl", s.dsem("mixl"))
            bmix = [s.buf(f"mix{i}") for i in range(len(BLKS1))]
            bwost = [s.buf(f"wost{i}", s.dsem(f"wost{i}")) for i in range(2)]
            bwobf = [s.buf(f"wobf{k}") for k in range(8)]
            bhg = [s.buf(f"hg{i}", s.dsem(f"hg{i}")) for i in range(2)]
            bt = [s.buf(f"t{i}") for i in range(4)]
            s.dma("act", mix_sb[:, 0:2, :], nadf[:, 0:2, :], W=[bmixl])
            s.dma("act", mix_sb[:, 6:8, :], nadf[:, 2:4, :], W=[bmixl])
            for k in range(8):
                sl = k % 2
                s.dma("act", wo_st[:, sl, :], wov[:, k, :], W=[bwost[sl]])
                s.op("pool", lambda e: e.tensor_copy(out=wo_bf[:, k, :], in_=wo_st[:, sl, :]), R=[bwost[sl]], W=[bwobf[k]])
            for bi, (t0, n) in enumerate(BLKS1):
                sl = 0
                s.dma("sp", hg_sb[:, sl, :, 0:n], hg[:, :, t0:t0 + n], W=[bhg[sl]])
                for ch in range(4):
                    hf_ = hg_sb[:, sl, ch, 0:n]; hb_ = hg_sb[:, sl, 4 + ch, 0:n]; gr_ = hg_sb[:, sl, 8 + ch, 0:n]
                    s.op("dve", lambda e: e.tensor_tensor(out=t_sb[:, 0, 0:n], in0=hf_, in1=hb_, op=ALU.add),
                         R=[bhg[sl]], W=[bt[0]])
                    s.op("dve", lambda e: e.tensor_tensor(out=t_sb[:, 1, 0:n], in0=gr_, in1=gr_, op=ALU.mult),
                         R=[bhg[sl]], W=[bt[1]])
                    s.op("dve", lambda e: e.tensor_scalar(out=t_sb[:, 1, 0:n], in0=t_sb[:, 1, 0:n], scalar1=0.044715,
                                                          scalar2=1.0, op0=ALU.mult, op1=ALU.add), R=[bt[1]], W=[bt[1]])
                    s.op("pool", lambda e: e.tensor_tensor(out=t_sb[:, 2, 0:n], in0=t_sb[:, 1, 0:n], in1=gr_, op=ALU.mult),
                         R=[bt[1], bhg[sl]], W=[bt[2]])
                    s.op("act", lambda e: e.activation(out=t_sb[:, 2, 0:n], in_=t_sb[:, 2, 0:n], func=AF.Sigmoid,
                                                       scale=GELU_C), R=[bt[2]], W=[bt[2]])
                    s.op("pool", lambda e: e.tensor_tensor(out=t_sb[:, 3, 0:n], in0=t_sb[:, 2, 0:n], in1=gr_, op=ALU.mult),
                         R=[bt[2], bhg[sl]], W=[bt[3]])
                    s.op("pool", lambda e: e.tensor_tensor(out=mix_sb[:, 2 + ch, t0:t0 + n], in0=t_sb[:, 3, 0:n],
                                                           in1=t_sb[:, 0, 0:n], op=ALU.mult),
                         R=[bt[3], bt[0]], W=[bmix[bi]])
            pi = 0
            for bi, (t0, n) in enumerate(BLKS1):
                garow = 5 if bi == 4 else 1
                for dc in range(8):
                    pb = pi % 8; pi += 1
                    for k in range(8):
                        mm(s, ps[:, pb, 0:n], wo_bf[:, k, dc * 128:(dc + 1) * 128], mix_sb[:, k, t0:t0 + n],
                           k == 0, k == 7, R=[bwobf[k], bmix[bi], bmixl], W=[bps[pb]])
                    s.op("dve", lambda e: e.scalar_tensor_tensor(out=x_sb[:, dc, t0:t0 + n], in0=ps[:, pb, 0:n],
                                                                 scalar=mod_sb[:, garow, dc:dc + 1],
                                                                 in1=x_sb[:, dc, t0:t0 + n], op0=ALU.mult, op1=ALU.add),
                         R=[bps[pb], bmod, bxb[bi]], W=[bxb[bi]])
            s.barrier()

        with (nc.sbuf_tensor("s2_sq", [128, 8, 512], BF16) as sq_sb,
              nc.sbuf_tensor("s2_rstd", [128, 2, 512], F32) as rstd_sb,
              nc.sbuf_tensor("s2_tmp", [128, 4, 512], F32) as tmp_sb,
              nc.sbuf_tensor("s2_hf", [128, 2, 8, 512], F32) as hf_sb,
              nc.sbuf_tensor("s2_ab", [128, 2, 8], F32) as ab_sb,
              nc.sbuf_tensor("s2_wge", [128, 8, 36], F32) as wge_sb,
              nc.sbuf_tensor("s2_bge", [128, 36], F32) as bge_sb,
              nc.sbuf_tensor("s2_id", [128, 128], F32) as id_sb,
              nc.sbuf_tensor("s2_rt", [128, 2, 128], F32) as rt_sb):
            bsq = s.buf("sq"); brs = [s.buf(f"rs{i}") for i in range(2)]
            btmp = [s.buf(f"tmp{i}") for i in range(4)]
            bhf = [s.buf(f"hf{i}") for i in range(2)]
            bab = s.buf("ab")
            bwge = s.buf("wge", cs); bbge = s.buf("bge", cs); bid = s.buf("id", cs)
            brt = [s.buf(f"rt{i}") for i in range(2)]
            s.dma("act", wge_sb[:], wge, W=[bwge])
            s.dma("act", bge_sb[:], bge, W=[bbge])
            s.dma("act", id_sb[:], ident, W=[bid])
            for t in range(2):
                s.op("dve", lambda e: e.scalar_tensor_tensor(
                    out=ab_sb[:, t, :], in0=mod_sb[:, 2 + 4 * t, :], scalar=1.0, in1=mod_sb[:, 0, :],
                    op0=ALU.add, op1=ALU.mult), R=[bmod], W=[bab])
            ti_g = 0
            for bi, (t0, n) in enumerate(BLKS1):
                sl = bi % 2
                isctx = bi == 4
                s.op("act", lambda e: e.activation(out=sq_sb[:, :, 0:n], in_=x_sb[:, :, t0:t0 + n], func=AF.Square),
                     R=[bxb[bi]], W=[bsq])
                for k in range(8):
                    mm(s, ps[:, sl, 0:n], ones_sb[:], sq_sb[:, k, 0:n], k == 0, k == 7, R=[bones, bsq], W=[bps[sl]])
                s.op("act", lambda e: e.activation(out=rstd_sb[:, sl, 0:n], in_=ps[:, sl, 0:n], func=AF.Sqrt,
                                                   scale=1.0 / D, bias=EPS), R=[bps[sl]], W=[brs[sl]])
                s.op("dve", lambda e: e.reciprocal(out=rstd_sb[:, sl, 0:n], in_=rstd_sb[:, sl, 0:n]),
                     R=[brs[sl]], W=[brs[sl]])
                ai = 1 if isctx else 0
                shrow = 7 if isctx else 3
                for k in range(8):
                    tb = k % 4
                    s.op("dve", lambda e: e.scalar_tensor_tensor(
                        out=tmp_sb[:, tb, 0:n], in0=x_sb[:, k, t0:t0 + n], scalar=ab_sb[:, ai, k:k + 1],
                        in1=rstd_sb[:, sl, 0:n], op0=ALU.mult, op1=ALU.mult),
                        R=[bxb[bi], bab, brs[sl]], W=[btmp[tb]])
                    s.op("act", lambda e: e.activation(out=hf_sb[:, sl, k, 0:n], in_=tmp_sb[:, tb, 0:n],
                                                       func=AF.Identity, bias=mod_sb[:, shrow, k:k + 1], scale=1.0),
                         R=[btmp[tb], bmod], W=[bhf[sl]])
                s.op("pool", lambda e: e.tensor_copy(out=hl2_sb[:, :, t0:t0 + n], in_=hf_sb[:, sl, :, 0:n]),
                     R=[bhf[sl]], W=[bhl2[bi]])
                for tt in range((n + 127) // 128):
                    c0 = tt * 128
                    m = min(128, n - c0)
                    rs_ = ti_g % 2; ti_g += 1
                    pb = 2 + rs_
                    rt = rt_sb[0:m, rs_, :]
                    brr = brt[rs_]
                    for k in range(8):
                        mm(s, ps[0:m, pb, 0:36], hf_sb[:, sl, k, c0:c0 + m], wge_sb[:, k, :], k == 0, k == 7,
                           R=[bhf[sl], bwge], W=[bps[pb]])
                    V = lambda eng, fn, R_=(), W_=(): s.op(eng, fn, R=[brr] + list(R_), W=[brr] + list(W_))
                    lg = rt[:, 0:36]
                    s.op("dve", lambda e: e.tensor_tensor(out=lg, in0=ps[0:m, pb, 0:36], in1=bge_sb[0:m, :], op=ALU.add),
                         R=[bps[pb], bbge], W=[brr])
                    gmax = rt[:, 36:37]; ngmax = rt[:, 37:38]; sume = rt[:, 38:39]; gtop = rt[:, 39:40]
                    eg = rt[:, 40:44]; ohg = rt[:, 44:48]; sel = rt[:, 48:56]; top8 = rt[:, 56:64]
                    dd = rt[:, 64:65]; ed = rt[:, 65:66]; w1_ = rt[:, 66:67]; wt1 = rt[:, 67:68]; wt2 = rt[:, 68:69]
                    ea = rt[:, 72:80]; eb_ = rt[:, 80:88]; wd = rt[:, 96:128]
                    V("dve", lambda e: e.reduce_max(out=gmax, in_=lg[:, 0:4], axis=AX.X))
                    V("dve", lambda e: e.tensor_scalar(out=ngmax, in0=gmax, scalar1=-1.0, scalar2=None, op0=ALU.mult))
                    V("act", lambda e: e.activation(out=eg, in_=lg[:, 0:4], func=AF.Exp, bias=ngmax, scale=1.0,
                                                    accum_out=sume))
                    V("dve", lambda e: e.reciprocal(out=gtop, in_=sume))
                    V("dve", lambda e: e.tensor_scalar(out=ohg, in0=lg[:, 0:4], scalar1=gmax, scalar2=None,
                                                       op0=ALU.is_equal))
                    V("dve", lambda e: e.tensor_scalar(out=sel, in0=lg[:, 4:12], scalar1=ohg[:, 0:1], scalar2=None,
                                                       op0=ALU.mult))
                    for g in range(1, 4):
                        V("dve", lambda e: e.scalar_tensor_tensor(out=sel, in0=lg[:, 4 + 8 * g:12 + 8 * g],
                                                                  scalar=ohg[:, g:g + 1], in1=sel,
                                                                  op0=ALU.mult, op1=ALU.add))
                    V("dve", lambda e: e.max(out=top8, in_=sel))
                    V("dve", lambda e: e.tensor_tensor(out=dd, in0=top8[:, 1:2], in1=top8[:, 0:1], op=ALU.subtract))
                    V("act", lambda e: e.activation(out=ed, in_=dd, func=AF.Exp))
                    V("dve", lambda e: e.tensor_scalar(out=w1_, in0=ed, scalar1=1.0, scalar2=None, op0=ALU.add))
                    V("dve", lambda e: e.reciprocal(out=w1_, in_=w1_))
                    V("dve", lambda e: e.tensor_tensor(out=wt1, in0=w1_, in1=gtop, op=ALU.mult))
                    V("dve", lambda e: e.tensor_tensor(out=wt2, in0=wt1, in1=ed, op=ALU.mult))
                    V("dve", lambda e: e.tensor_scalar(out=ea, in0=sel, scalar1=top8[:, 0:1], scalar2=wt1,
                                                       op0=ALU.is_equal, op1=ALU.mult))
                    V("dve", lambda e: e.tensor_scalar(out=eb_, in0=sel, scalar1=top8[:, 1:2], scalar2=wt2,
                                                       op0=ALU.is_equal, op1=ALU.mult))
                    V("dve", lambda e: e.tensor_tensor(out=ea, in0=ea, in1=eb_, op=ALU.add))
                    for g in range(4):
                        V("dve", lambda e: e.tensor_scalar(out=wd[:, 8 * g:8 * g + 8], in0=ea, scalar1=ohg[:, g:g + 1],
                                                           scalar2=None, op0=ALU.mult))
                    pt = 4 + rs_
                    s.op("pe", lambda e: e.transpose(ps[0:32, pt, 0:m], wd, id_sb[0:m, 0:m]), R=[brr, bid], W=[bps[pt]])
                    s.op("act", lambda e: e.copy(out=wdt_sb[:, 0, t0 + c0:t0 + c0 + m], in_=ps[0:32, pt, 0:m]),
                         R=[bps[pt]], W=[bwdt[bi]])
                    s.op("dve", lambda e: e.tensor_tensor(out=wdt_sb[:, 1, t0 + c0:t0 + c0 + m], in0=ps[0:32, pt, 0:m],
                                                          in1=wdt_sb[:, 0, t0 + c0:t0 + c0 + m], op=ALU.subtract),
                         R=[bps[pt], bwdt[bi]], W=[bwdt[bi]])
            s.barrier()

        with (nc.sbuf_tensor("s3_st", [128, 3, 2048], F32) as st_sb,
              nc.sbuf_tensor("s3_wb", [128, 2, 6, 2048], BF16) as wb_sb,
              nc.sbuf_tensor("s3_sel", [32, NEXP * 128], BF16) as sel_sb,
              nc.sbuf_tensor("s3_wbc", [128, 2, 512], F32) as wbc_sb,
              nc.sbuf_tensor("s3_sg", [128, 2, 512], F32) as sg_sb,
              nc.sbuf_tensor("s3_t", [128, 2, 512], F32) as t3_sb,
              nc.sbuf_tensor("s3_g", [128, 2, 4, 512], BF16) as g_sb):
            bst = [s.buf(f"st{i}", s.dsem(f"st{i}")) for i in range(3)]
            bwb = [[s.buf(f"wb{a}_{p}") for p in range(6)] for a in range(2)]
            bsel = s.buf("sel", cs)
            bwbc = [s.buf(f"wbc{i}") for i in range(2)]
            bsg = [s.buf(f"sg{i}") for i in range(2)]
            bt3 = [s.buf(f"t3{i}") for i in range(2)]
            bg = [s.buf(f"g{i}") for i in range(2)]
            s.dma("act", sel_sb[:], selc, W=[bsel])
            w1v = w1.rearrange("e (kc p) f -> e p kc f", p=128)
            w3v = w3.rearrange("e (kc p) f -> e p kc f", p=128)
            w2v = w2.rearrange("e (fc p) d -> e p fc d", p=128)

            def piece_src(e, p):
                if p < 2:
                    return w1v[e, :, 4 * p:4 * p + 4, :]
                if p < 4:
                    return w3v[e, :, 4 * (p - 2):4 * (p - 2) + 4, :]
                return w2v[e, :, 2 * (p - 4):2 * (p - 4) + 2, :]

            def piece_dma(P):
                e, p = divmod(P, 6)
                if e >= NEXP:
                    return
                sl = P % 3
                dst = st_sb[:, sl, :]
                dst = dst.rearrange("q (a b) -> q a b", a=4) if p < 4 else dst.rearrange("q (a b) -> q a b", a=2)
                s.dma("sp", dst, piece_src(e, p), W=[bst[sl]])

            def piece_cast(P):
                e, p = divmod(P, 6)
                if e >= NEXP:
                    return
                sl = P % 3
                s.op("pool", lambda en: en.tensor_copy(out=wb_sb[:, e % 2, p, :], in_=st_sb[:, sl, :]),
                     R=[bst[sl]], W=[bwb[e % 2][p]])

            for P in range(3):
                piece_dma(P)
            for P in range(6):
                piece_cast(P)
                piece_dma(P + 3)
            gi = 0
            for ex in range(NEXP):
                a = ex % 2
                for bi, (t0, n) in enumerate(BLKS1):
                    if bi < 3:
                        for P in (6 * (ex + 1) + 2 * bi, 6 * (ex + 1) + 2 * bi + 1):
                            piece_cast(P)
                            piece_dma(P + 3)
                    garow = 8 if bi == 4 else 4
                    wr = gi % 2
                    gs = gi % 2
                    gi += 1
                    mm(s, ps[:, 6, 0:n], sel_sb[:, ex * 128:(ex + 1) * 128], wdt_sb[:, 0, t0:t0 + n], True, False,
                       R=[bsel, bwdt[bi]], W=[bps[6]])
                    mm(s, ps[:, 6, 0:n], sel_sb[:, ex * 128:(ex + 1) * 128], wdt_sb[:, 1, t0:t0 + n], False, True,
                       R=[bsel, bwdt[bi]], W=[bps[6]])
                    s.op("act", lambda e: e.copy(out=wbc_sb[:, wr, 0:n], in_=ps[:, 6, 0:n]), R=[bps[6]], W=[bwbc[wr]])
                    for fc in range(4):
                        pr = fc % 2
                        for which in range(2):
                            bank = 2 * pr + which
                            for k in range(8):
                                wv = wb_sb[:, a, 2 * which + k // 4, :].rearrange("q (a b) -> q a b", a=4)
                                mm(s, ps[:, bank, 0:n], wv[:, k % 4, fc * 128:(fc + 1) * 128], hl2_sb[:, k, t0:t0 + n],
                                   k == 0, k == 7, R=[bwb[a][2 * which + k // 4], bhl2[bi]], W=[bps[bank]])
                        s.op("act", lambda e: e.activation(out=sg_sb[:, pr, 0:n], in_=ps[:, 2 * pr, 0:n], func=AF.Silu),
                             R=[bps[2 * pr]], W=[bsg[pr]])
                        s.op("dve", lambda e: e.tensor_tensor(out=t3_sb[:, pr, 0:n], in0=ps[:, 2 * pr + 1, 0:n],
                                                              in1=sg_sb[:, pr, 0:n], op=ALU.mult),
                             R=[bps[2 * pr + 1], bsg[pr]], W=[bt3[pr]])
                        s.op("pool", lambda e: e.tensor_tensor(out=g_sb[:, gs, fc, 0:n], in0=t3_sb[:, pr, 0:n],
                                                               in1=wbc_sb[:, wr, 0:n], op=ALU.mult),
                             R=[bt3[pr], bwbc[wr]], W=[bg[gs]])
                    for dc in range(8):
                        bank = 4 + dc % 2
                        for fc in range(4):
                            wv = wb_sb[:, a, 4 + fc // 2, :].rearrange("q (a b) -> q a b", a=2)
                            mm(s, ps[:, bank, 0:n], wv[:, fc % 2, dc * 128:(dc + 1) * 128], g_sb[:, gs, fc, 0:n],
                               fc == 0, fc == 3, R=[bwb[a][4 + fc // 2], bg[gs]], W=[bps[bank]])
                        s.op("dve", lambda e: e.scalar_tensor_tensor(out=x_sb[:, dc, t0:t0 + n], in0=ps[:, bank, 0:n],
                                                                     scalar=mod_sb[:, garow, dc:dc + 1],
                                                                     in1=x_sb[:, dc, t0:t0 + n], op0=ALU.mult, op1=ALU.add),
                             R=[bps[bank], bmod, bxb[bi]], W=[bxb[bi]])
            s.barrier()

        with (nc.sbuf_tensor("s4_sq", [128, 8, 512], BF16) as sq_sb,
              nc.sbuf_tensor("s4_rstd", [128, 2, 512], F32) as rstd_sb):
            bsq = s.buf("sq4"); brs = [s.buf(f"rs4{i}") for i in range(2)]
            for bi, (t0, n) in enumerate(BLKS1):
                if final:
                    sl = bi % 2
                    s.op("act", lambda e: e.activation(out=sq_sb[:, :, 0:n], in_=x_sb[:, :, t0:t0 + n], func=AF.Square),
                         R=[bxb[bi]], W=[bsq])
                    for k in range(8):
                        mm(s, ps[:, sl, 0:n], ones_sb[:], sq_sb[:, k, 0:n], k == 0, k == 7, R=[bones, bsq], W=[bps[sl]])
                    s.op("act", lambda e: e.activation(out=rstd_sb[:, sl, 0:n], in_=ps[:, sl, 0:n], func=AF.Sqrt,
                                                       scale=1.0 / D, bias=EPS), R=[bps[sl]], W=[brs[sl]])
                    s.op("dve", lambda e: e.reciprocal(out=rstd_sb[:, sl, 0:n], in_=rstd_sb[:, sl, 0:n]),
                         R=[brs[sl]], W=[brs[sl]])
                    for k in range(8):
                        s.op("dve", lambda e: e.scalar_tensor_tensor(
                            out=x_sb[:, k, t0:t0 + n], in0=x_sb[:, k, t0:t0 + n], scalar=mod_sb[:, 9, k:k + 1],
                            in1=rstd_sb[:, sl, 0:n], op0=ALU.mult, op1=ALU.mult),
                            R=[bxb[bi], bmod, brs[sl]], W=[bxb[bi]])
                s.dma("sp", xo[:, :, t0:t0 + n], x_sb[:, :, t0:t0 + n], R=[bxb[bi]])
            s.finish(bxb)
    return nc


def build_p3a():
    nc = bass.Bass("TRN2", target_bir_lowering=False)
    din = lambda n, shp, dt: nc.dram_tensor(n, shp, dt, kind="ExternalInput").ap()
    xT = din("xT", [128, 8, NT1], F32)
    nadf = din("nadf", [128, 4, NT1], BF16)
    hg = din("hg", [128, 12, NT1], F32)
    wout = din("wout", [D, D], F32)
    mod = din("mod", [128, 10, 8], F32)
    wge = din("wge", [128, 8, 36], F32)
    bge = din("bge", [128, 36], F32)
    iota4 = din("iota4", [128, 4], F32)
    ident = din("ident", [128, 128], F32)
    xo = nc.dram_tensor("xo", [128, 8, NT1], F32, kind="ExternalOutput").ap()
    hl2o = nc.dram_tensor("hl2o", [128, 8, NT1], BF16, kind="ExternalOutput").ap()
    wdto = nc.dram_tensor("wdto", [32, 2, NT1], BF16, kind="ExternalOutput").ap()
    gido = nc.dram_tensor("gido", [128, 17], F32, kind="ExternalOutput").ap()
    wd32o = nc.dram_tensor("wd32o", [128, 17, 32], F32, kind="ExternalOutput").ap()
    s = Sched(nc)
    wov = wout.rearrange("(kc p) n -> p kc n", p=128)
    with (nc.psum_tensor("ps", [128, 8, 512], F32) as ps,
          nc.sbuf_tensor("x_sb", [128, 8, NT1], F32) as x_sb,
          nc.sbuf_tensor("mod_sb", [128, 10, 8], F32) as mod_sb,
          nc.sbuf_tensor("hl2_sb", [128, 8, NT1], BF16) as hl2_sb,
          nc.sbuf_tensor("wdt_sb", [32, 2, NT1], BF16) as wdt_sb,
          nc.sbuf_tensor("ones_sb", [128, 128], BF16) as ones_sb):
        bps = [s.buf(f"ps{i}") for i in range(8)]
        cs = s.dsem("const")
        bxb = [s.buf(f"x{i}", s.dsem(f"x{i}")) for i in range(len(BLKS1))]
        bmod = s.buf("mod", cs)
        bhl2 = [s.buf(f"hl2_{i}") for i in range(len(BLKS1))]
        bwdt = [s.buf(f"wdt{i}") for i in range(len(BLKS1))]
        bones = s.buf("ones")
        s.dma("sp", mod_sb[:], mod, W=[bmod])
        for bi, (t0, n) in enumerate(BLKS1):
            s.dma("sp", x_sb[:, :, t0:t0 + n], xT[:, :, t0:t0 + n], W=[bxb[bi]])
        s.op("pool", lambda e: e.memset(ones_sb[:], 1.0), W=[bones])

        with (nc.sbuf_tensor("s1_mix", [128, 8, NT1], BF16) as mix_sb,
              nc.sbuf_tensor("s1_wobf", [128, 8, D], BF16) as wo_bf,
              nc.sbuf_tensor("s1_wost", [128, 2, D], F32) as wo_st,
              nc.sbuf_tensor("s1_hg", [128, 1, 12, 512], F32) as hg_sb,
              nc.sbuf_tensor("s1_t", [128, 4, 512], F32) as t_sb):
            bmixl = s.buf("mixl", s.dsem("mixl"))
            bmix = [s.buf(f"mix{i}") for i in range(len(BLKS1))]
            bwost = [s.buf(f"wost{i}", s.dsem(f"wost{i}")) for i in range(2)]
            bwobf = [s.buf(f"wobf{k}") for k in range(8)]
            bhg = [s.buf(f"hg{i}", s.dsem(f"hg{i}")) for i in range(2)]
            bt = [s.buf(f"t{i}") for i in range(4)]
            s.dma("act", mix_sb[:, 0:2, :], nadf[:, 0:2, :], W=[bmixl])
            s.dma("act", mix_sb[:, 6:8, :], nadf[:, 2:4, :], W=[bmixl])
            for k in range(8):
                sl = k % 2
                s.dma("act", wo_st[:, sl, :], wov[:, k, :], W=[bwost[sl]])
                s.op("pool", lambda e: e.tensor_copy(out=wo_bf[:, k, :], in_=wo_st[:, sl, :]), R=[bwost[sl]], W=[bwobf[k]])
            for bi, (t0, n) in enumerate(BLKS1):
                sl = 0
                s.dma("sp", hg_sb[:, sl, :, 0:n], hg[:, :, t0:t0 + n], W=[bhg[sl]])
                for ch in range(4):
                    hf_ = hg_sb[:, sl, ch, 0:n]; hb_ = hg_sb[:, sl, 4 + ch, 0:n]; gr_ = hg_sb[:, sl, 8 + ch, 0:n]
                    s.op("dve", lambda e: e.tensor_tensor(out=t_sb[:, 0, 0:n], in0=hf_, in1=hb_, op=ALU.add),
                         R=[bhg[sl]], W=[bt[0]])
                    s.op("dve", lambda e: e.tensor_tensor(out=t_sb[:, 1, 0:n], in0=gr_, in1=gr_, op=ALU.mult),
                         R=[bhg[sl]], W=[bt[1]])
                    s.op("dve", lambda e: e.tensor_scalar(out=t_sb[:, 1, 0:n], in0=t_sb[:, 1, 0:n], scalar1=0.044715,
                                                          scalar2=1.0, op0=ALU.mult, op1=ALU.add), R=[bt[1]], W=[bt[1]])
                    s.op("pool", lambda e: e.tensor_tensor(out=t_sb[:, 2, 0:n], in0=t_sb[:, 1, 0:n], in1=gr_, op=ALU.mult),
                         R=[bt[1], bhg[sl]], W=[bt[2]])
                    s.op("act", lambda e: e.activation(out=t_sb[:, 2, 0:n], in_=t_sb[:, 2, 0:n], func=AF.Sigmoid,
                                                       scale=GELU_C), R=[bt[2]], W=[bt[2]])
                    s.op("pool", lambda e: e.tensor_tensor(out=t_sb[:, 3, 0:n], in0=t_sb[:, 2, 0:n], in1=gr_, op=ALU.mult),
                         R=[bt[2], bhg[sl]], W=[bt[3]])
                    s.op("pool", lambda e: e.tensor_tensor(out=mix_sb[:, 2 + ch, t0:t0 + n], in0=t_sb[:, 3, 0:n],
                                                           in1=t_sb[:, 0, 0:n], op=ALU.mult),
                         R=[bt[3], bt[0]], W=[bmix[bi]])
            pi = 0
            for bi, (t0, n) in enumerate(BLKS1):
                garow = 5 if bi == 4 else 1
                for dc in range(8):
                    pb = pi % 8; pi += 1
                    for k in range(8):
                        mm(s, ps[:, pb, 0:n], wo_bf[:, k, dc * 128:(dc + 1) * 128], mix_sb[:, k, t0:t0 + n],
                           k == 0, k == 7, R=[bwobf[k], bmix[bi], bmixl], W=[bps[pb]])
                    s.op("dve", lambda e: e.scalar_tensor_tensor(out=x_sb[:, dc, t0:t0 + n], in0=ps[:, pb, 0:n],
                                                                 scalar=mod_sb[:, garow, dc:dc + 1],
                                                                 in1=x_sb[:, dc, t0:t0 + n], op0=ALU.mult, op1=ALU.add),
                         R=[bps[pb], bmod, bxb[bi]], W=[bxb[bi]])
            s.barrier()

        es_ = ExitStack()
        io4_sb = es_.enter_context(nc.sbuf_tensor("s2_io4", [128, 4], F32))
        gid_sb = es_.enter_context(nc.sbuf_tensor("s2_gid", [128, 17], F32))
        junk4_sb = es_.enter_context(nc.sbuf_tensor("s2_junk", [128, 4], F32))
        wd32_sb = es_.enter_context(nc.sbuf_tensor("s2_wd32", [128, 17, 32], F32))
        with (nc.sbuf_tensor("s2_sq", [128, 8, 512], BF16) as sq_sb,
              nc.sbuf_tensor("s2_rstd", [128, 2, 512], F32) as rstd_sb,
              nc.sbuf_tensor("s2_tmp", [128, 4, 512], F32) as tmp_sb,
              nc.sbuf_tensor("s2_hf", [128, 2, 8, 512], F32) as hf_sb,
              nc.sbuf_tensor("s2_ab", [128, 2, 8], F32) as ab_sb,
              nc.sbuf_tensor("s2_wge", [128, 8, 36], F32) as wge_sb,
              nc.sbuf_tensor("s2_bge", [128, 36], F32) as bge_sb,
              nc.sbuf_tensor("s2_id", [128, 128], F32) as id_sb,
              nc.sbuf_tensor("s2_rt", [128, 2, 128], F32) as rt_sb):
            bsq = s.buf("sq"); brs = [s.buf(f"rs{i}") for i in range(2)]
            btmp = [s.buf(f"tmp{i}") for i in range(4)]
            bhf = [s.buf(f"hf{i}") for i in range(2)]
            bab = s.buf("ab")
            bwge = s.buf("wge", cs); bbge = s.buf("bge", cs); bid = s.buf("id", cs)
            brt = [s.buf(f"rt{i}") for i in range(2)]
            s.dma("act", wge_sb[:], wge, W=[bwge])
            s.dma("act", bge_sb[:], bge, W=[bbge])
            s.dma("act", id_sb[:], ident, W=[bid])
            bio4 = s.buf("io4", cs); bgid = s.buf("gid", s.dsem("gid")); bjk = s.buf("junk4")
            s.dma("act", io4_sb[:], iota4, W=[bio4])
            s.op("pool", lambda e: e.memset(gid_sb[:], 0.0), W=[bgid])
            bwd32 = s.buf("wd32", s.dsem("wd32"))
            s.op("pool", lambda e: e.memset(wd32_sb[:], 0.0), W=[bwd32])
            for t in range(2):
                s.op("dve", lambda e: e.scalar_tensor_tensor(
                    out=ab_sb[:, t, :], in0=mod_sb[:, 2 + 4 * t, :], scalar=1.0, in1=mod_sb[:, 0, :],
                    op0=ALU.add, op1=ALU.mult), R=[bmod], W=[bab])
            ti_g = 0
            for bi, (t0, n) in enumerate(BLKS1):
                sl = bi % 2
                isctx = bi == 4
                s.op("act", lambda e: e.activation(out=sq_sb[:, :, 0:n], in_=x_sb[:, :, t0:t0 + n], func=AF.Square),
                     R=[bxb[bi]], W=[bsq])
                for k in range(8):
                    mm(s, ps[:, sl, 0:n], ones_sb[:], sq_sb[:, k, 0:n], k == 0, k == 7, R=[bones, bsq], W=[bps[sl]])
                s.op("act", lambda e: e.activation(out=rstd_sb[:, sl, 0:n], in_=ps[:, sl, 0:n], func=AF.Sqrt,
                                                   scale=1.0 / D, bias=EPS), R=[bps[sl]], W=[brs[sl]])
                s.op("dve", lambda e: e.reciprocal(out=rstd_sb[:, sl, 0:n], in_=rstd_sb[:, sl, 0:n]),
                     R=[brs[sl]], W=[brs[sl]])
                ai = 1 if isctx else 0
                shrow = 7 if isctx else 3
                for k in range(8):
                    tb = k % 4
                    s.op("dve", lambda e: e.scalar_tensor_tensor(
                        out=tmp_sb[:, tb, 0:n], in0=x_sb[:, k, t0:t0 + n], scalar=ab_sb[:, ai, k:k + 1],
                        in1=rstd_sb[:, sl, 0:n], op0=ALU.mult, op1=ALU.mult),
                        R=[bxb[bi], bab, brs[sl]], W=[btmp[tb]])
                    s.op("act", lambda e: e.activation(out=hf_sb[:, sl, k, 0:n], in_=tmp_sb[:, tb, 0:n],
                                                       func=AF.Identity, bias=mod_sb[:, shrow, k:k + 1], scale=1.0),
                         R=[btmp[tb], bmod], W=[bhf[sl]])
                s.op("pool", lambda e: e.tensor_copy(out=hl2_sb[:, :, t0:t0 + n], in_=hf_sb[:, sl, :, 0:n]),
                     R=[bhf[sl]], W=[bhl2[bi]])
                for tt in range((n + 127) // 128):
                    c0 = tt * 128
                    m = min(128, n - c0)
                    rs_ = ti_g % 2; ti_g += 1
                    pb = 2 + rs_
                    rt = rt_sb[0:m, rs_, :]
                    brr = brt[rs_]
                    for k in range(8):
                        mm(s, ps[0:m, pb, 0:36], hf_sb[:, sl, k, c0:c0 + m], wge_sb[:, k, :], k == 0, k == 7,
                           R=[bhf[sl], bwge], W=[bps[pb]])
                    V = lambda eng, fn, R_=(), W_=(): s.op(eng, fn, R=[brr] + list(R_), W=[brr] + list(W_))
                    lg = rt[:, 0:36]
                    s.op("dve", lambda e: e.tensor_tensor(out=lg, in0=ps[0:m, pb, 0:36], in1=bge_sb[0:m, :], op=ALU.add),
                         R=[bps[pb], bbge], W=[brr])
                    gmax = rt[:, 36:37]; ngmax = rt[:, 37:38]; sume = rt[:, 38:39]; gtop = rt[:, 39:40]
                    eg = rt[:, 40:44]; ohg = rt[:, 44:48]; sel = rt[:, 48:56]; top8 = rt[:, 56:64]
                    dd = rt[:, 64:65]; ed = rt[:, 65:66]; w1_ = rt[:, 66:67]; wt1 = rt[:, 67:68]; wt2 = rt[:, 68:69]
                    ea = rt[:, 72:80]; eb_ = rt[:, 80:88]; wd = rt[:, 96:128]
                    V("dve", lambda e: e.reduce_max(out=gmax, in_=lg[:, 0:4], axis=AX.X))
                    V("dve", lambda e: e.tensor_scalar(out=ngmax, in0=gmax, scalar1=-1.0, scalar2=None, op0=ALU.mult))
                    V("act", lambda e: e.activation(out=eg, in_=lg[:, 0:4], func=AF.Exp, bias=ngmax, scale=1.0,
                                                    accum_out=sume))
                    V("dve", lambda e: e.reciprocal(out=gtop, in_=sume))
                    V("dve", lambda e: e.tensor_scalar(out=ohg, in0=lg[:, 0:4], scalar1=gmax, scalar2=None,
                                                       op0=ALU.is_equal))
                    tgl = (t0 + c0) // 128
                    s.op("dve", lambda e: e.scalar_tensor_tensor(out=junk4_sb[0:m, :], in0=ohg, scalar=1.0,
                                                                 in1=io4_sb[0:m, :], op0=ALU.mult, op1=ALU.mult,
                                                                 accum_out=gid_sb[0:m, tgl:tgl + 1]),
                         R=[brr, bio4, bjk], W=[bjk, bgid])
                    V("dve", lambda e: e.tensor_scalar(out=sel, in0=lg[:, 4:12], scalar1=ohg[:, 0:1], scalar2=None,
                                                       op0=ALU.mult))
                    for g in range(1, 4):
                        V("dve", lambda e: e.scalar_tensor_tensor(out=sel, in0=lg[:, 4 + 8 * g:12 + 8 * g],
                                                                  scalar=ohg[:, g:g + 1], in1=sel,
                                                                  op0=ALU.mult, op1=ALU.add))
                    V("dve", lambda e: e.max(out=top8, in_=sel))
                    V("dve", lambda e: e.tensor_tensor(out=dd, in0=top8[:, 1:2], in1=top8[:, 0:1], op=ALU.subtract))
                    V("act", lambda e: e.activation(out=ed, in_=dd, func=AF.Exp))
                    V("dve", lambda e: e.tensor_scalar(out=w1_, in0=ed, scalar1=1.0, scalar2=None, op0=ALU.add))
                    V("dve", lambda e: e.reciprocal(out=w1_, in_=w1_))
                    V("dve", lambda e: e.tensor_tensor(out=wt1, in0=w1_, in1=gtop, op=ALU.mult))
                    V("dve", lambda e: e.tensor_tensor(out=wt2, in0=wt1, in1=ed, op=ALU.mult))
                    V("dve", lambda e: e.tensor_scalar(out=ea, in0=sel, scalar1=top8[:, 0:1], scalar2=wt1,
                                                       op0=ALU.is_equal, op1=ALU.mult))
                    V("dve", lambda e: e.tensor_scalar(out=eb_, in0=sel, scalar1=top8[:, 1:2], scalar2=wt2,
                                                       op0=ALU.is_equal, op1=ALU.mult))
                    V("dve", lambda e: e.tensor_tensor(out=ea, in0=ea, in1=eb_, op=ALU.add))
                    for g in range(4):
                        V("dve", lambda e: e.tensor_scalar(out=wd[:, 8 * g:8 * g + 8], in0=ea, scalar1=ohg[:, g:g + 1],
                                                           scalar2=None, op0=ALU.mult))
                    s.op("dve", lambda e: e.tensor_copy(out=wd32_sb[0:m, tgl, :], in_=wd), R=[brr], W=[bwd32])
                    pt = 4 + rs_
                    s.op("pe", lambda e: e.transpose(ps[0:32, pt, 0:m], wd, id_sb[0:m, 0:m]), R=[brr, bid], W=[bps[pt]])
                    s.op("act", lambda e: e.copy(out=wdt_sb[:, 0, t0 + c0:t0 + c0 + m], in_=ps[0:32, pt, 0:m]),
                         R=[bps[pt]], W=[bwdt[bi]])
                    s.op("dve", lambda e: e.tensor_tensor(out=wdt_sb[:, 1, t0 + c0:t0 + c0 + m], in0=ps[0:32, pt, 0:m],
                                                          in1=wdt_sb[:, 0, t0 + c0:t0 + c0 + m], op=ALU.subtract),
                         R=[bps[pt], bwdt[bi]], W=[bwdt[bi]])
            bxo = s.buf("xout", s.dsem("xout"))
            s.dma("sp", xo, x_sb[:], R=bxb, W=[bxo])
            s.dma("sp", hl2o, hl2_sb[:], R=bhl2, W=[bxo])
            s.dma("sp", wdto, wdt_sb[:], R=bwdt, W=[bxo])
            s.dma("sp", gido, gid_sb[:], R=[bgid], W=[bxo])
            s.dma("sp", wd32o, wd32_sb[:], R=[bwd32], W=[bxo])
            s.finish([bxo])
        es_.close()
    return nc


def build_p3b(ntb):
    NE = 8
    nblk_all = ntb // 512
    nh = 2 if ntb > 2048 else 1
    nblk = -(-nblk_all // nh)
    nth = nblk * 512
    nc = bass.Bass("TRN2", target_bir_lowering=False)
    din = lambda n, shp, dt: nc.dram_tensor(n, shp, dt, kind="ExternalInput").ap()
    hl2 = din("hl2", [128, 8, ntb], BF16)
    wdt = din("wdt", [8, 2, ntb], BF16)
    selc = din("selc", [8, NE * 128], BF16)
    w1 = din("w1", [NE, D, 512], F32)
    w3 = din("w3", [NE, D, 512], F32)
    w2 = din("w2", [NE, 512, D], F32)
    yo = nc.dram_tensor("yo", [128, 8, ntb], F32, kind="ExternalOutput").ap()
    s = Sched(nc)
    with (nc.psum_tensor("ps", [128, 8, 512], F32) as ps,
          nc.sbuf_tensor("y_sb", [128, 8, nth], F32) as y_sb,
          nc.sbuf_tensor("hl2_sb", [128, 8, nth], BF16) as hl2_sb,
          nc.sbuf_tensor("wdt_sb", [8, 2, ntb], BF16) as wdt_sb,
          nc.sbuf_tensor("s3_st", [128, 3, 2048], F32) as st_sb,
          nc.sbuf_tensor("s3_wb", [128, 2, 6, 2048], BF16) as wb_sb,
          nc.sbuf_tensor("s3_sel", [8, NE * 128], BF16) as sel_sb,
          nc.sbuf_tensor("s3_wbc", [128, 2, 512], F32) as wbc_sb,
          nc.sbuf_tensor("s3_sg", [128, 2, 512], F32) as sg_sb,
          nc.sbuf_tensor("s3_t", [128, 2, 512], F32) as t3_sb,
          nc.sbuf_tensor("s3_g", [128, 2, 4, 512], BF16) as g_sb):
        bps = [s.buf(f"ps{i}") for i in range(8)]
        cs = s.dsem("const")
        by = [s.buf(f"y{i}", s.dsem(f"y{i}")) for i in range(nblk)]
        bhl2 = [s.buf(f"hl2_{i}", s.dsem(f"hl{i}")) for i in range(nblk)]
        bwdt = s.buf("wdt", cs)
        bst = [s.buf(f"st{i}", s.dsem(f"st{i}")) for i in range(3)]
        bwb = [[s.buf(f"wb{a}_{p}") for p in range(6)] for a in range(2)]
        bsel = s.buf("sel", cs)
        bwbc = [s.buf(f"wbc{i}") for i in range(2)]
        bsg = [s.buf(f"sg{i}") for i in range(2)]
        bt3 = [s.buf(f"t3{i}") for i in range(2)]
        bg = [s.buf(f"g{i}") for i in range(2)]
        s.dma("act", sel_sb[:], selc, W=[bsel])
        s.dma("act", wdt_sb[:], wdt, W=[bwdt])
        w1v = w1.rearrange("e (kc p) f -> e p kc f", p=128)
        w3v = w3.rearrange("e (kc p) f -> e p kc f", p=128)
        w2v = w2.rearrange("e (fc p) d -> e p fc d", p=128)

        def piece_src(e, p):
            if p < 2:
                return w1v[e, :, 4 * p:4 * p + 4, :]
            if p < 4:
                return w3v[e, :, 4 * (p - 2):4 * (p - 2) + 4, :]
            return w2v[e, :, 2 * (p - 4):2 * (p - 4) + 2, :]

        def piece_dma(P):
            e, p = divmod(P, 6)
            if e >= NE * nh:
                return
            e = e % NE
            sl = P % 3
            dst = st_sb[:, sl, :]
            dst = dst.rearrange("q (a b) -> q a b", a=4) if p < 4 else dst.rearrange("q (a b) -> q a b", a=2)
            s.dma("sp", dst, piece_src(e, p), W=[bst[sl]])

        def piece_cast(P):
            e, p = divmod(P, 6)
            if e >= NE * nh:
                return
            sl = P % 3
            s.op("pool", lambda en: en.tensor_copy(out=wb_sb[:, e % 2, p, :], in_=st_sb[:, sl, :]),
                 R=[bst[sl]], W=[bwb[e % 2][p]])

        for P in range(3):
            piece_dma(P)
        for P in range(6):
            piece_cast(P)
            piece_dma(P + 3)
        gi = 0
        n = 512
        pend = [6 * 1 + i for i in range(6)]
        for vx in range(NE * nh):
            half, ex = divmod(vx, NE)
            a = vx % 2
            pend = [6 * (vx + 1) + i for i in range(6)]
            hb0 = half * nblk
            nb_h = min(nblk, nblk_all - hb0)
            if ex == 0:
                for bi in range(nb_h):
                    s.dma("act", hl2_sb[:, :, bi * 512:(bi + 1) * 512], hl2[:, :, (hb0 + bi) * 512:(hb0 + bi + 1) * 512],
                          W=[bhl2[bi]])
            for bi in range(nb_h):
                t0 = bi * 512
                tg = (hb0 + bi) * 512
                npc = (6 + nb_h - 1) // nb_h
                for P in pend[bi * npc:(bi + 1) * npc]:
                    piece_cast(P)
                    piece_dma(P + 3)
                wr = gi % 2
                gs = gi % 2
                gi += 1
                mm(s, ps[:, 6, 0:n], sel_sb[:, ex * 128:(ex + 1) * 128], wdt_sb[:, 0, tg:tg + n], True, False,
                   R=[bsel, bwdt], W=[bps[6]])
                mm(s, ps[:, 6, 0:n], sel_sb[:, ex * 128:(ex + 1) * 128], wdt_sb[:, 1, tg:tg + n], False, True,
                   R=[bsel, bwdt], W=[bps[6]])
                s.op("act", lambda e: e.copy(out=wbc_sb[:, wr, 0:n], in_=ps[:, 6, 0:n]), R=[bps[6]], W=[bwbc[wr]])
                for fc in range(4):
                    pr = fc % 2
                    for which in range(2):
                        bank = 2 * pr + which
                        for k in range(8):
                            wv = wb_sb[:, a, 2 * which + k // 4, :].rearrange("q (a b) -> q a b", a=4)
                            mm(s, ps[:, bank, 0:n], wv[:, k % 4, fc * 128:(fc + 1) * 128], hl2_sb[:, k, t0:t0 + n],
                               k == 0, k == 7, R=[bwb[a][2 * which + k // 4], bhl2[bi]], W=[bps[bank]])
                    s.op("act", lambda e: e.activation(out=sg_sb[:, pr, 0:n], in_=ps[:, 2 * pr, 0:n], func=AF.Silu),
                         R=[bps[2 * pr]], W=[bsg[pr]])
                    s.op("dve", lambda e: e.tensor_tensor(out=t3_sb[:, pr, 0:n], in0=ps[:, 2 * pr + 1, 0:n],
                                                          in1=sg_sb[:, pr, 0:n], op=ALU.mult),
                         R=[bps[2 * pr + 1], bsg[pr]], W=[bt3[pr]])
                    s.op("pool", lambda e: e.tensor_tensor(out=g_sb[:, gs, fc, 0:n], in0=t3_sb[:, pr, 0:n],
                                                           in1=wbc_sb[:, wr, 0:n], op=ALU.mult),
                         R=[bt3[pr], bwbc[wr]], W=[bg[gs]])
                for dc in range(8):
                    bank = 4 + dc % 2
                    for fc in range(4):
                        wv = wb_sb[:, a, 4 + fc // 2, :].rearrange("q (a b) -> q a b", a=2)
                        mm(s, ps[:, bank, 0:n], wv[:, fc % 2, dc * 128:(dc + 1) * 128], g_sb[:, gs, fc, 0:n],
                           fc == 0, fc == 3, R=[bwb[a][4 + fc // 2], bg[gs]], W=[bps[bank]])
                    if ex == 0:
                        s.op("dve", lambda e: e.tensor_copy(out=y_sb[:, dc, t0:t0 + n], in_=ps[:, bank, 0:n]),
                             R=[bps[bank]], W=[by[bi]])
                    else:
                        s.op("dve", lambda e: e.tensor_tensor(out=y_sb[:, dc, t0:t0 + n], in0=ps[:, bank, 0:n],
                                                              in1=y_sb[:, dc, t0:t0 + n], op=ALU.add),
                             R=[bps[bank], by[bi]], W=[by[bi]])
            if ex == NE - 1:
                for bi in range(nb_h):
                    s.dma("sp", yo[:, :, (hb0 + bi) * 512:(hb0 + bi + 1) * 512], y_sb[:, :, bi * 512:(bi + 1) * 512],
                          R=[by[bi]])
        s.finish(by)
    return nc


def build_pc(final):
    nc = bass.Bass("TRN2", target_bir_lowering=False)
    din = lambda n, shp, dt: nc.dram_tensor(n, shp, dt, kind="ExternalInput").ap()
    xT = din("xT", [128, 8, NT1], F32)
    yT = din("yT", [128, 8, NT1], F32)
    mod = din("mod", [128, 3, 8], F32)
    xo = nc.dram_tensor("xo", [128, 8, NT1], F32, kind="ExternalOutput").ap()
    s = Sched(nc)
    with (nc.psum_tensor("ps", [128, 2, 512], F32) as ps,
          nc.sbuf_tensor("x_sb", [128, 8, NT1], F32) as x_sb,
          nc.sbuf_tensor("y_sb", [128, 8, NT1], F32) as y_sb,
          nc.sbuf_tensor("mod_sb", [128, 3, 8], F32) as mod_sb,
          nc.sbuf_tensor("ones_sb", [128, 128], BF16) as ones_sb,
          nc.sbuf_tensor("sq_sb", [128, 8, 512], BF16) as sq_sb,
          nc.sbuf_tensor("rstd_sb", [128, 2, 512], F32) as rstd_sb):
        bxb = [s.buf(f"x{i}", s.dsem(f"x{i}")) for i in range(len(BLKS1))]
        byb = [s.buf(f"y{i}", s.dsem(f"yy{i}")) for i in range(len(BLKS1))]
        bmod = s.buf("mod", s.dsem("mod"))
        bones = s.buf("ones"); bsq = s.buf("sq"); brs = [s.buf(f"rs{i}") for i in range(2)]
        bps = [s.buf(f"ps{i}") for i in range(2)]
        s.dma("sp", mod_sb[:], mod, W=[bmod])
        s.op("pool", lambda e: e.memset(ones_sb[:], 1.0), W=[bones])
        for bi, (t0, n) in enumerate(BLKS1):
            s.dma("sp", x_sb[:, :, t0:t0 + n], xT[:, :, t0:t0 + n], W=[bxb[bi]])
            s.dma("act", y_sb[:, :, t0:t0 + n], yT[:, :, t0:t0 + n], W=[byb[bi]])
        for bi, (t0, n) in enumerate(BLKS1):
            garow = 1 if bi == 4 else 0
            sl = bi % 2
            for k in range(8):
                s.op("dve", lambda e: e.scalar_tensor_tensor(
                    out=x_sb[:, k, t0:t0 + n], in0=y_sb[:, k, t0:t0 + n], scalar=mod_sb[:, garow, k:k + 1],
                    in1=x_sb[:, k, t0:t0 + n], op0=ALU.mult, op1=ALU.add),
                    R=[byb[bi], bmod, bxb[bi]], W=[bxb[bi]])
            if final:
                s.op("act", lambda e: e.activation(out=sq_sb[:, :, 0:n], in_=x_sb[:, :, t0:t0 + n], func=AF.Square),
                     R=[bxb[bi]], W=[bsq])
                for k in range(8):
                    mm(s, ps[:, sl, 0:n], ones_sb[:], sq_sb[:, k, 0:n], k == 0, k == 7, R=[bones, bsq], W=[bps[sl]])
                s.op("act", lambda e: e.activation(out=rstd_sb[:, sl, 0:n], in_=ps[:, sl, 0:n], func=AF.Sqrt,
                                                   scale=1.0 / D, bias=EPS), R=[bps[sl]], W=[brs[sl]])
                s.op("dve", lambda e: e.reciprocal(out=rstd_sb[:, sl, 0:n], in_=rstd_sb[:, sl, 0:n]),
                     R=[brs[sl]], W=[brs[sl]])
                for k in range(8):
                    s.op("dve", lambda e: e.scalar_tensor_tensor(
                        out=x_sb[:, k, t0:t0 + n], in0=x_sb[:, k, t0:t0 + n], scalar=mod_sb[:, 2, k:k + 1],
                        in1=rstd_sb[:, sl, 0:n], op0=ALU.mult, op1=ALU.mult),
                        R=[bxb[bi], bmod, brs[sl]], W=[bxb[bi]])
            s.dma("sp", xo[:, :, t0:t0 + n], x_sb[:, :, t0:t0 + n], R=[bxb[bi]])
        s.finish(bxb)
    return nc


def run_p3(nc3, l, xl, xc, yna, ydf, hf, hb, fmf, mods_l, inp, g_final):
    wge = np.concatenate([inp["router_w_group"][l], inp["router_w_expert"][l]], axis=1)
    wge = np.ascontiguousarray(wge.reshape(8, 128, 36).transpose(1, 0, 2))
    bge = np.concatenate([inp["router_b_group"][l], inp["router_b_expert"][l]])
    bge = np.ascontiguousarray(np.tile(bge[None, :], (128, 1))).astype(np.float32)
    selc = np.zeros((32, NEXP, 128), np.float32)
    for e in range(NEXP):
        selc[e, e, :] = 1.0
    selc = selc.reshape(32, NEXP * 128).astype(ml_dtypes.bfloat16)
    ident = np.eye(128, dtype=np.float32)
    in_maps = []
    for i in range(NCORES):
        b, j = i // 4, i % 4
        lat = slice(2048 * j, 2048 * (j + 1))
        ctxs = slice(S + 64 * j, S + 64 * (j + 1))
        xx = np.concatenate([xl[b, lat], xc[b, 64 * j:64 * (j + 1)]], axis=0)
        na = np.concatenate([yna[b, lat], yna[b, ctxs]], axis=0)
        df = np.concatenate([ydf[b, lat], ydf[b, ctxs]], axis=0)
        nadf = np.concatenate([na, df], axis=1)
        nadf = np.ascontiguousarray(nadf.T.reshape(4, 128, NT1).transpose(1, 0, 2))
        def tk(a):
            aa = np.concatenate([a[:, lat], a[:, ctxs]], axis=1)
            return aa.reshape(4, 128, NT1).transpose(1, 0, 2)
        hgt = np.ascontiguousarray(np.concatenate([tk(hf[b]), tk(hb[b]), tk(fmf[b, 512:1024])], axis=1))
        m = mods_l
        rows = [inp["g_ffn"][l], m[b, 2048:3072], m[b, 4096:5120], m[b, 3072:4096], m[b, 5120:6144],
                m[2, 2048:3072], m[2, 4096:5120], m[2, 3072:4096], m[2, 5120:6144], g_final]
        mod = np.ascontiguousarray(np.stack([vec_pk(r) for r in rows], axis=1)).astype(np.float32)
        in_maps.append({"xT": chunkT(xx), "nadf": nadf, "hg": hgt, "wout": inp["w_out"][l], "mod": mod,
                        "wge": wge, "bge": bge, "selc": selc, "ident": ident,
                        "w1": inp["moe_w1"][l], "w3": inp["moe_w3"][l], "w2": inp["moe_w2"][l]})
    res = run_bass_kernel_spmd(nc3, in_maps, core_ids=list(range(NCORES)))
    xl2 = np.zeros_like(xl); xc2 = np.zeros_like(xc)
    for i in range(NCORES):
        b, j = i // 4, i % 4
        o = res.results[i]["xo"].transpose(1, 0, 2).reshape(D, NT1).T
        xl2[b, 2048 * j:2048 * (j + 1)] = o[:2048]
        xc2[b, 64 * j:64 * (j + 1)] = o[2048:]
    return xl2, xc2


def build_p3e(caps):
    NE = 4
    captot = sum(caps)
    nc = bass.Bass("TRN2", target_bir_lowering=False)
    din = lambda n, shp, dt: nc.dram_tensor(n, shp, dt, kind="ExternalInput").ap()
    xs = din("xs", [128, 8, captot], BF16)
    w1 = din("w1", [NE, D, 512], F32)
    w3 = din("w3", [NE, D, 512], F32)
    w2 = din("w2", [NE, 512, D], F32)
    yo = nc.dram_tensor("yo", [128, 8, captot], F32, kind="ExternalOutput").ap()
    s = Sched(nc)
    with (nc.psum_tensor("ps", [128, 8, 512], F32) as ps,
          nc.sbuf_tensor("x_sb", [128, 2, 8, 512], BF16) as x_sb,
          nc.sbuf_tensor("y_sb", [128, 2, 8, 512], F32) as y_sb,
          nc.sbuf_tensor("s3_st", [128, 6, 2048], F32) as st_sb,
          nc.sbuf_tensor("s3_wb", [128, 2, 6, 2048], BF16) as wb_sb,
          nc.sbuf_tensor("s3_sg", [128, 2, 512], F32) as sg_sb,
          nc.sbuf_tensor("s3_g", [128, 2, 4, 512], BF16) as g_sb):
        bps = [s.buf(f"ps{i}") for i in range(8)]
        bx = [s.buf(f"x{i}", s.dsem(f"x{i}")) for i in range(2)]
        by = [s.buf(f"y{i}", s.dsem(f"y{i}")) for i in range(2)]
        bst = [s.buf(f"st{i}", s.dsem(f"st{i}")) for i in range(6)]
        bwb = [[s.buf(f"wb{a}_{p}") for p in range(6)] for a in range(2)]
        bsg = [s.buf(f"sg{i}") for i in range(2)]
        bg = [s.buf(f"g{i}") for i in range(2)]
        w1v = w1.rearrange("e (kc p) f -> e p kc f", p=128)
        w3v = w3.rearrange("e (kc p) f -> e p kc f", p=128)
        w2v = w2.rearrange("e (fc p) d -> e p fc d", p=128)

        def piece_src(e, p):
            if p < 2:
                return w1v[e, :, 4 * p:4 * p + 4, :]
            if p < 4:
                return w3v[e, :, 4 * (p - 2):4 * (p - 2) + 4, :]
            return w2v[e, :, 2 * (p - 4):2 * (p - 4) + 2, :]

        def piece_dma(P):
            e, p = divmod(P, 6)
            if e >= NE:
                return
            sl = P % 6
            dst = st_sb[:, sl, :]
            dst = dst.rearrange("q (a b) -> q a b", a=4) if p < 4 else dst.rearrange("q (a b) -> q a b", a=2)
            s.dma("sp", dst, piece_src(e, p), W=[bst[sl]])

        def piece_cast(P):
            e, p = divmod(P, 6)
            if e >= NE:
                return
            sl = P % 6
            s.op("pool", lambda en: en.tensor_copy(out=wb_sb[:, e % 2, p, :], in_=st_sb[:, sl, :]),
                 R=[bst[sl]], W=[bwb[e % 2][p]])

        for P in range(6):
            piece_dma(P)
        for P in range(6):
            piece_cast(P)
            piece_dma(P + 6)
        gi = 0
        seg0 = 0
        for ex in range(NE):
            a = ex % 2
            pend = [6 * (ex + 1) + i for i in range(6)]
            blocks = [(seg0 + t, min(512, caps[ex] - t)) for t in range(0, caps[ex], 512)]
            seg0 += caps[ex]
            nb = len(blocks)
            npc = (6 + nb - 1) // nb
            for bi, (t0, n) in enumerate(blocks):
                for P in pend[bi * npc:(bi + 1) * npc]:
                    piece_cast(P)
                    piece_dma(P + 6)
                xs_ = gi % 2
                gs = gi % 2
                gi += 1
                s.dma("act", x_sb[:, xs_, :, 0:n], xs[:, :, t0:t0 + n], W=[bx[xs_]])
                for fc in range(4):
                    pr = fc % 2
                    for which in range(2):
                        bank = 2 * pr + which
                        for k in range(8):
                            wv = wb_sb[:, a, 2 * which + k // 4, :].rearrange("q (a b) -> q a b", a=4)
                            mm(s, ps[:, bank, 0:n], wv[:, k % 4, fc * 128:(fc + 1) * 128], x_sb[:, xs_, k, 0:n],
                               k == 0, k == 7, R=[bwb[a][2 * which + k // 4], bx[xs_]], W=[bps[bank]])
                    s.op("act", lambda e: e.activation(out=sg_sb[:, pr, 0:n], in_=ps[:, 2 * pr, 0:n], func=AF.Silu),
                         R=[bps[2 * pr]], W=[bsg[pr]])
                    s.op("dve", lambda e: e.tensor_tensor(out=g_sb[:, gs, fc, 0:n], in0=ps[:, 2 * pr + 1, 0:n],
                                                          in1=sg_sb[:, pr, 0:n], op=ALU.mult),
                         R=[bps[2 * pr + 1], bsg[pr]], W=[bg[gs]])
                for dc in range(8):
                    bank = 4 + dc % 4
                    for fc in range(4):
                        wv = wb_sb[:, a, 4 + fc // 2, :].rearrange("q (a b) -> q a b", a=2)
                        mm(s, ps[:, bank, 0:n], wv[:, fc % 2, dc * 128:(dc + 1) * 128], g_sb[:, gs, fc, 0:n],
                           fc == 0, fc == 3, R=[bwb[a][4 + fc // 2], bg[gs]], W=[bps[bank]])
                    if dc % 2 == 0:
                        s.op("dve", lambda e: e.tensor_copy(out=y_sb[:, xs_, dc, 0:n], in_=ps[:, bank, 0:n]),
                             R=[bps[bank]], W=[by[xs_]])
                    else:
                        s.op("act", lambda e: e.copy(out=y_sb[:, xs_, dc, 0:n], in_=ps[:, bank, 0:n]),
                             R=[bps[bank]], W=[by[xs_]])
                s.dma("sp", yo[:, :, t0:t0 + n], y_sb[:, xs_, :, 0:n], R=[by[xs_]])
        s.finish(by)
    return nc


def build_pc2(final):
    nc = bass.Bass("TRN2", target_bir_lowering=False)
    din = lambda n, shp, dt: nc.dram_tensor(n, shp, dt, kind="ExternalInput").ap()
    xT = din("xT", [128, 8, NT1], F32)
    yA = din("yA", [128, 8, NT1], F32)
    yB = din("yB", [128, 8, NT1], F32)
    wab = din("wab", [128, 2, NT1], F32)
    mod = din("mod", [128, 3, 8], F32)
    xo = nc.dram_tensor("xo", [128, 8, NT1], F32, kind="ExternalOutput").ap()
    s = Sched(nc)
    with (nc.psum_tensor("ps", [128, 2, 512], F32) as ps,
          nc.sbuf_tensor("x_sb", [128, 8, NT1], F32) as x_sb,
          nc.sbuf_tensor("ya_sb", [128, 2, 8, 512], F32) as ya_sb,
          nc.sbuf_tensor("yb_sb", [128, 2, 8, 512], F32) as yb_sb,
          nc.sbuf_tensor("wab_sb", [128, 2, NT1], F32) as wab_sb,
          nc.sbuf_tensor("mod_sb", [128, 3, 8], F32) as mod_sb,
          nc.sbuf_tensor("ones_sb", [128, 128], BF16) as ones_sb,
          nc.sbuf_tensor("sq_sb", [128, 8, 512], BF16) as sq_sb,
          nc.sbuf_tensor("rstd_sb", [128, 2, 512], F32) as rstd_sb):
        bxb = [s.buf(f"x{i}", s.dsem(f"x{i}")) for i in range(len(BLKS1))]
        bya = [s.buf(f"ya{i}", s.dsem(f"ya{i}")) for i in range(2)]
        byb = [s.buf(f"yb{i}", s.dsem(f"yb{i}")) for i in range(2)]
        bmod = s.buf("mod", s.dsem("mod"))
        bwab = s.buf("wab", bmod.dsem)
        bones = s.buf("ones"); bsq = s.buf("sq"); brs = [s.buf(f"rs{i}") for i in range(2)]
        bps = [s.buf(f"ps{i}") for i in range(2)]
        s.dma("sp", mod_sb[:], mod, W=[bmod])
        s.dma("sp", wab_sb[:], wab, W=[bwab])
        s.op("pool", lambda e: e.memset(ones_sb[:], 1.0), W=[bones])
        for bi, (t0, n) in enumerate(BLKS1):
            s.dma("sp", x_sb[:, :, t0:t0 + n], xT[:, :, t0:t0 + n], W=[bxb[bi]])
        for bi, (t0, n) in enumerate(BLKS1):
            garow = 1 if bi == 4 else 0
            sl = bi % 2
            s.dma("act", ya_sb[:, sl, :, 0:n], yA[:, :, t0:t0 + n], W=[bya[sl]])
            s.dma("act", yb_sb[:, sl, :, 0:n], yB[:, :, t0:t0 + n], W=[byb[sl]])
            for k in range(8):
                s.op("pool", lambda e: e.tensor_tensor(out=ya_sb[:, sl, k, 0:n], in0=ya_sb[:, sl, k, 0:n],
                                                       in1=wab_sb[:, 0, t0:t0 + n], op=ALU.mult),
                     R=[bya[sl], bwab], W=[bya[sl]])
                s.op("dve", lambda e: e.tensor_tensor(out=yb_sb[:, sl, k, 0:n], in0=yb_sb[:, sl, k, 0:n],
                                                      in1=wab_sb[:, 1, t0:t0 + n], op=ALU.mult),
                     R=[byb[sl], bwab], W=[byb[sl]])
                s.op("dve", lambda e: e.tensor_tensor(out=ya_sb[:, sl, k, 0:n], in0=ya_sb[:, sl, k, 0:n],
                                                      in1=yb_sb[:, sl, k, 0:n], op=ALU.add),
                     R=[bya[sl], byb[sl]], W=[bya[sl]])
                s.op("dve", lambda e: e.scalar_tensor_tensor(
                    out=x_sb[:, k, t0:t0 + n], in0=ya_sb[:, sl, k, 0:n], scalar=mod_sb[:, garow, k:k + 1],
                    in1=x_sb[:, k, t0:t0 + n], op0=ALU.mult, op1=ALU.add),
                    R=[bya[sl], bmod, bxb[bi]], W=[bxb[bi]])
            if final:
                s.op("act", lambda e: e.activation(out=sq_sb[:, :, 0:n], in_=x_sb[:, :, t0:t0 + n], func=AF.Square),
                     R=[bxb[bi]], W=[bsq])
                for k in range(8):
                    mm(s, ps[:, sl, 0:n], ones_sb[:], sq_sb[:, k, 0:n], k == 0, k == 7, R=[bones, bsq], W=[bps[sl]])
                s.op("act", lambda e: e.activation(out=rstd_sb[:, sl, 0:n], in_=ps[:, sl, 0:n], func=AF.Sqrt,
                                                   scale=1.0 / D, bias=EPS), R=[bps[sl]], W=[brs[sl]])
                s.op("dve", lambda e: e.reciprocal(out=rstd_sb[:, sl, 0:n], in_=rstd_sb[:, sl, 0:n]),
                     R=[brs[sl]], W=[brs[sl]])
                for k in range(8):
                    s.op("dve", lambda e: e.scalar_tensor_tensor(
                        out=x_sb[:, k, t0:t0 + n], in0=x_sb[:, k, t0:t0 + n], scalar=mod_sb[:, 2, k:k + 1],
                        in1=rstd_sb[:, sl, 0:n], op0=ALU.mult, op1=ALU.mult),
                        R=[bxb[bi], bmod, brs[sl]], W=[bxb[bi]])
            s.dma("sp", xo[:, :, t0:t0 + n], x_sb[:, :, t0:t0 + n], R=[bxb[bi]])
        s.finish(bxb)
    return nc


def run_p3_expert(l, xl, xc, yna, ydf, hf, hb, fmf, mods_l, inp, g_final, final):
    in_maps = p3_inmaps_common(l, xl, xc, yna, ydf, hf, hb, fmf, mods_l, inp, g_final)
    resa = run_bass_kernel_spmd(build_p3a(), in_maps, core_ids=list(range(NCORES))).results
    NTT = NCORES * NT1
    HL2 = np.zeros((D, NTT), ml_dtypes.bfloat16)
    WD = np.zeros((NTT, 32), np.float32)
    for i in range(NCORES):
        HL2[:, i * NT1:(i + 1) * NT1] = resa[i]["hl2o"].transpose(1, 0, 2).reshape(D, NT1)
        WD[i * NT1:(i + 1) * NT1] = resa[i]["wd32o"].transpose(1, 0, 2).reshape(17 * 128, 32)[:NT1]
    top2 = np.sort(np.argpartition(-WD, 1, axis=1)[:, :2], axis=1)
    eA, eB = top2[:, 0], top2[:, 1]
    ar = np.arange(NTT)
    wA = WD[ar, eA]; wB = WD[ar, eB]
    tokE = []
    for e in range(32):
        tokE.append(np.nonzero((eA == e) | (eB == e))[0])
    caps = []
    for sl in range(4):
        mx = max(len(tokE[4 * c + sl]) for c in range(NCORES))
        caps.append(max(128, int(-(-mx // 128) * 128)))
    captot = sum(caps)
    mapse = []
    for c in range(NCORES):
        xsa = np.zeros((D, captot), ml_dtypes.bfloat16)
        o = 0
        for sl in range(4):
            tk_ = tokE[4 * c + sl]
            xsa[:, o:o + len(tk_)] = HL2[:, tk_]
            o += caps[sl]
        mapse.append({"xs": np.ascontiguousarray(xsa.reshape(8, 128, captot).transpose(1, 0, 2)),
                      "w1": np.ascontiguousarray(inp["moe_w1"][l][4 * c:4 * c + 4]),
                      "w3": np.ascontiguousarray(inp["moe_w3"][l][4 * c:4 * c + 4]),
                      "w2": np.ascontiguousarray(inp["moe_w2"][l][4 * c:4 * c + 4])})
    rese = run_bass_kernel_spmd(build_p3e(caps), mapse, core_ids=list(range(NCORES))).results
    YA = np.zeros((D, NTT), np.float32)
    YB = np.zeros((D, NTT), np.float32)
    for c in range(NCORES):
        yy = rese[c]["yo"].transpose(1, 0, 2).reshape(D, captot)
        o = 0
        for sl in range(4):
            e = 4 * c + sl
            tk_ = tokE[e]
            cols = yy[:, o:o + len(tk_)]
            isA = eA[tk_] == e
            YA[:, tk_[isA]] = cols[:, isA]
            YB[:, tk_[~isA]] = cols[:, ~isA]
            o += caps[sl]
    mapsc = []
    for i in range(NCORES):
        b = i // 4
        sl_ = slice(i * NT1, (i + 1) * NT1)
        modc = np.stack([vec_pk(mods_l[b, 5120:6144]), vec_pk(mods_l[2, 5120:6144]), vec_pk(g_final)], axis=1)
        wab = np.stack([np.tile(wA[sl_][None, :], (128, 1)), np.tile(wB[sl_][None, :], (128, 1))], axis=1)
        mapsc.append({"xT": resa[i]["xo"], "mod": np.ascontiguousarray(modc).astype(np.float32),
                      "wab": np.ascontiguousarray(wab).astype(np.float32),
                      "yA": np.ascontiguousarray(YA[:, sl_].reshape(8, 128, NT1).transpose(1, 0, 2)),
                      "yB": np.ascontiguousarray(YB[:, sl_].reshape(8, 128, NT1).transpose(1, 0, 2))})
    resc = run_bass_kernel_spmd(build_pc2(final), mapsc, core_ids=list(range(NCORES))).results
    xl2 = np.zeros_like(xl); xc2 = np.zeros_like(xc)
    for i in range(NCORES):
        b, j = i // 4, i % 4
        o = resc[i]["xo"].transpose(1, 0, 2).reshape(D, NT1).T
        xl2[b, 2048 * j:2048 * (j + 1)] = o[:2048]
        xc2[b, 64 * j:64 * (j + 1)] = o[2048:]
    return xl2, xc2


def p3_inmaps_common(l, xl, xc, yna, ydf, hf, hb, fmf, mods_l, inp, g_final):
    wge = np.concatenate([inp["router_w_group"][l], inp["router_w_expert"][l]], axis=1)
    wge = np.ascontiguousarray(wge.reshape(8, 128, 36).transpose(1, 0, 2))
    bge = np.concatenate([inp["router_b_group"][l], inp["router_b_expert"][l]])
    bge = np.ascontiguousarray(np.tile(bge[None, :], (128, 1))).astype(np.float32)
    ident = np.eye(128, dtype=np.float32)
    iota4 = np.ascontiguousarray(np.tile(np.arange(4, dtype=np.float32)[None, :], (128, 1)))
    in_maps = []
    for i in range(NCORES):
        b, j = i // 4, i % 4
        lat = slice(2048 * j, 2048 * (j + 1))
        ctxs = slice(S + 64 * j, S + 64 * (j + 1))
        xx = np.concatenate([xl[b, lat], xc[b, 64 * j:64 * (j + 1)]], axis=0)
        na = np.concatenate([yna[b, lat], yna[b, ctxs]], axis=0)
        df = np.concatenate([ydf[b, lat], ydf[b, ctxs]], axis=0)
        nadf = np.concatenate([na, df], axis=1)
        nadf = np.ascontiguousarray(nadf.T.reshape(4, 128, NT1).transpose(1, 0, 2))

        def tk(a):
            aa = np.concatenate([a[:, lat], a[:, ctxs]], axis=1)
            return aa.reshape(4, 128, NT1).transpose(1, 0, 2)
        hgt = np.ascontiguousarray(np.concatenate([tk(hf[b]), tk(hb[b]), tk(fmf[b, 512:1024])], axis=1))
        m = mods_l
        rows = [inp["g_ffn"][l], m[b, 2048:3072], m[b, 4096:5120], m[b, 3072:4096], m[b, 5120:6144],
                m[2, 2048:3072], m[2, 4096:5120], m[2, 3072:4096], m[2, 5120:6144], g_final]
        mod = np.ascontiguousarray(np.stack([vec_pk(r) for r in rows], axis=1)).astype(np.float32)
        in_maps.append({"xT": chunkT(xx), "nadf": nadf, "hg": hgt, "wout": inp["w_out"][l], "mod": mod,
                        "wge": wge, "bge": bge, "ident": ident, "iota4": iota4})
    return in_maps


def run_p3_sparse(l, xl, xc, yna, ydf, hf, hb, fmf, mods_l, inp, g_final, final):
    in_maps = p3_inmaps_common(l, xl, xc, yna, ydf, hf, hb, fmf, mods_l, inp, g_final)
    resa = run_bass_kernel_spmd(build_p3a(), in_maps, core_ids=list(range(NCORES))).results
    NTT = NCORES * NT1
    HL2 = np.zeros((D, NTT), ml_dtypes.bfloat16)
    WDT = np.zeros((32, 2, NTT), ml_dtypes.bfloat16)
    gid = np.zeros(NTT, np.int64)
    for i in range(NCORES):
        HL2[:, i * NT1:(i + 1) * NT1] = resa[i]["hl2o"].transpose(1, 0, 2).reshape(D, NT1)
        WDT[:, :, i * NT1:(i + 1) * NT1] = resa[i]["wdto"]
        g = resa[i]["gido"]
        gid[i * NT1:(i + 1) * NT1] = np.rint(g.T.reshape(-1)[:NT1]).astype(np.int64)
    toks = [np.nonzero(gid == g)[0] for g in range(4)]
    ncg = [1, 1, 1, 1]
    for _ in range(NCORES - 4):
        gbig = max(range(4), key=lambda g: len(toks[g]) / ncg[g])
        ncg[gbig] += 1
    idxs = []
    cgroup = []
    for g in range(4):
        parts = np.array_split(toks[g], ncg[g])
        for pp in parts:
            idxs.append(pp); cgroup.append(g)
    ntb = max(512, int(-(-max(len(ix) for ix in idxs) // 512) * 512))
    sel8 = np.zeros((8, 8, 128), np.float32)
    for e in range(8):
        sel8[e, e, :] = 1.0
    sel8 = sel8.reshape(8, 1024).astype(ml_dtypes.bfloat16)
    mapsb = []
    for c in range(NCORES):
        g = cgroup[c]
        ix = idxs[c]
        h2 = np.zeros((D, ntb), ml_dtypes.bfloat16)
        h2[:, :len(ix)] = HL2[:, ix]
        wd = np.zeros((8, 2, ntb), ml_dtypes.bfloat16)
        wd[:, :, :len(ix)] = WDT[8 * g:8 * g + 8][:, :, ix]
        mapsb.append({"hl2": np.ascontiguousarray(h2.reshape(8, 128, ntb).transpose(1, 0, 2)), "wdt": wd, "selc": sel8,
                      "w1": np.ascontiguousarray(inp["moe_w1"][l][8 * g:8 * g + 8]),
                      "w3": np.ascontiguousarray(inp["moe_w3"][l][8 * g:8 * g + 8]),
                      "w2": np.ascontiguousarray(inp["moe_w2"][l][8 * g:8 * g + 8])})
    resb = run_bass_kernel_spmd(build_p3b(ntb), mapsb, core_ids=list(range(NCORES))).results
    Y = np.zeros((D, NTT), np.float32)
    for c in range(NCORES):
        ix = idxs[c]
        Y[:, ix] = resb[c]["yo"].transpose(1, 0, 2).reshape(D, ntb)[:, :len(ix)]
    mapsc = []
    for i in range(NCORES):
        b = i // 4
        modc = np.stack([vec_pk(mods_l[b, 5120:6144]), vec_pk(mods_l[2, 5120:6144]), vec_pk(g_final)], axis=1)
        mapsc.append({"xT": resa[i]["xo"], "mod": np.ascontiguousarray(modc).astype(np.float32),
                      "yT": np.ascontiguousarray(Y[:, i * NT1:(i + 1) * NT1].reshape(8, 128, NT1).transpose(1, 0, 2))})
    resc = run_bass_kernel_spmd(build_pc(final), mapsc, core_ids=list(range(NCORES))).results
    xl2 = np.zeros_like(xl); xc2 = np.zeros_like(xc)
    for i in range(NCORES):
        b, j = i // 4, i % 4
        o = resc[i]["xo"].transpose(1, 0, 2).reshape(D, NT1).T
        xl2[b, 2048 * j:2048 * (j + 1)] = o[:2048]
        xc2[b, 64 * j:64 * (j + 1)] = o[2048:]
    return xl2, xc2


def kernel(**inputs):
    inp = {k: np.asarray(v) for k, v in inputs.items()}
    x = np.ascontiguousarray(inp["x"], dtype=np.float32)
    ctx = np.ascontiguousarray(inp["ctx"], dtype=np.float32)
    mods = run_p0(inp["c"], inp["c_ctx"], inp["w_ada"], inp["b_ada"])
    cosT, sinT = rope_tables()
    xl, xc = x, ctx
    for l in range(DEPTH):
        fmb, fmf, tm = run_p1(build_p1(), xl, xc, mods[l], inp["g_mix"][l], inp["w_in"][l], cosT, sinT)
        lam_init = 0.8 - 0.6 * math.exp(-0.3 * l)
        yna, ydf, hf, hb = run_p2(build_p2(lam_init), l, fmb, fmf, tm, inp)
        xl, xc = run_p3_expert(l, xl, xc, yna, ydf, hf, hb, fmf, mods[l], inp, inp["g_final"], l == DEPTH - 1)
    return np.ascontiguousarray(xl, dtype=np.float32)
```

```python
import math
from contextlib import ExitStack
import numpy as np
import ml_dtypes
import concourse.bass as bass
import concourse.mybir as mybir
from concourse.bass_utils import run_bass_kernel_spmd

F32 = mybir.dt.float32
BF16 = mybir.dt.bfloat16
I32 = mybir.dt.int32
U32 = mybir.dt.uint32
AF = mybir.ActivationFunctionType
ALU = mybir.AluOpType
AX = mybir.AxisListType

NCORES = 8
D = 1024
B = 2
S = 8192
L = 256
DEPTH = 4
GRID_W = 64
EPS = 1e-6


class Buf:
    __slots__ = ("name", "w", "r", "dsem")

    def __init__(self, name, dsem=None):
        self.name = name
        self.w = None
        self.r = []
        self.dsem = dsem


class DmaSem:
    def __init__(self, sched, name):
        self.sem = sched.nc.alloc_semaphore(name)
        self.key = ("dma", name)
        self.total = 0
        sched.sems[self.key] = self


class Sched:
    def __init__(self, nc):
        self.nc = nc
        self.eng = {"pe": nc.tensor, "dve": nc.vector, "act": nc.scalar,
                    "pool": nc.gpsimd, "sp": nc.sync}
        self.sems = {}
        self.esem = {}
        self.cnt = {}
        for k in self.eng:
            self.esem[k] = nc.alloc_semaphore("e_" + k)
            self.cnt[k] = 0
        self.seen = {}
        self.nbuf = 0
        self.out_tokens = []

    def buf(self, name=None, dsem=None):
        self.nbuf += 1
        return Buf(name or f"b{self.nbuf}", dsem)

    def dsem(self, name):
        return DmaSem(self, name)

    def _semof(self, key):
        if key[0] == "dma":
            return self.sems[key].sem
        return self.esem[key[0]]

    def _wait(self, engname, deps):
        e = self.eng[engname]
        for key, val in deps.items():
            if key[0] == "dma":
                val = max(val, 0)
            if self.seen.get((engname, key), 0) >= val:
                continue
            self.seen[(engname, key)] = val
            e.wait_ge(self._semof(key), val)

    def _deps(self, R, W):
        deps = {}

        def add(tok):
            if tok is None:
                return
            key, val = tok
            if key[0] == "dma":
                val = self.sems[key].total
            if deps.get(key, 0) < val:
                deps[key] = val
        for b in R:
            add(b.w)
        for b in W:
            add(b.w)
            for t in b.r:
                add(t)
        return deps

    def _commit(self, tok, R, W):
        for b in R:
            b.r.append(tok)
        for b in W:
            b.w = tok
            b.r = []

    def op(self, engname, fn, R=(), W=()):
        deps = self._deps(R, W)
        if engname == "pe":
            deps.pop(("pe",), None)
        self._wait(engname, deps)
        ins = fn(self.eng[engname])
        self.cnt[engname] += 1
        ins.then_inc(self.esem[engname], 1)
        tok = ((engname,), self.cnt[engname])
        self._commit(tok, R, W)
        return tok

    def dma(self, q, out, in_, R=(), W=(), sem=None, **kw):
        deps = self._deps(R, W)
        self._wait(q, deps)
        ds = sem
        if ds is None:
            for b in list(W) + list(R):
                if b.dsem is not None:
                    ds = b.dsem
                    break
        assert ds is not None, "dma needs a DmaSem"
        ins = self.eng[q].dma_start(out=out, in_=in_, **kw)
        ds.total += 16
        ins.then_inc(ds.sem, 16)
        tok = (ds.key, ds.total)
        self._commit(tok, R, W)
        return tok

    def barrier(self, bufs=()):
        deps = {}
        for k in self.eng:
            if self.cnt[k]:
                deps[(k,)] = self.cnt[k]
        for key, ds in self.sems.items():
            if ds.total:
                deps[key] = ds.total
        for k in self.eng:
            d = {kk: v for kk, v in deps.items() if kk != (k,)}
            self._wait(k, d)

    def coll(self, kind, ins, outs, R=(), W=(), groups=None):
        deps = self._deps(R, W)
        self._wait("pool", deps)
        ds = None
        for b in list(W) + list(R):
            if b.dsem is not None:
                ds = b.dsem
                break
        g = groups or [[0, 1, 2, 3], [4, 5, 6, 7]]
        ins_ = self.nc.gpsimd.collective_compute(kind, ALU.bypass, replica_groups=g, ins=ins, outs=outs)
        ds.total += 16
        ins_.then_inc(ds.sem, 16)
        tok = (ds.key, ds.total)
        self._commit(tok, R, W)
        return tok

    def finish(self, bufs, engname="sp"):
        deps = {}
        for b in bufs:
            for tok in ([b.w] if b.w else []) + b.r:
                key, val = tok
                if key[0] == "dma":
                    val = self.sems[key].total
                deps[key] = max(deps.get(key, 0), val)
        self._wait(engname, deps)


def mm(s, out, lhsT, rhs, start, stop, R, W):
    return s.op("pe", lambda e: e.matmul(out, lhsT, rhs, start=start, stop=stop), R=R, W=W)


def build_p0():
    nc = bass.Bass("TRN2", target_bir_lowering=False)
    NCOL = 3072
    NJ = NCOL // 128
    cT = nc.dram_tensor("cT", [128, 8, 4], F32, kind="ExternalInput").ap()
    w = nc.dram_tensor("w", [D, NCOL], F32, kind="ExternalInput").ap()
    bvec = nc.dram_tensor("bvec", [128, NJ], F32, kind="ExternalInput").ap()
    out = nc.dram_tensor("out", [128, NJ, 4], F32, kind="ExternalOutput").ap()
    s = Sched(nc)
    wv = w.rearrange("(kc p) n -> p kc n", p=128)
    with (nc.sbuf_tensor("w_sb", [128, 8, NCOL], F32) as w_sb,
          nc.sbuf_tensor("c_sb", [128, 8, 4], F32) as c_sb,
          nc.sbuf_tensor("s_sb", [128, 8, 4], F32) as s_sb,
          nc.sbuf_tensor("b_sb", [128, NJ], F32) as b_sb,
          nc.sbuf_tensor("r_sb", [128, NJ, 4], F32) as r_sb,
          nc.psum_tensor("ps", [128, 8, 512], F32) as ps):
        bw = [s.buf(f"w{k}", s.dsem(f"w{k}")) for k in range(8)]
        bc = s.buf("c", s.dsem("c"))
        bb = s.buf("b", bc.dsem)
        bs = s.buf("s")
        br = s.buf("r", s.dsem("r"))
        bps = [s.buf(f"ps{i}") for i in range(8)]
        s.dma("sp", c_sb[:], cT, W=[bc])
        s.dma("sp", b_sb[:], bvec, W=[bb])
        for k in range(8):
            s.dma("sp" if k % 2 == 0 else "act", w_sb[:, k, :], wv[:, k, :], W=[bw[k]])
        s.op("act", lambda e: e.activation(out=s_sb[:], in_=c_sb[:], func=AF.Silu), R=[bc], W=[bs])
        for j in range(NJ):
            pb = bps[j % 8]
            for k in range(8):
                mm(s, ps[:, j % 8, 0:4], w_sb[:, k, j * 128:(j + 1) * 128], s_sb[:, k, :],
                   k == 0, k == 7, R=[bw[k], bs], W=[pb])
            s.op("dve", lambda e: e.tensor_scalar(out=r_sb[:, j, :], in0=ps[:, j % 8, 0:4],
                                                  scalar1=b_sb[:, j:j + 1], scalar2=None, op0=ALU.add),
                 R=[pb, bb], W=[br])
        s.dma("sp", out, r_sb[:], R=[br])
        s.finish([br])
    return nc


def silu_np_layout_c(c, c_ctx):
    cc = np.stack([c[0], c[1], c_ctx, c_ctx], axis=1)
    return np.ascontiguousarray(cc.reshape(8, 128, 4).transpose(1, 0, 2))


def run_p0(c, c_ctx, w_ada, b_ada):
    nc = build_p0()
    cT = silu_np_layout_c(c, c_ctx)
    in_maps = []
    for i in range(NCORES):
        l, h = i // 2, i % 2
        in_maps.append({
            "cT": cT,
            "w": np.ascontiguousarray(w_ada[l][:, h * 3072:(h + 1) * 3072]),
            "bvec": np.ascontiguousarray(b_ada[l][h * 3072:(h + 1) * 3072].reshape(24, 128).T),
        })
    res = run_bass_kernel_spmd(nc, in_maps, core_ids=list(range(NCORES)))
    mods = np.zeros((DEPTH, 3, 6 * D), np.float32)
    for i in range(NCORES):
        l, h = i // 2, i % 2
        o = res.results[i]["out"]
        m = o.transpose(1, 0, 2).reshape(3072, 4)
        mods[l, :, h * 3072:(h + 1) * 3072] = m[:, :3].T
    return mods


NT1 = 2112
NW1 = 3072
BLKS1 = [(0, 512), (512, 512), (1024, 512), (1536, 512), (2048, 64)]


def build_p1():
    nc = bass.Bass("TRN2", target_bir_lowering=False)
    xT = nc.dram_tensor("xT", [128, 8, NT1], F32, kind="ExternalInput").ap()
    w = nc.dram_tensor("w", [D, NW1], F32, kind="ExternalInput").ap()
    gsc = nc.dram_tensor("gsc", [128, 5, 8], F32, kind="ExternalInput").ap()
    cosT = nc.dram_tensor("cosT", [128, 2048], F32, kind="ExternalInput").ap()
    sinT = nc.dram_tensor("sinT", [128, 2048], F32, kind="ExternalInput").ap()
    fmb = nc.dram_tensor("fmb", [128, 8, NT1], BF16, kind="ExternalOutput").ap()
    fmf = nc.dram_tensor("fmf", [128, 8, NT1], F32, kind="ExternalOutput").ap()
    tm = nc.dram_tensor("tm", [NT1, 512], BF16, kind="ExternalOutput").ap()
    s = Sched(nc)
    wv = w.rearrange("(kc p) n -> p kc n", p=128)
    with (nc.sbuf_tensor("x_sb", [128, 2, 8, 512], F32) as x_sb,
          nc.sbuf_tensor("h_sb", [128, 8, NT1], BF16) as h_sb,
          nc.sbuf_tensor("w_bf", [128, 8, NW1], BF16) as w_bf,
          nc.sbuf_tensor("w_st", [128, 2, NW1], F32) as w_st,
          nc.sbuf_tensor("o_sb", [128, 4, 512], F32) as o_sb,
          nc.sbuf_tensor("ob_sb", [128, 4, 512], BF16) as ob_sb,
          nc.sbuf_tensor("cos_sb", [128, 2048], F32) as cos_sb,
          nc.sbuf_tensor("sin_sb", [128, 2048], F32) as sin_sb,
          nc.sbuf_tensor("gsc_sb", [128, 5, 8], F32) as gsc_sb,
          nc.sbuf_tensor("ab_sb", [128, 2, 8], F32) as ab_sb,
          nc.sbuf_tensor("sq_sb", [128, 8, 512], BF16) as sq_sb,
          nc.sbuf_tensor("ones_sb", [128, 128], BF16) as ones_sb,
          nc.sbuf_tensor("rstd_sb", [128, 2, 512], F32) as rstd_sb,
          nc.sbuf_tensor("tmp_sb", [128, 4, 512], F32) as tmp_sb,
          nc.psum_tensor("ps", [128, 8, 512], F32) as ps):
        bx = [s.buf(f"x{i}", s.dsem(f"x{i}")) for i in range(2)]
        bh = [s.buf(f"h{i}") for i in range(len(BLKS1))]
        bwst = [s.buf(f"wst{i}", s.dsem(f"wst{i}")) for i in range(2)]
        bwbf = [s.buf(f"wbf{k}") for k in range(8)]
        bo = [s.buf(f"o{i}", s.dsem(f"o{i}")) for i in range(4)]
        bob = [s.buf(f"ob{i}", s.dsem(f"ob{i}")) for i in range(4)]
        cs = s.dsem("const")
        bcos = s.buf("cos", cs); bsin = s.buf("sin", cs); bgsc = s.buf("gsc", cs)
        bab = s.buf("ab"); bsq = s.buf("sq"); bones = s.buf("ones")
        brs = [s.buf(f"rs{i}") for i in range(2)]
        btmp = [s.buf(f"tmp{i}") for i in range(4)]
        bps = [s.buf(f"ps{i}") for i in range(8)]

        s.dma("sp", gsc_sb[:], gsc, W=[bgsc])
        s.dma("sp", cos_sb[:], cosT, W=[bcos])
        s.dma("sp", sin_sb[:], sinT, W=[bsin])
        s.op("pool", lambda e: e.memset(ones_sb[:], 1.0), W=[bones])
        for t in range(2):
            s.op("dve", lambda e: e.scalar_tensor_tensor(
                out=ab_sb[:, t, :], in0=gsc_sb[:, 1 + 2 * t, :], scalar=1.0, in1=gsc_sb[:, 0, :],
                op0=ALU.add, op1=ALU.mult), R=[bgsc], W=[bab])
        for k in range(8):
            sl = k % 2
            s.dma("act", w_st[:, sl, :], wv[:, k, :], W=[bwst[sl]])
            s.op("pool", lambda e: e.tensor_copy(out=w_bf[:, k, :], in_=w_st[:, sl, :]),
                 R=[bwst[sl]], W=[bwbf[k]])
        for bi, (t0, n) in enumerate(BLKS1):
            sl = bi % 2
            isctx = bi == 4
            s.dma("sp", x_sb[:, sl, :, 0:n], xT[:, :, t0:t0 + n], W=[bx[sl]])
            s.op("act", lambda e: e.activation(out=sq_sb[:, :, 0:n], in_=x_sb[:, sl, :, 0:n], func=AF.Square),
                 R=[bx[sl]], W=[bsq])
            pst = bps[sl]
            for k in range(8):
                mm(s, ps[:, sl, 0:n], ones_sb[:], sq_sb[:, k, 0:n], k == 0, k == 7, R=[bones, bsq], W=[pst])
            s.op("act", lambda e: e.activation(out=rstd_sb[:, sl, 0:n], in_=ps[:, sl, 0:n], func=AF.Sqrt,
                                               scale=1.0 / D, bias=EPS), R=[pst], W=[brs[sl]])
            s.op("dve", lambda e: e.reciprocal(out=rstd_sb[:, sl, 0:n], in_=rstd_sb[:, sl, 0:n]),
                 R=[brs[sl]], W=[brs[sl]])
            ai = 1 if isctx else 0
            shrow = 4 if isctx else 2
            for k in range(8):
                tb = k % 4
                s.op("dve", lambda e: e.scalar_tensor_tensor(
                    out=tmp_sb[:, tb, 0:n], in0=x_sb[:, sl, k, 0:n], scalar=ab_sb[:, ai, k:k + 1],
                    in1=rstd_sb[:, sl, 0:n], op0=ALU.mult, op1=ALU.mult),
                    R=[bx[sl], bab, brs[sl]], W=[btmp[tb]])
                s.op("act", lambda e: e.activation(out=h_sb[:, k, t0:t0 + n], in_=tmp_sb[:, tb, 0:n],
                                                   func=AF.Identity, bias=gsc_sb[:, shrow, k:k + 1], scale=1.0),
                     R=[btmp[tb], bgsc], W=[bh[bi]])
        oi = 0
        obi = 0
        pi = 0
        for bi, (t0, n) in enumerate(BLKS1):
            isctx = bi == 4
            for c in range(16):
                rope = (c >= 12) and not isctx
                isb = c < 4 or c >= 12
                dst = (fmb[:, c if c < 4 else c - 8, t0:t0 + n]) if isb else fmf[:, c - 4, t0:t0 + n]
                pA = 2 + (pi % 6); pi += 1
                for k in range(8):
                    mm(s, ps[:, pA, 0:n], w_bf[:, k, c * 128:(c + 1) * 128], h_sb[:, k, t0:t0 + n],
                       k == 0, k == 7, R=[bwbf[k], bh[bi]], W=[bps[pA]])
                if isb:
                    ob = obi % 4; obi += 1
                    osl = ob_sb[:, ob, 0:n]; obuf = bob[ob]
                else:
                    ob = oi % 4; oi += 1
                    osl = o_sb[:, ob, 0:n]; obuf = bo[ob]
                if not rope:
                    if c % 2 == 0:
                        s.op("act", lambda e: e.copy(out=osl, in_=ps[:, pA, 0:n]), R=[bps[pA]], W=[obuf])
                    else:
                        s.op("dve", lambda e: e.tensor_copy(out=osl, in_=ps[:, pA, 0:n]), R=[bps[pA]], W=[obuf])
                else:
                    pB = 2 + (pi % 6); pi += 1
                    c2 = c + 4
                    for k in range(8):
                        mm(s, ps[:, pB, 0:n], w_bf[:, k, c2 * 128:(c2 + 1) * 128], h_sb[:, k, t0:t0 + n],
                           k == 0, k == 7, R=[bwbf[k], bh[bi]], W=[bps[pB]])
                    s.op("dve", lambda e: e.tensor_tensor(out=tmp_sb[:, 0, 0:n], in0=ps[:, pA, 0:n],
                                                          in1=cos_sb[:, t0:t0 + n], op=ALU.mult),
                         R=[bps[pA], bcos], W=[btmp[0]])
                    s.op("dve", lambda e: e.tensor_tensor(out=tmp_sb[:, 1, 0:n], in0=ps[:, pB, 0:n],
                                                          in1=sin_sb[:, t0:t0 + n], op=ALU.mult),
                         R=[bps[pB], bsin], W=[btmp[1]])
                    s.op("pool", lambda e: e.tensor_tensor(out=osl, in0=tmp_sb[:, 0, 0:n],
                                                           in1=tmp_sb[:, 1, 0:n], op=ALU.add),
                         R=[btmp[0], btmp[1]], W=[obuf])
                s.dma("sp", dst, osl, R=[obuf])
        ntile = NT1 // 128 + 1
        for ti in range(ntile):
            t0 = ti * 128
            n = min(128, NT1 - t0)
            bi = min(t0 // 512, 4)
            pA = 2 + (pi % 6); pi += 1
            for k in range(8):
                mm(s, ps[0:n, pA, :], h_sb[:, k, t0:t0 + n], w_bf[:, k, 2560:3072],
                   k == 0, k == 7, R=[bwbf[k], bh[bi]], W=[bps[pA]])
            ob = obi % 4; obi += 1
            s.op("act", lambda e: e.copy(out=ob_sb[0:n, ob, :], in_=ps[0:n, pA, :]), R=[bps[pA]], W=[bob[ob]])
            s.dma("sp", tm[t0:t0 + n, :], ob_sb[0:n, ob, :], R=[bob[ob]])
        s.finish(bo + bob)
    return nc


def rope_tables():
    t = np.arange(S)
    row = (t // GRID_W).astype(np.float32)
    col = (t % GRID_W).astype(np.float32)
    inv = (10000.0 ** (-np.arange(0, 16, 2, dtype=np.float32) / 16.0)).astype(np.float32)
    ang_r = row[:, None] * inv
    ang_c = col[:, None] * inv
    cosT = np.zeros((32, S), np.float32)
    sinT = np.zeros((32, S), np.float32)
    for d in range(32):
        ang = ang_r if d < 16 else ang_c
        i = d % 8
        cosT[d] = np.cos(ang[:, i])
        sgn = -1.0 if (d % 16) < 8 else 1.0
        sinT[d] = sgn * np.sin(ang[:, i])
    return np.tile(cosT, (4, 1)), np.tile(sinT, (4, 1))


def p1_wcols():
    sw = np.array([(d + 8) if (d % 16) < 8 else (d - 8) for d in range(32)])
    f = np.arange(256)
    swf = (f // 32) * 32 + sw[f % 32]
    cols = np.concatenate([np.arange(0, 256), np.arange(256, 512), np.arange(768, 1280), np.arange(1280, 1792),
                           np.arange(1792, 2048), np.arange(2048, 2304), 1792 + swf, 2048 + swf,
                           np.arange(512, 768), np.arange(2304, 2560)])
    return cols


def chunkT(a):
    T = a.shape[0]
    return np.ascontiguousarray(a.T.reshape(8, 128, T).transpose(1, 0, 2))


def vec_pk(v):
    return np.ascontiguousarray(v.reshape(8, 128).T)


def run_p1(nc1, xl, xc, mods_l, g_mix_l, w_in_l, cosT, sinT):
    wl = np.ascontiguousarray(w_in_l[:, p1_wcols()])
    in_maps = []
    for i in range(NCORES):
        b, j = i // 4, i % 4
        xx = np.concatenate([xl[b, 2048 * j:2048 * (j + 1)], xc[b, 64 * j:64 * (j + 1)]], axis=0)
        gsc = np.stack([vec_pk(g_mix_l), vec_pk(mods_l[b, 1024:2048]), vec_pk(mods_l[b, 0:1024]),
                        vec_pk(mods_l[2, 1024:2048]), vec_pk(mods_l[2, 0:1024])], axis=1)
        in_maps.append({"xT": chunkT(xx), "w": wl, "gsc": np.ascontiguousarray(gsc),
                        "cosT": np.ascontiguousarray(cosT[:, 2048 * j:2048 * (j + 1)]),
                        "sinT": np.ascontiguousarray(sinT[:, 2048 * j:2048 * (j + 1)])})
    res = run_bass_kernel_spmd(nc1, in_maps, core_ids=list(range(NCORES)))
    fmb = np.zeros((B, 1024, S + L), ml_dtypes.bfloat16)
    fmf = np.zeros((B, 1024, S + L), np.float32)
    tm = np.zeros((B, S + L, 512), ml_dtypes.bfloat16)
    for i in range(NCORES):
        b, j = i // 4, i % 4
        r = res.results[i]
        for dst, key in ((fmb, "fmb"), (fmf, "fmf")):
            f = r[key].transpose(1, 0, 2).reshape(1024, NT1)
            dst[b, :, 2048 * j:2048 * (j + 1)] = f[:, :2048]
            dst[b, :, S + 64 * j:S + 64 * (j + 1)] = f[:, 2048:]
        t = r["tm"]
        tm[b, 2048 * j:2048 * (j + 1)] = t[:2048]
        tm[b, S + 64 * j:S + 64 * (j + 1)] = t[2048:]
    return fmb, fmf, tm


NTOK = S + L
NKT = NTOK // 128
NEB = 21


def na_tile_lists():
    out = []
    for m in range(64):
        if 2 <= m <= 61:
            out.append(([m - 2, m - 1, m, m + 1, m + 2], 0))
        elif m < 2:
            out.append(([0, 1, 2, 3], 5 + 4 * m))
        else:
            out.append(([60, 61, 62, 63], 5 + 4 * (m - 60)))
    return out


def na_bias_index():
    MASKED = 15 * 31
    idx = np.full((NEB, 128, 128), MASKED, np.int64)
    lists = na_tile_lists()
    reps = {0: 10}
    qq = np.arange(128); kk = np.arange(128)

    def fill(e0, m, kts):
        for ii, n in enumerate(kts):
            qr = 2 * m + qq // 64; qc = qq % 64
            kr = 2 * n + kk // 64; kc = kk % 64
            r0 = np.clip(qr - 4, 0, 120)
            cs = np.clip(qc - 8, 0, 48)
            valid = ((kr[:, None] >= r0[None, :]) & (kr[:, None] < r0[None, :] + 8) &
                     (kc[:, None] >= cs[None, :]) & (kc[:, None] < cs[None, :] + 16))
            dr = kr[:, None] - qr[None, :]
            dc = np.clip(kc[:, None] - qc[None, :], -15, 15)
            v = (np.clip(dr, -7, 7) + 7) * 31 + dc + 15
            idx[e0 + ii] = np.where(valid, v, MASKED)
    fill(0, 10, lists[10][0])
    for m in (0, 1, 62, 63):
        fill(lists[m][1], m, lists[m][0])
    return idx


def build_p2(lam_init):
    nc = bass.Bass("TRN2", target_bir_lowering=False)
    din = lambda n, shp, dt: nc.dram_tensor(n, shp, dt, kind="ExternalInput").ap()
    dout = lambda n, shp, dt: nc.dram_tensor(n, shp, dt, kind="ExternalOutput").ap()
    xrg = din("xrg", [2, 128, NTOK], F32)
    wbd = din("wbd", [4, 128, 128], F32)
    rgv = din("rgv", [128, 2, 8], F32)
    hout = dout("hout", [2, 128, NTOK], F32)
    qaT = din("qaT", [64, NTOK], BF16)
    kaT = din("kaT", [64, NTOK], BF16)
    vaP = din("vaP", [128, NKT, 64], BF16)
    btT = din("btT", [128, NEB, 128], F32)
    ynaP = dout("ynaP", [128, NKT, 64], BF16)
    qdT = din("qdT", [2, 32, NTOK], BF16)
    kdT = din("kdT", [2, 32, NTOK], BF16)
    vdP = din("vdP", [128, NKT, 64], BF16)
    dlam = din("dlam", [128, 128], F32)
    dg = din("dg", [128, 64], F32)
    ydfP = dout("ydfP", [128, NKT, 64], BF16)
    s = Sched(nc)
    with nc.psum_tensor("ps", [128, 8, 512], F32) as ps:
        bps = [s.buf(f"ps{i}") for i in range(8)]

        CH = 2048
        with (nc.sbuf_tensor("x_sb", [128, NTOK], F32) as x_sb,
              nc.sbuf_tensor("wst_sb", [128, 4, 128], F32) as wst_sb,
              nc.sbuf_tensor("wbf_sb", [128, 4, 128], BF16) as wbf_sb,
              nc.sbuf_tensor("rgv_sb", [128, 2, 8], F32) as rgv_sb,
              nc.sbuf_tensor("cneg_sb", [128, 2], F32) as cneg_sb,
              nc.sbuf_tensor("xcv_sb", [128, CH], F32) as xcv_sb,
              nc.sbuf_tensor("xcb_sb", [128, CH], BF16) as xcb_sb,
              nc.sbuf_tensor("r_sb", [128, CH], F32) as r_sb,
              nc.sbuf_tensor("i_sb", [128, CH], F32) as i_sb,
              nc.sbuf_tensor("a_sb", [128, CH], F32) as a_sb,
              nc.sbuf_tensor("q_sb", [128, CH], F32) as q_sb,
              nc.sbuf_tensor("h_sb", [128, 2, CH], F32) as h_sb,
              nc.sbuf_tensor("carry_sb", [128, 1], F32) as carry_sb):
            bx = s.buf("x", s.dsem("rgx"))
            bw = s.buf("w", s.dsem("rgw"))
            bwb = s.buf("wb")
            bv = s.buf("v", bw.dsem)
            bcn = s.buf("cneg")
            bxcv = s.buf("xcv"); bxcb = s.buf("xcb"); br = s.buf("r"); bi_ = s.buf("i")
            ba = s.buf("a"); bq = s.buf("q"); bcar = s.buf("carry")
            bh = [s.buf(f"h{i}", s.dsem(f"rgh{i}")) for i in range(2)]
            s.dma("act", wst_sb[:], wbd.rearrange("f c d -> c f d"), W=[bw])
            s.dma("act", rgv_sb[:], rgv, W=[bv])
            s.op("pool", lambda e: e.tensor_copy(out=wbf_sb[:], in_=wst_sb[:]), R=[bw], W=[bwb])
            s.op("act", lambda e: e.activation(out=cneg_sb[:], in_=rgv_sb[:, :, 7], func=AF.Exp, scale=-1.0),
                 R=[bv], W=[bcn])
            s.op("act", lambda e: e.activation(out=cneg_sb[:], in_=cneg_sb[:], func=AF.Ln, bias=1.0, scale=1.0),
                 R=[bcn], W=[bcn])
            s.op("dve", lambda e: e.tensor_scalar(out=cneg_sb[:], in0=cneg_sb[:], scalar1=-8.0, scalar2=None,
                                                  op0=ALU.mult), R=[bcn], W=[bcn])
            hi = 0
            for dr in range(2):
                offs = [-2, -1, 0, 1] if dr == 0 else [2, 1, 0, -1]
                s.dma("sp", x_sb[:, 0:4224], xrg[dr, :, 0:4224], W=[bx])
                s.dma("sp", x_sb[:, 4224:NTOK], xrg[dr, :, 4224:NTOK], W=[bx])
                chunks = [(0, 256, 0, 256)] + [(256 + CH * i, CH, 256, NTOK) for i in range(4)]
                for ci, (c0, n, s0, s1) in enumerate(chunks):
                    s.op("dve", lambda e: e.tensor_scalar(
                        out=xcv_sb[:, 0:n], in0=x_sb[:, c0:c0 + n], scalar1=rgv_sb[:, dr, 2:3],
                        scalar2=rgv_sb[:, dr, 4:5], op0=ALU.mult, op1=ALU.add), R=[bx, bv], W=[bxcv])
                    for jt in (0, 1, 3):
                        o = offs[jt]
                        lo = max(c0, s0 - o); hi_ = min(c0 + n, s1 - o)
                        s.op("dve", lambda e: e.scalar_tensor_tensor(
                            out=xcv_sb[:, lo - c0:hi_ - c0], in0=x_sb[:, lo + o:hi_ + o],
                            scalar=rgv_sb[:, dr, jt:jt + 1], in1=xcv_sb[:, lo - c0:hi_ - c0],
                            op0=ALU.mult, op1=ALU.add), R=[bx, bv, bxcv], W=[bxcv])
                    s.op("pool", lambda e: e.tensor_copy(out=xcb_sb[:, 0:n], in_=xcv_sb[:, 0:n]), R=[bxcv], W=[bxcb])
                    nsb = (n + 511) // 512
                    for sb in range(nsb):
                        w_ = min(512, n - sb * 512)
                        mm(s, ps[:, sb, 0:w_], wbf_sb[:, 2 * dr, :], xcb_sb[:, sb * 512:sb * 512 + w_], True, True,
                           R=[bwb, bxcb], W=[bps[sb]])
                        mm(s, ps[:, 4 + sb, 0:w_], wbf_sb[:, 2 * dr + 1, :], xcb_sb[:, sb * 512:sb * 512 + w_], True, True,
                           R=[bwb, bxcb], W=[bps[4 + sb]])
                    if n == CH:
                        rin = ps[:, 0:4, :]; iin = ps[:, 4:8, :]
                        rout = r_sb[:, 0:n].rearrange("p (a b) -> p a b", b=512)
                        iout = i_sb[:, 0:n].rearrange("p (a b) -> p a b", b=512)
                    else:
                        rin = ps[:, 0, 0:n]; iin = ps[:, 4, 0:n]
                        rout = r_sb[:, 0:n]; iout = i_sb[:, 0:n]
                    s.op("act", lambda e: e.activation(out=rout, in_=rin, func=AF.Sigmoid,
                                                       bias=rgv_sb[:, dr, 5:6], scale=1.0),
                         R=bps[0:4] + [bv], W=[br])
                    s.op("act", lambda e: e.activation(out=iout, in_=iin, func=AF.Sigmoid,
                                                       bias=rgv_sb[:, dr, 6:7], scale=1.0),
                         R=bps[4:8] + [bv], W=[bi_])
                    s.op("act", lambda e: e.activation(out=a_sb[:, 0:n], in_=r_sb[:, 0:n], func=AF.Exp,
                                                       scale=cneg_sb[:, dr:dr + 1]), R=[br, bcn], W=[ba])
                    s.op("pool", lambda e: e.tensor_tensor(out=q_sb[:, 0:n], in0=a_sb[:, 0:n], in1=a_sb[:, 0:n],
                                                           op=ALU.mult), R=[ba], W=[bq])
                    s.op("act", lambda e: e.activation(out=q_sb[:, 0:n], in_=q_sb[:, 0:n], func=AF.Sqrt,
                                                       scale=-1.0, bias=1.0), R=[bq], W=[bq])
                    s.op("pool", lambda e: e.tensor_tensor(out=i_sb[:, 0:n], in0=i_sb[:, 0:n], in1=xcv_sb[:, 0:n],
                                                           op=ALU.mult), R=[bi_, bxcv], W=[bi_])
                    s.op("pool", lambda e: e.tensor_tensor(out=q_sb[:, 0:n], in0=q_sb[:, 0:n], in1=i_sb[:, 0:n],
                                                           op=ALU.mult), R=[bq, bi_], W=[bq])
                    hs = hi % 2; hi += 1
                    init = 0.0 if ci == 0 else carry_sb[:, 0:1]
                    s.op("dve", lambda e: e.tensor_tensor_scan(out=h_sb[:, hs, 0:n], data0=a_sb[:, 0:n],
                                                               data1=q_sb[:, 0:n], initial=init,
                                                               op0=ALU.mult, op1=ALU.add),
                         R=[ba, bq] + ([bcar] if ci else []), W=[bh[hs]])
                    s.op("dve", lambda e: e.tensor_copy(out=carry_sb[:, 0:1], in_=h_sb[:, hs, n - 1:n]),
                         R=[bh[hs]], W=[bcar])
                    s.dma("sp", hout[dr, :, c0:c0 + n], h_sb[:, hs, 0:n], R=[bh[hs]])
            s.barrier(bh)

        with (nc.sbuf_tensor("na_q_sb", [64, NTOK], BF16) as q_sb,
              nc.sbuf_tensor("na_k_sb", [64, NTOK], BF16) as k_sb,
              nc.sbuf_tensor("na_v_sb", [128, NKT, 65], BF16) as v_sb,
              nc.sbuf_tensor("na_bt_sb", [128, NEB * 128], F32) as bt_sb,
              nc.sbuf_tensor("na_eb_sb", [128, NEB * 128], BF16) as eb_sb,
              nc.sbuf_tensor("na_e_sb", [128, 2, 640], F32) as e_sb,
              nc.sbuf_tensor("na_p_sb", [128, 2, 896], BF16) as p_sb,
              nc.sbuf_tensor("na_y_sb", [128, NKT, 64], BF16) as y_sb,
              nc.sbuf_tensor("na_rec_sb", [128, 2], F32) as rec_sb):
            ld = s.dsem("nald")
            bq = s.buf("q", ld); bk = s.buf("k", ld); bv = s.buf("v", ld); bbt = s.buf("bt", ld)
            beb = s.buf("eb")
            be = [s.buf(f"e{i}") for i in range(2)]
            bp = [s.buf(f"p{i}") for i in range(2)]
            brec = [s.buf(f"rec{i}") for i in range(2)]
            by = s.buf("y", s.dsem("nay"))
            s.dma("sp", q_sb[:], qaT, W=[bq])
            s.dma("act", k_sb[:], kaT, W=[bk])
            s.dma("sp", v_sb[:, :, 0:64], vaP, W=[bv])
            s.dma("act", bt_sb[:], btT.rearrange("p e q -> p (e q)"), W=[bbt])
            s.op("pool", lambda e: e.memset(v_sb[:, :, 64:65], 1.0), W=[bv])
            s.op("act", lambda e: e.activation(out=eb_sb[:], in_=bt_sb[:], func=AF.Exp), R=[bbt], W=[beb])
            lists = na_tile_lists()
            for m in range(NKT):
                sl = m % 2
                if m < 64:
                    kts, eb0 = lists[m]
                else:
                    kts, eb0 = [], 0
                nl = len(kts)
                bA = bps[2 * sl]; bB = bps[2 * sl + 1]; bAcc = bps[4 + sl]
                qs = q_sb[:, m * 128:(m + 1) * 128]
                for ii, n in enumerate(kts):
                    bank = 2 * sl + (0 if ii < 4 else 1)
                    col = (ii % 4) * 128
                    mm(s, ps[:, bank, col:col + 128], k_sb[:, n * 128:(n + 1) * 128], qs, True, True,
                       R=[bk, bq], W=[bps[bank]])
                for ci in range(2):
                    n = 64 + ci
                    mm(s, ps[:, 2 * sl + 1, 128 + ci * 128:256 + ci * 128], k_sb[:, n * 128:(n + 1) * 128], qs,
                       True, True, R=[bk, bq], W=[bB])
                if nl:
                    na_ = min(nl, 4) * 128
                    s.op("act", lambda e: e.activation(out=e_sb[:, sl, 0:na_], in_=ps[:, 2 * sl, 0:na_], func=AF.Exp,
                                                       scale=0.125), R=[bA], W=[be[sl]])
                    if nl == 5:
                        s.op("act", lambda e: e.activation(out=e_sb[:, sl, 512:640], in_=ps[:, 2 * sl + 1, 0:128],
                                                           func=AF.Exp, scale=0.125), R=[bB], W=[be[sl]])
                s.op("act", lambda e: e.activation(out=p_sb[:, sl, 640:896], in_=ps[:, 2 * sl + 1, 128:384],
                                                   func=AF.Exp, scale=0.125), R=[bB], W=[bp[sl]])
                if nl:
                    s.op("dve", lambda e: e.tensor_tensor(out=p_sb[:, sl, 0:nl * 128], in0=e_sb[:, sl, 0:nl * 128],
                                                          in1=eb_sb[:, eb0 * 128:(eb0 + nl) * 128], op=ALU.mult),
                         R=[be[sl], beb], W=[bp[sl]])
                tiles = [(ii * 128, n) for ii, n in enumerate(kts)] + [(640, 64), (768, 65)]
                for ti, (pc, n) in enumerate(tiles):
                    mm(s, ps[:, 4 + sl, 0:65], p_sb[:, sl, pc:pc + 128], v_sb[:, n, :], ti == 0, ti == len(tiles) - 1,
                       R=[bp[sl], bv], W=[bAcc])
                s.op("dve", lambda e: e.reciprocal(out=rec_sb[:, sl:sl + 1], in_=ps[:, 4 + sl, 64:65]),
                     R=[bAcc], W=[brec[sl]])
                s.op("dve", lambda e: e.tensor_scalar(out=y_sb[:, m, :], in0=ps[:, 4 + sl, 0:64],
                                                      scalar1=rec_sb[:, sl:sl + 1], scalar2=None, op0=ALU.mult),
                     R=[bAcc, brec[sl]], W=[by])
            s.dma("sp", ynaP, y_sb[:], R=[by])
            s.barrier([by])

        with (nc.sbuf_tensor("df_q4_sb", [96, NTOK], BF16) as q4_sb,
              nc.sbuf_tensor("df_k4_sb", [96, NTOK], BF16) as k4_sb,
              nc.sbuf_tensor("df_q4b_sb", [96, NTOK], BF16) as q4b_sb,
              nc.sbuf_tensor("df_k4b_sb", [96, NTOK], BF16) as k4b_sb,
              nc.sbuf_tensor("df_v_sb", [128, NKT, 65], BF16) as v_sb,
              nc.sbuf_tensor("df_p_sb", [128, 8, 512], BF16) as p_sb,
              nc.sbuf_tensor("df_y_sb", [128, NKT, 64], BF16) as y_sb,
              nc.sbuf_tensor("df_dl_sb", [128, 128], F32) as dl_sb,
              nc.sbuf_tensor("df_g_sb", [128, 64], F32) as g_sb,
              nc.sbuf_tensor("df_lam_sb", [128, 4], F32) as lam_sb,
              nc.sbuf_tensor("df_r_sb", [128, 2, 2, 4], F32) as r_sb,
              nc.sbuf_tensor("df_ss_sb", [128, 2, 4], F32) as ss_sb,
              nc.sbuf_tensor("df_o_sb", [128, 2, 4, 64], F32) as o_sb,
              nc.sbuf_tensor("df_t_sb", [128, 2, 4, 64], F32) as t_sb,
              nc.sbuf_tensor("df_junk_sb", [128, 64], F32) as junk_sb):
            ld = s.dsem("dfld")
            bq = s.buf("q", ld); bk = s.buf("k", ld); bv = s.buf("v", ld); bdl = s.buf("dl", ld); bg = s.buf("g", ld)
            blam = s.buf("lam")
            bp = [s.buf(f"p{i}") for i in range(8)]
            by = s.buf("y", s.dsem("dfy"))
            br = [s.buf(f"r{i}") for i in range(2)]
            bss = [s.buf(f"ss{i}") for i in range(2)]
            bo = [s.buf(f"o{i}") for i in range(2)]
            bt = [s.buf(f"t{i}") for i in range(2)]
            bj = s.buf("junk")
            for (qt, kt_, order) in ((q4_sb, k4_sb, (0, 1, 0)), (q4b_sb, k4b_sb, (1, 0, 1))):
                for ri, cc in enumerate(order):
                    s.dma("sp", qt[32 * ri:32 * ri + 32, :], qdT[cc], W=[bq])
                    s.dma("act", kt_[32 * ri:32 * ri + 32, :], kdT[cc], W=[bk])
            s.dma("sp", v_sb[:, :, 0:64], vdP, W=[bv])
            s.dma("act", dl_sb[:], dlam, W=[bdl])
            s.dma("act", g_sb[:], dg, W=[bg])
            s.op("pool", lambda e: e.memset(v_sb[:, :, 64:65], 1.0), W=[bv])
            s.op("dve", lambda e: e.scalar_tensor_tensor(out=junk_sb[:, 0:32], in0=dl_sb[:, 0:32], scalar=1.0,
                                                         in1=dl_sb[:, 32:64], op0=ALU.mult, op1=ALU.mult,
                                                         accum_out=lam_sb[:, 0:1]), R=[bdl], W=[bj, blam])
            s.op("dve", lambda e: e.scalar_tensor_tensor(out=junk_sb[:, 0:32], in0=dl_sb[:, 64:96], scalar=1.0,
                                                         in1=dl_sb[:, 96:128], op0=ALU.mult, op1=ALU.mult,
                                                         accum_out=lam_sb[:, 1:2]), R=[bdl, bj], W=[bj, blam])
            s.op("act", lambda e: e.activation(out=lam_sb[:, 0:2], in_=lam_sb[:, 0:2], func=AF.Exp), R=[blam], W=[blam])
            s.op("dve", lambda e: e.tensor_tensor(out=lam_sb[:, 2:3], in0=lam_sb[:, 1:2], in1=lam_sb[:, 0:1],
                                                  op=ALU.subtract), R=[blam], W=[blam])
            s.op("dve", lambda e: e.tensor_scalar(out=lam_sb[:, 2:3], in0=lam_sb[:, 2:3], scalar1=-lam_init,
                                                  scalar2=None, op0=ALU.add), R=[blam], W=[blam])
            s.op("dve", lambda e: e.tensor_scalar(out=g_sb[:], in0=g_sb[:], scalar1=1.0 - lam_init, scalar2=None,
                                                  op0=ALU.mult), R=[bg], W=[bg])
            qblocks = [(512 * i, 512, list(range(NKT))) for i in range(16)] + [(S, 256, [64, 65])]
            sc = 32 ** -0.5
            gs_ = 0
            for qi, (q0, nq, kts) in enumerate(qblocks):
                par = qi % 2
                nsub = nq // 128
                steps = [(kt, c) for kt in kts for c in range(2)]
                ns = len(steps)
                groups = [list(range(i, min(i + 3, ns))) for i in range(0, ns, 3)]
                started = [False, False]
                for gi_ in range(len(groups) + 1):
                    if gi_ < len(groups):
                        grp = groups[gi_]
                        layB = steps[grp[0]][1] == 1
                        qt = q4b_sb if layB else q4_sb
                        kt_t = k4b_sb if layB else k4_sb
                        for ri, si in enumerate(grp):
                            kt, c = steps[si]
                            g = gs_ + si
                            mm(s, ps[:, g % 4, 0:nq], kt_t[32 * ri:32 * ri + 32, kt * 128:(kt + 1) * 128],
                               qt[32 * ri:32 * ri + 32, q0:q0 + nq], True, True, R=[bk, bq], W=[bps[g % 4]])
                        for ri, si in enumerate(grp):
                            g = gs_ + si
                            s.op("act", lambda e: e.activation(out=p_sb[:, g % 8, 0:nq], in_=ps[:, g % 4, 0:nq],
                                                               func=AF.Exp, scale=sc), R=[bps[g % 4]], W=[bp[g % 8]])
                    if gi_ >= 1:
                        for si in groups[gi_ - 1]:
                            kt, c = steps[si]
                            g = gs_ + si
                            bank = 4 + 2 * par + c
                            for sub in range(nsub):
                                st = not started[c]
                                started[c] = True
                                s.op("pe", lambda e: e.matmul(ps[:, bank, sub * 65:(sub + 1) * 65],
                                                              p_sb[:, g % 8, sub * 128:(sub + 1) * 128], v_sb[:, kt, :],
                                                              start=st, stop=(kt == kts[-1]), skip_group_check=True),
                                     R=[bp[g % 8], bv], W=[bps[bank]])
                gs_ += ns
                a0 = ps[:, 4 + 2 * par, 0:260].rearrange("p (s e) -> p s e", e=65)
                a1 = ps[:, 5 + 2 * par, 0:260].rearrange("p (s e) -> p s e", e=65)
                b0 = bps[4 + 2 * par]; b1 = bps[5 + 2 * par]
                s.op("dve", lambda e: e.reciprocal(out=r_sb[:, par, 0, 0:nsub], in_=a0[:, 0:nsub, 64]), R=[b0], W=[br[par]])
                s.op("dve", lambda e: e.reciprocal(out=r_sb[:, par, 1, 0:nsub], in_=a1[:, 0:nsub, 64]), R=[b1], W=[br[par]])
                s.op("dve", lambda e: e.tensor_scalar(out=r_sb[:, par, 1, 0:nsub], in0=r_sb[:, par, 1, 0:nsub],
                                                      scalar1=lam_sb[:, 2:3], scalar2=None, op0=ALU.mult),
                     R=[br[par], blam], W=[br[par]])
                for sub in range(nsub):
                    s.op("dve", lambda e: e.tensor_scalar(out=t_sb[:, par, sub, :], in0=a1[:, sub, 0:64],
                                                          scalar1=r_sb[:, par, 1, sub:sub + 1], scalar2=None,
                                                          op0=ALU.mult), R=[b1, br[par]], W=[bt[par]])
                    s.op("dve", lambda e: e.scalar_tensor_tensor(out=o_sb[:, par, sub, :], in0=a0[:, sub, 0:64],
                                                                 scalar=r_sb[:, par, 0, sub:sub + 1],
                                                                 in1=t_sb[:, par, sub, :], op0=ALU.mult, op1=ALU.add),
                         R=[b0, br[par], bt[par]], W=[bo[par]])
                    s.op("dve", lambda e: e.scalar_tensor_tensor(out=junk_sb[:], in0=o_sb[:, par, sub, :], scalar=1.0,
                                                                 in1=o_sb[:, par, sub, :], op0=ALU.mult, op1=ALU.mult,
                                                                 accum_out=ss_sb[:, par, sub:sub + 1]),
                         R=[bo[par], bj], W=[bj, bss[par]])
                s.op("act", lambda e: e.activation(out=ss_sb[:, par, 0:nsub], in_=ss_sb[:, par, 0:nsub], func=AF.Ln,
                                                   scale=1.0 / 64, bias=EPS), R=[bss[par]], W=[bss[par]])
                s.op("act", lambda e: e.activation(out=ss_sb[:, par, 0:nsub], in_=ss_sb[:, par, 0:nsub], func=AF.Exp,
                                                   scale=-0.5), R=[bss[par]], W=[bss[par]])
                for sub in range(nsub):
                    s.op("dve", lambda e: e.scalar_tensor_tensor(out=y_sb[:, q0 // 128 + sub, :], in0=o_sb[:, par, sub, :],
                                                                 scalar=ss_sb[:, par, sub:sub + 1], in1=g_sb[:],
                                                                 op0=ALU.mult, op1=ALU.mult),
                         R=[bo[par], bss[par], bg], W=[by])
            s.dma("sp", ydfP, y_sb[:], R=[by])
            s.finish([by])
    return nc


def tileP(a):
    return np.ascontiguousarray(a.reshape(NKT, 128, a.shape[1]).transpose(1, 0, 2))


def untileP(a):
    return a.transpose(1, 0, 2).reshape(NTOK, a.shape[2])


_NA_IDX = None


def run_p2(nc2, l, fmb, fmf, tm, inp):
    global _NA_IDX
    if _NA_IDX is None:
        _NA_IDX = na_bias_index()
    in_maps = []
    for i in range(NCORES):
        b, j = i // 4, i % 4
        xr = fmf[b, 128 * j:128 * (j + 1)]
        xf = np.concatenate([xr[:, S:], xr[:, :S]], axis=1)
        xb = np.concatenate([xr[:, S:][:, ::-1], xr[:, :S][:, ::-1]], axis=1)
        wbd = np.zeros((4, 128, 128), np.float32)
        rgv = np.zeros((128, 2, 8), np.float32)
        ch = slice(128 * j, 128 * (j + 1))
        for dr in range(2):
            for gi, wk in enumerate(("rg_w_r", "rg_w_i")):
                for bb in range(2):
                    wbd[dr * 2 + gi, 64 * bb:64 * (bb + 1), 64 * bb:64 * (bb + 1)] = inp[wk][l, dr, 2 * j + bb]
            rgv[:, dr, 0:4] = inp["rg_conv_w"][l][:, ch].T
            rgv[:, dr, 4] = inp["rg_conv_b"][l][ch]
            rgv[:, dr, 5] = inp["rg_b_r"][l, dr, ch]
            rgv[:, dr, 6] = inp["rg_b_i"][l, dr, ch]
            rgv[:, dr, 7] = inp["rg_lambda"][l, dr, ch]
        rext = np.concatenate([inp["na_rpb"][l, j].ravel(), np.array([-30000.0], np.float32)])
        bt = rext[_NA_IDX]
        in_maps.append({
            "xrg": np.ascontiguousarray(np.stack([xf, xb])), "wbd": wbd, "rgv": rgv,
            "qaT": np.ascontiguousarray(fmb[b, 64 * j:64 * (j + 1)]),
            "kaT": np.ascontiguousarray(fmb[b, 256 + 64 * j:256 + 64 * (j + 1)]),
            "vaP": tileP(tm[b][:, 64 * j:64 * (j + 1)]),
            "btT": np.ascontiguousarray(bt.transpose(1, 0, 2)),
            "qdT": np.ascontiguousarray(fmb[b, 512 + 64 * j:512 + 64 * (j + 1)].reshape(2, 32, NTOK)),
            "kdT": np.ascontiguousarray(fmb[b, 768 + 64 * j:768 + 64 * (j + 1)].reshape(2, 32, NTOK)),
            "vdP": tileP(tm[b][:, 256 + 64 * j:256 + 64 * (j + 1)]),
            "dlam": np.ascontiguousarray(np.tile(inp["diff_lambda"][l].reshape(1, 128), (128, 1))),
            "dg": np.ascontiguousarray(np.tile(inp["diff_subln_g"][l].reshape(1, 64), (128, 1))),
        })
    res = run_bass_kernel_spmd(nc2, in_maps, core_ids=list(range(NCORES)))
    yna = np.zeros((B, NTOK, 256), ml_dtypes.bfloat16)
    ydf = np.zeros((B, NTOK, 256), ml_dtypes.bfloat16)
    hf = np.zeros((B, 512, NTOK), np.float32)
    hb = np.zeros((B, 512, NTOK), np.float32)
    for i in range(NCORES):
        b, j = i // 4, i % 4
        r = res.results[i]
        yna[b, :, 64 * j:64 * (j + 1)] = untileP(r["ynaP"])
        ydf[b, :, 64 * j:64 * (j + 1)] = untileP(r["ydfP"])
        h = r["hout"]
        hf[b, 128 * j:128 * (j + 1), S:] = h[0][:, :L]
        hf[b, 128 * j:128 * (j + 1), :S] = h[0][:, L:]
        hb[b, 128 * j:128 * (j + 1), S:] = h[1][:, :L][:, ::-1]
        hb[b, 128 * j:128 * (j + 1), :S] = h[1][:, L:][:, ::-1]
    return yna, ydf, hf, hb


NEXP = 32
GELU_C = 1.5957691216057308


def build_p3(final):
    nc = bass.Bass("TRN2", target_bir_lowering=False)
    din = lambda n, shp, dt: nc.dram_tensor(n, shp, dt, kind="ExternalInput").ap()
    xT = din("xT", [128, 8, NT1], F32)
    nadf = din("nadf", [128, 4, NT1], BF16)
    hg = din("hg", [128, 12, NT1], F32)
    wout = din("wout", [D, D], F32)
    mod = din("mod", [128, 10, 8], F32)
    wge = din("wge", [128, 8, 36], F32)
    bge = din("bge", [128, 36], F32)
    selc = din("selc", [32, NEXP * 128], BF16)
    ident = din("ident", [128, 128], F32)
    w1 = din("w1", [NEXP, D, 512], F32)
    w3 = din("w3", [NEXP, D, 512], F32)
    w2 = din("w2", [NEXP, 512, D], F32)
    xo = nc.dram_tensor("xo", [128, 8, NT1], F32, kind="ExternalOutput").ap()
    s = Sched(nc)
    wov = wout.rearrange("(kc p) n -> p kc n", p=128)
    with (nc.psum_tensor("ps", [128, 8, 512], F32) as ps,
          nc.sbuf_tensor("x_sb", [128, 8, NT1], F32) as x_sb,
          nc.sbuf_tensor("mod_sb", [128, 10, 8], F32) as mod_sb,
          nc.sbuf_tensor("hl2_sb", [128, 8, NT1], BF16) as hl2_sb,
          nc.sbuf_tensor("wdt_sb", [32, 2, NT1], BF16) as wdt_sb,
          nc.sbuf_tensor("ones_sb", [128, 128], BF16) as ones_sb):
        bps = [s.buf(f"ps{i}") for i in range(8)]
        cs = s.dsem("const")
        bxb = [s.buf(f"x{i}", s.dsem(f"x{i}")) for i in range(len(BLKS1))]
        bmod = s.buf("mod", cs)
        bhl2 = [s.buf(f"hl2_{i}") for i in range(len(BLKS1))]
        bwdt = [s.buf(f"wdt{i}") for i in range(len(BLKS1))]
        bones = s.buf("ones")
        s.dma("sp", mod_sb[:], mod, W=[bmod])
        for bi, (t0, n) in enumerate(BLKS1):
            s.dma("sp", x_sb[:, :, t0:t0 + n], xT[:, :, t0:t0 + n], W=[bxb[bi]])
        s.op("pool", lambda e: e.memset(ones_sb[:], 1.0), W=[bones])

        with (nc.sbuf_tensor("s1_mix", [128, 8, NT1], BF16) as mix_sb,
              nc.sbuf_tensor("s1_wobf", [128, 8, D], BF16) as wo_bf,
              nc.sbuf_tensor("s1_wost", [128, 2, D], F32) as wo_st,
              nc.sbuf_tensor("s1_hg", [128, 1, 12, 512], F32) as hg_sb,
              nc.sbuf_tensor("s1_t", [128, 4, 512], F32) as t_sb):
            bmixl = s.buf("mixl", s.dsem("mixl"))
            bmix = [s.buf(f"mix{i}") for i in range(len(BLKS1))]
            bwost = [s.buf(f"wost{i}", s.dsem(f"wost{i}")) for i in range(2)]
            bwobf = [s.buf(f"wobf{k}") for k in range(8)]
            bhg = [s.buf(f"hg{i}", s.dsem(f"hg{i}")) for i in range(2)]
            bt = [s.buf(f"t{i}") for i in range(4)]
            s.dma("act", mix_sb[:, 0:2, :], nadf[:, 0:2, :], W=[bmixl])
            s.dma("act", mix_sb[:, 6:8, :], nadf[:, 2:4, :], W=[bmixl])
            for k in range(8):
                sl = k % 2
                s.dma("act", wo_st[:, sl, :], wov[:, k, :], W=[bwost[sl]])
                s.op("pool", lambda e: e.tensor_copy(out=wo_bf[:, k, :], in_=wo_st[:, sl, :]), R=[bwost[sl]], W=[bwobf[k]])
            for bi, (t0, n) in enumerate(BLKS1):
                sl = 0
                s.dma("sp", hg_sb[:, sl, :, 0:n], hg[:, :, t0:t0 + n], W=[bhg[sl]])
                for ch in range(4):
                    hf_ = hg_sb[:, sl, ch, 0:n]; hb_ = hg_sb[:, sl, 4 + ch, 0:n]; gr_ = hg_sb[:, sl, 8 + ch, 0:n]
                    s.op("dve", lambda e: e.tensor_tensor(out=t_sb[:, 0, 0:n], in0=hf_, in1=hb_, op=ALU.add),
                         R=[bhg[sl]], W=[bt[0]])
                    s.op("dve", lambda e: e.tensor_tensor(out=t_sb[:, 1, 0:n], in0=gr_, in1=gr_, op=ALU.mult),
                         R=[bhg[sl]], W=[bt[1]])
                    s.op("dve", lambda e: e.tensor_scalar(out=t_sb[:, 1, 0:n], in0=t_sb[:, 1, 0:n], scalar1=0.044715,
                                                          scalar2=1.0, op0=ALU.mult, op1=ALU.add), R=[bt[1]], W=[bt[1]])
                    s.op("pool", lambda e: e.tensor_tensor(out=t_sb[:, 2, 0:n], in0=t_sb[:, 1, 0:n], in1=gr_, op=ALU.mult),
                         R=[bt[1], bhg[sl]], W=[bt[2]])
                    s.op("act", lambda e: e.activation(out=t_sb[:, 2, 0:n], in_=t_sb[:, 2, 0:n], func=AF.Sigmoid,
                                                       scale=GELU_C), R=[bt[2]], W=[bt[2]])
                    s.op("pool", lambda e: e.tensor_tensor(out=t_sb[:, 3, 0:n], in0=t_sb[:, 2, 0:n], in1=gr_, op=ALU.mult),
                         R=[bt[2], bhg[sl]], W=[bt[3]])
                    s.op("pool", lambda e: e.tensor_tensor(out=mix_sb[:, 2 + ch, t0:t0 + n], in0=t_sb[:, 3, 0:n],
                                                           in1=t_sb[:, 0, 0:n], op=ALU.mult),
                         R=[bt[3], bt[0]], W=[bmix[bi]])
            pi = 0
            for bi, (t0, n) in enumerate(BLKS1):
                garow = 5 if bi == 4 else 1
                for dc in range(8):
                    pb = pi % 8; pi += 1
                    for k in range(8):
                        mm(s, ps[:, pb, 0:n], wo_bf[:, k, dc * 128:(dc + 1) * 128], mix_sb[:, k, t0:t0 + n],
                           k == 0, k == 7, R=[bwobf[k], bmix[bi], bmixl], W=[bps[pb]])
                    s.op("dve", lambda e: e.scalar_tensor_tensor(out=x_sb[:, dc, t0:t0 + n], in0=ps[:, pb, 0:n],
                                                                 scalar=mod_sb[:, garow, dc:dc + 1],
                                                                 in1=x_sb[:, dc, t0:t0 + n], op0=ALU.mult, op1=ALU.add),
                         R=[bps[pb], bmod, bxb[bi]], W=[bxb[bi]])
            s.barrier()

        with (nc.sbuf_tensor("s2_sq", [128, 8, 512], BF16) as sq_sb,
              nc.sbuf_tensor("s2_rstd", [128, 2, 512], F32) as rstd_sb,
              nc.sbuf_tensor("s2_tmp", [128, 4, 512], F32) as tmp_sb,
              nc.sbuf_tensor("s2_hf", [128, 2, 8, 512], F32) as hf_sb,
              nc.sbuf_tensor("s2_ab", [128, 2, 8], F32) as ab_sb,
              nc.sbuf_tensor("s2_wge", [128, 8, 36], F32) as wge_sb,
              nc.sbuf_tensor("s2_bge", [128, 36], F32) as bge_sb,
              nc.sbuf_tensor("s2_id", [128, 128], F32) as id_sb,
              nc.sbuf_tensor("s2_rt", [128, 2, 128], F32) as rt_sb):
            bsq = s.buf("sq"); brs = [s.buf(f"rs{i}") for i in range(2)]
            btmp = [s.buf(f"tmp{i}") for i in range(4)]
            bhf = [s.buf(f"hf{i}") for i in range(2)]
            bab = s.buf("ab")
            bwge = s.buf("wge", cs); bbge = s.buf("bge", cs); bid = s.buf("id", cs)
            brt = [s.buf(f"rt{i}") for i in range(2)]
            s.dma("act", wge_sb[:], wge, W=[bwge])
            s.dma("act", bge_sb[:], bge, W=[bbge])
            s.dma("act", id_sb[:], ident, W=[bid])
            for t in range(2):
                s.op("dve", lambda e: e.scalar_tensor_tensor(
                    out=ab_sb[:, t, :], in0=mod_sb[:, 2 + 4 * t, :], scalar=1.0, in1=mod_sb[:, 0, :],
                    op0=ALU.add, op1=ALU.mult), R=[bmod], W=[bab])
            ti_g = 0
            for bi, (t0, n) in enumerate(BLKS1):
                sl = bi % 2
                isctx = bi == 4
                s.op("act", lambda e: e.activation(out=sq_sb[:, :, 0:n], in_=x_sb[:, :, t0:t0 + n], func=AF.Square),
                     R=[bxb[bi]], W=[bsq])
                for k in range(8):
                    mm(s, ps[:, sl, 0:n], ones_sb[:], sq_sb[:, k, 0:n], k == 0, k == 7, R=[bones, bsq], W=[bps[sl]])
                s.op("act", lambda e: e.activation(out=rstd_sb[:, sl, 0:n], in_=ps[:, sl, 0:n], func=AF.Sqrt,
                                                   scale=1.0 / D, bias=EPS), R=[bps[sl]], W=[brs[sl]])
                s.op("dve", lambda e: e.reciprocal(out=rstd_sb[:, sl, 0:n], in_=rstd_sb[:, sl, 0:n]),
                     R=[brs[sl]], W=[brs[sl]])
                ai = 1 if isctx else 0
                shrow = 7 if isctx else 3
                for k in range(8):
                    tb = k % 4
                    s.op("dve", lambda e: e.scalar_tensor_tensor(
                        out=tmp_sb[:, tb, 0:n], in0=x_sb[:, k, t0:t0 + n], scalar=ab_sb[:, ai, k:k + 1],
                        in1=rstd_sb[:, sl, 0:n], op0=ALU.mult, op1=ALU.mult),
                        R=[bxb[bi], bab, brs[sl]], W=[btmp[tb]])
                    s.op("act", lambda e: e.activation(out=hf_sb[:, sl, k, 0:n], in_=tmp_sb[:, tb, 0:n],
                                                       func=AF.Identity, bias=mod_sb[:, shrow, k:k + 1], scale=1.0),
                         R=[btmp[tb], bmod], W=[bhf[sl]])
                s.op("pool", lambda e: e.tensor_copy(out=hl2_sb[:, :, t0:t0 + n], in_=hf_sb[:, sl, :, 0:n]),
                     R=[bhf[sl]], W=[bhl2[bi]])
                for tt in range((n + 127) // 128):
                    c0 = tt * 128
                    m = min(128, n - c0)
                    rs_ = ti_g % 2; ti_g += 1
                    pb = 2 + rs_
                    rt = rt_sb[0:m, rs_, :]
                    brr = brt[rs_]
                    for k in range(8):
                        mm(s, ps[0:m, pb, 0:36], hf_sb[:, sl, k, c0:c0 + m], wge_sb[:, k, :], k == 0, k == 7,
                           R=[bhf[sl], bwge], W=[bps[pb]])
                    V = lambda eng, fn, R_=(), W_=(): s.op(eng, fn, R=[brr] + list(R_), W=[brr] + list(W_))
                    lg = rt[:, 0:36]
                    s.op("dve", lambda e: e.tensor_tensor(out=lg, in0=ps[0:m, pb, 0:36], in1=bge_sb[0:m, :], op=ALU.add),
                         R=[bps[pb], bbge], W=[brr])
                    gmax = rt[:, 36:37]; ngmax = rt[:, 37:38]; sume = rt[:, 38:39]; gtop = rt[:, 39:40]
                    eg = rt[:, 40:44]; ohg = rt[:, 44:48]; sel = rt[:, 48:56]; top8 = rt[:, 56:64]
                    dd = rt[:, 64:65]; ed = rt[:, 65:66]; w1_ = rt[:, 66:67]; wt1 = rt[:, 67:68]; wt2 = rt[:, 68:69]
                    ea = rt[:, 72:80]; eb_ = rt[:, 80:88]; wd = rt[:, 96:128]
                    V("dve", lambda e: e.reduce_max(out=gmax, in_=lg[:, 0:4], axis=AX.X))
                    V("dve", lambda e: e.tensor_scalar(out=ngmax, in0=gmax, scalar1=-1.0, scalar2=None, op0=ALU.mult))
                    V("act", lambda e: e.activation(out=eg, in_=lg[:, 0:4], func=AF.Exp, bias=ngmax, scale=1.0,
                                                    accum_out=sume))
                    V("dve", lambda e: e.reciprocal(out=gtop, in_=sume))
                    V("dve", lambda e: e.tensor_scalar(out=ohg, in0=lg[:, 0:4], scalar1=gmax, scalar2=None,
                                                       op0=ALU.is_equal))
                    V("dve", lambda e: e.tensor_scalar(out=sel, in0=lg[:, 4:12], scalar1=ohg[:, 0:1], scalar2=None,
                                                       op0=ALU.mult))
                    for g in range(1, 4):
                        V("dve", lambda e: e.scalar_tensor_tensor(out=sel, in0=lg[:, 4 + 8 * g:12 + 8 * g],
                                                                  scalar=ohg[:, g:g + 1], in1=sel,
                                                                  op0=ALU.mult, op1=ALU.add))
                    V("dve", lambda e: e.max(out=top8, in_=sel))
                    V("dve", lambda e: e.tensor_tensor(out=dd, in0=top8[:, 1:2], in1=top8[:, 0:1], op=ALU.subtract))
                    V("act", lambda e: e.activation(out=ed, in_=dd, func=AF.Exp))
                    V("dve", lambda e: e.tensor_scalar(out=w1_, in0=ed, scalar1=1.0, scalar2=None, op0=ALU.add))
                    V("dve", lambda e: e.reciprocal(out=w1_, in_=w1_))
                    V("dve", lambda e: e.tensor_tensor(out=wt1, in0=w1_, in1=gtop, op=ALU.mult))
                    V("dve", lambda e: e.tensor_tensor(out=wt2, in0=wt1, in1=ed, op=ALU.mult))
                    V("dve", lambda e: e.tensor_scalar(out=ea, in0=sel, scalar1=top8[:, 0:1], scalar2=wt1,
                                                       op0=ALU.is_equal, op1=ALU.mult))
                    V("dve", lambda e: e.tensor_scalar(out=eb_, in0=sel, scalar1=top8[:, 1:2], scalar2=wt2,
                                                       op0=ALU.is_equal, op1=ALU.mult))
                    V("dve", lambda e: e.tensor_tensor(out=ea, in0=ea, in1=eb_, op=ALU.add))
                    for g in range(4):
                        V("dve", lambda e: e.tensor_scalar(out=wd[:, 8 * g:8 * g + 8], in0=ea, scalar1=ohg[:, g:g + 1],
                                                           scalar2=None, op0=ALU.mult))
                    pt = 4 + rs_
                    s.op("pe", lambda e: e.transpose(ps[0:32, pt, 0:m], wd, id_sb[0:m, 0:m]), R=[brr, bid], W=[bps[pt]])
                    s.op("act", lambda e: e.copy(out=wdt_sb[:, 0, t0 + c0:t0 + c0 + m], in_=ps[0:32, pt, 0:m]),
                         R=[bps[pt]], W=[bwdt[bi]])
                    s.op("dve", lambda e: e.tensor_tensor(out=wdt_sb[:, 1, t0 + c0:t0 + c0 + m], in0=ps[0:32, pt, 0:m],
                                                          in1=wdt_sb[:, 0, t0 + c0:t0 + c0 + m], op=ALU.subtract),
                         R=[bps[pt], bwdt[bi]], W=[bwdt[bi]])
            s.barrier()

        with (nc.sbuf_tensor("s3_st", [128, 3, 2048], F32) as st_sb,
              nc.sbuf_tensor("s3_wb", [128, 2, 6, 2048], BF16) as wb_sb,
              nc.sbuf_tensor("s3_sel", [32, NEXP * 128], BF16) as sel_sb,
              nc.sbuf_tensor("s3_wbc", [128, 2, 512], F32) as wbc_sb,
              nc.sbuf_tensor("s3_sg", [128, 2, 512], F32) as sg_sb,
              nc.sbuf_tensor("s3_t", [128, 2, 512], F32) as t3_sb,
              nc.sbuf_tensor("s3_g", [128, 2, 4, 512], BF16) as g_sb):
            bst = [s.buf(f"st{i}", s.dsem(f"st{i}")) for i in range(3)]
            bwb = [[s.buf(f"wb{a}_{p}") for p in range(6)] for a in range(2)]
            bsel = s.buf("sel", cs)
            bwbc = [s.buf(f"wbc{i}") for i in range(2)]
            bsg = [s.buf(f"sg{i}") for i in range(2)]
            bt3 = [s.buf(f"t3{i}") for i in range(2)]
            bg = [s.buf(f"g{i}") for i in range(2)]
            s.dma("act", sel_sb[:], selc, W=[bsel])
            w1v = w1.rearrange("e (kc p) f -> e p kc f", p=128)
            w3v = w3.rearrange("e (kc p) f -> e p kc f", p=128)
            w2v = w2.rearrange("e (fc p) d -> e p fc d", p=128)

            def piece_src(e, p):
                if p < 2:
                    return w1v[e, :, 4 * p:4 * p + 4, :]
                if p < 4:
                    return w3v[e, :, 4 * (p - 2):4 * (p - 2) + 4, :]
                return w2v[e, :, 2 * (p - 4):2 * (p - 4) + 2, :]

            def piece_dma(P):
                e, p = divmod(P, 6)
                if e >= NEXP:
                    return
                sl = P % 3
                dst = st_sb[:, sl, :]
                dst = dst.rearrange("q (a b) -> q a b", a=4) if p < 4 else dst.rearrange("q (a b) -> q a b", a=2)
                s.dma("sp", dst, piece_src(e, p), W=[bst[sl]])

            def piece_cast(P):
                e, p = divmod(P, 6)
                if e >= NEXP:
                    return
                sl = P % 3
                s.op("pool", lambda en: en.tensor_copy(out=wb_sb[:, e % 2, p, :], in_=st_sb[:, sl, :]),
                     R=[bst[sl]], W=[bwb[e % 2][p]])

            for P in range(3):
                piece_dma(P)
            for P in range(6):
                piece_cast(P)
                piece_dma(P + 3)
            gi = 0
            for ex in range(NEXP):
                a = ex % 2
                for bi, (t0, n) in enumerate(BLKS1):
                    if bi < 3:
                        for P in (6 * (ex + 1) + 2 * bi, 6 * (ex + 1) + 2 * bi + 1):
                            piece_cast(P)
                            piece_dma(P + 3)
                    garow = 8 if bi == 4 else 4
                    wr = gi % 2
                    gs = gi % 2
                    gi += 1
                    mm(s, ps[:, 6, 0:n], sel_sb[:, ex * 128:(ex + 1) * 128], wdt_sb[:, 0, t0:t0 + n], True, False,
                       R=[bsel, bwdt[bi]], W=[bps[6]])
                    mm(s, ps[:, 6, 0:n], sel_sb[:, ex * 128:(ex + 1) * 128], wdt_sb[:, 1, t0:t0 + n], False, True,
                       R=[bsel, bwdt[bi]], W=[bps[6]])
                    s.op("act", lambda e: e.copy(out=wbc_sb[:, wr, 0:n], in_=ps[:, 6, 0:n]), R=[bps[6]], W=[bwbc[wr]])
                    for fc in range(4):
                        pr = fc % 2
                        for which in range(2):
                            bank = 2 * pr + which
                            for k in range(8):
                                wv = wb_sb[:, a, 2 * which + k // 4, :].rearrange("q (a b) -> q a b", a=4)
                                mm(s, ps[:, bank, 0:n], wv[:, k % 4, fc * 128:(fc + 1) * 128], hl2_sb[:, k, t0:t0 + n],
                                   k == 0, k == 7, R=[bwb[a][2 * which + k // 4], bhl2[bi]], W=[bps[bank]])
                        s.op("act", lambda e: e.activation(out=sg_sb[:, pr, 0:n], in_=ps[:, 2 * pr, 0:n], func=AF.Silu),
                             R=[bps[2 * pr]], W=[bsg[pr]])
                        s.op("dve", lambda e: e.tensor_tensor(out=t3_sb[:, pr, 0:n], in0=ps[:, 2 * pr + 1, 0:n],
                                                              in1=sg_sb[:, pr, 0:n], op=ALU.mult),
                             R=[bps[2 * pr + 1], bsg[pr]], W=[bt3[pr]])
                        s.op("pool", lambda e: e.tensor_tensor(out=g_sb[:, gs, fc, 0:n], in0=t3_sb[:, pr, 0:n],
                                                               in1=wbc_sb[:, wr, 0:n], op=ALU.mult),
                             R=[bt3[pr], bwbc[wr]], W=[bg[gs]])
                    for dc in range(8):
                        bank = 4 + dc % 2
                        for fc in range(4):
                            wv = wb_sb[:, a, 4 + fc // 2, :].rearrange("q (a b) -> q a b", a=2)
                            mm(s, ps[:, bank, 0:n], wv[:, fc % 2, dc * 128:(dc + 1) * 128], g_sb[:, gs, fc, 0:n],
                               fc == 0, fc == 3, R=[bwb[a][4 + fc // 2], bg[gs]], W=[bps[bank]])
                        s.op("dve", lambda e: e.scalar_tensor_tensor(out=x_sb[:, dc, t0:t0 + n], in0=ps[:, bank, 0:n],
                                                                     scalar=mod_sb[:, garow, dc:dc + 1],
                                                                     in1=x_sb[:, dc, t0:t0 + n], op0=ALU.mult, op1=ALU.add),
                             R=[bps[bank], bmod, bxb[bi]], W=[bxb[bi]])
            s.barrier()

        with (nc.sbuf_tensor("s4_sq", [128, 8, 512], BF16) as sq_sb,
              nc.sbuf_tensor("s4_rstd", [128, 2, 512], F32) as rstd_sb):
            bsq = s.buf("sq4"); brs = [s.buf(f"rs4{i}") for i in range(2)]
            for bi, (t0, n) in enumerate(BLKS1):
                if final:
                    sl = bi % 2
                    s.op("act", lambda e: e.activation(out=sq_sb[:, :, 0:n], in_=x_sb[:, :, t0:t0 + n], func=AF.Square),
                         R=[bxb[bi]], W=[bsq])
                    for k in range(8):
                        mm(s, ps[:, sl, 0:n], ones_sb[:], sq_sb[:, k, 0:n], k == 0, k == 7, R=[bones, bsq], W=[bps[sl]])
                    s.op("act", lambda e: e.activation(out=rstd_sb[:, sl, 0:n], in_=ps[:, sl, 0:n], func=AF.Sqrt,
                                                       scale=1.0 / D, bias=EPS), R=[bps[sl]], W=[brs[sl]])
                    s.op("dve", lambda e: e.reciprocal(out=rstd_sb[:, sl, 0:n], in_=rstd_sb[:, sl, 0:n]),
                         R=[brs[sl]], W=[brs[sl]])
                    for k in range(8):
                        s.op("dve", lambda e: e.scalar_tensor_tensor(
                            out=x_sb[:, k, t0:t0 + n], in0=x_sb[:, k, t0:t0 + n], scalar=mod_sb[:, 9, k:k + 1],
                            in1=rstd_sb[:, sl, 0:n], op0=ALU.mult, op1=ALU.mult),
                            R=[bxb[bi], bmod, brs[sl]], W=[bxb[bi]])
                s.dma("sp", xo[:, :, t0:t0 + n], x_sb[:, :, t0:t0 + n], R=[bxb[bi]])
            s.finish(bxb)
    return nc


def build_p3a():
    nc = bass.Bass("TRN2", target_bir_lowering=False)
    din = lambda n, shp, dt: nc.dram_tensor(n, shp, dt, kind="ExternalInput").ap()
    xT = din("xT", [128, 8, NT1], F32)
    nadf = din("nadf", [128, 4, NT1], BF16)
    hg = din("hg", [128, 12, NT1], F32)
    wout = din("wout", [D, D], F32)
    mod = din("mod", [128, 10, 8], F32)
    wge = din("wge", [128, 8, 36], F32)
    bge = din("bge", [128, 36], F32)
    iota4 = din("iota4", [128, 4], F32)
    ident = din("ident", [128, 128], F32)
    xo = nc.dram_tensor("xo", [128, 8, NT1], F32, kind="ExternalOutput").ap()
    hl2o = nc.dram_tensor("hl2o", [128, 8, NT1], BF16, kind="ExternalOutput").ap()
    wdto = nc.dram_tensor("wdto", [32, 2, NT1], BF16, kind="ExternalOutput").ap()
    gido = nc.dram_tensor("gido", [128, 17], F32, kind="ExternalOutput").ap()
    wd32o = nc.dram_tensor("wd32o", [128, 17, 32], F32, kind="ExternalOutput").ap()
    s = Sched(nc)
    wov = wout.rearrange("(kc p) n -> p kc n", p=128)
    with (nc.psum_tensor("ps", [128, 8, 512], F32) as ps,
          nc.sbuf_tensor("x_sb", [128, 8, NT1], F32) as x_sb,
          nc.sbuf_tensor("mod_sb", [128, 10, 8], F32) as mod_sb,
          nc.sbuf_tensor("hl2_sb", [128, 8, NT1], BF16) as hl2_sb,
          nc.sbuf_tensor("wdt_sb", [32, 2, NT1], BF16) as wdt_sb,
          nc.sbuf_tensor("ones_sb", [128, 128], BF16) as ones_sb):
        bps = [s.buf(f"ps{i}") for i in range(8)]
        cs = s.dsem("const")
        bxb = [s.buf(f"x{i}", s.dsem(f"x{i}")) for i in range(len(BLKS1))]
        bmod = s.buf("mod", cs)
        bhl2 = [s.buf(f"hl2_{i}") for i in range(len(BLKS1))]
        bwdt = [s.buf(f"wdt{i}") for i in range(len(BLKS1))]
        bones = s.buf("ones")
        s.dma("sp", mod_sb[:], mod, W=[bmod])
        for bi, (t0, n) in enumerate(BLKS1):
            s.dma("sp", x_sb[:, :, t0:t0 + n], xT[:, :, t0:t0 + n], W=[bxb[bi]])
        s.op("pool", lambda e: e.memset(ones_sb[:], 1.0), W=[bones])

        with (nc.sbuf_tensor("s1_mix", [128, 8, NT1], BF16) as mix_sb,
              nc.sbuf_tensor("s1_wobf", [128, 8, D], BF16) as wo_bf,
              nc.sbuf_tensor("s1_wost", [128, 2, D], F32) as wo_st,
              nc.sbuf_tensor("s1_hg", [128, 1, 12, 512], F32) as hg_sb,
              nc.sbuf_tensor("s1_t", [128, 4, 512], F32) as t_sb):
            bmixl = s.buf("mixl", s.dsem("mixl"))
            bmix = [s.buf(f"mix{i}") for i in range(len(BLKS1))]
            bwost = [s.buf(f"wost{i}", s.dsem(f"wost{i}")) for i in range(2)]
            bwobf = [s.buf(f"wobf{k}") for k in range(8)]
            bhg = [s.buf(f"hg{i}", s.dsem(f"hg{i}")) for i in range(2)]
            bt = [s.buf(f"t{i}") for i in range(4)]
            s.dma("act", mix_sb[:, 0:2, :], nadf[:, 0:2, :], W=[bmixl])
            s.dma("act", mix_sb[:, 6:8, :], nadf[:, 2:4, :], W=[bmixl])
            for k in range(8):
                sl = k % 2
                s.dma("act", wo_st[:, sl, :], wov[:, k, :], W=[bwost[sl]])
                s.op("pool", lambda e: e.tensor_copy(out=wo_bf[:, k, :], in_=wo_st[:, sl, :]), R=[bwost[sl]], W=[bwobf[k]])
            for bi, (t0, n) in enumerate(BLKS1):
                sl = 0
                s.dma("sp", hg_sb[:, sl, :, 0:n], hg[:, :, t0:t0 + n], W=[bhg[sl]])
                for ch in range(4):
                    hf_ = hg_sb[:, sl, ch, 0:n]; hb_ = hg_sb[:, sl, 4 + ch, 0:n]; gr_ = hg_sb[:, sl, 8 + ch, 0:n]
                    s.op("dve", lambda e: e.tensor_tensor(out=t_sb[:, 0, 0:n], in0=hf_, in1=hb_, op=ALU.add),
                         R=[bhg[sl]], W=[bt[0]])
                    s.op("dve", lambda e: e.tensor_tensor(out=t_sb[:, 1, 0:n], in0=gr_, in1=gr_, op=ALU.mult),
                         R=[bhg[sl]], W=[bt[1]])
                    s.op("dve", lambda e: e.tensor_scalar(out=t_sb[:, 1, 0:n], in0=t_sb[:, 1, 0:n], scalar1=0.044715,
                                                          scalar2=1.0, op0=ALU.mult, op1=ALU.add), R=[bt[1]], W=[bt[1]])
                    s.op("pool", lambda e: e.tensor_tensor(out=t_sb[:, 2, 0:n], in0=t_sb[:, 1, 0:n], in1=gr_, op=ALU.mult),
                         R=[bt[1], bhg[sl]], W=[bt[2]])
                    s.op("act", lambda e: e.activation(out=t_sb[:, 2, 0:n], in_=t_sb[:, 2, 0:n], func=AF.Sigmoid,
                                                       scale=GELU_C), R=[bt[2]], W=[bt[2]])
                    s.op("pool", lambda e: e.tensor_tensor(out=t_sb[:, 3, 0:n], in0=t_sb[:, 2, 0:n], in1=gr_, op=ALU.mult),
                         R=[bt[2], bhg[sl]], W=[bt[3]])
                    s.op("pool", lambda e: e.tensor_tensor(out=mix_sb[:, 2 + ch, t0:t0 + n], in0=t_sb[:, 3, 0:n],
                                                           in1=t_sb[:, 0, 0:n], op=ALU.mult),
                         R=[bt[3], bt[0]], W=[bmix[bi]])
            pi = 0
            for bi, (t0, n) in enumerate(BLKS1):
                garow = 5 if bi == 4 else 1
                for dc in range(8):
                    pb = pi % 8; pi += 1
                    for k in range(8):
                        mm(s, ps[:, pb, 0:n], wo_bf[:, k, dc * 128:(dc + 1) * 128], mix_sb[:, k, t0:t0 + n],
                           k == 0, k == 7, R=[bwobf[k], bmix[bi], bmixl], W=[bps[pb]])
                    s.op("dve", lambda e: e.scalar_tensor_tensor(out=x_sb[:, dc, t0:t0 + n], in0=ps[:, pb, 0:n],
                                                                 scalar=mod_sb[:, garow, dc:dc + 1],
                                                                 in1=x_sb[:, dc, t0:t0 + n], op0=ALU.mult, op1=ALU.add),
                         R=[bps[pb], bmod, bxb[bi]], W=[bxb[bi]])
            s.barrier()

        es_ = ExitStack()
        io4_sb = es_.enter_context(nc.sbuf_tensor("s2_io4", [128, 4], F32))
        gid_sb = es_.enter_context(nc.sbuf_tensor("s2_gid", [128, 17], F32))
        junk4_sb = es_.enter_context(nc.sbuf_tensor("s2_junk", [128, 4], F32))
        wd32_sb = es_.enter_context(nc.sbuf_tensor("s2_wd32", [128, 17, 32], F32))
        with (nc.sbuf_tensor("s2_sq", [128, 8, 512], BF16) as sq_sb,
              nc.sbuf_tensor("s2_rstd", [128, 2, 512], F32) as rstd_sb,
              nc.sbuf_tensor("s2_tmp", [128, 4, 512], F32) as tmp_sb,
              nc.sbuf_tensor("s2_hf", [128, 2, 8, 512], F32) as hf_sb,
              nc.sbuf_tensor("s2_ab", [128, 2, 8], F32) as ab_sb,
              nc.sbuf_tensor("s2_wge", [128, 8, 36], F32) as wge_sb,
              nc.sbuf_tensor("s2_bge", [128, 36], F32) as bge_sb,
              nc.sbuf_tensor("s2_id", [128, 128], F32) as id_sb,
              nc.sbuf_tensor("s2_rt", [128, 2, 128], F32) as rt_sb):
            bsq = s.buf("sq"); brs = [s.buf(f"rs{i}") for i in range(2)]
            btmp = [s.buf(f"tmp{i}") for i in range(4)]
            bhf = [s.buf(f"hf{i}") for i in range(2)]
            bab = s.buf("ab")
            bwge = s.buf("wge", cs); bbge = s.buf("bge", cs); bid = s.buf("id", cs)
            brt = [s.buf(f"rt{i}") for i in range(2)]
            s.dma("act", wge_sb[:], wge, W=[bwge])
            s.dma("act", bge_sb[:], bge, W=[bbge])
            s.dma("act", id_sb[:], ident, W=[bid])
            bio4 = s.buf("io4", cs); bgid = s.buf("gid", s.dsem("gid")); bjk = s.buf("junk4")
            s.dma("act", io4_sb[:], iota4, W=[bio4])
            s.op("pool", lambda e: e.memset(gid_sb[:], 0.0), W=[bgid])
            bwd32 = s.buf("wd32", s.dsem("wd32"))
            s.op("pool", lambda e: e.memset(wd32_sb[:], 0.0), W=[bwd32])
            for t in range(2):
                s.op("dve", lambda e: e.scalar_tensor_tensor(
                    out=ab_sb[:, t, :], in0=mod_sb[:, 2 + 4 * t, :], scalar=1.0, in1=mod_sb[:, 0, :],
                    op0=ALU.add, op1=ALU.mult), R=[bmod], W=[bab])
            ti_g = 0
            for bi, (t0, n) in enumerate(BLKS1):
                sl = bi % 2
                isctx = bi == 4
                s.op("act", lambda e: e.activation(out=sq_sb[:, :, 0:n], in_=x_sb[:, :, t0:t0 + n], func=AF.Square),
                     R=[bxb[bi]], W=[bsq])
                for k in range(8):
                    mm(s, ps[:, sl, 0:n], ones_sb[:], sq_sb[:, k, 0:n], k == 0, k == 7, R=[bones, bsq], W=[bps[sl]])
                s.op("act", lambda e: e.activation(out=rstd_sb[:, sl, 0:n], in_=ps[:, sl, 0:n], func=AF.Sqrt,
                                                   scale=1.0 / D, bias=EPS), R=[bps[sl]], W=[brs[sl]])
                s.op("dve", lambda e: e.reciprocal(out=rstd_sb[:, sl, 0:n], in_=rstd_sb[:, sl, 0:n]),
                     R=[brs[sl]], W=[brs[sl]])
                ai = 1 if isctx else 0
                shrow = 7 if isctx else 3
                for k in range(8):
                    tb = k % 4
                    s.op("dve", lambda e: e.scalar_tensor_tensor(
                        out=tmp_sb[:, tb, 0:n], in0=x_sb[:, k, t0:t0 + n], scalar=ab_sb[:, ai, k:k + 1],
                        in1=rstd_sb[:, sl, 0:n], op0=ALU.mult, op1=ALU.mult),
                        R=[bxb[bi], bab, brs[sl]], W=[btmp[tb]])
                    s.op("act", lambda e: e.activation(out=hf_sb[:, sl, k, 0:n], in_=tmp_sb[:, tb, 0:n],
                                                       func=AF.Identity, bias=mod_sb[:, shrow, k:k + 1], scale=1.0),
                         R=[btmp[tb], bmod], W=[bhf[sl]])
                s.op("pool", lambda e: e.tensor_copy(out=hl2_sb[:, :, t0:t0 + n], in_=hf_sb[:, sl, :, 0:n]),
                     R=[bhf[sl]], W=[bhl2[bi]])
                for tt in range((n + 127) // 128):
                    c0 = tt * 128
                    m = min(128, n - c0)
                    rs_ = ti_g % 2; ti_g += 1
                    pb = 2 + rs_
                    rt = rt_sb[0:m, rs_, :]
                    brr = brt[rs_]
                    for k in range(8):
                        mm(s, ps[0:m, pb, 0:36], hf_sb[:, sl, k, c0:c0 + m], wge_sb[:, k, :], k == 0, k == 7,
                           R=[bhf[sl], bwge], W=[bps[pb]])
                    V = lambda eng, fn, R_=(), W_=(): s.op(eng, fn, R=[brr] + list(R_), W=[brr] + list(W_))
                    lg = rt[:, 0:36]
                    s.op("dve", lambda e: e.tensor_tensor(out=lg, in0=ps[0:m, pb, 0:36], in1=bge_sb[0:m, :], op=ALU.add),
                         R=[bps[pb], bbge], W=[brr])
                    gmax = rt[:, 36:37]; ngmax = rt[:, 37:38]; sume = rt[:, 38:39]; gtop = rt[:, 39:40]
                    eg = rt[:, 40:44]; ohg = rt[:, 44:48]; sel = rt[:, 48:56]; top8 = rt[:, 56:64]
                    dd = rt[:, 64:65]; ed = rt[:, 65:66]; w1_ = rt[:, 66:67]; wt1 = rt[:, 67:68]; wt2 = rt[:, 68:69]
                    ea = rt[:, 72:80]; eb_ = rt[:, 80:88]; wd = rt[:, 96:128]
                    V("dve", lambda e: e.reduce_max(out=gmax, in_=lg[:, 0:4], axis=AX.X))
                    V("dve", lambda e: e.tensor_scalar(out=ngmax, in0=gmax, scalar1=-1.0, scalar2=None, op0=ALU.mult))
                    V("act", lambda e: e.activation(out=eg, in_=lg[:, 0:4], func=AF.Exp, bias=ngmax, scale=1.0,
                                                    accum_out=sume))
                    V("dve", lambda e: e.reciprocal(out=gtop, in_=sume))
                    V("dve", lambda e: e.tensor_scalar(out=ohg, in0=lg[:, 0:4], scalar1=gmax, scalar2=None,
                                                       op0=ALU.is_equal))
                    tgl = (t0 + c0) // 128
                    s.op("dve", lambda e: e.scalar_tensor_tensor(out=junk4_sb[0:m, :], in0=ohg, scalar=1.0,
                                                                 in1=io4_sb[0:m, :], op0=ALU.mult, op1=ALU.mult,
                                                                 accum_out=gid_sb[0:m, tgl:tgl + 1]),
                         R=[brr, bio4, bjk], W=[bjk, bgid])
                    V("dve", lambda e: e.tensor_scalar(out=sel, in0=lg[:, 4:12], scalar1=ohg[:, 0:1], scalar2=None,
                                                       op0=ALU.mult))
                    for g in range(1, 4):
                        V("dve", lambda e: e.scalar_tensor_tensor(out=sel, in0=lg[:, 4 + 8 * g:12 + 8 * g],
                                                                  scalar=ohg[:, g:g + 1], in1=sel,
                                                                  op0=ALU.mult, op1=ALU.add))
                    V("dve", lambda e: e.max(out=top8, in_=sel))
                    V("dve", lambda e: e.tensor_tensor(out=dd, in0=top8[:, 1:2], in1=top8[:, 0:1], op=ALU.subtract))
                    V("act", lambda e: e.activation(out=ed, in_=dd, func=AF.Exp))
                    V("dve", lambda e: e.tensor_scalar(out=w1_, in0=ed, scalar1=1.0, scalar2=None, op0=ALU.add))
                    V("dve", lambda e: e.reciprocal(out=w1_, in_=w1_))
                    V("dve", lambda e: e.tensor_tensor(out=wt1, in0=w1_, in1=gtop, op=ALU.mult))
                    V("dve", lambda e: e.tensor_tensor(out=wt2, in0=wt1, in1=ed, op=ALU.mult))
                    V("dve", lambda e: e.tensor_scalar(out=ea, in0=sel, scalar1=top8[:, 0:1], scalar2=wt1,
                                                       op0=ALU.is_equal, op1=ALU.mult))
                    V("dve", lambda e: e.tensor_scalar(out=eb_, in0=sel, scalar1=top8[:, 1:2], scalar2=wt2,
                                                       op0=ALU.is_equal, op1=ALU.mult))
                    V("dve", lambda e: e.tensor_tensor(out=ea, in0=ea, in1=eb_, op=ALU.add))
                    for g in range(4):
                        V("dve", lambda e: e.tensor_scalar(out=wd[:, 8 * g:8 * g + 8], in0=ea, scalar1=ohg[:, g:g + 1],
                                                           scalar2=None, op0=ALU.mult))
                    s.op("dve", lambda e: e.tensor_copy(out=wd32_sb[0:m, tgl, :], in_=wd), R=[brr], W=[bwd32])
                    pt = 4 + rs_
                    s.op("pe", lambda e: e.transpose(ps[0:32, pt, 0:m], wd, id_sb[0:m, 0:m]), R=[brr, bid], W=[bps[pt]])
                    s.op("act", lambda e: e.copy(out=wdt_sb[:, 0, t0 + c0:t0 + c0 + m], in_=ps[0:32, pt, 0:m]),
                         R=[bps[pt]], W=[bwdt[bi]])
                    s.op("dve", lambda e: e.tensor_tensor(out=wdt_sb[:, 1, t0 + c0:t0 + c0 + m], in0=ps[0:32, pt, 0:m],
                                                          in1=wdt_sb[:, 0, t0 + c0:t0 + c0 + m], op=ALU.subtract),
                         R=[bps[pt], bwdt[bi]], W=[bwdt[bi]])
            bxo = s.buf("xout", s.dsem("xout"))
            s.dma("sp", xo, x_sb[:], R=bxb, W=[bxo])
            s.dma("sp", hl2o, hl2_sb[:], R=bhl2, W=[bxo])
            s.dma("sp", wdto, wdt_sb[:], R=bwdt, W=[bxo])
            s.dma("sp", gido, gid_sb[:], R=[bgid], W=[bxo])
            s.dma("sp", wd32o, wd32_sb[:], R=[bwd32], W=[bxo])
            s.finish([bxo])
        es_.close()
    return nc


def build_p3b(ntb):
    NE = 8
    nblk_all = ntb // 512
    nh = 2 if ntb > 2048 else 1
    nblk = -(-nblk_all // nh)
    nth = nblk * 512
    nc = bass.Bass("TRN2", target_bir_lowering=False)
    din = lambda n, shp, dt: nc.dram_tensor(n, shp, dt, kind="ExternalInput").ap()
    hl2 = din("hl2", [128, 8, ntb], BF16)
    wdt = din("wdt", [8, 2, ntb], BF16)
    selc = din("selc", [8, NE * 128], BF16)
    w1 = din("w1", [NE, D, 512], F32)
    w3 = din("w3", [NE, D, 512], F32)
    w2 = din("w2", [NE, 512, D], F32)
    yo = nc.dram_tensor("yo", [128, 8, ntb], F32, kind="ExternalOutput").ap()
    s = Sched(nc)
    with (nc.psum_tensor("ps", [128, 8, 512], F32) as ps,
          nc.sbuf_tensor("y_sb", [128, 8, nth], F32) as y_sb,
          nc.sbuf_tensor("hl2_sb", [128, 8, nth], BF16) as hl2_sb,
          nc.sbuf_tensor("wdt_sb", [8, 2, ntb], BF16) as wdt_sb,
          nc.sbuf_tensor("s3_st", [128, 3, 2048], F32) as st_sb,
          nc.sbuf_tensor("s3_wb", [128, 2, 6, 2048], BF16) as wb_sb,
          nc.sbuf_tensor("s3_sel", [8, NE * 128], BF16) as sel_sb,
          nc.sbuf_tensor("s3_wbc", [128, 2, 512], F32) as wbc_sb,
          nc.sbuf_tensor("s3_sg", [128, 2, 512], F32) as sg_sb,
          nc.sbuf_tensor("s3_t", [128, 2, 512], F32) as t3_sb,
          nc.sbuf_tensor("s3_g", [128, 2, 4, 512], BF16) as g_sb):
        bps = [s.buf(f"ps{i}") for i in range(8)]
        cs = s.dsem("const")
        by = [s.buf(f"y{i}", s.dsem(f"y{i}")) for i in range(nblk)]
        bhl2 = [s.buf(f"hl2_{i}", s.dsem(f"hl{i}")) for i in range(nblk)]
        bwdt = s.buf("wdt", cs)
        bst = [s.buf(f"st{i}", s.dsem(f"st{i}")) for i in range(3)]
        bwb = [[s.buf(f"wb{a}_{p}") for p in range(6)] for a in range(2)]
        bsel = s.buf("sel", cs)
        bwbc = [s.buf(f"wbc{i}") for i in range(2)]
        bsg = [s.buf(f"sg{i}") for i in range(2)]
        bt3 = [s.buf(f"t3{i}") for i in range(2)]
        bg = [s.buf(f"g{i}") for i in range(2)]
        s.dma("act", sel_sb[:], selc, W=[bsel])
        s.dma("act", wdt_sb[:], wdt, W=[bwdt])
        w1v = w1.rearrange("e (kc p) f -> e p kc f", p=128)
        w3v = w3.rearrange("e (kc p) f -> e p kc f", p=128)
        w2v = w2.rearrange("e (fc p) d -> e p fc d", p=128)

        def piece_src(e, p):
            if p < 2:
                return w1v[e, :, 4 * p:4 * p + 4, :]
            if p < 4:
                return w3v[e, :, 4 * (p - 2):4 * (p - 2) + 4, :]
            return w2v[e, :, 2 * (p - 4):2 * (p - 4) + 2, :]

        def piece_dma(P):
            e, p = divmod(P, 6)
            if e >= NE * nh:
                return
            e = e % NE
            sl = P % 3
            dst = st_sb[:, sl, :]
            dst = dst.rearrange("q (a b) -> q a b", a=4) if p < 4 else dst.rearrange("q (a b) -> q a b", a=2)
            s.dma("sp", dst, piece_src(e, p), W=[bst[sl]])

        def piece_cast(P):
            e, p = divmod(P, 6)
            if e >= NE * nh:
                return
            sl = P % 3
            s.op("pool", lambda en: en.tensor_copy(out=wb_sb[:, e % 2, p, :], in_=st_sb[:, sl, :]),
                 R=[bst[sl]], W=[bwb[e % 2][p]])

        for P in range(3):
            piece_dma(P)
        for P in range(6):
            piece_cast(P)
            piece_dma(P + 3)
        gi = 0
        n = 512
        pend = [6 * 1 + i for i in range(6)]
        for vx in range(NE * nh):
            half, ex = divmod(vx, NE)
            a = vx % 2
            pend = [6 * (vx + 1) + i for i in range(6)]
            hb0 = half * nblk
            nb_h = min(nblk, nblk_all - hb0)
            if ex == 0:
                for bi in range(nb_h):
                    s.dma("act", hl2_sb[:, :, bi * 512:(bi + 1) * 512], hl2[:, :, (hb0 + bi) * 512:(hb0 + bi + 1) * 512],
                          W=[bhl2[bi]])
            for bi in range(nb_h):
                t0 = bi * 512
                tg = (hb0 + bi) * 512
                npc = (6 + nb_h - 1) // nb_h
                for P in pend[bi * npc:(bi + 1) * npc]:
                    piece_cast(P)
                    piece_dma(P + 3)
                wr = gi % 2
                gs = gi % 2
                gi += 1
                mm(s, ps[:, 6, 0:n], sel_sb[:, ex * 128:(ex + 1) * 128], wdt_sb[:, 0, tg:tg + n], True, False,
                   R=[bsel, bwdt], W=[bps[6]])
                mm(s, ps[:, 6, 0:n], sel_sb[:, ex * 128:(ex + 1) * 128], wdt_sb[:, 1, tg:tg + n], False, True,
                   R=[bsel, bwdt], W=[bps[6]])
                s.op("act", lambda e: e.copy(out=wbc_sb[:, wr, 0:n], in_=ps[:, 6, 0:n]), R=[bps[6]], W=[bwbc[wr]])
                for fc in range(4):
                    pr = fc % 2
                    for which in range(2):
                        bank = 2 * pr + which
                        for k in range(8):
                            wv = wb_sb[:, a, 2 * which + k // 4, :].rearrange("q (a b) -> q a b", a=4)
                            mm(s, ps[:, bank, 0:n], wv[:, k % 4, fc * 128:(fc + 1) * 128], hl2_sb[:, k, t0:t0 + n],
                               k == 0, k == 7, R=[bwb[a][2 * which + k // 4], bhl2[bi]], W=[bps[bank]])
                    s.op("act", lambda e: e.activation(out=sg_sb[:, pr, 0:n], in_=ps[:, 2 * pr, 0:n], func=AF.Silu),
                         R=[bps[2 * pr]], W=[bsg[pr]])
                    s.op("dve", lambda e: e.tensor_tensor(out=t3_sb[:, pr, 0:n], in0=ps[:, 2 * pr + 1, 0:n],
                                                          in1=sg_sb[:, pr, 0:n], op=ALU.mult),
                         R=[bps[2 * pr + 1], bsg[pr]], W=[bt3[pr]])
                    s.op("pool", lambda e: e.tensor_tensor(out=g_sb[:, gs, fc, 0:n], in0=t3_sb[:, pr, 0:n],
                                                           in1=wbc_sb[:, wr, 0:n], op=ALU.mult),
                         R=[bt3[pr], bwbc[wr]], W=[bg[gs]])
                for dc in range(8):
                    bank = 4 + dc % 2
                    for fc in range(4):
                        wv = wb_sb[:, a, 4 + fc // 2, :].rearrange("q (a b) -> q a b", a=2)
                        mm(s, ps[:, bank, 0:n], wv[:, fc % 2, dc * 128:(dc + 1) * 128], g_sb[:, gs, fc, 0:n],
                           fc == 0, fc == 3, R=[bwb[a][4 + fc // 2], bg[gs]], W=[bps[bank]])
                    if ex == 0:
                        s.op("dve", lambda e: e.tensor_copy(out=y_sb[:, dc, t0:t0 + n], in_=ps[:, bank, 0:n]),
                             R=[bps[bank]], W=[by[bi]])
                    else:
                        s.op("dve", lambda e: e.tensor_tensor(out=y_sb[:, dc, t0:t0 + n], in0=ps[:, bank, 0:n],
                                                              in1=y_sb[:, dc, t0:t0 + n], op=ALU.add),
                             R=[bps[bank], by[bi]], W=[by[bi]])
            if ex == NE - 1:
                for bi in range(nb_h):
                    s.dma("sp", yo[:, :, (hb0 + bi) * 512:(hb0 + bi + 1) * 512], y_sb[:, :, bi * 512:(bi + 1) * 512],
                          R=[by[bi]])
        s.finish(by)
    return nc


def build_pc(final):
    nc = bass.Bass("TRN2", target_bir_lowering=False)
    din = lambda n, shp, dt: nc.dram_tensor(n, shp, dt, kind="ExternalInput").ap()
    xT = din("xT", [128, 8, NT1], F32)
    yT = din("yT", [128, 8, NT1], F32)
    mod = din("mod", [128, 3, 8], F32)
    xo = nc.dram_tensor("xo", [128, 8, NT1], F32, kind="ExternalOutput").ap()
    s = Sched(nc)
    with (nc.psum_tensor("ps", [128, 2, 512], F32) as ps,
          nc.sbuf_tensor("x_sb", [128, 8, NT1], F32) as x_sb,
          nc.sbuf_tensor("y_sb", [128, 8, NT1], F32) as y_sb,
          nc.sbuf_tensor("mod_sb", [128, 3, 8], F32) as mod_sb,
          nc.sbuf_tensor("ones_sb", [128, 128], BF16) as ones_sb,
          nc.sbuf_tensor("sq_sb", [128, 8, 512], BF16) as sq_sb,
          nc.sbuf_tensor("rstd_sb", [128, 2, 512], F32) as rstd_sb):
        bxb = [s.buf(f"x{i}", s.dsem(f"x{i}")) for i in range(len(BLKS1))]
        byb = [s.buf(f"y{i}", s.dsem(f"yy{i}")) for i in range(len(BLKS1))]
        bmod = s.buf("mod", s.dsem("mod"))
        bones = s.buf("ones"); bsq = s.buf("sq"); brs = [s.buf(f"rs{i}") for i in range(2)]
        bps = [s.buf(f"ps{i}") for i in range(2)]
        s.dma("sp", mod_sb[:], mod, W=[bmod])
        s.op("pool", lambda e: e.memset(ones_sb[:], 1.0), W=[bones])
        for bi, (t0, n) in enumerate(BLKS1):
            s.dma("sp", x_sb[:, :, t0:t0 + n], xT[:, :, t0:t0 + n], W=[bxb[bi]])
            s.dma("act", y_sb[:, :, t0:t0 + n], yT[:, :, t0:t0 + n], W=[byb[bi]])
        for bi, (t0, n) in enumerate(BLKS1):
            garow = 1 if bi == 4 else 0
            sl = bi % 2
            for k in range(8):
                s.op("dve", lambda e: e.scalar_tensor_tensor(
                    out=x_sb[:, k, t0:t0 + n], in0=y_sb[:, k, t0:t0 + n], scalar=mod_sb[:, garow, k:k + 1],
                    in1=x_sb[:, k, t0:t0 + n], op0=ALU.mult, op1=ALU.add),
                    R=[byb[bi], bmod, bxb[bi]], W=[bxb[bi]])
            if final:
                s.op("act", lambda e: e.activation(out=sq_sb[:, :, 0:n], in_=x_sb[:, :, t0:t0 + n], func=AF.Square),
                     R=[bxb[bi]], W=[bsq])
                for k in range(8):
                    mm(s, ps[:, sl, 0:n], ones_sb[:], sq_sb[:, k, 0:n], k == 0, k == 7, R=[bones, bsq], W=[bps[sl]])
                s.op("act", lambda e: e.activation(out=rstd_sb[:, sl, 0:n], in_=ps[:, sl, 0:n], func=AF.Sqrt,
                                                   scale=1.0 / D, bias=EPS), R=[bps[sl]], W=[brs[sl]])
                s.op("dve", lambda e: e.reciprocal(out=rstd_sb[:, sl, 0:n], in_=rstd_sb[:, sl, 0:n]),
                     R=[brs[sl]], W=[brs[sl]])
                for k in range(8):
                    s.op("dve", lambda e: e.scalar_tensor_tensor(
                        out=x_sb[:, k, t0:t0 + n], in0=x_sb[:, k, t0:t0 + n], scalar=mod_sb[:, 2, k:k + 1],
                        in1=rstd_sb[:, sl, 0:n], op0=ALU.mult, op1=ALU.mult),
                        R=[bxb[bi], bmod, brs[sl]], W=[bxb[bi]])
            s.dma("sp", xo[:, :, t0:t0 + n], x_sb[:, :, t0:t0 + n], R=[bxb[bi]])
        s.finish(bxb)
    return nc


def run_p3(nc3, l, xl, xc, yna, ydf, hf, hb, fmf, mods_l, inp, g_final):
    wge = np.concatenate([inp["router_w_group"][l], inp["router_w_expert"][l]], axis=1)
    wge = np.ascontiguousarray(wge.reshape(8, 128, 36).transpose(1, 0, 2))
    bge = np.concatenate([inp["router_b_group"][l], inp["router_b_expert"][l]])
    bge = np.ascontiguousarray(np.tile(bge[None, :], (128, 1))).astype(np.float32)
    selc = np.zeros((32, NEXP, 128), np.float32)
    for e in range(NEXP):
        selc[e, e, :] = 1.0
    selc = selc.reshape(32, NEXP * 128).astype(ml_dtypes.bfloat16)
    ident = np.eye(128, dtype=np.float32)
    in_maps = []
    for i in range(NCORES):
        b, j = i // 4, i % 4
        lat = slice(2048 * j, 2048 * (j + 1))
        ctxs = slice(S + 64 * j, S + 64 * (j + 1))
        xx = np.concatenate([xl[b, lat], xc[b, 64 * j:64 * (j + 1)]], axis=0)
        na = np.concatenate([yna[b, lat], yna[b, ctxs]], axis=0)
        df = np.concatenate([ydf[b, lat], ydf[b, ctxs]], axis=0)
        nadf = np.concatenate([na, df], axis=1)
        nadf = np.ascontiguousarray(nadf.T.reshape(4, 128, NT1).transpose(1, 0, 2))
        def tk(a):
            aa = np.concatenate([a[:, lat], a[:, ctxs]], axis=1)
            return aa.reshape(4, 128, NT1).transpose(1, 0, 2)
        hgt = np.ascontiguousarray(np.concatenate([tk(hf[b]), tk(hb[b]), tk(fmf[b, 512:1024])], axis=1))
        m = mods_l
        rows = [inp["g_ffn"][l], m[b, 2048:3072], m[b, 4096:5120], m[b, 3072:4096], m[b, 5120:6144],
                m[2, 2048:3072], m[2, 4096:5120], m[2, 3072:4096], m[2, 5120:6144], g_final]
        mod = np.ascontiguousarray(np.stack([vec_pk(r) for r in rows], axis=1)).astype(np.float32)
        in_maps.append({"xT": chunkT(xx), "nadf": nadf, "hg": hgt, "wout": inp["w_out"][l], "mod": mod,
                        "wge": wge, "bge": bge, "selc": selc, "ident": ident,
                        "w1": inp["moe_w1"][l], "w3": inp["moe_w3"][l], "w2": inp["moe_w2"][l]})
    res = run_bass_kernel_spmd(nc3, in_maps, core_ids=list(range(NCORES)))
    xl2 = np.zeros_like(xl); xc2 = np.zeros_like(xc)
    for i in range(NCORES):
        b, j = i // 4, i % 4
        o = res.results[i]["xo"].transpose(1, 0, 2).reshape(D, NT1).T
        xl2[b, 2048 * j:2048 * (j + 1)] = o[:2048]
        xc2[b, 64 * j:64 * (j + 1)] = o[2048:]
    return xl2, xc2


def build_p3e(caps):
    NE = 4
    captot = sum(caps)
    nc = bass.Bass("TRN2", target_bir_lowering=False)
    din = lambda n, shp, dt: nc.dram_tensor(n, shp, dt, kind="ExternalInput").ap()
    xs = din("xs", [128, 8, captot], BF16)
    w1 = din("w1", [NE, D, 512], F32)
    w3 = din("w3", [NE, D, 512], F32)
    w2 = din("w2", [NE, 512, D], F32)
    yo = nc.dram_tensor("yo", [128, 8, captot], F32, kind="ExternalOutput").ap()
    s = Sched(nc)
    with (nc.psum_tensor("ps", [128, 8, 512], F32) as ps,
          nc.sbuf_tensor("x_sb", [128, 2, 8, 512], BF16) as x_sb,
          nc.sbuf_tensor("y_sb", [128, 2, 8, 512], F32) as y_sb,
          nc.sbuf_tensor("s3_st", [128, 6, 2048], F32) as st_sb,
          nc.sbuf_tensor("s3_wb", [128, 2, 6, 2048], BF16) as wb_sb,
          nc.sbuf_tensor("s3_sg", [128, 2, 512], F32) as sg_sb,
          nc.sbuf_tensor("s3_g", [128, 2, 4, 512], BF16) as g_sb):
        bps = [s.buf(f"ps{i}") for i in range(8)]
        bx = [s.buf(f"x{i}", s.dsem(f"x{i}")) for i in range(2)]
        by = [s.buf(f"y{i}", s.dsem(f"y{i}")) for i in range(2)]
        bst = [s.buf(f"st{i}", s.dsem(f"st{i}")) for i in range(6)]
        bwb = [[s.buf(f"wb{a}_{p}") for p in range(6)] for a in range(2)]
        bsg = [s.buf(f"sg{i}") for i in range(2)]
        bg = [s.buf(f"g{i}") for i in range(2)]
        w1v = w1.rearrange("e (kc p) f -> e p kc f", p=128)
        w3v = w3.rearrange("e (kc p) f -> e p kc f", p=128)
        w2v = w2.rearrange("e (fc p) d -> e p fc d", p=128)

        def piece_src(e, p):
            if p < 2:
                return w1v[e, :, 4 * p:4 * p + 4, :]
            if p < 4:
                return w3v[e, :, 4 * (p - 2):4 * (p - 2) + 4, :]
            return w2v[e, :, 2 * (p - 4):2 * (p - 4) + 2, :]

        def piece_dma(P):
            e, p = divmod(P, 6)
            if e >= NE:
                return
            sl = P % 6
            dst = st_sb[:, sl, :]
            dst = dst.rearrange("q (a b) -> q a b", a=4) if p < 4 else dst.rearrange("q (a b) -> q a b", a=2)
            s.dma("sp", dst, piece_src(e, p), W=[bst[sl]])

        def piece_cast(P):
            e, p = divmod(P, 6)
            if e >= NE:
                return
            sl = P % 6
            s.op("pool", lambda en: en.tensor_copy(out=wb_sb[:, e % 2, p, :], in_=st_sb[:, sl, :]),
                 R=[bst[sl]], W=[bwb[e % 2][p]])

        for P in range(6):
            piece_dma(P)
        for P in range(6):
            piece_cast(P)
            piece_dma(P + 6)
        gi = 0
        seg0 = 0
        for ex in range(NE):
            a = ex % 2
            pend = [6 * (ex + 1) + i for i in range(6)]
            blocks = [(seg0 + t, min(512, caps[ex] - t)) for t in range(0, caps[ex], 512)]
            seg0 += caps[ex]
            nb = len(blocks)
            npc = (6 + nb - 1) // nb
            for bi, (t0, n) in enumerate(blocks):
                for P in pend[bi * npc:(bi + 1) * npc]:
                    piece_cast(P)
                    piece_dma(P + 6)
                xs_ = gi % 2
                gs = gi % 2
                gi += 1
                s.dma("act", x_sb[:, xs_, :, 0:n], xs[:, :, t0:t0 + n], W=[bx[xs_]])
                for fc in range(4):
                    pr = fc % 2
                    for which in range(2):
                        bank = 2 * pr + which
                        for k in range(8):
                            wv = wb_sb[:, a, 2 * which + k // 4, :].rearrange("q (a b) -> q a b", a=4)
                            mm(s, ps[:, bank, 0:n], wv[:, k % 4, fc * 128:(fc + 1) * 128], x_sb[:, xs_, k, 0:n],
                               k == 0, k == 7, R=[bwb[a][2 * which + k // 4], bx[xs_]], W=[bps[bank]])
                    s.op("act", lambda e: e.activation(out=sg_sb[:, pr, 0:n], in_=ps[:, 2 * pr, 0:n], func=AF.Silu),
                         R=[bps[2 * pr]], W=[bsg[pr]])
                    s.op("dve", lambda e: e.tensor_tensor(out=g_sb[:, gs, fc, 0:n], in0=ps[:, 2 * pr + 1, 0:n],
                                                          in1=sg_sb[:, pr, 0:n], op=ALU.mult),
                         R=[bps[2 * pr + 1], bsg[pr]], W=[bg[gs]])
                for dc in range(8):
                    bank = 4 + dc % 4
                    for fc in range(4):
                        wv = wb_sb[:, a, 4 + fc // 2, :].rearrange("q (a b) -> q a b", a=2)
                        mm(s, ps[:, bank, 0:n], wv[:, fc % 2, dc * 128:(dc + 1) * 128], g_sb[:, gs, fc, 0:n],
                           fc == 0, fc == 3, R=[bwb[a][4 + fc // 2], bg[gs]], W=[bps[bank]])
                    if dc % 2 == 0:
                        s.op("dve", lambda e: e.tensor_copy(out=y_sb[:, xs_, dc, 0:n], in_=ps[:, bank, 0:n]),
                             R=[bps[bank]], W=[by[xs_]])
                    else:
                        s.op("act", lambda e: e.copy(out=y_sb[:, xs_, dc, 0:n], in_=ps[:, bank, 0:n]),
                             R=[bps[bank]], W=[by[xs_]])
                s.dma("sp", yo[:, :, t0:t0 + n], y_sb[:, xs_, :, 0:n], R=[by[xs_]])
        s.finish(by)
    return nc


def build_pc2(final):
    nc = bass.Bass("TRN2", target_bir_lowering=False)
    din = lambda n, shp, dt: nc.dram_tensor(n, shp, dt, kind="ExternalInput").ap()
    xT = din("xT", [128, 8, NT1], F32)
    yA = din("yA", [128, 8, NT1], F32)
    yB = din("yB", [128, 8, NT1], F32)
    wab = din("wab", [128, 2, NT1], F32)
    mod = din("mod", [128, 3, 8], F32)
    xo = nc.dram_tensor("xo", [128, 8, NT1], F32, kind="ExternalOutput").ap()
    s = Sched(nc)
    with (nc.psum_tensor("ps", [128, 2, 512], F32) as ps,
          nc.sbuf_tensor("x_sb", [128, 8, NT1], F32) as x_sb,
          nc.sbuf_tensor("ya_sb", [128, 2, 8, 512], F32) as ya_sb,
          nc.sbuf_tensor("yb_sb", [128, 2, 8, 512], F32) as yb_sb,
          nc.sbuf_tensor("wab_sb", [128, 2, NT1], F32) as wab_sb,
          nc.sbuf_tensor("mod_sb", [128, 3, 8], F32) as mod_sb,
          nc.sbuf_tensor("ones_sb", [128, 128], BF16) as ones_sb,
          nc.sbuf_tensor("sq_sb", [128, 8, 512], BF16) as sq_sb,
          nc.sbuf_tensor("rstd_sb", [128, 2, 512], F32) as rstd_sb):
        bxb = [s.buf(f"x{i}", s.dsem(f"x{i}")) for i in range(len(BLKS1))]
        bya = [s.buf(f"ya{i}", s.dsem(f"ya{i}")) for i in range(2)]
        byb = [s.buf(f"yb{i}", s.dsem(f"yb{i}")) for i in range(2)]
        bmod = s.buf("mod", s.dsem("mod"))
        bwab = s.buf("wab", bmod.dsem)
        bones = s.buf("ones"); bsq = s.buf("sq"); brs = [s.buf(f"rs{i}") for i in range(2)]
        bps = [s.buf(f"ps{i}") for i in range(2)]
        s.dma("sp", mod_sb[:], mod, W=[bmod])
        s.dma("sp", wab_sb[:], wab, W=[bwab])
        s.op("pool", lambda e: e.memset(ones_sb[:], 1.0), W=[bones])
        for bi, (t0, n) in enumerate(BLKS1):
            s.dma("sp", x_sb[:, :, t0:t0 + n], xT[:, :, t0:t0 + n], W=[bxb[bi]])
        for bi, (t0, n) in enumerate(BLKS1):
            garow = 1 if bi == 4 else 0
            sl = bi % 2
            s.dma("act", ya_sb[:, sl, :, 0:n], yA[:, :, t0:t0 + n], W=[bya[sl]])
            s.dma("act", yb_sb[:, sl, :, 0:n], yB[:, :, t0:t0 + n], W=[byb[sl]])
            for k in range(8):
                s.op("pool", lambda e: e.tensor_tensor(out=ya_sb[:, sl, k, 0:n], in0=ya_sb[:, sl, k, 0:n],
                                                       in1=wab_sb[:, 0, t0:t0 + n], op=ALU.mult),
                     R=[bya[sl], bwab], W=[bya[sl]])
                s.op("dve", lambda e: e.tensor_tensor(out=yb_sb[:, sl, k, 0:n], in0=yb_sb[:, sl, k, 0:n],
                                                      in1=wab_sb[:, 1, t0:t0 + n], op=ALU.mult),
                     R=[byb[sl], bwab], W=[byb[sl]])
                s.op("dve", lambda e: e.tensor_tensor(out=ya_sb[:, sl, k, 0:n], in0=ya_sb[:, sl, k, 0:n],
                                                      in1=yb_sb[:, sl, k, 0:n], op=ALU.add),
                     R=[bya[sl], byb[sl]], W=[bya[sl]])
                s.op("dve", lambda e: e.scalar_tensor_tensor(
                    out=x_sb[:, k, t0:t0 + n], in0=ya_sb[:, sl, k, 0:n], scalar=mod_sb[:, garow, k:k + 1],
                    in1=x_sb[:, k, t0:t0 + n], op0=ALU.mult, op1=ALU.add),
                    R=[bya[sl], bmod, bxb[bi]], W=[bxb[bi]])
            if final:
                s.op("act", lambda e: e.activation(out=sq_sb[:, :, 0:n], in_=x_sb[:, :, t0:t0 + n], func=AF.Square),
                     R=[bxb[bi]], W=[bsq])
                for k in range(8):
                    mm(s, ps[:, sl, 0:n], ones_sb[:], sq_sb[:, k, 0:n], k == 0, k == 7, R=[bones, bsq], W=[bps[sl]])
                s.op("act", lambda e: e.activation(out=rstd_sb[:, sl, 0:n], in_=ps[:, sl, 0:n], func=AF.Sqrt,
                                                   scale=1.0 / D, bias=EPS), R=[bps[sl]], W=[brs[sl]])
                s.op("dve", lambda e: e.reciprocal(out=rstd_sb[:, sl, 0:n], in_=rstd_sb[:, sl, 0:n]),
                     R=[brs[sl]], W=[brs[sl]])
                for k in range(8):
                    s.op("dve", lambda e: e.scalar_tensor_tensor(
                        out=x_sb[:, k, t0:t0 + n], in0=x_sb[:, k, t0:t0 + n], scalar=mod_sb[:, 2, k:k + 1],
                        in1=rstd_sb[:, sl, 0:n], op0=ALU.mult, op1=ALU.mult),
                        R=[bxb[bi], bmod, brs[sl]], W=[bxb[bi]])
            s.dma("sp", xo[:, :, t0:t0 + n], x_sb[:, :, t0:t0 + n], R=[bxb[bi]])
        s.finish(bxb)
    return nc


def run_p3_expert(l, xl, xc, yna, ydf, hf, hb, fmf, mods_l, inp, g_final, final):
    in_maps = p3_inmaps_common(l, xl, xc, yna, ydf, hf, hb, fmf, mods_l, inp, g_final)
    resa = run_bass_kernel_spmd(build_p3a(), in_maps, core_ids=list(range(NCORES))).results
    NTT = NCORES * NT1
    HL2 = np.zeros((D, NTT), ml_dtypes.bfloat16)
    WD = np.zeros((NTT, 32), np.float32)
    for i in range(NCORES):
        HL2[:, i * NT1:(i + 1) * NT1] = resa[i]["hl2o"].transpose(1, 0, 2).reshape(D, NT1)
        WD[i * NT1:(i + 1) * NT1] = resa[i]["wd32o"].transpose(1, 0, 2).reshape(17 * 128, 32)[:NT1]
    top2 = np.sort(np.argpartition(-WD, 1, axis=1)[:, :2], axis=1)
    eA, eB = top2[:, 0], top2[:, 1]
    ar = np.arange(NTT)
    wA = WD[ar, eA]; wB = WD[ar, eB]
    tokE = []
    for e in range(32):
        tokE.append(np.nonzero((eA == e) | (eB == e))[0])
    order = np.argsort(-np.array([len(t) for t in tokE]), kind="stable")
    assign = [[int(order[8 * sl + c]) for sl in range(4)] for c in range(NCORES)]
    caps = []
    for sl in range(4):
        mx = max(len(tokE[assign[c][sl]]) for c in range(NCORES))
        caps.append(max(128, int(-(-mx // 128) * 128)))
    captot = sum(caps)
    mapse = []
    for c in range(NCORES):
        xsa = np.zeros((D, captot), ml_dtypes.bfloat16)
        o = 0
        for sl in range(4):
            tk_ = tokE[assign[c][sl]]
            xsa[:, o:o + len(tk_)] = HL2[:, tk_]
            o += caps[sl]
        mapse.append({"xs": np.ascontiguousarray(xsa.reshape(8, 128, captot).transpose(1, 0, 2)),
                      "w1": np.ascontiguousarray(inp["moe_w1"][l][assign[c]]),
                      "w3": np.ascontiguousarray(inp["moe_w3"][l][assign[c]]),
                      "w2": np.ascontiguousarray(inp["moe_w2"][l][assign[c]])})
    rese = run_bass_kernel_spmd(build_p3e(caps), mapse, core_ids=list(range(NCORES))).results
    YA = np.zeros((D, NTT), np.float32)
    YB = np.zeros((D, NTT), np.float32)
    for c in range(NCORES):
        yy = rese[c]["yo"].transpose(1, 0, 2).reshape(D, captot)
        o = 0
        for sl in range(4):
            e = assign[c][sl]
            tk_ = tokE[e]
            cols = yy[:, o:o + len(tk_)]
            isA = eA[tk_] == e
            YA[:, tk_[isA]] = cols[:, isA]
            YB[:, tk_[~isA]] = cols[:, ~isA]
            o += caps[sl]
    mapsc = []
    for i in range(NCORES):
        b = i // 4
        sl_ = slice(i * NT1, (i + 1) * NT1)
        modc = np.stack([vec_pk(mods_l[b, 5120:6144]), vec_pk(mods_l[2, 5120:6144]), vec_pk(g_final)], axis=1)
        wab = np.stack([np.tile(wA[sl_][None, :], (128, 1)), np.tile(wB[sl_][None, :], (128, 1))], axis=1)
        mapsc.append({"xT": resa[i]["xo"], "mod": np.ascontiguousarray(modc).astype(np.float32),
                      "wab": np.ascontiguousarray(wab).astype(np.float32),
                      "yA": np.ascontiguousarray(YA[:, sl_].reshape(8, 128, NT1).transpose(1, 0, 2)),
                      "yB": np.ascontiguousarray(YB[:, sl_].reshape(8, 128, NT1).transpose(1, 0, 2))})
    resc = run_bass_kernel_spmd(build_pc2(final), mapsc, core_ids=list(range(NCORES))).results
    xl2 = np.zeros_like(xl); xc2 = np.zeros_like(xc)
    for i in range(NCORES):
        b, j = i // 4, i % 4
        o = resc[i]["xo"].transpose(1, 0, 2).reshape(D, NT1).T
        xl2[b, 2048 * j:2048 * (j + 1)] = o[:2048]
        xc2[b, 64 * j:64 * (j + 1)] = o[2048:]
    return xl2, xc2


def p3_inmaps_common(l, xl, xc, yna, ydf, hf, hb, fmf, mods_l, inp, g_final):
    wge = np.concatenate([inp["router_w_group"][l], inp["router_w_expert"][l]], axis=1)
    wge = np.ascontiguousarray(wge.reshape(8, 128, 36).transpose(1, 0, 2))
    bge = np.concatenate([inp["router_b_group"][l], inp["router_b_expert"][l]])
    bge = np.ascontiguousarray(np.tile(bge[None, :], (128, 1))).astype(np.float32)
    ident = np.eye(128, dtype=np.float32)
    iota4 = np.ascontiguousarray(np.tile(np.arange(4, dtype=np.float32)[None, :], (128, 1)))
    in_maps = []
    for i in range(NCORES):
        b, j = i // 4, i % 4
        lat = slice(2048 * j, 2048 * (j + 1))
        ctxs = slice(S + 64 * j, S + 64 * (j + 1))
        xx = np.concatenate([xl[b, lat], xc[b, 64 * j:64 * (j + 1)]], axis=0)
        na = np.concatenate([yna[b, lat], yna[b, ctxs]], axis=0)
        df = np.concatenate([ydf[b, lat], ydf[b, ctxs]], axis=0)
        nadf = np.concatenate([na, df], axis=1)
        nadf = np.ascontiguousarray(nadf.T.reshape(4, 128, NT1).transpose(1, 0, 2))

        def tk(a):
            aa = np.concatenate([a[:, lat], a[:, ctxs]], axis=1)
            return aa.reshape(4, 128, NT1).transpose(1, 0, 2)
        hgt = np.ascontiguousarray(np.concatenate([tk(hf[b]), tk(hb[b]), tk(fmf[b, 512:1024])], axis=1))
        m = mods_l
        rows = [inp["g_ffn"][l], m[b, 2048:3072], m[b, 4096:5120], m[b, 3072:4096], m[b, 5120:6144],
                m[2, 2048:3072], m[2, 4096:5120], m[2, 3072:4096], m[2, 5120:6144], g_final]
        mod = np.ascontiguousarray(np.stack([vec_pk(r) for r in rows], axis=1)).astype(np.float32)
        in_maps.append({"xT": chunkT(xx), "nadf": nadf, "hg": hgt, "wout": inp["w_out"][l], "mod": mod,
                        "wge": wge, "bge": bge, "ident": ident, "iota4": iota4})
    return in_maps


def run_p3_sparse(l, xl, xc, yna, ydf, hf, hb, fmf, mods_l, inp, g_final, final):
    in_maps = p3_inmaps_common(l, xl, xc, yna, ydf, hf, hb, fmf, mods_l, inp, g_final)
    resa = run_bass_kernel_spmd(build_p3a(), in_maps, core_ids=list(range(NCORES))).results
    NTT = NCORES * NT1
    HL2 = np.zeros((D, NTT), ml_dtypes.bfloat16)
    WDT = np.zeros((32, 2, NTT), ml_dtypes.bfloat16)
    gid = np.zeros(NTT, np.int64)
    for i in range(NCORES):
        HL2[:, i * NT1:(i + 1) * NT1] = resa[i]["hl2o"].transpose(1, 0, 2).reshape(D, NT1)
        WDT[:, :, i * NT1:(i + 1) * NT1] = resa[i]["wdto"]
        g = resa[i]["gido"]
        gid[i * NT1:(i + 1) * NT1] = np.rint(g.T.reshape(-1)[:NT1]).astype(np.int64)
    toks = [np.nonzero(gid == g)[0] for g in range(4)]
    ncg = [1, 1, 1, 1]
    for _ in range(NCORES - 4):
        gbig = max(range(4), key=lambda g: len(toks[g]) / ncg[g])
        ncg[gbig] += 1
    idxs = []
    cgroup = []
    for g in range(4):
        parts = np.array_split(toks[g], ncg[g])
        for pp in parts:
            idxs.append(pp); cgroup.append(g)
    ntb = max(512, int(-(-max(len(ix) for ix in idxs) // 512) * 512))
    sel8 = np.zeros((8, 8, 128), np.float32)
    for e in range(8):
        sel8[e, e, :] = 1.0
    sel8 = sel8.reshape(8, 1024).astype(ml_dtypes.bfloat16)
    mapsb = []
    for c in range(NCORES):
        g = cgroup[c]
        ix = idxs[c]
        h2 = np.zeros((D, ntb), ml_dtypes.bfloat16)
        h2[:, :len(ix)] = HL2[:, ix]
        wd = np.zeros((8, 2, ntb), ml_dtypes.bfloat16)
        wd[:, :, :len(ix)] = WDT[8 * g:8 * g + 8][:, :, ix]
        mapsb.append({"hl2": np.ascontiguousarray(h2.reshape(8, 128, ntb).transpose(1, 0, 2)), "wdt": wd, "selc": sel8,
                      "w1": np.ascontiguousarray(inp["moe_w1"][l][8 * g:8 * g + 8]),
                      "w3": np.ascontiguousarray(inp["moe_w3"][l][8 * g:8 * g + 8]),
                      "w2": np.ascontiguousarray(inp["moe_w2"][l][8 * g:8 * g + 8])})
    resb = run_bass_kernel_spmd(build_p3b(ntb), mapsb, core_ids=list(range(NCORES))).results
    Y = np.zeros((D, NTT), np.float32)
    for c in range(NCORES):
        ix = idxs[c]
        Y[:, ix] = resb[c]["yo"].transpose(1, 0, 2).reshape(D, ntb)[:, :len(ix)]
    mapsc = []
    for i in range(NCORES):
        b = i // 4
        modc = np.stack([vec_pk(mods_l[b, 5120:6144]), vec_pk(mods_l[2, 5120:6144]), vec_pk(g_final)], axis=1)
        mapsc.append({"xT": resa[i]["xo"], "mod": np.ascontiguousarray(modc).astype(np.float32),
                      "yT": np.ascontiguousarray(Y[:, i * NT1:(i + 1) * NT1].reshape(8, 128, NT1).transpose(1, 0, 2))})
    resc = run_bass_kernel_spmd(build_pc(final), mapsc, core_ids=list(range(NCORES))).results
    xl2 = np.zeros_like(xl); xc2 = np.zeros_like(xc)
    for i in range(NCORES):
        b, j = i // 4, i % 4
        o = resc[i]["xo"].transpose(1, 0, 2).reshape(D, NT1).T
        xl2[b, 2048 * j:2048 * (j + 1)] = o[:2048]
        xc2[b, 64 * j:64 * (j + 1)] = o[2048:]
    return xl2, xc2


def kernel(**inputs):
    inp = {k: np.asarray(v) for k, v in inputs.items()}
    x = np.ascontiguousarray(inp["x"], dtype=np.float32)
    ctx = np.ascontiguousarray(inp["ctx"], dtype=np.float32)
    mods = run_p0(inp["c"], inp["c_ctx"], inp["w_ada"], inp["b_ada"])
    cosT, sinT = rope_tables()
    xl, xc = x, ctx
    for l in range(DEPTH):
        fmb, fmf, tm = run_p1(build_p1(), xl, xc, mods[l], inp["g_mix"][l], inp["w_in"][l], cosT, sinT)
        lam_init = 0.8 - 0.6 * math.exp(-0.3 * l)
        yna, ydf, hf, hb = run_p2(build_p2(lam_init), l, fmb, fmf, tm, inp)
        xl, xc = run_p3_expert(l, xl, xc, yna, ydf, hf, hb, fmf, mods[l], inp, inp["g_final"], l == DEPTH - 1)
    return np.ascontiguousarray(xl, dtype=np.float32)
```

```python
import math
from contextlib import ExitStack
import numpy as np
import ml_dtypes
import concourse.bass as bass
import concourse.mybir as mybir
from concourse.bass_utils import run_bass_kernel_spmd

F32 = mybir.dt.float32
BF16 = mybir.dt.bfloat16
I32 = mybir.dt.int32
U32 = mybir.dt.uint32
AF = mybir.ActivationFunctionType
ALU = mybir.AluOpType
AX = mybir.AxisListType

NCORES = 8
D = 1024
B = 2
S = 8192
L = 256
DEPTH = 4
GRID_W = 64
EPS = 1e-6


class Buf:
    __slots__ = ("name", "w", "r", "dsem")

    def __init__(self, name, dsem=None):
        self.name = name
        self.w = None
        self.r = []
        self.dsem = dsem


class DmaSem:
    def __init__(self, sched, name):
        self.sem = sched.nc.alloc_semaphore(name)
        self.key = ("dma", name)
        self.total = 0
        sched.sems[self.key] = self


class Sched:
    def __init__(self, nc):
        self.nc = nc
        self.eng = {"pe": nc.tensor, "dve": nc.vector, "act": nc.scalar,
                    "pool": nc.gpsimd, "sp": nc.sync}
        self.sems = {}
        self.esem = {}
        self.cnt = {}
        for k in self.eng:
            self.esem[k] = nc.alloc_semaphore("e_" + k)
            self.cnt[k] = 0
        self.seen = {}
        self.nbuf = 0
        self.out_tokens = []

    def buf(self, name=None, dsem=None):
        self.nbuf += 1
        return Buf(name or f"b{self.nbuf}", dsem)

    def dsem(self, name):
        return DmaSem(self, name)

    def _semof(self, key):
        if key[0] == "dma":
            return self.sems[key].sem
        return self.esem[key[0]]

    def _wait(self, engname, deps):
        e = self.eng[engname]
        for key, val in deps.items():
            if key[0] == "dma":
                val = max(val, 0)
            if self.seen.get((engname, key), 0) >= val:
                continue
            self.seen[(engname, key)] = val
            e.wait_ge(self._semof(key), val)

    def _deps(self, R, W):
        deps = {}

        def add(tok):
            if tok is None:
                return
            key, val = tok
            if key[0] == "dma":
                val = self.sems[key].total
            if deps.get(key, 0) < val:
                deps[key] = val
        for b in R:
            add(b.w)
        for b in W:
            add(b.w)
            for t in b.r:
                add(t)
        return deps

    def _commit(self, tok, R, W):
        for b in R:
            b.r.append(tok)
        for b in W:
            b.w = tok
            b.r = []

    def op(self, engname, fn, R=(), W=()):
        deps = self._deps(R, W)
        if engname == "pe":
            deps.pop(("pe",), None)
        self._wait(engname, deps)
        ins = fn(self.eng[engname])
        self.cnt[engname] += 1
        ins.then_inc(self.esem[engname], 1)
        tok = ((engname,), self.cnt[engname])
        self._commit(tok, R, W)
        return tok

    def dma(self, q, out, in_, R=(), W=(), sem=None, **kw):
        deps = self._deps(R, W)
        self._wait(q, deps)
        ds = sem
        if ds is None:
            for b in list(W) + list(R):
                if b.dsem is not None:
                    ds = b.dsem
                    break
        assert ds is not None, "dma needs a DmaSem"
        ins = self.eng[q].dma_start(out=out, in_=in_, **kw)
        ds.total += 16
        ins.then_inc(ds.sem, 16)
        tok = (ds.key, ds.total)
        self._commit(tok, R, W)
        return tok

    def barrier(self, bufs=()):
        deps = {}
        for k in self.eng:
            if self.cnt[k]:
                deps[(k,)] = self.cnt[k]
        for key, ds in self.sems.items():
            if ds.total:
                deps[key] = ds.total
        for k in self.eng:
            d = {kk: v for kk, v in deps.items() if kk != (k,)}
            self._wait(k, d)

    def coll(self, kind, ins, outs, R=(), W=(), groups=None):
        deps = self._deps(R, W)
        self._wait("pool", deps)
        ds = None
        for b in list(W) + list(R):
            if b.dsem is not None:
                ds = b.dsem
                break
        g = groups or [[0, 1, 2, 3], [4, 5, 6, 7]]
        ins_ = self.nc.gpsimd.collective_compute(kind, ALU.bypass, replica_groups=g, ins=ins, outs=outs)
        ds.total += 16
        ins_.then_inc(ds.sem, 16)
        tok = (ds.key, ds.total)
        self._commit(tok, R, W)
        return tok

    def finish(self, bufs, engname="sp"):
        deps = {}
        for b in bufs:
            for tok in ([b.w] if b.w else []) + b.r:
                key, val = tok
                if key[0] == "dma":
                    val = self.sems[key].total
                deps[key] = max(deps.get(key, 0), val)
        self._wait(engname, deps)


def mm(s, out, lhsT, rhs, start, stop, R, W):
    return s.op("pe", lambda e: e.matmul(out, lhsT, rhs, start=start, stop=stop), R=R, W=W)


def build_p0():
    nc = bass.Bass("TRN2", target_bir_lowering=False)
    NCOL = 3072
    NJ = NCOL // 128
    cT = nc.dram_tensor("cT", [128, 8, 4], F32, kind="ExternalInput").ap()
    w = nc.dram_tensor("w", [D, NCOL], F32, kind="ExternalInput").ap()
    bvec = nc.dram_tensor("bvec", [128, NJ], F32, kind="ExternalInput").ap()
    out = nc.dram_tensor("out", [128, NJ, 4], F32, kind="ExternalOutput").ap()
    s = Sched(nc)
    wv = w.rearrange("(kc p) n -> p kc n", p=128)
    with (nc.sbuf_tensor("w_sb", [128, 8, NCOL], F32) as w_sb,
          nc.sbuf_tensor("c_sb", [128, 8, 4], F32) as c_sb,
          nc.sbuf_tensor("s_sb", [128, 8, 4], F32) as s_sb,
          nc.sbuf_tensor("b_sb", [128, NJ], F32) as b_sb,
          nc.sbuf_tensor("r_sb", [128, NJ, 4], F32) as r_sb,
          nc.psum_tensor("ps", [128, 8, 512], F32) as ps):
        bw = [s.buf(f"w{k}", s.dsem(f"w{k}")) for k in range(8)]
        bc = s.buf("c", s.dsem("c"))
        bb = s.buf("b", bc.dsem)
        bs = s.buf("s")
        br = s.buf("r", s.dsem("r"))
        bps = [s.buf(f"ps{i}") for i in range(8)]
        s.dma("sp", c_sb[:], cT, W=[bc])
        s.dma("sp", b_sb[:], bvec, W=[bb])
        for k in range(8):
            s.dma("sp" if k % 2 == 0 else "act", w_sb[:, k, :], wv[:, k, :], W=[bw[k]])
        s.op("act", lambda e: e.activation(out=s_sb[:], in_=c_sb[:], func=AF.Silu), R=[bc], W=[bs])
        for j in range(NJ):
            pb = bps[j % 8]
            for k in range(8):
                mm(s, ps[:, j % 8, 0:4], w_sb[:, k, j * 128:(j + 1) * 128], s_sb[:, k, :],
                   k == 0, k == 7, R=[bw[k], bs], W=[pb])
            s.op("dve", lambda e: e.tensor_scalar(out=r_sb[:, j, :], in0=ps[:, j % 8, 0:4],
                                                  scalar1=b_sb[:, j:j + 1], scalar2=None, op0=ALU.add),
                 R=[pb, bb], W=[br])
        s.dma("sp", out, r_sb[:], R=[br])
        s.finish([br])
    return nc


def silu_np_layout_c(c, c_ctx):
    cc = np.stack([c[0], c[1], c_ctx, c_ctx], axis=1)
    return np.ascontiguousarray(cc.reshape(8, 128, 4).transpose(1, 0, 2))


def run_p0(c, c_ctx, w_ada, b_ada):
    nc = build_p0()
    cT = silu_np_layout_c(c, c_ctx)
    in_maps = []
    for i in range(NCORES):
        l, h = i // 2, i % 2
        in_maps.append({
            "cT": cT,
            "w": np.ascontiguousarray(w_ada[l][:, h * 3072:(h + 1) * 3072]),
            "bvec": np.ascontiguousarray(b_ada[l][h * 3072:(h + 1) * 3072].reshape(24, 128).T),
        })
    res = run_bass_kernel_spmd(nc, in_maps, core_ids=list(range(NCORES)))
    mods = np.zeros((DEPTH, 3, 6 * D), np.float32)
    for i in range(NCORES):
        l, h = i // 2, i % 2
        o = res.results[i]["out"]
        m = o.transpose(1, 0, 2).reshape(3072, 4)
        mods[l, :, h * 3072:(h + 1) * 3072] = m[:, :3].T
    return mods


NT1 = 2112
NW1 = 3072
BLKS1 = [(0, 512), (512, 512), (1024, 512), (1536, 512), (2048, 64)]


def build_p1():
    nc = bass.Bass("TRN2", target_bir_lowering=False)
    xT = nc.dram_tensor("xT", [128, 8, NT1], F32, kind="ExternalInput").ap()
    w = nc.dram_tensor("w", [D, NW1], F32, kind="ExternalInput").ap()
    gsc = nc.dram_tensor("gsc", [128, 5, 8], F32, kind="ExternalInput").ap()
    cosT = nc.dram_tensor("cosT", [128, 2048], F32, kind="ExternalInput").ap()
    sinT = nc.dram_tensor("sinT", [128, 2048], F32, kind="ExternalInput").ap()
    fmb = nc.dram_tensor("fmb", [128, 8, NT1], BF16, kind="ExternalOutput").ap()
    fmf = nc.dram_tensor("fmf", [128, 8, NT1], F32, kind="ExternalOutput").ap()
    tm = nc.dram_tensor("tm", [NT1, 512], BF16, kind="ExternalOutput").ap()
    s = Sched(nc)
    wv = w.rearrange("(kc p) n -> p kc n", p=128)
    with (nc.sbuf_tensor("x_sb", [128, 2, 8, 512], F32) as x_sb,
          nc.sbuf_tensor("h_sb", [128, 8, NT1], BF16) as h_sb,
          nc.sbuf_tensor("w_bf", [128, 8, NW1], BF16) as w_bf,
          nc.sbuf_tensor("w_st", [128, 2, NW1], F32) as w_st,
          nc.sbuf_tensor("o_sb", [128, 4, 512], F32) as o_sb,
          nc.sbuf_tensor("ob_sb", [128, 4, 512], BF16) as ob_sb,
          nc.sbuf_tensor("cos_sb", [128, 2048], F32) as cos_sb,
          nc.sbuf_tensor("sin_sb", [128, 2048], F32) as sin_sb,
          nc.sbuf_tensor("gsc_sb", [128, 5, 8], F32) as gsc_sb,
          nc.sbuf_tensor("ab_sb", [128, 2, 8], F32) as ab_sb,
          nc.sbuf_tensor("sq_sb", [128, 8, 512], BF16) as sq_sb,
          nc.sbuf_tensor("ones_sb", [128, 128], BF16) as ones_sb,
          nc.sbuf_tensor("rstd_sb", [128, 2, 512], F32) as rstd_sb,
          nc.sbuf_tensor("tmp_sb", [128, 4, 512], F32) as tmp_sb,
          nc.psum_tensor("ps", [128, 8, 512], F32) as ps):
        bx = [s.buf(f"x{i}", s.dsem(f"x{i}")) for i in range(2)]
        bh = [s.buf(f"h{i}") for i in range(len(BLKS1))]
        bwst = [s.buf(f"wst{i}", s.dsem(f"wst{i}")) for i in range(2)]
        bwbf = [s.buf(f"wbf{k}") for k in range(8)]
        bo = [s.buf(f"o{i}", s.dsem(f"o{i}")) for i in range(4)]
        bob = [s.buf(f"ob{i}", s.dsem(f"ob{i}")) for i in range(4)]
        cs = s.dsem("const")
        bcos = s.buf("cos", cs); bsin = s.buf("sin", cs); bgsc = s.buf("gsc", cs)
        bab = s.buf("ab"); bsq = s.buf("sq"); bones = s.buf("ones")
        brs = [s.buf(f"rs{i}") for i in range(2)]
        btmp = [s.buf(f"tmp{i}") for i in range(4)]
        bps = [s.buf(f"ps{i}") for i in range(8)]

        s.dma("sp", gsc_sb[:], gsc, W=[bgsc])
        s.dma("sp", cos_sb[:], cosT, W=[bcos])
        s.dma("sp", sin_sb[:], sinT, W=[bsin])
        s.op("pool", lambda e: e.memset(ones_sb[:], 1.0), W=[bones])
        for t in range(2):
            s.op("dve", lambda e: e.scalar_tensor_tensor(
                out=ab_sb[:, t, :], in0=gsc_sb[:, 1 + 2 * t, :], scalar=1.0, in1=gsc_sb[:, 0, :],
                op0=ALU.add, op1=ALU.mult), R=[bgsc], W=[bab])
        for k in range(8):
            sl = k % 2
            s.dma("act", w_st[:, sl, :], wv[:, k, :], W=[bwst[sl]])
            s.op("pool", lambda e: e.tensor_copy(out=w_bf[:, k, :], in_=w_st[:, sl, :]),
                 R=[bwst[sl]], W=[bwbf[k]])
        for bi, (t0, n) in enumerate(BLKS1):
            sl = bi % 2
            isctx = bi == 4
            s.dma("sp", x_sb[:, sl, :, 0:n], xT[:, :, t0:t0 + n], W=[bx[sl]])
            s.op("act", lambda e: e.activation(out=sq_sb[:, :, 0:n], in_=x_sb[:, sl, :, 0:n], func=AF.Square),
                 R=[bx[sl]], W=[bsq])
            pst = bps[sl]
            for k in range(8):
                mm(s, ps[:, sl, 0:n], ones_sb[:], sq_sb[:, k, 0:n], k == 0, k == 7, R=[bones, bsq], W=[pst])
            s.op("act", lambda e: e.activation(out=rstd_sb[:, sl, 0:n], in_=ps[:, sl, 0:n], func=AF.Sqrt,
                                               scale=1.0 / D, bias=EPS), R=[pst], W=[brs[sl]])
            s.op("dve", lambda e: e.reciprocal(out=rstd_sb[:, sl, 0:n], in_=rstd_sb[:, sl, 0:n]),
                 R=[brs[sl]], W=[brs[sl]])
            ai = 1 if isctx else 0
            shrow = 4 if isctx else 2
            for k in range(8):
                tb = k % 4
                s.op("dve", lambda e: e.scalar_tensor_tensor(
                    out=tmp_sb[:, tb, 0:n], in0=x_sb[:, sl, k, 0:n], scalar=ab_sb[:, ai, k:k + 1],
                    in1=rstd_sb[:, sl, 0:n], op0=ALU.mult, op1=ALU.mult),
                    R=[bx[sl], bab, brs[sl]], W=[btmp[tb]])
                s.op("act", lambda e: e.activation(out=h_sb[:, k, t0:t0 + n], in_=tmp_sb[:, tb, 0:n],
                                                   func=AF.Identity, bias=gsc_sb[:, shrow, k:k + 1], scale=1.0),
                     R=[btmp[tb], bgsc], W=[bh[bi]])
        oi = 0
        obi = 0
        pi = 0
        for bi, (t0, n) in enumerate(BLKS1):
            isctx = bi == 4
            for c in range(16):
                rope = (c >= 12) and not isctx
                isb = c < 4 or c >= 12
                dst = (fmb[:, c if c < 4 else c - 8, t0:t0 + n]) if isb else fmf[:, c - 4, t0:t0 + n]
                pA = 2 + (pi % 6); pi += 1
                for k in range(8):
                    mm(s, ps[:, pA, 0:n], w_bf[:, k, c * 128:(c + 1) * 128], h_sb[:, k, t0:t0 + n],
                       k == 0, k == 7, R=[bwbf[k], bh[bi]], W=[bps[pA]])
                if isb:
                    ob = obi % 4; obi += 1
                    osl = ob_sb[:, ob, 0:n]; obuf = bob[ob]
                else:
                    ob = oi % 4; oi += 1
                    osl = o_sb[:, ob, 0:n]; obuf = bo[ob]
                if not rope:
                    if c % 2 == 0:
                        s.op("act", lambda e: e.copy(out=osl, in_=ps[:, pA, 0:n]), R=[bps[pA]], W=[obuf])
                    else:
                        s.op("dve", lambda e: e.tensor_copy(out=osl, in_=ps[:, pA, 0:n]), R=[bps[pA]], W=[obuf])
                else:
                    pB = 2 + (pi % 6); pi += 1
                    c2 = c + 4
                    for k in range(8):
                        mm(s, ps[:, pB, 0:n], w_bf[:, k, c2 * 128:(c2 + 1) * 128], h_sb[:, k, t0:t0 + n],
                           k == 0, k == 7, R=[bwbf[k], bh[bi]], W=[bps[pB]])
                    s.op("dve", lambda e: e.tensor_tensor(out=tmp_sb[:, 0, 0:n], in0=ps[:, pA, 0:n],
                                                          in1=cos_sb[:, t0:t0 + n], op=ALU.mult),
                         R=[bps[pA], bcos], W=[btmp[0]])
                    s.op("dve", lambda e: e.tensor_tensor(out=tmp_sb[:, 1, 0:n], in0=ps[:, pB, 0:n],
                                                          in1=sin_sb[:, t0:t0 + n], op=ALU.mult),
                         R=[bps[pB], bsin], W=[btmp[1]])
                    s.op("pool", lambda e: e.tensor_tensor(out=osl, in0=tmp_sb[:, 0, 0:n],
                                                           in1=tmp_sb[:, 1, 0:n], op=ALU.add),
                         R=[btmp[0], btmp[1]], W=[obuf])
                s.dma("sp", dst, osl, R=[obuf])
        ntile = NT1 // 128 + 1
        for ti in range(ntile):
            t0 = ti * 128
            n = min(128, NT1 - t0)
            bi = min(t0 // 512, 4)
            pA = 2 + (pi % 6); pi += 1
            for k in range(8):
                mm(s, ps[0:n, pA, :], h_sb[:, k, t0:t0 + n], w_bf[:, k, 2560:3072],
                   k == 0, k == 7, R=[bwbf[k], bh[bi]], W=[bps[pA]])
            ob = obi % 4; obi += 1
            s.op("act", lambda e: e.copy(out=ob_sb[0:n, ob, :], in_=ps[0:n, pA, :]), R=[bps[pA]], W=[bob[ob]])
            s.dma("sp", tm[t0:t0 + n, :], ob_sb[0:n, ob, :], R=[bob[ob]])
        s.finish(bo + bob)
    return nc


def rope_tables():
    t = np.arange(S)
    row = (t // GRID_W).astype(np.float32)
    col = (t % GRID_W).astype(np.float32)
    inv = (10000.0 ** (-np.arange(0, 16, 2, dtype=np.float32) / 16.0)).astype(np.float32)
    ang_r = row[:, None] * inv
    ang_c = col[:, None] * inv
    cosT = np.zeros((32, S), np.float32)
    sinT = np.zeros((32, S), np.float32)
    for d in range(32):
        ang = ang_r if d < 16 else ang_c
        i = d % 8
        cosT[d] = np.cos(ang[:, i])
        sgn = -1.0 if (d % 16) < 8 else 1.0
        sinT[d] = sgn * np.sin(ang[:, i])
    return np.tile(cosT, (4, 1)), np.tile(sinT, (4, 1))


def p1_wcols():
    sw = np.array([(d + 8) if (d % 16) < 8 else (d - 8) for d in range(32)])
    f = np.arange(256)
    swf = (f // 32) * 32 + sw[f % 32]
    cols = np.concatenate([np.arange(0, 256), np.arange(256, 512), np.arange(768, 1280), np.arange(1280, 1792),
                           np.arange(1792, 2048), np.arange(2048, 2304), 1792 + swf, 2048 + swf,
                           np.arange(512, 768), np.arange(2304, 2560)])
    return cols


def chunkT(a):
    T = a.shape[0]
    return np.ascontiguousarray(a.T.reshape(8, 128, T).transpose(1, 0, 2))


def vec_pk(v):
    return np.ascontiguousarray(v.reshape(8, 128).T)


def run_p1(nc1, xl, xc, mods_l, g_mix_l, w_in_l, cosT, sinT):
    wl = np.ascontiguousarray(w_in_l[:, p1_wcols()])
    in_maps = []
    for i in range(NCORES):
        b, j = i // 4, i % 4
        xx = np.concatenate([xl[b, 2048 * j:2048 * (j + 1)], xc[b, 64 * j:64 * (j + 1)]], axis=0)
        gsc = np.stack([vec_pk(g_mix_l), vec_pk(mods_l[b, 1024:2048]), vec_pk(mods_l[b, 0:1024]),
                        vec_pk(mods_l[2, 1024:2048]), vec_pk(mods_l[2, 0:1024])], axis=1)
        in_maps.append({"xT": chunkT(xx), "w": wl, "gsc": np.ascontiguousarray(gsc),
                        "cosT": np.ascontiguousarray(cosT[:, 2048 * j:2048 * (j + 1)]),
                        "sinT": np.ascontiguousarray(sinT[:, 2048 * j:2048 * (j + 1)])})
    res = run_bass_kernel_spmd(nc1, in_maps, core_ids=list(range(NCORES)))
    fmb = np.zeros((B, 1024, S + L), ml_dtypes.bfloat16)
    fmf = np.zeros((B, 1024, S + L), np.float32)
    tm = np.zeros((B, S + L, 512), ml_dtypes.bfloat16)
    for i in range(NCORES):
        b, j = i // 4, i % 4
        r = res.results[i]
        for dst, key in ((fmb, "fmb"), (fmf, "fmf")):
            f = r[key].transpose(1, 0, 2).reshape(1024, NT1)
            dst[b, :, 2048 * j:2048 * (j + 1)] = f[:, :2048]
            dst[b, :, S + 64 * j:S + 64 * (j + 1)] = f[:, 2048:]
        t = r["tm"]
        tm[b, 2048 * j:2048 * (j + 1)] = t[:2048]
        tm[b, S + 64 * j:S + 64 * (j + 1)] = t[2048:]
    return fmb, fmf, tm


NTOK = S + L
NKT = NTOK // 128
NEB = 21


def na_tile_lists():
    out = []
    for m in range(64):
        if 2 <= m <= 61:
            out.append(([m - 2, m - 1, m, m + 1, m + 2], 0))
        elif m < 2:
            out.append(([0, 1, 2, 3], 5 + 4 * m))
        else:
            out.append(([60, 61, 62, 63], 5 + 4 * (m - 60)))
    return out


def na_bias_index():
    MASKED = 15 * 31
    idx = np.full((NEB, 128, 128), MASKED, np.int64)
    lists = na_tile_lists()
    reps = {0: 10}
    qq = np.arange(128); kk = np.arange(128)

    def fill(e0, m, kts):
        for ii, n in enumerate(kts):
            qr = 2 * m + qq // 64; qc = qq % 64
            kr = 2 * n + kk // 64; kc = kk % 64
            r0 = np.clip(qr - 4, 0, 120)
            cs = np.clip(qc - 8, 0, 48)
            valid = ((kr[:, None] >= r0[None, :]) & (kr[:, None] < r0[None, :] + 8) &
                     (kc[:, None] >= cs[None, :]) & (kc[:, None] < cs[None, :] + 16))
            dr = kr[:, None] - qr[None, :]
            dc = np.clip(kc[:, None] - qc[None, :], -15, 15)
            v = (np.clip(dr, -7, 7) + 7) * 31 + dc + 15
            idx[e0 + ii] = np.where(valid, v, MASKED)
    fill(0, 10, lists[10][0])
    for m in (0, 1, 62, 63):
        fill(lists[m][1], m, lists[m][0])
    return idx


def build_p2(lam_init):
    nc = bass.Bass("TRN2", target_bir_lowering=False)
    din = lambda n, shp, dt: nc.dram_tensor(n, shp, dt, kind="ExternalInput").ap()
    dout = lambda n, shp, dt: nc.dram_tensor(n, shp, dt, kind="ExternalOutput").ap()
    xrg = din("xrg", [2, 128, NTOK], F32)
    wbd = din("wbd", [4, 128, 128], F32)
    rgv = din("rgv", [128, 2, 8], F32)
    hout = dout("hout", [2, 128, NTOK], F32)
    qaT = din("qaT", [64, NTOK], BF16)
    kaT = din("kaT", [64, NTOK], BF16)
    vaP = din("vaP", [128, NKT, 64], BF16)
    btT = din("btT", [128, NEB, 128], F32)
    ynaP = dout("ynaP", [128, NKT, 64], BF16)
    qdT = din("qdT", [2, 32, NTOK], BF16)
    kdT = din("kdT", [2, 32, NTOK], BF16)
    vdP = din("vdP", [128, NKT, 64], BF16)
    dlam = din("dlam", [128, 128], F32)
    dg = din("dg", [128, 64], F32)
    ydfP = dout("ydfP", [128, NKT, 64], BF16)
    s = Sched(nc)
    with nc.psum_tensor("ps", [128, 8, 512], F32) as ps:
        bps = [s.buf(f"ps{i}") for i in range(8)]

        CH = 2048
        with (nc.sbuf_tensor("x_sb", [128, NTOK], F32) as x_sb,
              nc.sbuf_tensor("wst_sb", [128, 4, 128], F32) as wst_sb,
              nc.sbuf_tensor("wbf_sb", [128, 4, 128], BF16) as wbf_sb,
              nc.sbuf_tensor("rgv_sb", [128, 2, 8], F32) as rgv_sb,
              nc.sbuf_tensor("cneg_sb", [128, 2], F32) as cneg_sb,
              nc.sbuf_tensor("xcv_sb", [128, 2, CH], F32) as xcv_sb,
              nc.sbuf_tensor("xcb_sb", [128, 2, CH], BF16) as xcb_sb,
              nc.sbuf_tensor("r_sb", [128, 2, CH], F32) as r_sb,
              nc.sbuf_tensor("i_sb", [128, 2, CH], F32) as i_sb,
              nc.sbuf_tensor("a_sb", [128, 2, CH], F32) as a_sb,
              nc.sbuf_tensor("q_sb", [128, 2, CH], F32) as q_sb,
              nc.sbuf_tensor("h_sb", [128, 2, CH], F32) as h_sb,
              nc.sbuf_tensor("carry_sb", [128, 1], F32) as carry_sb):
            bx = s.buf("x", s.dsem("rgx"))
            bw = s.buf("w", s.dsem("rgw"))
            bwb = s.buf("wb")
            bv = s.buf("v", bw.dsem)
            bcn = s.buf("cneg")
            bxcv2 = [s.buf(f"xcv{i}") for i in range(2)]; bxcb2 = [s.buf(f"xcb{i}") for i in range(2)]
            br2 = [s.buf(f"r{i}") for i in range(2)]; bi2 = [s.buf(f"i{i}") for i in range(2)]
            ba2 = [s.buf(f"a{i}") for i in range(2)]; bq2 = [s.buf(f"q{i}") for i in range(2)]; bcar = s.buf("carry")
            bh = [s.buf(f"h{i}", s.dsem(f"rgh{i}")) for i in range(2)]
            s.dma("act", wst_sb[:], wbd.rearrange("f c d -> c f d"), W=[bw])
            s.dma("act", rgv_sb[:], rgv, W=[bv])
            s.op("pool", lambda e: e.tensor_copy(out=wbf_sb[:], in_=wst_sb[:]), R=[bw], W=[bwb])
            s.op("act", lambda e: e.activation(out=cneg_sb[:], in_=rgv_sb[:, :, 7], func=AF.Exp, scale=-1.0),
                 R=[bv], W=[bcn])
            s.op("act", lambda e: e.activation(out=cneg_sb[:], in_=cneg_sb[:], func=AF.Ln, bias=1.0, scale=1.0),
                 R=[bcn], W=[bcn])
            s.op("dve", lambda e: e.tensor_scalar(out=cneg_sb[:], in0=cneg_sb[:], scalar1=-8.0, scalar2=None,
                                                  op0=ALU.mult), R=[bcn], W=[bcn])
            units = []
            for dr in range(2):
                chunks = [(0, 256, 0, 256)] + [(256 + CH * i, CH, 256, NTOK) for i in range(4)]
                for ci, ch_ in enumerate(chunks):
                    units.append((dr, ci) + ch_)

            def emitA(ui):
                dr, ci, c0, n, s0, s1 = units[ui]
                cp = ui % 2
                bxcv = bxcv2[cp]; bxcb = bxcb2[cp]; br = br2[cp]; bi_ = bi2[cp]; ba = ba2[cp]; bq = bq2[cp]
                offs = [-2, -1, 0, 1] if dr == 0 else [2, 1, 0, -1]
                if ci == 0:
                    s.dma("sp", x_sb[:, 0:4224], xrg[dr, :, 0:4224], W=[bx])
                    s.dma("sp", x_sb[:, 4224:NTOK], xrg[dr, :, 4224:NTOK], W=[bx])
                s.op("dve", lambda e: e.tensor_scalar(
                    out=xcv_sb[:, cp, 0:n], in0=x_sb[:, c0:c0 + n], scalar1=rgv_sb[:, dr, 2:3],
                    scalar2=rgv_sb[:, dr, 4:5], op0=ALU.mult, op1=ALU.add), R=[bx, bv], W=[bxcv])
                for jt in (0, 1, 3):
                    o = offs[jt]
                    lo = max(c0, s0 - o); hi_ = min(c0 + n, s1 - o)
                    s.op("dve", lambda e: e.scalar_tensor_tensor(
                        out=xcv_sb[:, cp, lo - c0:hi_ - c0], in0=x_sb[:, lo + o:hi_ + o],
                        scalar=rgv_sb[:, dr, jt:jt + 1], in1=xcv_sb[:, cp, lo - c0:hi_ - c0],
                        op0=ALU.mult, op1=ALU.add), R=[bx, bv, bxcv], W=[bxcv])
                s.op("pool", lambda e: e.tensor_copy(out=xcb_sb[:, cp, 0:n], in_=xcv_sb[:, cp, 0:n]), R=[bxcv], W=[bxcb])
                nsb = (n + 511) // 512
                for sb in range(nsb):
                    w_ = min(512, n - sb * 512)
                    mm(s, ps[:, sb, 0:w_], wbf_sb[:, 2 * dr, :], xcb_sb[:, cp, sb * 512:sb * 512 + w_], True, True,
                       R=[bwb, bxcb], W=[bps[sb]])
                    mm(s, ps[:, 4 + sb, 0:w_], wbf_sb[:, 2 * dr + 1, :], xcb_sb[:, cp, sb * 512:sb * 512 + w_], True, True,
                       R=[bwb, bxcb], W=[bps[4 + sb]])
                if n == CH:
                    rin = ps[:, 0:4, :]; iin = ps[:, 4:8, :]
                    rout = r_sb[:, cp, 0:n].rearrange("p (a b) -> p a b", b=512)
                    iout = i_sb[:, cp, 0:n].rearrange("p (a b) -> p a b", b=512)
                else:
                    rin = ps[:, 0, 0:n]; iin = ps[:, 4, 0:n]
                    rout = r_sb[:, cp, 0:n]; iout = i_sb[:, cp, 0:n]
                s.op("act", lambda e: e.activation(out=rout, in_=rin, func=AF.Sigmoid,
                                                   bias=rgv_sb[:, dr, 5:6], scale=1.0), R=bps[0:4] + [bv], W=[br])
                s.op("act", lambda e: e.activation(out=iout, in_=iin, func=AF.Sigmoid,
                                                   bias=rgv_sb[:, dr, 6:7], scale=1.0), R=bps[4:8] + [bv], W=[bi_])
                s.op("act", lambda e: e.activation(out=a_sb[:, cp, 0:n], in_=r_sb[:, cp, 0:n], func=AF.Exp,
                                                   scale=cneg_sb[:, dr:dr + 1]), R=[br, bcn], W=[ba])
                s.op("pool", lambda e: e.tensor_tensor(out=q_sb[:, cp, 0:n], in0=a_sb[:, cp, 0:n], in1=a_sb[:, cp, 0:n],
                                                       op=ALU.mult), R=[ba], W=[bq])
                s.op("act", lambda e: e.activation(out=q_sb[:, cp, 0:n], in_=q_sb[:, cp, 0:n], func=AF.Sqrt,
                                                   scale=-1.0, bias=1.0), R=[bq], W=[bq])
                s.op("pool", lambda e: e.tensor_tensor(out=i_sb[:, cp, 0:n], in0=i_sb[:, cp, 0:n], in1=xcv_sb[:, cp, 0:n],
                                                       op=ALU.mult), R=[bi_, bxcv], W=[bi_])
                s.op("pool", lambda e: e.tensor_tensor(out=q_sb[:, cp, 0:n], in0=q_sb[:, cp, 0:n], in1=i_sb[:, cp, 0:n],
                                                       op=ALU.mult), R=[bq, bi_], W=[bq])

            def emitB(ui):
                dr, ci, c0, n, s0, s1 = units[ui]
                cp = ui % 2
                hs = ui % 2
                init = 0.0 if ci == 0 else carry_sb[:, 0:1]
                s.op("dve", lambda e: e.tensor_tensor_scan(out=h_sb[:, hs, 0:n], data0=a_sb[:, cp, 0:n],
                                                           data1=q_sb[:, cp, 0:n], initial=init,
                                                           op0=ALU.mult, op1=ALU.add),
                     R=[ba2[cp], bq2[cp]] + ([bcar] if ci else []), W=[bh[hs]])
                s.op("dve", lambda e: e.tensor_copy(out=carry_sb[:, 0:1], in_=h_sb[:, hs, n - 1:n]),
                     R=[bh[hs]], W=[bcar])
                s.dma("sp", hout[dr, :, c0:c0 + n], h_sb[:, hs, 0:n], R=[bh[hs]])

            for ui in range(len(units)):
                emitA(ui)
                if ui >= 1:
                    emitB(ui - 1)
            emitB(len(units) - 1)
            s.barrier(bh)

        with (nc.sbuf_tensor("na_q_sb", [64, NTOK], BF16) as q_sb,
              nc.sbuf_tensor("na_k_sb", [64, NTOK], BF16) as k_sb,
              nc.sbuf_tensor("na_v_sb", [128, NKT, 65], BF16) as v_sb,
              nc.sbuf_tensor("na_bt_sb", [128, NEB * 128], F32) as bt_sb,
              nc.sbuf_tensor("na_eb_sb", [128, NEB * 128], BF16) as eb_sb,
              nc.sbuf_tensor("na_e_sb", [128, 2, 640], F32) as e_sb,
              nc.sbuf_tensor("na_p_sb", [128, 2, 896], BF16) as p_sb,
              nc.sbuf_tensor("na_y_sb", [128, NKT, 64], BF16) as y_sb,
              nc.sbuf_tensor("na_rec_sb", [128, 2], F32) as rec_sb):
            ld = s.dsem("nald")
            bq = s.buf("q", ld); bk = s.buf("k", ld); bv = s.buf("v", ld); bbt = s.buf("bt", ld)
            beb = s.buf("eb")
            be = [s.buf(f"e{i}") for i in range(2)]
            bp = [s.buf(f"p{i}") for i in range(2)]
            brec = [s.buf(f"rec{i}") for i in range(2)]
            by = s.buf("y", s.dsem("nay"))
            s.dma("sp", q_sb[:], qaT, W=[bq])
            s.dma("act", k_sb[:], kaT, W=[bk])
            s.dma("sp", v_sb[:, :, 0:64], vaP, W=[bv])
            s.dma("act", bt_sb[:], btT.rearrange("p e q -> p (e q)"), W=[bbt])
            s.op("pool", lambda e: e.memset(v_sb[:, :, 64:65], 1.0), W=[bv])
            s.op("act", lambda e: e.activation(out=eb_sb[:], in_=bt_sb[:], func=AF.Exp), R=[bbt], W=[beb])
            lists = na_tile_lists()
            for m in range(NKT):
                sl = m % 2
                if m < 64:
                    kts, eb0 = lists[m]
                else:
                    kts, eb0 = [], 0
                nl = len(kts)
                bA = bps[2 * sl]; bB = bps[2 * sl + 1]; bAcc = bps[4 + sl]
                qs = q_sb[:, m * 128:(m + 1) * 128]
                for ii, n in enumerate(kts):
                    bank = 2 * sl + (0 if ii < 4 else 1)
                    col = (ii % 4) * 128
                    mm(s, ps[:, bank, col:col + 128], k_sb[:, n * 128:(n + 1) * 128], qs, True, True,
                       R=[bk, bq], W=[bps[bank]])
                for ci in range(2):
                    n = 64 + ci
                    mm(s, ps[:, 2 * sl + 1, 128 + ci * 128:256 + ci * 128], k_sb[:, n * 128:(n + 1) * 128], qs,
                       True, True, R=[bk, bq], W=[bB])
                if nl:
                    na_ = min(nl, 4) * 128
                    s.op("act", lambda e: e.activation(out=e_sb[:, sl, 0:na_], in_=ps[:, 2 * sl, 0:na_], func=AF.Exp,
                                                       scale=0.125), R=[bA], W=[be[sl]])
                    if nl == 5:
                        s.op("act", lambda e: e.activation(out=e_sb[:, sl, 512:640], in_=ps[:, 2 * sl + 1, 0:128],
                                                           func=AF.Exp, scale=0.125), R=[bB], W=[be[sl]])
                s.op("act", lambda e: e.activation(out=p_sb[:, sl, 640:896], in_=ps[:, 2 * sl + 1, 128:384],
                                                   func=AF.Exp, scale=0.125), R=[bB], W=[bp[sl]])
                if nl:
                    s.op("dve", lambda e: e.tensor_tensor(out=p_sb[:, sl, 0:nl * 128], in0=e_sb[:, sl, 0:nl * 128],
                                                          in1=eb_sb[:, eb0 * 128:(eb0 + nl) * 128], op=ALU.mult),
                         R=[be[sl], beb], W=[bp[sl]])
                tiles = [(ii * 128, n) for ii, n in enumerate(kts)] + [(640, 64), (768, 65)]
                for ti, (pc, n) in enumerate(tiles):
                    mm(s, ps[:, 4 + sl, 0:65], p_sb[:, sl, pc:pc + 128], v_sb[:, n, :], ti == 0, ti == len(tiles) - 1,
                       R=[bp[sl], bv], W=[bAcc])
                s.op("dve", lambda e: e.reciprocal(out=rec_sb[:, sl:sl + 1], in_=ps[:, 4 + sl, 64:65]),
                     R=[bAcc], W=[brec[sl]])
                s.op("dve", lambda e: e.tensor_scalar(out=y_sb[:, m, :], in0=ps[:, 4 + sl, 0:64],
                                                      scalar1=rec_sb[:, sl:sl + 1], scalar2=None, op0=ALU.mult),
                     R=[bAcc, brec[sl]], W=[by])
            s.dma("sp", ynaP, y_sb[:], R=[by])
            s.barrier([by])

        with (nc.sbuf_tensor("df_q4_sb", [96, NTOK], BF16) as q4_sb,
              nc.sbuf_tensor("df_k4_sb", [96, NTOK], BF16) as k4_sb,
              nc.sbuf_tensor("df_q4b_sb", [96, NTOK], BF16) as q4b_sb,
              nc.sbuf_tensor("df_k4b_sb", [96, NTOK], BF16) as k4b_sb,
              nc.sbuf_tensor("df_v_sb", [128, NKT, 65], BF16) as v_sb,
              nc.sbuf_tensor("df_p_sb", [128, 8, 512], BF16) as p_sb,
              nc.sbuf_tensor("df_y_sb", [128, NKT, 64], BF16) as y_sb,
              nc.sbuf_tensor("df_dl_sb", [128, 128], F32) as dl_sb,
              nc.sbuf_tensor("df_g_sb", [128, 64], F32) as g_sb,
              nc.sbuf_tensor("df_lam_sb", [128, 4], F32) as lam_sb,
              nc.sbuf_tensor("df_r_sb", [128, 2, 2, 4], F32) as r_sb,
              nc.sbuf_tensor("df_ss_sb", [128, 2, 4], F32) as ss_sb,
              nc.sbuf_tensor("df_o_sb", [128, 2, 4, 64], F32) as o_sb,
              nc.sbuf_tensor("df_t_sb", [128, 2, 4, 64], F32) as t_sb,
              nc.sbuf_tensor("df_junk_sb", [128, 64], F32) as junk_sb):
            ld = s.dsem("dfld")
            bq = s.buf("q", ld); bk = s.buf("k", ld); bv = s.buf("v", ld); bdl = s.buf("dl", ld); bg = s.buf("g", ld)
            blam = s.buf("lam")
            bp = [s.buf(f"p{i}") for i in range(8)]
            by = s.buf("y", s.dsem("dfy"))
            br = [s.buf(f"r{i}") for i in range(2)]
            bss = [s.buf(f"ss{i}") for i in range(2)]
            bo = [s.buf(f"o{i}") for i in range(2)]
            bt = [s.buf(f"t{i}") for i in range(2)]
            bj = s.buf("junk")
            for (qt, kt_, order) in ((q4_sb, k4_sb, (0, 1, 0)), (q4b_sb, k4b_sb, (1, 0, 1))):
                for ri, cc in enumerate(order):
                    s.dma("sp", qt[32 * ri:32 * ri + 32, :], qdT[cc], W=[bq])
                    s.dma("act", kt_[32 * ri:32 * ri + 32, :], kdT[cc], W=[bk])
            s.dma("sp", v_sb[:, :, 0:64], vdP, W=[bv])
            s.dma("act", dl_sb[:], dlam, W=[bdl])
            s.dma("act", g_sb[:], dg, W=[bg])
            s.op("pool", lambda e: e.memset(v_sb[:, :, 64:65], 1.0), W=[bv])
            s.op("dve", lambda e: e.scalar_tensor_tensor(out=junk_sb[:, 0:32], in0=dl_sb[:, 0:32], scalar=1.0,
                                                         in1=dl_sb[:, 32:64], op0=ALU.mult, op1=ALU.mult,
                                                         accum_out=lam_sb[:, 0:1]), R=[bdl], W=[bj, blam])
            s.op("dve", lambda e: e.scalar_tensor_tensor(out=junk_sb[:, 0:32], in0=dl_sb[:, 64:96], scalar=1.0,
                                                         in1=dl_sb[:, 96:128], op0=ALU.mult, op1=ALU.mult,
                                                         accum_out=lam_sb[:, 1:2]), R=[bdl, bj], W=[bj, blam])
            s.op("act", lambda e: e.activation(out=lam_sb[:, 0:2], in_=lam_sb[:, 0:2], func=AF.Exp), R=[blam], W=[blam])
            s.op("dve", lambda e: e.tensor_tensor(out=lam_sb[:, 2:3], in0=lam_sb[:, 1:2], in1=lam_sb[:, 0:1],
                                                  op=ALU.subtract), R=[blam], W=[blam])
            s.op("dve", lambda e: e.tensor_scalar(out=lam_sb[:, 2:3], in0=lam_sb[:, 2:3], scalar1=-lam_init,
                                                  scalar2=None, op0=ALU.add), R=[blam], W=[blam])
            s.op("dve", lambda e: e.tensor_scalar(out=g_sb[:], in0=g_sb[:], scalar1=1.0 - lam_init, scalar2=None,
                                                  op0=ALU.mult), R=[bg], W=[bg])
            qblocks = [(512 * i, 512, list(range(NKT))) for i in range(16)] + [(S, 256, [64, 65])]
            sc = 32 ** -0.5
            gs_ = 0
            for qi, (q0, nq, kts) in enumerate(qblocks):
                par = qi % 2
                nsub = nq // 128
                steps = [(kt, c) for kt in kts for c in range(2)]
                ns = len(steps)
                groups = [list(range(i, min(i + 3, ns))) for i in range(0, ns, 3)]
                started = [False, False]
                for gi_ in range(len(groups) + 1):
                    if gi_ < len(groups):
                        grp = groups[gi_]
                        layB = steps[grp[0]][1] == 1
                        qt = q4b_sb if layB else q4_sb
                        kt_t = k4b_sb if layB else k4_sb
                        for ri, si in enumerate(grp):
                            kt, c = steps[si]
                            g = gs_ + si
                            mm(s, ps[:, g % 4, 0:nq], kt_t[32 * ri:32 * ri + 32, kt * 128:(kt + 1) * 128],
                               qt[32 * ri:32 * ri + 32, q0:q0 + nq], True, True, R=[bk, bq], W=[bps[g % 4]])
                        for ri, si in enumerate(grp):
                            g = gs_ + si
                            s.op("act", lambda e: e.activation(out=p_sb[:, g % 8, 0:nq], in_=ps[:, g % 4, 0:nq],
                                                               func=AF.Exp, scale=sc), R=[bps[g % 4]], W=[bp[g % 8]])
                    if gi_ >= 1:
                        for si in groups[gi_ - 1]:
                            kt, c = steps[si]
                            g = gs_ + si
                            bank = 4 + 2 * par + c
                            for sub in range(nsub):
                                st = not started[c]
                                started[c] = True
                                s.op("pe", lambda e: e.matmul(ps[:, bank, sub * 65:(sub + 1) * 65],
                                                              p_sb[:, g % 8, sub * 128:(sub + 1) * 128], v_sb[:, kt, :],
                                                              start=st, stop=(kt == kts[-1]), skip_group_check=True),
                                     R=[bp[g % 8], bv], W=[bps[bank]])
                gs_ += ns
                a0 = ps[:, 4 + 2 * par, 0:260].rearrange("p (s e) -> p s e", e=65)
                a1 = ps[:, 5 + 2 * par, 0:260].rearrange("p (s e) -> p s e", e=65)
                b0 = bps[4 + 2 * par]; b1 = bps[5 + 2 * par]
                s.op("dve", lambda e: e.reciprocal(out=r_sb[:, par, 0, 0:nsub], in_=a0[:, 0:nsub, 64]), R=[b0], W=[br[par]])
                s.op("dve", lambda e: e.reciprocal(out=r_sb[:, par, 1, 0:nsub], in_=a1[:, 0:nsub, 64]), R=[b1], W=[br[par]])
                s.op("dve", lambda e: e.tensor_scalar(out=r_sb[:, par, 1, 0:nsub], in0=r_sb[:, par, 1, 0:nsub],
                                                      scalar1=lam_sb[:, 2:3], scalar2=None, op0=ALU.mult),
                     R=[br[par], blam], W=[br[par]])
                for sub in range(nsub):
                    s.op("dve", lambda e: e.tensor_scalar(out=t_sb[:, par, sub, :], in0=a1[:, sub, 0:64],
                                                          scalar1=r_sb[:, par, 1, sub:sub + 1], scalar2=None,
                                                          op0=ALU.mult), R=[b1, br[par]], W=[bt[par]])
                    s.op("dve", lambda e: e.scalar_tensor_tensor(out=o_sb[:, par, sub, :], in0=a0[:, sub, 0:64],
                                                                 scalar=r_sb[:, par, 0, sub:sub + 1],
                                                                 in1=t_sb[:, par, sub, :], op0=ALU.mult, op1=ALU.add),
                         R=[b0, br[par], bt[par]], W=[bo[par]])
                    s.op("dve", lambda e: e.scalar_tensor_tensor(out=junk_sb[:], in0=o_sb[:, par, sub, :], scalar=1.0,
                                                                 in1=o_sb[:, par, sub, :], op0=ALU.mult, op1=ALU.mult,
                                                                 accum_out=ss_sb[:, par, sub:sub + 1]),
                         R=[bo[par], bj], W=[bj, bss[par]])
                s.op("act", lambda e: e.activation(out=ss_sb[:, par, 0:nsub], in_=ss_sb[:, par, 0:nsub], func=AF.Ln,
                                                   scale=1.0 / 64, bias=EPS), R=[bss[par]], W=[bss[par]])
                s.op("act", lambda e: e.activation(out=ss_sb[:, par, 0:nsub], in_=ss_sb[:, par, 0:nsub], func=AF.Exp,
                                                   scale=-0.5), R=[bss[par]], W=[bss[par]])
                for sub in range(nsub):
                    s.op("dve", lambda e: e.scalar_tensor_tensor(out=y_sb[:, q0 // 128 + sub, :], in0=o_sb[:, par, sub, :],
                                                                 scalar=ss_sb[:, par, sub:sub + 1], in1=g_sb[:],
                                                                 op0=ALU.mult, op1=ALU.mult),
                         R=[bo[par], bss[par], bg], W=[by])
            s.dma("sp", ydfP, y_sb[:], R=[by])
            s.finish([by])
    return nc


def tileP(a):
    return np.ascontiguousarray(a.reshape(NKT, 128, a.shape[1]).transpose(1, 0, 2))


def untileP(a):
    return a.transpose(1, 0, 2).reshape(NTOK, a.shape[2])


_NA_IDX = None


def run_p2(nc2, l, fmb, fmf, tm, inp):
    global _NA_IDX
    if _NA_IDX is None:
        _NA_IDX = na_bias_index()
    in_maps = []
    for i in range(NCORES):
        b, j = i // 4, i % 4
        xr = fmf[b, 128 * j:128 * (j + 1)]
        xf = np.concatenate([xr[:, S:], xr[:, :S]], axis=1)
        xb = np.concatenate([xr[:, S:][:, ::-1], xr[:, :S][:, ::-1]], axis=1)
        wbd = np.zeros((4, 128, 128), np.float32)
        rgv = np.zeros((128, 2, 8), np.float32)
        ch = slice(128 * j, 128 * (j + 1))
        for dr in range(2):
            for gi, wk in enumerate(("rg_w_r", "rg_w_i")):
                for bb in range(2):
                    wbd[dr * 2 + gi, 64 * bb:64 * (bb + 1), 64 * bb:64 * (bb + 1)] = inp[wk][l, dr, 2 * j + bb]
            rgv[:, dr, 0:4] = inp["rg_conv_w"][l][:, ch].T
            rgv[:, dr, 4] = inp["rg_conv_b"][l][ch]
            rgv[:, dr, 5] = inp["rg_b_r"][l, dr, ch]
            rgv[:, dr, 6] = inp["rg_b_i"][l, dr, ch]
            rgv[:, dr, 7] = inp["rg_lambda"][l, dr, ch]
        rext = np.concatenate([inp["na_rpb"][l, j].ravel(), np.array([-30000.0], np.float32)])
        bt = rext[_NA_IDX]
        in_maps.append({
            "xrg": np.ascontiguousarray(np.stack([xf, xb])), "wbd": wbd, "rgv": rgv,
            "qaT": np.ascontiguousarray(fmb[b, 64 * j:64 * (j + 1)]),
            "kaT": np.ascontiguousarray(fmb[b, 256 + 64 * j:256 + 64 * (j + 1)]),
            "vaP": tileP(tm[b][:, 64 * j:64 * (j + 1)]),
            "btT": np.ascontiguousarray(bt.transpose(1, 0, 2)),
            "qdT": np.ascontiguousarray(fmb[b, 512 + 64 * j:512 + 64 * (j + 1)].reshape(2, 32, NTOK)),
            "kdT": np.ascontiguousarray(fmb[b, 768 + 64 * j:768 + 64 * (j + 1)].reshape(2, 32, NTOK)),
            "vdP": tileP(tm[b][:, 256 + 64 * j:256 + 64 * (j + 1)]),
            "dlam": np.ascontiguousarray(np.tile(inp["diff_lambda"][l].reshape(1, 128), (128, 1))),
            "dg": np.ascontiguousarray(np.tile(inp["diff_subln_g"][l].reshape(1, 64), (128, 1))),
        })
    res = run_bass_kernel_spmd(nc2, in_maps, core_ids=list(range(NCORES)))
    yna = np.zeros((B, NTOK, 256), ml_dtypes.bfloat16)
    ydf = np.zeros((B, NTOK, 256), ml_dtypes.bfloat16)
    hf = np.zeros((B, 512, NTOK), np.float32)
    hb = np.zeros((B, 512, NTOK), np.float32)
    for i in range(NCORES):
        b, j = i // 4, i % 4
        r = res.results[i]
        yna[b, :, 64 * j:64 * (j + 1)] = untileP(r["ynaP"])
        ydf[b, :, 64 * j:64 * (j + 1)] = untileP(r["ydfP"])
        h = r["hout"]
        hf[b, 128 * j:128 * (j + 1), S:] = h[0][:, :L]
        hf[b, 128 * j:128 * (j + 1), :S] = h[0][:, L:]
        hb[b, 128 * j:128 * (j + 1), S:] = h[1][:, :L][:, ::-1]
        hb[b, 128 * j:128 * (j + 1), :S] = h[1][:, L:][:, ::-1]
    return yna, ydf, hf, hb


NEXP = 32
GELU_C = 1.5957691216057308


def build_p3(final):
    nc = bass.Bass("TRN2", target_bir_lowering=False)
    din = lambda n, shp, dt: nc.dram_tensor(n, shp, dt, kind="ExternalInput").ap()
    xT = din("xT", [128, 8, NT1], F32)
    nadf = din("nadf", [128, 4, NT1], BF16)
    hg = din("hg", [128, 12, NT1], F32)
    wout = din("wout", [D, D], F32)
    mod = din("mod", [128, 10, 8], F32)
    wge = din("wge", [128, 8, 36], F32)
    bge = din("bge", [128, 36], F32)
    selc = din("selc", [32, NEXP * 128], BF16)
    ident = din("ident", [128, 128], F32)
    w1 = din("w1", [NEXP, D, 512], F32)
    w3 = din("w3", [NEXP, D, 512], F32)
    w2 = din("w2", [NEXP, 512, D], F32)
    xo = nc.dram_tensor("xo", [128, 8, NT1], F32, kind="ExternalOutput").ap()
    s = Sched(nc)
    wov = wout.rearrange("(kc p) n -> p kc n", p=128)
    with (nc.psum_tensor("ps", [128, 8, 512], F32) as ps,
          nc.sbuf_tensor("x_sb", [128, 8, NT1], F32) as x_sb,
          nc.sbuf_tensor("mod_sb", [128, 10, 8], F32) as mod_sb,
          nc.sbuf_tensor("hl2_sb", [128, 8, NT1], BF16) as hl2_sb,
          nc.sbuf_tensor("wdt_sb", [32, 2, NT1], BF16) as wdt_sb,
          nc.sbuf_tensor("ones_sb", [128, 128], BF16) as ones_sb):
        bps = [s.buf(f"ps{i}") for i in range(8)]
        cs = s.dsem("const")
        bxb = [s.buf(f"x{i}", s.dsem(f"x{i}")) for i in range(len(BLKS1))]
        bmod = s.buf("mod", cs)
        bhl2 = [s.buf(f"hl2_{i}") for i in range(len(BLKS1))]
        bwdt = [s.buf(f"wdt{i}") for i in range(len(BLKS1))]
        bones = s.buf("ones")
        s.dma("sp", mod_sb[:], mod, W=[bmod])
        for bi, (t0, n) in enumerate(BLKS1):
            s.dma("sp", x_sb[:, :, t0:t0 + n], xT[:, :, t0:t0 + n], W=[bxb[bi]])
        s.op("pool", lambda e: e.memset(ones_sb[:], 1.0), W=[bones])

        with (nc.sbuf_tensor("s1_mix", [128, 8, NT1], BF16) as mix_sb,
              nc.sbuf_tensor("s1_wobf", [128, 8, D], BF16) as wo_bf,
              nc.sbuf_tensor("s1_wost", [128, 2, D], F32) as wo_st,
              nc.sbuf_tensor("s1_hg", [128, 1, 12, 512], F32) as hg_sb,
              nc.sbuf_tensor("s1_t", [128, 4, 512], F32) as t_sb):
            bmixl = s.buf("mixl", s.dsem("mixl"))
            bmix = [s.buf(f"mix{i}") for i in range(len(BLKS1))]
            bwost = [s.buf(f"wost{i}", s.dsem(f"wost{i}")) for i in range(2)]
            bwobf = [s.buf(f"wobf{k}") for k in range(8)]
            bhg = [s.buf(f"hg{i}", s.dsem(f"hg{i}")) for i in range(2)]
            bt = [s.buf(f"t{i}") for i in range(4)]
            s.dma("act", mix_sb[:, 0:2, :], nadf[:, 0:2, :], W=[bmixl])
            s.dma("act", mix_sb[:, 6:8, :], nadf[:, 2:4, :], W=[bmixl])
            for k in range(8):
                sl = k % 2
                s.dma("act", wo_st[:, sl, :], wov[:, k, :], W=[bwost[sl]])
                s.op("pool", lambda e: e.tensor_copy(out=wo_bf[:, k, :], in_=wo_st[:, sl, :]), R=[bwost[sl]], W=[bwobf[k]])
            for bi, (t0, n) in enumerate(BLKS1):
                sl = 0
                s.dma("sp", hg_sb[:, sl, :, 0:n], hg[:, :, t0:t0 + n], W=[bhg[sl]])
                for ch in range(4):
                    hf_ = hg_sb[:, sl, ch, 0:n]; hb_ = hg_sb[:, sl, 4 + ch, 0:n]; gr_ = hg_sb[:, sl, 8 + ch, 0:n]
                    s.op("dve", lambda e: e.tensor_tensor(out=t_sb[:, 0, 0:n], in0=hf_, in1=hb_, op=ALU.add),
                         R=[bhg[sl]], W=[bt[0]])
                    s.op("dve", lambda e: e.tensor_tensor(out=t_sb[:, 1, 0:n], in0=gr_, in1=gr_, op=ALU.mult),
                         R=[bhg[sl]], W=[bt[1]])
                    s.op("dve", lambda e: e.tensor_scalar(out=t_sb[:, 1, 0:n], in0=t_sb[:, 1, 0:n], scalar1=0.044715,
                                                          scalar2=1.0, op0=ALU.mult, op1=ALU.add), R=[bt[1]], W=[bt[1]])
                    s.op("pool", lambda e: e.tensor_tensor(out=t_sb[:, 2, 0:n], in0=t_sb[:, 1, 0:n], in1=gr_, op=ALU.mult),
                         R=[bt[1], bhg[sl]], W=[bt[2]])
                    s.op("act", lambda e: e.activation(out=t_sb[:, 2, 0:n], in_=t_sb[:, 2, 0:n], func=AF.Sigmoid,
                                                       scale=GELU_C), R=[bt[2]], W=[bt[2]])
                    s.op("pool", lambda e: e.tensor_tensor(out=t_sb[:, 3, 0:n], in0=t_sb[:, 2, 0:n], in1=gr_, op=ALU.mult),
                         R=[bt[2], bhg[sl]], W=[bt[3]])
                    s.op("pool", lambda e: e.tensor_tensor(out=mix_sb[:, 2 + ch, t0:t0 + n], in0=t_sb[:, 3, 0:n],
                                                           in1=t_sb[:, 0, 0:n], op=ALU.mult),
                         R=[bt[3], bt[0]], W=[bmix[bi]])
            pi = 0
            for bi, (t0, n) in enumerate(BLKS1):
                garow = 5 if bi == 4 else 1
                for dc in range(8):
                    pb = pi % 8; pi += 1
                    for k in range(8):
                        mm(s, ps[:, pb, 0:n], wo_bf[:, k, dc * 128:(dc + 1) * 128], mix_sb[:, k, t0:t0 + n],
                           k == 0, k == 7, R=[bwobf[k], bmix[bi], bmixl], W=[bps[pb]])
                    s.op("dve", lambda e: e.scalar_tensor_tensor(out=x_sb[:, dc, t0:t0 + n], in0=ps[:, pb, 0:n],
                                                                 scalar=mod_sb[:, garow, dc:dc + 1],
                                                                 in1=x_sb[:, dc, t0:t0 + n], op0=ALU.mult, op1=ALU.add),
                         R=[bps[pb], bmod, bxb[bi]], W=[bxb[bi]])
            s.barrier()

        with (nc.sbuf_tensor("s2_sq", [128, 8, 512], BF16) as sq_sb,
              nc.sbuf_tensor("s2_rstd", [128, 2, 512], F32) as rstd_sb,
              nc.sbuf_tensor("s2_tmp", [128, 4, 512], F32) as tmp_sb,
              nc.sbuf_tensor("s2_hf", [128, 2, 8, 512], F32) as hf_sb,
              nc.sbuf_tensor("s2_ab", [128, 2, 8], F32) as ab_sb,
              nc.sbuf_tensor("s2_wge", [128, 8, 36], F32) as wge_sb,
              nc.sbuf_tensor("s2_bge", [128, 36], F32) as bge_sb,
              nc.sbuf_tensor("s2_id", [128, 128], F32) as id_sb,
              nc.sbuf_tensor("s2_rt", [128, 2, 128], F32) as rt_sb):
            bsq = s.buf("sq"); brs = [s.buf(f"rs{i}") for i in range(2)]
            btmp = [s.buf(f"tmp{i}") for i in range(4)]
            bhf = [s.buf(f"hf{i}") for i in range(2)]
            bab = s.buf("ab")
            bwge = s.buf("wge", cs); bbge = s.buf("bge", cs); bid = s.buf("id", cs)
            brt = [s.buf(f"rt{i}") for i in range(2)]
            s.dma("act", wge_sb[:], wge, W=[bwge])
            s.dma("act", bge_sb[:], bge, W=[bbge])
            s.dma("act", id_sb[:], ident, W=[bid])
            for t in range(2):
                s.op("dve", lambda e: e.scalar_tensor_tensor(
                    out=ab_sb[:, t, :], in0=mod_sb[:, 2 + 4 * t, :], scalar=1.0, in1=mod_sb[:, 0, :],
                    op0=ALU.add, op1=ALU.mult), R=[bmod], W=[bab])
            ti_g = 0
            for bi, (t0, n) in enumerate(BLKS1):
                sl = bi % 2
                isctx = bi == 4
                s.op("act", lambda e: e.activation(out=sq_sb[:, :, 0:n], in_=x_sb[:, :, t0:t0 + n], func=AF.Square),
                     R=[bxb[bi]], W=[bsq])
                for k in range(8):
                    mm(s, ps[:, sl, 0:n], ones_sb[:], sq_sb[:, k, 0:n], k == 0, k == 7, R=[bones, bsq], W=[bps[sl]])
                s.op("act", lambda e: e.activation(out=rstd_sb[:, sl, 0:n], in_=ps[:, sl, 0:n], func=AF.Sqrt,
                                                   scale=1.0 / D, bias=EPS), R=[bps[sl]], W=[brs[sl]])
                s.op("dve", lambda e: e.reciprocal(out=rstd_sb[:, sl, 0:n], in_=rstd_sb[:, sl, 0:n]),
                     R=[brs[sl]], W=[brs[sl]])
                ai = 1 if isctx else 0
                shrow = 7 if isctx else 3
                for k in range(8):
                    tb = k % 4
                    s.op("dve", lambda e: e.scalar_tensor_tensor(
                        out=tmp_sb[:, tb, 0:n], in0=x_sb[:, k, t0:t0 + n], scalar=ab_sb[:, ai, k:k + 1],
                        in1=rstd_sb[:, sl, 0:n], op0=ALU.mult, op1=ALU.mult),
                        R=[bxb[bi], bab, brs[sl]], W=[btmp[tb]])
                    s.op("act", lambda e: e.activation(out=hf_sb[:, sl, k, 0:n], in_=tmp_sb[:, tb, 0:n],
                                                       func=AF.Identity, bias=mod_sb[:, shrow, k:k + 1], scale=1.0),
                         R=[btmp[tb], bmod], W=[bhf[sl]])
                s.op("pool", lambda e: e.tensor_copy(out=hl2_sb[:, :, t0:t0 + n], in_=hf_sb[:, sl, :, 0:n]),
                     R=[bhf[sl]], W=[bhl2[bi]])
                for tt in range((n + 127) // 128):
                    c0 = tt * 128
                    m = min(128, n - c0)
                    rs_ = ti_g % 2; ti_g += 1
                    pb = 2 + rs_
                    rt = rt_sb[0:m, rs_, :]
                    brr = brt[rs_]
                    for k in range(8):
                        mm(s, ps[0:m, pb, 0:36], hf_sb[:, sl, k, c0:c0 + m], wge_sb[:, k, :], k == 0, k == 7,
                           R=[bhf[sl], bwge], W=[bps[pb]])
                    V = lambda eng, fn, R_=(), W_=(): s.op(eng, fn, R=[brr] + list(R_), W=[brr] + list(W_))
                    lg = rt[:, 0:36]
                    s.op("dve", lambda e: e.tensor_tensor(out=lg, in0=ps[0:m, pb, 0:36], in1=bge_sb[0:m, :], op=ALU.add),
                         R=[bps[pb], bbge], W=[brr])
                    gmax = rt[:, 36:37]; ngmax = rt[:, 37:38]; sume = rt[:, 38:39]; gtop = rt[:, 39:40]
                    eg = rt[:, 40:44]; ohg = rt[:, 44:48]; sel = rt[:, 48:56]; top8 = rt[:, 56:64]
                    dd = rt[:, 64:65]; ed = rt[:, 65:66]; w1_ = rt[:, 66:67]; wt1 = rt[:, 67:68]; wt2 = rt[:, 68:69]
                    ea = rt[:, 72:80]; eb_ = rt[:, 80:88]; wd = rt[:, 96:128]
                    V("dve", lambda e: e.reduce_max(out=gmax, in_=lg[:, 0:4], axis=AX.X))
                    V("dve", lambda e: e.tensor_scalar(out=ngmax, in0=gmax, scalar1=-1.0, scalar2=None, op0=ALU.mult))
                    V("act", lambda e: e.activation(out=eg, in_=lg[:, 0:4], func=AF.Exp, bias=ngmax, scale=1.0,
                                                    accum_out=sume))
                    V("dve", lambda e: e.reciprocal(out=gtop, in_=sume))
                    V("dve", lambda e: e.tensor_scalar(out=ohg, in0=lg[:, 0:4], scalar1=gmax, scalar2=None,
                                                       op0=ALU.is_equal))
                    V("dve", lambda e: e.tensor_scalar(out=sel, in0=lg[:, 4:12], scalar1=ohg[:, 0:1], scalar2=None,
                                                       op0=ALU.mult))
                    for g in range(1, 4):
                        V("dve", lambda e: e.scalar_tensor_tensor(out=sel, in0=lg[:, 4 + 8 * g:12 + 8 * g],
                                                                  scalar=ohg[:, g:g + 1], in1=sel,
                                                                  op0=ALU.mult, op1=ALU.add))
                    V("dve", lambda e: e.max(out=top8, in_=sel))
                    V("dve", lambda e: e.tensor_tensor(out=dd, in0=top8[:, 1:2], in1=top8[:, 0:1], op=ALU.subtract))
                    V("act", lambda e: e.activation(out=ed, in_=dd, func=AF.Exp))
                    V("dve", lambda e: e.tensor_scalar(out=w1_, in0=ed, scalar1=1.0, scalar2=None, op0=ALU.add))
                    V("dve", lambda e: e.reciprocal(out=w1_, in_=w1_))
                    V("dve", lambda e: e.tensor_tensor(out=wt1, in0=w1_, in1=gtop, op=ALU.mult))
                    V("dve", lambda e: e.tensor_tensor(out=wt2, in0=wt1, in1=ed, op=ALU.mult))
                    V("dve", lambda e: e.tensor_scalar(out=ea, in0=sel, scalar1=top8[:, 0:1], scalar2=wt1,
                                                       op0=ALU.is_equal, op1=ALU.mult))
                    V("dve", lambda e: e.tensor_scalar(out=eb_, in0=sel, scalar1=top8[:, 1:2], scalar2=wt2,
                                                       op0=ALU.is_equal, op1=ALU.mult))
                    V("dve", lambda e: e.tensor_tensor(out=ea, in0=ea, in1=eb_, op=ALU.add))
                    for g in range(4):
                        V("dve", lambda e: e.tensor_scalar(out=wd[:, 8 * g:8 * g + 8], in0=ea, scalar1=ohg[:, g:g + 1],
                                                           scalar2=None, op0=ALU.mult))
                    pt = 4 + rs_
                    s.op("pe", lambda e: e.transpose(ps[0:32, pt, 0:m], wd, id_sb[0:m, 0:m]), R=[brr, bid], W=[bps[pt]])
                    s.op("act", lambda e: e.copy(out=wdt_sb[:, 0, t0 + c0:t0 + c0 + m], in_=ps[0:32, pt, 0:m]),
                         R=[bps[pt]], W=[bwdt[bi]])
                    s.op("dve", lambda e: e.tensor_tensor(out=wdt_sb[:, 1, t0 + c0:t0 + c0 + m], in0=ps[0:32, pt, 0:m],
                                                          in1=wdt_sb[:, 0, t0 + c0:t0 + c0 + m], op=ALU.subtract),
                         R=[bps[pt], bwdt[bi]], W=[bwdt[bi]])
            s.barrier()

        with (nc.sbuf_tensor("s3_st", [128, 3, 2048], F32) as st_sb,
              nc.sbuf_tensor("s3_wb", [128, 2, 6, 2048], BF16) as wb_sb,
              nc.sbuf_tensor("s3_sel", [32, NEXP * 128], BF16) as sel_sb,
              nc.sbuf_tensor("s3_wbc", [128, 2, 512], F32) as wbc_sb,
              nc.sbuf_tensor("s3_sg", [128, 2, 512], F32) as sg_sb,
              nc.sbuf_tensor("s3_t", [128, 2, 512], F32) as t3_sb,
              nc.sbuf_tensor("s3_g", [128, 2, 4, 512], BF16) as g_sb):
            bst = [s.buf(f"st{i}", s.dsem(f"st{i}")) for i in range(3)]
            bwb = [[s.buf(f"wb{a}_{p}") for p in range(6)] for a in range(2)]
            bsel = s.buf("sel", cs)
            bwbc = [s.buf(f"wbc{i}") for i in range(2)]
            bsg = [s.buf(f"sg{i}") for i in range(2)]
            bt3 = [s.buf(f"t3{i}") for i in range(2)]
            bg = [s.buf(f"g{i}") for i in range(2)]
            s.dma("act", sel_sb[:], selc, W=[bsel])
            w1v = w1.rearrange("e (kc p) f -> e p kc f", p=128)
            w3v = w3.rearrange("e (kc p) f -> e p kc f", p=128)
            w2v = w2.rearrange("e (fc p) d -> e p fc d", p=128)

            def piece_src(e, p):
                if p < 2:
                    return w1v[e, :, 4 * p:4 * p + 4, :]
                if p < 4:
                    return w3v[e, :, 4 * (p - 2):4 * (p - 2) + 4, :]
                return w2v[e, :, 2 * (p - 4):2 * (p - 4) + 2, :]

            def piece_dma(P):
                e, p = divmod(P, 6)
                if e >= NEXP:
                    return
                sl = P % 3
                dst = st_sb[:, sl, :]
                dst = dst.rearrange("q (a b) -> q a b", a=4) if p < 4 else dst.rearrange("q (a b) -> q a b", a=2)
                s.dma("sp", dst, piece_src(e, p), W=[bst[sl]])

            def piece_cast(P):
                e, p = divmod(P, 6)
                if e >= NEXP:
                    return
                sl = P % 3
                s.op("pool", lambda en: en.tensor_copy(out=wb_sb[:, e % 2, p, :], in_=st_sb[:, sl, :]),
                     R=[bst[sl]], W=[bwb[e % 2][p]])

            for P in range(3):
                piece_dma(P)
            for P in range(6):
                piece_cast(P)
                piece_dma(P + 3)
            gi = 0
            for ex in range(NEXP):
                a = ex % 2
                for bi, (t0, n) in enumerate(BLKS1):
                    if bi < 3:
                        for P in (6 * (ex + 1) + 2 * bi, 6 * (ex + 1) + 2 * bi + 1):
                            piece_cast(P)
                            piece_dma(P + 3)
                    garow = 8 if bi == 4 else 4
                    wr = gi % 2
                    gs = gi % 2
                    gi += 1
                    mm(s, ps[:, 6, 0:n], sel_sb[:, ex * 128:(ex + 1) * 128], wdt_sb[:, 0, t0:t0 + n], True, False,
                       R=[bsel, bwdt[bi]], W=[bps[6]])
                    mm(s, ps[:, 6, 0:n], sel_sb[:, ex * 128:(ex + 1) * 128], wdt_sb[:, 1, t0:t0 + n], False, True,
                       R=[bsel, bwdt[bi]], W=[bps[6]])
                    s.op("act", lambda e: e.copy(out=wbc_sb[:, wr, 0:n], in_=ps[:, 6, 0:n]), R=[bps[6]], W=[bwbc[wr]])
                    for fc in range(4):
                        pr = fc % 2
                        for which in range(2):
                            bank = 2 * pr + which
                            for k in range(8):
                                wv = wb_sb[:, a, 2 * which + k // 4, :].rearrange("q (a b) -> q a b", a=4)
                                mm(s, ps[:, bank, 0:n], wv[:, k % 4, fc * 128:(fc + 1) * 128], hl2_sb[:, k, t0:t0 + n],
                                   k == 0, k == 7, R=[bwb[a][2 * which + k // 4], bhl2[bi]], W=[bps[bank]])
                        s.op("act", lambda e: e.activation(out=sg_sb[:, pr, 0:n], in_=ps[:, 2 * pr, 0:n], func=AF.Silu),
                             R=[bps[2 * pr]], W=[bsg[pr]])
                        s.op("dve", lambda e: e.tensor_tensor(out=t3_sb[:, pr, 0:n], in0=ps[:, 2 * pr + 1, 0:n],
                                                              in1=sg_sb[:, pr, 0:n], op=ALU.mult),
                             R=[bps[2 * pr + 1], bsg[pr]], W=[bt3[pr]])
                        s.op("pool", lambda e: e.tensor_tensor(out=g_sb[:, gs, fc, 0:n], in0=t3_sb[:, pr, 0:n],
                                                               in1=wbc_sb[:, wr, 0:n], op=ALU.mult),
                             R=[bt3[pr], bwbc[wr]], W=[bg[gs]])
                    for dc in range(8):
                        bank = 4 + dc % 2
                        for fc in range(4):
                            wv = wb_sb[:, a, 4 + fc // 2, :].rearrange("q (a b) -> q a b", a=2)
                            mm(s, ps[:, bank, 0:n], wv[:, fc % 2, dc * 128:(dc + 1) * 128], g_sb[:, gs, fc, 0:n],
                               fc == 0, fc == 3, R=[bwb[a][4 + fc // 2], bg[gs]], W=[bps[bank]])
                        s.op("dve", lambda e: e.scalar_tensor_tensor(out=x_sb[:, dc, t0:t0 + n], in0=ps[:, bank, 0:n],
                                                                     scalar=mod_sb[:, garow, dc:dc + 1],
                                                                     in1=x_sb[:, dc, t0:t0 + n], op0=ALU.mult, op1=ALU.add),
                             R=[bps[bank], bmod, bxb[bi]], W=[bxb[bi]])
            s.barrier()

        with (nc.sbuf_tensor("s4_sq", [128, 8, 512], BF16) as sq_sb,
              nc.sbuf_tensor("s4_rstd", [128, 2, 512], F32) as rstd_sb):
            bsq = s.buf("sq4"); brs = [s.buf(f"rs4{i}") for i in range(2)]
            for bi, (t0, n) in enumerate(BLKS1):
                if final:
                    sl = bi % 2
                    s.op("act", lambda e: e.activation(out=sq_sb[:, :, 0:n], in_=x_sb[:, :, t0:t0 + n], func=AF.Square),
                         R=[bxb[bi]], W=[bsq])
                    for k in range(8):
                        mm(s, ps[:, sl, 0:n], ones_sb[:], sq_sb[:, k, 0:n], k == 0, k == 7, R=[bones, bsq], W=[bps[sl]])
                    s.op("act", lambda e: e.activation(out=rstd_sb[:, sl, 0:n], in_=ps[:, sl, 0:n], func=AF.Sqrt,
                                                       scale=1.0 / D, bias=EPS), R=[bps[sl]], W=[brs[sl]])
                    s.op("dve", lambda e: e.reciprocal(out=rstd_sb[:, sl, 0:n], in_=rstd_sb[:, sl, 0:n]),
                         R=[brs[sl]], W=[brs[sl]])
                    for k in range(8):
                        s.op("dve", lambda e: e.scalar_tensor_tensor(
                            out=x_sb[:, k, t0:t0 + n], in0=x_sb[:, k, t0:t0 + n], scalar=mod_sb[:, 9, k:k + 1],
                            in1=rstd_sb[:, sl, 0:n], op0=ALU.mult, op1=ALU.mult),
                            R=[bxb[bi], bmod, brs[sl]], W=[bxb[bi]])
                s.dma("sp", xo[:, :, t0:t0 + n], x_sb[:, :, t0:t0 + n], R=[bxb[bi]])
            s.finish(bxb)
    return nc


def build_p3a():
    nc = bass.Bass("TRN2", target_bir_lowering=False)
    din = lambda n, shp, dt: nc.dram_tensor(n, shp, dt, kind="ExternalInput").ap()
    xT = din("xT", [128, 8, NT1], F32)
    nadf = din("nadf", [128, 4, NT1], BF16)
    hg = din("hg", [128, 12, NT1], F32)
    wout = din("wout", [D, D], F32)
    mod = din("mod", [128, 10, 8], F32)
    wge = din("wge", [128, 8, 36], F32)
    bge = din("bge", [128, 36], F32)
    iota4 = din("iota4", [128, 4], F32)
    ident = din("ident", [128, 128], F32)
    xo = nc.dram_tensor("xo", [128, 8, NT1], F32, kind="ExternalOutput").ap()
    hl2o = nc.dram_tensor("hl2o", [128, 8, NT1], BF16, kind="ExternalOutput").ap()
    wdto = nc.dram_tensor("wdto", [32, 2, NT1], BF16, kind="ExternalOutput").ap()
    gido = nc.dram_tensor("gido", [128, 17], F32, kind="ExternalOutput").ap()
    wd32o = nc.dram_tensor("wd32o", [128, 17, 32], F32, kind="ExternalOutput").ap()
    s = Sched(nc)
    wov = wout.rearrange("(kc p) n -> p kc n", p=128)
    with (nc.psum_tensor("ps", [128, 8, 512], F32) as ps,
          nc.sbuf_tensor("x_sb", [128, 8, NT1], F32) as x_sb,
          nc.sbuf_tensor("mod_sb", [128, 10, 8], F32) as mod_sb,
          nc.sbuf_tensor("hl2_sb", [128, 8, NT1], BF16) as hl2_sb,
          nc.sbuf_tensor("wdt_sb", [32, 2, NT1], BF16) as wdt_sb,
          nc.sbuf_tensor("ones_sb", [128, 128], BF16) as ones_sb):
        bps = [s.buf(f"ps{i}") for i in range(8)]
        cs = s.dsem("const")
        bxb = [s.buf(f"x{i}", s.dsem(f"x{i}")) for i in range(len(BLKS1))]
        bmod = s.buf("mod", cs)
        bhl2 = [s.buf(f"hl2_{i}") for i in range(len(BLKS1))]
        bwdt = [s.buf(f"wdt{i}") for i in range(len(BLKS1))]
        bones = s.buf("ones")
        s.dma("sp", mod_sb[:], mod, W=[bmod])
        for bi, (t0, n) in enumerate(BLKS1):
            s.dma("sp", x_sb[:, :, t0:t0 + n], xT[:, :, t0:t0 + n], W=[bxb[bi]])
        s.op("pool", lambda e: e.memset(ones_sb[:], 1.0), W=[bones])

        with (nc.sbuf_tensor("s1_mix", [128, 8, NT1], BF16) as mix_sb,
              nc.sbuf_tensor("s1_wobf", [128, 8, D], BF16) as wo_bf,
              nc.sbuf_tensor("s1_wost", [128, 2, D], F32) as wo_st,
              nc.sbuf_tensor("s1_hg", [128, 1, 12, 512], F32) as hg_sb,
              nc.sbuf_tensor("s1_t", [128, 4, 512], F32) as t_sb):
            bmixl = s.buf("mixl", s.dsem("mixl"))
            bmix = [s.buf(f"mix{i}") for i in range(len(BLKS1))]
            bwost = [s.buf(f"wost{i}", s.dsem(f"wost{i}")) for i in range(2)]
            bwobf = [s.buf(f"wobf{k}") for k in range(8)]
            bhg = [s.buf(f"hg{i}", s.dsem(f"hg{i}")) for i in range(2)]
            bt = [s.buf(f"t{i}") for i in range(4)]
            s.dma("act", mix_sb[:, 0:2, :], nadf[:, 0:2, :], W=[bmixl])
            s.dma("act", mix_sb[:, 6:8, :], nadf[:, 2:4, :], W=[bmixl])
            for k in range(8):
                sl = k % 2
                s.dma("act", wo_st[:, sl, :], wov[:, k, :], W=[bwost[sl]])
                s.op("pool", lambda e: e.tensor_copy(out=wo_bf[:, k, :], in_=wo_st[:, sl, :]), R=[bwost[sl]], W=[bwobf[k]])
            for bi, (t0, n) in enumerate(BLKS1):
                sl = 0
                s.dma("sp", hg_sb[:, sl, :, 0:n], hg[:, :, t0:t0 + n], W=[bhg[sl]])
                for ch in range(4):
                    hf_ = hg_sb[:, sl, ch, 0:n]; hb_ = hg_sb[:, sl, 4 + ch, 0:n]; gr_ = hg_sb[:, sl, 8 + ch, 0:n]
                    s.op("dve", lambda e: e.tensor_tensor(out=t_sb[:, 0, 0:n], in0=hf_, in1=hb_, op=ALU.add),
                         R=[bhg[sl]], W=[bt[0]])
                    s.op("dve", lambda e: e.tensor_tensor(out=t_sb[:, 1, 0:n], in0=gr_, in1=gr_, op=ALU.mult),
                         R=[bhg[sl]], W=[bt[1]])
                    s.op("dve", lambda e: e.tensor_scalar(out=t_sb[:, 1, 0:n], in0=t_sb[:, 1, 0:n], scalar1=0.044715,
                                                          scalar2=1.0, op0=ALU.mult, op1=ALU.add), R=[bt[1]], W=[bt[1]])
                    s.op("pool", lambda e: e.tensor_tensor(out=t_sb[:, 2, 0:n], in0=t_sb[:, 1, 0:n], in1=gr_, op=ALU.mult),
                         R=[bt[1], bhg[sl]], W=[bt[2]])
                    s.op("act", lambda e: e.activation(out=t_sb[:, 2, 0:n], in_=t_sb[:, 2, 0:n], func=AF.Sigmoid,
                                                       scale=GELU_C), R=[bt[2]], W=[bt[2]])
                    s.op("pool", lambda e: e.tensor_tensor(out=t_sb[:, 3, 0:n], in0=t_sb[:, 2, 0:n], in1=gr_, op=ALU.mult),
                         R=[bt[2], bhg[sl]], W=[bt[3]])
                    s.op("pool", lambda e: e.tensor_tensor(out=mix_sb[:, 2 + ch, t0:t0 + n], in0=t_sb[:, 3, 0:n],
                                                           in1=t_sb[:, 0, 0:n], op=ALU.mult),
                         R=[bt[3], bt[0]], W=[bmix[bi]])
            pi = 0
            for bi, (t0, n) in enumerate(BLKS1):
                garow = 5 if bi == 4 else 1
                for dc in range(8):
                    pb = pi % 8; pi += 1
                    for k in range(8):
                        mm(s, ps[:, pb, 0:n], wo_bf[:, k, dc * 128:(dc + 1) * 128], mix_sb[:, k, t0:t0 + n],
                           k == 0, k == 7, R=[bwobf[k], bmix[bi], bmixl], W=[bps[pb]])
                    s.op("dve", lambda e: e.scalar_tensor_tensor(out=x_sb[:, dc, t0:t0 + n], in0=ps[:, pb, 0:n],
                                                                 scalar=mod_sb[:, garow, dc:dc + 1],
                                                                 in1=x_sb[:, dc, t0:t0 + n], op0=ALU.mult, op1=ALU.add),
                         R=[bps[pb], bmod, bxb[bi]], W=[bxb[bi]])
            s.barrier()

        es_ = ExitStack()
        io4_sb = es_.enter_context(nc.sbuf_tensor("s2_io4", [128, 4], F32))
        gid_sb = es_.enter_context(nc.sbuf_tensor("s2_gid", [128, 17], F32))
        junk4_sb = es_.enter_context(nc.sbuf_tensor("s2_junk", [128, 4], F32))
        wd32_sb = es_.enter_context(nc.sbuf_tensor("s2_wd32", [128, 17, 32], F32))
        with (nc.sbuf_tensor("s2_sq", [128, 8, 512], BF16) as sq_sb,
              nc.sbuf_tensor("s2_rstd", [128, 2, 512], F32) as rstd_sb,
              nc.sbuf_tensor("s2_tmp", [128, 4, 512], F32) as tmp_sb,
              nc.sbuf_tensor("s2_hf", [128, 2, 8, 512], F32) as hf_sb,
              nc.sbuf_tensor("s2_ab", [128, 2, 8], F32) as ab_sb,
              nc.sbuf_tensor("s2_wge", [128, 8, 36], F32) as wge_sb,
              nc.sbuf_tensor("s2_bge", [128, 36], F32) as bge_sb,
              nc.sbuf_tensor("s2_id", [128, 128], F32) as id_sb,
              nc.sbuf_tensor("s2_rt", [128, 2, 128], F32) as rt_sb):
            bsq = s.buf("sq"); brs = [s.buf(f"rs{i}") for i in range(2)]
            btmp = [s.buf(f"tmp{i}") for i in range(4)]
            bhf = [s.buf(f"hf{i}") for i in range(2)]
            bab = s.buf("ab")
            bwge = s.buf("wge", cs); bbge = s.buf("bge", cs); bid = s.buf("id", cs)
            brt = [s.buf(f"rt{i}") for i in range(2)]
            s.dma("act", wge_sb[:], wge, W=[bwge])
            s.dma("act", bge_sb[:], bge, W=[bbge])
            s.dma("act", id_sb[:], ident, W=[bid])
            bio4 = s.buf("io4", cs); bgid = s.buf("gid", s.dsem("gid")); bjk = s.buf("junk4")
            s.dma("act", io4_sb[:], iota4, W=[bio4])
            s.op("pool", lambda e: e.memset(gid_sb[:], 0.0), W=[bgid])
            bwd32 = s.buf("wd32", s.dsem("wd32"))
            s.op("pool", lambda e: e.memset(wd32_sb[:], 0.0), W=[bwd32])
            for t in range(2):
                s.op("dve", lambda e: e.scalar_tensor_tensor(
                    out=ab_sb[:, t, :], in0=mod_sb[:, 2 + 4 * t, :], scalar=1.0, in1=mod_sb[:, 0, :],
                    op0=ALU.add, op1=ALU.mult), R=[bmod], W=[bab])
            ti_g = 0
            for bi, (t0, n) in enumerate(BLKS1):
                sl = bi % 2
                isctx = bi == 4
                s.op("act", lambda e: e.activation(out=sq_sb[:, :, 0:n], in_=x_sb[:, :, t0:t0 + n], func=AF.Square),
                     R=[bxb[bi]], W=[bsq])
                for k in range(8):
                    mm(s, ps[:, sl, 0:n], ones_sb[:], sq_sb[:, k, 0:n], k == 0, k == 7, R=[bones, bsq], W=[bps[sl]])
                s.op("act", lambda e: e.activation(out=rstd_sb[:, sl, 0:n], in_=ps[:, sl, 0:n], func=AF.Sqrt,
                                                   scale=1.0 / D, bias=EPS), R=[bps[sl]], W=[brs[sl]])
                s.op("dve", lambda e: e.reciprocal(out=rstd_sb[:, sl, 0:n], in_=rstd_sb[:, sl, 0:n]),
                     R=[brs[sl]], W=[brs[sl]])
                ai = 1 if isctx else 0
                shrow = 7 if isctx else 3
                for k in range(8):
                    tb = k % 4
                    s.op("dve", lambda e: e.scalar_tensor_tensor(
                        out=tmp_sb[:, tb, 0:n], in0=x_sb[:, k, t0:t0 + n], scalar=ab_sb[:, ai, k:k + 1],
                        in1=rstd_sb[:, sl, 0:n], op0=ALU.mult, op1=ALU.mult),
                        R=[bxb[bi], bab, brs[sl]], W=[btmp[tb]])
                    s.op("act", lambda e: e.activation(out=hf_sb[:, sl, k, 0:n], in_=tmp_sb[:, tb, 0:n],
                                                       func=AF.Identity, bias=mod_sb[:, shrow, k:k + 1], scale=1.0),
                         R=[btmp[tb], bmod], W=[bhf[sl]])
                s.op("pool", lambda e: e.tensor_copy(out=hl2_sb[:, :, t0:t0 + n], in_=hf_sb[:, sl, :, 0:n]),
                     R=[bhf[sl]], W=[bhl2[bi]])
                for tt in range((n + 127) // 128):
                    c0 = tt * 128
                    m = min(128, n - c0)
                    rs_ = ti_g % 2; ti_g += 1
                    pb = 2 + rs_
                    rt = rt_sb[0:m, rs_, :]
                    brr = brt[rs_]
                    for k in range(8):
                        mm(s, ps[0:m, pb, 0:36], hf_sb[:, sl, k, c0:c0 + m], wge_sb[:, k, :], k == 0, k == 7,
                           R=[bhf[sl], bwge], W=[bps[pb]])
                    V = lambda eng, fn, R_=(), W_=(): s.op(eng, fn, R=[brr] + list(R_), W=[brr] + list(W_))
                    lg = rt[:, 0:36]
                    s.op("dve", lambda e: e.tensor_tensor(out=lg, in0=ps[0:m, pb, 0:36], in1=bge_sb[0:m, :], op=ALU.add),
                         R=[bps[pb], bbge], W=[brr])
                    gmax = rt[:, 36:37]; ngmax = rt[:, 37:38]; sume = rt[:, 38:39]; gtop = rt[:, 39:40]
                    eg = rt[:, 40:44]; ohg = rt[:, 44:48]; sel = rt[:, 48:56]; top8 = rt[:, 56:64]
                    dd = rt[:, 64:65]; ed = rt[:, 65:66]; w1_ = rt[:, 66:67]; wt1 = rt[:, 67:68]; wt2 = rt[:, 68:69]
                    ea = rt[:, 72:80]; eb_ = rt[:, 80:88]; wd = rt[:, 96:128]
                    V("dve", lambda e: e.reduce_max(out=gmax, in_=lg[:, 0:4], axis=AX.X))
                    V("dve", lambda e: e.tensor_scalar(out=ngmax, in0=gmax, scalar1=-1.0, scalar2=None, op0=ALU.mult))
                    V("act", lambda e: e.activation(out=eg, in_=lg[:, 0:4], func=AF.Exp, bias=ngmax, scale=1.0,
                                                    accum_out=sume))
                    V("dve", lambda e: e.reciprocal(out=gtop, in_=sume))
                    V("dve", lambda e: e.tensor_scalar(out=ohg, in0=lg[:, 0:4], scalar1=gmax, scalar2=None,
                                                       op0=ALU.is_equal))
                    tgl = (t0 + c0) // 128
                    s.op("dve", lambda e: e.scalar_tensor_tensor(out=junk4_sb[0:m, :], in0=ohg, scalar=1.0,
                                                                 in1=io4_sb[0:m, :], op0=ALU.mult, op1=ALU.mult,
                                                                 accum_out=gid_sb[0:m, tgl:tgl + 1]),
                         R=[brr, bio4, bjk], W=[bjk, bgid])
                    V("dve", lambda e: e.tensor_scalar(out=sel, in0=lg[:, 4:12], scalar1=ohg[:, 0:1], scalar2=None,
                                                       op0=ALU.mult))
                    for g in range(1, 4):
                        V("dve", lambda e: e.scalar_tensor_tensor(out=sel, in0=lg[:, 4 + 8 * g:12 + 8 * g],
                                                                  scalar=ohg[:, g:g + 1], in1=sel,
                                                                  op0=ALU.mult, op1=ALU.add))
                    V("dve", lambda e: e.max(out=top8, in_=sel))
                    V("dve", lambda e: e.tensor_tensor(out=dd, in0=top8[:, 1:2], in1=top8[:, 0:1], op=ALU.subtract))
                    V("act", lambda e: e.activation(out=ed, in_=dd, func=AF.Exp))
                    V("dve", lambda e: e.tensor_scalar(out=w1_, in0=ed, scalar1=1.0, scalar2=None, op0=ALU.add))
                    V("dve", lambda e: e.reciprocal(out=w1_, in_=w1_))
                    V("dve", lambda e: e.tensor_tensor(out=wt1, in0=w1_, in1=gtop, op=ALU.mult))
                    V("dve", lambda e: e.tensor_tensor(out=wt2, in0=wt1, in1=ed, op=ALU.mult))
                    V("dve", lambda e: e.tensor_scalar(out=ea, in0=sel, scalar1=top8[:, 0:1], scalar2=wt1,
                                                       op0=ALU.is_equal, op1=ALU.mult))
                    V("dve", lambda e: e.tensor_scalar(out=eb_, in0=sel, scalar1=top8[:, 1:2], scalar2=wt2,
                                                       op0=ALU.is_equal, op1=ALU.mult))
                    V("dve", lambda e: e.tensor_tensor(out=ea, in0=ea, in1=eb_, op=ALU.add))
                    for g in range(4):
                        V("dve", lambda e: e.tensor_scalar(out=wd[:, 8 * g:8 * g + 8], in0=ea, scalar1=ohg[:, g:g + 1],
                                                           scalar2=None, op0=ALU.mult))
                    s.op("dve", lambda e: e.tensor_copy(out=wd32_sb[0:m, tgl, :], in_=wd), R=[brr], W=[bwd32])
                    pt = 4 + rs_
                    s.op("pe", lambda e: e.transpose(ps[0:32, pt, 0:m], wd, id_sb[0:m, 0:m]), R=[brr, bid], W=[bps[pt]])
                    s.op("act", lambda e: e.copy(out=wdt_sb[:, 0, t0 + c0:t0 + c0 + m], in_=ps[0:32, pt, 0:m]),
                         R=[bps[pt]], W=[bwdt[bi]])
                    s.op("dve", lambda e: e.tensor_tensor(out=wdt_sb[:, 1, t0 + c0:t0 + c0 + m], in0=ps[0:32, pt, 0:m],
                                                          in1=wdt_sb[:, 0, t0 + c0:t0 + c0 + m], op=ALU.subtract),
                         R=[bps[pt], bwdt[bi]], W=[bwdt[bi]])
            bxo = s.buf("xout", s.dsem("xout"))
            s.dma("sp", xo, x_sb[:], R=bxb, W=[bxo])
            s.dma("sp", hl2o, hl2_sb[:], R=bhl2, W=[bxo])
            s.dma("sp", wdto, wdt_sb[:], R=bwdt, W=[bxo])
            s.dma("sp", gido, gid_sb[:], R=[bgid], W=[bxo])
            s.dma("sp", wd32o, wd32_sb[:], R=[bwd32], W=[bxo])
            s.finish([bxo])
        es_.close()
    return nc


def build_p3b(ntb):
    NE = 8
    nblk_all = ntb // 512
    nh = 2 if ntb > 2048 else 1
    nblk = -(-nblk_all // nh)
    nth = nblk * 512
    nc = bass.Bass("TRN2", target_bir_lowering=False)
    din = lambda n, shp, dt: nc.dram_tensor(n, shp, dt, kind="ExternalInput").ap()
    hl2 = din("hl2", [128, 8, ntb], BF16)
    wdt = din("wdt", [8, 2, ntb], BF16)
    selc = din("selc", [8, NE * 128], BF16)
    w1 = din("w1", [NE, D, 512], F32)
    w3 = din("w3", [NE, D, 512], F32)
    w2 = din("w2", [NE, 512, D], F32)
    yo = nc.dram_tensor("yo", [128, 8, ntb], F32, kind="ExternalOutput").ap()
    s = Sched(nc)
    with (nc.psum_tensor("ps", [128, 8, 512], F32) as ps,
          nc.sbuf_tensor("y_sb", [128, 8, nth], F32) as y_sb,
          nc.sbuf_tensor("hl2_sb", [128, 8, nth], BF16) as hl2_sb,
          nc.sbuf_tensor("wdt_sb", [8, 2, ntb], BF16) as wdt_sb,
          nc.sbuf_tensor("s3_st", [128, 3, 2048], F32) as st_sb,
          nc.sbuf_tensor("s3_wb", [128, 2, 6, 2048], BF16) as wb_sb,
          nc.sbuf_tensor("s3_sel", [8, NE * 128], BF16) as sel_sb,
          nc.sbuf_tensor("s3_wbc", [128, 2, 512], F32) as wbc_sb,
          nc.sbuf_tensor("s3_sg", [128, 2, 512], F32) as sg_sb,
          nc.sbuf_tensor("s3_t", [128, 2, 512], F32) as t3_sb,
          nc.sbuf_tensor("s3_g", [128, 2, 4, 512], BF16) as g_sb):
        bps = [s.buf(f"ps{i}") for i in range(8)]
        cs = s.dsem("const")
        by = [s.buf(f"y{i}", s.dsem(f"y{i}")) for i in range(nblk)]
        bhl2 = [s.buf(f"hl2_{i}", s.dsem(f"hl{i}")) for i in range(nblk)]
        bwdt = s.buf("wdt", cs)
        bst = [s.buf(f"st{i}", s.dsem(f"st{i}")) for i in range(3)]
        bwb = [[s.buf(f"wb{a}_{p}") for p in range(6)] for a in range(2)]
        bsel = s.buf("sel", cs)
        bwbc = [s.buf(f"wbc{i}") for i in range(2)]
        bsg = [s.buf(f"sg{i}") for i in range(2)]
        bt3 = [s.buf(f"t3{i}") for i in range(2)]
        bg = [s.buf(f"g{i}") for i in range(2)]
        s.dma("act", sel_sb[:], selc, W=[bsel])
        s.dma("act", wdt_sb[:], wdt, W=[bwdt])
        w1v = w1.rearrange("e (kc p) f -> e p kc f", p=128)
        w3v = w3.rearrange("e (kc p) f -> e p kc f", p=128)
        w2v = w2.rearrange("e (fc p) d -> e p fc d", p=128)

        def piece_src(e, p):
            if p < 2:
                return w1v[e, :, 4 * p:4 * p + 4, :]
            if p < 4:
                return w3v[e, :, 4 * (p - 2):4 * (p - 2) + 4, :]
            return w2v[e, :, 2 * (p - 4):2 * (p - 4) + 2, :]

        def piece_dma(P):
            e, p = divmod(P, 6)
            if e >= NE * nh:
                return
            e = e % NE
            sl = P % 3
            dst = st_sb[:, sl, :]
            dst = dst.rearrange("q (a b) -> q a b", a=4) if p < 4 else dst.rearrange("q (a b) -> q a b", a=2)
            s.dma("sp", dst, piece_src(e, p), W=[bst[sl]])

        def piece_cast(P):
            e, p = divmod(P, 6)
            if e >= NE * nh:
                return
            sl = P % 3
            s.op("pool", lambda en: en.tensor_copy(out=wb_sb[:, e % 2, p, :], in_=st_sb[:, sl, :]),
                 R=[bst[sl]], W=[bwb[e % 2][p]])

        for P in range(3):
            piece_dma(P)
        for P in range(6):
            piece_cast(P)
            piece_dma(P + 3)
        gi = 0
        n = 512
        pend = [6 * 1 + i for i in range(6)]
        for vx in range(NE * nh):
            half, ex = divmod(vx, NE)
            a = vx % 2
            pend = [6 * (vx + 1) + i for i in range(6)]
            hb0 = half * nblk
            nb_h = min(nblk, nblk_all - hb0)
            if ex == 0:
                for bi in range(nb_h):
                    s.dma("act", hl2_sb[:, :, bi * 512:(bi + 1) * 512], hl2[:, :, (hb0 + bi) * 512:(hb0 + bi + 1) * 512],
                          W=[bhl2[bi]])
            for bi in range(nb_h):
                t0 = bi * 512
                tg = (hb0 + bi) * 512
                npc = (6 + nb_h - 1) // nb_h
                for P in pend[bi * npc:(bi + 1) * npc]:
                    piece_cast(P)
                    piece_dma(P + 3)
                wr = gi % 2
                gs = gi % 2
                gi += 1
                mm(s, ps[:, 6, 0:n], sel_sb[:, ex * 128:(ex + 1) * 128], wdt_sb[:, 0, tg:tg + n], True, False,
                   R=[bsel, bwdt], W=[bps[6]])
                mm(s, ps[:, 6, 0:n], sel_sb[:, ex * 128:(ex + 1) * 128], wdt_sb[:, 1, tg:tg + n], False, True,
                   R=[bsel, bwdt], W=[bps[6]])
                s.op("act", lambda e: e.copy(out=wbc_sb[:, wr, 0:n], in_=ps[:, 6, 0:n]), R=[bps[6]], W=[bwbc[wr]])
                for fc in range(4):
                    pr = fc % 2
                    for which in range(2):
                        bank = 2 * pr + which
                        for k in range(8):
                            wv = wb_sb[:, a, 2 * which + k // 4, :].rearrange("q (a b) -> q a b", a=4)
                            mm(s, ps[:, bank, 0:n], wv[:, k % 4, fc * 128:(fc + 1) * 128], hl2_sb[:, k, t0:t0 + n],
                               k == 0, k == 7, R=[bwb[a][2 * which + k // 4], bhl2[bi]], W=[bps[bank]])
                    s.op("act", lambda e: e.activation(out=sg_sb[:, pr, 0:n], in_=ps[:, 2 * pr, 0:n], func=AF.Silu),
                         R=[bps[2 * pr]], W=[bsg[pr]])
                    s.op("dve", lambda e: e.tensor_tensor(out=t3_sb[:, pr, 0:n], in0=ps[:, 2 * pr + 1, 0:n],
                                                          in1=sg_sb[:, pr, 0:n], op=ALU.mult),
                         R=[bps[2 * pr + 1], bsg[pr]], W=[bt3[pr]])
                    s.op("pool", lambda e: e.tensor_tensor(out=g_sb[:, gs, fc, 0:n], in0=t3_sb[:, pr, 0:n],
                                                           in1=wbc_sb[:, wr, 0:n], op=ALU.mult),
                         R=[bt3[pr], bwbc[wr]], W=[bg[gs]])
                for dc in range(8):
                    bank = 4 + dc % 2
                    for fc in range(4):
                        wv = wb_sb[:, a, 4 + fc // 2, :].rearrange("q (a b) -> q a b", a=2)
                        mm(s, ps[:, bank, 0:n], wv[:, fc % 2, dc * 128:(dc + 1) * 128], g_sb[:, gs, fc, 0:n],
                           fc == 0, fc == 3, R=[bwb[a][4 + fc // 2], bg[gs]], W=[bps[bank]])
                    if ex == 0:
                        s.op("dve", lambda e: e.tensor_copy(out=y_sb[:, dc, t0:t0 + n], in_=ps[:, bank, 0:n]),
                             R=[bps[bank]], W=[by[bi]])
                    else:
                        s.op("dve", lambda e: e.tensor_tensor(out=y_sb[:, dc, t0:t0 + n], in0=ps[:, bank, 0:n],
                                                              in1=y_sb[:, dc, t0:t0 + n], op=ALU.add),
                             R=[bps[bank], by[bi]], W=[by[bi]])
            if ex == NE - 1:
                for bi in range(nb_h):
                    s.dma("sp", yo[:, :, (hb0 + bi) * 512:(hb0 + bi + 1) * 512], y_sb[:, :, bi * 512:(bi + 1) * 512],
                          R=[by[bi]])
        s.finish(by)
    return nc


def build_pc(final):
    nc = bass.Bass("TRN2", target_bir_lowering=False)
    din = lambda n, shp, dt: nc.dram_tensor(n, shp, dt, kind="ExternalInput").ap()
    xT = din("xT", [128, 8, NT1], F32)
    yT = din("yT", [128, 8, NT1], F32)
    mod = din("mod", [128, 3, 8], F32)
    xo = nc.dram_tensor("xo", [128, 8, NT1], F32, kind="ExternalOutput").ap()
    s = Sched(nc)
    with (nc.psum_tensor("ps", [128, 2, 512], F32) as ps,
          nc.sbuf_tensor("x_sb", [128, 8, NT1], F32) as x_sb,
          nc.sbuf_tensor("y_sb", [128, 8, NT1], F32) as y_sb,
          nc.sbuf_tensor("mod_sb", [128, 3, 8], F32) as mod_sb,
          nc.sbuf_tensor("ones_sb", [128, 128], BF16) as ones_sb,
          nc.sbuf_tensor("sq_sb", [128, 8, 512], BF16) as sq_sb,
          nc.sbuf_tensor("rstd_sb", [128, 2, 512], F32) as rstd_sb):
        bxb = [s.buf(f"x{i}", s.dsem(f"x{i}")) for i in range(len(BLKS1))]
        byb = [s.buf(f"y{i}", s.dsem(f"yy{i}")) for i in range(len(BLKS1))]
        bmod = s.buf("mod", s.dsem("mod"))
        bones = s.buf("ones"); bsq = s.buf("sq"); brs = [s.buf(f"rs{i}") for i in range(2)]
        bps = [s.buf(f"ps{i}") for i in range(2)]
        s.dma("sp", mod_sb[:], mod, W=[bmod])
        s.op("pool", lambda e: e.memset(ones_sb[:], 1.0), W=[bones])
        for bi, (t0, n) in enumerate(BLKS1):
            s.dma("sp", x_sb[:, :, t0:t0 + n], xT[:, :, t0:t0 + n], W=[bxb[bi]])
            s.dma("act", y_sb[:, :, t0:t0 + n], yT[:, :, t0:t0 + n], W=[byb[bi]])
        for bi, (t0, n) in enumerate(BLKS1):
            garow = 1 if bi == 4 else 0
            sl = bi % 2
            for k in range(8):
                s.op("dve", lambda e: e.scalar_tensor_tensor(
                    out=x_sb[:, k, t0:t0 + n], in0=y_sb[:, k, t0:t0 + n], scalar=mod_sb[:, garow, k:k + 1],
                    in1=x_sb[:, k, t0:t0 + n], op0=ALU.mult, op1=ALU.add),
                    R=[byb[bi], bmod, bxb[bi]], W=[bxb[bi]])
            if final:
                s.op("act", lambda e: e.activation(out=sq_sb[:, :, 0:n], in_=x_sb[:, :, t0:t0 + n], func=AF.Square),
                     R=[bxb[bi]], W=[bsq])
                for k in range(8):
                    mm(s, ps[:, sl, 0:n], ones_sb[:], sq_sb[:, k, 0:n], k == 0, k == 7, R=[bones, bsq], W=[bps[sl]])
                s.op("act", lambda e: e.activation(out=rstd_sb[:, sl, 0:n], in_=ps[:, sl, 0:n], func=AF.Sqrt,
                                                   scale=1.0 / D, bias=EPS), R=[bps[sl]], W=[brs[sl]])
                s.op("dve", lambda e: e.reciprocal(out=rstd_sb[:, sl, 0:n], in_=rstd_sb[:, sl, 0:n]),
                     R=[brs[sl]], W=[brs[sl]])
                for k in range(8):
                    s.op("dve", lambda e: e.scalar_tensor_tensor(
                        out=x_sb[:, k, t0:t0 + n], in0=x_sb[:, k, t0:t0 + n], scalar=mod_sb[:, 2, k:k + 1],
                        in1=rstd_sb[:, sl, 0:n], op0=ALU.mult, op1=ALU.mult),
                        R=[bxb[bi], bmod, brs[sl]], W=[bxb[bi]])
            s.dma("sp", xo[:, :, t0:t0 + n], x_sb[:, :, t0:t0 + n], R=[bxb[bi]])
        s.finish(bxb)
    return nc


def run_p3(nc3, l, xl, xc, yna, ydf, hf, hb, fmf, mods_l, inp, g_final):
    wge = np.concatenate([inp["router_w_group"][l], inp["router_w_expert"][l]], axis=1)
    wge = np.ascontiguousarray(wge.reshape(8, 128, 36).transpose(1, 0, 2))
    bge = np.concatenate([inp["router_b_group"][l], inp["router_b_expert"][l]])
    bge = np.ascontiguousarray(np.tile(bge[None, :], (128, 1))).astype(np.float32)
    selc = np.zeros((32, NEXP, 128), np.float32)
    for e in range(NEXP):
        selc[e, e, :] = 1.0
    selc = selc.reshape(32, NEXP * 128).astype(ml_dtypes.bfloat16)
    ident = np.eye(128, dtype=np.float32)
    in_maps = []
    for i in range(NCORES):
        b, j = i // 4, i % 4
        lat = slice(2048 * j, 2048 * (j + 1))
        ctxs = slice(S + 64 * j, S + 64 * (j + 1))
        xx = np.concatenate([xl[b, lat], xc[b, 64 * j:64 * (j + 1)]], axis=0)
        na = np.concatenate([yna[b, lat], yna[b, ctxs]], axis=0)
        df = np.concatenate([ydf[b, lat], ydf[b, ctxs]], axis=0)
        nadf = np.concatenate([na, df], axis=1)
        nadf = np.ascontiguousarray(nadf.T.reshape(4, 128, NT1).transpose(1, 0, 2))
        def tk(a):
            aa = np.concatenate([a[:, lat], a[:, ctxs]], axis=1)
            return aa.reshape(4, 128, NT1).transpose(1, 0, 2)
        hgt = np.ascontiguousarray(np.concatenate([tk(hf[b]), tk(hb[b]), tk(fmf[b, 512:1024])], axis=1))
        m = mods_l
        rows = [inp["g_ffn"][l], m[b, 2048:3072], m[b, 4096:5120], m[b, 3072:4096], m[b, 5120:6144],
                m[2, 2048:3072], m[2, 4096:5120], m[2, 3072:4096], m[2, 5120:6144], g_final]
        mod = np.ascontiguousarray(np.stack([vec_pk(r) for r in rows], axis=1)).astype(np.float32)
        in_maps.append({"xT": chunkT(xx), "nadf": nadf, "hg": hgt, "wout": inp["w_out"][l], "mod": mod,
                        "wge": wge, "bge": bge, "selc": selc, "ident": ident,
                        "w1": inp["moe_w1"][l], "w3": inp["moe_w3"][l], "w2": inp["moe_w2"][l]})
    res = run_bass_kernel_spmd(nc3, in_maps, core_ids=list(range(NCORES)))
    xl2 = np.zeros_like(xl); xc2 = np.zeros_like(xc)
    for i in range(NCORES):
        b, j = i // 4, i % 4
        o = res.results[i]["xo"].transpose(1, 0, 2).reshape(D, NT1).T
        xl2[b, 2048 * j:2048 * (j + 1)] = o[:2048]
        xc2[b, 64 * j:64 * (j + 1)] = o[2048:]
    return xl2, xc2


def build_p3e(caps):
    NE = 4
    captot = sum(caps)
    nc = bass.Bass("TRN2", target_bir_lowering=False)
    din = lambda n, shp, dt: nc.dram_tensor(n, shp, dt, kind="ExternalInput").ap()
    xs = din("xs", [128, 8, captot], BF16)
    w1 = din("w1", [NE, D, 512], F32)
    w3 = din("w3", [NE, D, 512], F32)
    w2 = din("w2", [NE, 512, D], F32)
    yo = nc.dram_tensor("yo", [128, 8, captot], F32, kind="ExternalOutput").ap()
    s = Sched(nc)
    with (nc.psum_tensor("ps", [128, 8, 512], F32) as ps,
          nc.sbuf_tensor("x_sb", [128, 2, 8, 512], BF16) as x_sb,
          nc.sbuf_tensor("y_sb", [128, 2, 8, 512], F32) as y_sb,
          nc.sbuf_tensor("s3_st", [128, 6, 2048], F32) as st_sb,
          nc.sbuf_tensor("s3_wb", [128, 2, 6, 2048], BF16) as wb_sb,
          nc.sbuf_tensor("s3_sg", [128, 2, 512], F32) as sg_sb,
          nc.sbuf_tensor("s3_g", [128, 2, 4, 512], BF16) as g_sb):
        bps = [s.buf(f"ps{i}") for i in range(8)]
        bx = [s.buf(f"x{i}", s.dsem(f"x{i}")) for i in range(2)]
        by = [s.buf(f"y{i}", s.dsem(f"y{i}")) for i in range(2)]
        bst = [s.buf(f"st{i}", s.dsem(f"st{i}")) for i in range(6)]
        bwb = [[s.buf(f"wb{a}_{p}") for p in range(6)] for a in range(2)]
        bsg = [s.buf(f"sg{i}") for i in range(2)]
        bg = [s.buf(f"g{i}") for i in range(2)]
        w1v = w1.rearrange("e (kc p) f -> e p kc f", p=128)
        w3v = w3.rearrange("e (kc p) f -> e p kc f", p=128)
        w2v = w2.rearrange("e (fc p) d -> e p fc d", p=128)

        def piece_src(e, p):
            if p < 2:
                return w1v[e, :, 4 * p:4 * p + 4, :]
            if p < 4:
                return w3v[e, :, 4 * (p - 2):4 * (p - 2) + 4, :]
            return w2v[e, :, 2 * (p - 4):2 * (p - 4) + 2, :]

        def piece_dma(P):
            e, p = divmod(P, 6)
            if e >= NE:
                return
            sl = P % 6
            dst = st_sb[:, sl, :]
            dst = dst.rearrange("q (a b) -> q a b", a=4) if p < 4 else dst.rearrange("q (a b) -> q a b", a=2)
            s.dma("sp", dst, piece_src(e, p), W=[bst[sl]])

        def piece_cast(P):
            e, p = divmod(P, 6)
            if e >= NE:
                return
            sl = P % 6
            s.op("pool", lambda en: en.tensor_copy(out=wb_sb[:, e % 2, p, :], in_=st_sb[:, sl, :]),
                 R=[bst[sl]], W=[bwb[e % 2][p]])

        for P in range(6):
            piece_dma(P)
        for P in range(6):
            piece_cast(P)
            piece_dma(P + 6)
        gi = 0
        seg0 = 0
        for ex in range(NE):
            a = ex % 2
            pend = [6 * (ex + 1) + i for i in range(6)]
            blocks = [(seg0 + t, min(512, caps[ex] - t)) for t in range(0, caps[ex], 512)]
            seg0 += caps[ex]
            nb = len(blocks)
            npc = (6 + nb - 1) // nb
            for bi, (t0, n) in enumerate(blocks):
                for P in pend[bi * npc:(bi + 1) * npc]:
                    piece_cast(P)
                    piece_dma(P + 6)
                xs_ = gi % 2
                gs = gi % 2
                gi += 1
                s.dma("act", x_sb[:, xs_, :, 0:n], xs[:, :, t0:t0 + n], W=[bx[xs_]])
                for fc in range(4):
                    pr = fc % 2
                    for which in range(2):
                        bank = 2 * pr + which
                        for k in range(8):
                            wv = wb_sb[:, a, 2 * which + k // 4, :].rearrange("q (a b) -> q a b", a=4)
                            mm(s, ps[:, bank, 0:n], wv[:, k % 4, fc * 128:(fc + 1) * 128], x_sb[:, xs_, k, 0:n],
                               k == 0, k == 7, R=[bwb[a][2 * which + k // 4], bx[xs_]], W=[bps[bank]])
                    s.op("act", lambda e: e.activation(out=sg_sb[:, pr, 0:n], in_=ps[:, 2 * pr, 0:n], func=AF.Silu),
                         R=[bps[2 * pr]], W=[bsg[pr]])
                    s.op("dve", lambda e: e.tensor_tensor(out=g_sb[:, gs, fc, 0:n], in0=ps[:, 2 * pr + 1, 0:n],
                                                          in1=sg_sb[:, pr, 0:n], op=ALU.mult),
                         R=[bps[2 * pr + 1], bsg[pr]], W=[bg[gs]])
                for dc in range(8):
                    bank = 4 + dc % 4
                    for fc in range(4):
                        wv = wb_sb[:, a, 4 + fc // 2, :].rearrange("q (a b) -> q a b", a=2)
                        mm(s, ps[:, bank, 0:n], wv[:, fc % 2, dc * 128:(dc + 1) * 128], g_sb[:, gs, fc, 0:n],
                           fc == 0, fc == 3, R=[bwb[a][4 + fc // 2], bg[gs]], W=[bps[bank]])
                    if dc % 2 == 0:
                        s.op("dve", lambda e: e.tensor_copy(out=y_sb[:, xs_, dc, 0:n], in_=ps[:, bank, 0:n]),
                             R=[bps[bank]], W=[by[xs_]])
                    else:
                        s.op("act", lambda e: e.copy(out=y_sb[:, xs_, dc, 0:n], in_=ps[:, bank, 0:n]),
                             R=[bps[bank]], W=[by[xs_]])
                s.dma("sp", yo[:, :, t0:t0 + n], y_sb[:, xs_, :, 0:n], R=[by[xs_]])
        s.finish(by)
    return nc


def build_pc2(final):
    nc = bass.Bass("TRN2", target_bir_lowering=False)
    din = lambda n, shp, dt: nc.dram_tensor(n, shp, dt, kind="ExternalInput").ap()
    xT = din("xT", [128, 8, NT1], F32)
    yA = din("yA", [128, 8, NT1], F32)
    yB = din("yB", [128, 8, NT1], F32)
    wab = din("wab", [128, 2, NT1], F32)
    mod = din("mod", [128, 3, 8], F32)
    xo = nc.dram_tensor("xo", [128, 8, NT1], F32, kind="ExternalOutput").ap()
    s = Sched(nc)
    with (nc.psum_tensor("ps", [128, 2, 512], F32) as ps,
          nc.sbuf_tensor("x_sb", [128, 8, NT1], F32) as x_sb,
          nc.sbuf_tensor("ya_sb", [128, 2, 8, 512], F32) as ya_sb,
          nc.sbuf_tensor("yb_sb", [128, 2, 8, 512], F32) as yb_sb,
          nc.sbuf_tensor("wab_sb", [128, 2, NT1], F32) as wab_sb,
          nc.sbuf_tensor("mod_sb", [128, 3, 8], F32) as mod_sb,
          nc.sbuf_tensor("ones_sb", [128, 128], BF16) as ones_sb,
          nc.sbuf_tensor("sq_sb", [128, 8, 512], BF16) as sq_sb,
          nc.sbuf_tensor("rstd_sb", [128, 2, 512], F32) as rstd_sb):
        bxb = [s.buf(f"x{i}", s.dsem(f"x{i}")) for i in range(len(BLKS1))]
        bya = [s.buf(f"ya{i}", s.dsem(f"ya{i}")) for i in range(2)]
        byb = [s.buf(f"yb{i}", s.dsem(f"yb{i}")) for i in range(2)]
        bmod = s.buf("mod", s.dsem("mod"))
        bwab = s.buf("wab", bmod.dsem)
        bones = s.buf("ones"); bsq = s.buf("sq"); brs = [s.buf(f"rs{i}") for i in range(2)]
        bps = [s.buf(f"ps{i}") for i in range(2)]
        s.dma("sp", mod_sb[:], mod, W=[bmod])
        s.dma("sp", wab_sb[:], wab, W=[bwab])
        s.op("pool", lambda e: e.memset(ones_sb[:], 1.0), W=[bones])
        for bi, (t0, n) in enumerate(BLKS1):
            s.dma("sp", x_sb[:, :, t0:t0 + n], xT[:, :, t0:t0 + n], W=[bxb[bi]])
        for bi, (t0, n) in enumerate(BLKS1):
            garow = 1 if bi == 4 else 0
            sl = bi % 2
            s.dma("act", ya_sb[:, sl, :, 0:n], yA[:, :, t0:t0 + n], W=[bya[sl]])
            s.dma("act", yb_sb[:, sl, :, 0:n], yB[:, :, t0:t0 + n], W=[byb[sl]])
            for k in range(8):
                s.op("pool", lambda e: e.tensor_tensor(out=ya_sb[:, sl, k, 0:n], in0=ya_sb[:, sl, k, 0:n],
                                                       in1=wab_sb[:, 0, t0:t0 + n], op=ALU.mult),
                     R=[bya[sl], bwab], W=[bya[sl]])
                s.op("dve", lambda e: e.tensor_tensor(out=yb_sb[:, sl, k, 0:n], in0=yb_sb[:, sl, k, 0:n],
                                                      in1=wab_sb[:, 1, t0:t0 + n], op=ALU.mult),
                     R=[byb[sl], bwab], W=[byb[sl]])
                s.op("dve", lambda e: e.tensor_tensor(out=ya_sb[:, sl, k, 0:n], in0=ya_sb[:, sl, k, 0:n],
                                                      in1=yb_sb[:, sl, k, 0:n], op=ALU.add),
                     R=[bya[sl], byb[sl]], W=[bya[sl]])
                s.op("dve", lambda e: e.scalar_tensor_tensor(
                    out=x_sb[:, k, t0:t0 + n], in0=ya_sb[:, sl, k, 0:n], scalar=mod_sb[:, garow, k:k + 1],
                    in1=x_sb[:, k, t0:t0 + n], op0=ALU.mult, op1=ALU.add),
                    R=[bya[sl], bmod, bxb[bi]], W=[bxb[bi]])
            if final:
                s.op("act", lambda e: e.activation(out=sq_sb[:, :, 0:n], in_=x_sb[:, :, t0:t0 + n], func=AF.Square),
                     R=[bxb[bi]], W=[bsq])
                for k in range(8):
                    mm(s, ps[:, sl, 0:n], ones_sb[:], sq_sb[:, k, 0:n], k == 0, k == 7, R=[bones, bsq], W=[bps[sl]])
                s.op("act", lambda e: e.activation(out=rstd_sb[:, sl, 0:n], in_=ps[:, sl, 0:n], func=AF.Sqrt,
                                                   scale=1.0 / D, bias=EPS), R=[bps[sl]], W=[brs[sl]])
                s.op("dve", lambda e: e.reciprocal(out=rstd_sb[:, sl, 0:n], in_=rstd_sb[:, sl, 0:n]),
                     R=[brs[sl]], W=[brs[sl]])
                for k in range(8):
                    s.op("dve", lambda e: e.scalar_tensor_tensor(
                        out=x_sb[:, k, t0:t0 + n], in0=x_sb[:, k, t0:t0 + n], scalar=mod_sb[:, 2, k:k + 1],
                        in1=rstd_sb[:, sl, 0:n], op0=ALU.mult, op1=ALU.mult),
                        R=[bxb[bi], bmod, brs[sl]], W=[bxb[bi]])
            s.dma("sp", xo[:, :, t0:t0 + n], x_sb[:, :, t0:t0 + n], R=[bxb[bi]])
        s.finish(bxb)
    return nc


def run_p3_expert(l, xl, xc, yna, ydf, hf, hb, fmf, mods_l, inp, g_final, final):
    in_maps = p3_inmaps_common(l, xl, xc, yna, ydf, hf, hb, fmf, mods_l, inp, g_final)
    resa = run_bass_kernel_spmd(build_p3a(), in_maps, core_ids=list(range(NCORES))).results
    NTT = NCORES * NT1
    HL2 = np.zeros((D, NTT), ml_dtypes.bfloat16)
    WD = np.zeros((NTT, 32), np.float32)
    for i in range(NCORES):
        HL2[:, i * NT1:(i + 1) * NT1] = resa[i]["hl2o"].transpose(1, 0, 2).reshape(D, NT1)
        WD[i * NT1:(i + 1) * NT1] = resa[i]["wd32o"].transpose(1, 0, 2).reshape(17 * 128, 32)[:NT1]
    top2 = np.sort(np.argpartition(-WD, 1, axis=1)[:, :2], axis=1)
    eA, eB = top2[:, 0], top2[:, 1]
    ar = np.arange(NTT)
    wA = WD[ar, eA]; wB = WD[ar, eB]
    tokE = []
    for e in range(32):
        tokE.append(np.nonzero((eA == e) | (eB == e))[0])
    order = np.argsort(-np.array([len(t) for t in tokE]), kind="stable")
    assign = [[int(order[8 * sl + c]) for sl in range(4)] for c in range(NCORES)]
    caps = []
    for sl in range(4):
        mx = max(len(tokE[assign[c][sl]]) for c in range(NCORES))
        caps.append(max(128, int(-(-mx // 128) * 128)))
    captot = sum(caps)
    mapse = []
    for c in range(NCORES):
        xsa = np.zeros((D, captot), ml_dtypes.bfloat16)
        o = 0
        for sl in range(4):
            tk_ = tokE[assign[c][sl]]
            xsa[:, o:o + len(tk_)] = HL2[:, tk_]
            o += caps[sl]
        mapse.append({"xs": np.ascontiguousarray(xsa.reshape(8, 128, captot).transpose(1, 0, 2)),
                      "w1": np.ascontiguousarray(inp["moe_w1"][l][assign[c]]),
                      "w3": np.ascontiguousarray(inp["moe_w3"][l][assign[c]]),
                      "w2": np.ascontiguousarray(inp["moe_w2"][l][assign[c]])})
    rese = run_bass_kernel_spmd(build_p3e(caps), mapse, core_ids=list(range(NCORES))).results
    YA = np.zeros((D, NTT), np.float32)
    YB = np.zeros((D, NTT), np.float32)
    for c in range(NCORES):
        yy = rese[c]["yo"].transpose(1, 0, 2).reshape(D, captot)
        o = 0
        for sl in range(4):
            e = assign[c][sl]
            tk_ = tokE[e]
            cols = yy[:, o:o + len(tk_)]
            isA = eA[tk_] == e
            YA[:, tk_[isA]] = cols[:, isA]
            YB[:, tk_[~isA]] = cols[:, ~isA]
            o += caps[sl]
    mapsc = []
    for i in range(NCORES):
        b = i // 4
        sl_ = slice(i * NT1, (i + 1) * NT1)
        modc = np.stack([vec_pk(mods_l[b, 5120:6144]), vec_pk(mods_l[2, 5120:6144]), vec_pk(g_final)], axis=1)
        wab = np.stack([np.tile(wA[sl_][None, :], (128, 1)), np.tile(wB[sl_][None, :], (128, 1))], axis=1)
        mapsc.append({"xT": resa[i]["xo"], "mod": np.ascontiguousarray(modc).astype(np.float32),
                      "wab": np.ascontiguousarray(wab).astype(np.float32),
                      "yA": np.ascontiguousarray(YA[:, sl_].reshape(8, 128, NT1).transpose(1, 0, 2)),
                      "yB": np.ascontiguousarray(YB[:, sl_].reshape(8, 128, NT1).transpose(1, 0, 2))})
    resc = run_bass_kernel_spmd(build_pc2(final), mapsc, core_ids=list(range(NCORES))).results
    xl2 = np.zeros_like(xl); xc2 = np.zeros_like(xc)
    for i in range(NCORES):
        b, j = i // 4, i % 4
        o = resc[i]["xo"].transpose(1, 0, 2).reshape(D, NT1).T
        xl2[b, 2048 * j:2048 * (j + 1)] = o[:2048]
        xc2[b, 64 * j:64 * (j + 1)] = o[2048:]
    return xl2, xc2


def p3_inmaps_common(l, xl, xc, yna, ydf, hf, hb, fmf, mods_l, inp, g_final):
    wge = np.concatenate([inp["router_w_group"][l], inp["router_w_expert"][l]], axis=1)
    wge = np.ascontiguousarray(wge.reshape(8, 128, 36).transpose(1, 0, 2))
    bge = np.concatenate([inp["router_b_group"][l], inp["router_b_expert"][l]])
    bge = np.ascontiguousarray(np.tile(bge[None, :], (128, 1))).astype(np.float32)
    ident = np.eye(128, dtype=np.float32)
    iota4 = np.ascontiguousarray(np.tile(np.arange(4, dtype=np.float32)[None, :], (128, 1)))
    in_maps = []
    for i in range(NCORES):
        b, j = i // 4, i % 4
        lat = slice(2048 * j, 2048 * (j + 1))
        ctxs = slice(S + 64 * j, S + 64 * (j + 1))
        xx = np.concatenate([xl[b, lat], xc[b, 64 * j:64 * (j + 1)]], axis=0)
        na = np.concatenate([yna[b, lat], yna[b, ctxs]], axis=0)
        df = np.concatenate([ydf[b, lat], ydf[b, ctxs]], axis=0)
        nadf = np.concatenate([na, df], axis=1)
        nadf = np.ascontiguousarray(nadf.T.reshape(4, 128, NT1).transpose(1, 0, 2))

        def tk(a):
            aa = np.concatenate([a[:, lat], a[:, ctxs]], axis=1)
            return aa.reshape(4, 128, NT1).transpose(1, 0, 2)
        hgt = np.ascontiguousarray(np.concatenate([tk(hf[b]), tk(hb[b]), tk(fmf[b, 512:1024])], axis=1))
        m = mods_l
        rows = [inp["g_ffn"][l], m[b, 2048:3072], m[b, 4096:5120], m[b, 3072:4096], m[b, 5120:6144],
                m[2, 2048:3072], m[2, 4096:5120], m[2, 3072:4096], m[2, 5120:6144], g_final]
        mod = np.ascontiguousarray(np.stack([vec_pk(r) for r in rows], axis=1)).astype(np.float32)
        in_maps.append({"xT": chunkT(xx), "nadf": nadf, "hg": hgt, "wout": inp["w_out"][l], "mod": mod,
                        "wge": wge, "bge": bge, "ident": ident, "iota4": iota4})
    return in_maps


def run_p3_sparse(l, xl, xc, yna, ydf, hf, hb, fmf, mods_l, inp, g_final, final):
    in_maps = p3_inmaps_common(l, xl, xc, yna, ydf, hf, hb, fmf, mods_l, inp, g_final)
    resa = run_bass_kernel_spmd(build_p3a(), in_maps, core_ids=list(range(NCORES))).results
    NTT = NCORES * NT1
    HL2 = np.zeros((D, NTT), ml_dtypes.bfloat16)
    WDT = np.zeros((32, 2, NTT), ml_dtypes.bfloat16)
    gid = np.zeros(NTT, np.int64)
    for i in range(NCORES):
        HL2[:, i * NT1:(i + 1) * NT1] = resa[i]["hl2o"].transpose(1, 0, 2).reshape(D, NT1)
        WDT[:, :, i * NT1:(i + 1) * NT1] = resa[i]["wdto"]
        g = resa[i]["gido"]
        gid[i * NT1:(i + 1) * NT1] = np.rint(g.T.reshape(-1)[:NT1]).astype(np.int64)
    toks = [np.nonzero(gid == g)[0] for g in range(4)]
    ncg = [1, 1, 1, 1]
    for _ in range(NCORES - 4):
        gbig = max(range(4), key=lambda g: len(toks[g]) / ncg[g])
        ncg[gbig] += 1
    idxs = []
    cgroup = []
    for g in range(4):
        parts = np.array_split(toks[g], ncg[g])
        for pp in parts:
            idxs.append(pp); cgroup.append(g)
    ntb = max(512, int(-(-max(len(ix) for ix in idxs) // 512) * 512))
    sel8 = np.zeros((8, 8, 128), np.float32)
    for e in range(8):
        sel8[e, e, :] = 1.0
    sel8 = sel8.reshape(8, 1024).astype(ml_dtypes.bfloat16)
    mapsb = []
    for c in range(NCORES):
        g = cgroup[c]
        ix = idxs[c]
        h2 = np.zeros((D, ntb), ml_dtypes.bfloat16)
        h2[:, :len(ix)] = HL2[:, ix]
        wd = np.zeros((8, 2, ntb), ml_dtypes.bfloat16)
        wd[:, :, :len(ix)] = WDT[8 * g:8 * g + 8][:, :, ix]
        mapsb.append({"hl2": np.ascontiguousarray(h2.reshape(8, 128, ntb).transpose(1, 0, 2)), "wdt": wd, "selc": sel8,
                      "w1": np.ascontiguousarray(inp["moe_w1"][l][8 * g:8 * g + 8]),
                      "w3": np.ascontiguousarray(inp["moe_w3"][l][8 * g:8 * g + 8]),
                      "w2": np.ascontiguousarray(inp["moe_w2"][l][8 * g:8 * g + 8])})
    resb = run_bass_kernel_spmd(build_p3b(ntb), mapsb, core_ids=list(range(NCORES))).results
    Y = np.zeros((D, NTT), np.float32)
    for c in range(NCORES):
        ix = idxs[c]
        Y[:, ix] = resb[c]["yo"].transpose(1, 0, 2).reshape(D, ntb)[:, :len(ix)]
    mapsc = []
    for i in range(NCORES):
        b = i // 4
        modc = np.stack([vec_pk(mods_l[b, 5120:6144]), vec_pk(mods_l[2, 5120:6144]), vec_pk(g_final)], axis=1)
        mapsc.append({"xT": resa[i]["xo"], "mod": np.ascontiguousarray(modc).astype(np.float32),
                      "yT": np.ascontiguousarray(Y[:, i * NT1:(i + 1) * NT1].reshape(8, 128, NT1).transpose(1, 0, 2))})
    resc = run_bass_kernel_spmd(build_pc(final), mapsc, core_ids=list(range(NCORES))).results
    xl2 = np.zeros_like(xl); xc2 = np.zeros_like(xc)
    for i in range(NCORES):
        b, j = i // 4, i % 4
        o = resc[i]["xo"].transpose(1, 0, 2).reshape(D, NT1).T
        xl2[b, 2048 * j:2048 * (j + 1)] = o[:2048]
        xc2[b, 64 * j:64 * (j + 1)] = o[2048:]
    return xl2, xc2


def kernel(**inputs):
    inp = {k: np.asarray(v) for k, v in inputs.items()}
    x = np.ascontiguousarray(inp["x"], dtype=np.float32)
    ctx = np.ascontiguousarray(inp["ctx"], dtype=np.float32)
    mods = run_p0(inp["c"], inp["c_ctx"], inp["w_ada"], inp["b_ada"])
    cosT, sinT = rope_tables()
    xl, xc = x, ctx
    for l in range(DEPTH):
        fmb, fmf, tm = run_p1(build_p1(), xl, xc, mods[l], inp["g_mix"][l], inp["w_in"][l], cosT, sinT)
        lam_init = 0.8 - 0.6 * math.exp(-0.3 * l)
        yna, ydf, hf, hb = run_p2(build_p2(lam_init), l, fmb, fmf, tm, inp)
        xl, xc = run_p3_expert(l, xl, xc, yna, ydf, hf, hb, fmf, mods[l], inp, inp["g_final"], l == DEPTH - 1)
    return np.ascontiguousarray(xl, dtype=np.float32)
```

```python
import math
from contextlib import ExitStack
import numpy as np
import ml_dtypes
import concourse.bass as bass
import concourse.mybir as mybir
from concourse.bass_utils import run_bass_kernel_spmd

F32 = mybir.dt.float32
BF16 = mybir.dt.bfloat16
I32 = mybir.dt.int32
U32 = mybir.dt.uint32
AF = mybir.ActivationFunctionType
ALU = mybir.AluOpType
AX = mybir.AxisListType

NCORES = 8
D = 1024
B = 2
S = 8192
L = 256
DEPTH = 4
GRID_W = 64
EPS = 1e-6


class Buf:
    __slots__ = ("name", "w", "r", "dsem")

    def __init__(self, name, dsem=None):
        self.name = name
        self.w = None
        self.r = []
        self.dsem = dsem


class DmaSem:
    def __init__(self, sched, name):
        self.sem = sched.nc.alloc_semaphore(name)
        self.key = ("dma", name)
        self.total = 0
        sched.sems[self.key] = self


class Sched:
    def __init__(self, nc):
        self.nc = nc
        self.eng = {"pe": nc.tensor, "dve": nc.vector, "act": nc.scalar,
                    "pool": nc.gpsimd, "sp": nc.sync}
        self.sems = {}
        self.esem = {}
        self.cnt = {}
        for k in self.eng:
            self.esem[k] = nc.alloc_semaphore("e_" + k)
            self.cnt[k] = 0
        self.seen = {}
        self.nbuf = 0
        self.out_tokens = []

    def buf(self, name=None, dsem=None):
        self.nbuf += 1
        return Buf(name or f"b{self.nbuf}", dsem)

    def dsem(self, name):
        return DmaSem(self, name)

    def _semof(self, key):
        if key[0] == "dma":
            return self.sems[key].sem
        return self.esem[key[0]]

    def _wait(self, engname, deps):
        e = self.eng[engname]
        for key, val in deps.items():
            if key[0] == "dma":
                val = max(val, 0)
            if self.seen.get((engname, key), 0) >= val:
                continue
            self.seen[(engname, key)] = val
            e.wait_ge(self._semof(key), val)

    def _deps(self, R, W):
        deps = {}

        def add(tok):
            if tok is None:
                return
            key, val = tok
            if key[0] == "dma":
                val = self.sems[key].total
            if deps.get(key, 0) < val:
                deps[key] = val
        for b in R:
            add(b.w)
        for b in W:
            add(b.w)
            for t in b.r:
                add(t)
        return deps

    def _commit(self, tok, R, W):
        for b in R:
            b.r.append(tok)
        for b in W:
            b.w = tok
            b.r = []

    def op(self, engname, fn, R=(), W=()):
        deps = self._deps(R, W)
        if engname == "pe":
            deps.pop(("pe",), None)
        self._wait(engname, deps)
        ins = fn(self.eng[engname])
        self.cnt[engname] += 1
        ins.then_inc(self.esem[engname], 1)
        tok = ((engname,), self.cnt[engname])
        self._commit(tok, R, W)
        return tok

    def dma(self, q, out, in_, R=(), W=(), sem=None, **kw):
        deps = self._deps(R, W)
        self._wait(q, deps)
        ds = sem
        if ds is None:
            for b in list(W) + list(R):
                if b.dsem is not None:
                    ds = b.dsem
                    break
        assert ds is not None, "dma needs a DmaSem"
        ins = self.eng[q].dma_start(out=out, in_=in_, **kw)
        ds.total += 16
        ins.then_inc(ds.sem, 16)
        tok = (ds.key, ds.total)
        self._commit(tok, R, W)
        return tok

    def barrier(self, bufs=()):
        deps = {}
        for k in self.eng:
            if self.cnt[k]:
                deps[(k,)] = self.cnt[k]
        for key, ds in self.sems.items():
            if ds.total:
                deps[key] = ds.total
        for k in self.eng:
            d = {kk: v for kk, v in deps.items() if kk != (k,)}
            self._wait(k, d)

    def coll(self, kind, ins, outs, R=(), W=(), groups=None):
        deps = self._deps(R, W)
        self._wait("pool", deps)
        ds = None
        for b in list(W) + list(R):
            if b.dsem is not None:
                ds = b.dsem
                break
        g = groups or [[0, 1, 2, 3], [4, 5, 6, 7]]
        ins_ = self.nc.gpsimd.collective_compute(kind, ALU.bypass, replica_groups=g, ins=ins, outs=outs)
        ds.total += 16
        ins_.then_inc(ds.sem, 16)
        tok = (ds.key, ds.total)
        self._commit(tok, R, W)
        return tok

    def finish(self, bufs, engname="sp"):
        deps = {}
        for b in bufs:
            for tok in ([b.w] if b.w else []) + b.r:
                key, val = tok
                if key[0] == "dma":
                    val = self.sems[key].total
                deps[key] = max(deps.get(key, 0), val)
        self._wait(engname, deps)


def mm(s, out, lhsT, rhs, start, stop, R, W):
    return s.op("pe", lambda e: e.matmul(out, lhsT, rhs, start=start, stop=stop), R=R, W=W)


def build_p0():
    nc = bass.Bass("TRN2", target_bir_lowering=False)
    NCOL = 3072
    NJ = NCOL // 128
    cT = nc.dram_tensor("cT", [128, 8, 4], F32, kind="ExternalInput").ap()
    w = nc.dram_tensor("w", [D, NCOL], F32, kind="ExternalInput").ap()
    bvec = nc.dram_tensor("bvec", [128, NJ], F32, kind="ExternalInput").ap()
    out = nc.dram_tensor("out", [128, NJ, 4], F32, kind="ExternalOutput").ap()
    s = Sched(nc)
    wv = w.rearrange("(kc p) n -> p kc n", p=128)
    with (nc.sbuf_tensor("w_sb", [128, 8, NCOL], F32) as w_sb,
          nc.sbuf_tensor("c_sb", [128, 8, 4], F32) as c_sb,
          nc.sbuf_tensor("s_sb", [128, 8, 4], F32) as s_sb,
          nc.sbuf_tensor("b_sb", [128, NJ], F32) as b_sb,
          nc.sbuf_tensor("r_sb", [128, NJ, 4], F32) as r_sb,
          nc.psum_tensor("ps", [128, 8, 512], F32) as ps):
        bw = [s.buf(f"w{k}", s.dsem(f"w{k}")) for k in range(8)]
        bc = s.buf("c", s.dsem("c"))
        bb = s.buf("b", bc.dsem)
        bs = s.buf("s")
        br = s.buf("r", s.dsem("r"))
        bps = [s.buf(f"ps{i}") for i in range(8)]
        s.dma("sp", c_sb[:], cT, W=[bc])
        s.dma("sp", b_sb[:], bvec, W=[bb])
        for k in range(8):
            s.dma("sp" if k % 2 == 0 else "act", w_sb[:, k, :], wv[:, k, :], W=[bw[k]])
        s.op("act", lambda e: e.activation(out=s_sb[:], in_=c_sb[:], func=AF.Silu), R=[bc], W=[bs])
        for j in range(NJ):
            pb = bps[j % 8]
            for k in range(8):
                mm(s, ps[:, j % 8, 0:4], w_sb[:, k, j * 128:(j + 1) * 128], s_sb[:, k, :],
                   k == 0, k == 7, R=[bw[k], bs], W=[pb])
            s.op("dve", lambda e: e.tensor_scalar(out=r_sb[:, j, :], in0=ps[:, j % 8, 0:4],
                                                  scalar1=b_sb[:, j:j + 1], scalar2=None, op0=ALU.add),
                 R=[pb, bb], W=[br])
        s.dma("sp", out, r_sb[:], R=[br])
        s.finish([br])
    return nc


def silu_np_layout_c(c, c_ctx):
    cc = np.stack([c[0], c[1], c_ctx, c_ctx], axis=1)
    return np.ascontiguousarray(cc.reshape(8, 128, 4).transpose(1, 0, 2))


def run_p0(c, c_ctx, w_ada, b_ada):
    nc = build_p0()
    cT = silu_np_layout_c(c, c_ctx)
    in_maps = []
    for i in range(NCORES):
        l, h = i // 2, i % 2
        in_maps.append({
            "cT": cT,
            "w": np.ascontiguousarray(w_ada[l][:, h * 3072:(h + 1) * 3072]),
            "bvec": np.ascontiguousarray(b_ada[l][h * 3072:(h + 1) * 3072].reshape(24, 128).T),
        })
    res = run_bass_kernel_spmd(nc, in_maps, core_ids=list(range(NCORES)))
    mods = np.zeros((DEPTH, 3, 6 * D), np.float32)
    for i in range(NCORES):
        l, h = i // 2, i % 2
        o = res.results[i]["out"]
        m = o.transpose(1, 0, 2).reshape(3072, 4)
        mods[l, :, h * 3072:(h + 1) * 3072] = m[:, :3].T
    return mods


NT1 = 2112
NW1 = 3072
BLKS1 = [(0, 512), (512, 512), (1024, 512), (1536, 512), (2048, 64)]


def build_p1():
    nc = bass.Bass("TRN2", target_bir_lowering=False)
    xT = nc.dram_tensor("xT", [128, 8, NT1], F32, kind="ExternalInput").ap()
    w = nc.dram_tensor("w", [D, NW1], F32, kind="ExternalInput").ap()
    gsc = nc.dram_tensor("gsc", [128, 5, 8], F32, kind="ExternalInput").ap()
    cosT = nc.dram_tensor("cosT", [128, 2048], F32, kind="ExternalInput").ap()
    sinT = nc.dram_tensor("sinT", [128, 2048], F32, kind="ExternalInput").ap()
    fmb = nc.dram_tensor("fmb", [128, 8, NT1], BF16, kind="ExternalOutput").ap()
    fmf = nc.dram_tensor("fmf", [128, 8, NT1], F32, kind="ExternalOutput").ap()
    tm = nc.dram_tensor("tm", [NT1, 512], BF16, kind="ExternalOutput").ap()
    s = Sched(nc)
    wv = w.rearrange("(kc p) n -> p kc n", p=128)
    with (nc.sbuf_tensor("x_sb", [128, 2, 8, 512], F32) as x_sb,
          nc.sbuf_tensor("h_sb", [128, 8, NT1], BF16) as h_sb,
          nc.sbuf_tensor("w_bf", [128, 8, NW1], BF16) as w_bf,
          nc.sbuf_tensor("w_st", [128, 2, NW1], F32) as w_st,
          nc.sbuf_tensor("o_sb", [128, 4, 512], F32) as o_sb,
          nc.sbuf_tensor("ob_sb", [128, 4, 512], BF16) as ob_sb,
          nc.sbuf_tensor("cos_sb", [128, 2048], F32) as cos_sb,
          nc.sbuf_tensor("sin_sb", [128, 2048], F32) as sin_sb,
          nc.sbuf_tensor("gsc_sb", [128, 5, 8], F32) as gsc_sb,
          nc.sbuf_tensor("ab_sb", [128, 2, 8], F32) as ab_sb,
          nc.sbuf_tensor("sq_sb", [128, 8, 512], BF16) as sq_sb,
          nc.sbuf_tensor("ones_sb", [128, 128], BF16) as ones_sb,
          nc.sbuf_tensor("rstd_sb", [128, 2, 512], F32) as rstd_sb,
          nc.sbuf_tensor("tmp_sb", [128, 4, 512], F32) as tmp_sb,
          nc.psum_tensor("ps", [128, 8, 512], F32) as ps):
        bx = [s.buf(f"x{i}", s.dsem(f"x{i}")) for i in range(2)]
        bh = [s.buf(f"h{i}") for i in range(len(BLKS1))]
        bwst = [s.buf(f"wst{i}", s.dsem(f"wst{i}")) for i in range(2)]
        bwbf = [s.buf(f"wbf{k}") for k in range(8)]
        bo = [s.buf(f"o{i}", s.dsem(f"o{i}")) for i in range(4)]
        bob = [s.buf(f"ob{i}", s.dsem(f"ob{i}")) for i in range(4)]
        cs = s.dsem("const")
        bcos = s.buf("cos", cs); bsin = s.buf("sin", cs); bgsc = s.buf("gsc", cs)
        bab = s.buf("ab"); bsq = s.buf("sq"); bones = s.buf("ones")
        brs = [s.buf(f"rs{i}") for i in range(2)]
        btmp = [s.buf(f"tmp{i}") for i in range(4)]
        bps = [s.buf(f"ps{i}") for i in range(8)]

        s.dma("sp", gsc_sb[:], gsc, W=[bgsc])
        s.dma("sp", cos_sb[:], cosT, W=[bcos])
        s.dma("sp", sin_sb[:], sinT, W=[bsin])
        s.op("pool", lambda e: e.memset(ones_sb[:], 1.0), W=[bones])
        for t in range(2):
            s.op("dve", lambda e: e.scalar_tensor_tensor(
                out=ab_sb[:, t, :], in0=gsc_sb[:, 1 + 2 * t, :], scalar=1.0, in1=gsc_sb[:, 0, :],
                op0=ALU.add, op1=ALU.mult), R=[bgsc], W=[bab])
        for k in range(8):
            sl = k % 2
            s.dma("act", w_st[:, sl, :], wv[:, k, :], W=[bwst[sl]])
            s.op("pool", lambda e: e.tensor_copy(out=w_bf[:, k, :], in_=w_st[:, sl, :]),
                 R=[bwst[sl]], W=[bwbf[k]])
        for bi, (t0, n) in enumerate(BLKS1):
            sl = bi % 2
            isctx = bi == 4
            s.dma("sp", x_sb[:, sl, :, 0:n], xT[:, :, t0:t0 + n], W=[bx[sl]])
            s.op("act", lambda e: e.activation(out=sq_sb[:, :, 0:n], in_=x_sb[:, sl, :, 0:n], func=AF.Square),
                 R=[bx[sl]], W=[bsq])
            pst = bps[sl]
            for k in range(8):
                mm(s, ps[:, sl, 0:n], ones_sb[:], sq_sb[:, k, 0:n], k == 0, k == 7, R=[bones, bsq], W=[pst])
            s.op("act", lambda e: e.activation(out=rstd_sb[:, sl, 0:n], in_=ps[:, sl, 0:n], func=AF.Sqrt,
                                               scale=1.0 / D, bias=EPS), R=[pst], W=[brs[sl]])
            s.op("dve", lambda e: e.reciprocal(out=rstd_sb[:, sl, 0:n], in_=rstd_sb[:, sl, 0:n]),
                 R=[brs[sl]], W=[brs[sl]])
            ai = 1 if isctx else 0
            shrow = 4 if isctx else 2
            for k in range(8):
                tb = k % 4
                s.op("dve", lambda e: e.scalar_tensor_tensor(
                    out=tmp_sb[:, tb, 0:n], in0=x_sb[:, sl, k, 0:n], scalar=ab_sb[:, ai, k:k + 1],
                    in1=rstd_sb[:, sl, 0:n], op0=ALU.mult, op1=ALU.mult),
                    R=[bx[sl], bab, brs[sl]], W=[btmp[tb]])
                s.op("act", lambda e: e.activation(out=h_sb[:, k, t0:t0 + n], in_=tmp_sb[:, tb, 0:n],
                                                   func=AF.Identity, bias=gsc_sb[:, shrow, k:k + 1], scale=1.0),
                     R=[btmp[tb], bgsc], W=[bh[bi]])
        oi = 0
        obi = 0
        pi = 0
        for bi, (t0, n) in enumerate(BLKS1):
            isctx = bi == 4
            for c in range(16):
                rope = (c >= 12) and not isctx
                isb = c < 4 or c >= 12
                dst = (fmb[:, c if c < 4 else c - 8, t0:t0 + n]) if isb else fmf[:, c - 4, t0:t0 + n]
                pA = 2 + (pi % 6); pi += 1
                for k in range(8):
                    mm(s, ps[:, pA, 0:n], w_bf[:, k, c * 128:(c + 1) * 128], h_sb[:, k, t0:t0 + n],
                       k == 0, k == 7, R=[bwbf[k], bh[bi]], W=[bps[pA]])
                if isb:
                    ob = obi % 4; obi += 1
                    osl = ob_sb[:, ob, 0:n]; obuf = bob[ob]
                else:
                    ob = oi % 4; oi += 1
                    osl = o_sb[:, ob, 0:n]; obuf = bo[ob]
                if not rope:
                    if c % 2 == 0:
                        s.op("act", lambda e: e.copy(out=osl, in_=ps[:, pA, 0:n]), R=[bps[pA]], W=[obuf])
                    else:
                        s.op("dve", lambda e: e.tensor_copy(out=osl, in_=ps[:, pA, 0:n]), R=[bps[pA]], W=[obuf])
                else:
                    pB = 2 + (pi % 6); pi += 1
                    c2 = c + 4
                    for k in range(8):
                        mm(s, ps[:, pB, 0:n], w_bf[:, k, c2 * 128:(c2 + 1) * 128], h_sb[:, k, t0:t0 + n],
                           k == 0, k == 7, R=[bwbf[k], bh[bi]], W=[bps[pB]])
                    s.op("dve", lambda e: e.tensor_tensor(out=tmp_sb[:, 0, 0:n], in0=ps[:, pA, 0:n],
                                                          in1=cos_sb[:, t0:t0 + n], op=ALU.mult),
                         R=[bps[pA], bcos], W=[btmp[0]])
                    s.op("dve", lambda e: e.tensor_tensor(out=tmp_sb[:, 1, 0:n], in0=ps[:, pB, 0:n],
                                                          in1=sin_sb[:, t0:t0 + n], op=ALU.mult),
                         R=[bps[pB], bsin], W=[btmp[1]])
                    s.op("pool", lambda e: e.tensor_tensor(out=osl, in0=tmp_sb[:, 0, 0:n],
                                                           in1=tmp_sb[:, 1, 0:n], op=ALU.add),
                         R=[btmp[0], btmp[1]], W=[obuf])
                s.dma("sp", dst, osl, R=[obuf])
        ntile = NT1 // 128 + 1
        for ti in range(ntile):
            t0 = ti * 128
            n = min(128, NT1 - t0)
            bi = min(t0 // 512, 4)
            pA = 2 + (pi % 6); pi += 1
            for k in range(8):
                mm(s, ps[0:n, pA, :], h_sb[:, k, t0:t0 + n], w_bf[:, k, 2560:3072],
                   k == 0, k == 7, R=[bwbf[k], bh[bi]], W=[bps[pA]])
            ob = obi % 4; obi += 1
            s.op("act", lambda e: e.copy(out=ob_sb[0:n, ob, :], in_=ps[0:n, pA, :]), R=[bps[pA]], W=[bob[ob]])
            s.dma("sp", tm[t0:t0 + n, :], ob_sb[0:n, ob, :], R=[bob[ob]])
        s.finish(bo + bob)
    return nc


def rope_tables():
    t = np.arange(S)
    row = (t // GRID_W).astype(np.float32)
    col = (t % GRID_W).astype(np.float32)
    inv = (10000.0 ** (-np.arange(0, 16, 2, dtype=np.float32) / 16.0)).astype(np.float32)
    ang_r = row[:, None] * inv
    ang_c = col[:, None] * inv
    cosT = np.zeros((32, S), np.float32)
    sinT = np.zeros((32, S), np.float32)
    for d in range(32):
        ang = ang_r if d < 16 else ang_c
        i = d % 8
        cosT[d] = np.cos(ang[:, i])
        sgn = -1.0 if (d % 16) < 8 else 1.0
        sinT[d] = sgn * np.sin(ang[:, i])
    return np.tile(cosT, (4, 1)), np.tile(sinT, (4, 1))


def p1_wcols():
    sw = np.array([(d + 8) if (d % 16) < 8 else (d - 8) for d in range(32)])
    f = np.arange(256)
    swf = (f // 32) * 32 + sw[f % 32]
    cols = np.concatenate([np.arange(0, 256), np.arange(256, 512), np.arange(768, 1280), np.arange(1280, 1792),
                           np.arange(1792, 2048), np.arange(2048, 2304), 1792 + swf, 2048 + swf,
                           np.arange(512, 768), np.arange(2304, 2560)])
    return cols


def chunkT(a):
    T = a.shape[0]
    return np.ascontiguousarray(a.T.reshape(8, 128, T).transpose(1, 0, 2))


def vec_pk(v):
    return np.ascontiguousarray(v.reshape(8, 128).T)


def run_p1(nc1, xl, xc, mods_l, g_mix_l, w_in_l, cosT, sinT):
    wl = np.ascontiguousarray(w_in_l[:, p1_wcols()])
    in_maps = []
    for i in range(NCORES):
        b, j = i // 4, i % 4
        xx = np.concatenate([xl[b, 2048 * j:2048 * (j + 1)], xc[b, 64 * j:64 * (j + 1)]], axis=0)
        gsc = np.stack([vec_pk(g_mix_l), vec_pk(mods_l[b, 1024:2048]), vec_pk(mods_l[b, 0:1024]),
                        vec_pk(mods_l[2, 1024:2048]), vec_pk(mods_l[2, 0:1024])], axis=1)
        in_maps.append({"xT": chunkT(xx), "w": wl, "gsc": np.ascontiguousarray(gsc),
                        "cosT": np.ascontiguousarray(cosT[:, 2048 * j:2048 * (j + 1)]),
                        "sinT": np.ascontiguousarray(sinT[:, 2048 * j:2048 * (j + 1)])})
    res = run_bass_kernel_spmd(nc1, in_maps, core_ids=list(range(NCORES)))
    fmb = np.zeros((B, 1024, S + L), ml_dtypes.bfloat16)
    fmf = np.zeros((B, 1024, S + L), np.float32)
    tm = np.zeros((B, S + L, 512), ml_dtypes.bfloat16)
    for i in range(NCORES):
        b, j = i // 4, i % 4
        r = res.results[i]
        for dst, key in ((fmb, "fmb"), (fmf, "fmf")):
            f = r[key].transpose(1, 0, 2).reshape(1024, NT1)
            dst[b, :, 2048 * j:2048 * (j + 1)] = f[:, :2048]
            dst[b, :, S + 64 * j:S + 64 * (j + 1)] = f[:, 2048:]
        t = r["tm"]
        tm[b, 2048 * j:2048 * (j + 1)] = t[:2048]
        tm[b, S + 64 * j:S + 64 * (j + 1)] = t[2048:]
    return fmb, fmf, tm


NTOK = S + L
NKT = NTOK // 128
NEB = 21


def na_tile_lists():
    out = []
    for m in range(64):
        if 2 <= m <= 61:
            out.append(([m - 2, m - 1, m, m + 1, m + 2], 0))
        elif m < 2:
            out.append(([0, 1, 2, 3], 5 + 4 * m))
        else:
            out.append(([60, 61, 62, 63], 5 + 4 * (m - 60)))
    return out


def na_bias_index():
    MASKED = 15 * 31
    idx = np.full((NEB, 128, 128), MASKED, np.int64)
    lists = na_tile_lists()
    reps = {0: 10}
    qq = np.arange(128); kk = np.arange(128)

    def fill(e0, m, kts):
        for ii, n in enumerate(kts):
            qr = 2 * m + qq // 64; qc = qq % 64
            kr = 2 * n + kk // 64; kc = kk % 64
            r0 = np.clip(qr - 4, 0, 120)
            cs = np.clip(qc - 8, 0, 48)
            valid = ((kr[:, None] >= r0[None, :]) & (kr[:, None] < r0[None, :] + 8) &
                     (kc[:, None] >= cs[None, :]) & (kc[:, None] < cs[None, :] + 16))
            dr = kr[:, None] - qr[None, :]
            dc = np.clip(kc[:, None] - qc[None, :], -15, 15)
            v = (np.clip(dr, -7, 7) + 7) * 31 + dc + 15
            idx[e0 + ii] = np.where(valid, v, MASKED)
    fill(0, 10, lists[10][0])
    for m in (0, 1, 62, 63):
        fill(lists[m][1], m, lists[m][0])
    return idx


def build_p2(lam_init):
    nc = bass.Bass("TRN2", target_bir_lowering=False)
    din = lambda n, shp, dt: nc.dram_tensor(n, shp, dt, kind="ExternalInput").ap()
    dout = lambda n, shp, dt: nc.dram_tensor(n, shp, dt, kind="ExternalOutput").ap()
    xrg = din("xrg", [2, 128, NTOK], F32)
    wbd = din("wbd", [4, 128, 128], F32)
    rgv = din("rgv", [128, 2, 8], F32)
    hout = dout("hout", [2, 128, NTOK], F32)
    qaT = din("qaT", [64, NTOK], BF16)
    kaT = din("kaT", [64, NTOK], BF16)
    vaP = din("vaP", [128, NKT, 64], BF16)
    btT = din("btT", [128, NEB, 128], F32)
    ynaP = dout("ynaP", [128, NKT, 64], BF16)
    qdT = din("qdT", [2, 32, NTOK], BF16)
    kdT = din("kdT", [2, 32, NTOK], BF16)
    vdP = din("vdP", [128, NKT, 64], BF16)
    dlam = din("dlam", [128, 128], F32)
    dg = din("dg", [128, 64], F32)
    ydfP = dout("ydfP", [128, NKT, 64], BF16)
    s = Sched(nc)
    with nc.psum_tensor("ps", [128, 8, 512], F32) as ps:
        bps = [s.buf(f"ps{i}") for i in range(8)]

        CH = 2048
        with (nc.sbuf_tensor("x_sb", [128, NTOK], F32) as x_sb,
              nc.sbuf_tensor("wst_sb", [128, 4, 128], F32) as wst_sb,
              nc.sbuf_tensor("wbf_sb", [128, 4, 128], BF16) as wbf_sb,
              nc.sbuf_tensor("rgv_sb", [128, 2, 8], F32) as rgv_sb,
              nc.sbuf_tensor("cneg_sb", [128, 2], F32) as cneg_sb,
              nc.sbuf_tensor("xcv_sb", [128, 2, CH], F32) as xcv_sb,
              nc.sbuf_tensor("xcb_sb", [128, 2, CH], BF16) as xcb_sb,
              nc.sbuf_tensor("r_sb", [128, 2, CH], F32) as r_sb,
              nc.sbuf_tensor("i_sb", [128, 2, CH], F32) as i_sb,
              nc.sbuf_tensor("a_sb", [128, 2, CH], F32) as a_sb,
              nc.sbuf_tensor("q_sb", [128, 2, CH], F32) as q_sb,
              nc.sbuf_tensor("h_sb", [128, 2, CH], F32) as h_sb,
              nc.sbuf_tensor("carry_sb", [128, 1], F32) as carry_sb):
            bx = s.buf("x", s.dsem("rgx"))
            bw = s.buf("w", s.dsem("rgw"))
            bwb = s.buf("wb")
            bv = s.buf("v", bw.dsem)
            bcn = s.buf("cneg")
            bxcv2 = [s.buf(f"xcv{i}") for i in range(2)]; bxcb2 = [s.buf(f"xcb{i}") for i in range(2)]
            br2 = [s.buf(f"r{i}") for i in range(2)]; bi2 = [s.buf(f"i{i}") for i in range(2)]
            ba2 = [s.buf(f"a{i}") for i in range(2)]; bq2 = [s.buf(f"q{i}") for i in range(2)]; bcar = s.buf("carry")
            bh = [s.buf(f"h{i}", s.dsem(f"rgh{i}")) for i in range(2)]
            s.dma("act", wst_sb[:], wbd.rearrange("f c d -> c f d"), W=[bw])
            s.dma("act", rgv_sb[:], rgv, W=[bv])
            s.op("pool", lambda e: e.tensor_copy(out=wbf_sb[:], in_=wst_sb[:]), R=[bw], W=[bwb])
            s.op("act", lambda e: e.activation(out=cneg_sb[:], in_=rgv_sb[:, :, 7], func=AF.Exp, scale=-1.0),
                 R=[bv], W=[bcn])
            s.op("act", lambda e: e.activation(out=cneg_sb[:], in_=cneg_sb[:], func=AF.Ln, bias=1.0, scale=1.0),
                 R=[bcn], W=[bcn])
            s.op("dve", lambda e: e.tensor_scalar(out=cneg_sb[:], in0=cneg_sb[:], scalar1=-8.0, scalar2=None,
                                                  op0=ALU.mult), R=[bcn], W=[bcn])
            units = []
            for dr in range(2):
                chunks = [(0, 256, 0, 256)] + [(256 + CH * i, CH, 256, NTOK) for i in range(4)]
                for ci, ch_ in enumerate(chunks):
                    units.append((dr, ci) + ch_)

            def emitA(ui):
                dr, ci, c0, n, s0, s1 = units[ui]
                cp = ui % 2
                bxcv = bxcv2[cp]; bxcb = bxcb2[cp]; br = br2[cp]; bi_ = bi2[cp]; ba = ba2[cp]; bq = bq2[cp]
                offs = [-2, -1, 0, 1] if dr == 0 else [2, 1, 0, -1]
                if ci == 0:
                    s.dma("sp", x_sb[:, 0:4224], xrg[dr, :, 0:4224], W=[bx])
                    s.dma("sp", x_sb[:, 4224:NTOK], xrg[dr, :, 4224:NTOK], W=[bx])
                s.op("dve", lambda e: e.tensor_scalar(
                    out=xcv_sb[:, cp, 0:n], in0=x_sb[:, c0:c0 + n], scalar1=rgv_sb[:, dr, 2:3],
                    scalar2=rgv_sb[:, dr, 4:5], op0=ALU.mult, op1=ALU.add), R=[bx, bv], W=[bxcv])
                for jt in (0, 1, 3):
                    o = offs[jt]
                    lo = max(c0, s0 - o); hi_ = min(c0 + n, s1 - o)
                    s.op("dve", lambda e: e.scalar_tensor_tensor(
                        out=xcv_sb[:, cp, lo - c0:hi_ - c0], in0=x_sb[:, lo + o:hi_ + o],
                        scalar=rgv_sb[:, dr, jt:jt + 1], in1=xcv_sb[:, cp, lo - c0:hi_ - c0],
                        op0=ALU.mult, op1=ALU.add), R=[bx, bv, bxcv], W=[bxcv])
                s.op("pool", lambda e: e.tensor_copy(out=xcb_sb[:, cp, 0:n], in_=xcv_sb[:, cp, 0:n]), R=[bxcv], W=[bxcb])
                nsb = (n + 511) // 512
                for sb in range(nsb):
                    w_ = min(512, n - sb * 512)
                    mm(s, ps[:, sb, 0:w_], wbf_sb[:, 2 * dr, :], xcb_sb[:, cp, sb * 512:sb * 512 + w_], True, True,
                       R=[bwb, bxcb], W=[bps[sb]])
                    mm(s, ps[:, 4 + sb, 0:w_], wbf_sb[:, 2 * dr + 1, :], xcb_sb[:, cp, sb * 512:sb * 512 + w_], True, True,
                       R=[bwb, bxcb], W=[bps[4 + sb]])
                if n == CH:
                    rin = ps[:, 0:4, :]; iin = ps[:, 4:8, :]
                    rout = r_sb[:, cp, 0:n].rearrange("p (a b) -> p a b", b=512)
                    iout = i_sb[:, cp, 0:n].rearrange("p (a b) -> p a b", b=512)
                else:
                    rin = ps[:, 0, 0:n]; iin = ps[:, 4, 0:n]
                    rout = r_sb[:, cp, 0:n]; iout = i_sb[:, cp, 0:n]
                s.op("act", lambda e: e.activation(out=rout, in_=rin, func=AF.Sigmoid,
                                                   bias=rgv_sb[:, dr, 5:6], scale=1.0), R=bps[0:4] + [bv], W=[br])
                s.op("act", lambda e: e.activation(out=iout, in_=iin, func=AF.Sigmoid,
                                                   bias=rgv_sb[:, dr, 6:7], scale=1.0), R=bps[4:8] + [bv], W=[bi_])
                s.op("act", lambda e: e.activation(out=a_sb[:, cp, 0:n], in_=r_sb[:, cp, 0:n], func=AF.Exp,
                                                   scale=cneg_sb[:, dr:dr + 1]), R=[br, bcn], W=[ba])
                s.op("pool", lambda e: e.tensor_tensor(out=q_sb[:, cp, 0:n], in0=a_sb[:, cp, 0:n], in1=a_sb[:, cp, 0:n],
                                                       op=ALU.mult), R=[ba], W=[bq])
                s.op("act", lambda e: e.activation(out=q_sb[:, cp, 0:n], in_=q_sb[:, cp, 0:n], func=AF.Sqrt,
                                                   scale=-1.0, bias=1.0), R=[bq], W=[bq])
                s.op("pool", lambda e: e.tensor_tensor(out=i_sb[:, cp, 0:n], in0=i_sb[:, cp, 0:n], in1=xcv_sb[:, cp, 0:n],
                                                       op=ALU.mult), R=[bi_, bxcv], W=[bi_])
                s.op("pool", lambda e: e.tensor_tensor(out=q_sb[:, cp, 0:n], in0=q_sb[:, cp, 0:n], in1=i_sb[:, cp, 0:n],
                                                       op=ALU.mult), R=[bq, bi_], W=[bq])

            def emitB(ui):
                dr, ci, c0, n, s0, s1 = units[ui]
                cp = ui % 2
                hs = ui % 2
                init = 0.0 if ci == 0 else carry_sb[:, 0:1]
                s.op("dve", lambda e: e.tensor_tensor_scan(out=h_sb[:, hs, 0:n], data0=a_sb[:, cp, 0:n],
                                                           data1=q_sb[:, cp, 0:n], initial=init,
                                                           op0=ALU.mult, op1=ALU.add),
                     R=[ba2[cp], bq2[cp]] + ([bcar] if ci else []), W=[bh[hs]])
                s.op("dve", lambda e: e.tensor_copy(out=carry_sb[:, 0:1], in_=h_sb[:, hs, n - 1:n]),
                     R=[bh[hs]], W=[bcar])
                s.dma("sp", hout[dr, :, c0:c0 + n], h_sb[:, hs, 0:n], R=[bh[hs]])

            for ui in range(len(units)):
                emitA(ui)
                if ui >= 1:
                    emitB(ui - 1)
            emitB(len(units) - 1)
            s.barrier(bh)

        with (nc.sbuf_tensor("na_q_sb", [64, NTOK], BF16) as q_sb,
              nc.sbuf_tensor("na_k_sb", [64, NTOK], BF16) as k_sb,
              nc.sbuf_tensor("na_v_sb", [128, NKT, 65], BF16) as v_sb,
              nc.sbuf_tensor("na_bt_sb", [128, NEB * 128], F32) as bt_sb,
              nc.sbuf_tensor("na_eb_sb", [128, NEB * 128], BF16) as eb_sb,
              nc.sbuf_tensor("na_e_sb", [128, 2, 640], F32) as e_sb,
              nc.sbuf_tensor("na_p_sb", [128, 2, 896], BF16) as p_sb,
              nc.sbuf_tensor("na_y_sb", [128, NKT, 64], BF16) as y_sb,
              nc.sbuf_tensor("na_rec_sb", [128, 2], F32) as rec_sb):
            ld = s.dsem("nald")
            bq = s.buf("q", ld); bk = s.buf("k", ld); bv = s.buf("v", ld); bbt = s.buf("bt", ld)
            beb = s.buf("eb")
            be = [s.buf(f"e{i}") for i in range(2)]
            bp = [s.buf(f"p{i}") for i in range(2)]
            brec = [s.buf(f"rec{i}") for i in range(2)]
            by = s.buf("y", s.dsem("nay"))
            s.dma("sp", q_sb[:], qaT, W=[bq])
            s.dma("act", k_sb[:], kaT, W=[bk])
            s.dma("sp", v_sb[:, :, 0:64], vaP, W=[bv])
            s.dma("act", bt_sb[:], btT.rearrange("p e q -> p (e q)"), W=[bbt])
            s.op("pool", lambda e: e.memset(v_sb[:, :, 64:65], 1.0), W=[bv])
            s.op("act", lambda e: e.activation(out=eb_sb[:], in_=bt_sb[:], func=AF.Exp), R=[bbt], W=[beb])
            lists = na_tile_lists()
            for m in range(NKT):
                sl = m % 2
                if m < 64:
                    kts, eb0 = lists[m]
                else:
                    kts, eb0 = [], 0
                nl = len(kts)
                bA = bps[2 * sl]; bB = bps[2 * sl + 1]; bAcc = bps[4 + sl]
                qs = q_sb[:, m * 128:(m + 1) * 128]
                for ii, n in enumerate(kts):
                    bank = 2 * sl + (0 if ii < 4 else 1)
                    col = (ii % 4) * 128
                    mm(s, ps[:, bank, col:col + 128], k_sb[:, n * 128:(n + 1) * 128], qs, True, True,
                       R=[bk, bq], W=[bps[bank]])
                for ci in range(2):
                    n = 64 + ci
                    mm(s, ps[:, 2 * sl + 1, 128 + ci * 128:256 + ci * 128], k_sb[:, n * 128:(n + 1) * 128], qs,
                       True, True, R=[bk, bq], W=[bB])
                if nl:
                    na_ = min(nl, 4) * 128
                    s.op("act", lambda e: e.activation(out=e_sb[:, sl, 0:na_], in_=ps[:, 2 * sl, 0:na_], func=AF.Exp,
                                                       scale=0.125), R=[bA], W=[be[sl]])
                    if nl == 5:
                        s.op("act", lambda e: e.activation(out=e_sb[:, sl, 512:640], in_=ps[:, 2 * sl + 1, 0:128],
                                                           func=AF.Exp, scale=0.125), R=[bB], W=[be[sl]])
                s.op("act", lambda e: e.activation(out=p_sb[:, sl, 640:896], in_=ps[:, 2 * sl + 1, 128:384],
                                                   func=AF.Exp, scale=0.125), R=[bB], W=[bp[sl]])
                if nl:
                    s.op("dve", lambda e: e.tensor_tensor(out=p_sb[:, sl, 0:nl * 128], in0=e_sb[:, sl, 0:nl * 128],
                                                          in1=eb_sb[:, eb0 * 128:(eb0 + nl) * 128], op=ALU.mult),
                         R=[be[sl], beb], W=[bp[sl]])
                tiles = [(ii * 128, n) for ii, n in enumerate(kts)] + [(640, 64), (768, 65)]
                for ti, (pc, n) in enumerate(tiles):
                    mm(s, ps[:, 4 + sl, 0:65], p_sb[:, sl, pc:pc + 128], v_sb[:, n, :], ti == 0, ti == len(tiles) - 1,
                       R=[bp[sl], bv], W=[bAcc])
                s.op("dve", lambda e: e.reciprocal(out=rec_sb[:, sl:sl + 1], in_=ps[:, 4 + sl, 64:65]),
                     R=[bAcc], W=[brec[sl]])
                s.op("dve", lambda e: e.tensor_scalar(out=y_sb[:, m, :], in0=ps[:, 4 + sl, 0:64],
                                                      scalar1=rec_sb[:, sl:sl + 1], scalar2=None, op0=ALU.mult),
                     R=[bAcc, brec[sl]], W=[by])
            s.dma("sp", ynaP, y_sb[:], R=[by])
            s.barrier([by])

        with (nc.sbuf_tensor("df_q4_sb", [96, NTOK], BF16) as q4_sb,
              nc.sbuf_tensor("df_k4_sb", [96, NTOK], BF16) as k4_sb,
              nc.sbuf_tensor("df_q4b_sb", [96, NTOK], BF16) as q4b_sb,
              nc.sbuf_tensor("df_k4b_sb", [96, NTOK], BF16) as k4b_sb,
              nc.sbuf_tensor("df_v_sb", [128, NKT, 65], BF16) as v_sb,
              nc.sbuf_tensor("df_p_sb", [128, 8, 512], BF16) as p_sb,
              nc.sbuf_tensor("df_y_sb", [128, NKT, 64], BF16) as y_sb,
              nc.sbuf_tensor("df_dl_sb", [128, 128], F32) as dl_sb,
              nc.sbuf_tensor("df_g_sb", [128, 64], F32) as g_sb,
              nc.sbuf_tensor("df_lam_sb", [128, 4], F32) as lam_sb,
              nc.sbuf_tensor("df_r_sb", [128, 2, 2, 4], F32) as r_sb,
              nc.sbuf_tensor("df_ss_sb", [128, 2, 4], F32) as ss_sb,
              nc.sbuf_tensor("df_o_sb", [128, 2, 4, 64], F32) as o_sb,
              nc.sbuf_tensor("df_t_sb", [128, 2, 4, 64], F32) as t_sb,
              nc.sbuf_tensor("df_junk_sb", [128, 64], F32) as junk_sb):
            ld = s.dsem("dfld")
            bq = s.buf("q", ld); bk = s.buf("k", ld); bv = s.buf("v", ld); bdl = s.buf("dl", ld); bg = s.buf("g", ld)
            blam = s.buf("lam")
            bp = [s.buf(f"p{i}") for i in range(8)]
            by = s.buf("y", s.dsem("dfy"))
            br = [s.buf(f"r{i}") for i in range(2)]
            bss = [s.buf(f"ss{i}") for i in range(2)]
            bo = [s.buf(f"o{i}") for i in range(2)]
            bt = [s.buf(f"t{i}") for i in range(2)]
            bj = s.buf("junk")
            bq_l = []; bk_l = []
            for (qt, kt_, order) in ((q4_sb, k4_sb, (0, 1, 0)), (q4b_sb, k4b_sb, (1, 0, 1))):
                for ri, cc in enumerate(order):
                    b1 = s.buf("ql", ld); b2 = s.buf("kl", ld)
                    bq_l.append(b1); bk_l.append(b2)
                    s.dma("sp", qt[32 * ri:32 * ri + 32, :], qdT[cc], W=[b1])
                    s.dma("sp", kt_[32 * ri:32 * ri + 32, :], kdT[cc], W=[b2])
            s.dma("sp", v_sb[:, :, 0:64], vdP, W=[bv])
            s.dma("sp", dl_sb[:], dlam, W=[bdl])
            s.dma("sp", g_sb[:], dg, W=[bg])
            s.op("pool", lambda e: e.memset(v_sb[:, :, 64:65], 1.0), W=[bv])
            s.op("dve", lambda e: e.scalar_tensor_tensor(out=junk_sb[:, 0:32], in0=dl_sb[:, 0:32], scalar=1.0,
                                                         in1=dl_sb[:, 32:64], op0=ALU.mult, op1=ALU.mult,
                                                         accum_out=lam_sb[:, 0:1]), R=[bdl], W=[bj, blam])
            s.op("dve", lambda e: e.scalar_tensor_tensor(out=junk_sb[:, 0:32], in0=dl_sb[:, 64:96], scalar=1.0,
                                                         in1=dl_sb[:, 96:128], op0=ALU.mult, op1=ALU.mult,
                                                         accum_out=lam_sb[:, 1:2]), R=[bdl, bj], W=[bj, blam])
            s.op("act", lambda e: e.activation(out=lam_sb[:, 0:2], in_=lam_sb[:, 0:2], func=AF.Exp), R=[blam], W=[blam])
            s.op("dve", lambda e: e.tensor_tensor(out=lam_sb[:, 2:3], in0=lam_sb[:, 1:2], in1=lam_sb[:, 0:1],
                                                  op=ALU.subtract), R=[blam], W=[blam])
            s.op("dve", lambda e: e.tensor_scalar(out=lam_sb[:, 2:3], in0=lam_sb[:, 2:3], scalar1=-lam_init,
                                                  scalar2=None, op0=ALU.add), R=[blam], W=[blam])
            s.op("dve", lambda e: e.tensor_scalar(out=g_sb[:], in0=g_sb[:], scalar1=1.0 - lam_init, scalar2=None,
                                                  op0=ALU.mult), R=[bg], W=[bg])
            qblocks = [(512 * i, 512, list(range(NKT))) for i in range(16)] + [(S, 256, [64, 65])]
            sc = 32 ** -0.5
            gs_ = 0
            for qi, (q0, nq, kts) in enumerate(qblocks):
                par = qi % 2
                nsub = nq // 128
                steps = [(kt, c) for kt in kts for c in range(2)]
                ns = len(steps)
                groups = [list(range(i, min(i + 3, ns))) for i in range(0, ns, 3)]
                started = [False, False]
                for gi_ in range(len(groups) + 1):
                    if gi_ < len(groups):
                        grp = groups[gi_]
                        layB = steps[grp[0]][1] == 1
                        qt = q4b_sb if layB else q4_sb
                        kt_t = k4b_sb if layB else k4_sb
                        for ri, si in enumerate(grp):
                            kt, c = steps[si]
                            g = gs_ + si
                            mm(s, ps[:, g % 4, 0:nq], kt_t[32 * ri:32 * ri + 32, kt * 128:(kt + 1) * 128],
                               qt[32 * ri:32 * ri + 32, q0:q0 + nq], True, True, R=bk_l + bq_l, W=[bps[g % 4]])
                        for ri, si in enumerate(grp):
                            g = gs_ + si
                            s.op("act", lambda e: e.activation(out=p_sb[:, g % 8, 0:nq], in_=ps[:, g % 4, 0:nq],
                                                               func=AF.Exp, scale=sc), R=[bps[g % 4]], W=[bp[g % 8]])
                    if gi_ >= 1:
                        for si in groups[gi_ - 1]:
                            kt, c = steps[si]
                            g = gs_ + si
                            bank = 4 + 2 * par + c
                            for sub in range(nsub):
                                st = not started[c]
                                started[c] = True
                                s.op("pe", lambda e: e.matmul(ps[:, bank, sub * 65:(sub + 1) * 65],
                                                              p_sb[:, g % 8, sub * 128:(sub + 1) * 128], v_sb[:, kt, :],
                                                              start=st, stop=(kt == kts[-1]), skip_group_check=True),
                                     R=[bp[g % 8], bv], W=[bps[bank]])
                gs_ += ns
                a0 = ps[:, 4 + 2 * par, 0:260].rearrange("p (s e) -> p s e", e=65)
                a1 = ps[:, 5 + 2 * par, 0:260].rearrange("p (s e) -> p s e", e=65)
                b0 = bps[4 + 2 * par]; b1 = bps[5 + 2 * par]
                s.op("dve", lambda e: e.reciprocal(out=r_sb[:, par, 0, 0:nsub], in_=a0[:, 0:nsub, 64]), R=[b0], W=[br[par]])
                s.op("dve", lambda e: e.reciprocal(out=r_sb[:, par, 1, 0:nsub], in_=a1[:, 0:nsub, 64]), R=[b1], W=[br[par]])
                s.op("dve", lambda e: e.tensor_scalar(out=r_sb[:, par, 1, 0:nsub], in0=r_sb[:, par, 1, 0:nsub],
                                                      scalar1=lam_sb[:, 2:3], scalar2=None, op0=ALU.mult),
                     R=[br[par], blam], W=[br[par]])
                for sub in range(nsub):
                    s.op("dve", lambda e: e.tensor_scalar(out=t_sb[:, par, sub, :], in0=a1[:, sub, 0:64],
                                                          scalar1=r_sb[:, par, 1, sub:sub + 1], scalar2=None,
                                                          op0=ALU.mult), R=[b1, br[par]], W=[bt[par]])
                    s.op("dve", lambda e: e.scalar_tensor_tensor(out=o_sb[:, par, sub, :], in0=a0[:, sub, 0:64],
                                                                 scalar=r_sb[:, par, 0, sub:sub + 1],
                                                                 in1=t_sb[:, par, sub, :], op0=ALU.mult, op1=ALU.add),
                         R=[b0, br[par], bt[par]], W=[bo[par]])
                    s.op("dve", lambda e: e.scalar_tensor_tensor(out=junk_sb[:], in0=o_sb[:, par, sub, :], scalar=1.0,
                                                                 in1=o_sb[:, par, sub, :], op0=ALU.mult, op1=ALU.mult,
                                                                 accum_out=ss_sb[:, par, sub:sub + 1]),
                         R=[bo[par], bj], W=[bj, bss[par]])
                s.op("act", lambda e: e.activation(out=ss_sb[:, par, 0:nsub], in_=ss_sb[:, par, 0:nsub], func=AF.Ln,
                                                   scale=1.0 / 64, bias=EPS), R=[bss[par]], W=[bss[par]])
                s.op("act", lambda e: e.activation(out=ss_sb[:, par, 0:nsub], in_=ss_sb[:, par, 0:nsub], func=AF.Exp,
                                                   scale=-0.5), R=[bss[par]], W=[bss[par]])
                for sub in range(nsub):
                    s.op("dve", lambda e: e.scalar_tensor_tensor(out=y_sb[:, q0 // 128 + sub, :], in0=o_sb[:, par, sub, :],
                                                                 scalar=ss_sb[:, par, sub:sub + 1], in1=g_sb[:],
                                                                 op0=ALU.mult, op1=ALU.mult),
                         R=[bo[par], bss[par], bg], W=[by])
            s.dma("sp", ydfP, y_sb[:], R=[by])
            s.finish([by])
    return nc


def tileP(a):
    return np.ascontiguousarray(a.reshape(NKT, 128, a.shape[1]).transpose(1, 0, 2))


def untileP(a):
    return a.transpose(1, 0, 2).reshape(NTOK, a.shape[2])


_NA_IDX = None


def run_p2(nc2, l, fmb, fmf, tm, inp):
    global _NA_IDX
    if _NA_IDX is None:
        _NA_IDX = na_bias_index()
    in_maps = []
    for i in range(NCORES):
        b, j = i // 4, i % 4
        xr = fmf[b, 128 * j:128 * (j + 1)]
        xf = np.concatenate([xr[:, S:], xr[:, :S]], axis=1)
        xb = np.concatenate([xr[:, S:][:, ::-1], xr[:, :S][:, ::-1]], axis=1)
        wbd = np.zeros((4, 128, 128), np.float32)
        rgv = np.zeros((128, 2, 8), np.float32)
        ch = slice(128 * j, 128 * (j + 1))
        for dr in range(2):
            for gi, wk in enumerate(("rg_w_r", "rg_w_i")):
                for bb in range(2):
                    wbd[dr * 2 + gi, 64 * bb:64 * (bb + 1), 64 * bb:64 * (bb + 1)] = inp[wk][l, dr, 2 * j + bb]
            rgv[:, dr, 0:4] = inp["rg_conv_w"][l][:, ch].T
            rgv[:, dr, 4] = inp["rg_conv_b"][l][ch]
            rgv[:, dr, 5] = inp["rg_b_r"][l, dr, ch]
            rgv[:, dr, 6] = inp["rg_b_i"][l, dr, ch]
            rgv[:, dr, 7] = inp["rg_lambda"][l, dr, ch]
        rext = np.concatenate([inp["na_rpb"][l, j].ravel(), np.array([-30000.0], np.float32)])
        bt = rext[_NA_IDX]
        in_maps.append({
            "xrg": np.ascontiguousarray(np.stack([xf, xb])), "wbd": wbd, "rgv": rgv,
            "qaT": np.ascontiguousarray(fmb[b, 64 * j:64 * (j + 1)]),
            "kaT": np.ascontiguousarray(fmb[b, 256 + 64 * j:256 + 64 * (j + 1)]),
            "vaP": tileP(tm[b][:, 64 * j:64 * (j + 1)]),
            "btT": np.ascontiguousarray(bt.transpose(1, 0, 2)),
            "qdT": np.ascontiguousarray(fmb[b, 512 + 64 * j:512 + 64 * (j + 1)].reshape(2, 32, NTOK)),
            "kdT": np.ascontiguousarray(fmb[b, 768 + 64 * j:768 + 64 * (j + 1)].reshape(2, 32, NTOK)),
            "vdP": tileP(tm[b][:, 256 + 64 * j:256 + 64 * (j + 1)]),
            "dlam": np.ascontiguousarray(np.tile(inp["diff_lambda"][l].reshape(1, 128), (128, 1))),
            "dg": np.ascontiguousarray(np.tile(inp["diff_subln_g"][l].reshape(1, 64), (128, 1))),
        })
    res = run_bass_kernel_spmd(nc2, in_maps, core_ids=list(range(NCORES)))
    yna = np.zeros((B, NTOK, 256), ml_dtypes.bfloat16)
    ydf = np.zeros((B, NTOK, 256), ml_dtypes.bfloat16)
    hf = np.zeros((B, 512, NTOK), np.float32)
    hb = np.zeros((B, 512, NTOK), np.float32)
    for i in range(NCORES):
        b, j = i // 4, i % 4
        r = res.results[i]
        yna[b, :, 64 * j:64 * (j + 1)] = untileP(r["ynaP"])
        ydf[b, :, 64 * j:64 * (j + 1)] = untileP(r["ydfP"])
        h = r["hout"]
        hf[b, 128 * j:128 * (j + 1), S:] = h[0][:, :L]
        hf[b, 128 * j:128 * (j + 1), :S] = h[0][:, L:]
        hb[b, 128 * j:128 * (j + 1), S:] = h[1][:, :L][:, ::-1]
        hb[b, 128 * j:128 * (j + 1), :S] = h[1][:, L:][:, ::-1]
    return yna, ydf, hf, hb


NEXP = 32
GELU_C = 1.5957691216057308


def build_p3(final):
    nc = bass.Bass("TRN2", target_bir_lowering=False)
    din = lambda n, shp, dt: nc.dram_tensor(n, shp, dt, kind="ExternalInput").ap()
    xT = din("xT", [128, 8, NT1], F32)
    nadf = din("nadf", [128, 4, NT1], BF16)
    hg = din("hg", [128, 12, NT1], F32)
    wout = din("wout", [D, D], F32)
    mod = din("mod", [128, 10, 8], F32)
    wge = din("wge", [128, 8, 36], F32)
    bge = din("bge", [128, 36], F32)
    selc = din("selc", [32, NEXP * 128], BF16)
    ident = din("ident", [128, 128], F32)
    w1 = din("w1", [NEXP, D, 512], F32)
    w3 = din("w3", [NEXP, D, 512], F32)
    w2 = din("w2", [NEXP, 512, D], F32)
    xo = nc.dram_tensor("xo", [128, 8, NT1], F32, kind="ExternalOutput").ap()
    s = Sched(nc)
    wov = wout.rearrange("(kc p) n -> p kc n", p=128)
    with (nc.psum_tensor("ps", [128, 8, 512], F32) as ps,
          nc.sbuf_tensor("x_sb", [128, 8, NT1], F32) as x_sb,
          nc.sbuf_tensor("mod_sb", [128, 10, 8], F32) as mod_sb,
          nc.sbuf_tensor("hl2_sb", [128, 8, NT1], BF16) as hl2_sb,
          nc.sbuf_tensor("wdt_sb", [32, 2, NT1], BF16) as wdt_sb,
          nc.sbuf_tensor("ones_sb", [128, 128], BF16) as ones_sb):
        bps = [s.buf(f"ps{i}") for i in range(8)]
        cs = s.dsem("const")
        bxb = [s.buf(f"x{i}", s.dsem(f"x{i}")) for i in range(len(BLKS1))]
        bmod = s.buf("mod", cs)
        bhl2 = [s.buf(f"hl2_{i}") for i in range(len(BLKS1))]
        bwdt = [s.buf(f"wdt{i}") for i in range(len(BLKS1))]
        bones = s.buf("ones")
        s.dma("sp", mod_sb[:], mod, W=[bmod])
        for bi, (t0, n) in enumerate(BLKS1):
            s.dma("sp", x_sb[:, :, t0:t0 + n], xT[:, :, t0:t0 + n], W=[bxb[bi]])
        s.op("pool", lambda e: e.memset(ones_sb[:], 1.0), W=[bones])

        with (nc.sbuf_tensor("s1_mix", [128, 8, NT1], BF16) as mix_sb,
              nc.sbuf_tensor("s1_wobf", [128, 8, D], BF16) as wo_bf,
              nc.sbuf_tensor("s1_wost", [128, 2, D], F32) as wo_st,
              nc.sbuf_tensor("s1_hg", [128, 1, 12, 512], F32) as hg_sb,
              nc.sbuf_tensor("s1_t", [128, 4, 512], F32) as t_sb):
            bmixl = s.buf("mixl", s.dsem("mixl"))
            bmix = [s.buf(f"mix{i}") for i in range(len(BLKS1))]
            bwost = [s.buf(f"wost{i}", s.dsem(f"wost{i}")) for i in range(2)]
            bwobf = [s.buf(f"wobf{k}") for k in range(8)]
            bhg = [s.buf(f"hg{i}", s.dsem(f"hg{i}")) for i in range(2)]
            bt = [s.buf(f"t{i}") for i in range(4)]
            s.dma("act", mix_sb[:, 0:2, :], nadf[:, 0:2, :], W=[bmixl])
            s.dma("act", mix_sb[:, 6:8, :], nadf[:, 2:4, :], W=[bmixl])
            for k in range(8):
                sl = k % 2
                s.dma("act", wo_st[:, sl, :], wov[:, k, :], W=[bwost[sl]])
                s.op("pool", lambda e: e.tensor_copy(out=wo_bf[:, k, :], in_=wo_st[:, sl, :]), R=[bwost[sl]], W=[bwobf[k]])
            for bi, (t0, n) in enumerate(BLKS1):
                sl = 0
                s.dma("sp", hg_sb[:, sl, :, 0:n], hg[:, :, t0:t0 + n], W=[bhg[sl]])
                for ch in range(4):
                    hf_ = hg_sb[:, sl, ch, 0:n]; hb_ = hg_sb[:, sl, 4 + ch, 0:n]; gr_ = hg_sb[:, sl, 8 + ch, 0:n]
                    s.op("dve", lambda e: e.tensor_tensor(out=t_sb[:, 0, 0:n], in0=hf_, in1=hb_, op=ALU.add),
                         R=[bhg[sl]], W=[bt[0]])
                    s.op("dve", lambda e: e.tensor_tensor(out=t_sb[:, 1, 0:n], in0=gr_, in1=gr_, op=ALU.mult),
                         R=[bhg[sl]], W=[bt[1]])
                    s.op("dve", lambda e: e.tensor_scalar(out=t_sb[:, 1, 0:n], in0=t_sb[:, 1, 0:n], scalar1=0.044715,
                                                          scalar2=1.0, op0=ALU.mult, op1=ALU.add), R=[bt[1]], W=[bt[1]])
                    s.op("pool", lambda e: e.tensor_tensor(out=t_sb[:, 2, 0:n], in0=t_sb[:, 1, 0:n], in1=gr_, op=ALU.mult),
                         R=[bt[1], bhg[sl]], W=[bt[2]])
                    s.op("act", lambda e: e.activation(out=t_sb[:, 2, 0:n], in_=t_sb[:, 2, 0:n], func=AF.Sigmoid,
                                                       scale=GELU_C), R=[bt[2]], W=[bt[2]])
                    s.op("pool", lambda e: e.tensor_tensor(out=t_sb[:, 3, 0:n], in0=t_sb[:, 2, 0:n], in1=gr_, op=ALU.mult),
                         R=[bt[2], bhg[sl]], W=[bt[3]])
                    s.op("pool", lambda e: e.tensor_tensor(out=mix_sb[:, 2 + ch, t0:t0 + n], in0=t_sb[:, 3, 0:n],
                                                           in1=t_sb[:, 0, 0:n], op=ALU.mult),
                         R=[bt[3], bt[0]], W=[bmix[bi]])
            pi = 0
            for bi, (t0, n) in enumerate(BLKS1):
                garow = 5 if bi == 4 else 1
                for dc in range(8):
                    pb = pi % 8; pi += 1
                    for k in range(8):
                        mm(s, ps[:, pb, 0:n], wo_bf[:, k, dc * 128:(dc + 1) * 128], mix_sb[:, k, t0:t0 + n],
                           k == 0, k == 7, R=[bwobf[k], bmix[bi], bmixl], W=[bps[pb]])
                    s.op("dve", lambda e: e.scalar_tensor_tensor(out=x_sb[:, dc, t0:t0 + n], in0=ps[:, pb, 0:n],
                                                                 scalar=mod_sb[:, garow, dc:dc + 1],
                                                                 in1=x_sb[:, dc, t0:t0 + n], op0=ALU.mult, op1=ALU.add),
                         R=[bps[pb], bmod, bxb[bi]], W=[bxb[bi]])
            s.barrier()

        with (nc.sbuf_tensor("s2_sq", [128, 8, 512], BF16) as sq_sb,
              nc.sbuf_tensor("s2_rstd", [128, 2, 512], F32) as rstd_sb,
              nc.sbuf_tensor("s2_tmp", [128, 4, 512], F32) as tmp_sb,
              nc.sbuf_tensor("s2_hf", [128, 2, 8, 512], F32) as hf_sb,
              nc.sbuf_tensor("s2_ab", [128, 2, 8], F32) as ab_sb,
              nc.sbuf_tensor("s2_wge", [128, 8, 36], F32) as wge_sb,
              nc.sbuf_tensor("s2_bge", [128, 36], F32) as bge_sb,
              nc.sbuf_tensor("s2_id", [128, 128], F32) as id_sb,
              nc.sbuf_tensor("s2_rt", [128, 2, 128], F32) as rt_sb):
            bsq = s.buf("sq"); brs = [s.buf(f"rs{i}") for i in range(2)]
            btmp = [s.buf(f"tmp{i}") for i in range(4)]
            bhf = [s.buf(f"hf{i}") for i in range(2)]
            bab = s.buf("ab")
            bwge = s.buf("wge", cs); bbge = s.buf("bge", cs); bid = s.buf("id", cs)
            brt = [s.buf(f"rt{i}") for i in range(2)]
            s.dma("act", wge_sb[:], wge, W=[bwge])
            s.dma("act", bge_sb[:], bge, W=[bbge])
            s.dma("act", id_sb[:], ident, W=[bid])
            for t in range(2):
                s.op("dve", lambda e: e.scalar_tensor_tensor(
                    out=ab_sb[:, t, :], in0=mod_sb[:, 2 + 4 * t, :], scalar=1.0, in1=mod_sb[:, 0, :],
                    op0=ALU.add, op1=ALU.mult), R=[bmod], W=[bab])
            ti_g = 0
            for bi, (t0, n) in enumerate(BLKS1):
                sl = bi % 2
                isctx = bi == 4
                s.op("act", lambda e: e.activation(out=sq_sb[:, :, 0:n], in_=x_sb[:, :, t0:t0 + n], func=AF.Square),
                     R=[bxb[bi]], W=[bsq])
                for k in range(8):
                    mm(s, ps[:, sl, 0:n], ones_sb[:], sq_sb[:, k, 0:n], k == 0, k == 7, R=[bones, bsq], W=[bps[sl]])
                s.op("act", lambda e: e.activation(out=rstd_sb[:, sl, 0:n], in_=ps[:, sl, 0:n], func=AF.Sqrt,
                                                   scale=1.0 / D, bias=EPS), R=[bps[sl]], W=[brs[sl]])
                s.op("dve", lambda e: e.reciprocal(out=rstd_sb[:, sl, 0:n], in_=rstd_sb[:, sl, 0:n]),
                     R=[brs[sl]], W=[brs[sl]])
                ai = 1 if isctx else 0
                shrow = 7 if isctx else 3
                for k in range(8):
                    tb = k % 4
                    s.op("dve", lambda e: e.scalar_tensor_tensor(
                        out=tmp_sb[:, tb, 0:n], in0=x_sb[:, k, t0:t0 + n], scalar=ab_sb[:, ai, k:k + 1],
                        in1=rstd_sb[:, sl, 0:n], op0=ALU.mult, op1=ALU.mult),
                        R=[bxb[bi], bab, brs[sl]], W=[btmp[tb]])
                    s.op("act", lambda e: e.activation(out=hf_sb[:, sl, k, 0:n], in_=tmp_sb[:, tb, 0:n],
                                                       func=AF.Identity, bias=mod_sb[:, shrow, k:k + 1], scale=1.0),
                         R=[btmp[tb], bmod], W=[bhf[sl]])
                s.op("pool", lambda e: e.tensor_copy(out=hl2_sb[:, :, t0:t0 + n], in_=hf_sb[:, sl, :, 0:n]),
                     R=[bhf[sl]], W=[bhl2[bi]])
                for tt in range((n + 127) // 128):
                    c0 = tt * 128
                    m = min(128, n - c0)
                    rs_ = ti_g % 2; ti_g += 1
                    pb = 2 + rs_
                    rt = rt_sb[0:m, rs_, :]
                    brr = brt[rs_]
                    for k in range(8):
                        mm(s, ps[0:m, pb, 0:36], hf_sb[:, sl, k, c0:c0 + m], wge_sb[:, k, :], k == 0, k == 7,
                           R=[bhf[sl], bwge], W=[bps[pb]])
                    V = lambda eng, fn, R_=(), W_=(): s.op(eng, fn, R=[brr] + list(R_), W=[brr] + list(W_))
                    lg = rt[:, 0:36]
                    s.op("dve", lambda e: e.tensor_tensor(out=lg, in0=ps[0:m, pb, 0:36], in1=bge_sb[0:m, :], op=ALU.add),
                         R=[bps[pb], bbge], W=[brr])
                    gmax = rt[:, 36:37]; ngmax = rt[:, 37:38]; sume = rt[:, 38:39]; gtop = rt[:, 39:40]
                    eg = rt[:, 40:44]; ohg = rt[:, 44:48]; sel = rt[:, 48:56]; top8 = rt[:, 56:64]
                    dd = rt[:, 64:65]; ed = rt[:, 65:66]; w1_ = rt[:, 66:67]; wt1 = rt[:, 67:68]; wt2 = rt[:, 68:69]
                    ea = rt[:, 72:80]; eb_ = rt[:, 80:88]; wd = rt[:, 96:128]
                    V("dve", lambda e: e.reduce_max(out=gmax, in_=lg[:, 0:4], axis=AX.X))
                    V("dve", lambda e: e.tensor_scalar(out=ngmax, in0=gmax, scalar1=-1.0, scalar2=None, op0=ALU.mult))
                    V("act", lambda e: e.activation(out=eg, in_=lg[:, 0:4], func=AF.Exp, bias=ngmax, scale=1.0,
                                                    accum_out=sume))
                    V("dve", lambda e: e.reciprocal(out=gtop, in_=sume))
                    V("dve", lambda e: e.tensor_scalar(out=ohg, in0=lg[:, 0:4], scalar1=gmax, scalar2=None,
                                                       op0=ALU.is_equal))
                    V("dve", lambda e: e.tensor_scalar(out=sel, in0=lg[:, 4:12], scalar1=ohg[:, 0:1], scalar2=None,
                                                       op0=ALU.mult))
                    for g in range(1, 4):
                        V("dve", lambda e: e.scalar_tensor_tensor(out=sel, in0=lg[:, 4 + 8 * g:12 + 8 * g],
                                                                  scalar=ohg[:, g:g + 1], in1=sel,
                                                                  op0=ALU.mult, op1=ALU.add))
                    V("dve", lambda e: e.max(out=top8, in_=sel))
                    V("dve", lambda e: e.tensor_tensor(out=dd, in0=top8[:, 1:2], in1=top8[:, 0:1], op=ALU.subtract))
                    V("act", lambda e: e.activation(out=ed, in_=dd, func=AF.Exp))
                    V("dve", lambda e: e.tensor_scalar(out=w1_, in0=ed, scalar1=1.0, scalar2=None, op0=ALU.add))
                    V("dve", lambda e: e.reciprocal(out=w1_, in_=w1_))
                    V("dve", lambda e: e.tensor_tensor(out=wt1, in0=w1_, in1=gtop, op=ALU.mult))
                    V("dve", lambda e: e.tensor_tensor(out=wt2, in0=wt1, in1=ed, op=ALU.mult))
                    V("dve", lambda e: e.tensor_scalar(out=ea, in0=sel, scalar1=top8[:, 0:1], scalar2=wt1,
                                                       op0=ALU.is_equal, op1=ALU.mult))
                    V("dve", lambda e: e.tensor_scalar(out=eb_, in0=sel, scalar1=top8[:, 1:2], scalar2=wt2,
                                                       op0=ALU.is_equal, op1=ALU.mult))
                    V("dve", lambda e: e.tensor_tensor(out=ea, in0=ea, in1=eb_, op=ALU.add))
                    for g in range(4):
                        V("dve", lambda e: e.tensor_scalar(out=wd[:, 8 * g:8 * g + 8], in0=ea, scalar1=ohg[:, g:g + 1],
                                                           scalar2=None, op0=ALU.mult))
                    pt = 4 + rs_
                    s.op("pe", lambda e: e.transpose(ps[0:32, pt, 0:m], wd, id_sb[0:m, 0:m]), R=[brr, bid], W=[bps[pt]])
                    s.op("act", lambda e: e.copy(out=wdt_sb[:, 0, t0 + c0:t0 + c0 + m], in_=ps[0:32, pt, 0:m]),
                         R=[bps[pt]], W=[bwdt[bi]])
                    s.op("dve", lambda e: e.tensor_tensor(out=wdt_sb[:, 1, t0 + c0:t0 + c0 + m], in0=ps[0:32, pt, 0:m],
                                                          in1=wdt_sb[:, 0, t0 + c0:t0 + c0 + m], op=ALU.subtract),
                         R=[bps[pt], bwdt[bi]], W=[bwdt[bi]])
            s.barrier()

        with (nc.sbuf_tensor("s3_st", [128, 3, 2048], F32) as st_sb,
              nc.sbuf_tensor("s3_wb", [128, 2, 6, 2048], BF16) as wb_sb,
              nc.sbuf_tensor("s3_sel", [32, NEXP * 128], BF16) as sel_sb,
              nc.sbuf_tensor("s3_wbc", [128, 2, 512], F32) as wbc_sb,
              nc.sbuf_tensor("s3_sg", [128, 2, 512], F32) as sg_sb,
              nc.sbuf_tensor("s3_t", [128, 2, 512], F32) as t3_sb,
              nc.sbuf_tensor("s3_g", [128, 2, 4, 512], BF16) as g_sb):
            bst = [s.buf(f"st{i}", s.dsem(f"st{i}")) for i in range(3)]
            bwb = [[s.buf(f"wb{a}_{p}") for p in range(6)] for a in range(2)]
            bsel = s.buf("sel", cs)
            bwbc = [s.buf(f"wbc{i}") for i in range(2)]
            bsg = [s.buf(f"sg{i}") for i in range(2)]
            bt3 = [s.buf(f"t3{i}") for i in range(2)]
            bg = [s.buf(f"g{i}") for i in range(2)]
            s.dma("act", sel_sb[:], selc, W=[bsel])
            w1v = w1.rearrange("e (kc p) f -> e p kc f", p=128)
            w3v = w3.rearrange("e (kc p) f -> e p kc f", p=128)
            w2v = w2.rearrange("e (fc p) d -> e p fc d", p=128)

            def piece_src(e, p):
                if p < 2:
                    return w1v[e, :, 4 * p:4 * p + 4, :]
                if p < 4:
                    return w3v[e, :, 4 * (p - 2):4 * (p - 2) + 4, :]
                return w2v[e, :, 2 * (p - 4):2 * (p - 4) + 2, :]

            def piece_dma(P):
                e, p = divmod(P, 6)
                if e >= NEXP:
                    return
                sl = P % 3
                dst = st_sb[:, sl, :]
                dst = dst.rearrange("q (a b) -> q a b", a=4) if p < 4 else dst.rearrange("q (a b) -> q a b", a=2)
                s.dma("sp", dst, piece_src(e, p), W=[bst[sl]])

            def piece_cast(P):
                e, p = divmod(P, 6)
                if e >= NEXP:
                    return
                sl = P % 3
                s.op("pool", lambda en: en.tensor_copy(out=wb_sb[:, e % 2, p, :], in_=st_sb[:, sl, :]),
                     R=[bst[sl]], W=[bwb[e % 2][p]])

            for P in range(3):
                piece_dma(P)
            for P in range(6):
                piece_cast(P)
                piece_dma(P + 3)
            gi = 0
            for ex in range(NEXP):
                a = ex % 2
                for bi, (t0, n) in enumerate(BLKS1):
                    if bi < 3:
                        for P in (6 * (ex + 1) + 2 * bi, 6 * (ex + 1) + 2 * bi + 1):
                            piece_cast(P)
                            piece_dma(P + 3)
                    garow = 8 if bi == 4 else 4
                    wr = gi % 2
                    gs = gi % 2
                    gi += 1
                    mm(s, ps[:, 6, 0:n], sel_sb[:, ex * 128:(ex + 1) * 128], wdt_sb[:, 0, t0:t0 + n], True, False,
                       R=[bsel, bwdt[bi]], W=[bps[6]])
                    mm(s, ps[:, 6, 0:n], sel_sb[:, ex * 128:(ex + 1) * 128], wdt_sb[:, 1, t0:t0 + n], False, True,
                       R=[bsel, bwdt[bi]], W=[bps[6]])
                    s.op("act", lambda e: e.copy(out=wbc_sb[:, wr, 0:n], in_=ps[:, 6, 0:n]), R=[bps[6]], W=[bwbc[wr]])
                    for fc in range(4):
                        pr = fc % 2
                        for which in range(2):
                            bank = 2 * pr + which
                            for k in range(8):
                                wv = wb_sb[:, a, 2 * which + k // 4, :].rearrange("q (a b) -> q a b", a=4)
                                mm(s, ps[:, bank, 0:n], wv[:, k % 4, fc * 128:(fc + 1) * 128], hl2_sb[:, k, t0:t0 + n],
                                   k == 0, k == 7, R=[bwb[a][2 * which + k // 4], bhl2[bi]], W=[bps[bank]])
                        s.op("act", lambda e: e.activation(out=sg_sb[:, pr, 0:n], in_=ps[:, 2 * pr, 0:n], func=AF.Silu),
                             R=[bps[2 * pr]], W=[bsg[pr]])
                        s.op("dve", lambda e: e.tensor_tensor(out=t3_sb[:, pr, 0:n], in0=ps[:, 2 * pr + 1, 0:n],
                                                              in1=sg_sb[:, pr, 0:n], op=ALU.mult),
                             R=[bps[2 * pr + 1], bsg[pr]], W=[bt3[pr]])
                        s.op("pool", lambda e: e.tensor_tensor(out=g_sb[:, gs, fc, 0:n], in0=t3_sb[:, pr, 0:n],
                                                               in1=wbc_sb[:, wr, 0:n], op=ALU.mult),
                             R=[bt3[pr], bwbc[wr]], W=[bg[gs]])
                    for dc in range(8):
                        bank = 4 + dc % 2
                        for fc in range(4):
                            wv = wb_sb[:, a, 4 + fc // 2, :].rearrange("q (a b) -> q a b", a=2)
                            mm(s, ps[:, bank, 0:n], wv[:, fc % 2, dc * 128:(dc + 1) * 128], g_sb[:, gs, fc, 0:n],
                               fc == 0, fc == 3, R=[bwb[a][4 + fc // 2], bg[gs]], W=[bps[bank]])
                        s.op("dve", lambda e: e.scalar_tensor_tensor(out=x_sb[:, dc, t0:t0 + n], in0=ps[:, bank, 0:n],
                                                                     scalar=mod_sb[:, garow, dc:dc + 1],
                                                                     in1=x_sb[:, dc, t0:t0 + n], op0=ALU.mult, op1=ALU.add),
                             R=[bps[bank], bmod, bxb[bi]], W=[bxb[bi]])
            s.barrier()

        with (nc.sbuf_tensor("s4_sq", [128, 8, 512], BF16) as sq_sb,
              nc.sbuf_tensor("s4_rstd", [128, 2, 512], F32) as rstd_sb):
            bsq = s.buf("sq4"); brs = [s.buf(f"rs4{i}") for i in range(2)]
            for bi, (t0, n) in enumerate(BLKS1):
                if final:
                    sl = bi % 2
                    s.op("act", lambda e: e.activation(out=sq_sb[:, :, 0:n], in_=x_sb[:, :, t0:t0 + n], func=AF.Square),
                         R=[bxb[bi]], W=[bsq])
                    for k in range(8):
                        mm(s, ps[:, sl, 0:n], ones_sb[:], sq_sb[:, k, 0:n], k == 0, k == 7, R=[bones, bsq], W=[bps[sl]])
                    s.op("act", lambda e: e.activation(out=rstd_sb[:, sl, 0:n], in_=ps[:, sl, 0:n], func=AF.Sqrt,
                                                       scale=1.0 / D, bias=EPS), R=[bps[sl]], W=[brs[sl]])
                    s.op("dve", lambda e: e.reciprocal(out=rstd_sb[:, sl, 0:n], in_=rstd_sb[:, sl, 0:n]),
                         R=[brs[sl]], W=[brs[sl]])
                    for k in range(8):
                        s.op("dve", lambda e: e.scalar_tensor_tensor(
                            out=x_sb[:, k, t0:t0 + n], in0=x_sb[:, k, t0:t0 + n], scalar=mod_sb[:, 9, k:k + 1],
                            in1=rstd_sb[:, sl, 0:n], op0=ALU.mult, op1=ALU.mult),
                            R=[bxb[bi], bmod, brs[sl]], W=[bxb[bi]])
                s.dma("sp", xo[:, :, t0:t0 + n], x_sb[:, :, t0:t0 + n], R=[bxb[bi]])
            s.finish(bxb)
    return nc


def build_p3a():
    nc = bass.Bass("TRN2", target_bir_lowering=False)
    din = lambda n, shp, dt: nc.dram_tensor(n, shp, dt, kind="ExternalInput").ap()
    xT = din("xT", [128, 8, NT1], F32)
    nadf = din("nadf", [128, 4, NT1], BF16)
    hg = din("hg", [128, 12, NT1], F32)
    wout = din("wout", [D, D], F32)
    mod = din("mod", [128, 10, 8], F32)
    wge = din("wge", [128, 8, 36], F32)
    bge = din("bge", [128, 36], F32)
    iota4 = din("iota4", [128, 4], F32)
    ident = din("ident", [128, 128], F32)
    xo = nc.dram_tensor("xo", [128, 8, NT1], F32, kind="ExternalOutput").ap()
    hl2o = nc.dram_tensor("hl2o", [128, 8, NT1], BF16, kind="ExternalOutput").ap()
    wdto = nc.dram_tensor("wdto", [32, 2, NT1], BF16, kind="ExternalOutput").ap()
    gido = nc.dram_tensor("gido", [128, 17], F32, kind="ExternalOutput").ap()
    wd32o = nc.dram_tensor("wd32o", [128, 17, 32], F32, kind="ExternalOutput").ap()
    s = Sched(nc)
    wov = wout.rearrange("(kc p) n -> p kc n", p=128)
    with (nc.psum_tensor("ps", [128, 8, 512], F32) as ps,
          nc.sbuf_tensor("x_sb", [128, 8, NT1], F32) as x_sb,
          nc.sbuf_tensor("mod_sb", [128, 10, 8], F32) as mod_sb,
          nc.sbuf_tensor("hl2_sb", [128, 8, NT1], BF16) as hl2_sb,
          nc.sbuf_tensor("wdt_sb", [32, 2, NT1], BF16) as wdt_sb,
          nc.sbuf_tensor("ones_sb", [128, 128], BF16) as ones_sb):
        bps = [s.buf(f"ps{i}") for i in range(8)]
        cs = s.dsem("const")
        bxb = [s.buf(f"x{i}", s.dsem(f"x{i}")) for i in range(len(BLKS1))]
        bmod = s.buf("mod", cs)
        bhl2 = [s.buf(f"hl2_{i}") for i in range(len(BLKS1))]
        bwdt = [s.buf(f"wdt{i}") for i in range(len(BLKS1))]
        bones = s.buf("ones")
        s.dma("sp", mod_sb[:], mod, W=[bmod])
        for bi, (t0, n) in enumerate(BLKS1):
            s.dma("sp", x_sb[:, :, t0:t0 + n], xT[:, :, t0:t0 + n], W=[bxb[bi]])
        s.op("pool", lambda e: e.memset(ones_sb[:], 1.0), W=[bones])

        with (nc.sbuf_tensor("s1_mix", [128, 8, NT1], BF16) as mix_sb,
              nc.sbuf_tensor("s1_wobf", [128, 8, D], BF16) as wo_bf,
              nc.sbuf_tensor("s1_wost", [128, 2, D], F32) as wo_st,
              nc.sbuf_tensor("s1_hg", [128, 1, 12, 512], F32) as hg_sb,
              nc.sbuf_tensor("s1_t", [128, 4, 512], F32) as t_sb):
            bmixl = s.buf("mixl", s.dsem("mixl"))
            bmix = [s.buf(f"mix{i}") for i in range(len(BLKS1))]
            bwost = [s.buf(f"wost{i}", s.dsem(f"wost{i}")) for i in range(2)]
            bwobf = [s.buf(f"wobf{k}") for k in range(8)]
            bhg = [s.buf(f"hg{i}", s.dsem(f"hg{i}")) for i in range(2)]
            bt = [s.buf(f"t{i}") for i in range(4)]
            s.dma("act", mix_sb[:, 0:2, :], nadf[:, 0:2, :], W=[bmixl])
            s.dma("act", mix_sb[:, 6:8, :], nadf[:, 2:4, :], W=[bmixl])
            for k in range(8):
                sl = k % 2
                s.dma("act", wo_st[:, sl, :], wov[:, k, :], W=[bwost[sl]])
                s.op("pool", lambda e: e.tensor_copy(out=wo_bf[:, k, :], in_=wo_st[:, sl, :]), R=[bwost[sl]], W=[bwobf[k]])
            for bi, (t0, n) in enumerate(BLKS1):
                sl = 0
                s.dma("sp", hg_sb[:, sl, :, 0:n], hg[:, :, t0:t0 + n], W=[bhg[sl]])
                for ch in range(4):
                    hf_ = hg_sb[:, sl, ch, 0:n]; hb_ = hg_sb[:, sl, 4 + ch, 0:n]; gr_ = hg_sb[:, sl, 8 + ch, 0:n]
                    s.op("dve", lambda e: e.tensor_tensor(out=t_sb[:, 0, 0:n], in0=hf_, in1=hb_, op=ALU.add),
                         R=[bhg[sl]], W=[bt[0]])
                    s.op("dve", lambda e: e.tensor_tensor(out=t_sb[:, 1, 0:n], in0=gr_, in1=gr_, op=ALU.mult),
                         R=[bhg[sl]], W=[bt[1]])
                    s.op("dve", lambda e: e.tensor_scalar(out=t_sb[:, 1, 0:n], in0=t_sb[:, 1, 0:n], scalar1=0.044715,
                                                          scalar2=1.0, op0=ALU.mult, op1=ALU.add), R=[bt[1]], W=[bt[1]])
                    s.op("pool", lambda e: e.tensor_tensor(out=t_sb[:, 2, 0:n], in0=t_sb[:, 1, 0:n], in1=gr_, op=ALU.mult),
                         R=[bt[1], bhg[sl]], W=[bt[2]])
                    s.op("act", lambda e: e.activation(out=t_sb[:, 2, 0:n], in_=t_sb[:, 2, 0:n], func=AF.Sigmoid,
                                                       scale=GELU_C), R=[bt[2]], W=[bt[2]])
                    s.op("pool", lambda e: e.tensor_tensor(out=t_sb[:, 3, 0:n], in0=t_sb[:, 2, 0:n], in1=gr_, op=ALU.mult),
                         R=[bt[2], bhg[sl]], W=[bt[3]])
                    s.op("pool", lambda e: e.tensor_tensor(out=mix_sb[:, 2 + ch, t0:t0 + n], in0=t_sb[:, 3, 0:n],
                                                           in1=t_sb[:, 0, 0:n], op=ALU.mult),
                         R=[bt[3], bt[0]], W=[bmix[bi]])
            pi = 0
            for bi, (t0, n) in enumerate(BLKS1):
                garow = 5 if bi == 4 else 1
                for dc in range(8):
                    pb = pi % 8; pi += 1
                    for k in range(8):
                        mm(s, ps[:, pb, 0:n], wo_bf[:, k, dc * 128:(dc + 1) * 128], mix_sb[:, k, t0:t0 + n],
                           k == 0, k == 7, R=[bwobf[k], bmix[bi], bmixl], W=[bps[pb]])
                    s.op("dve", lambda e: e.scalar_tensor_tensor(out=x_sb[:, dc, t0:t0 + n], in0=ps[:, pb, 0:n],
                                                                 scalar=mod_sb[:, garow, dc:dc + 1],
                                                                 in1=x_sb[:, dc, t0:t0 + n], op0=ALU.mult, op1=ALU.add),
                         R=[bps[pb], bmod, bxb[bi]], W=[bxb[bi]])
            s.barrier()

        es_ = ExitStack()
        io4_sb = es_.enter_context(nc.sbuf_tensor("s2_io4", [128, 4], F32))
        gid_sb = es_.enter_context(nc.sbuf_tensor("s2_gid", [128, 17], F32))
        junk4_sb = es_.enter_context(nc.sbuf_tensor("s2_junk", [128, 4], F32))
        wd32_sb = es_.enter_context(nc.sbuf_tensor("s2_wd32", [128, 17, 32], F32))
        with (nc.sbuf_tensor("s2_sq", [128, 8, 512], BF16) as sq_sb,
              nc.sbuf_tensor("s2_rstd", [128, 2, 512], F32) as rstd_sb,
              nc.sbuf_tensor("s2_tmp", [128, 4, 512], F32) as tmp_sb,
              nc.sbuf_tensor("s2_hf", [128, 2, 8, 512], F32) as hf_sb,
              nc.sbuf_tensor("s2_ab", [128, 2, 8], F32) as ab_sb,
              nc.sbuf_tensor("s2_wge", [128, 8, 36], F32) as wge_sb,
              nc.sbuf_tensor("s2_bge", [128, 36], F32) as bge_sb,
              nc.sbuf_tensor("s2_id", [128, 128], F32) as id_sb,
              nc.sbuf_tensor("s2_rt", [128, 2, 128], F32) as rt_sb):
            bsq = s.buf("sq"); brs = [s.buf(f"rs{i}") for i in range(2)]
            btmp = [s.buf(f"tmp{i}") for i in range(4)]
            bhf = [s.buf(f"hf{i}") for i in range(2)]
            bab = s.buf("ab")
            bwge = s.buf("wge", cs); bbge = s.buf("bge", cs); bid = s.buf("id", cs)
            brt = [s.buf(f"rt{i}") for i in range(2)]
            s.dma("act", wge_sb[:], wge, W=[bwge])
            s.dma("act", bge_sb[:], bge, W=[bbge])
            s.dma("act", id_sb[:], ident, W=[bid])
            bio4 = s.buf("io4", cs); bgid = s.buf("gid", s.dsem("gid")); bjk = s.buf("junk4")
            s.dma("act", io4_sb[:], iota4, W=[bio4])
            s.op("pool", lambda e: e.memset(gid_sb[:], 0.0), W=[bgid])
            bwd32 = s.buf("wd32", s.dsem("wd32"))
            s.op("pool", lambda e: e.memset(wd32_sb[:], 0.0), W=[bwd32])
            for t in range(2):
                s.op("dve", lambda e: e.scalar_tensor_tensor(
                    out=ab_sb[:, t, :], in0=mod_sb[:, 2 + 4 * t, :], scalar=1.0, in1=mod_sb[:, 0, :],
                    op0=ALU.add, op1=ALU.mult), R=[bmod], W=[bab])
            ti_g = 0
            for bi, (t0, n) in enumerate(BLKS1):
                sl = bi % 2
                isctx = bi == 4
                s.op("act", lambda e: e.activation(out=sq_sb[:, :, 0:n], in_=x_sb[:, :, t0:t0 + n], func=AF.Square),
                     R=[bxb[bi]], W=[bsq])
                for k in range(8):
                    mm(s, ps[:, sl, 0:n], ones_sb[:], sq_sb[:, k, 0:n], k == 0, k == 7, R=[bones, bsq], W=[bps[sl]])
                s.op("act", lambda e: e.activation(out=rstd_sb[:, sl, 0:n], in_=ps[:, sl, 0:n], func=AF.Sqrt,
                                                   scale=1.0 / D, bias=EPS), R=[bps[sl]], W=[brs[sl]])
                s.op("dve", lambda e: e.reciprocal(out=rstd_sb[:, sl, 0:n], in_=rstd_sb[:, sl, 0:n]),
                     R=[brs[sl]], W=[brs[sl]])
                ai = 1 if isctx else 0
                shrow = 7 if isctx else 3
                for k in range(8):
                    tb = k % 4
                    s.op("dve", lambda e: e.scalar_tensor_tensor(
                        out=tmp_sb[:, tb, 0:n], in0=x_sb[:, k, t0:t0 + n], scalar=ab_sb[:, ai, k:k + 1],
                        in1=rstd_sb[:, sl, 0:n], op0=ALU.mult, op1=ALU.mult),
                        R=[bxb[bi], bab, brs[sl]], W=[btmp[tb]])
                    s.op("act", lambda e: e.activation(out=hf_sb[:, sl, k, 0:n], in_=tmp_sb[:, tb, 0:n],
                                                       func=AF.Identity, bias=mod_sb[:, shrow, k:k + 1], scale=1.0),
                         R=[btmp[tb], bmod], W=[bhf[sl]])
                s.op("pool", lambda e: e.tensor_copy(out=hl2_sb[:, :, t0:t0 + n], in_=hf_sb[:, sl, :, 0:n]),
                     R=[bhf[sl]], W=[bhl2[bi]])
                for tt in range((n + 127) // 128):
                    c0 = tt * 128
                    m = min(128, n - c0)
                    rs_ = ti_g % 2; ti_g += 1
                    pb = 2 + rs_
                    rt = rt_sb[0:m, rs_, :]
                    brr = brt[rs_]
                    for k in range(8):
                        mm(s, ps[0:m, pb, 0:36], hf_sb[:, sl, k, c0:c0 + m], wge_sb[:, k, :], k == 0, k == 7,
                           R=[bhf[sl], bwge], W=[bps[pb]])
                    V = lambda eng, fn, R_=(), W_=(): s.op(eng, fn, R=[brr] + list(R_), W=[brr] + list(W_))
                    lg = rt[:, 0:36]
                    s.op("dve", lambda e: e.tensor_tensor(out=lg, in0=ps[0:m, pb, 0:36], in1=bge_sb[0:m, :], op=ALU.add),
                         R=[bps[pb], bbge], W=[brr])
                    gmax = rt[:, 36:37]; ngmax = rt[:, 37:38]; sume = rt[:, 38:39]; gtop = rt[:, 39:40]
                    eg = rt[:, 40:44]; ohg = rt[:, 44:48]; sel = rt[:, 48:56]; top8 = rt[:, 56:64]
                    dd = rt[:, 64:65]; ed = rt[:, 65:66]; w1_ = rt[:, 66:67]; wt1 = rt[:, 67:68]; wt2 = rt[:, 68:69]
                    ea = rt[:, 72:80]; eb_ = rt[:, 80:88]; wd = rt[:, 96:128]
                    V("dve", lambda e: e.reduce_max(out=gmax, in_=lg[:, 0:4], axis=AX.X))
                    V("dve", lambda e: e.tensor_scalar(out=ngmax, in0=gmax, scalar1=-1.0, scalar2=None, op0=ALU.mult))
                    V("act", lambda e: e.activation(out=eg, in_=lg[:, 0:4], func=AF.Exp, bias=ngmax, scale=1.0,
                                                    accum_out=sume))
                    V("dve", lambda e: e.reciprocal(out=gtop, in_=sume))
                    V("dve", lambda e: e.tensor_scalar(out=ohg, in0=lg[:, 0:4], scalar1=gmax, scalar2=None,
                                                       op0=ALU.is_equal))
                    tgl = (t0 + c0) // 128
                    s.op("dve", lambda e: e.scalar_tensor_tensor(out=junk4_sb[0:m, :], in0=ohg, scalar=1.0,
                                                                 in1=io4_sb[0:m, :], op0=ALU.mult, op1=ALU.mult,
                                                                 accum_out=gid_sb[0:m, tgl:tgl + 1]),
                         R=[brr, bio4, bjk], W=[bjk, bgid])
                    V("dve", lambda e: e.tensor_scalar(out=sel, in0=lg[:, 4:12], scalar1=ohg[:, 0:1], scalar2=None,
                                                       op0=ALU.mult))
                    for g in range(1, 4):
                        V("dve", lambda e: e.scalar_tensor_tensor(out=sel, in0=lg[:, 4 + 8 * g:12 + 8 * g],
                                                                  scalar=ohg[:, g:g + 1], in1=sel,
                                                                  op0=ALU.mult, op1=ALU.add))
                    V("dve", lambda e: e.max(out=top8, in_=sel))
                    V("dve", lambda e: e.tensor_tensor(out=dd, in0=top8[:, 1:2], in1=top8[:, 0:1], op=ALU.subtract))
                    V("act", lambda e: e.activation(out=ed, in_=dd, func=AF.Exp))
                    V("dve", lambda e: e.tensor_scalar(out=w1_, in0=ed, scalar1=1.0, scalar2=None, op0=ALU.add))
                    V("dve", lambda e: e.reciprocal(out=w1_, in_=w1_))
                    V("dve", lambda e: e.tensor_tensor(out=wt1, in0=w1_, in1=gtop, op=ALU.mult))
                    V("dve", lambda e: e.tensor_tensor(out=wt2, in0=wt1, in1=ed, op=ALU.mult))
                    V("dve", lambda e: e.tensor_scalar(out=ea, in0=sel, scalar1=top8[:, 0:1], scalar2=wt1,
                                                       op0=ALU.is_equal, op1=ALU.mult))
                    V("dve", lambda e: e.tensor_scalar(out=eb_, in0=sel, scalar1=top8[:, 1:2], scalar2=wt2,
                                                       op0=ALU.is_equal, op1=ALU.mult))
                    V("dve", lambda e: e.tensor_tensor(out=ea, in0=ea, in1=eb_, op=ALU.add))
                    for g in range(4):
                        V("dve", lambda e: e.tensor_scalar(out=wd[:, 8 * g:8 * g + 8], in0=ea, scalar1=ohg[:, g:g + 1],
                                                           scalar2=None, op0=ALU.mult))
                    s.op("dve", lambda e: e.tensor_copy(out=wd32_sb[0:m, tgl, :], in_=wd), R=[brr], W=[bwd32])
                    pt = 4 + rs_
                    s.op("pe", lambda e: e.transpose(ps[0:32, pt, 0:m], wd, id_sb[0:m, 0:m]), R=[brr, bid], W=[bps[pt]])
                    s.op("act", lambda e: e.copy(out=wdt_sb[:, 0, t0 + c0:t0 + c0 + m], in_=ps[0:32, pt, 0:m]),
                         R=[bps[pt]], W=[bwdt[bi]])
                    s.op("dve", lambda e: e.tensor_tensor(out=wdt_sb[:, 1, t0 + c0:t0 + c0 + m], in0=ps[0:32, pt, 0:m],
                                                          in1=wdt_sb[:, 0, t0 + c0:t0 + c0 + m], op=ALU.subtract),
                         R=[bps[pt], bwdt[bi]], W=[bwdt[bi]])
            bxo = s.buf("xout", s.dsem("xout"))
            s.dma("sp", xo, x_sb[:], R=bxb, W=[bxo])
            s.dma("sp", hl2o, hl2_sb[:], R=bhl2, W=[bxo])
            s.dma("sp", wdto, wdt_sb[:], R=bwdt, W=[bxo])
            s.dma("sp", gido, gid_sb[:], R=[bgid], W=[bxo])
            s.dma("sp", wd32o, wd32_sb[:], R=[bwd32], W=[bxo])
            s.finish([bxo])
        es_.close()
    return nc


def build_p3b(ntb):
    NE = 8
    nblk_all = ntb // 512
    nh = 2 if ntb > 2048 else 1
    nblk = -(-nblk_all // nh)
    nth = nblk * 512
    nc = bass.Bass("TRN2", target_bir_lowering=False)
    din = lambda n, shp, dt: nc.dram_tensor(n, shp, dt, kind="ExternalInput").ap()
    hl2 = din("hl2", [128, 8, ntb], BF16)
    wdt = din("wdt", [8, 2, ntb], BF16)
    selc = din("selc", [8, NE * 128], BF16)
    w1 = din("w1", [NE, D, 512], F32)
    w3 = din("w3", [NE, D, 512], F32)
    w2 = din("w2", [NE, 512, D], F32)
    yo = nc.dram_tensor("yo", [128, 8, ntb], F32, kind="ExternalOutput").ap()
    s = Sched(nc)
    with (nc.psum_tensor("ps", [128, 8, 512], F32) as ps,
          nc.sbuf_tensor("y_sb", [128, 8, nth], F32) as y_sb,
          nc.sbuf_tensor("hl2_sb", [128, 8, nth], BF16) as hl2_sb,
          nc.sbuf_tensor("wdt_sb", [8, 2, ntb], BF16) as wdt_sb,
          nc.sbuf_tensor("s3_st", [128, 3, 2048], F32) as st_sb,
          nc.sbuf_tensor("s3_wb", [128, 2, 6, 2048], BF16) as wb_sb,
          nc.sbuf_tensor("s3_sel", [8, NE * 128], BF16) as sel_sb,
          nc.sbuf_tensor("s3_wbc", [128, 2, 512], F32) as wbc_sb,
          nc.sbuf_tensor("s3_sg", [128, 2, 512], F32) as sg_sb,
          nc.sbuf_tensor("s3_t", [128, 2, 512], F32) as t3_sb,
          nc.sbuf_tensor("s3_g", [128, 2, 4, 512], BF16) as g_sb):
        bps = [s.buf(f"ps{i}") for i in range(8)]
        cs = s.dsem("const")
        by = [s.buf(f"y{i}", s.dsem(f"y{i}")) for i in range(nblk)]
        bhl2 = [s.buf(f"hl2_{i}", s.dsem(f"hl{i}")) for i in range(nblk)]
        bwdt = s.buf("wdt", cs)
        bst = [s.buf(f"st{i}", s.dsem(f"st{i}")) for i in range(3)]
        bwb = [[s.buf(f"wb{a}_{p}") for p in range(6)] for a in range(2)]
        bsel = s.buf("sel", cs)
        bwbc = [s.buf(f"wbc{i}") for i in range(2)]
        bsg = [s.buf(f"sg{i}") for i in range(2)]
        bt3 = [s.buf(f"t3{i}") for i in range(2)]
        bg = [s.buf(f"g{i}") for i in range(2)]
        s.dma("act", sel_sb[:], selc, W=[bsel])
        s.dma("act", wdt_sb[:], wdt, W=[bwdt])
        w1v = w1.rearrange("e (kc p) f -> e p kc f", p=128)
        w3v = w3.rearrange("e (kc p) f -> e p kc f", p=128)
        w2v = w2.rearrange("e (fc p) d -> e p fc d", p=128)

        def piece_src(e, p):
            if p < 2:
                return w1v[e, :, 4 * p:4 * p + 4, :]
            if p < 4:
                return w3v[e, :, 4 * (p - 2):4 * (p - 2) + 4, :]
            return w2v[e, :, 2 * (p - 4):2 * (p - 4) + 2, :]

        def piece_dma(P):
            e, p = divmod(P, 6)
            if e >= NE * nh:
                return
            e = e % NE
            sl = P % 3
            dst = st_sb[:, sl, :]
            dst = dst.rearrange("q (a b) -> q a b", a=4) if p < 4 else dst.rearrange("q (a b) -> q a b", a=2)
            s.dma("sp", dst, piece_src(e, p), W=[bst[sl]])

        def piece_cast(P):
            e, p = divmod(P, 6)
            if e >= NE * nh:
                return
            sl = P % 3
            s.op("pool", lambda en: en.tensor_copy(out=wb_sb[:, e % 2, p, :], in_=st_sb[:, sl, :]),
                 R=[bst[sl]], W=[bwb[e % 2][p]])

        for P in range(3):
            piece_dma(P)
        for P in range(6):
            piece_cast(P)
            piece_dma(P + 3)
        gi = 0
        n = 512
        pend = [6 * 1 + i for i in range(6)]
        for vx in range(NE * nh):
            half, ex = divmod(vx, NE)
            a = vx % 2
            pend = [6 * (vx + 1) + i for i in range(6)]
            hb0 = half * nblk
            nb_h = min(nblk, nblk_all - hb0)
            if ex == 0:
                for bi in range(nb_h):
                    s.dma("act", hl2_sb[:, :, bi * 512:(bi + 1) * 512], hl2[:, :, (hb0 + bi) * 512:(hb0 + bi + 1) * 512],
                          W=[bhl2[bi]])
            for bi in range(nb_h):
                t0 = bi * 512
                tg = (hb0 + bi) * 512
                npc = (6 + nb_h - 1) // nb_h
                for P in pend[bi * npc:(bi + 1) * npc]:
                    piece_cast(P)
                    piece_dma(P + 3)
                wr = gi % 2
                gs = gi % 2
                gi += 1
                mm(s, ps[:, 6, 0:n], sel_sb[:, ex * 128:(ex + 1) * 128], wdt_sb[:, 0, tg:tg + n], True, False,
                   R=[bsel, bwdt], W=[bps[6]])
                mm(s, ps[:, 6, 0:n], sel_sb[:, ex * 128:(ex + 1) * 128], wdt_sb[:, 1, tg:tg + n], False, True,
                   R=[bsel, bwdt], W=[bps[6]])
                s.op("act", lambda e: e.copy(out=wbc_sb[:, wr, 0:n], in_=ps[:, 6, 0:n]), R=[bps[6]], W=[bwbc[wr]])
                for fc in range(4):
                    pr = fc % 2
                    for which in range(2):
                        bank = 2 * pr + which
                        for k in range(8):
                            wv = wb_sb[:, a, 2 * which + k // 4, :].rearrange("q (a b) -> q a b", a=4)
                            mm(s, ps[:, bank, 0:n], wv[:, k % 4, fc * 128:(fc + 1) * 128], hl2_sb[:, k, t0:t0 + n],
                               k == 0, k == 7, R=[bwb[a][2 * which + k // 4], bhl2[bi]], W=[bps[bank]])
                    s.op("act", lambda e: e.activation(out=sg_sb[:, pr, 0:n], in_=ps[:, 2 * pr, 0:n], func=AF.Silu),
                         R=[bps[2 * pr]], W=[bsg[pr]])
                    s.op("dve", lambda e: e.tensor_tensor(out=t3_sb[:, pr, 0:n], in0=ps[:, 2 * pr + 1, 0:n],
                                                          in1=sg_sb[:, pr, 0:n], op=ALU.mult),
                         R=[bps[2 * pr + 1], bsg[pr]], W=[bt3[pr]])
                    s.op("pool", lambda e: e.tensor_tensor(out=g_sb[:, gs, fc, 0:n], in0=t3_sb[:, pr, 0:n],
                                                           in1=wbc_sb[:, wr, 0:n], op=ALU.mult),
                         R=[bt3[pr], bwbc[wr]], W=[bg[gs]])
                for dc in range(8):
                    bank = 4 + dc % 2
                    for fc in range(4):
                        wv = wb_sb[:, a, 4 + fc // 2, :].rearrange("q (a b) -> q a b", a=2)
                        mm(s, ps[:, bank, 0:n], wv[:, fc % 2, dc * 128:(dc + 1) * 128], g_sb[:, gs, fc, 0:n],
                           fc == 0, fc == 3, R=[bwb[a][4 + fc // 2], bg[gs]], W=[bps[bank]])
                    if ex == 0:
                        s.op("dve", lambda e: e.tensor_copy(out=y_sb[:, dc, t0:t0 + n], in_=ps[:, bank, 0:n]),
                             R=[bps[bank]], W=[by[bi]])
                    else:
                        s.op("dve", lambda e: e.tensor_tensor(out=y_sb[:, dc, t0:t0 + n], in0=ps[:, bank, 0:n],
                                                              in1=y_sb[:, dc, t0:t0 + n], op=ALU.add),
                             R=[bps[bank], by[bi]], W=[by[bi]])
            if ex == NE - 1:
                for bi in range(nb_h):
                    s.dma("sp", yo[:, :, (hb0 + bi) * 512:(hb0 + bi + 1) * 512], y_sb[:, :, bi * 512:(bi + 1) * 512],
                          R=[by[bi]])
        s.finish(by)
    return nc


def build_pc(final):
    nc = bass.Bass("TRN2", target_bir_lowering=False)
    din = lambda n, shp, dt: nc.dram_tensor(n, shp, dt, kind="ExternalInput").ap()
    xT = din("xT", [128, 8, NT1], F32)
    yT = din("yT", [128, 8, NT1], F32)
    mod = din("mod", [128, 3, 8], F32)
    xo = nc.dram_tensor("xo", [128, 8, NT1], F32, kind="ExternalOutput").ap()
    s = Sched(nc)
    with (nc.psum_tensor("ps", [128, 2, 512], F32) as ps,
          nc.sbuf_tensor("x_sb", [128, 8, NT1], F32) as x_sb,
          nc.sbuf_tensor("y_sb", [128, 8, NT1], F32) as y_sb,
          nc.sbuf_tensor("mod_sb", [128, 3, 8], F32) as mod_sb,
          nc.sbuf_tensor("ones_sb", [128, 128], BF16) as ones_sb,
          nc.sbuf_tensor("sq_sb", [128, 8, 512], BF16) as sq_sb,
          nc.sbuf_tensor("rstd_sb", [128, 2, 512], F32) as rstd_sb):
        bxb = [s.buf(f"x{i}", s.dsem(f"x{i}")) for i in range(len(BLKS1))]
        byb = [s.buf(f"y{i}", s.dsem(f"yy{i}")) for i in range(len(BLKS1))]
        bmod = s.buf("mod", s.dsem("mod"))
        bones = s.buf("ones"); bsq = s.buf("sq"); brs = [s.buf(f"rs{i}") for i in range(2)]
        bps = [s.buf(f"ps{i}") for i in range(2)]
        s.dma("sp", mod_sb[:], mod, W=[bmod])
        s.op("pool", lambda e: e.memset(ones_sb[:], 1.0), W=[bones])
        for bi, (t0, n) in enumerate(BLKS1):
            s.dma("sp", x_sb[:, :, t0:t0 + n], xT[:, :, t0:t0 + n], W=[bxb[bi]])
            s.dma("act", y_sb[:, :, t0:t0 + n], yT[:, :, t0:t0 + n], W=[byb[bi]])
        for bi, (t0, n) in enumerate(BLKS1):
            garow = 1 if bi == 4 else 0
            sl = bi % 2
            for k in range(8):
                s.op("dve", lambda e: e.scalar_tensor_tensor(
                    out=x_sb[:, k, t0:t0 + n], in0=y_sb[:, k, t0:t0 + n], scalar=mod_sb[:, garow, k:k + 1],
                    in1=x_sb[:, k, t0:t0 + n], op0=ALU.mult, op1=ALU.add),
                    R=[byb[bi], bmod, bxb[bi]], W=[bxb[bi]])
            if final:
                s.op("act", lambda e: e.activation(out=sq_sb[:, :, 0:n], in_=x_sb[:, :, t0:t0 + n], func=AF.Square),
                     R=[bxb[bi]], W=[bsq])
                for k in range(8):
                    mm(s, ps[:, sl, 0:n], ones_sb[:], sq_sb[:, k, 0:n], k == 0, k == 7, R=[bones, bsq], W=[bps[sl]])
                s.op("act", lambda e: e.activation(out=rstd_sb[:, sl, 0:n], in_=ps[:, sl, 0:n], func=AF.Sqrt,
                                                   scale=1.0 / D, bias=EPS), R=[bps[sl]], W=[brs[sl]])
                s.op("dve", lambda e: e.reciprocal(out=rstd_sb[:, sl, 0:n], in_=rstd_sb[:, sl, 0:n]),
                     R=[brs[sl]], W=[brs[sl]])
                for k in range(8):
                    s.op("dve", lambda e: e.scalar_tensor_tensor(
                        out=x_sb[:, k, t0:t0 + n], in0=x_sb[:, k, t0:t0 + n], scalar=mod_sb[:, 2, k:k + 1],
                        in1=rstd_sb[:, sl, 0:n], op0=ALU.mult, op1=ALU.mult),
                        R=[bxb[bi], bmod, brs[sl]], W=[bxb[bi]])
            s.dma("sp", xo[:, :, t0:t0 + n], x_sb[:, :, t0:t0 + n], R=[bxb[bi]])
        s.finish(bxb)
    return nc


def run_p3(nc3, l, xl, xc, yna, ydf, hf, hb, fmf, mods_l, inp, g_final):
    wge = np.concatenate([inp["router_w_group"][l], inp["router_w_expert"][l]], axis=1)
    wge = np.ascontiguousarray(wge.reshape(8, 128, 36).transpose(1, 0, 2))
    bge = np.concatenate([inp["router_b_group"][l], inp["router_b_expert"][l]])
    bge = np.ascontiguousarray(np.tile(bge[None, :], (128, 1))).astype(np.float32)
    selc = np.zeros((32, NEXP, 128), np.float32)
    for e in range(NEXP):
        selc[e, e, :] = 1.0
    selc = selc.reshape(32, NEXP * 128).astype(ml_dtypes.bfloat16)
    ident = np.eye(128, dtype=np.float32)
    in_maps = []
    for i in range(NCORES):
        b, j = i // 4, i % 4
        lat = slice(2048 * j, 2048 * (j + 1))
        ctxs = slice(S + 64 * j, S + 64 * (j + 1))
        xx = np.concatenate([xl[b, lat], xc[b, 64 * j:64 * (j + 1)]], axis=0)
        na = np.concatenate([yna[b, lat], yna[b, ctxs]], axis=0)
        df = np.concatenate([ydf[b, lat], ydf[b, ctxs]], axis=0)
        nadf = np.concatenate([na, df], axis=1)
        nadf = np.ascontiguousarray(nadf.T.reshape(4, 128, NT1).transpose(1, 0, 2))
        def tk(a):
            aa = np.concatenate([a[:, lat], a[:, ctxs]], axis=1)
            return aa.reshape(4, 128, NT1).transpose(1, 0, 2)
        hgt = np.ascontiguousarray(np.concatenate([tk(hf[b]), tk(hb[b]), tk(fmf[b, 512:1024])], axis=1))
        m = mods_l
        rows = [inp["g_ffn"][l], m[b, 2048:3072], m[b, 4096:5120], m[b, 3072:4096], m[b, 5120:6144],
                m[2, 2048:3072], m[2, 4096:5120], m[2, 3072:4096], m[2, 5120:6144], g_final]
        mod = np.ascontiguousarray(np.stack([vec_pk(r) for r in rows], axis=1)).astype(np.float32)
        in_maps.append({"xT": chunkT(xx), "nadf": nadf, "hg": hgt, "wout": inp["w_out"][l], "mod": mod,
                        "wge": wge, "bge": bge, "selc": selc, "ident": ident,
                        "w1": inp["moe_w1"][l], "w3": inp["moe_w3"][l], "w2": inp["moe_w2"][l]})
    res = run_bass_kernel_spmd(nc3, in_maps, core_ids=list(range(NCORES)))
    xl2 = np.zeros_like(xl); xc2 = np.zeros_like(xc)
    for i in range(NCORES):
        b, j = i // 4, i % 4
        o = res.results[i]["xo"].transpose(1, 0, 2).reshape(D, NT1).T
        xl2[b, 2048 * j:2048 * (j + 1)] = o[:2048]
        xc2[b, 64 * j:64 * (j + 1)] = o[2048:]
    return xl2, xc2


def build_p3e(caps):
    NE = 4
    captot = sum(caps)
    nc = bass.Bass("TRN2", target_bir_lowering=False)
    din = lambda n, shp, dt: nc.dram_tensor(n, shp, dt, kind="ExternalInput").ap()
    xs = din("xs", [128, 8, captot], BF16)
    w1 = din("w1", [NE, D, 512], F32)
    w3 = din("w3", [NE, D, 512], F32)
    w2 = din("w2", [NE, 512, D], F32)
    yo = nc.dram_tensor("yo", [128, 8, captot], F32, kind="ExternalOutput").ap()
    s = Sched(nc)
    with (nc.psum_tensor("ps", [128, 8, 512], F32) as ps,
          nc.sbuf_tensor("x_sb", [128, 2, 8, 512], BF16) as x_sb,
          nc.sbuf_tensor("y_sb", [128, 2, 8, 512], F32) as y_sb,
          nc.sbuf_tensor("s3_st", [128, 6, 2048], F32) as st_sb,
          nc.sbuf_tensor("s3_wb", [128, 2, 6, 2048], BF16) as wb_sb,
          nc.sbuf_tensor("s3_sg", [128, 2, 512], F32) as sg_sb,
          nc.sbuf_tensor("s3_g", [128, 2, 4, 512], BF16) as g_sb):
        bps = [s.buf(f"ps{i}") for i in range(8)]
        bx = [s.buf(f"x{i}", s.dsem(f"x{i}")) for i in range(2)]
        by = [s.buf(f"y{i}", s.dsem(f"y{i}")) for i in range(2)]
        bst = [s.buf(f"st{i}", s.dsem(f"st{i}")) for i in range(6)]
        bwb = [[s.buf(f"wb{a}_{p}") for p in range(6)] for a in range(2)]
        bsg = [s.buf(f"sg{i}") for i in range(2)]
        bg = [s.buf(f"g{i}") for i in range(2)]
        w1v = w1.rearrange("e (kc p) f -> e p kc f", p=128)
        w3v = w3.rearrange("e (kc p) f -> e p kc f", p=128)
        w2v = w2.rearrange("e (fc p) d -> e p fc d", p=128)

        def piece_src(e, p):
            if p < 2:
                return w1v[e, :, 4 * p:4 * p + 4, :]
            if p < 4:
                return w3v[e, :, 4 * (p - 2):4 * (p - 2) + 4, :]
            return w2v[e, :, 2 * (p - 4):2 * (p - 4) + 2, :]

        def piece_dma(P):
            e, p = divmod(P, 6)
            if e >= NE:
                return
            sl = P % 6
            dst = st_sb[:, sl, :]
            dst = dst.rearrange("q (a b) -> q a b", a=4) if p < 4 else dst.rearrange("q (a b) -> q a b", a=2)
            s.dma("sp", dst, piece_src(e, p), W=[bst[sl]])

        def piece_cast(P):
            e, p = divmod(P, 6)
            if e >= NE:
                return
            sl = P % 6
            s.op("pool", lambda en: en.tensor_copy(out=wb_sb[:, e % 2, p, :], in_=st_sb[:, sl, :]),
                 R=[bst[sl]], W=[bwb[e % 2][p]])

        for P in range(6):
            piece_dma(P)
        for P in range(6):
            piece_cast(P)
            piece_dma(P + 6)
        gi = 0
        seg0 = 0
        for ex in range(NE):
            a = ex % 2
            pend = [6 * (ex + 1) + i for i in range(6)]
            blocks = [(seg0 + t, min(512, caps[ex] - t)) for t in range(0, caps[ex], 512)]
            seg0 += caps[ex]
            nb = len(blocks)
            npc = (6 + nb - 1) // nb
            for bi, (t0, n) in enumerate(blocks):
                for P in pend[bi * npc:(bi + 1) * npc]:
                    piece_cast(P)
                    piece_dma(P + 6)
                xs_ = gi % 2
                gs = gi % 2
                gi += 1
                s.dma("act", x_sb[:, xs_, :, 0:n], xs[:, :, t0:t0 + n], W=[bx[xs_]])
                for fc in range(4):
                    pr = fc % 2
                    for which in range(2):
                        bank = 2 * pr + which
                        for k in range(8):
                            wv = wb_sb[:, a, 2 * which + k // 4, :].rearrange("q (a b) -> q a b", a=4)
                            mm(s, ps[:, bank, 0:n], wv[:, k % 4, fc * 128:(fc + 1) * 128], x_sb[:, xs_, k, 0:n],
                               k == 0, k == 7, R=[bwb[a][2 * which + k // 4], bx[xs_]], W=[bps[bank]])
                    s.op("act", lambda e: e.activation(out=sg_sb[:, pr, 0:n], in_=ps[:, 2 * pr, 0:n], func=AF.Silu),
                         R=[bps[2 * pr]], W=[bsg[pr]])
                    s.op("dve", lambda e: e.tensor_tensor(out=g_sb[:, gs, fc, 0:n], in0=ps[:, 2 * pr + 1, 0:n],
                                                          in1=sg_sb[:, pr, 0:n], op=ALU.mult),
                         R=[bps[2 * pr + 1], bsg[pr]], W=[bg[gs]])
                for dc in range(8):
                    bank = 4 + dc % 4
                    for fc in range(4):
                        wv = wb_sb[:, a, 4 + fc // 2, :].rearrange("q (a b) -> q a b", a=2)
                        mm(s, ps[:, bank, 0:n], wv[:, fc % 2, dc * 128:(dc + 1) * 128], g_sb[:, gs, fc, 0:n],
                           fc == 0, fc == 3, R=[bwb[a][4 + fc // 2], bg[gs]], W=[bps[bank]])
                    if dc % 2 == 0:
                        s.op("dve", lambda e: e.tensor_copy(out=y_sb[:, xs_, dc, 0:n], in_=ps[:, bank, 0:n]),
                             R=[bps[bank]], W=[by[xs_]])
                    else:
                        s.op("act", lambda e: e.copy(out=y_sb[:, xs_, dc, 0:n], in_=ps[:, bank, 0:n]),
                             R=[bps[bank]], W=[by[xs_]])
                s.dma("sp", yo[:, :, t0:t0 + n], y_sb[:, xs_, :, 0:n], R=[by[xs_]])
        s.finish(by)
    return nc


def build_pc2(final):
    nc = bass.Bass("TRN2", target_bir_lowering=False)
    din = lambda n, shp, dt: nc.dram_tensor(n, shp, dt, kind="ExternalInput").ap()
    xT = din("xT", [128, 8, NT1], F32)
    yA = din("yA", [128, 8, NT1], F32)
    yB = din("yB", [128, 8, NT1], F32)
    wab = din("wab", [128, 2, NT1], F32)
    mod = din("mod", [128, 3, 8], F32)
    xo = nc.dram_tensor("xo", [128, 8, NT1], F32, kind="ExternalOutput").ap()
    s = Sched(nc)
    with (nc.psum_tensor("ps", [128, 2, 512], F32) as ps,
          nc.sbuf_tensor("x_sb", [128, 8, NT1], F32) as x_sb,
          nc.sbuf_tensor("ya_sb", [128, 2, 8, 512], F32) as ya_sb,
          nc.sbuf_tensor("yb_sb", [128, 2, 8, 512], F32) as yb_sb,
          nc.sbuf_tensor("wab_sb", [128, 2, NT1], F32) as wab_sb,
          nc.sbuf_tensor("mod_sb", [128, 3, 8], F32) as mod_sb,
          nc.sbuf_tensor("ones_sb", [128, 128], BF16) as ones_sb,
          nc.sbuf_tensor("sq_sb", [128, 8, 512], BF16) as sq_sb,
          nc.sbuf_tensor("rstd_sb", [128, 2, 512], F32) as rstd_sb):
        bxb = [s.buf(f"x{i}", s.dsem(f"x{i}")) for i in range(len(BLKS1))]
        bya = [s.buf(f"ya{i}", s.dsem(f"ya{i}")) for i in range(2)]
        byb = [s.buf(f"yb{i}", s.dsem(f"yb{i}")) for i in range(2)]
        bmod = s.buf("mod", s.dsem("mod"))
        bwab = s.buf("wab", bmod.dsem)
        bones = s.buf("ones"); bsq = s.buf("sq"); brs = [s.buf(f"rs{i}") for i in range(2)]
        bps = [s.buf(f"ps{i}") for i in range(2)]
        s.dma("sp", mod_sb[:], mod, W=[bmod])
        s.dma("sp", wab_sb[:], wab, W=[bwab])
        s.op("pool", lambda e: e.memset(ones_sb[:], 1.0), W=[bones])
        for bi, (t0, n) in enumerate(BLKS1):
            s.dma("sp", x_sb[:, :, t0:t0 + n], xT[:, :, t0:t0 + n], W=[bxb[bi]])
        for bi, (t0, n) in enumerate(BLKS1):
            garow = 1 if bi == 4 else 0
            sl = bi % 2
            s.dma("act", ya_sb[:, sl, :, 0:n], yA[:, :, t0:t0 + n], W=[bya[sl]])
            s.dma("act", yb_sb[:, sl, :, 0:n], yB[:, :, t0:t0 + n], W=[byb[sl]])
            for k in range(8):
                s.op("pool", lambda e: e.tensor_tensor(out=ya_sb[:, sl, k, 0:n], in0=ya_sb[:, sl, k, 0:n],
                                                       in1=wab_sb[:, 0, t0:t0 + n], op=ALU.mult),
                     R=[bya[sl], bwab], W=[bya[sl]])
                s.op("dve", lambda e: e.tensor_tensor(out=yb_sb[:, sl, k, 0:n], in0=yb_sb[:, sl, k, 0:n],
                                                      in1=wab_sb[:, 1, t0:t0 + n], op=ALU.mult),
                     R=[byb[sl], bwab], W=[byb[sl]])
                s.op("dve", lambda e: e.tensor_tensor(out=ya_sb[:, sl, k, 0:n], in0=ya_sb[:, sl, k, 0:n],
                                                      in1=yb_sb[:, sl, k, 0:n], op=ALU.add),
                     R=[bya[sl], byb[sl]], W=[bya[sl]])
                s.op("dve", lambda e: e.scalar_tensor_tensor(
                    out=x_sb[:, k, t0:t0 + n], in0=ya_sb[:, sl, k, 0:n], scalar=mod_sb[:, garow, k:k + 1],
                    in1=x_sb[:, k, t0:t0 + n], op0=ALU.mult, op1=ALU.add),
                    R=[bya[sl], bmod, bxb[bi]], W=[bxb[bi]])
            if final:
                s.op("act", lambda e: e.activation(out=sq_sb[:, :, 0:n], in_=x_sb[:, :, t0:t0 + n], func=AF.Square),
                     R=[bxb[bi]], W=[bsq])
                for k in range(8):
                    mm(s, ps[:, sl, 0:n], ones_sb[:], sq_sb[:, k, 0:n], k == 0, k == 7, R=[bones, bsq], W=[bps[sl]])
                s.op("act", lambda e: e.activation(out=rstd_sb[:, sl, 0:n], in_=ps[:, sl, 0:n], func=AF.Sqrt,
                                                   scale=1.0 / D, bias=EPS), R=[bps[sl]], W=[brs[sl]])
                s.op("dve", lambda e: e.reciprocal(out=rstd_sb[:, sl, 0:n], in_=rstd_sb[:, sl, 0:n]),
                     R=[brs[sl]], W=[brs[sl]])
                for k in range(8):
                    s.op("dve", lambda e: e.scalar_tensor_tensor(
                        out=x_sb[:, k, t0:t0 + n], in0=x_sb[:, k, t0:t0 + n], scalar=mod_sb[:, 2, k:k + 1],
                        in1=rstd_sb[:, sl, 0:n], op0=ALU.mult, op1=ALU.mult),
                        R=[bxb[bi], bmod, brs[sl]], W=[bxb[bi]])
            s.dma("sp", xo[:, :, t0:t0 + n], x_sb[:, :, t0:t0 + n], R=[bxb[bi]])
        s.finish(bxb)
    return nc


def run_p3_expert(l, xl, xc, yna, ydf, hf, hb, fmf, mods_l, inp, g_final, final):
    in_maps = p3_inmaps_common(l, xl, xc, yna, ydf, hf, hb, fmf, mods_l, inp, g_final)
    resa = run_bass_kernel_spmd(build_p3a(), in_maps, core_ids=list(range(NCORES))).results
    NTT = NCORES * NT1
    HL2 = np.zeros((D, NTT), ml_dtypes.bfloat16)
    WD = np.zeros((NTT, 32), np.float32)
    for i in range(NCORES):
        HL2[:, i * NT1:(i + 1) * NT1] = resa[i]["hl2o"].transpose(1, 0, 2).reshape(D, NT1)
        WD[i * NT1:(i + 1) * NT1] = resa[i]["wd32o"].transpose(1, 0, 2).reshape(17 * 128, 32)[:NT1]
    top2 = np.sort(np.argpartition(-WD, 1, axis=1)[:, :2], axis=1)
    eA, eB = top2[:, 0], top2[:, 1]
    ar = np.arange(NTT)
    wA = WD[ar, eA]; wB = WD[ar, eB]
    tokE = []
    for e in range(32):
        tokE.append(np.nonzero((eA == e) | (eB == e))[0])
    order = np.argsort(-np.array([len(t) for t in tokE]), kind="stable")
    assign = [[int(order[8 * sl + c]) for sl in range(4)] for c in range(NCORES)]
    caps = []
    for sl in range(4):
        mx = max(len(tokE[assign[c][sl]]) for c in range(NCORES))
        caps.append(max(128, int(-(-mx // 128) * 128)))
    captot = sum(caps)
    mapse = []
    for c in range(NCORES):
        xsa = np.zeros((D, captot), ml_dtypes.bfloat16)
        o = 0
        for sl in range(4):
            tk_ = tokE[assign[c][sl]]
            xsa[:, o:o + len(tk_)] = HL2[:, tk_]
            o += caps[sl]
        mapse.append({"xs": np.ascontiguousarray(xsa.reshape(8, 128, captot).transpose(1, 0, 2)),
                      "w1": np.ascontiguousarray(inp["moe_w1"][l][assign[c]]),
                      "w3": np.ascontiguousarray(inp["moe_w3"][l][assign[c]]),
                      "w2": np.ascontiguousarray(inp["moe_w2"][l][assign[c]])})
    rese = run_bass_kernel_spmd(build_p3e(caps), mapse, core_ids=list(range(NCORES))).results
    YA = np.zeros((D, NTT), np.float32)
    YB = np.zeros((D, NTT), np.float32)
    for c in range(NCORES):
        yy = rese[c]["yo"].transpose(1, 0, 2).reshape(D, captot)
        o = 0
        for sl in range(4):
            e = assign[c][sl]
            tk_ = tokE[e]
            cols = yy[:, o:o + len(tk_)]
            isA = eA[tk_] == e
            YA[:, tk_[isA]] = cols[:, isA]
            YB[:, tk_[~isA]] = cols[:, ~isA]
            o += caps[sl]
    mapsc = []
    for i in range(NCORES):
        b = i // 4
        sl_ = slice(i * NT1, (i + 1) * NT1)
        modc = np.stack([vec_pk(mods_l[b, 5120:6144]), vec_pk(mods_l[2, 5120:6144]), vec_pk(g_final)], axis=1)
        wab = np.stack([np.tile(wA[sl_][None, :], (128, 1)), np.tile(wB[sl_][None, :], (128, 1))], axis=1)
        mapsc.append({"xT": resa[i]["xo"], "mod": np.ascontiguousarray(modc).astype(np.float32),
                      "wab": np.ascontiguousarray(wab).astype(np.float32),
                      "yA": np.ascontiguousarray(YA[:, sl_].reshape(8, 128, NT1).transpose(1, 0, 2)),
                      "yB": np.ascontiguousarray(YB[:, sl_].reshape(8, 128, NT1).transpose(1, 0, 2))})
    resc = run_bass_kernel_spmd(build_pc2(final), mapsc, core_ids=list(range(NCORES))).results
    xl2 = np.zeros_like(xl); xc2 = np.zeros_like(xc)
    for i in range(NCORES):
        b, j = i // 4, i % 4
        o = resc[i]["xo"].transpose(1, 0, 2).reshape(D, NT1).T
        xl2[b, 2048 * j:2048 * (j + 1)] = o[:2048]
        xc2[b, 64 * j:64 * (j + 1)] = o[2048:]
    return xl2, xc2


def p3_inmaps_common(l, xl, xc, yna, ydf, hf, hb, fmf, mods_l, inp, g_final):
    wge = np.concatenate([inp["router_w_group"][l], inp["router_w_expert"][l]], axis=1)
    wge = np.ascontiguousarray(wge.reshape(8, 128, 36).transpose(1, 0, 2))
    bge = np.concatenate([inp["router_b_group"][l], inp["router_b_expert"][l]])
    bge = np.ascontiguousarray(np.tile(bge[None, :], (128, 1))).astype(np.float32)
    ident = np.eye(128, dtype=np.float32)
    iota4 = np.ascontiguousarray(np.tile(np.arange(4, dtype=np.float32)[None, :], (128, 1)))
    in_maps = []
    for i in range(NCORES):
        b, j = i // 4, i % 4
        lat = slice(2048 * j, 2048 * (j + 1))
        ctxs = slice(S + 64 * j, S + 64 * (j + 1))
        xx = np.concatenate([xl[b, lat], xc[b, 64 * j:64 * (j + 1)]], axis=0)
        na = np.concatenate([yna[b, lat], yna[b, ctxs]], axis=0)
        df = np.concatenate([ydf[b, lat], ydf[b, ctxs]], axis=0)
        nadf = np.concatenate([na, df], axis=1)
        nadf = np.ascontiguousarray(nadf.T.reshape(4, 128, NT1).transpose(1, 0, 2))

        def tk(a):
            aa = np.concatenate([a[:, lat], a[:, ctxs]], axis=1)
            return aa.reshape(4, 128, NT1).transpose(1, 0, 2)
        hgt = np.ascontiguousarray(np.concatenate([tk(hf[b]), tk(hb[b]), tk(fmf[b, 512:1024])], axis=1))
        m = mods_l
        rows = [inp["g_ffn"][l], m[b, 2048:3072], m[b, 4096:5120], m[b, 3072:4096], m[b, 5120:6144],
                m[2, 2048:3072], m[2, 4096:5120], m[2, 3072:4096], m[2, 5120:6144], g_final]
        mod = np.ascontiguousarray(np.stack([vec_pk(r) for r in rows], axis=1)).astype(np.float32)
        in_maps.append({"xT": chunkT(xx), "nadf": nadf, "hg": hgt, "wout": inp["w_out"][l], "mod": mod,
                        "wge": wge, "bge": bge, "ident": ident, "iota4": iota4})
    return in_maps


def run_p3_sparse(l, xl, xc, yna, ydf, hf, hb, fmf, mods_l, inp, g_final, final):
    in_maps = p3_inmaps_common(l, xl, xc, yna, ydf, hf, hb, fmf, mods_l, inp, g_final)
    resa = run_bass_kernel_spmd(build_p3a(), in_maps, core_ids=list(range(NCORES))).results
    NTT = NCORES * NT1
    HL2 = np.zeros((D, NTT), ml_dtypes.bfloat16)
    WDT = np.zeros((32, 2, NTT), ml_dtypes.bfloat16)
    gid = np.zeros(NTT, np.int64)
    for i in range(NCORES):
        HL2[:, i * NT1:(i + 1) * NT1] = resa[i]["hl2o"].transpose(1, 0, 2).reshape(D, NT1)
        WDT[:, :, i * NT1:(i + 1) * NT1] = resa[i]["wdto"]
        g = resa[i]["gido"]
        gid[i * NT1:(i + 1) * NT1] = np.rint(g.T.reshape(-1)[:NT1]).astype(np.int64)
    toks = [np.nonzero(gid == g)[0] for g in range(4)]
    ncg = [1, 1, 1, 1]
    for _ in range(NCORES - 4):
        gbig = max(range(4), key=lambda g: len(toks[g]) / ncg[g])
        ncg[gbig] += 1
    idxs = []
    cgroup = []
    for g in range(4):
        parts = np.array_split(toks[g], ncg[g])
        for pp in parts:
            idxs.append(pp); cgroup.append(g)
    ntb = max(512, int(-(-max(len(ix) for ix in idxs) // 512) * 512))
    sel8 = np.zeros((8, 8, 128), np.float32)
    for e in range(8):
        sel8[e, e, :] = 1.0
    sel8 = sel8.reshape(8, 1024).astype(ml_dtypes.bfloat16)
    mapsb = []
    for c in range(NCORES):
        g = cgroup[c]
        ix = idxs[c]
        h2 = np.zeros((D, ntb), ml_dtypes.bfloat16)
        h2[:, :len(ix)] = HL2[:, ix]
        wd = np.zeros((8, 2, ntb), ml_dtypes.bfloat16)
        wd[:, :, :len(ix)] = WDT[8 * g:8 * g + 8][:, :, ix]
        mapsb.append({"hl2": np.ascontiguousarray(h2.reshape(8, 128, ntb).transpose(1, 0, 2)), "wdt": wd, "selc": sel8,
                      "w1": np.ascontiguousarray(inp["moe_w1"][l][8 * g:8 * g + 8]),
                      "w3": np.ascontiguousarray(inp["moe_w3"][l][8 * g:8 * g + 8]),
                      "w2": np.ascontiguousarray(inp["moe_w2"][l][8 * g:8 * g + 8])})
    resb = run_bass_kernel_spmd(build_p3b(ntb), mapsb, core_ids=list(range(NCORES))).results
    Y = np.zeros((D, NTT), np.float32)
    for c in range(NCORES):
        ix = idxs[c]
        Y[:, ix] = resb[c]["yo"].transpose(1, 0, 2).reshape(D, ntb)[:, :len(ix)]
    mapsc = []
    for i in range(NCORES):
        b = i // 4
        modc = np.stack([vec_pk(mods_l[b, 5120:6144]), vec_pk(mods_l[2, 5120:6144]), vec_pk(g_final)], axis=1)
        mapsc.append({"xT": resa[i]["xo"], "mod": np.ascontiguousarray(modc).astype(np.float32),
                      "yT": np.ascontiguousarray(Y[:, i * NT1:(i + 1) * NT1].reshape(8, 128, NT1).transpose(1, 0, 2))})
    resc = run_bass_kernel_spmd(build_pc(final), mapsc, core_ids=list(range(NCORES))).results
    xl2 = np.zeros_like(xl); xc2 = np.zeros_like(xc)
    for i in range(NCORES):
        b, j = i // 4, i % 4
        o = resc[i]["xo"].transpose(1, 0, 2).reshape(D, NT1).T
        xl2[b, 2048 * j:2048 * (j + 1)] = o[:2048]
        xc2[b, 64 * j:64 * (j + 1)] = o[2048:]
    return xl2, xc2


def kernel(**inputs):
    inp = {k: np.asarray(v) for k, v in inputs.items()}
    x = np.ascontiguousarray(inp["x"], dtype=np.float32)
    ctx = np.ascontiguousarray(inp["ctx"], dtype=np.float32)
    mods = run_p0(inp["c"], inp["c_ctx"], inp["w_ada"], inp["b_ada"])
    cosT, sinT = rope_tables()
    xl, xc = x, ctx
    for l in range(DEPTH):
        fmb, fmf, tm = run_p1(build_p1(), xl, xc, mods[l], inp["g_mix"][l], inp["w_in"][l], cosT, sinT)
        lam_init = 0.8 - 0.6 * math.exp(-0.3 * l)
        yna, ydf, hf, hb = run_p2(build_p2(lam_init), l, fmb, fmf, tm, inp)
        xl, xc = run_p3_expert(l, xl, xc, yna, ydf, hf, hb, fmf, mods[l], inp, inp["g_final"], l == DEPTH - 1)
    return np.ascontiguousarray(xl, dtype=np.float32)
```
